# Optimizing a Trainium2 kernel written in Bass

```python
import jax, jax.numpy as jnp
from jax import lax
import numpy as np

D_MODEL = 1024
BATCH = 32
SEQ = 2048
DEPTH = 2

ROPE_THETA = 10000.0
NORM_EPS = 1e-6
ATTN_BLOCK = 128
NEG_INF = -1e30

MLA_HEADS = 8
MLA_NOPE_DIM = 64
MLA_ROPE_DIM = 32
MLA_QK_DIM = MLA_NOPE_DIM + MLA_ROPE_DIM
MLA_V_DIM = 64
MLA_KV_RANK = 256
MLA_Q_RANK = 384

RET_HEADS = 8
RET_QK_DIM = 64
RET_V_DIM = 64
RET_CHUNK = 128

SWA_HEADS = 16
SWA_KV_HEADS = 2
SWA_HEAD_DIM = 64
SWA_WINDOW = 128

N_EXPERTS = 32
TOP_K = 4
D_EXPERT = 1024
SWIGLU_LIMIT = 7.0
SWIGLU_ALPHA = 1.702
MOE_BLOCK = 512

N_EVEN = (DEPTH + 1) // 2
N_ODD = DEPTH // 2

HYB_SPLITS = [MLA_Q_RANK, MLA_KV_RANK, MLA_ROPE_DIM,
              RET_HEADS * RET_QK_DIM, RET_HEADS * RET_QK_DIM,
              RET_HEADS * RET_V_DIM, RET_HEADS * RET_V_DIM]
HYB_IN = sum(HYB_SPLITS)
HYB_MIX = MLA_HEADS * MLA_V_DIM + RET_HEADS * RET_V_DIM
SWA_QKV = (SWA_HEADS + 2 * SWA_KV_HEADS) * SWA_HEAD_DIM

kernel_name = "hybrid_mla_retention_swa_moe_adaln"


def rms_norm(x, g):
    xf = x.astype(jnp.float32)
    y = xf * lax.rsqrt(jnp.mean(xf * xf, axis=-1, keepdims=True) + NORM_EPS)
    return (y * g).astype(x.dtype)


def rope(x, positions):
    half = x.shape[-1] // 2
    inv_freq = ROPE_THETA ** (-jnp.arange(half, dtype=jnp.float32) / half)
    ang = positions.astype(jnp.float32)[:, :, None] * inv_freq
    cos = jnp.cos(ang)[:, :, None, :]
    sin = jnp.sin(ang)[:, :, None, :]
    xf = x.astype(jnp.float32)
    x1, x2 = xf[..., :half], xf[..., half:]
    return jnp.concatenate([x1 * cos - x2 * sin, x2 * cos + x1 * sin], axis=-1).astype(x.dtype)


def causal_block_attention(q, k, v):
    B, S, H, dq = q.shape
    nb = S // ATTN_BLOCK
    scale = dq ** -0.5
    qb = q.reshape(B, nb, ATTN_BLOCK, H, dq).transpose(1, 0, 2, 3, 4)
    k_idx = jnp.arange(S)

    def one_block(args):
        q_blk, n = args
        s = jnp.einsum('bqhd,bkhd->bhqk', q_blk, k).astype(jnp.float32) * scale
        q_idx = n * ATTN_BLOCK + jnp.arange(ATTN_BLOCK)
        s = jnp.where(k_idx[None, :] <= q_idx[:, None], s, NEG_INF)
        p = jax.nn.softmax(s, axis=-1).astype(v.dtype)
        return jnp.einsum('bhqk,bkhd->bqhd', p, v)

    out = lax.map(one_block, (qb, jnp.arange(nb)))
    return out.transpose(1, 0, 2, 3, 4).reshape(B, S, H, v.shape[-1])


def retention_chunkwise(q, k, v, log_gamma):
    B, S, H, dk = q.shape
    dv = v.shape[-1]
    C = RET_CHUNK
    nc = S // C
    q = q.reshape(B, nc, C, H, dk)
    k = k.reshape(B, nc, C, H, dk)
    v = v.reshape(B, nc, C, H, dv)
    idx = jnp.arange(C, dtype=jnp.float32)
    diff = idx[:, None] - idx[None, :]
    decay_intra = jnp.where(diff >= 0, jnp.exp(log_gamma[:, None, None] * jnp.maximum(diff, 0.0)), 0.0)
    scores = jnp.einsum('bnqhd,bnkhd->bnhqk', q, k) * decay_intra
    inner = jnp.einsum('bnhqk,bnkhe->bnqhe', scores, v)
    k_decay = jnp.exp(log_gamma[None, :] * (C - 1 - idx)[:, None])
    kv = jnp.einsum('bnkhd,bnkhe->bnhde', k * k_decay[:, :, None], v)
    chunk_decay = jnp.exp(log_gamma * C).astype(kv.dtype)[:, None, None]

    def step(state, kv_n):
        return state * chunk_decay + kv_n, state

    _, state_before = lax.scan(step, jnp.zeros((B, H, dk, dv), kv.dtype), kv.transpose(1, 0, 2, 3, 4))
    q_decay = jnp.exp(log_gamma[None, :] * (idx + 1.0)[:, None])
    cross = jnp.einsum('bnqhd,nbhde->bnqhe', q * q_decay[:, :, None], state_before)
    return (inner + cross).reshape(B, S, H, dv)


def head_group_norm(y, g):
    yf = y.astype(jnp.float32)
    mu = jnp.mean(yf, axis=-1, keepdims=True)
    var = jnp.mean((yf - mu) ** 2, axis=-1, keepdims=True)
    return (yf - mu) * lax.rsqrt(var + NORM_EPS) * g


def mla_retention_mixer(h, positions, w_in, cq_norm_g, ckv_norm_g, w_uq, w_ukv,
                        q_head_g, k_head_g, ret_norm_g, w_out):
    B, S, _ = h.shape
    proj = h @ w_in
    cuts = list(np.cumsum(HYB_SPLITS)[:-1])
    cq, ckv, k_rope_in, rq, rk, rv, rg = jnp.split(proj, cuts, axis=-1)
    q = (rms_norm(cq, cq_norm_g) @ w_uq).reshape(B, S, MLA_HEADS, MLA_QK_DIM)
    q_nope = rms_norm(q[..., :MLA_NOPE_DIM], q_head_g[:MLA_NOPE_DIM])
    q_rope = rope(rms_norm(q[..., MLA_NOPE_DIM:], q_head_g[MLA_NOPE_DIM:]), positions)
    kv = (rms_norm(ckv, ckv_norm_g) @ w_ukv).reshape(B, S, MLA_HEADS, MLA_NOPE_DIM + MLA_V_DIM)
    k_nope = rms_norm(kv[..., :MLA_NOPE_DIM], k_head_g[:MLA_NOPE_DIM])
    v = kv[..., MLA_NOPE_DIM:]
    k_rope = rope(rms_norm(k_rope_in, k_head_g[MLA_NOPE_DIM:])[:, :, None, :], positions)
    q_full = jnp.concatenate([q_nope, q_rope], axis=-1)
    k_full = jnp.concatenate([k_nope, jnp.broadcast_to(k_rope, (B, S, MLA_HEADS, MLA_ROPE_DIM))], axis=-1)
    attn = causal_block_attention(q_full, k_full, v).reshape(B, S, MLA_HEADS * MLA_V_DIM)
    log_gamma = jnp.log1p(-jnp.exp2(-5.0 - jnp.arange(RET_HEADS, dtype=jnp.float32)))
    rq = rope(rq.reshape(B, S, RET_HEADS, RET_QK_DIM), positions)
    rk = rope(rk.reshape(B, S, RET_HEADS, RET_QK_DIM), positions) * (RET_QK_DIM ** -0.5)
    rv = rv.reshape(B, S, RET_HEADS, RET_V_DIM)
    y = retention_chunkwise(rq, rk, rv, log_gamma)
    y = head_group_norm(y, ret_norm_g).reshape(B, S, RET_HEADS * RET_V_DIM)
    y = (y * jax.nn.silu(rg.astype(jnp.float32))).astype(h.dtype)
    return jnp.concatenate([attn, y], axis=-1) @ w_out


def sliding_window_attention(q, k, v, sinks):
    B, S, Hq, d = q.shape
    Hkv = k.shape[2]
    R = Hq // Hkv
    nb = S // SWA_WINDOW
    scale = d ** -0.5

    def band(t):
        tb = t.reshape(B, nb, SWA_WINDOW, Hkv, t.shape[-1])
        prev = jnp.concatenate([jnp.zeros_like(tb[:, :1]), tb[:, :-1]], axis=1)
        return jnp.concatenate([prev, tb], axis=2).transpose(1, 0, 2, 3, 4)

    kb, vb = band(k), band(v)
    qb = q.reshape(B, nb, SWA_WINDOW, Hkv, R, d).transpose(1, 0, 2, 3, 4, 5)
    i = jnp.arange(SWA_WINDOW)[:, None]
    j = jnp.arange(2 * SWA_WINDOW)[None, :]
    rel = i + SWA_WINDOW - j
    local_mask = (rel >= 0) & (rel < SWA_WINDOW)
    sink = sinks.astype(jnp.float32).reshape(Hkv, R)

    def one_block(args):
        q_blk, k_blk, v_blk, n = args
        s = jnp.einsum('bqgrd,bkgd->bgrqk', q_blk, k_blk).astype(jnp.float32) * scale
        mask = local_mask & ((n - 1) * SWA_WINDOW + j >= 0)
        s = jnp.where(mask, s, NEG_INF)
        sink_col = jnp.broadcast_to(sink[None, :, :, None, None], s.shape[:-1] + (1,))
        p = jax.nn.softmax(jnp.concatenate([s, sink_col], axis=-1), axis=-1)[..., :-1].astype(v_blk.dtype)
        return jnp.einsum('bgrqk,bkgd->bqgrd', p, v_blk)

    out = lax.map(one_block, (qb, kb, vb, jnp.arange(nb)))
    return out.transpose(1, 0, 2, 3, 4, 5).reshape(B, S, Hq, d)


def swa_mixer(h, positions, w_qkv, b_qkv, q_head_g, k_head_g, sinks, w_out, b_out):
    B, S, _ = h.shape
    qkv = h @ w_qkv + b_qkv
    nq = SWA_HEADS * SWA_HEAD_DIM
    nk = SWA_KV_HEADS * SWA_HEAD_DIM
    q, k, v = jnp.split(qkv, [nq, nq + nk], axis=-1)
    q = rope(rms_norm(q.reshape(B, S, SWA_HEADS, SWA_HEAD_DIM), q_head_g), positions)
    k = rope(rms_norm(k.reshape(B, S, SWA_KV_HEADS, SWA_HEAD_DIM), k_head_g), positions)
    v = v.reshape(B, S, SWA_KV_HEADS, SWA_HEAD_DIM)
    o = sliding_window_attention(q, k, v, sinks).reshape(B, S, nq)
    return o @ w_out + b_out


def moe_ffn(h, router_w, router_b, w_gu, b_gu, w_down, b_down):
    B, S, D = h.shape
    xf = h.reshape(-1, D)
    N = xf.shape[0]
    logits = (xf @ router_w + router_b).astype(jnp.float32)
    top_v, top_i = lax.top_k(logits, TOP_K)
    gates = jax.nn.softmax(top_v, axis=-1)
    M = N * TOP_K
    flat_e = top_i.reshape(-1)
    flat_tok = jnp.arange(M, dtype=jnp.int32) // TOP_K
    flat_g = gates.reshape(-1)
    order = jnp.argsort(flat_e)
    sorted_e = flat_e[order]
    counts = jnp.bincount(flat_e, length=N_EXPERTS)
    padded = ((counts + MOE_BLOCK - 1) // MOE_BLOCK) * MOE_BLOCK
    start = jnp.cumsum(counts) - counts
    pad_end = jnp.cumsum(padded)
    pad_start = pad_end - padded
    dest = pad_start[sorted_e] + (jnp.arange(M) - start[sorted_e])
    n_blocks = -(-M // MOE_BLOCK) + N_EXPERTS
    P = n_blocks * MOE_BLOCK
    slot_tok = jnp.full((P,), N, jnp.int32).at[dest].set(flat_tok[order])
    slot_gate = jnp.zeros((P,), jnp.float32).at[dest].set(flat_g[order])
    block_e = jnp.minimum(jnp.searchsorted(pad_end, jnp.arange(n_blocks) * MOE_BLOCK, side='right'),
                          N_EXPERTS - 1)
    x_pad = jnp.concatenate([xf, jnp.zeros((1, D), xf.dtype)], axis=0)

    def body(y, blk):
        tok, g, e = blk
        gu = x_pad[tok] @ w_gu[e] + b_gu[e]
        glu, lin = gu[:, :D_EXPERT], gu[:, D_EXPERT:]
        glu = jnp.minimum(glu, SWIGLU_LIMIT)
        lin = jnp.clip(lin, -SWIGLU_LIMIT, SWIGLU_LIMIT)
        act = glu * jax.nn.sigmoid(SWIGLU_ALPHA * glu) * (lin + 1.0)
        out = (act @ w_down[e] + b_down[e]) * g[:, None]
        return y.at[tok].add(out.astype(y.dtype)), None

    y, _ = lax.scan(body, jnp.zeros((N + 1, D), h.dtype),
                    (slot_tok.reshape(n_blocks, MOE_BLOCK), slot_gate.reshape(n_blocks, MOE_BLOCK), block_e))
    return y[:N].reshape(B, S, D)


def setup_inputs(seed: int = 0) -> dict:
    key = jax.random.key(seed)
    ks = jax.random.split(key, 32)
    f32 = jnp.float32

    def nrm(k, shape, scale):
        return jax.random.normal(k, shape, f32) * scale

    def gain(k, shape):
        return 1.0 + 0.02 * jax.random.normal(k, shape, f32)

    D = D_MODEL
    offsets = jax.random.randint(ks[2], (BATCH, 1), 0, SEQ, dtype=jnp.int32)
    positions = offsets + jnp.arange(SEQ, dtype=jnp.int32)[None, :]
    return {
        "x": nrm(ks[0], (BATCH, SEQ, D), 1.0),
        "c": nrm(ks[1], (BATCH, D), 1.0),
        "positions": positions,
        "ada_w": nrm(ks[3], (DEPTH, D, 6 * D), 0.5 * D ** -0.5),
        "ada_b": nrm(ks[4], (DEPTH, 6 * D), 0.02),
        "norm1_g": gain(ks[5], (DEPTH, D)),
        "norm2_g": gain(ks[6], (DEPTH, D)),
        "hyb_w_in": nrm(ks[7], (N_EVEN, D, HYB_IN), D ** -0.5),
        "mla_cq_norm_g": gain(ks[8], (N_EVEN, MLA_Q_RANK)),
        "mla_ckv_norm_g": gain(ks[9], (N_EVEN, MLA_KV_RANK)),
        "mla_w_uq": nrm(ks[10], (N_EVEN, MLA_Q_RANK, MLA_HEADS * MLA_QK_DIM), MLA_Q_RANK ** -0.5),
        "mla_w_ukv": nrm(ks[11], (N_EVEN, MLA_KV_RANK, MLA_HEADS * (MLA_NOPE_DIM + MLA_V_DIM)), MLA_KV_RANK ** -0.5),
        "mla_q_head_g": gain(ks[12], (N_EVEN, MLA_QK_DIM)),
        "mla_k_head_g": gain(ks[13], (N_EVEN, MLA_QK_DIM)),
        "ret_norm_g": gain(ks[14], (N_EVEN, RET_HEADS, RET_V_DIM)),
        "hyb_w_out": nrm(ks[15], (N_EVEN, HYB_MIX, D), HYB_MIX ** -0.5),
        "swa_w_qkv": nrm(ks[16], (N_ODD, D, SWA_QKV), D ** -0.5),
        "swa_b_qkv": nrm(ks[17], (N_ODD, SWA_QKV), 0.02),
        "swa_q_head_g": gain(ks[18], (N_ODD, SWA_HEAD_DIM)),
        "swa_k_head_g": gain(ks[19], (N_ODD, SWA_HEAD_DIM)),
        "swa_sinks": nrm(ks[20], (N_ODD, SWA_HEADS), 0.5),
        "swa_w_out": nrm(ks[21], (N_ODD, SWA_HEADS * SWA_HEAD_DIM, D), (SWA_HEADS * SWA_HEAD_DIM) ** -0.5),
        "swa_b_out": nrm(ks[22], (N_ODD, D), 0.02),
        "router_w": nrm(ks[23], (DEPTH, D, N_EXPERTS), D ** -0.5),
        "router_b": nrm(ks[24], (DEPTH, N_EXPERTS), 0.01),
        "exp_w_gu": nrm(ks[25], (DEPTH, N_EXPERTS, D, 2 * D_EXPERT), D ** -0.5),
        "exp_b_gu": nrm(ks[26], (DEPTH, N_EXPERTS, 2 * D_EXPERT), 0.02),
        "exp_w_down": nrm(ks[27], (DEPTH, N_EXPERTS, D_EXPERT, D), D_EXPERT ** -0.5),
        "exp_b_down": nrm(ks[28], (DEPTH, N_EXPERTS, D), 0.02),
    }


def reference(x, c, positions, ada_w, ada_b, norm1_g, norm2_g, hyb_w_in, mla_cq_norm_g,
              mla_ckv_norm_g, mla_w_uq, mla_w_ukv, mla_q_head_g, mla_k_head_g, ret_norm_g,
              hyb_w_out, swa_w_qkv, swa_b_qkv, swa_q_head_g, swa_k_head_g, swa_sinks,
              swa_w_out, swa_b_out, router_w, router_b, exp_w_gu, exp_b_gu, exp_w_down,
              exp_b_down):
    c_act = jax.nn.silu(c)
    for layer in range(DEPTH):
        mod = (c_act @ ada_w[layer] + ada_b[layer])[:, None, :]
        shift1, scale1, gate1, shift2, scale2, gate2 = jnp.split(mod, 6, axis=-1)
        j = layer // 2
        h = rms_norm(x, norm1_g[layer]) * (1.0 + scale1) + shift1
        if layer % 2 == 0:
            mix = mla_retention_mixer(h, positions, hyb_w_in[j], mla_cq_norm_g[j], mla_ckv_norm_g[j],
                                      mla_w_uq[j], mla_w_ukv[j], mla_q_head_g[j], mla_k_head_g[j],
                                      ret_norm_g[j], hyb_w_out[j])
        else:
            mix = swa_mixer(h, positions, swa_w_qkv[j], swa_b_qkv[j], swa_q_head_g[j],
                            swa_k_head_g[j], swa_sinks[j], swa_w_out[j], swa_b_out[j])
        x = x + gate1 * mix
        h = rms_norm(x, norm2_g[layer]) * (1.0 + scale2) + shift2
        x = x + gate2 * moe_ffn(h, router_w[layer], router_b[layer], exp_w_gu[layer], exp_b_gu[layer],
                                exp_w_down[layer], exp_b_down[layer])
    return x
```

```python
import contextlib
import os
import math
import numpy as np
import concourse.bass as bass
import concourse.mybir as mybir
from concourse.bass_utils import run_bass_kernel_spmd

F32 = mybir.dt.float32
BF16 = mybir.dt.bfloat16
I32 = mybir.dt.int32
AF = mybir.ActivationFunctionType
ALU = mybir.AluOpType
AX = mybir.AxisListType

SAME_ENGINE_SYNC = True
DMA_RING = 6
N_CORES = 8
_STOP = float(os.environ.get('KSTOP', '99'))
SEQ = 2048
D = 1024
NT = SEQ // 128
EPS = 1e-6
PI = math.pi


class Buf:
    __slots__ = ("w", "rs")

    def __init__(self):
        self.w = None
        self.rs = {}


class KB:
    ENGS = ("pe", "act", "dve", "pool", "sp")

    def __init__(self, nc):
        self.nc = nc
        self.stacks = [contextlib.ExitStack()]
        self.eng = dict(pe=nc.tensor, act=nc.scalar, dve=nc.vector, pool=nc.gpsimd, sp=nc.sync)
        self.cnt = {e: 0 for e in self.ENGS}
        self.seen = {e: {} for e in self.ENGS}
        self.sems = {}
        for e in self.ENGS:
            self.sems[e] = self.stacks[0].enter_context(nc.semaphore("s_" + e))
        self.dma_n = {}
        for e in ("sp", "pool", "act"):
            self.dma_n[e] = 0
            for j in range(DMA_RING):
                self.sems[("d", e, j)] = self.stacks[0].enter_context(nc.semaphore("d_%s_%d" % (e, j)))
        self.uid = 0

    def push(self):
        self.stacks.append(contextlib.ExitStack())

    def pop(self):
        self.barrier()
        self.stacks.pop().close()

    def sb(self, name, shape, dt):
        self.uid += 1
        return self.stacks[-1].enter_context(self.nc.sbuf_tensor("%s_%d" % (name, self.uid), list(shape), dt))

    def ps(self, name, shape, dt):
        self.uid += 1
        return self.stacks[-1].enter_context(self.nc.psum_tensor("%s_%d" % (name, self.uid), list(shape), dt))

    def _deps(self, e, reads, writes):
        toks = {}
        for b in reads:
            if b.w is not None and toks.get(b.w[0], 0) < b.w[1]:
                toks[b.w[0]] = b.w[1]
        for b in writes:
            if b.w is not None and toks.get(b.w[0], 0) < b.w[1]:
                toks[b.w[0]] = b.w[1]
            for k, v in b.rs.items():
                if toks.get(k, 0) < v:
                    toks[k] = v
        waits = []
        seen = self.seen[e]
        for k, v in toks.items():
            if k == e and (e == "pe" or not SAME_ENGINE_SYNC):
                continue
            if seen.get(k, 0) < v:
                seen[k] = v
                waits.append((k, v))
        return waits

    def _mark(self, tok, reads, writes):
        k, v = tok
        for b in reads:
            if b.rs.get(k, 0) < v:
                b.rs[k] = v
        for b in writes:
            b.w = tok
            b.rs = {}

    def _emit(self, e, waits, fn, key, inc):
        engine = self.eng[e]
        for k, v in waits:
            engine.wait_ge(self.sems[k], v)
        if fn is not None:
            fn(engine).then_inc(self.sems[key], inc)

    def op(self, e, fn, reads=(), writes=()):
        waits = self._deps(e, reads, writes)
        self.cnt[e] += 1
        tok = (e, self.cnt[e])
        self._emit(e, waits, fn, e, 1)
        self._mark(tok, reads, writes)
        return tok

    def dma(self, e, out, in_, reads=(), writes=(), **kw):
        waits = self._deps(e, reads, writes)
        n = self.dma_n[e]
        self.dma_n[e] += 1
        key = ("d", e, n % DMA_RING)
        val = 16 * (n // DMA_RING + 1)
        if n >= DMA_RING and self.seen[e].get(key, 0) < val - 16:
            self.seen[e][key] = val - 16
            waits.append((key, val - 16))
        tok = (key, val)
        self._emit(e, waits, (lambda eng: eng.dma_start(out=out, in_=in_, **kw)), key, 16)
        self._mark(tok, reads, writes)
        return tok

    def idma(self, out, out_idx, in_, in_idx, bound, reads=(), writes=()):
        e = "pool"
        waits = self._deps(e, reads, writes)
        n = self.dma_n[e]
        self.dma_n[e] += 1
        key = ("d", e, n % DMA_RING)
        val = 16 * (n // DMA_RING + 1)
        if n >= DMA_RING and self.seen[e].get(key, 0) < val - 16:
            self.seen[e][key] = val - 16
            waits.append((key, val - 16))
        if not hasattr(self, "_bregs"):
            self._bregs = {}
        if bound not in self._bregs:
            self._bregs[bound] = self.nc.gpsimd.to_reg(bound)
        bound = self._bregs[bound]
        oo_ = bass.IndirectOffsetOnAxis(ap=out_idx, axis=0) if out_idx is not None else None
        io_ = bass.IndirectOffsetOnAxis(ap=in_idx, axis=0) if in_idx is not None else None
        self._emit(e, waits, (lambda eng: eng.indirect_dma_start(out=out, out_offset=oo_, in_=in_, in_offset=io_,
                                                                 bounds_check=bound, oob_is_err=False)), key, 16)
        self._mark((key, val), reads, writes)

    def all_tokens(self):
        toks = [(e, self.cnt[e]) for e in self.ENGS if self.cnt[e] > 0]
        for e in ("sp", "pool", "act"):
            n = self.dma_n[e]
            for j in range(DMA_RING):
                c = (n - j + DMA_RING - 1) // DMA_RING if n > j else 0
                if c > 0:
                    toks.append((("d", e, j), 16 * c))
        return toks

    def barrier(self):
        toks = self.all_tokens()
        for e in self.ENGS:
            waits = []
            for k, v in toks:
                if k == e:
                    continue
                if self.seen[e].get(k, 0) < v:
                    self.seen[e][k] = v
                    waits.append((k, v))
            self._emit(e, waits, None, None, 0)

    def finish(self):
        self.barrier()
        while self.stacks:
            self.stacks.pop().close()


def bc_mid(ap2, n):
    return ap2.unsqueeze(1).broadcast_to([ap2.shape[0], n, ap2.shape[1]])


def bc_last(ap2, n):
    return ap2.unsqueeze(2).broadcast_to([ap2.shape[0], ap2.shape[1], n])


class Prog:
    def __init__(self, nseq, debug=False, phases=("p0", "p1a", "p1b", "p2a", "p3", "p2b"), ntl=NT, cap_tiles=None):
        self.nseq = nseq
        self.ntl = ntl
        if cap_tiles is None:
            mean = nseq * ntl * 128 * 4 // 32
            cap_tiles = max(4, 4 * ((2 * mean + 511) // 512))
        self.cap = cap_tiles * 128
        self.debug = debug
        self.phases = phases
        nc = self.nc = bass.Bass("TRN2", target_bir_lowering=False)
        self.kb = KB(nc)
        ntok = nseq * SEQ
        self.ntok = ntok

        def inp(name, shape, dt=F32):
            return nc.dram_tensor(name, list(shape), dt, kind="ExternalInput").ap()

        def scr(name, shape, dt=F32):
            kind = "ExternalOutput" if debug else "Internal"
            return nc.dram_tensor(name, list(shape), dt, kind=kind).ap()

        I = self.I = {}
        I["x"] = inp("x", [ntok, D])
        I["c_pk"] = inp("c_pk", [nseq, 128, 8])
        I["pos_pt"] = inp("pos_pt", [nseq, 128, NT], I32)
        I["ada_w"] = inp("ada_w", [2, D, 6 * D])
        I["ada_b"] = inp("ada_b", [2, 6 * D])
        I["norm1_g"] = inp("norm1_g", [2, D])
        I["norm2_g"] = inp("norm2_g", [2, D])
        I["hyb_w_in"] = inp("hyb_w_in", [D, 2720])
        I["mla_cq_norm_g"] = inp("mla_cq_norm_g", [384])
        I["mla_ckv_norm_g"] = inp("mla_ckv_norm_g", [256])
        I["mla_w_uq"] = inp("mla_w_uq", [384, 768])
        I["mla_w_ukv"] = inp("mla_w_ukv", [256, 1024])
        I["mla_q_head_g"] = inp("mla_q_head_g", [96])
        I["mla_k_head_g"] = inp("mla_k_head_g", [96])
        I["ret_norm_g"] = inp("ret_norm_g", [512])
        I["hyb_w_out"] = inp("hyb_w_out", [D, D])
        I["swa_w_qkv"] = inp("swa_w_qkv", [D, 1280])
        I["swa_b_qkv"] = inp("swa_b_qkv", [1280])
        I["swa_q_head_g"] = inp("swa_q_head_g", [64])
        I["swa_k_head_g"] = inp("swa_k_head_g", [64])
        I["swa_sinks"] = inp("swa_sinks", [16])
        I["swa_w_out"] = inp("swa_w_out", [D, D])
        I["swa_b_out"] = inp("swa_b_out", [D])
        I["router_w"] = inp("router_w", [2, D, 32])
        I["router_b"] = inp("router_b", [2, 32])
        I["exp_w_gu"] = inp("exp_w_gu", [2, 32, D, 2048])
        I["exp_b_gu_pj"] = inp("exp_b_gu_pj", [2, 32, 128, 16])
        I["exp_w_down"] = inp("exp_w_down", [2, 32, D, D])
        I["exp_b_down"] = inp("exp_b_down", [2, 32, D])
        I["k_invf16"] = inp("k_invf16", [16])
        I["k_invf32"] = inp("k_invf32", [32])
        I["k_decayT"] = inp("k_decayT", [128, 8 * 128])
        I["k_qdec"] = inp("k_qdec", [128, 8])
        I["k_kdec"] = inp("k_kdec", [128, 8])
        I["k_cdec"] = inp("k_cdec", [128, 4])

        S = self.S = {}
        S["MOD"] = scr("MOD", [nseq, 2, 6, 128, D])
        S["ATT"] = scr("ATT", [nseq * NT, 128, 512], BF16)
        S["XA"] = scr("XA", [ntok, D])
        S["XB"] = scr("XB", [ntok, D])
        S["H2T"] = scr("H2T", [nseq * NT, 128, 8, 128], BF16)
        S["GS"] = scr("GS", [nseq * NT, 128, 32])
        S["XG"] = scr("XG", [32 * self.cap, D], BF16)
        S["YG"] = scr("YG", [32 * self.cap, D])
        S["SLOT"] = scr("SLOT", [nseq * NT, 128, 4], I32)
        S["GK"] = scr("GK", [nseq * NT, 128, 4])
        self.out = nc.dram_tensor("out", [ntok, D], F32, kind="ExternalOutput").ap()
        self.dbufs = {}

    def db(self, name, idx):
        k = (name, idx)
        if k not in self.dbufs:
            self.dbufs[k] = Buf()
        return self.dbufs[k]

    def setup_consts(self):
        kb = self.kb
        self.ident_b = kb.sb("ident_b", [128, 128], BF16)
        self.ident_f = kb.sb("ident_f", [128, 128], F32)
        self.b_const = Buf()
        bc = self.b_const
        for idt in (self.ident_b, self.ident_f):
            kb.op("pool", lambda e, idt=idt: e.memset(idt[:], 1.0), writes=[bc])
            kb.op("pool", lambda e, idt=idt: e.affine_select(out=idt[:], in_=idt[:], pattern=[[-1, 128]],
                                                             compare_op=ALU.is_equal, fill=0.0, base=0,
                                                             channel_multiplier=1), reads=[bc], writes=[bc])
        self.mask_le = kb.sb("mask_le", [128, 128], BF16)
        self.mask_gt = kb.sb("mask_gt", [128, 128], BF16)
        kb.op("pool", lambda e: e.memset(self.mask_le[:], 1.0), writes=[bc])
        kb.op("pool", lambda e: e.affine_select(out=self.mask_le[:], in_=self.mask_le[:], pattern=[[1, 128]],
                                                compare_op=ALU.is_ge, fill=0.0, base=0, channel_multiplier=-1),
              reads=[bc], writes=[bc])
        kb.op("pool", lambda e: e.memset(self.mask_gt[:], 1.0), writes=[bc])
        kb.op("pool", lambda e: e.affine_select(out=self.mask_gt[:], in_=self.mask_gt[:], pattern=[[-1, 128]],
                                                compare_op=ALU.is_gt, fill=0.0, base=0, channel_multiplier=1),
              reads=[bc], writes=[bc])
        self.U_b = kb.sb("U_b", [128, 128], BF16)
        self.ones_b = kb.sb("ones_b", [128, 128], BF16)
        kb.op("pool", lambda e: e.memset(self.ones_b[:], 1.0), writes=[bc])
        kb.op("pool", lambda e: e.memset(self.U_b[:], 1.0), writes=[bc])
        kb.op("pool", lambda e: e.affine_select(out=self.U_b[:], in_=self.U_b[:], pattern=[[1, 128]],
                                                compare_op=ALU.is_ge, fill=0.0, base=-1, channel_multiplier=-1),
              reads=[bc], writes=[bc])
        iot_i = kb.sb("iot_i", [128, 32], I32)
        self.iotaE = kb.sb("iotaE", [128, 32], F32)
        kb.op("pool", lambda e: e.iota(out=iot_i[:], pattern=[[1, 32]], base=0, channel_multiplier=0), writes=[bc])
        kb.op("dve", lambda e: e.tensor_copy(out=self.iotaE[:], in_=iot_i[:]), reads=[bc], writes=[bc])
        kb.op("dve", lambda e: e.tensor_scalar(out=self.iotaE[:], in0=self.iotaE[:], scalar1=float(self.cap), scalar2=None,
                                               op0=ALU.mult), reads=[bc], writes=[bc])
        self.neghalf = kb.sb("neghalf", [128, 16], F32)
        kb.op("pool", lambda e: e.memset(self.neghalf[:], -0.5), writes=[bc])

    def rstd_of(self, ss, n, width, bss, tag):
        kb = self.kb
        kb.op("dve", lambda e: e.tensor_scalar(out=ss, in0=ss, scalar1=1.0 / n, scalar2=EPS,
                                               op0=ALU.mult, op1=ALU.add), reads=[bss], writes=[bss])
        kb.op("pool", lambda e: e.tensor_tensor(out=ss, in0=ss, in1=self.neghalf[:, 0:width], op=ALU.pow),
              reads=[bss, self.b_const], writes=[bss])

    def rope_tables(self, s, half, invf_name, tag):
        kb, I = self.kb, self.I
        b = Buf()
        pos_i = kb.sb("pos_i" + tag, [128, NT], I32)
        pos_f = kb.sb("pos_f" + tag, [128, NT], F32)
        invf = kb.sb("invf" + tag, [128, half], F32)
        ang = kb.sb("ang" + tag, [128, NT, half], F32)
        kq = kb.sb("kq" + tag, [128, NT, half], F32)
        ki = kb.sb("ki" + tag, [128, NT, half], I32)
        ys = kb.sb("ys" + tag, [128, NT, half], F32)
        mm = kb.sb("mmk" + tag, [128, NT, half], F32)
        cos = kb.sb("cos" + tag, [128, NT, half], F32)
        sin = kb.sb("sin" + tag, [128, NT, half], F32)
        kb.dma("sp", pos_i[:], I["pos_pt"][s], writes=[b])
        kb.dma("sp", invf[:], I[invf_name].partition_broadcast(128), writes=[b])
        kb.op("dve", lambda e: e.tensor_copy(out=pos_f[:], in_=pos_i[:]), reads=[b], writes=[b])
        kb.op("dve", lambda e: e.tensor_tensor(out=ang[:], in0=bc_last(pos_f[:, :], half), in1=bc_mid(invf[:, :], NT),
                                               op=ALU.mult), reads=[b], writes=[b])
        kb.op("dve", lambda e: e.tensor_scalar(out=kq[:], in0=ang[:], scalar1=1.0 / (2 * PI), scalar2=None,
                                               op0=ALU.mult), reads=[b], writes=[b])
        kb.op("dve", lambda e: e.tensor_copy(out=ki[:], in_=kq[:]), reads=[b], writes=[b])
        kb.op("dve", lambda e: e.tensor_copy(out=kq[:], in_=ki[:]), reads=[b], writes=[b])
        kb.op("dve", lambda e: e.scalar_tensor_tensor(out=ang[:], in0=kq[:], scalar=-2 * PI, in1=ang[:],
                                                      op0=ALU.mult, op1=ALU.add), reads=[b], writes=[b])
        lim = 3.1415925
        for shift, dst in ((0.0, sin), (PI / 2, cos)):
            kb.op("dve", lambda e, shift=shift: e.tensor_scalar(out=ys[:], in0=ang[:], scalar1=shift, scalar2=None,
                                                                op0=ALU.add), reads=[b], writes=[b])
            kb.op("dve", lambda e: e.tensor_scalar(out=mm[:], in0=ys[:], scalar1=PI, scalar2=-2 * PI,
                                                   op0=ALU.is_gt, op1=ALU.mult), reads=[b], writes=[b])
            kb.op("dve", lambda e: e.tensor_tensor(out=ys[:], in0=ys[:], in1=mm[:], op=ALU.add), reads=[b], writes=[b])
            kb.op("dve", lambda e: e.tensor_scalar(out=ys[:], in0=ys[:], scalar1=lim, scalar2=-lim,
                                                   op0=ALU.min, op1=ALU.max), reads=[b], writes=[b])
            kb.op("act", lambda e, dst=dst: e.activation(out=dst[:], in_=ys[:], func=AF.Sin), reads=[b], writes=[b])
        return cos, sin, b

    def load_w_bf16(self, dst, src, kchunks, bw):
        v = src.rearrange("(k p) n -> p k n", p=128)
        for k in range(kchunks):
            self.kb.dma("pool", dst[:, k, :], v[:, k, :], writes=[bw])

    def bcast_load(self, dst, src1d, b):
        self.kb.dma("sp", dst, src1d.partition_broadcast(128), writes=[b])

    def norm_mod_T(self, x_t, bx, gmod, shift, bmod, tmp, btmp, h_bf, bh, tp, btp, hT, bhT, ss, bss):
        kb = self.kb
        kb.op("dve", lambda e: e.scalar_tensor_tensor(out=tmp[:], in0=x_t[:], scalar=1.0, in1=x_t[:], op0=ALU.mult, op1=ALU.mult, accum_out=ss[:, 0:1]),
              reads=[bx], writes=[btmp, bss])
        self.rstd_of(ss[:, 0:1], D, 1, bss, "")
        kb.op("dve", lambda e: e.scalar_tensor_tensor(out=tmp[:], in0=x_t[:], scalar=ss[:, 0:1], in1=gmod[:],
                                                      op0=ALU.mult, op1=ALU.mult), reads=[bx, bss, bmod], writes=[btmp])
        kb.op("pool", lambda e: e.tensor_tensor(out=h_bf[:], in0=tmp[:], in1=shift[:], op=ALU.add),
              reads=[btmp, bmod], writes=[bh])
        for k in range(8):
            kb.op("pe", lambda e, k=k: e.transpose(out=tp[:, k, :], in_=h_bf[:, k * 128:(k + 1) * 128],
                                                   identity=self.ident_b[:]), reads=[bh, self.b_const], writes=[btp])
        kb.op("act", lambda e: e.copy(out=hT[:], in_=tp[:]), reads=[btp], writes=[bhT])

    def phase0(self):
        kb, I, S, nseq = self.kb, self.I, self.S, self.nseq
        kb.push()
        cin = kb.sb("cin", [128, nseq, 8], F32)
        cact = kb.sb("cact", [128, nseq, 8], F32)
        crep = kb.sb("crep", [128, nseq, 8, 128], F32)
        bcr = Buf()
        for s in range(nseq):
            kb.dma("sp", cin[:, s, :], I["c_pk"][s], writes=[bcr])
        kb.op("act", lambda e: e.activation(out=cact[:], in_=cin[:], func=AF.Silu), reads=[bcr], writes=[bcr])
        for s in range(nseq):
            kb.op("dve", lambda e, s=s: e.tensor_copy(out=crep[:, s, :, :], in_=bc_last(cact[:, s, :], 128)),
                  reads=[bcr], writes=[bcr])
        adab = kb.sb("adab", [128, 6 * D], F32)
        ng = [kb.sb("ng1", [128, D], F32), kb.sb("ng2", [128, D], F32)]
        bab = Buf()
        wch = [kb.sb("wch%d" % i, [128, 8, 512], F32) for i in range(2)]
        bwch = [Buf(), Buf()]
        pm = [kb.ps("p0pm%d" % i, [128, 512], F32) for i in range(2)]
        bpm = [Buf(), Buf()]
        modt = [kb.sb("modt%d" % i, [128, 512], F32) for i in range(3)]
        bmodt = [Buf() for _ in range(3)]
        n = 0
        u = 0
        for l in range(2):
            for q in range(6):
                kb.dma("sp", adab[:, q * D:(q + 1) * D], I["ada_b"][l, q * D:(q + 1) * D].partition_broadcast(128),
                       writes=[bab])
            self.bcast_load(ng[0][:], I["norm1_g"][l], bab)
            self.bcast_load(ng[1][:], I["norm2_g"][l], bab)
            for j in range(12):
                wv = I["ada_w"][l][:, j * 512:(j + 1) * 512].rearrange("(k p) n -> p k n", p=128)
                w_ = wch[n % 2]
                kb.dma("sp", w_[:], wv, writes=[bwch[n % 2]])
                for s in range(nseq):
                    p_ = pm[u % 2]
                    for k in range(8):
                        kb.op("pe", lambda e, k=k, s=s, p_=p_, w_=w_: e.matmul(p_[:], lhsT=crep[:, s, k, :], rhs=w_[:, k, :],
                                                                             start=(k == 0), stop=(k == 7)),
                              reads=[bcr, bwch[n % 2]], writes=[bpm[u % 2]])
                    m_ = modt[u % 3]
                    bm_ = bmodt[u % 3]
                    kb.op("dve", lambda e, p_=p_, m_=m_, j=j: e.tensor_tensor(out=m_[:], in0=p_[:],
                                                                           in1=adab[:, j * 512:(j + 1) * 512], op=ALU.add),
                          reads=[bpm[u % 2], bab], writes=[bm_])
                    part, half = j // 2, j % 2
                    if part in (1, 4):
                        g_ = ng[0] if part == 1 else ng[1]
                        kb.op("dve", lambda e, m_=m_, g_=g_, half=half: e.scalar_tensor_tensor(
                            out=m_[:], in0=m_[:], scalar=1.0, in1=g_[:, half * 512:(half + 1) * 512],
                            op0=ALU.add, op1=ALU.mult), reads=[bm_, bab], writes=[bm_])
                    kb.dma("sp", S["MOD"][s, l, part, :, half * 512:(half + 1) * 512], m_[:],
                           reads=[bm_], writes=[self.db("MOD", (s, l, part))])
                    u += 1
                n += 1
        kb.pop()

    def phase1a(self):
        kb, I, S, nseq = self.kb, self.I, self.S, self.nseq
        kb.push()
        self.zero_xg()
        bw = Buf()
        w_in = kb.sb("w_in_a", [128, 8, 672], BF16)
        w_uq = kb.sb("w_uq", [128, 3, 768], BF16)
        w_ukv = kb.sb("w_ukv", [128, 2, 1024], BF16)
        self.load_w_bf16(w_in, I["hyb_w_in"][:, 0:672], 8, bw)
        self.load_w_bf16(w_uq, I["mla_w_uq"], 3, bw)
        self.load_w_bf16(w_ukv, I["mla_w_ukv"], 2, bw)
        gcq = kb.sb("gcq", [128, 384], F32)
        gckv = kb.sb("gckv", [128, 256], F32)
        gq = kb.sb("gq", [128, 96], F32)
        gk = kb.sb("gk", [128, 96], F32)
        self.bcast_load(gcq[:], I["mla_cq_norm_g"], bw)
        self.bcast_load(gckv[:], I["mla_ckv_norm_g"], bw)
        self.bcast_load(gq[:], I["mla_q_head_g"], bw)
        self.bcast_load(gk[:], I["mla_k_head_g"], bw)

        kT = kb.sb("kT", [96, 8, SEQ], BF16)
        V = kb.sb("Vc", [128, NT, 8, 65], BF16)
        bkT = [Buf() for _ in range(NT)]
        bV = [Buf() for _ in range(NT)]
        bVones = Buf()
        kb.op("pool", lambda e: e.memset(V[:, :, :, 64:65], 1.0), writes=[bVones])

        gmod = kb.sb("gmod1", [128, D], F32)
        shift = kb.sb("shift1", [128, D], F32)
        bmod = Buf()
        x_t = [kb.sb("x_t%d" % i, [128, D], F32) for i in range(2)]
        bx = [Buf(), Buf()]
        tmp = kb.sb("tmp", [128, D], F32); btmp = Buf()
        h_bf = kb.sb("h_bf", [128, D], BF16); bh = Buf()
        hT = kb.sb("hT", [128, 8, 128], BF16); bhT = Buf()
        ss = kb.sb("ss", [128, 4], F32); bss = Buf()
        proj = kb.sb("proj", [128, 672], F32); bproj = Buf()
        sq = kb.sb("sq", [128, 8, 96], F32); bsq = Buf()
        cqn = kb.sb("cqn", [128, 384], BF16); bcqn = Buf()
        cqT = kb.sb("cqT", [128, 3, 128], BF16); bcqT = Buf()
        ckvn = kb.sb("ckvn", [128, 256], BF16); bckvn = Buf()
        ckvT = kb.sb("ckvT", [128, 2, 128], BF16); bckvT = Buf()
        q_sb = kb.sb("q_sb", [128, 8, 96], F32); bq = Buf()
        qn = kb.sb("qn", [128, 8, 96], F32); bqn = Buf()
        rq8 = kb.sb("rq8", [128, 16], F32); brq8 = Buf()
        R = kb.sb("Rr", [128, 8, 96], F32); bR = Buf()
        q_full = kb.sb("q_full", [128, 8, 96], BF16); bqf = Buf()
        qT = kb.sb("qT", [96, 8, 128], BF16); bqT = Buf()
        kv_sb = kb.sb("kv_sb", [128, 8, 128], F32); bkv = Buf()
        k_full = kb.sb("k_full", [128, 8, 96], BF16); bkf = Buf()
        kr = kb.sb("kr", [128, 32], F32); bkr = Buf()
        kr2 = kb.sb("kr2", [128, 32], F32)
        rt = [kb.sb("rt%d" % i, [128, 8, 16], F32) for i in range(4)]; brt = Buf()
        PT = [kb.sb("PT%d" % i, [128, 4, 128], BF16) for i in range(3)]; bPT = [Buf() for _ in range(3)]
        attn = kb.sb("attn", [128, 8, 64], BF16); battn = Buf()
        rden = kb.sb("rden", [128, 8], F32); brden = Buf()

        tp = [kb.ps("tp%d" % i, [128, 8, 128], BF16) for i in range(2)]; btp = [Buf(), Buf()]
        mm = [kb.ps("mm%d" % i, [128, 512], F32) for i in range(2)]; bmm = [Buf(), Buf()]
        s2 = kb.ps("s2", [128, 2, 512], F32); bs2 = [Buf(), Buf()]
        oo = [kb.ps("oo%d" % i, [128, 512], F32) for i in range(2)]; boo = [Buf(), Buf()]
        scale = 96.0 ** -0.5
        xi = 0
        ucount = 0
        for s in range(nseq):
            cos, sin, brope = self.rope_tables(s, 16, "k_invf16", "a%d" % s)
            kb.dma("sp", gmod[:], S["MOD"][s, 0, 1], reads=[self.db("MOD", (s, 0, 1))], writes=[bmod])
            kb.dma("sp", shift[:], S["MOD"][s, 0, 0], reads=[self.db("MOD", (s, 0, 0))], writes=[bmod])
            for t in range(self.ntl):
                X = x_t[xi % 2]; bX = bx[xi % 2]; xi += 1
                kb.dma("sp", X[:], I["x"][(s * NT + t) * 128:(s * NT + t + 1) * 128, :], writes=[bX])
                self.norm_mod_T(X, bX, gmod, shift, bmod, tmp, btmp, h_bf, bh, tp[0], btp[0], hT, bhT, ss, bss)
                for gi, (c0, c1) in enumerate(((0, 512), (512, 672))):
                    for k in range(8):
                        kb.op("pe", lambda e, k=k, gi=gi, c0=c0, c1=c1: e.matmul(mm[gi][:, 0:c1 - c0], lhsT=hT[:, k, :],
                                                                              rhs=w_in[:, k, c0:c1], start=(k == 0), stop=(k == 7)),
                              reads=[bhT, bw], writes=[bmm[gi]])
                    kb.op("act", lambda e, gi=gi, c0=c0, c1=c1: e.copy(out=proj[:, c0:c1], in_=mm[gi][:, 0:c1 - c0]),
                          reads=[bmm[gi]], writes=[bproj])
                for ci, (c0, w) in enumerate(((0, 384), (384, 256), (640, 32))):
                    kb.op("dve", lambda e, c0=c0, w=w, ci=ci: e.scalar_tensor_tensor(out=tmp[:, 0:w], in0=proj[:, c0:c0 + w], scalar=1.0, in1=proj[:, c0:c0 + w], op0=ALU.mult, op1=ALU.mult, accum_out=ss[:, 1 + ci:2 + ci]), reads=[bproj], writes=[btmp, bss])
                kb.op("dve", lambda e: e.tensor_scalar(out=ss[:, 1:2], in0=ss[:, 1:2], scalar1=1.0 / 384, scalar2=EPS,
                                                       op0=ALU.mult, op1=ALU.add), reads=[bss], writes=[bss])
                kb.op("dve", lambda e: e.tensor_scalar(out=ss[:, 2:3], in0=ss[:, 2:3], scalar1=1.0 / 256, scalar2=EPS,
                                                       op0=ALU.mult, op1=ALU.add), reads=[bss], writes=[bss])
                kb.op("dve", lambda e: e.tensor_scalar(out=ss[:, 3:4], in0=ss[:, 3:4], scalar1=1.0 / 32, scalar2=EPS,
                                                       op0=ALU.mult, op1=ALU.add), reads=[bss], writes=[bss])
                kb.op("pool", lambda e: e.tensor_tensor(out=ss[:, 1:4], in0=ss[:, 1:4], in1=self.neghalf[:, 0:3], op=ALU.pow),
                      reads=[bss, self.b_const], writes=[bss])
                kb.op("dve", lambda e: e.scalar_tensor_tensor(out=cqn[:], in0=proj[:, 0:384], scalar=ss[:, 1:2], in1=gcq[:],
                                                              op0=ALU.mult, op1=ALU.mult), reads=[bproj, bss, bw], writes=[bcqn])
                kb.op("dve", lambda e: e.scalar_tensor_tensor(out=ckvn[:], in0=proj[:, 384:640], scalar=ss[:, 2:3], in1=gckv[:],
                                                              op0=ALU.mult, op1=ALU.mult), reads=[bproj, bss, bw], writes=[bckvn])
                kb.op("dve", lambda e: e.scalar_tensor_tensor(out=kr[:], in0=proj[:, 640:672], scalar=ss[:, 3:4], in1=gk[:, 64:96],
                                                              op0=ALU.mult, op1=ALU.mult), reads=[bproj, bss, bw], writes=[bkr])
                for k in range(3):
                    kb.op("pe", lambda e, k=k: e.transpose(out=tp[1][:, k, :], in_=cqn[:, k * 128:(k + 1) * 128],
                                                           identity=self.ident_b[:]), reads=[bcqn, self.b_const], writes=[btp[1]])
                for k in range(2):
                    kb.op("pe", lambda e, k=k: e.transpose(out=tp[1][:, 3 + k, :], in_=ckvn[:, k * 128:(k + 1) * 128],
                                                           identity=self.ident_b[:]), reads=[bckvn, self.b_const], writes=[btp[1]])
                kb.op("act", lambda e: e.copy(out=cqT[:], in_=tp[1][:, 0:3, :]), reads=[btp[1]], writes=[bcqT])
                kb.op("act", lambda e: e.copy(out=ckvT[:], in_=tp[1][:, 3:5, :]), reads=[btp[1]], writes=[bckvT])
                for gi, (c0, c1) in enumerate(((0, 512), (512, 768))):
                    for k in range(3):
                        kb.op("pe", lambda e, k=k, gi=gi, c0=c0, c1=c1: e.matmul(mm[gi][:, 0:c1 - c0], lhsT=cqT[:, k, :],
                                                                              rhs=w_uq[:, k, c0:c1], start=(k == 0), stop=(k == 2)),
                              reads=[bcqT, bw], writes=[bmm[gi]])
                    kb.op("act", lambda e, gi=gi, c0=c0, c1=c1: e.copy(
                        out=q_sb[:].rearrange("p h d -> p (h d)")[:, c0:c1], in_=mm[gi][:, 0:c1 - c0]),
                        reads=[bmm[gi]], writes=[bq])
                kb.op("pool", lambda e: e.tensor_tensor(out=sq[:], in0=q_sb[:], in1=q_sb[:], op=ALU.mult), reads=[bq], writes=[bsq])
                kb.op("dve", lambda e: e.tensor_reduce(out=rq8[:, 0:8], in_=sq[:, :, 0:64], axis=AX.X, op=ALU.add),
                      reads=[bsq], writes=[brq8])
                kb.op("dve", lambda e: e.tensor_reduce(out=rq8[:, 8:16], in_=sq[:, :, 64:96], axis=AX.X, op=ALU.add),
                      reads=[bsq], writes=[brq8])
                kb.op("dve", lambda e: e.tensor_scalar(out=rq8[:, 0:8], in0=rq8[:, 0:8], scalar1=1.0 / 64, scalar2=EPS,
                                                       op0=ALU.mult, op1=ALU.add), reads=[brq8], writes=[brq8])
                kb.op("dve", lambda e: e.tensor_scalar(out=rq8[:, 8:16], in0=rq8[:, 8:16], scalar1=1.0 / 32, scalar2=EPS,
                                                       op0=ALU.mult, op1=ALU.add), reads=[brq8], writes=[brq8])
                kb.op("pool", lambda e: e.tensor_tensor(out=rq8[:], in0=rq8[:], in1=self.neghalf[:, 0:16], op=ALU.pow),
                      reads=[brq8, self.b_const], writes=[brq8])
                kb.op("dve", lambda e: e.tensor_tensor(out=qn[:, :, 0:64], in0=q_sb[:, :, 0:64], in1=bc_last(rq8[:, 0:8], 64),
                                                       op=ALU.mult), reads=[bq, brq8], writes=[bqn])
                kb.op("dve", lambda e: e.tensor_tensor(out=qn[:, :, 64:96], in0=q_sb[:, :, 64:96], in1=bc_last(rq8[:, 8:16], 32),
                                                       op=ALU.mult), reads=[bq, brq8], writes=[bqn])
                kb.op("pool", lambda e: e.tensor_tensor(out=qn[:], in0=qn[:], in1=bc_mid(gq[:, :], 8), op=ALU.mult),
                      reads=[bqn, bw], writes=[bqn])
                kb.op("act", lambda e: e.copy(out=q_full[:, :, 0:64], in_=qn[:, :, 0:64]), reads=[bqn], writes=[bqf])
                cb = bc_mid(cos[:, t, :], 8)
                sb_ = bc_mid(sin[:, t, :], 8)
                x1 = qn[:, :, 64:80]
                x2 = qn[:, :, 80:96]
                kb.op("dve", lambda e: e.tensor_tensor(out=rt[0][:], in0=x1, in1=cb, op=ALU.mult), reads=[bqn, brope], writes=[brt])
                kb.op("dve", lambda e: e.tensor_tensor(out=rt[1][:], in0=x2, in1=sb_, op=ALU.mult), reads=[bqn, brope], writes=[brt])
                kb.op("pool", lambda e: e.tensor_tensor(out=rt[2][:], in0=x2, in1=cb, op=ALU.mult), reads=[bqn, brope], writes=[brt])
                kb.op("pool", lambda e: e.tensor_tensor(out=rt[3][:], in0=x1, in1=sb_, op=ALU.mult), reads=[bqn, brope], writes=[brt])
                kb.op("dve", lambda e: e.tensor_tensor(out=q_full[:, :, 64:80], in0=rt[0][:], in1=rt[1][:], op=ALU.subtract),
                      reads=[brt], writes=[bqf])
                kb.op("dve", lambda e: e.tensor_tensor(out=q_full[:, :, 80:96], in0=rt[2][:], in1=rt[3][:], op=ALU.add),
                      reads=[brt], writes=[bqf])
                for h in range(8):
                    kb.op("pe", lambda e, h=h: e.transpose(out=tp[0][0:96, h, :], in_=q_full[:, h, :], identity=self.ident_b[:]),
                          reads=[bqf, self.b_const], writes=[btp[0]])
                kb.op("act", lambda e: e.copy(out=qT[:], in_=tp[0][0:96, :, :]), reads=[btp[0]], writes=[bqT])
                for gi in range(2):
                    for k in range(2):
                        kb.op("pe", lambda e, k=k, gi=gi: e.matmul(mm[gi][:], lhsT=ckvT[:, k, :], rhs=w_ukv[:, k, gi * 512:(gi + 1) * 512],
                                                                 start=(k == 0), stop=(k == 1)), reads=[bckvT, bw], writes=[bmm[gi]])
                    kb.op("act", lambda e, gi=gi: e.copy(out=kv_sb[:].rearrange("p h d -> p (h d)")[:, gi * 512:(gi + 1) * 512],
                                                        in_=mm[gi][:]), reads=[bmm[gi]], writes=[bkv])
                kb.op("pool", lambda e, t=t: e.tensor_copy(out=V[:, t, :, 0:64], in_=kv_sb[:, :, 64:128]),
                      reads=[bkv, bVones], writes=[bV[t]])
                kb.op("pool", lambda e: e.tensor_tensor(out=sq[:, :, 0:64], in0=kv_sb[:, :, 0:64], in1=kv_sb[:, :, 0:64], op=ALU.mult),
                      reads=[bkv], writes=[bsq])
                kb.op("dve", lambda e: e.tensor_reduce(out=rq8[:, 0:8], in_=sq[:, :, 0:64], axis=AX.X, op=ALU.add),
                      reads=[bsq], writes=[brq8])
                kb.op("dve", lambda e: e.tensor_scalar(out=rq8[:, 0:8], in0=rq8[:, 0:8], scalar1=1.0 / 64, scalar2=EPS,
                                                       op0=ALU.mult, op1=ALU.add), reads=[brq8], writes=[brq8])
                kb.op("pool", lambda e: e.tensor_tensor(out=rq8[:, 0:8], in0=rq8[:, 0:8], in1=self.neghalf[:, 0:8], op=ALU.pow),
                      reads=[brq8, self.b_const], writes=[brq8])
                kb.op("dve", lambda e: e.tensor_tensor(out=sq[:, :, 0:64], in0=kv_sb[:, :, 0:64], in1=bc_last(rq8[:, 0:8], 64),
                                                       op=ALU.mult), reads=[bkv, brq8], writes=[bsq])
                kb.op("pool", lambda e: e.tensor_tensor(out=k_full[:, :, 0:64], in0=sq[:, :, 0:64], in1=bc_mid(gk[:, 0:64], 8),
                                                        op=ALU.mult), reads=[bsq, bw], writes=[bkf])
                c1_ = cos[:, t, :]
                s1_ = sin[:, t, :]
                kb.op("dve", lambda e: e.tensor_tensor(out=rt[0][:, 0, :], in0=kr[:, 0:16], in1=c1_, op=ALU.mult), reads=[bkr, brope], writes=[brt])
                kb.op("dve", lambda e: e.tensor_tensor(out=rt[1][:, 0, :], in0=kr[:, 16:32], in1=s1_, op=ALU.mult), reads=[bkr, brope], writes=[brt])
                kb.op("dve", lambda e: e.tensor_tensor(out=rt[2][:, 0, :], in0=kr[:, 16:32], in1=c1_, op=ALU.mult), reads=[bkr, brope], writes=[brt])
                kb.op("dve", lambda e: e.tensor_tensor(out=rt[3][:, 0, :], in0=kr[:, 0:16], in1=s1_, op=ALU.mult), reads=[bkr, brope], writes=[brt])
                kb.op("dve", lambda e: e.tensor_tensor(out=kr2[:, 0:16], in0=rt[0][:, 0, :], in1=rt[1][:, 0, :], op=ALU.subtract),
                      reads=[brt], writes=[bkr])
                kb.op("dve", lambda e: e.tensor_tensor(out=kr2[:, 16:32], in0=rt[2][:, 0, :], in1=rt[3][:, 0, :], op=ALU.add),
                      reads=[brt], writes=[bkr])
                kb.op("dve", lambda e: e.tensor_copy(out=k_full[:, :, 64:96], in_=bc_mid(kr2[:, :], 8)), reads=[bkr], writes=[bkf])
                for h in range(8):
                    kb.op("pe", lambda e, h=h: e.transpose(out=tp[1][0:96, h, :], in_=k_full[:, h, :], identity=self.ident_b[:]),
                          reads=[bkf, self.b_const], writes=[btp[1]])
                kb.op("act", lambda e, t=t: e.copy(out=kT[:, :, t * 128:(t + 1) * 128], in_=tp[1][0:96, :, :]),
                      reads=[btp[1]], writes=[bkT[t]])
                units = []
                for h in range(8):
                    for a in range(0, t + 1, 4):
                        units.append((h, a, min(a + 4, t + 1)))

                def emit_S(ui, u):
                    h, a, b = u
                    bank = ui % 2
                    for kt in range(a, b):
                        kb.op("pe", lambda e, kt=kt, h=h, a=a, bank=bank: e.matmul(
                            s2[:, bank, (kt - a) * 128:(kt - a + 1) * 128], lhsT=kT[:, h, kt * 128:(kt + 1) * 128],
                            rhs=qT[:, h, :], start=True, stop=True), reads=[bkT[kt], bqT], writes=[bs2[bank]])

                base = ucount
                emit_S(base, units[0])
                for i, u in enumerate(units):
                    ui = base + i
                    h, a, b = u
                    if i + 1 < len(units):
                        emit_S(ui + 1, units[i + 1])
                    bank = ui % 2
                    P = PT[ui % 3]; bP = bPT[ui % 3]
                    n = (b - a) * 128
                    kb.op("act", lambda e, P=P, bank=bank, n=n: e.activation(
                        out=P[:].rearrange("p a b -> p (a b)")[:, 0:n], in_=s2[:, bank, 0:n], func=AF.Exp, scale=scale),
                        reads=[bs2[bank]], writes=[bP])
                    if b == t + 1:
                        kb.op("dve", lambda e, P=P, j=t - a: e.tensor_tensor(out=P[:, j, :], in0=P[:, j, :], in1=self.mask_le[:],
                                                                           op=ALU.mult), reads=[bP, self.b_const], writes=[bP])
                    ob = oo[h // 4]
                    for kt in range(a, b):
                        kb.op("pe", lambda e, kt=kt, h=h, a=a, P=P, ob=ob: e.matmul(
                            ob[:, (h % 4) * 65:(h % 4) * 65 + 65], lhsT=P[:, kt - a, :], rhs=V[:, kt, h, :],
                            start=(kt == 0), stop=(kt == t)), reads=[bP, bV[kt], bVones], writes=[boo[h // 4]])
                ucount += len(units)
                for hb in range(2):
                    ov = oo[hb][:, 0:260].rearrange("p (h d) -> p h d", d=65)
                    kb.op("dve", lambda e, hb=hb, ov=ov: e.reciprocal(out=rden[:, hb * 4:(hb + 1) * 4], in_=ov[:, :, 64]),
                          reads=[boo[hb]], writes=[brden])
                    kb.op("dve", lambda e, hb=hb, ov=ov: e.tensor_tensor(out=attn[:, hb * 4:(hb + 1) * 4, :], in0=ov[:, :, 0:64],
                                                                        in1=bc_last(rden[:, hb * 4:(hb + 1) * 4], 64), op=ALU.mult),
                          reads=[boo[hb], brden], writes=[battn])
                kb.dma("sp", S["ATT"][s * NT + t], attn[:].rearrange("p h d -> p (h d)"), reads=[battn],
                       writes=[self.db("ATT", s * NT + t)])
        kb.pop()

    def alloc_router(self, l):
        kb, I = self.kb, self.I
        r = {}
        r["bw"] = Buf()
        r["rw"] = kb.sb("rw", [128, 8, 32], F32)
        kb.dma("sp", r["rw"][:], I["router_w"][l].rearrange("(k p) n -> p k n", p=128), writes=[r["bw"]])
        r["rb"] = kb.sb("rb", [128, 32], F32)
        self.bcast_load(r["rb"][:], I["router_b"][l], r["bw"])
        r["rwh"] = kb.sb("rwh", [128, 8, 32], BF16)
        r["rwl"] = kb.sb("rwl", [128, 8, 32], BF16)
        kb.op("dve", lambda e: e.tensor_copy(out=r["rwh"][:], in_=r["rw"][:]), reads=[r["bw"]], writes=[r["bw"]])
        kb.op("dve", lambda e: e.tensor_tensor(out=r["rwl"][:], in0=r["rw"][:], in1=r["rwh"][:], op=ALU.subtract),
              reads=[r["bw"]], writes=[r["bw"]])
        r["h2f"] = kb.sb("h2f", [128, D], F32); r["bh2f"] = Buf()
        r["h2hi"] = kb.sb("h2hi", [128, D], BF16); r["bh2hi"] = Buf()
        r["h2lo"] = kb.sb("h2lo", [128, D], BF16); r["bh2lo"] = Buf()
        r["h2Tl"] = kb.sb("h2Tl", [128, 8, 128], BF16); r["bh2Tl"] = Buf()
        r["h2Tb"] = kb.sb("h2Tb", [128, 8, 128], BF16); r["bh2Tb"] = Buf()
        r["lg"] = kb.sb("lg", [128, 32], F32); r["blg"] = Buf()
        r["m8"] = kb.sb("m8", [128, 8], F32)
        r["msk"] = kb.sb("msk", [128, 32], F32)
        r["ex"] = kb.sb("ex", [128, 32], F32)
        r["den"] = kb.sb("den", [128, 2], F32)
        r["G"] = kb.sb("Gt", [128, 32], F32); r["bG"] = Buf()
        r["ss"] = kb.sb("ss2", [128, 1], F32); r["bss"] = Buf()
        r["cnt"] = kb.sb("cnt_b", [128, 32], F32); r["bcnt"] = Buf()
        kb.op("pool", lambda e: e.memset(r["cnt"][:], 0.0), writes=[r["bcnt"]])
        r["Mb"] = kb.sb("Mb", [128, 32], BF16)
        for n_ in ("posf", "valid", "slotm", "oh", "junk", "Gv"):
            r[n_] = kb.sb(n_, [128, 32], F32)
        r["slotf"] = kb.sb("slotf", [128, 4], F32)
        r["sloti"] = kb.sb("sloti", [128, 4], I32); r["bsloti"] = Buf()
        r["gk"] = kb.sb("gk", [128, 4], F32); r["bgk"] = Buf()
        r["brt"] = Buf()
        return r

    def norm2_router(self, r, x1, bx1, gmod2, shift2, bmod, tmp, btmp, tp, btp, mmp, bmmp, tile_idx):
        kb, S = self.kb, self.S
        ss, bss = r["ss"], r["bss"]
        kb.op("dve", lambda e: e.scalar_tensor_tensor(out=tmp[:], in0=x1[:], scalar=1.0, in1=x1[:], op0=ALU.mult, op1=ALU.mult, accum_out=ss[:, 0:1]),
              reads=[bx1], writes=[btmp, bss])
        self.rstd_of(ss[:, 0:1], D, 1, bss, "")
        kb.op("dve", lambda e: e.scalar_tensor_tensor(out=tmp[:], in0=x1[:], scalar=ss[:, 0:1], in1=gmod2[:],
                                                      op0=ALU.mult, op1=ALU.mult), reads=[bx1, bss, bmod], writes=[btmp])
        kb.op("pool", lambda e: e.tensor_tensor(out=r["h2f"][:], in0=tmp[:], in1=shift2[:], op=ALU.add),
              reads=[btmp, bmod], writes=[r["bh2f"]])
        kb.op("act", lambda e: e.copy(out=r["h2hi"][:], in_=r["h2f"][:]), reads=[r["bh2f"]], writes=[r["bh2hi"]])
        kb.op("dve", lambda e: e.tensor_tensor(out=r["h2lo"][:], in0=r["h2f"][:], in1=r["h2hi"][:], op=ALU.subtract),
              reads=[r["bh2f"], r["bh2hi"]], writes=[r["bh2lo"]])
        for k in range(8):
            kb.op("pe", lambda e, k=k: e.transpose(out=tp[0][:, k, :], in_=r["h2hi"][:, k * 128:(k + 1) * 128],
                                                   identity=self.ident_b[:]), reads=[r["bh2hi"], self.b_const], writes=[btp[0]])
        for k in range(8):
            kb.op("pe", lambda e, k=k: e.transpose(out=tp[1][:, k, :], in_=r["h2lo"][:, k * 128:(k + 1) * 128],
                                                   identity=self.ident_b[:]), reads=[r["bh2lo"], self.b_const], writes=[btp[1]])
        kb.op("act", lambda e: e.copy(out=r["h2Tb"][:], in_=tp[0][:]), reads=[btp[0]], writes=[r["bh2Tb"]])
        kb.op("dve", lambda e: e.tensor_copy(out=r["h2Tl"][:], in_=tp[1][:]), reads=[btp[1]], writes=[r["bh2Tl"]])
        if self.debug:
            kb.dma("sp", S["H2T"][tile_idx], r["h2Tb"][:], reads=[r["bh2Tb"]], writes=[self.db("H2T", tile_idx)])
        if _STOP <= 6:
            return
        passes = [("h2Tb", "rwh"), ("h2Tl", "rwh"), ("h2Tb", "rwl")]
        for pi, (a_, w_) in enumerate(passes):
            for k in range(8):
                kb.op("pe", lambda e, k=k, a_=a_, w_=w_, pi=pi: e.matmul(mmp[:, 0:32], lhsT=r[a_][:, k, :], rhs=r[w_][:, k, :],
                                                                       start=(pi == 0 and k == 0), stop=(pi == 2 and k == 7)),
                      reads=[r["bh2Tb"], r["bh2Tl"], r["bw"]], writes=[bmmp])
        lg, m8, msk, ex, den, G = r["lg"], r["m8"], r["msk"], r["ex"], r["den"], r["G"]
        bl = r["blg"]
        kb.op("dve", lambda e: e.tensor_tensor(out=lg[:], in0=mmp[:, 0:32], in1=r["rb"][:], op=ALU.add),
              reads=[bmmp, r["bw"]], writes=[bl])
        if _STOP <= 7:
            return
        kb.op("dve", lambda e: e.max(out=m8[:], in_=lg[:]), reads=[bl], writes=[bl])
        kb.op("dve", lambda e: e.tensor_scalar(out=msk[:], in0=lg[:], scalar1=m8[:, 3:4], scalar2=None, op0=ALU.is_ge),
              reads=[bl], writes=[bl])
        kb.op("dve", lambda e: e.tensor_scalar(out=den[:, 1:2], in0=m8[:, 0:1], scalar1=-1.0, scalar2=None, op0=ALU.mult),
              reads=[bl], writes=[bl])
        kb.op("act", lambda e: e.activation(out=ex[:], in_=lg[:], func=AF.Exp, bias=den[:, 1:2], scale=1.0),
              reads=[bl], writes=[bl])
        kb.op("dve", lambda e: e.scalar_tensor_tensor(out=ex[:], in0=ex[:], scalar=1.0, in1=msk[:], op0=ALU.mult, op1=ALU.mult, accum_out=den[:, 0:1]), reads=[bl], writes=[bl])
        kb.op("dve", lambda e: e.reciprocal(out=den[:, 0:1], in_=den[:, 0:1]), reads=[bl], writes=[bl])
        kb.op("dve", lambda e: e.tensor_scalar(out=G[:], in0=ex[:], scalar1=den[:, 0:1], scalar2=None, op0=ALU.mult),
              reads=[bl], writes=[r["bG"]])
        if self.debug:
            kb.dma("sp", S["GS"][tile_idx], G[:], reads=[r["bG"]], writes=[self.db("GS", tile_idx)])
        cap = self.cap
        brt = r["brt"]
        kb.op("dve", lambda e: e.tensor_copy(out=r["Mb"][:], in_=msk[:]), reads=[bl], writes=[brt])
        kb.op("pe", lambda e: e.matmul(mmp[:, 32:64], lhsT=self.U_b[:], rhs=r["Mb"][:], start=True, stop=True),
              reads=[brt, self.b_const], writes=[bmmp])
        kb.op("pe", lambda e: e.matmul(mmp[:, 64:96], lhsT=self.ones_b[:], rhs=r["Mb"][:], start=True, stop=True),
              reads=[brt, self.b_const], writes=[bmmp])
        kb.op("dve", lambda e: e.tensor_tensor(out=r["posf"][:], in0=mmp[:, 32:64], in1=r["cnt"][:], op=ALU.add),
              reads=[bmmp, r["bcnt"]], writes=[brt])
        kb.op("dve", lambda e: e.tensor_tensor(out=r["cnt"][:], in0=mmp[:, 64:96], in1=r["cnt"][:], op=ALU.add),
              reads=[bmmp, r["bcnt"]], writes=[r["bcnt"]])
        kb.op("dve", lambda e: e.tensor_scalar(out=r["valid"][:], in0=r["posf"][:], scalar1=float(cap), scalar2=None, op0=ALU.is_lt),
              reads=[brt], writes=[brt])
        kb.op("dve", lambda e: e.tensor_tensor(out=r["slotm"][:], in0=r["posf"][:], in1=self.iotaE[:], op=ALU.add),
              reads=[brt, self.b_const], writes=[brt])
        kb.op("dve", lambda e: e.tensor_scalar(out=r["junk"][:], in0=r["valid"][:], scalar1=-1.0e6, scalar2=1.0e6,
                                               op0=ALU.mult, op1=ALU.add), reads=[brt], writes=[brt])
        kb.op("dve", lambda e: e.tensor_tensor(out=r["slotm"][:], in0=r["slotm"][:], in1=r["junk"][:], op=ALU.add),
              reads=[brt], writes=[brt])
        kb.op("dve", lambda e: e.tensor_tensor(out=r["Gv"][:], in0=G[:], in1=r["valid"][:], op=ALU.mult),
              reads=[brt, r["bG"]], writes=[brt])
        for k in range(4):
            kb.op("dve", lambda e, k=k: e.tensor_scalar(out=r["oh"][:], in0=lg[:], scalar1=m8[:, k:k + 1], scalar2=None, op0=ALU.is_equal),
                  reads=[bl, brt], writes=[brt])
            kb.op("dve", lambda e, k=k: e.scalar_tensor_tensor(out=r["junk"][:], in0=r["oh"][:], scalar=1.0, in1=r["slotm"][:],
                                                              op0=ALU.mult, op1=ALU.mult, accum_out=r["slotf"][:, k:k + 1]),
                  reads=[brt], writes=[brt])
            kb.op("dve", lambda e, k=k: e.scalar_tensor_tensor(out=r["junk"][:], in0=r["oh"][:], scalar=1.0, in1=r["Gv"][:],
                                                              op0=ALU.mult, op1=ALU.mult, accum_out=r["gk"][:, k:k + 1]),
                  reads=[brt, r["bgk"]], writes=[brt, r["bgk"]])
        kb.op("dve", lambda e: e.tensor_copy(out=r["sloti"][:], in_=r["slotf"][:]), reads=[brt, r["bsloti"]], writes=[r["bsloti"]])
        kb.dma("sp", S["SLOT"][tile_idx], r["sloti"][:], reads=[r["bsloti"]], writes=[self.db("SLOT", tile_idx)])
        kb.dma("sp", S["GK"][tile_idx], r["gk"][:], reads=[r["bgk"]], writes=[self.db("GK", tile_idx)])
        for k in range(4):
            kb.idma(S["XG"], r["sloti"][:, k:k + 1], r["h2hi"][:], None, 32 * cap - 1, reads=[r["bsloti"], r["bh2hi"]])

    def phase1b(self):
        kb, I, S, nseq = self.kb, self.I, self.S, self.nseq
        kb.push()
        bw = Buf()
        w_in = kb.sb("w_in_b", [128, 8, 2048], BF16)
        w_out = kb.sb("w_out", [128, 8, D], BF16)
        self.load_w_bf16(w_in, I["hyb_w_in"][:, 672:2720], 8, bw)
        self.load_w_bf16(w_out, I["hyb_w_out"], 8, bw)
        retg = kb.sb("retg", [128, 512], F32)
        self.bcast_load(retg[:], I["ret_norm_g"], bw)
        decT = kb.sb("decT", [128, 8 * 128], F32)
        qdec = kb.sb("qdec", [128, 8], F32)
        kdec = kb.sb("kdec", [128, 8], F32)
        cdec = kb.sb("cdec", [128, 4], F32)
        kb.dma("sp", decT[:], I["k_decayT"], writes=[bw])
        kb.dma("sp", qdec[:], I["k_qdec"], writes=[bw])
        kb.dma("sp", kdec[:], I["k_kdec"], writes=[bw])
        kb.dma("sp", cdec[:], I["k_cdec"], writes=[bw])
        r = self.alloc_router(0)

        mods = {n: kb.sb(n, [128, D], F32) for n in ("gmod1", "shift1", "gate1", "gmod2", "shift2")}
        bmod = Buf()
        x_t = [kb.sb("x_t%d" % i, [128, D], F32) for i in range(2)]; bx = [Buf(), Buf()]
        tmp = kb.sb("tmp", [128, D], F32); btmp = Buf()
        h_bf = kb.sb("h_bf", [128, D], BF16); bh = Buf()
        hT = kb.sb("hT", [128, 8, 128], BF16); bhT = Buf()
        ss = kb.sb("ss", [128, 4], F32); bss = Buf()
        raw = [kb.sb("raw%d" % i, [128, 8, 64], F32) for i in range(2)]; braw = [Buf(), Buf()]
        rr = [kb.sb("rr%d" % i, [128, 8, 64], F32) for i in range(2)]; brr = [Buf(), Buf()]
        rt = [kb.sb("rt%d" % i, [128, 8, 32], F32) for i in range(4)]; brt = Buf()
        rq_bf = kb.sb("rq_bf", [128, 8, 64], BF16); brqb = Buf()
        rqd_bf = kb.sb("rqd_bf", [128, 8, 64], BF16); brqd = Buf()
        rk_bf = kb.sb("rk_bf", [128, 8, 64], BF16); brkb = Buf()
        rkd_bf = kb.sb("rkd_bf", [128, 8, 64], BF16); brkd = Buf()
        v_bf = kb.sb("v_bf", [128, 8, 64], BF16); bv = Buf()
        sg = kb.sb("sg", [128, 512], F32); bsg = Buf()
        rqT = kb.sb("rqT", [128, 8, 128], BF16); brqT = Buf()
        rkT = kb.sb("rkT", [128, 4, 128], BF16); brkT = Buf()
        Sd = kb.sb("Sd", [128, 8, 128], BF16); bSd = Buf()
        st_f = kb.sb("st_f", [128, 4, 128], F32); bstf = Buf()
        st_b = kb.sb("st_b", [128, 4, 128], BF16); bstb = Buf()
        kb.op("pool", lambda e: e.memset(st_f[:], 0.0), writes=[bstf])
        o_sb = kb.sb("o_sb", [128, 8, 64], F32); bo = Buf()
        oc = kb.sb("oc", [128, 8, 64], F32); boc = Buf()
        st8 = kb.sb("st8", [128, 16], F32); bst8 = Buf()
        mixcat = kb.sb("mixcat", [128, D], BF16); bmixa = Buf(); bmixy = Buf()
        mixT = kb.sb("mixT", [128, 8, 128], BF16); bmixT = Buf()
        x1 = kb.sb("x1", [128, D], F32); bx1 = Buf()

        tp = [kb.ps("tp%d" % i, [128, 8, 128], BF16) for i in range(2)]; btp = [Buf(), Buf()]
        mm = [kb.ps("mm%d" % i, [128, 512], F32) for i in range(2)]; bmm = [Buf(), Buf()]
        s2 = kb.ps("s2", [128, 2, 512], F32); bs2 = [Buf(), Buf()]
        oo = [kb.ps("oo%d" % i, [128, 512], F32) for i in range(2)]; boo = [Buf(), Buf()]
        xi = 0
        for s in range(nseq):
            cos, sin, brope = self.rope_tables(s, 32, "k_invf32", "b%d" % s)
            for n_, part in (("gmod1", 1), ("shift1", 0), ("gate1", 2), ("gmod2", 4), ("shift2", 3)):
                kb.dma("sp", mods[n_][:], S["MOD"][s, 0, part], reads=[self.db("MOD", (s, 0, part))], writes=[bmod])
            for t in range(self.ntl):
                ti = s * NT + t
                X = x_t[xi % 2]; bX = bx[xi % 2]; xi += 1
                kb.dma("sp", X[:], I["x"][ti * 128:(ti + 1) * 128, :], writes=[bX])
                kb.dma("sp", mixcat[:, 0:512], S["ATT"][ti], reads=[self.db("ATT", ti)], writes=[bmixa])
                self.norm_mod_T(X, bX, mods["gmod1"], mods["shift1"], bmod, tmp, btmp, h_bf, bh, tp[0], btp[0], hT, bhT, ss, bss)
                cb = bc_mid(cos[:, t, :], 8)
                sb_ = bc_mid(sin[:, t, :], 8)
                for gi in range(4):
                    p_ = mm[gi % 2]; bp_ = bmm[gi % 2]
                    for k in range(8):
                        kb.op("pe", lambda e, k=k, gi=gi, p_=p_: e.matmul(p_[:], lhsT=hT[:, k, :], rhs=w_in[:, k, gi * 512:(gi + 1) * 512],
                                                                       start=(k == 0), stop=(k == 7)), reads=[bhT, bw], writes=[bp_])
                    if gi < 2:
                        rw_ = raw[gi]; brw_ = braw[gi]; ro = rr[gi]; bro = brr[gi]
                        kb.op("act", lambda e, p_=p_, rw_=rw_: e.copy(out=rw_[:].rearrange("p h d -> p (h d)"), in_=p_[:]),
                              reads=[bp_], writes=[brw_])
                        x1_ = rw_[:, :, 0:32]; x2_ = rw_[:, :, 32:64]
                        kb.op("dve", lambda e, x1_=x1_: e.tensor_tensor(out=rt[0][:], in0=x1_, in1=cb, op=ALU.mult), reads=[brw_, brope], writes=[brt])
                        kb.op("dve", lambda e, x2_=x2_: e.tensor_tensor(out=rt[1][:], in0=x2_, in1=sb_, op=ALU.mult), reads=[brw_, brope], writes=[brt])
                        kb.op("pool", lambda e, x2_=x2_: e.tensor_tensor(out=rt[2][:], in0=x2_, in1=cb, op=ALU.mult), reads=[brw_, brope], writes=[brt])
                        kb.op("pool", lambda e, x1_=x1_: e.tensor_tensor(out=rt[3][:], in0=x1_, in1=sb_, op=ALU.mult), reads=[brw_, brope], writes=[brt])
                        kb.op("dve", lambda e, ro=ro: e.tensor_tensor(out=ro[:, :, 0:32], in0=rt[0][:], in1=rt[1][:], op=ALU.subtract),
                              reads=[brt], writes=[bro])
                        kb.op("pool", lambda e, ro=ro: e.tensor_tensor(out=ro[:, :, 32:64], in0=rt[2][:], in1=rt[3][:], op=ALU.add),
                              reads=[brt], writes=[bro])
                        if gi == 0:
                            kb.op("act", lambda e, ro=ro: e.copy(out=rq_bf[:], in_=ro[:]), reads=[bro], writes=[brqb])
                            kb.op("pool", lambda e, ro=ro: e.tensor_tensor(out=rqd_bf[:], in0=ro[:], in1=bc_last(qdec[:, :], 64), op=ALU.mult),
                                  reads=[bro, bw], writes=[brqd])
                        else:
                            kb.op("act", lambda e, ro=ro: e.mul(out=rk_bf[:], in_=ro[:], mul=0.125), reads=[bro], writes=[brkb])
                            kb.op("dve", lambda e, ro=ro: e.scalar_tensor_tensor(out=rkd_bf[:], in0=ro[:], scalar=0.125,
                                                                                in1=bc_last(kdec[:, :], 64), op0=ALU.mult, op1=ALU.mult),
                                  reads=[bro, bw], writes=[brkd])
                    elif gi == 2:
                        kb.op("act", lambda e, p_=p_: e.copy(out=v_bf[:].rearrange("p h d -> p (h d)"), in_=p_[:]), reads=[bp_], writes=[bv])
                    else:
                        kb.op("act", lambda e, p_=p_: e.activation(out=sg[:], in_=p_[:], func=AF.Silu), reads=[bp_], writes=[bsg])
                for i in range(4):
                    kb.op("pe", lambda e, i=i: e.transpose(out=tp[1][:, i, :], in_=rq_bf[:, 2 * i:2 * i + 2, :].rearrange("p h d -> p (h d)"),
                                                           identity=self.ident_b[:]), reads=[brqb, self.b_const], writes=[btp[1]])
                for i in range(4):
                    kb.op("pe", lambda e, i=i: e.transpose(out=tp[1][:, 4 + i, :], in_=rqd_bf[:, 2 * i:2 * i + 2, :].rearrange("p h d -> p (h d)"),
                                                           identity=self.ident_b[:]), reads=[brqd, self.b_const], writes=[btp[1]])
                for i in range(4):
                    kb.op("pe", lambda e, i=i: e.transpose(out=tp[0][:, i, :], in_=rk_bf[:, 2 * i:2 * i + 2, :].rearrange("p h d -> p (h d)"),
                                                           identity=self.ident_b[:]), reads=[brkb, self.b_const], writes=[btp[0]])
                kb.op("act", lambda e: e.copy(out=rqT[:], in_=tp[1][:]), reads=[btp[1]], writes=[brqT])
                kb.op("dve", lambda e: e.tensor_copy(out=rkT[:], in_=tp[0][:, 0:4, :]), reads=[btp[0]], writes=[brkT])
                if _STOP <= 1:
                    continue
                for h in range(8):
                    i, o = h // 2, (h % 2) * 64
                    kb.op("pe", lambda e, h=h, i=i, o=o: e.matmul(s2[:, h % 2, i * 128:(i + 1) * 128],
                                                                lhsT=rkT[o:o + 64, i, :], rhs=rqT[o:o + 64, i, :], start=True, stop=True),
                          reads=[brkT, brqT], writes=[bs2[h % 2]])
                for hb in range(2):
                    kb.op("dve", lambda e, hb=hb: e.tensor_tensor(out=Sd[:, hb * 4:(hb + 1) * 4, :].rearrange("p h q -> p (h q)"),
                                                                 in0=s2[:, hb, :], in1=decT[:, hb * 512:(hb + 1) * 512], op=ALU.mult),
                          reads=[bs2[hb], bw], writes=[bSd])
                if _STOP <= 1.5:
                    continue
                for i in range(4):
                    if t > 0:
                        kb.op("pe", lambda e, i=i: e.matmul(oo[0][:, i * 128:(i + 1) * 128], lhsT=rqT[:, 4 + i, :],
                                                            rhs=st_b[:, i, :], start=True, stop=False, skip_group_check=True),
                              reads=[brqT, bstb], writes=[boo[0]])
                    for par in range(2):
                        h = 2 * i + par
                        kb.op("pe", lambda e, h=h, i=i, par=par: e.matmul(oo[0][:, h * 64:(h + 1) * 64], lhsT=Sd[:, par * 4 + i, :],
                                                                        rhs=v_bf[:, h, :], start=(t == 0), stop=(t == 0 or par == 1),
                                                                        skip_group_check=(t > 0)),
                              reads=[bSd, bv], writes=[boo[0]])
                if _STOP <= 2:
                    continue
                for i in range(4):
                    kb.op("pe", lambda e, i=i: e.matmul(oo[1][:, i * 128:(i + 1) * 128],
                                                        lhsT=rkd_bf[:, 2 * i:2 * i + 2, :].rearrange("p h d -> p (h d)"),
                                                        rhs=v_bf[:, 2 * i:2 * i + 2, :].rearrange("p h d -> p (h d)"), start=True, stop=True),
                          reads=[brkd, bv], writes=[boo[1]])
                kvv = oo[1][:].rearrange("p (i c) -> p i c", c=128)
                for half in range(2):
                    po = half * 64
                    if t == 0:
                        kb.op("dve", lambda e, po=po: e.tensor_copy(out=st_f[po:po + 64, :, po:po + 64], in_=kvv[po:po + 64, :, po:po + 64]),
                              reads=[boo[1]], writes=[bstf])
                    else:
                        for i in range(4):
                            kb.op("dve", lambda e, po=po, i=i: e.scalar_tensor_tensor(
                                out=st_f[po:po + 64, i, po:po + 64], in0=st_f[po:po + 64, i, po:po + 64], scalar=cdec[po:po + 64, i:i + 1],
                                in1=kvv[po:po + 64, i, po:po + 64], op0=ALU.mult, op1=ALU.add), reads=[boo[1], bstf, bw], writes=[bstf])
                kb.op("act", lambda e: e.copy(out=st_b[:], in_=st_f[:]), reads=[bstf], writes=[bstb])
                if _STOP <= 3:
                    continue
                kb.op("act", lambda e: e.copy(out=o_sb[:].rearrange("p h d -> p (h d)"), in_=oo[0][:]), reads=[boo[0]], writes=[bo])
                kb.op("dve", lambda e: e.tensor_reduce(out=st8[:, 0:8], in_=o_sb[:], axis=AX.X, op=ALU.add), reads=[bo], writes=[bst8])
                kb.op("dve", lambda e: e.tensor_scalar(out=st8[:, 0:8], in0=st8[:, 0:8], scalar1=-1.0 / 64, scalar2=None, op0=ALU.mult),
                      reads=[bst8], writes=[bst8])
                kb.op("pool", lambda e: e.tensor_tensor(out=oc[:], in0=o_sb[:], in1=bc_last(st8[:, 0:8], 64), op=ALU.add),
                      reads=[bo, bst8], writes=[boc])
                kb.op("pool", lambda e: e.tensor_tensor(out=o_sb[:], in0=oc[:], in1=oc[:], op=ALU.mult), reads=[boc], writes=[bo])
                kb.op("dve", lambda e: e.tensor_reduce(out=st8[:, 8:16], in_=o_sb[:], axis=AX.X, op=ALU.add), reads=[bo], writes=[bst8])
                kb.op("dve", lambda e: e.tensor_scalar(out=st8[:, 8:16], in0=st8[:, 8:16], scalar1=1.0 / 64, scalar2=EPS,
                                                       op0=ALU.mult, op1=ALU.add), reads=[bst8], writes=[bst8])
                kb.op("pool", lambda e: e.tensor_tensor(out=st8[:, 8:16], in0=st8[:, 8:16], in1=self.neghalf[:, 0:8], op=ALU.pow),
                      reads=[bst8, self.b_const], writes=[bst8])
                kb.op("dve", lambda e: e.tensor_tensor(out=oc[:], in0=oc[:], in1=bc_last(st8[:, 8:16], 64), op=ALU.mult),
                      reads=[boc, bst8], writes=[boc])
                kb.op("pool", lambda e: e.tensor_tensor(out=oc[:].rearrange("p h d -> p (h d)"), in0=oc[:].rearrange("p h d -> p (h d)"),
                                                        in1=retg[:], op=ALU.mult), reads=[boc, bw], writes=[boc])
                kb.op("dve", lambda e: e.tensor_tensor(out=mixcat[:, 512:1024], in0=oc[:].rearrange("p h d -> p (h d)"), in1=sg[:],
                                                       op=ALU.mult), reads=[boc, bsg], writes=[bmixy])
                if _STOP <= 4:
                    continue
                for k in range(8):
                    kb.op("pe", lambda e, k=k: e.transpose(out=tp[0][:, k, :], in_=mixcat[:, k * 128:(k + 1) * 128], identity=self.ident_b[:]),
                          reads=[bmixa, bmixy, self.b_const], writes=[btp[0]])
                kb.op("act", lambda e: e.copy(out=mixT[:], in_=tp[0][:]), reads=[btp[0]], writes=[bmixT])
                for half in range(2):
                    for k in range(8):
                        kb.op("pe", lambda e, k=k, half=half: e.matmul(mm[half][:], lhsT=mixT[:, k, :], rhs=w_out[:, k, half * 512:(half + 1) * 512],
                                                                     start=(k == 0), stop=(k == 7)), reads=[bmixT, bw], writes=[bmm[half]])
                    hs = slice(half * 512, (half + 1) * 512)
                    kb.op("dve", lambda e, half=half, hs=hs: e.tensor_tensor(out=tmp[:, hs], in0=mm[half][:], in1=mods["gate1"][:, hs], op=ALU.mult),
                          reads=[bmm[half], bmod], writes=[btmp])
                    kb.op("pool", lambda e, hs=hs: e.tensor_tensor(out=x1[:, hs], in0=tmp[:, hs], in1=X[:, hs], op=ALU.add),
                          reads=[btmp, bX], writes=[bx1])
                kb.dma("sp", S["XA"][ti * 128:(ti + 1) * 128, :], x1[:], reads=[bx1], writes=[self.db("XA", ti)])
                if _STOP <= 5:
                    continue
                self.norm2_router(r, x1, bx1, mods["gmod2"], mods["shift2"], bmod, tmp, btmp, tp, btp, mm[0], bmm[0], ti)
        kb.pop()

    def zero_xg(self):
        kb, S = self.kb, self.S
        z = kb.sb("zeros", [128, 4096], BF16); bz = Buf()
        kb.op("pool", lambda e: e.memset(z[:], 0.0), writes=[bz])
        nrows = 32 * self.cap
        for r0 in range(0, nrows, 512):
            kb.dma("sp", S["XG"][r0:r0 + 512, :].rearrange("(p a) d -> p (a d)", p=128), z[:], reads=[bz])

    def phase2e(self, l):
        kb, I, S = self.kb, self.I, self.S
        kb.push()
        cap = self.cap
        nblk = cap // 512
        wgu = [kb.sb("wgu%d" % i, [128, 8, 2048], BF16) for i in range(2)]
        wdn = [kb.sb("wdn%d" % i, [128, 8, D], BF16) for i in range(2)]
        bgu = [kb.sb("bgu%d" % i, [128, 16], F32) for i in range(2)]
        bdn = [kb.sb("bdn%d" % i, [1, D], BF16) for i in range(2)]
        bwt = [Buf(), Buf()]
        ones1 = kb.sb("ones1", [1, 128], BF16); bones = Buf()
        kb.op("pool", lambda e: e.memset(ones1[:], 1.0), writes=[bones])
        xg = [kb.sb("xg%d" % i, [128, D], BF16) for i in range(3)]; bxg = [Buf() for _ in range(3)]
        xgT = [kb.sb("xgT%d" % i, [128, 8, 512], BF16) for i in range(2)]; bxgT = [Buf(), Buf()]
        glu = [kb.sb("glu%d" % i, [128, 512], F32) for i in range(2)]; bglu = [Buf(), Buf()]
        sig = [kb.sb("sig%d" % i, [128, 512], F32) for i in range(2)]; bsig = [Buf(), Buf()]
        lin = [kb.sb("lin%d" % i, [128, 512], F32) for i in range(2)]; blin = [Buf(), Buf()]
        actT = [kb.sb("actT%d" % i, [128, 8, 512], BF16) for i in range(2)]; bact = [Buf(), Buf()]
        yg = [kb.sb("yg%d" % i, [128, D], F32) for i in range(2)]; byg = [Buf(), Buf()]
        tp = [kb.ps("tp%d" % i, [128, 8, 128], BF16) for i in range(2)]; btp = [Buf(), Buf()]
        pA = [kb.ps("pA%d" % i, [128, 512], F32) for i in range(2)]; bpA = [Buf(), Buf()]
        pB = [kb.ps("pB%d" % i, [128, 512], F32) for i in range(2)]; bpB = [Buf(), Buf()]
        pC = [kb.ps("pC%d" % i, [128, 512], F32) for i in range(2)]; bpC = [Buf(), Buf()]

        def load_expert(e, slot):
            self.load_w_bf16(wgu[slot], I["exp_w_gu"][l, e], 8, bwt[slot])
            self.load_w_bf16(wdn[slot], I["exp_w_down"][l, e], 8, bwt[slot])
            kb.dma("sp", bgu[slot][:], I["exp_b_gu_pj"][l, e], writes=[bwt[slot]])
            kb.op("dve", lambda en, slot=slot: en.tensor_scalar(out=bgu[slot][:, 8:16], in0=bgu[slot][:, 8:16], scalar1=1.0, scalar2=None,
                                                               op0=ALU.add), reads=[bwt[slot]], writes=[bwt[slot]])
            kb.dma("pool", bdn[slot][:], I["exp_b_down"][l, e:e + 1, :], writes=[bwt[slot]])

        load_expert(0, 0)
        xu = tu = pu = cu = yu = bu = 0
        for ex in range(32):
            slot = ex % 2
            if ex + 1 < 32:
                load_expert(ex + 1, (ex + 1) % 2)
            for blk in range(nblk):
                XT = xgT[bu % 2]; bXT = bxgT[bu % 2]
                A = actT[bu % 2]; bA = bact[bu % 2]
                bu += 1
                r0 = ex * cap + blk * 512
                for st in range(4):
                    X = xg[xu % 3]; bX = bxg[xu % 3]; xu += 1
                    kb.dma("sp", X[:], S["XG"][r0 + st * 128:r0 + (st + 1) * 128, :], writes=[bX])
                    T = tp[tu % 2]; bT = btp[tu % 2]; tu += 1
                    for k in range(8):
                        kb.op("pe", lambda e, k=k, X=X, T=T: e.transpose(out=T[:, k, :], in_=X[:, k * 128:(k + 1) * 128],
                                                                       identity=self.ident_b[:]), reads=[bX, self.b_const], writes=[bT])
                    kb.op("act", lambda e, T=T, XT=XT, st=st: e.copy(out=XT[:, :, st * 128:(st + 1) * 128], in_=T[:]),
                          reads=[bT], writes=[bXT])
                for j in range(8):
                    pa = pA[pu % 2]; bpa = bpA[pu % 2]; pb = pB[pu % 2]; bpb = bpB[pu % 2]
                    gl = glu[pu % 2]; bgl = bglu[pu % 2]; sg_ = sig[pu % 2]; bsg_ = bsig[pu % 2]; ln = lin[pu % 2]; bln = blin[pu % 2]
                    pu += 1
                    for k in range(8):
                        kb.op("pe", lambda e, k=k, j=j, pa=pa, slot=slot, XT=XT: e.matmul(
                            pa[:], lhsT=wgu[slot][:, k, j * 128:(j + 1) * 128], rhs=XT[:, k, :],
                            start=(k == 0), stop=(k == 7)), reads=[bwt[slot], bXT], writes=[bpa])
                    for k in range(8):
                        kb.op("pe", lambda e, k=k, j=j, pb=pb, slot=slot, XT=XT: e.matmul(
                            pb[:], lhsT=wgu[slot][:, k, 1024 + j * 128:1024 + (j + 1) * 128], rhs=XT[:, k, :],
                            start=(k == 0), stop=(k == 7)), reads=[bwt[slot], bXT], writes=[bpb])
                    kb.op("dve", lambda e, pa=pa, gl=gl, j=j, slot=slot: e.tensor_scalar(
                        out=gl[:], in0=pa[:], scalar1=bgu[slot][:, j:j + 1], scalar2=7.0, op0=ALU.add, op1=ALU.min),
                        reads=[bpa, bwt[slot]], writes=[bgl])
                    kb.op("act", lambda e, gl=gl, sg_=sg_: e.activation(out=sg_[:], in_=gl[:], func=AF.Sigmoid, scale=1.702),
                          reads=[bgl], writes=[bsg_])
                    kb.op("dve", lambda e, pb=pb, ln=ln, j=j, slot=slot: e.tensor_scalar(
                        out=ln[:], in0=pb[:], scalar1=bgu[slot][:, 8 + j:9 + j], scalar2=8.0, op0=ALU.add, op1=ALU.min),
                        reads=[bpb, bwt[slot]], writes=[bln])
                    kb.op("dve", lambda e, gl=gl, sg_=sg_: e.tensor_tensor(out=gl[:], in0=gl[:], in1=sg_[:], op=ALU.mult),
                          reads=[bgl, bsg_], writes=[bgl])
                    kb.op("dve", lambda e, gl=gl, ln=ln, A=A, j=j: e.scalar_tensor_tensor(out=A[:, j, :], in0=ln[:], scalar=-6.0, in1=gl[:],
                                                                                     op0=ALU.max, op1=ALU.mult),
                          reads=[bgl, bln], writes=[bA])
                for st in range(4):
                    Y = yg[yu % 2]; bY = byg[yu % 2]; yu += 1
                    for half in range(2):
                        pc = pC[cu % 2]; bpc = bpC[cu % 2]; cu += 1
                        for k in range(8):
                            kb.op("pe", lambda e, k=k, st=st, half=half, pc=pc, A=A, slot=slot: e.matmul(
                                pc[:], lhsT=A[:, k, st * 128:(st + 1) * 128], rhs=wdn[slot][:, k, half * 512:(half + 1) * 512],
                                start=(k == 0), stop=False), reads=[bA, bwt[slot]], writes=[bpc])
                        kb.op("pe", lambda e, half=half, pc=pc, slot=slot: e.matmul(
                            pc[:], lhsT=ones1[:, :], rhs=bdn[slot][:, half * 512:(half + 1) * 512], start=False, stop=True),
                            reads=[bones, bwt[slot]], writes=[bpc])
                        if half == 0:
                            kb.op("act", lambda e, pc=pc, Y=Y: e.copy(out=Y[:, 0:512], in_=pc[:]), reads=[bpc], writes=[bY])
                        else:
                            kb.op("dve", lambda e, pc=pc, Y=Y: e.tensor_copy(out=Y[:, 512:1024], in_=pc[:]), reads=[bpc], writes=[bY])
                    kb.dma("sp", S["YG"][r0 + st * 128:r0 + (st + 1) * 128, :], Y[:], reads=[bY])
        kb.pop()

    def phase2c(self, l, src, dst, dst_name, zero_after):
        kb, I, S, nseq = self.kb, self.I, self.S, self.nseq
        kb.push()
        cap = self.cap
        gate2 = kb.sb("gate2", [128, D], F32); bg2 = Buf()
        xin = [kb.sb("xin%d" % i, [128, D], F32) for i in range(2)]; bxin = [Buf(), Buf()]
        acc = [kb.sb("acc%d" % i, [128, D], F32) for i in range(2)]; bacc = [Buf(), Buf()]
        yb = [kb.sb("yb%d" % i, [128, D], F32) for i in range(8)]; byb = [Buf() for _ in range(8)]
        sl = [kb.sb("sl%d" % i, [128, 4], I32) for i in range(2)]; bsl = [Buf(), Buf()]
        gk = [kb.sb("gkc%d" % i, [128, 4], F32) for i in range(2)]; bgk = [Buf(), Buf()]
        for i in range(8):
            kb.op("pool", lambda e, i=i: e.memset(yb[i][:], 0.0), writes=[byb[i]])
        if zero_after:
            self.zero_xg()
        u = 0
        for s in range(nseq):
            kb.dma("sp", gate2[:], S["MOD"][s, l, 5], reads=[self.db("MOD", (s, l, 5))], writes=[bg2])
            for t in range(self.ntl):
                ti = s * NT + t
                X = xin[u % 2]; bX = bxin[u % 2]; A = acc[u % 2]; bA = bacc[u % 2]
                SL = sl[u % 2]; bSL = bsl[u % 2]; GK = gk[u % 2]; bGK = bgk[u % 2]
                kb.dma("sp", X[:], src[ti * 128:(ti + 1) * 128, :], reads=[self.db("XA", ti)], writes=[bX])
                kb.dma("sp", SL[:], S["SLOT"][ti], reads=[self.db("SLOT", ti)], writes=[bSL])
                kb.dma("sp", GK[:], S["GK"][ti], reads=[self.db("GK", ti)], writes=[bGK])
                for k in range(4):
                    Yk = yb[(u % 2) * 4 + k]; bYk = byb[(u % 2) * 4 + k]
                    kb.idma(Yk[:], None, S["YG"], SL[:, k:k + 1], 32 * cap - 1, reads=[bSL], writes=[bYk])
                    if k == 0:
                        kb.op("dve", lambda e, Yk=Yk, A=A, GK=GK: e.tensor_scalar(out=A[:], in0=Yk[:], scalar1=GK[:, 0:1], scalar2=None,
                                                                                 op0=ALU.mult), reads=[bYk, bGK], writes=[bA])
                    else:
                        kb.op("dve", lambda e, Yk=Yk, A=A, GK=GK, k=k: e.scalar_tensor_tensor(
                            out=A[:], in0=Yk[:], scalar=GK[:, k:k + 1], in1=A[:], op0=ALU.mult, op1=ALU.add),
                            reads=[bYk, bGK, bA], writes=[bA])
                kb.op("pool", lambda e, A=A: e.tensor_tensor(out=A[:], in0=A[:], in1=gate2[:], op=ALU.mult), reads=[bA, bg2], writes=[bA])
                kb.op("pool", lambda e, A=A, X=X: e.tensor_tensor(out=X[:], in0=A[:], in1=X[:], op=ALU.add), reads=[bA, bX], writes=[bX])
                kb.dma("sp", dst[ti * 128:(ti + 1) * 128, :], X[:], reads=[bX], writes=[self.db(dst_name, ti)])
                u += 1
        kb.pop()

    def phase3(self):
        kb, I, S, nseq = self.kb, self.I, self.S, self.nseq
        kb.push()
        bw = Buf()
        w_qkv = kb.sb("w_qkv", [128, 8, 1280], BF16)
        w_out = kb.sb("w_out", [128, 8, D], BF16)
        self.load_w_bf16(w_qkv, I["swa_w_qkv"], 8, bw)
        self.load_w_bf16(w_out, I["swa_w_out"], 8, bw)
        bqkv = kb.sb("bqkv", [128, 1280], F32)
        bout = kb.sb("bout", [128, D], F32)
        gq = kb.sb("gq", [128, 64], F32)
        gk = kb.sb("gk", [128, 64], F32)
        sk = kb.sb("sk", [128, 16], F32)
        self.bcast_load(bqkv[:], I["swa_b_qkv"], bw)
        self.bcast_load(bout[:], I["swa_b_out"], bw)
        self.bcast_load(gq[:], I["swa_q_head_g"], bw)
        self.bcast_load(gk[:], I["swa_k_head_g"], bw)
        self.bcast_load(sk[:], I["swa_sinks"], bw)
        kb.op("act", lambda e: e.activation(out=sk[:], in_=sk[:], func=AF.Exp), reads=[bw], writes=[bw])
        mask2 = kb.sb("mask2", [128, 4, 2, 128], BF16)
        for hh in range(4):
            kb.op("pool", lambda e, hh=hh: e.tensor_copy(out=mask2[:, hh, 0, :], in_=self.mask_gt[:]), reads=[self.b_const], writes=[bw])
            kb.op("pool", lambda e, hh=hh: e.tensor_copy(out=mask2[:, hh, 1, :], in_=self.mask_le[:]), reads=[self.b_const], writes=[bw])
        r = self.alloc_router(1)

        mods = {n: kb.sb(n, [128, D], F32) for n in ("gmod1", "shift1", "gate1", "gmod2", "shift2")}
        bmod = Buf()
        x_t = [kb.sb("x_t%d" % i, [128, D], F32) for i in range(2)]; bx = [Buf(), Buf()]
        tmp = kb.sb("tmp", [128, D], F32); btmp = Buf()
        h_bf = kb.sb("h_bf", [128, D], BF16); bh = Buf()
        hT = kb.sb("hT", [128, 8, 128], BF16); bhT = Buf()
        ss = kb.sb("ss", [128, 4], F32); bss = Buf()
        qkv = kb.sb("qkv", [128, 20, 64], F32); bqkvs = Buf()
        sq = kb.sb("sq", [128, 18, 64], F32); bsq = Buf()
        r18 = kb.sb("r18", [128, 18], F32); br18 = Buf()
        qn = kb.sb("qn", [128, 18, 64], F32); bqn = Buf()
        rt = [kb.sb("rt%d" % i, [128, 18, 32], F32) for i in range(4)]; brt = Buf()
        q_bf = kb.sb("q_bf", [128, 16, 64], BF16); bqb = Buf()
        kdup = kb.sb("kdup", [128, 2, 2, 64], BF16); bkd = Buf()
        qT = kb.sb("qT", [128, 8, 128], BF16); bqT = Buf()
        kT = [kb.sb("kT%d" % i, [128, 2, 128], BF16) for i in range(2)]; bkT = [Buf(), Buf()]
        Va = [kb.sb("Va%d" % i, [128, 2, 65], BF16) for i in range(2)]; bVa = [Buf(), Buf()]
        bVones = Buf()
        for i in range(2):
            kb.op("pool", lambda e, i=i: e.memset(Va[i][:, :, 64:65], 1.0), writes=[bVones])
        PT = [kb.sb("PT%d" % i, [128, 4, 2, 128], BF16) for i in range(2)]; bPT = [Buf(), Buf()]
        den = kb.sb("den", [128, 16], F32); bden = Buf()
        attn = kb.sb("attn", [128, 16, 64], BF16); battn = Buf()
        attT = kb.sb("attT", [128, 8, 128], BF16); battT = Buf()
        x1 = kb.sb("x1", [128, D], F32); bx1 = Buf()

        tp = [kb.ps("tp%d" % i, [128, 8, 128], BF16) for i in range(2)]; btp = [Buf(), Buf()]
        mm = [kb.ps("mm%d" % i, [128, 512], F32) for i in range(2)]; bmm = [Buf(), Buf()]
        s2 = kb.ps("s2", [128, 2, 512], F32); bs2 = [Buf(), Buf()]
        oo = [kb.ps("oo%d" % i, [128, 512], F32) for i in range(2)]; boo = [Buf(), Buf()]
        xi = 0
        gu_ = 0
        for s in range(nseq):
            cos, sin, brope = self.rope_tables(s, 32, "k_invf32", "c%d" % s)
            for n_, part in (("gmod1", 1), ("shift1", 0), ("gate1", 2), ("gmod2", 4), ("shift2", 3)):
                kb.dma("sp", mods[n_][:], S["MOD"][s, 1, part], reads=[self.db("MOD", (s, 1, part))], writes=[bmod])
            for t in range(self.ntl):
                ti = s * NT + t
                cur, prv = t % 2, (t + 1) % 2
                X = x_t[xi % 2]; bX = bx[xi % 2]; xi += 1
                kb.dma("sp", X[:], S["XB"][ti * 128:(ti + 1) * 128, :], reads=[self.db("XB", ti)], writes=[bX])
                self.norm_mod_T(X, bX, mods["gmod1"], mods["shift1"], bmod, tmp, btmp, h_bf, bh, tp[0], btp[0], hT, bhT, ss, bss)
                qkvf = qkv[:].rearrange("p h d -> p (h d)")
                for gi, (c0, c1) in enumerate(((0, 512), (512, 1024), (1024, 1280))):
                    p_ = mm[gi % 2]; bp_ = bmm[gi % 2]
                    for k in range(8):
                        kb.op("pe", lambda e, k=k, c0=c0, c1=c1, p_=p_: e.matmul(p_[:, 0:c1 - c0], lhsT=hT[:, k, :], rhs=w_qkv[:, k, c0:c1],
                                                                              start=(k == 0), stop=(k == 7)), reads=[bhT, bw], writes=[bp_])
                    kb.op("dve", lambda e, c0=c0, c1=c1, p_=p_: e.tensor_tensor(out=qkvf[:, c0:c1], in0=p_[:, 0:c1 - c0], in1=bqkv[:, c0:c1],
                                                                              op=ALU.add), reads=[bp_, bw], writes=[bqkvs])
                kb.op("pool", lambda e: e.tensor_tensor(out=sq[:], in0=qkv[:, 0:18, :], in1=qkv[:, 0:18, :], op=ALU.mult), reads=[bqkvs], writes=[bsq])
                kb.op("dve", lambda e: e.tensor_reduce(out=r18[:], in_=sq[:], axis=AX.X, op=ALU.add), reads=[bsq], writes=[br18])
                kb.op("dve", lambda e: e.tensor_scalar(out=r18[:], in0=r18[:], scalar1=1.0 / 64, scalar2=EPS, op0=ALU.mult, op1=ALU.add),
                      reads=[br18], writes=[br18])
                kb.op("pool", lambda e: e.tensor_tensor(out=r18[:, 0:16], in0=r18[:, 0:16], in1=self.neghalf[:, 0:16], op=ALU.pow),
                      reads=[br18, self.b_const], writes=[br18])
                kb.op("pool", lambda e: e.tensor_tensor(out=r18[:, 16:18], in0=r18[:, 16:18], in1=self.neghalf[:, 0:2], op=ALU.pow),
                      reads=[br18, self.b_const], writes=[br18])
                kb.op("dve", lambda e: e.tensor_tensor(out=qn[:], in0=qkv[:, 0:18, :], in1=bc_last(r18[:, :], 64), op=ALU.mult),
                      reads=[bqkvs, br18], writes=[bqn])
                kb.op("pool", lambda e: e.tensor_tensor(out=qn[:, 0:16, :], in0=qn[:, 0:16, :], in1=bc_mid(gq[:, :], 16), op=ALU.mult),
                      reads=[bqn, bw], writes=[bqn])
                kb.op("pool", lambda e: e.tensor_tensor(out=qn[:, 16:18, :], in0=qn[:, 16:18, :], in1=bc_mid(gk[:, :], 2), op=ALU.mult),
                      reads=[bqn, bw], writes=[bqn])
                cb = bc_mid(cos[:, t, :], 18)
                sb_ = bc_mid(sin[:, t, :], 18)
                x1_ = qn[:, :, 0:32]; x2_ = qn[:, :, 32:64]
                kb.op("dve", lambda e: e.tensor_tensor(out=rt[0][:], in0=x1_, in1=cb, op=ALU.mult), reads=[bqn, brope], writes=[brt])
                kb.op("dve", lambda e: e.tensor_tensor(out=rt[1][:], in0=x2_, in1=sb_, op=ALU.mult), reads=[bqn, brope], writes=[brt])
                kb.op("pool", lambda e: e.tensor_tensor(out=rt[2][:], in0=x2_, in1=cb, op=ALU.mult), reads=[bqn, brope], writes=[brt])
                kb.op("pool", lambda e: e.tensor_tensor(out=rt[3][:], in0=x1_, in1=sb_, op=ALU.mult), reads=[bqn, brope], writes=[brt])
                kb.op("dve", lambda e: e.tensor_tensor(out=q_bf[:, :, 0:32], in0=rt[0][:, 0:16, :], in1=rt[1][:, 0:16, :], op=ALU.subtract),
                      reads=[brt], writes=[bqb])
                kb.op("pool", lambda e: e.tensor_tensor(out=q_bf[:, :, 32:64], in0=rt[2][:, 0:16, :], in1=rt[3][:, 0:16, :], op=ALU.add),
                      reads=[brt], writes=[bqb])
                for dup in range(2):
                    kb.op("dve", lambda e, dup=dup: e.tensor_tensor(out=kdup[:, :, dup, 0:32], in0=rt[0][:, 16:18, :], in1=rt[1][:, 16:18, :],
                                                                   op=ALU.subtract), reads=[brt], writes=[bkd])
                    kb.op("pool", lambda e, dup=dup: e.tensor_tensor(out=kdup[:, :, dup, 32:64], in0=rt[2][:, 16:18, :], in1=rt[3][:, 16:18, :],
                                                                    op=ALU.add), reads=[brt], writes=[bkd])
                kb.op("act", lambda e, cur=cur: e.copy(out=Va[cur][:, :, 0:64], in_=qkv[:, 18:20, :]), reads=[bqkvs, bVones], writes=[bVa[cur]])
                for i in range(8):
                    kb.op("pe", lambda e, i=i: e.transpose(out=tp[1][:, i, :], in_=q_bf[:, 2 * i:2 * i + 2, :].rearrange("p h d -> p (h d)"),
                                                           identity=self.ident_b[:]), reads=[bqb, self.b_const], writes=[btp[1]])
                kb.op("act", lambda e: e.copy(out=qT[:], in_=tp[1][:]), reads=[btp[1]], writes=[bqT])
                for g in range(2):
                    kb.op("pe", lambda e, g=g: e.transpose(out=tp[0][:, g, :], in_=kdup[:, g, :, :].rearrange("p a d -> p (a d)"),
                                                           identity=self.ident_b[:]), reads=[bkd, self.b_const], writes=[btp[0]])
                kb.op("dve", lambda e, cur=cur: e.tensor_copy(out=kT[cur][:], in_=tp[0][:, 0:2, :]), reads=[btp[0]], writes=[bkT[cur]])
                for gq4 in range(4):
                    sbank = s2
                    bsb = bs2
                    P = PT[gu_ % 2]; bP = bPT[gu_ % 2]
                    ob = oo[gu_ % 2]; bob = boo[gu_ % 2]
                    gu_ += 1
                    for hh in range(4):
                        hq = gq4 * 4 + hh
                        i, o = hq // 2, (hq % 2) * 64
                        g = hq // 8
                        for w_, kt in ((0, prv), (1, cur)):
                            if t == 0 and w_ == 0:
                                continue
                            col = ((hh % 2) * 2 + hh // 2) * 256 + w_ * 128
                            kb.op("pe", lambda e, i=i, o=o, g=g, kt=kt, col=col, sbank=sbank: e.matmul(
                                sbank[:, col // 512, col % 512:col % 512 + 128], lhsT=kT[kt][o:o + 64, g, :], rhs=qT[o:o + 64, i, :],
                                start=True, stop=True), reads=[bkT[kt], bqT], writes=[bsb[col // 512]])
                    for bk in range(2):
                        if t == 0:
                            for hh2 in range(2):
                                kb.op("act", lambda e, bk=bk, hh2=hh2, P=P, sbank=sbank: e.activation(
                                    out=P[:, bk * 2 + hh2, 1, :], in_=sbank[:, bk, hh2 * 256 + 128:hh2 * 256 + 256], func=AF.Exp, scale=0.125),
                                    reads=[bsb[bk]], writes=[bP])
                        else:
                            kb.op("act", lambda e, bk=bk, P=P, sbank=sbank: e.activation(
                                out=P[:, bk * 2:bk * 2 + 2, :, :].rearrange("p a b c -> p (a b c)"), in_=sbank[:, bk, :], func=AF.Exp, scale=0.125),
                                reads=[bsb[bk]], writes=[bP])
                    if t == 0:
                        kb.op("dve", lambda e, P=P: e.tensor_tensor(out=P[:, :, 1, :], in0=P[:, :, 1, :], in1=mask2[:, :, 1, :], op=ALU.mult),
                              reads=[bP, bw], writes=[bP])
                    else:
                        kb.op("dve", lambda e, P=P: e.tensor_tensor(out=P[:].rearrange("p a b c -> p (a b c)"), in0=P[:].rearrange("p a b c -> p (a b c)"),
                                                                   in1=mask2[:].rearrange("p a b c -> p (a b c)"), op=ALU.mult),
                              reads=[bP, bw], writes=[bP])
                    for hh in range(4):
                        hq = gq4 * 4 + hh
                        g = hq // 8
                        sl = (hh % 2) * 2 + hh // 2
                        if t > 0:
                            kb.op("pe", lambda e, hh=hh, g=g, P=P, ob=ob, prv=prv, sl=sl: e.matmul(ob[:, hh * 65:hh * 65 + 65], lhsT=P[:, sl, 0, :],
                                                                                          rhs=Va[prv][:, g, :], start=True, stop=False),
                                  reads=[bP, bVa[prv]], writes=[bob])
                        kb.op("pe", lambda e, hh=hh, g=g, P=P, ob=ob, cur=cur, sl=sl: e.matmul(ob[:, hh * 65:hh * 65 + 65], lhsT=P[:, sl, 1, :],
                                                                                      rhs=Va[cur][:, g, :], start=(t == 0), stop=True),
                              reads=[bP, bVa[cur]], writes=[bob])
                    ov = ob[:, 0:260].rearrange("p (h d) -> p h d", d=65)
                    dsl = den[:, gq4 * 4:(gq4 + 1) * 4]
                    kb.op("dve", lambda e, ov=ov, dsl=dsl, gq4=gq4: e.tensor_tensor(out=dsl, in0=ov[:, :, 64], in1=sk[:, gq4 * 4:(gq4 + 1) * 4], op=ALU.add),
                          reads=[bob, bw], writes=[bden])
                    kb.op("dve", lambda e, dsl=dsl: e.reciprocal(out=dsl, in_=dsl), reads=[bden], writes=[bden])
                    kb.op("dve", lambda e, ov=ov, dsl=dsl, gq4=gq4: e.tensor_tensor(out=attn[:, gq4 * 4:(gq4 + 1) * 4, :], in0=ov[:, :, 0:64],
                                                                                 in1=bc_last(dsl, 64), op=ALU.mult), reads=[bob, bden], writes=[battn])
                af = attn[:].rearrange("p h d -> p (h d)")
                for k in range(8):
                    kb.op("pe", lambda e, k=k: e.transpose(out=tp[0][:, k, :], in_=af[:, k * 128:(k + 1) * 128], identity=self.ident_b[:]),
                          reads=[battn, self.b_const], writes=[btp[0]])
                kb.op("act", lambda e: e.copy(out=attT[:], in_=tp[0][:]), reads=[btp[0]], writes=[battT])
                for half in range(2):
                    hs = slice(half * 512, (half + 1) * 512)
                    for k in range(8):
                        kb.op("pe", lambda e, k=k, half=half, hs=hs: e.matmul(mm[half][:], lhsT=attT[:, k, :], rhs=w_out[:, k, hs],
                                                                            start=(k == 0), stop=(k == 7)), reads=[battT, bw], writes=[bmm[half]])
                    kb.op("dve", lambda e, half=half, hs=hs: e.tensor_tensor(out=tmp[:, hs], in0=mm[half][:], in1=bout[:, hs], op=ALU.add),
                          reads=[bmm[half], bw], writes=[btmp])
                    kb.op("pool", lambda e, hs=hs: e.tensor_tensor(out=tmp[:, hs], in0=tmp[:, hs], in1=mods["gate1"][:, hs], op=ALU.mult),
                          reads=[btmp, bmod], writes=[btmp])
                    kb.op("pool", lambda e, hs=hs: e.tensor_tensor(out=x1[:, hs], in0=tmp[:, hs], in1=X[:, hs], op=ALU.add),
                          reads=[btmp, bX], writes=[bx1])
                kb.dma("sp", S["XA"][ti * 128:(ti + 1) * 128, :], x1[:], reads=[bx1], writes=[self.db("XA", ti)])
                self.norm2_router(r, x1, bx1, mods["gmod2"], mods["shift2"], bmod, tmp, btmp, tp, btp, mm[0], bmm[0], ti)
        kb.pop()

    def build(self):
        self.setup_consts()
        ph = self.phases
        if "p0" in ph:
            self.phase0()
        if "p1a" in ph:
            self.phase1a()
        if "p1b" in ph:
            self.phase1b()
        if "p2a" in ph:
            self.phase2e(0)
            self.phase2c(0, self.S["XA"], self.S["XB"], "XB", True)
        if "p3" in ph:
            self.phase3()
        if "p2b" in ph:
            self.phase2e(1)
            self.phase2c(1, self.S["XA"], self.out, "OUT", False)
        self.kb.finish()
        return self.nc


def module_consts():
    idx = np.arange(128, dtype=np.float64)
    lg = np.log1p(-np.exp2(-5.0 - np.arange(8, dtype=np.float64)))
    k = {}
    k["k_invf16"] = (10000.0 ** (-np.arange(16, dtype=np.float32) / 16)).astype(np.float32)
    k["k_invf32"] = (10000.0 ** (-np.arange(32, dtype=np.float32) / 32)).astype(np.float32)
    diff = idx[None, :] - idx[:, None]
    dec = np.where(diff[:, None, :] >= 0, np.exp(lg[None, :, None] * np.maximum(diff[:, None, :], 0.0)), 0.0)
    k["k_decayT"] = np.ascontiguousarray(dec.reshape(128, 4, 2, 128).transpose(0, 2, 1, 3)).reshape(128, 8 * 128).astype(np.float32)
    k["k_qdec"] = np.exp(lg[None, :] * (idx + 1.0)[:, None]).astype(np.float32)
    k["k_kdec"] = np.exp(lg[None, :] * (127.0 - idx)[:, None]).astype(np.float32)
    cd = np.zeros((128, 4), np.float64)
    for i in range(4):
        cd[0:64, i] = np.exp(lg[2 * i] * 128)
        cd[64:128, i] = np.exp(lg[2 * i + 1] * 128)
    k["k_cdec"] = cd.astype(np.float32)
    return k


def make_in_maps(inputs, nseq, n_cores):
    f = lambda a: np.ascontiguousarray(np.asarray(a))
    shared = {}
    for name in ("ada_w", "ada_b", "norm1_g", "norm2_g", "router_w", "router_b", "exp_w_gu", "exp_w_down", "exp_b_down"):
        shared[name] = f(inputs[name])
    for name in ("hyb_w_in", "mla_cq_norm_g", "mla_ckv_norm_g", "mla_w_uq", "mla_w_ukv", "mla_q_head_g", "mla_k_head_g",
                 "hyb_w_out", "swa_w_qkv", "swa_b_qkv", "swa_q_head_g", "swa_k_head_g", "swa_sinks", "swa_w_out", "swa_b_out"):
        shared[name] = f(np.asarray(inputs[name])[0])
    shared["ret_norm_g"] = f(np.asarray(inputs["ret_norm_g"])[0].reshape(512))
    bgu = np.asarray(inputs["exp_b_gu"])
    shared["exp_b_gu_pj"] = f(bgu.reshape(2, 32, 16, 128).transpose(0, 1, 3, 2))
    shared.update(module_consts())
    x = np.asarray(inputs["x"]); c = np.asarray(inputs["c"]); pos = np.asarray(inputs["positions"])
    maps = []
    for i in range(n_cores):
        b0 = i * nseq
        m = dict(shared)
        m["x"] = f(x[b0:b0 + nseq].reshape(nseq * SEQ, D))
        m["c_pk"] = f(c[b0:b0 + nseq].reshape(nseq, 8, 128).transpose(0, 2, 1))
        m["pos_pt"] = f(pos[b0:b0 + nseq].reshape(nseq, NT, 128).transpose(0, 2, 1).astype(np.int32))
        maps.append(m)
    return maps


_PROG = {}


def kernel(**inputs):
    nseq = 32 // N_CORES
    if "nc" not in _PROG:
        _PROG["nc"] = Prog(nseq).build()
    maps = make_in_maps(inputs, nseq, N_CORES)
    res = run_bass_kernel_spmd(_PROG["nc"], maps, core_ids=list(range(N_CORES)))
    out = np.concatenate([np.asarray(r["out"]).reshape(nseq, SEQ, D) for r in res.results], axis=0)
    return out.astype(np.float32)
```

```python
import contextlib
import os
import math
import numpy as np
import concourse.bass as bass
import concourse.mybir as mybir
from concourse.bass_utils import run_bass_kernel_spmd

F32 = mybir.dt.float32
BF16 = mybir.dt.bfloat16
I32 = mybir.dt.int32
AF = mybir.ActivationFunctionType
ALU = mybir.AluOpType
AX = mybir.AxisListType

SAME_ENGINE_SYNC = os.environ.get('KSES', '1') == '1'
DMA_RING = 6
N_CORES = 8
_STOP = float(os.environ.get('KSTOP', '99'))
SEQ = 2048
D = 1024
NT = SEQ // 128
EPS = 1e-6
PI = math.pi


class Buf:
    __slots__ = ("w", "rs")

    def __init__(self):
        self.w = None
        self.rs = {}


class KB:
    ENGS = ("pe", "act", "dve", "pool", "sp")

    def __init__(self, nc):
        self.nc = nc
        self.stacks = [contextlib.ExitStack()]
        self.eng = dict(pe=nc.tensor, act=nc.scalar, dve=nc.vector, pool=nc.gpsimd, sp=nc.sync)
        self.cnt = {e: 0 for e in self.ENGS}
        self.seen = {e: {} for e in self.ENGS}
        self.sems = {}
        for e in self.ENGS:
            self.sems[e] = self.stacks[0].enter_context(nc.semaphore("s_" + e))
        self.dma_n = {}
        for e in ("sp", "pool", "act"):
            self.dma_n[e] = 0
            for j in range(DMA_RING):
                self.sems[("d", e, j)] = self.stacks[0].enter_context(nc.semaphore("d_%s_%d" % (e, j)))
        self.uid = 0

    def push(self):
        self.stacks.append(contextlib.ExitStack())

    def pop(self):
        self.barrier()
        self.stacks.pop().close()

    def sb(self, name, shape, dt):
        self.uid += 1
        return self.stacks[-1].enter_context(self.nc.sbuf_tensor("%s_%d" % (name, self.uid), list(shape), dt))

    def ps(self, name, shape, dt):
        self.uid += 1
        return self.stacks[-1].enter_context(self.nc.psum_tensor("%s_%d" % (name, self.uid), list(shape), dt))

    def _deps(self, e, reads, writes):
        toks = {}
        for b in reads:
            if b.w is not None and toks.get(b.w[0], 0) < b.w[1]:
                toks[b.w[0]] = b.w[1]
        for b in writes:
            if b.w is not None and toks.get(b.w[0], 0) < b.w[1]:
                toks[b.w[0]] = b.w[1]
            for k, v in b.rs.items():
                if toks.get(k, 0) < v:
                    toks[k] = v
        waits = []
        seen = self.seen[e]
        for k, v in toks.items():
            if k == e and (e == "pe" or not SAME_ENGINE_SYNC):
                continue
            if seen.get(k, 0) < v:
                seen[k] = v
                waits.append((k, v))
        return waits

    def _mark(self, tok, reads, writes):
        k, v = tok
        for b in reads:
            if b.rs.get(k, 0) < v:
                b.rs[k] = v
        for b in writes:
            b.w = tok
            b.rs = {}

    def _emit(self, e, waits, fn, key, inc):
        engine = self.eng[e]
        for k, v in waits:
            engine.wait_ge(self.sems[k], v)
        if fn is not None:
            fn(engine).then_inc(self.sems[key], inc)

    def op(self, e, fn, reads=(), writes=()):
        waits = self._deps(e, reads, writes)
        self.cnt[e] += 1
        tok = (e, self.cnt[e])
        self._emit(e, waits, fn, e, 1)
        self._mark(tok, reads, writes)
        return tok

    def dma(self, e, out, in_, reads=(), writes=(), **kw):
        waits = self._deps(e, reads, writes)
        n = self.dma_n[e]
        self.dma_n[e] += 1
        key = ("d", e, n % DMA_RING)
        val = 16 * (n // DMA_RING + 1)
        if n >= DMA_RING and self.seen[e].get(key, 0) < val - 16:
            self.seen[e][key] = val - 16
            waits.append((key, val - 16))
        tok = (key, val)
        self._emit(e, waits, (lambda eng: eng.dma_start(out=out, in_=in_, **kw)), key, 16)
        self._mark(tok, reads, writes)
        return tok

    def idma(self, out, out_idx, in_, in_idx, bound, reads=(), writes=()):
        e = "pool"
        waits = self._deps(e, reads, writes)
        n = self.dma_n[e]
        self.dma_n[e] += 1
        key = ("d", e, n % DMA_RING)
        val = 16 * (n // DMA_RING + 1)
        if n >= DMA_RING and self.seen[e].get(key, 0) < val - 16:
            self.seen[e][key] = val - 16
            waits.append((key, val - 16))
        if not hasattr(self, "_bregs"):
            self._bregs = {}
        if bound not in self._bregs:
            self._bregs[bound] = self.nc.gpsimd.to_reg(bound)
        bound = self._bregs[bound]
        oo_ = bass.IndirectOffsetOnAxis(ap=out_idx, axis=0) if out_idx is not None else None
        io_ = bass.IndirectOffsetOnAxis(ap=in_idx, axis=0) if in_idx is not None else None
        self._emit(e, waits, (lambda eng: eng.indirect_dma_start(out=out, out_offset=oo_, in_=in_, in_offset=io_,
                                                                 bounds_check=bound, oob_is_err=False)), key, 16)
        self._mark((key, val), reads, writes)

    def all_tokens(self):
        toks = [(e, self.cnt[e]) for e in self.ENGS if self.cnt[e] > 0]
        for e in ("sp", "pool", "act"):
            n = self.dma_n[e]
            for j in range(DMA_RING):
                c = (n - j + DMA_RING - 1) // DMA_RING if n > j else 0
                if c > 0:
                    toks.append((("d", e, j), 16 * c))
        return toks

    def barrier(self):
        toks = self.all_tokens()
        for e in self.ENGS:
            waits = []
            for k, v in toks:
                if k == e:
                    continue
                if self.seen[e].get(k, 0) < v:
                    self.seen[e][k] = v
                    waits.append((k, v))
            self._emit(e, waits, None, None, 0)

    def finish(self):
        self.barrier()
        while self.stacks:
            self.stacks.pop().close()


def bc_mid(ap2, n):
    return ap2.unsqueeze(1).broadcast_to([ap2.shape[0], n, ap2.shape[1]])


def bc_last(ap2, n):
    return ap2.unsqueeze(2).broadcast_to([ap2.shape[0], ap2.shape[1], n])


class Prog:
    def __init__(self, nseq, debug=False, phases=("p0", "p1a", "p1b", "p2a", "p3", "p2b"), ntl=NT, cap_tiles=None):
        self.nseq = nseq
        self.ntl = ntl
        if cap_tiles is None:
            mean = nseq * ntl * 128 * 4 // 32
            cap_tiles = max(4, 4 * ((2 * mean + 511) // 512))
        self.cap = cap_tiles * 128
        self.debug = debug
        self.phases = phases
        nc = self.nc = bass.Bass("TRN2", target_bir_lowering=False)
        self.kb = KB(nc)
        ntok = nseq * SEQ
        self.ntok = ntok

        def inp(name, shape, dt=F32):
            return nc.dram_tensor(name, list(shape), dt, kind="ExternalInput").ap()

        def scr(name, shape, dt=F32):
            kind = "ExternalOutput" if debug else "Internal"
            return nc.dram_tensor(name, list(shape), dt, kind=kind).ap()

        I = self.I = {}
        I["x"] = inp("x", [ntok, D])
        I["c_pk"] = inp("c_pk", [nseq, 128, 8])
        I["pos_pt"] = inp("pos_pt", [nseq, 128, NT], I32)
        I["ada_w"] = inp("ada_w", [2, D, 6 * D])
        I["ada_b"] = inp("ada_b", [2, 6 * D])
        I["norm1_g"] = inp("norm1_g", [2, D])
        I["norm2_g"] = inp("norm2_g", [2, D])
        I["hyb_w_in"] = inp("hyb_w_in", [D, 2720])
        I["mla_cq_norm_g"] = inp("mla_cq_norm_g", [384])
        I["mla_ckv_norm_g"] = inp("mla_ckv_norm_g", [256])
        I["mla_w_uq"] = inp("mla_w_uq", [384, 768])
        I["mla_w_ukv"] = inp("mla_w_ukv", [256, 1024])
        I["mla_q_head_g"] = inp("mla_q_head_g", [96])
        I["mla_k_head_g"] = inp("mla_k_head_g", [96])
        I["ret_norm_g"] = inp("ret_norm_g", [512])
        I["hyb_w_out"] = inp("hyb_w_out", [D, D])
        I["swa_w_qkv"] = inp("swa_w_qkv", [D, 1280])
        I["swa_b_qkv"] = inp("swa_b_qkv", [1280])
        I["swa_q_head_g"] = inp("swa_q_head_g", [64])
        I["swa_k_head_g"] = inp("swa_k_head_g", [64])
        I["swa_sinks"] = inp("swa_sinks", [16])
        I["swa_w_out"] = inp("swa_w_out", [D, D])
        I["swa_b_out"] = inp("swa_b_out", [D])
        I["router_w"] = inp("router_w", [2, D, 32])
        I["router_b"] = inp("router_b", [2, 32])
        I["exp_w_gu"] = inp("exp_w_gu", [2, 32, D, 2048])
        I["exp_b_gu_pj"] = inp("exp_b_gu_pj", [2, 32, 128, 16])
        I["exp_w_down"] = inp("exp_w_down", [2, 32, D, D])
        I["exp_b_down"] = inp("exp_b_down", [2, 32, D])
        I["k_invf16"] = inp("k_invf16", [16])
        I["k_invf32"] = inp("k_invf32", [32])
        I["k_decayT"] = inp("k_decayT", [128, 8 * 128])
        I["k_qdec"] = inp("k_qdec", [128, 8])
        I["k_kdec"] = inp("k_kdec", [128, 8])
        I["k_cdec"] = inp("k_cdec", [128, 4])

        S = self.S = {}
        S["MOD"] = scr("MOD", [nseq, 2, 6, 128, D])
        S["ATT"] = scr("ATT", [nseq * NT, 128, 512], BF16)
        S["XA"] = scr("XA", [ntok, D])
        S["XB"] = scr("XB", [ntok, D])
        S["H2T"] = scr("H2T", [nseq * NT, 128, 8, 128], BF16)
        S["GS"] = scr("GS", [nseq * NT, 128, 32])
        S["XG"] = scr("XG", [32 * self.cap, D], BF16)
        S["YG"] = scr("YG", [32 * self.cap, D])
        S["SLOT"] = scr("SLOT", [nseq * NT, 128, 4], I32)
        S["GK"] = scr("GK", [nseq * NT, 128, 4])
        self.out = nc.dram_tensor("out", [ntok, D], F32, kind="ExternalOutput").ap()
        self.dbufs = {}

    def db(self, name, idx):
        k = (name, idx)
        if k not in self.dbufs:
            self.dbufs[k] = Buf()
        return self.dbufs[k]

    def setup_consts(self):
        kb = self.kb
        self.ident_b = kb.sb("ident_b", [128, 128], BF16)
        self.ident_f = kb.sb("ident_f", [128, 128], F32)
        self.b_const = Buf()
        bc = self.b_const
        for idt in (self.ident_b, self.ident_f):
            kb.op("pool", lambda e, idt=idt: e.memset(idt[:], 1.0), writes=[bc])
            kb.op("pool", lambda e, idt=idt: e.affine_select(out=idt[:], in_=idt[:], pattern=[[-1, 128]],
                                                             compare_op=ALU.is_equal, fill=0.0, base=0,
                                                             channel_multiplier=1), reads=[bc], writes=[bc])
        self.mask_le = kb.sb("mask_le", [128, 128], BF16)
        self.mask_gt = kb.sb("mask_gt", [128, 128], BF16)
        kb.op("pool", lambda e: e.memset(self.mask_le[:], 1.0), writes=[bc])
        kb.op("pool", lambda e: e.affine_select(out=self.mask_le[:], in_=self.mask_le[:], pattern=[[1, 128]],
                                                compare_op=ALU.is_ge, fill=0.0, base=0, channel_multiplier=-1),
              reads=[bc], writes=[bc])
        kb.op("pool", lambda e: e.memset(self.mask_gt[:], 1.0), writes=[bc])
        kb.op("pool", lambda e: e.affine_select(out=self.mask_gt[:], in_=self.mask_gt[:], pattern=[[-1, 128]],
                                                compare_op=ALU.is_gt, fill=0.0, base=0, channel_multiplier=1),
              reads=[bc], writes=[bc])
        self.U_b = kb.sb("U_b", [128, 128], BF16)
        self.ones_b = kb.sb("ones_b", [128, 128], BF16)
        kb.op("pool", lambda e: e.memset(self.ones_b[:], 1.0), writes=[bc])
        kb.op("pool", lambda e: e.memset(self.U_b[:], 1.0), writes=[bc])
        kb.op("pool", lambda e: e.affine_select(out=self.U_b[:], in_=self.U_b[:], pattern=[[1, 128]],
                                                compare_op=ALU.is_ge, fill=0.0, base=-1, channel_multiplier=-1),
              reads=[bc], writes=[bc])
        iot_i = kb.sb("iot_i", [128, 32], I32)
        self.iotaE = kb.sb("iotaE", [128, 32], F32)
        kb.op("pool", lambda e: e.iota(out=iot_i[:], pattern=[[1, 32]], base=0, channel_multiplier=0), writes=[bc])
        kb.op("dve", lambda e: e.tensor_copy(out=self.iotaE[:], in_=iot_i[:]), reads=[bc], writes=[bc])
        kb.op("dve", lambda e: e.tensor_scalar(out=self.iotaE[:], in0=self.iotaE[:], scalar1=float(self.cap), scalar2=None,
                                               op0=ALU.mult), reads=[bc], writes=[bc])
        self.neghalf = kb.sb("neghalf", [128, 16], F32)
        kb.op("pool", lambda e: e.memset(self.neghalf[:], -0.5), writes=[bc])

    def rstd_of(self, ss, n, width, bss, tag):
        kb = self.kb
        kb.op("dve", lambda e: e.tensor_scalar(out=ss, in0=ss, scalar1=1.0 / n, scalar2=EPS,
                                               op0=ALU.mult, op1=ALU.add), reads=[bss], writes=[bss])
        kb.op("pool", lambda e: e.tensor_tensor(out=ss, in0=ss, in1=self.neghalf[:, 0:width], op=ALU.pow),
              reads=[bss, self.b_const], writes=[bss])

    def rope_tables(self, s, half, invf_name, tag):
        kb, I = self.kb, self.I
        b = Buf()
        pos_i = kb.sb("pos_i" + tag, [128, NT], I32)
        pos_f = kb.sb("pos_f" + tag, [128, NT], F32)
        invf = kb.sb("invf" + tag, [128, half], F32)
        ang = kb.sb("ang" + tag, [128, NT, half], F32)
        kq = kb.sb("kq" + tag, [128, NT, half], F32)
        ki = kb.sb("ki" + tag, [128, NT, half], I32)
        ys = kb.sb("ys" + tag, [128, NT, half], F32)
        mm = kb.sb("mmk" + tag, [128, NT, half], F32)
        cos = kb.sb("cos" + tag, [128, NT, half], F32)
        sin = kb.sb("sin" + tag, [128, NT, half], F32)
        kb.dma("sp", pos_i[:], I["pos_pt"][s], writes=[b])
        kb.dma("sp", invf[:], I[invf_name].partition_broadcast(128), writes=[b])
        kb.op("dve", lambda e: e.tensor_copy(out=pos_f[:], in_=pos_i[:]), reads=[b], writes=[b])
        kb.op("dve", lambda e: e.tensor_tensor(out=ang[:], in0=bc_last(pos_f[:, :], half), in1=bc_mid(invf[:, :], NT),
                                               op=ALU.mult), reads=[b], writes=[b])
        kb.op("dve", lambda e: e.tensor_scalar(out=kq[:], in0=ang[:], scalar1=1.0 / (2 * PI), scalar2=None,
                                               op0=ALU.mult), reads=[b], writes=[b])
        kb.op("dve", lambda e: e.tensor_copy(out=ki[:], in_=kq[:]), reads=[b], writes=[b])
        kb.op("dve", lambda e: e.tensor_copy(out=kq[:], in_=ki[:]), reads=[b], writes=[b])
        kb.op("dve", lambda e: e.scalar_tensor_tensor(out=ang[:], in0=kq[:], scalar=-2 * PI, in1=ang[:],
                                                      op0=ALU.mult, op1=ALU.add), reads=[b], writes=[b])
        lim = 3.1415925
        for shift, dst in ((0.0, sin), (PI / 2, cos)):
            kb.op("dve", lambda e, shift=shift: e.tensor_scalar(out=ys[:], in0=ang[:], scalar1=shift, scalar2=None,
                                                                op0=ALU.add), reads=[b], writes=[b])
            kb.op("dve", lambda e: e.tensor_scalar(out=mm[:], in0=ys[:], scalar1=PI, scalar2=-2 * PI,
                                                   op0=ALU.is_gt, op1=ALU.mult), reads=[b], writes=[b])
            kb.op("dve", lambda e: e.tensor_tensor(out=ys[:], in0=ys[:], in1=mm[:], op=ALU.add), reads=[b], writes=[b])
            kb.op("dve", lambda e: e.tensor_scalar(out=ys[:], in0=ys[:], scalar1=lim, scalar2=-lim,
                                                   op0=ALU.min, op1=ALU.max), reads=[b], writes=[b])
            kb.op("act", lambda e, dst=dst: e.activation(out=dst[:], in_=ys[:], func=AF.Sin), reads=[b], writes=[b])
        return cos, sin, b

    def load_w_bf16(self, dst, src, kchunks, bw):
        v = src.rearrange("(k p) n -> p k n", p=128)
        for k in range(kchunks):
            self.kb.dma("pool", dst[:, k, :], v[:, k, :], writes=[bw])

    def bcast_load(self, dst, src1d, b):
        self.kb.dma("sp", dst, src1d.partition_broadcast(128), writes=[b])

    def norm_mod_T(self, x_t, bx, gmod, shift, bmod, tmp, btmp, h_bf, bh, tp, btp, hT, bhT, ss, bss):
        kb = self.kb
        kb.op("dve", lambda e: e.scalar_tensor_tensor(out=tmp[:], in0=x_t[:], scalar=1.0, in1=x_t[:], op0=ALU.mult, op1=ALU.mult, accum_out=ss[:, 0:1]),
              reads=[bx], writes=[btmp, bss])
        self.rstd_of(ss[:, 0:1], D, 1, bss, "")
        kb.op("dve", lambda e: e.scalar_tensor_tensor(out=tmp[:], in0=x_t[:], scalar=ss[:, 0:1], in1=gmod[:],
                                                      op0=ALU.mult, op1=ALU.mult), reads=[bx, bss, bmod], writes=[btmp])
        kb.op("pool", lambda e: e.tensor_tensor(out=h_bf[:], in0=tmp[:], in1=shift[:], op=ALU.add),
              reads=[btmp, bmod], writes=[bh])
        for k in range(8):
            kb.op("pe", lambda e, k=k: e.transpose(out=tp[:, k, :], in_=h_bf[:, k * 128:(k + 1) * 128],
                                                   identity=self.ident_b[:]), reads=[bh, self.b_const], writes=[btp])
        kb.op("act", lambda e: e.copy(out=hT[:], in_=tp[:]), reads=[btp], writes=[bhT])

    def phase0(self):
        kb, I, S, nseq = self.kb, self.I, self.S, self.nseq
        kb.push()
        cin = kb.sb("cin", [128, nseq, 8], F32)
        cact = kb.sb("cact", [128, nseq, 8], F32)
        crep = kb.sb("crep", [128, nseq, 8, 128], F32)
        bcr = Buf()
        for s in range(nseq):
            kb.dma("sp", cin[:, s, :], I["c_pk"][s], writes=[bcr])
        kb.op("act", lambda e: e.activation(out=cact[:], in_=cin[:], func=AF.Silu), reads=[bcr], writes=[bcr])
        for s in range(nseq):
            kb.op("dve", lambda e, s=s: e.tensor_copy(out=crep[:, s, :, :], in_=bc_last(cact[:, s, :], 128)),
                  reads=[bcr], writes=[bcr])
        adab = kb.sb("adab", [128, 6 * D], F32)
        ng = [kb.sb("ng1", [128, D], F32), kb.sb("ng2", [128, D], F32)]
        bab = Buf()
        wch = [kb.sb("wch%d" % i, [128, 8, 512], F32) for i in range(2)]
        bwch = [Buf(), Buf()]
        pm = [kb.ps("p0pm%d" % i, [128, 512], F32) for i in range(2)]
        bpm = [Buf(), Buf()]
        modt = [kb.sb("modt%d" % i, [128, 512], F32) for i in range(3)]
        bmodt = [Buf() for _ in range(3)]
        n = 0
        u = 0
        for l in range(2):
            for q in range(6):
                kb.dma("sp", adab[:, q * D:(q + 1) * D], I["ada_b"][l, q * D:(q + 1) * D].partition_broadcast(128),
                       writes=[bab])
            self.bcast_load(ng[0][:], I["norm1_g"][l], bab)
            self.bcast_load(ng[1][:], I["norm2_g"][l], bab)
            for j in range(12):
                wv = I["ada_w"][l][:, j * 512:(j + 1) * 512].rearrange("(k p) n -> p k n", p=128)
                w_ = wch[n % 2]
                kb.dma("sp", w_[:], wv, writes=[bwch[n % 2]])
                for s in range(nseq):
                    p_ = pm[u % 2]
                    for k in range(8):
                        kb.op("pe", lambda e, k=k, s=s, p_=p_, w_=w_: e.matmul(p_[:], lhsT=crep[:, s, k, :], rhs=w_[:, k, :],
                                                                             start=(k == 0), stop=(k == 7)),
                              reads=[bcr, bwch[n % 2]], writes=[bpm[u % 2]])
                    m_ = modt[u % 3]
                    bm_ = bmodt[u % 3]
                    kb.op("dve", lambda e, p_=p_, m_=m_, j=j: e.tensor_tensor(out=m_[:], in0=p_[:],
                                                                           in1=adab[:, j * 512:(j + 1) * 512], op=ALU.add),
                          reads=[bpm[u % 2], bab], writes=[bm_])
                    part, half = j // 2, j % 2
                    if part in (1, 4):
                        g_ = ng[0] if part == 1 else ng[1]
                        kb.op("dve", lambda e, m_=m_, g_=g_, half=half: e.scalar_tensor_tensor(
                            out=m_[:], in0=m_[:], scalar=1.0, in1=g_[:, half * 512:(half + 1) * 512],
                            op0=ALU.add, op1=ALU.mult), reads=[bm_, bab], writes=[bm_])
                    kb.dma("sp", S["MOD"][s, l, part, :, half * 512:(half + 1) * 512], m_[:],
                           reads=[bm_], writes=[self.db("MOD", (s, l, part))])
                    u += 1
                n += 1
        kb.pop()

    def phase1a(self):
        kb, I, S, nseq = self.kb, self.I, self.S, self.nseq
        kb.push()
        self.zero_xg()
        bw = Buf()
        w_in = kb.sb("w_in_a", [128, 8, 672], BF16)
        w_uq = kb.sb("w_uq", [128, 3, 768], BF16)
        w_ukv = kb.sb("w_ukv", [128, 2, 1024], BF16)
        self.load_w_bf16(w_in, I["hyb_w_in"][:, 0:672], 8, bw)
        self.load_w_bf16(w_uq, I["mla_w_uq"], 3, bw)
        self.load_w_bf16(w_ukv, I["mla_w_ukv"], 2, bw)
        gcq = kb.sb("gcq", [128, 384], F32)
        gckv = kb.sb("gckv", [128, 256], F32)
        gq = kb.sb("gq", [128, 96], F32)
        gk = kb.sb("gk", [128, 96], F32)
        self.bcast_load(gcq[:], I["mla_cq_norm_g"], bw)
        self.bcast_load(gckv[:], I["mla_ckv_norm_g"], bw)
        self.bcast_load(gq[:], I["mla_q_head_g"], bw)
        self.bcast_load(gk[:], I["mla_k_head_g"], bw)

        kT = kb.sb("kT", [96, 8, SEQ], BF16)
        V = kb.sb("Vc", [128, NT, 8, 65], BF16)
        bkT = [Buf() for _ in range(NT)]
        bV = [Buf() for _ in range(NT)]
        bVones = Buf()
        kb.op("pool", lambda e: e.memset(V[:, :, :, 64:65], 1.0), writes=[bVones])

        gmod = kb.sb("gmod1", [128, D], F32)
        shift = kb.sb("shift1", [128, D], F32)
        bmod = Buf()
        x_t = [kb.sb("x_t%d" % i, [128, D], F32) for i in range(2)]
        bx = [Buf(), Buf()]
        tmp = kb.sb("tmp", [128, D], F32); btmp = Buf()
        h_bf = kb.sb("h_bf", [128, D], BF16); bh = Buf()
        hT = kb.sb("hT", [128, 8, 128], BF16); bhT = Buf()
        ss = kb.sb("ss", [128, 4], F32); bss = Buf()
        proj = kb.sb("proj", [128, 672], F32); bproj = Buf()
        sq = kb.sb("sq", [128, 8, 96], F32); bsq = Buf()
        cqn = kb.sb("cqn", [128, 384], BF16); bcqn = Buf()
        cqT = kb.sb("cqT", [128, 3, 128], BF16); bcqT = Buf()
        ckvn = kb.sb("ckvn", [128, 256], BF16); bckvn = Buf()
        ckvT = kb.sb("ckvT", [128, 2, 128], BF16); bckvT = Buf()
        q_sb = kb.sb("q_sb", [128, 8, 96], F32); bq = Buf()
        qn = kb.sb("qn", [128, 8, 96], F32); bqn = Buf()
        rq8 = kb.sb("rq8", [128, 16], F32); brq8 = Buf()
        R = kb.sb("Rr", [128, 8, 96], F32); bR = Buf()
        q_full = kb.sb("q_full", [128, 8, 96], BF16); bqf = Buf()
        qT = kb.sb("qT", [96, 8, 128], BF16); bqT = Buf()
        kv_sb = kb.sb("kv_sb", [128, 8, 128], F32); bkv = Buf()
        k_full = kb.sb("k_full", [128, 8, 96], BF16); bkf = Buf()
        kr = kb.sb("kr", [128, 32], F32); bkr = Buf()
        kr2 = kb.sb("kr2", [128, 32], F32)
        rt = [kb.sb("rt%d" % i, [128, 8, 16], F32) for i in range(4)]; brt = Buf()
        PT = [kb.sb("PT%d" % i, [128, 4, 128], BF16) for i in range(3)]; bPT = [Buf() for _ in range(3)]
        attn = kb.sb("attn", [128, 8, 64], BF16); battn = Buf()
        rden = kb.sb("rden", [128, 8], F32); brden = Buf()

        tp = [kb.ps("tp%d" % i, [128, 8, 128], BF16) for i in range(2)]; btp = [Buf(), Buf()]
        mm = [kb.ps("mm%d" % i, [128, 512], F32) for i in range(2)]; bmm = [Buf(), Buf()]
        s2 = kb.ps("s2", [128, 2, 512], F32); bs2 = [Buf(), Buf()]
        oo = [kb.ps("oo%d" % i, [128, 512], F32) for i in range(2)]; boo = [Buf(), Buf()]
        scale = 96.0 ** -0.5
        xi = 0
        ucount = 0
        for s in range(nseq):
            cos, sin, brope = self.rope_tables(s, 16, "k_invf16", "a%d" % s)
            kb.dma("sp", gmod[:], S["MOD"][s, 0, 1], reads=[self.db("MOD", (s, 0, 1))], writes=[bmod])
            kb.dma("sp", shift[:], S["MOD"][s, 0, 0], reads=[self.db("MOD", (s, 0, 0))], writes=[bmod])
            for t in range(self.ntl):
                X = x_t[xi % 2]; bX = bx[xi % 2]; xi += 1
                kb.dma("sp", X[:], I["x"][(s * NT + t) * 128:(s * NT + t + 1) * 128, :], writes=[bX])
                self.norm_mod_T(X, bX, gmod, shift, bmod, tmp, btmp, h_bf, bh, tp[0], btp[0], hT, bhT, ss, bss)
                for gi, (c0, c1) in enumerate(((0, 512), (512, 672))):
                    for k in range(8):
                        kb.op("pe", lambda e, k=k, gi=gi, c0=c0, c1=c1: e.matmul(mm[gi][:, 0:c1 - c0], lhsT=hT[:, k, :],
                                                                              rhs=w_in[:, k, c0:c1], start=(k == 0), stop=(k == 7)),
                              reads=[bhT, bw], writes=[bmm[gi]])
                    kb.op("act", lambda e, gi=gi, c0=c0, c1=c1: e.copy(out=proj[:, c0:c1], in_=mm[gi][:, 0:c1 - c0]),
                          reads=[bmm[gi]], writes=[bproj])
                for ci, (c0, w) in enumerate(((0, 384), (384, 256), (640, 32))):
                    kb.op("dve", lambda e, c0=c0, w=w, ci=ci: e.scalar_tensor_tensor(out=tmp[:, 0:w], in0=proj[:, c0:c0 + w], scalar=1.0, in1=proj[:, c0:c0 + w], op0=ALU.mult, op1=ALU.mult, accum_out=ss[:, 1 + ci:2 + ci]), reads=[bproj], writes=[btmp, bss])
                kb.op("dve", lambda e: e.tensor_scalar(out=ss[:, 1:2], in0=ss[:, 1:2], scalar1=1.0 / 384, scalar2=EPS,
                                                       op0=ALU.mult, op1=ALU.add), reads=[bss], writes=[bss])
                kb.op("dve", lambda e: e.tensor_scalar(out=ss[:, 2:3], in0=ss[:, 2:3], scalar1=1.0 / 256, scalar2=EPS,
                                                       op0=ALU.mult, op1=ALU.add), reads=[bss], writes=[bss])
                kb.op("dve", lambda e: e.tensor_scalar(out=ss[:, 3:4], in0=ss[:, 3:4], scalar1=1.0 / 32, scalar2=EPS,
                                                       op0=ALU.mult, op1=ALU.add), reads=[bss], writes=[bss])
                kb.op("pool", lambda e: e.tensor_tensor(out=ss[:, 1:4], in0=ss[:, 1:4], in1=self.neghalf[:, 0:3], op=ALU.pow),
                      reads=[bss, self.b_const], writes=[bss])
                kb.op("dve", lambda e: e.scalar_tensor_tensor(out=cqn[:], in0=proj[:, 0:384], scalar=ss[:, 1:2], in1=gcq[:],
                                                              op0=ALU.mult, op1=ALU.mult), reads=[bproj, bss, bw], writes=[bcqn])
                kb.op("dve", lambda e: e.scalar_tensor_tensor(out=ckvn[:], in0=proj[:, 384:640], scalar=ss[:, 2:3], in1=gckv[:],
                                                              op0=ALU.mult, op1=ALU.mult), reads=[bproj, bss, bw], writes=[bckvn])
                kb.op("dve", lambda e: e.scalar_tensor_tensor(out=kr[:], in0=proj[:, 640:672], scalar=ss[:, 3:4], in1=gk[:, 64:96],
                                                              op0=ALU.mult, op1=ALU.mult), reads=[bproj, bss, bw], writes=[bkr])
                for k in range(3):
                    kb.op("pe", lambda e, k=k: e.transpose(out=tp[1][:, k, :], in_=cqn[:, k * 128:(k + 1) * 128],
                                                           identity=self.ident_b[:]), reads=[bcqn, self.b_const], writes=[btp[1]])
                for k in range(2):
                    kb.op("pe", lambda e, k=k: e.transpose(out=tp[1][:, 3 + k, :], in_=ckvn[:, k * 128:(k + 1) * 128],
                                                           identity=self.ident_b[:]), reads=[bckvn, self.b_const], writes=[btp[1]])
                kb.op("act", lambda e: e.copy(out=cqT[:], in_=tp[1][:, 0:3, :]), reads=[btp[1]], writes=[bcqT])
                kb.op("act", lambda e: e.copy(out=ckvT[:], in_=tp[1][:, 3:5, :]), reads=[btp[1]], writes=[bckvT])
                for gi, (c0, c1) in enumerate(((0, 512), (512, 768))):
                    for k in range(3):
                        kb.op("pe", lambda e, k=k, gi=gi, c0=c0, c1=c1: e.matmul(mm[gi][:, 0:c1 - c0], lhsT=cqT[:, k, :],
                                                                              rhs=w_uq[:, k, c0:c1], start=(k == 0), stop=(k == 2)),
                              reads=[bcqT, bw], writes=[bmm[gi]])
                    kb.op("act", lambda e, gi=gi, c0=c0, c1=c1: e.copy(
                        out=q_sb[:].rearrange("p h d -> p (h d)")[:, c0:c1], in_=mm[gi][:, 0:c1 - c0]),
                        reads=[bmm[gi]], writes=[bq])
                kb.op("pool", lambda e: e.tensor_tensor(out=sq[:], in0=q_sb[:], in1=q_sb[:], op=ALU.mult), reads=[bq], writes=[bsq])
                kb.op("dve", lambda e: e.tensor_reduce(out=rq8[:, 0:8], in_=sq[:, :, 0:64], axis=AX.X, op=ALU.add),
                      reads=[bsq], writes=[brq8])
                kb.op("dve", lambda e: e.tensor_reduce(out=rq8[:, 8:16], in_=sq[:, :, 64:96], axis=AX.X, op=ALU.add),
                      reads=[bsq], writes=[brq8])
                kb.op("dve", lambda e: e.tensor_scalar(out=rq8[:, 0:8], in0=rq8[:, 0:8], scalar1=1.0 / 64, scalar2=EPS,
                                                       op0=ALU.mult, op1=ALU.add), reads=[brq8], writes=[brq8])
                kb.op("dve", lambda e: e.tensor_scalar(out=rq8[:, 8:16], in0=rq8[:, 8:16], scalar1=1.0 / 32, scalar2=EPS,
                                                       op0=ALU.mult, op1=ALU.add), reads=[brq8], writes=[brq8])
                kb.op("pool", lambda e: e.tensor_tensor(out=rq8[:], in0=rq8[:], in1=self.neghalf[:, 0:16], op=ALU.pow),
                      reads=[brq8, self.b_const], writes=[brq8])
                kb.op("dve", lambda e: e.tensor_tensor(out=qn[:, :, 0:64], in0=q_sb[:, :, 0:64], in1=bc_last(rq8[:, 0:8], 64),
                                                       op=ALU.mult), reads=[bq, brq8], writes=[bqn])
                kb.op("dve", lambda e: e.tensor_tensor(out=qn[:, :, 64:96], in0=q_sb[:, :, 64:96], in1=bc_last(rq8[:, 8:16], 32),
                                                       op=ALU.mult), reads=[bq, brq8], writes=[bqn])
                kb.op("pool", lambda e: e.tensor_tensor(out=qn[:], in0=qn[:], in1=bc_mid(gq[:, :], 8), op=ALU.mult),
                      reads=[bqn, bw], writes=[bqn])
                kb.op("act", lambda e: e.copy(out=q_full[:, :, 0:64], in_=qn[:, :, 0:64]), reads=[bqn], writes=[bqf])
                cb = bc_mid(cos[:, t, :], 8)
                sb_ = bc_mid(sin[:, t, :], 8)
                x1 = qn[:, :, 64:80]
                x2 = qn[:, :, 80:96]
                kb.op("dve", lambda e: e.tensor_tensor(out=rt[0][:], in0=x1, in1=cb, op=ALU.mult), reads=[bqn, brope], writes=[brt])
                kb.op("dve", lambda e: e.tensor_tensor(out=rt[1][:], in0=x2, in1=sb_, op=ALU.mult), reads=[bqn, brope], writes=[brt])
                kb.op("pool", lambda e: e.tensor_tensor(out=rt[2][:], in0=x2, in1=cb, op=ALU.mult), reads=[bqn, brope], writes=[brt])
                kb.op("pool", lambda e: e.tensor_tensor(out=rt[3][:], in0=x1, in1=sb_, op=ALU.mult), reads=[bqn, brope], writes=[brt])
                kb.op("dve", lambda e: e.tensor_tensor(out=q_full[:, :, 64:80], in0=rt[0][:], in1=rt[1][:], op=ALU.subtract),
                      reads=[brt], writes=[bqf])
                kb.op("dve", lambda e: e.tensor_tensor(out=q_full[:, :, 80:96], in0=rt[2][:], in1=rt[3][:], op=ALU.add),
                      reads=[brt], writes=[bqf])
                for h in range(8):
                    kb.op("pe", lambda e, h=h: e.transpose(out=tp[0][0:96, h, :], in_=q_full[:, h, :], identity=self.ident_b[:]),
                          reads=[bqf, self.b_const], writes=[btp[0]])
                kb.op("act", lambda e: e.copy(out=qT[:], in_=tp[0][0:96, :, :]), reads=[btp[0]], writes=[bqT])
                for gi in range(2):
                    for k in range(2):
                        kb.op("pe", lambda e, k=k, gi=gi: e.matmul(mm[gi][:], lhsT=ckvT[:, k, :], rhs=w_ukv[:, k, gi * 512:(gi + 1) * 512],
                                                                 start=(k == 0), stop=(k == 1)), reads=[bckvT, bw], writes=[bmm[gi]])
                    kb.op("act", lambda e, gi=gi: e.copy(out=kv_sb[:].rearrange("p h d -> p (h d)")[:, gi * 512:(gi + 1) * 512],
                                                        in_=mm[gi][:]), reads=[bmm[gi]], writes=[bkv])
                kb.op("pool", lambda e, t=t: e.tensor_copy(out=V[:, t, :, 0:64], in_=kv_sb[:, :, 64:128]),
                      reads=[bkv, bVones], writes=[bV[t]])
                kb.op("pool", lambda e: e.tensor_tensor(out=sq[:, :, 0:64], in0=kv_sb[:, :, 0:64], in1=kv_sb[:, :, 0:64], op=ALU.mult),
                      reads=[bkv], writes=[bsq])
                kb.op("dve", lambda e: e.tensor_reduce(out=rq8[:, 0:8], in_=sq[:, :, 0:64], axis=AX.X, op=ALU.add),
                      reads=[bsq], writes=[brq8])
                kb.op("dve", lambda e: e.tensor_scalar(out=rq8[:, 0:8], in0=rq8[:, 0:8], scalar1=1.0 / 64, scalar2=EPS,
                                                       op0=ALU.mult, op1=ALU.add), reads=[brq8], writes=[brq8])
                kb.op("pool", lambda e: e.tensor_tensor(out=rq8[:, 0:8], in0=rq8[:, 0:8], in1=self.neghalf[:, 0:8], op=ALU.pow),
                      reads=[brq8, self.b_const], writes=[brq8])
                kb.op("dve", lambda e: e.tensor_tensor(out=sq[:, :, 0:64], in0=kv_sb[:, :, 0:64], in1=bc_last(rq8[:, 0:8], 64),
                                                       op=ALU.mult), reads=[bkv, brq8], writes=[bsq])
                kb.op("pool", lambda e: e.tensor_tensor(out=k_full[:, :, 0:64], in0=sq[:, :, 0:64], in1=bc_mid(gk[:, 0:64], 8),
                                                        op=ALU.mult), reads=[bsq, bw], writes=[bkf])
                c1_ = cos[:, t, :]
                s1_ = sin[:, t, :]
                kb.op("dve", lambda e: e.tensor_tensor(out=rt[0][:, 0, :], in0=kr[:, 0:16], in1=c1_, op=ALU.mult), reads=[bkr, brope], writes=[brt])
                kb.op("dve", lambda e: e.tensor_tensor(out=rt[1][:, 0, :], in0=kr[:, 16:32], in1=s1_, op=ALU.mult), reads=[bkr, brope], writes=[brt])
                kb.op("dve", lambda e: e.tensor_tensor(out=rt[2][:, 0, :], in0=kr[:, 16:32], in1=c1_, op=ALU.mult), reads=[bkr, brope], writes=[brt])
                kb.op("dve", lambda e: e.tensor_tensor(out=rt[3][:, 0, :], in0=kr[:, 0:16], in1=s1_, op=ALU.mult), reads=[bkr, brope], writes=[brt])
                kb.op("dve", lambda e: e.tensor_tensor(out=kr2[:, 0:16], in0=rt[0][:, 0, :], in1=rt[1][:, 0, :], op=ALU.subtract),
                      reads=[brt], writes=[bkr])
                kb.op("dve", lambda e: e.tensor_tensor(out=kr2[:, 16:32], in0=rt[2][:, 0, :], in1=rt[3][:, 0, :], op=ALU.add),
                      reads=[brt], writes=[bkr])
                kb.op("dve", lambda e: e.tensor_copy(out=k_full[:, :, 64:96], in_=bc_mid(kr2[:, :], 8)), reads=[bkr], writes=[bkf])
                for h in range(8):
                    kb.op("pe", lambda e, h=h: e.transpose(out=tp[1][0:96, h, :], in_=k_full[:, h, :], identity=self.ident_b[:]),
                          reads=[bkf, self.b_const], writes=[btp[1]])
                kb.op("act", lambda e, t=t: e.copy(out=kT[:, :, t * 128:(t + 1) * 128], in_=tp[1][0:96, :, :]),
                      reads=[btp[1]], writes=[bkT[t]])
                units = []
                for h in range(8):
                    for a in range(0, t + 1, 4):
                        units.append((h, a, min(a + 4, t + 1)))

                def emit_S(ui, u):
                    h, a, b = u
                    bank = ui % 2
                    for kt in range(a, b):
                        kb.op("pe", lambda e, kt=kt, h=h, a=a, bank=bank: e.matmul(
                            s2[:, bank, (kt - a) * 128:(kt - a + 1) * 128], lhsT=kT[:, h, kt * 128:(kt + 1) * 128],
                            rhs=qT[:, h, :], start=True, stop=True), reads=[bkT[kt], bqT], writes=[bs2[bank]])

                base = ucount
                emit_S(base, units[0])
                for i, u in enumerate(units):
                    ui = base + i
                    h, a, b = u
                    if i + 1 < len(units):
                        emit_S(ui + 1, units[i + 1])
                    bank = ui % 2
                    P = PT[ui % 3]; bP = bPT[ui % 3]
                    n = (b - a) * 128
                    kb.op("act", lambda e, P=P, bank=bank, n=n: e.activation(
                        out=P[:].rearrange("p a b -> p (a b)")[:, 0:n], in_=s2[:, bank, 0:n], func=AF.Exp, scale=scale),
                        reads=[bs2[bank]], writes=[bP])
                    if b == t + 1:
                        kb.op("dve", lambda e, P=P, j=t - a: e.tensor_tensor(out=P[:, j, :], in0=P[:, j, :], in1=self.mask_le[:],
                                                                           op=ALU.mult), reads=[bP, self.b_const], writes=[bP])
                    ob = oo[h // 4]
                    for kt in range(a, b):
                        kb.op("pe", lambda e, kt=kt, h=h, a=a, P=P, ob=ob: e.matmul(
                            ob[:, (h % 4) * 65:(h % 4) * 65 + 65], lhsT=P[:, kt - a, :], rhs=V[:, kt, h, :],
                            start=(kt == 0), stop=(kt == t)), reads=[bP, bV[kt], bVones], writes=[boo[h // 4]])
                ucount += len(units)
                for hb in range(2):
                    ov = oo[hb][:, 0:260].rearrange("p (h d) -> p h d", d=65)
                    kb.op("dve", lambda e, hb=hb, ov=ov: e.reciprocal(out=rden[:, hb * 4:(hb + 1) * 4], in_=ov[:, :, 64]),
                          reads=[boo[hb]], writes=[brden])
                    kb.op("dve", lambda e, hb=hb, ov=ov: e.tensor_tensor(out=attn[:, hb * 4:(hb + 1) * 4, :], in0=ov[:, :, 0:64],
                                                                        in1=bc_last(rden[:, hb * 4:(hb + 1) * 4], 64), op=ALU.mult),
                          reads=[boo[hb], brden], writes=[battn])
                kb.dma("sp", S["ATT"][s * NT + t], attn[:].rearrange("p h d -> p (h d)"), reads=[battn],
                       writes=[self.db("ATT", s * NT + t)])
        kb.pop()

    def alloc_router(self, l):
        kb, I = self.kb, self.I
        r = {}
        r["bw"] = Buf()
        r["rw"] = kb.sb("rw", [128, 8, 32], F32)
        kb.dma("sp", r["rw"][:], I["router_w"][l].rearrange("(k p) n -> p k n", p=128), writes=[r["bw"]])
        r["rb"] = kb.sb("rb", [128, 32], F32)
        self.bcast_load(r["rb"][:], I["router_b"][l], r["bw"])
        r["rwh"] = kb.sb("rwh", [128, 8, 32], BF16)
        r["rwl"] = kb.sb("rwl", [128, 8, 32], BF16)
        kb.op("dve", lambda e: e.tensor_copy(out=r["rwh"][:], in_=r["rw"][:]), reads=[r["bw"]], writes=[r["bw"]])
        kb.op("dve", lambda e: e.tensor_tensor(out=r["rwl"][:], in0=r["rw"][:], in1=r["rwh"][:], op=ALU.subtract),
              reads=[r["bw"]], writes=[r["bw"]])
        r["h2f"] = kb.sb("h2f", [128, D], F32); r["bh2f"] = Buf()
        r["h2hi"] = kb.sb("h2hi", [128, D], BF16); r["bh2hi"] = Buf()
        r["h2lo"] = kb.sb("h2lo", [128, D], BF16); r["bh2lo"] = Buf()
        r["h2Tl"] = kb.sb("h2Tl", [128, 8, 128], BF16); r["bh2Tl"] = Buf()
        r["h2Tb"] = kb.sb("h2Tb", [128, 8, 128], BF16); r["bh2Tb"] = Buf()
        r["lg"] = kb.sb("lg", [128, 32], F32); r["blg"] = Buf()
        r["m8"] = kb.sb("m8", [128, 8], F32)
        r["msk"] = kb.sb("msk", [128, 32], F32)
        r["ex"] = kb.sb("ex", [128, 32], F32)
        r["den"] = kb.sb("den", [128, 2], F32)
        r["G"] = kb.sb("Gt", [128, 32], F32); r["bG"] = Buf()
        r["ss"] = kb.sb("ss2", [128, 1], F32); r["bss"] = Buf()
        r["cnt"] = kb.sb("cnt_b", [128, 32], F32); r["bcnt"] = Buf()
        kb.op("pool", lambda e: e.memset(r["cnt"][:], 0.0), writes=[r["bcnt"]])
        r["Mb"] = kb.sb("Mb", [128, 32], BF16)
        for n_ in ("posf", "valid", "slotm", "oh", "junk", "Gv"):
            r[n_] = kb.sb(n_, [128, 32], F32)
        r["slotf"] = kb.sb("slotf", [128, 4], F32)
        r["sloti"] = kb.sb("sloti", [128, 4], I32); r["bsloti"] = Buf()
        r["gk"] = kb.sb("gk", [128, 4], F32); r["bgk"] = Buf()
        r["brt"] = Buf()
        return r

    def norm2_router(self, r, x1, bx1, gmod2, shift2, bmod, tmp, btmp, tp, btp, mmp, bmmp, tile_idx):
        kb, S = self.kb, self.S
        ss, bss = r["ss"], r["bss"]
        kb.op("dve", lambda e: e.scalar_tensor_tensor(out=tmp[:], in0=x1[:], scalar=1.0, in1=x1[:], op0=ALU.mult, op1=ALU.mult, accum_out=ss[:, 0:1]),
              reads=[bx1], writes=[btmp, bss])
        self.rstd_of(ss[:, 0:1], D, 1, bss, "")
        kb.op("dve", lambda e: e.scalar_tensor_tensor(out=tmp[:], in0=x1[:], scalar=ss[:, 0:1], in1=gmod2[:],
                                                      op0=ALU.mult, op1=ALU.mult), reads=[bx1, bss, bmod], writes=[btmp])
        kb.op("pool", lambda e: e.tensor_tensor(out=r["h2f"][:], in0=tmp[:], in1=shift2[:], op=ALU.add),
              reads=[btmp, bmod], writes=[r["bh2f"]])
        kb.op("act", lambda e: e.copy(out=r["h2hi"][:], in_=r["h2f"][:]), reads=[r["bh2f"]], writes=[r["bh2hi"]])
        kb.op("dve", lambda e: e.tensor_tensor(out=r["h2lo"][:], in0=r["h2f"][:], in1=r["h2hi"][:], op=ALU.subtract),
              reads=[r["bh2f"], r["bh2hi"]], writes=[r["bh2lo"]])
        for k in range(8):
            kb.op("pe", lambda e, k=k: e.transpose(out=tp[0][:, k, :], in_=r["h2hi"][:, k * 128:(k + 1) * 128],
                                                   identity=self.ident_b[:]), reads=[r["bh2hi"], self.b_const], writes=[btp[0]])
        for k in range(8):
            kb.op("pe", lambda e, k=k: e.transpose(out=tp[1][:, k, :], in_=r["h2lo"][:, k * 128:(k + 1) * 128],
                                                   identity=self.ident_b[:]), reads=[r["bh2lo"], self.b_const], writes=[btp[1]])
        kb.op("act", lambda e: e.copy(out=r["h2Tb"][:], in_=tp[0][:]), reads=[btp[0]], writes=[r["bh2Tb"]])
        kb.op("dve", lambda e: e.tensor_copy(out=r["h2Tl"][:], in_=tp[1][:]), reads=[btp[1]], writes=[r["bh2Tl"]])
        if self.debug:
            kb.dma("sp", S["H2T"][tile_idx], r["h2Tb"][:], reads=[r["bh2Tb"]], writes=[self.db("H2T", tile_idx)])
        if _STOP <= 6:
            return
        passes = [("h2Tb", "rwh"), ("h2Tl", "rwh"), ("h2Tb", "rwl")]
        for pi, (a_, w_) in enumerate(passes):
            for k in range(8):
                kb.op("pe", lambda e, k=k, a_=a_, w_=w_, pi=pi: e.matmul(mmp[:, 0:32], lhsT=r[a_][:, k, :], rhs=r[w_][:, k, :],
                                                                       start=(pi == 0 and k == 0), stop=(pi == 2 and k == 7)),
                      reads=[r["bh2Tb"], r["bh2Tl"], r["bw"]], writes=[bmmp])
        lg, m8, msk, ex, den, G = r["lg"], r["m8"], r["msk"], r["ex"], r["den"], r["G"]
        bl = r["blg"]
        kb.op("dve", lambda e: e.tensor_tensor(out=lg[:], in0=mmp[:, 0:32], in1=r["rb"][:], op=ALU.add),
              reads=[bmmp, r["bw"]], writes=[bl])
        if _STOP <= 7:
            return
        kb.op("dve", lambda e: e.max(out=m8[:], in_=lg[:]), reads=[bl], writes=[bl])
        kb.op("dve", lambda e: e.tensor_scalar(out=msk[:], in0=lg[:], scalar1=m8[:, 3:4], scalar2=None, op0=ALU.is_ge),
              reads=[bl], writes=[bl])
        kb.op("dve", lambda e: e.tensor_scalar(out=den[:, 1:2], in0=m8[:, 0:1], scalar1=-1.0, scalar2=None, op0=ALU.mult),
              reads=[bl], writes=[bl])
        kb.op("act", lambda e: e.activation(out=ex[:], in_=lg[:], func=AF.Exp, bias=den[:, 1:2], scale=1.0),
              reads=[bl], writes=[bl])
        kb.op("dve", lambda e: e.scalar_tensor_tensor(out=ex[:], in0=ex[:], scalar=1.0, in1=msk[:], op0=ALU.mult, op1=ALU.mult, accum_out=den[:, 0:1]), reads=[bl], writes=[bl])
        kb.op("dve", lambda e: e.reciprocal(out=den[:, 0:1], in_=den[:, 0:1]), reads=[bl], writes=[bl])
        kb.op("dve", lambda e: e.tensor_scalar(out=G[:], in0=ex[:], scalar1=den[:, 0:1], scalar2=None, op0=ALU.mult),
              reads=[bl], writes=[r["bG"]])
        if self.debug:
            kb.dma("sp", S["GS"][tile_idx], G[:], reads=[r["bG"]], writes=[self.db("GS", tile_idx)])
        cap = self.cap
        brt = r["brt"]
        kb.op("dve", lambda e: e.tensor_copy(out=r["Mb"][:], in_=msk[:]), reads=[bl], writes=[brt])
        kb.op("pe", lambda e: e.matmul(mmp[:, 32:64], lhsT=self.U_b[:], rhs=r["Mb"][:], start=True, stop=True),
              reads=[brt, self.b_const], writes=[bmmp])
        kb.op("pe", lambda e: e.matmul(mmp[:, 64:96], lhsT=self.ones_b[:], rhs=r["Mb"][:], start=True, stop=True),
              reads=[brt, self.b_const], writes=[bmmp])
        kb.op("dve", lambda e: e.tensor_tensor(out=r["posf"][:], in0=mmp[:, 32:64], in1=r["cnt"][:], op=ALU.add),
              reads=[bmmp, r["bcnt"]], writes=[brt])
        kb.op("dve", lambda e: e.tensor_tensor(out=r["cnt"][:], in0=mmp[:, 64:96], in1=r["cnt"][:], op=ALU.add),
              reads=[bmmp, r["bcnt"]], writes=[r["bcnt"]])
        kb.op("dve", lambda e: e.tensor_scalar(out=r["valid"][:], in0=r["posf"][:], scalar1=float(cap), scalar2=None, op0=ALU.is_lt),
              reads=[brt], writes=[brt])
        kb.op("dve", lambda e: e.tensor_tensor(out=r["slotm"][:], in0=r["posf"][:], in1=self.iotaE[:], op=ALU.add),
              reads=[brt, self.b_const], writes=[brt])
        kb.op("dve", lambda e: e.tensor_scalar(out=r["junk"][:], in0=r["valid"][:], scalar1=-1.0e6, scalar2=1.0e6,
                                               op0=ALU.mult, op1=ALU.add), reads=[brt], writes=[brt])
        kb.op("dve", lambda e: e.tensor_tensor(out=r["slotm"][:], in0=r["slotm"][:], in1=r["junk"][:], op=ALU.add),
              reads=[brt], writes=[brt])
        kb.op("dve", lambda e: e.tensor_tensor(out=r["Gv"][:], in0=G[:], in1=r["valid"][:], op=ALU.mult),
              reads=[brt, r["bG"]], writes=[brt])
        for k in range(4):
            kb.op("dve", lambda e, k=k: e.tensor_scalar(out=r["oh"][:], in0=lg[:], scalar1=m8[:, k:k + 1], scalar2=None, op0=ALU.is_equal),
                  reads=[bl, brt], writes=[brt])
            kb.op("dve", lambda e, k=k: e.scalar_tensor_tensor(out=r["junk"][:], in0=r["oh"][:], scalar=1.0, in1=r["slotm"][:],
                                                              op0=ALU.mult, op1=ALU.mult, accum_out=r["slotf"][:, k:k + 1]),
                  reads=[brt], writes=[brt])
            kb.op("dve", lambda e, k=k: e.scalar_tensor_tensor(out=r["junk"][:], in0=r["oh"][:], scalar=1.0, in1=r["Gv"][:],
                                                              op0=ALU.mult, op1=ALU.mult, accum_out=r["gk"][:, k:k + 1]),
                  reads=[brt, r["bgk"]], writes=[brt, r["bgk"]])
        kb.op("dve", lambda e: e.tensor_copy(out=r["sloti"][:], in_=r["slotf"][:]), reads=[brt, r["bsloti"]], writes=[r["bsloti"]])
        kb.dma("sp", S["SLOT"][tile_idx], r["sloti"][:], reads=[r["bsloti"]], writes=[self.db("SLOT", tile_idx)])
        kb.dma("sp", S["GK"][tile_idx], r["gk"][:], reads=[r["bgk"]], writes=[self.db("GK", tile_idx)])
        for k in range(4):
            kb.idma(S["XG"], r["sloti"][:, k:k + 1], r["h2hi"][:], None, 32 * cap - 1, reads=[r["bsloti"], r["bh2hi"]])

    def phase1b(self):
        kb, I, S, nseq = self.kb, self.I, self.S, self.nseq
        kb.push()
        bw = Buf()
        w_in = kb.sb("w_in_b", [128, 8, 2048], BF16)
        w_out = kb.sb("w_out", [128, 8, D], BF16)
        self.load_w_bf16(w_in, I["hyb_w_in"][:, 672:2720], 8, bw)
        self.load_w_bf16(w_out, I["hyb_w_out"], 8, bw)
        retg = kb.sb("retg", [128, 512], F32)
        self.bcast_load(retg[:], I["ret_norm_g"], bw)
        decT = kb.sb("decT", [128, 8 * 128], F32)
        qdec = kb.sb("qdec", [128, 8], F32)
        kdec = kb.sb("kdec", [128, 8], F32)
        cdec = kb.sb("cdec", [128, 4], F32)
        kb.dma("sp", decT[:], I["k_decayT"], writes=[bw])
        kb.dma("sp", qdec[:], I["k_qdec"], writes=[bw])
        kb.dma("sp", kdec[:], I["k_kdec"], writes=[bw])
        kb.dma("sp", cdec[:], I["k_cdec"], writes=[bw])
        r = self.alloc_router(0)

        mods = {n: kb.sb(n, [128, D], F32) for n in ("gmod1", "shift1", "gate1", "gmod2", "shift2")}
        bmod = Buf()
        x_t = [kb.sb("x_t%d" % i, [128, D], F32) for i in range(2)]; bx = [Buf(), Buf()]
        tmp = kb.sb("tmp", [128, D], F32); btmp = Buf()
        h_bf = kb.sb("h_bf", [128, D], BF16); bh = Buf()
        hT = kb.sb("hT", [128, 8, 128], BF16); bhT = Buf()
        ss = kb.sb("ss", [128, 4], F32); bss = Buf()
        raw = [kb.sb("raw%d" % i, [128, 8, 64], F32) for i in range(2)]; braw = [Buf(), Buf()]
        rr = [kb.sb("rr%d" % i, [128, 8, 64], F32) for i in range(2)]; brr = [Buf(), Buf()]
        rt = [kb.sb("rt%d" % i, [128, 8, 32], F32) for i in range(4)]; brt = Buf()
        rq_bf = kb.sb("rq_bf", [128, 8, 64], BF16); brqb = Buf()
        rqd_bf = kb.sb("rqd_bf", [128, 8, 64], BF16); brqd = Buf()
        rk_bf = kb.sb("rk_bf", [128, 8, 64], BF16); brkb = Buf()
        rkd_bf = kb.sb("rkd_bf", [128, 8, 64], BF16); brkd = Buf()
        v_bf = kb.sb("v_bf", [128, 8, 64], BF16); bv = Buf()
        sg = kb.sb("sg", [128, 512], F32); bsg = Buf()
        rqT = kb.sb("rqT", [128, 8, 128], BF16); brqT = Buf()
        rkT = kb.sb("rkT", [128, 4, 128], BF16); brkT = Buf()
        Sd = kb.sb("Sd", [128, 8, 128], BF16); bSd = Buf()
        st_f = kb.sb("st_f", [128, 4, 128], F32); bstf = Buf()
        st_b = kb.sb("st_b", [128, 4, 128], BF16); bstb = Buf()
        kb.op("pool", lambda e: e.memset(st_f[:], 0.0), writes=[bstf])
        o_sb = kb.sb("o_sb", [128, 8, 64], F32); bo = Buf()
        oc = kb.sb("oc", [128, 8, 64], F32); boc = Buf()
        st8 = kb.sb("st8", [128, 16], F32); bst8 = Buf()
        mixcat = kb.sb("mixcat", [128, D], BF16); bmixa = Buf(); bmixy = Buf()
        mixT = kb.sb("mixT", [128, 8, 128], BF16); bmixT = Buf()
        x1 = kb.sb("x1", [128, D], F32); bx1 = Buf()

        tp = [kb.ps("tp%d" % i, [128, 8, 128], BF16) for i in range(2)]; btp = [Buf(), Buf()]
        mm = [kb.ps("mm%d" % i, [128, 512], F32) for i in range(2)]; bmm = [Buf(), Buf()]
        s2 = kb.ps("s2", [128, 2, 512], F32); bs2 = [Buf(), Buf()]
        oo = [kb.ps("oo%d" % i, [128, 512], F32) for i in range(2)]; boo = [Buf(), Buf()]
        xi = 0
        for s in range(nseq):
            cos, sin, brope = self.rope_tables(s, 32, "k_invf32", "b%d" % s)
            for n_, part in (("gmod1", 1), ("shift1", 0), ("gate1", 2), ("gmod2", 4), ("shift2", 3)):
                kb.dma("sp", mods[n_][:], S["MOD"][s, 0, part], reads=[self.db("MOD", (s, 0, part))], writes=[bmod])
            for t in range(self.ntl):
                ti = s * NT + t
                X = x_t[xi % 2]; bX = bx[xi % 2]; xi += 1
                kb.dma("sp", X[:], I["x"][ti * 128:(ti + 1) * 128, :], writes=[bX])
                kb.dma("sp", mixcat[:, 0:512], S["ATT"][ti], reads=[self.db("ATT", ti)], writes=[bmixa])
                self.norm_mod_T(X, bX, mods["gmod1"], mods["shift1"], bmod, tmp, btmp, h_bf, bh, tp[0], btp[0], hT, bhT, ss, bss)
                cb = bc_mid(cos[:, t, :], 8)
                sb_ = bc_mid(sin[:, t, :], 8)
                for gi in range(4):
                    p_ = mm[gi % 2]; bp_ = bmm[gi % 2]
                    for k in range(8):
                        kb.op("pe", lambda e, k=k, gi=gi, p_=p_: e.matmul(p_[:], lhsT=hT[:, k, :], rhs=w_in[:, k, gi * 512:(gi + 1) * 512],
                                                                       start=(k == 0), stop=(k == 7)), reads=[bhT, bw], writes=[bp_])
                    if gi < 2:
                        rw_ = raw[gi]; brw_ = braw[gi]; ro = rr[gi]; bro = brr[gi]
                        kb.op("act", lambda e, p_=p_, rw_=rw_: e.copy(out=rw_[:].rearrange("p h d -> p (h d)"), in_=p_[:]),
                              reads=[bp_], writes=[brw_])
                        x1_ = rw_[:, :, 0:32]; x2_ = rw_[:, :, 32:64]
                        kb.op("dve", lambda e, x1_=x1_: e.tensor_tensor(out=rt[0][:], in0=x1_, in1=cb, op=ALU.mult), reads=[brw_, brope], writes=[brt])
                        kb.op("dve", lambda e, x2_=x2_: e.tensor_tensor(out=rt[1][:], in0=x2_, in1=sb_, op=ALU.mult), reads=[brw_, brope], writes=[brt])
                        kb.op("pool", lambda e, x2_=x2_: e.tensor_tensor(out=rt[2][:], in0=x2_, in1=cb, op=ALU.mult), reads=[brw_, brope], writes=[brt])
                        kb.op("pool", lambda e, x1_=x1_: e.tensor_tensor(out=rt[3][:], in0=x1_, in1=sb_, op=ALU.mult), reads=[brw_, brope], writes=[brt])
                        kb.op("dve", lambda e, ro=ro: e.tensor_tensor(out=ro[:, :, 0:32], in0=rt[0][:], in1=rt[1][:], op=ALU.subtract),
                              reads=[brt], writes=[bro])
                        kb.op("pool", lambda e, ro=ro: e.tensor_tensor(out=ro[:, :, 32:64], in0=rt[2][:], in1=rt[3][:], op=ALU.add),
                              reads=[brt], writes=[bro])
                        if gi == 0:
                            kb.op("act", lambda e, ro=ro: e.copy(out=rq_bf[:], in_=ro[:]), reads=[bro], writes=[brqb])
                            kb.op("pool", lambda e, ro=ro: e.tensor_tensor(out=rqd_bf[:], in0=ro[:], in1=bc_last(qdec[:, :], 64), op=ALU.mult),
                                  reads=[bro, bw], writes=[brqd])
                        else:
                            kb.op("act", lambda e, ro=ro: e.mul(out=rk_bf[:], in_=ro[:], mul=0.125), reads=[bro], writes=[brkb])
                            kb.op("dve", lambda e, ro=ro: e.scalar_tensor_tensor(out=rkd_bf[:], in0=ro[:], scalar=0.125,
                                                                                in1=bc_last(kdec[:, :], 64), op0=ALU.mult, op1=ALU.mult),
                                  reads=[bro, bw], writes=[brkd])
                    elif gi == 2:
                        kb.op("act", lambda e, p_=p_: e.copy(out=v_bf[:].rearrange("p h d -> p (h d)"), in_=p_[:]), reads=[bp_], writes=[bv])
                    else:
                        kb.op("act", lambda e, p_=p_: e.activation(out=sg[:], in_=p_[:], func=AF.Silu), reads=[bp_], writes=[bsg])
                for i in range(4):
                    kb.op("pe", lambda e, i=i: e.transpose(out=tp[1][:, i, :], in_=rq_bf[:, 2 * i:2 * i + 2, :].rearrange("p h d -> p (h d)"),
                                                           identity=self.ident_b[:]), reads=[brqb, self.b_const], writes=[btp[1]])
                for i in range(4):
                    kb.op("pe", lambda e, i=i: e.transpose(out=tp[1][:, 4 + i, :], in_=rqd_bf[:, 2 * i:2 * i + 2, :].rearrange("p h d -> p (h d)"),
                                                           identity=self.ident_b[:]), reads=[brqd, self.b_const], writes=[btp[1]])
                for i in range(4):
                    kb.op("pe", lambda e, i=i: e.transpose(out=tp[0][:, i, :], in_=rk_bf[:, 2 * i:2 * i + 2, :].rearrange("p h d -> p (h d)"),
                                                           identity=self.ident_b[:]), reads=[brkb, self.b_const], writes=[btp[0]])
                kb.op("act", lambda e: e.copy(out=rqT[:], in_=tp[1][:]), reads=[btp[1]], writes=[brqT])
                kb.op("dve", lambda e: e.tensor_copy(out=rkT[:], in_=tp[0][:, 0:4, :]), reads=[btp[0]], writes=[brkT])
                if _STOP <= 1:
                    continue
                for h in range(8):
                    i, o = h // 2, (h % 2) * 64
                    kb.op("pe", lambda e, h=h, i=i, o=o: e.matmul(s2[:, h % 2, i * 128:(i + 1) * 128],
                                                                lhsT=rkT[o:o + 64, i, :], rhs=rqT[o:o + 64, i, :], start=True, stop=True),
                          reads=[brkT, brqT], writes=[bs2[h % 2]])
                for hb in range(2):
                    kb.op("dve", lambda e, hb=hb: e.tensor_tensor(out=Sd[:, hb * 4:(hb + 1) * 4, :].rearrange("p h q -> p (h q)"),
                                                                 in0=s2[:, hb, :], in1=decT[:, hb * 512:(hb + 1) * 512], op=ALU.mult),
                          reads=[bs2[hb], bw], writes=[bSd])
                if _STOP <= 1.5:
                    continue
                for i in range(4):
                    if t > 0:
                        kb.op("pe", lambda e, i=i: e.matmul(oo[0][:, i * 128:(i + 1) * 128], lhsT=rqT[:, 4 + i, :],
                                                            rhs=st_b[:, i, :], start=True, stop=False, skip_group_check=True),
                              reads=[brqT, bstb], writes=[boo[0]])
                    for par in range(2):
                        h = 2 * i + par
                        kb.op("pe", lambda e, h=h, i=i, par=par: e.matmul(oo[0][:, h * 64:(h + 1) * 64], lhsT=Sd[:, par * 4 + i, :],
                                                                        rhs=v_bf[:, h, :], start=(t == 0), stop=(t == 0 or par == 1),
                                                                        skip_group_check=(t > 0)),
                              reads=[bSd, bv], writes=[boo[0]])
                if _STOP <= 2:
                    continue
                for i in range(4):
                    kb.op("pe", lambda e, i=i: e.matmul(oo[1][:, i * 128:(i + 1) * 128],
                                                        lhsT=rkd_bf[:, 2 * i:2 * i + 2, :].rearrange("p h d -> p (h d)"),
                                                        rhs=v_bf[:, 2 * i:2 * i + 2, :].rearrange("p h d -> p (h d)"), start=True, stop=True),
                          reads=[brkd, bv], writes=[boo[1]])
                kvv = oo[1][:].rearrange("p (i c) -> p i c", c=128)
                for half in range(2):
                    po = half * 64
                    if t == 0:
                        kb.op("dve", lambda e, po=po: e.tensor_copy(out=st_f[po:po + 64, :, po:po + 64], in_=kvv[po:po + 64, :, po:po + 64]),
                              reads=[boo[1]], writes=[bstf])
                    else:
                        for i in range(4):
                            kb.op("dve", lambda e, po=po, i=i: e.scalar_tensor_tensor(
                                out=st_f[po:po + 64, i, po:po + 64], in0=st_f[po:po + 64, i, po:po + 64], scalar=cdec[po:po + 64, i:i + 1],
                                in1=kvv[po:po + 64, i, po:po + 64], op0=ALU.mult, op1=ALU.add), reads=[boo[1], bstf, bw], writes=[bstf])
                kb.op("act", lambda e: e.copy(out=st_b[:], in_=st_f[:]), reads=[bstf], writes=[bstb])
                if _STOP <= 3:
                    continue
                kb.op("act", lambda e: e.copy(out=o_sb[:].rearrange("p h d -> p (h d)"), in_=oo[0][:]), reads=[boo[0]], writes=[bo])
                kb.op("dve", lambda e: e.tensor_reduce(out=st8[:, 0:8], in_=o_sb[:], axis=AX.X, op=ALU.add), reads=[bo], writes=[bst8])
                kb.op("dve", lambda e: e.tensor_scalar(out=st8[:, 0:8], in0=st8[:, 0:8], scalar1=-1.0 / 64, scalar2=None, op0=ALU.mult),
                      reads=[bst8], writes=[bst8])
                kb.op("pool", lambda e: e.tensor_tensor(out=oc[:], in0=o_sb[:], in1=bc_last(st8[:, 0:8], 64), op=ALU.add),
                      reads=[bo, bst8], writes=[boc])
                kb.op("pool", lambda e: e.tensor_tensor(out=o_sb[:], in0=oc[:], in1=oc[:], op=ALU.mult), reads=[boc], writes=[bo])
                kb.op("dve", lambda e: e.tensor_reduce(out=st8[:, 8:16], in_=o_sb[:], axis=AX.X, op=ALU.add), reads=[bo], writes=[bst8])
                kb.op("dve", lambda e: e.tensor_scalar(out=st8[:, 8:16], in0=st8[:, 8:16], scalar1=1.0 / 64, scalar2=EPS,
                                                       op0=ALU.mult, op1=ALU.add), reads=[bst8], writes=[bst8])
                kb.op("pool", lambda e: e.tensor_tensor(out=st8[:, 8:16], in0=st8[:, 8:16], in1=self.neghalf[:, 0:8], op=ALU.pow),
                      reads=[bst8, self.b_const], writes=[bst8])
                kb.op("dve", lambda e: e.tensor_tensor(out=oc[:], in0=oc[:], in1=bc_last(st8[:, 8:16], 64), op=ALU.mult),
                      reads=[boc, bst8], writes=[boc])
                kb.op("pool", lambda e: e.tensor_tensor(out=oc[:].rearrange("p h d -> p (h d)"), in0=oc[:].rearrange("p h d -> p (h d)"),
                                                        in1=retg[:], op=ALU.mult), reads=[boc, bw], writes=[boc])
                kb.op("dve", lambda e: e.tensor_tensor(out=mixcat[:, 512:1024], in0=oc[:].rearrange("p h d -> p (h d)"), in1=sg[:],
                                                       op=ALU.mult), reads=[boc, bsg], writes=[bmixy])
                if _STOP <= 4:
                    continue
                for k in range(8):
                    kb.op("pe", lambda e, k=k: e.transpose(out=tp[0][:, k, :], in_=mixcat[:, k * 128:(k + 1) * 128], identity=self.ident_b[:]),
                          reads=[bmixa, bmixy, self.b_const], writes=[btp[0]])
                kb.op("act", lambda e: e.copy(out=mixT[:], in_=tp[0][:]), reads=[btp[0]], writes=[bmixT])
                for half in range(2):
                    for k in range(8):
                        kb.op("pe", lambda e, k=k, half=half: e.matmul(mm[half][:], lhsT=mixT[:, k, :], rhs=w_out[:, k, half * 512:(half + 1) * 512],
                                                                     start=(k == 0), stop=(k == 7)), reads=[bmixT, bw], writes=[bmm[half]])
                    hs = slice(half * 512, (half + 1) * 512)
                    kb.op("dve", lambda e, half=half, hs=hs: e.tensor_tensor(out=tmp[:, hs], in0=mm[half][:], in1=mods["gate1"][:, hs], op=ALU.mult),
                          reads=[bmm[half], bmod], writes=[btmp])
                    kb.op("pool", lambda e, hs=hs: e.tensor_tensor(out=x1[:, hs], in0=tmp[:, hs], in1=X[:, hs], op=ALU.add),
                          reads=[btmp, bX], writes=[bx1])
                kb.dma("sp", S["XA"][ti * 128:(ti + 1) * 128, :], x1[:], reads=[bx1], writes=[self.db("XA", ti)])
                if _STOP <= 5:
                    continue
                self.norm2_router(r, x1, bx1, mods["gmod2"], mods["shift2"], bmod, tmp, btmp, tp, btp, mm[0], bmm[0], ti)
        kb.pop()

    def zero_xg(self):
        kb, S = self.kb, self.S
        z = kb.sb("zeros", [128, 4096], BF16); bz = Buf()
        kb.op("pool", lambda e: e.memset(z[:], 0.0), writes=[bz])
        nrows = 32 * self.cap
        for r0 in range(0, nrows, 512):
            kb.dma("sp", S["XG"][r0:r0 + 512, :].rearrange("(p a) d -> p (a d)", p=128), z[:], reads=[bz])

    def phase2e(self, l):
        kb, I, S = self.kb, self.I, self.S
        kb.push()
        cap = self.cap
        nblk = cap // 512
        wgu = [kb.sb("wgu%d" % i, [128, 8, 2048], BF16) for i in range(2)]
        wdn = [kb.sb("wdn%d" % i, [128, 8, D], BF16) for i in range(2)]
        bgu = [kb.sb("bgu%d" % i, [128, 16], F32) for i in range(2)]
        bdn = [kb.sb("bdn%d" % i, [1, D], BF16) for i in range(2)]
        bwt = [Buf(), Buf()]
        ones1 = kb.sb("ones1", [1, 128], BF16); bones = Buf()
        kb.op("pool", lambda e: e.memset(ones1[:], 1.0), writes=[bones])
        xg = [kb.sb("xg%d" % i, [128, D], BF16) for i in range(12)]; bxg = [Buf() for _ in range(12)]
        xgT = [kb.sb("xgT%d" % i, [128, 8, 512], BF16) for i in range(2)]; bxgT = [Buf(), Buf()]
        glu = [kb.sb("glu%d" % i, [128, 512], F32) for i in range(2)]; bglu = [Buf(), Buf()]
        sig = [kb.sb("sig%d" % i, [128, 512], F32) for i in range(2)]; bsig = [Buf(), Buf()]
        lin = [kb.sb("lin%d" % i, [128, 512], F32) for i in range(2)]; blin = [Buf(), Buf()]
        actT = [kb.sb("actT%d" % i, [128, 8, 512], BF16) for i in range(2)]; bact = [Buf(), Buf()]
        yg = [kb.sb("yg%d" % i, [128, D], F32) for i in range(3)]; byg = [Buf() for _ in range(3)]
        tp = [kb.ps("tp%d" % i, [128, 8, 128], BF16) for i in range(2)]; btp = [Buf(), Buf()]
        pA = [kb.ps("pA%d" % i, [128, 512], F32) for i in range(2)]; bpA = [Buf(), Buf()]
        pB = [kb.ps("pB%d" % i, [128, 512], F32) for i in range(2)]; bpB = [Buf(), Buf()]
        pC = [kb.ps("pC%d" % i, [128, 512], F32) for i in range(2)]; bpC = [Buf(), Buf()]

        def load_expert(e, slot):
            self.load_w_bf16(wgu[slot], I["exp_w_gu"][l, e], 8, bwt[slot])
            self.load_w_bf16(wdn[slot], I["exp_w_down"][l, e], 8, bwt[slot])
            kb.dma("sp", bgu[slot][:], I["exp_b_gu_pj"][l, e], writes=[bwt[slot]])
            kb.op("dve", lambda en, slot=slot: en.tensor_scalar(out=bgu[slot][:, 8:16], in0=bgu[slot][:, 8:16], scalar1=1.0, scalar2=None,
                                                               op0=ALU.add), reads=[bwt[slot]], writes=[bwt[slot]])
            kb.dma("pool", bdn[slot][:], I["exp_b_down"][l, e:e + 1, :], writes=[bwt[slot]])

        load_expert(0, 0)
        blocks = [(ex, blk) for ex in range(32) for blk in range(nblk)]
        state = dict(xu=0, tu=0)

        def emit_loads(bi):
            ex, blk = blocks[bi]
            r0 = ex * cap + blk * 512
            tiles = []
            for st in range(4):
                i = state["xu"] % len(xg); state["xu"] += 1
                kb.dma("sp", xg[i][:], S["XG"][r0 + st * 128:r0 + (st + 1) * 128, :], writes=[bxg[i]])
                tiles.append(i)
            return tiles

        def emit_transposes(bi, tiles):
            XT = xgT[bi % 2]; bXT = bxgT[bi % 2]
            for st, i in enumerate(tiles):
                T = tp[state["tu"] % 2]; bT = btp[state["tu"] % 2]; state["tu"] += 1
                for k in range(8):
                    kb.op("pe", lambda e, k=k, i=i, T=T: e.transpose(out=T[:, k, :], in_=xg[i][:, k * 128:(k + 1) * 128],
                                                                   identity=self.ident_b[:]), reads=[bxg[i], self.b_const], writes=[bT])
                kb.op("act", lambda e, T=T, XT=XT, st=st: e.copy(out=XT[:, :, st * 128:(st + 1) * 128], in_=T[:]),
                      reads=[bT], writes=[bXT])

        pu = cu = yu = 0
        tl0 = emit_loads(0)
        tl1 = emit_loads(1) if len(blocks) > 1 else None
        emit_transposes(0, tl0)
        for bi, (ex, blk) in enumerate(blocks):
            slot = ex % 2
            if blk == 0 and ex + 1 < 32:
                load_expert(ex + 1, (ex + 1) % 2)
            XT = xgT[bi % 2]; bXT = bxgT[bi % 2]
            A = actT[bi % 2]; bA = bact[bi % 2]
            r0 = ex * cap + blk * 512
            for j in range(8):
                pa = pA[pu % 2]; bpa = bpA[pu % 2]; pb = pB[pu % 2]; bpb = bpB[pu % 2]
                gl = glu[pu % 2]; bgl = bglu[pu % 2]; sg_ = sig[pu % 2]; bsg_ = bsig[pu % 2]; ln = lin[pu % 2]; bln = blin[pu % 2]
                pu += 1
                for k in range(8):
                    kb.op("pe", lambda e, k=k, j=j, pa=pa, slot=slot, XT=XT: e.matmul(
                        pa[:], lhsT=wgu[slot][:, k, j * 128:(j + 1) * 128], rhs=XT[:, k, :],
                        start=(k == 0), stop=(k == 7)), reads=[bwt[slot], bXT], writes=[bpa])
                for k in range(8):
                    kb.op("pe", lambda e, k=k, j=j, pb=pb, slot=slot, XT=XT: e.matmul(
                        pb[:], lhsT=wgu[slot][:, k, 1024 + j * 128:1024 + (j + 1) * 128], rhs=XT[:, k, :],
                        start=(k == 0), stop=(k == 7)), reads=[bwt[slot], bXT], writes=[bpb])
                kb.op("dve", lambda e, pa=pa, gl=gl, j=j, slot=slot: e.tensor_scalar(
                    out=gl[:], in0=pa[:], scalar1=bgu[slot][:, j:j + 1], scalar2=7.0, op0=ALU.add, op1=ALU.min),
                    reads=[bpa, bwt[slot]], writes=[bgl])
                kb.op("act", lambda e, gl=gl, sg_=sg_: e.activation(out=sg_[:], in_=gl[:], func=AF.Sigmoid, scale=1.702),
                      reads=[bgl], writes=[bsg_])
                kb.op("dve", lambda e, pb=pb, ln=ln, j=j, slot=slot: e.tensor_scalar(
                    out=ln[:], in0=pb[:], scalar1=bgu[slot][:, 8 + j:9 + j], scalar2=8.0, op0=ALU.add, op1=ALU.min),
                    reads=[bpb, bwt[slot]], writes=[bln])
                kb.op("dve", lambda e, gl=gl, sg_=sg_: e.tensor_tensor(out=gl[:], in0=gl[:], in1=sg_[:], op=ALU.mult),
                      reads=[bgl, bsg_], writes=[bgl])
                kb.op("dve", lambda e, gl=gl, ln=ln, A=A, j=j: e.scalar_tensor_tensor(out=A[:, j, :], in0=ln[:], scalar=-6.0, in1=gl[:],
                                                                                 op0=ALU.max, op1=ALU.mult),
                      reads=[bgl, bln], writes=[bA])
            if bi + 1 < len(blocks):
                emit_transposes(bi + 1, tl1)
                tl0, tl1 = tl1, (emit_loads(bi + 2) if bi + 2 < len(blocks) else None)
            for st in range(4):
                Y = yg[yu % 3]; bY = byg[yu % 3]; yu += 1
                for half in range(2):
                    pc = pC[cu % 2]; bpc = bpC[cu % 2]; cu += 1
                    for k in range(8):
                        kb.op("pe", lambda e, k=k, st=st, half=half, pc=pc, A=A, slot=slot: e.matmul(
                            pc[:], lhsT=A[:, k, st * 128:(st + 1) * 128], rhs=wdn[slot][:, k, half * 512:(half + 1) * 512],
                            start=(k == 0), stop=False), reads=[bA, bwt[slot]], writes=[bpc])
                    kb.op("pe", lambda e, half=half, pc=pc, slot=slot: e.matmul(
                        pc[:], lhsT=ones1[:, :], rhs=bdn[slot][:, half * 512:(half + 1) * 512], start=False, stop=True),
                        reads=[bones, bwt[slot]], writes=[bpc])
                    if half == 0:
                        kb.op("act", lambda e, pc=pc, Y=Y: e.copy(out=Y[:, 0:512], in_=pc[:]), reads=[bpc], writes=[bY])
                    else:
                        kb.op("dve", lambda e, pc=pc, Y=Y: e.tensor_copy(out=Y[:, 512:1024], in_=pc[:]), reads=[bpc], writes=[bY])
                kb.dma("pool", S["YG"][r0 + st * 128:r0 + (st + 1) * 128, :], Y[:], reads=[bY])
        kb.pop()

    def phase2c(self, l, src, dst, dst_name, zero_after):
        kb, I, S, nseq = self.kb, self.I, self.S, self.nseq
        kb.push()
        cap = self.cap
        gate2 = kb.sb("gate2", [128, D], F32); bg2 = Buf()
        xin = [kb.sb("xin%d" % i, [128, D], F32) for i in range(2)]; bxin = [Buf(), Buf()]
        acc = [kb.sb("acc%d" % i, [128, D], F32) for i in range(2)]; bacc = [Buf(), Buf()]
        yb = [kb.sb("yb%d" % i, [128, D], F32) for i in range(8)]; byb = [Buf() for _ in range(8)]
        sl = [kb.sb("sl%d" % i, [128, 4], I32) for i in range(2)]; bsl = [Buf(), Buf()]
        gk = [kb.sb("gkc%d" % i, [128, 4], F32) for i in range(2)]; bgk = [Buf(), Buf()]
        for i in range(8):
            kb.op("pool", lambda e, i=i: e.memset(yb[i][:], 0.0), writes=[byb[i]])
        if zero_after:
            self.zero_xg()
        u = 0
        for s in range(nseq):
            kb.dma("sp", gate2[:], S["MOD"][s, l, 5], reads=[self.db("MOD", (s, l, 5))], writes=[bg2])
            for t in range(self.ntl):
                ti = s * NT + t
                X = xin[u % 2]; bX = bxin[u % 2]; A = acc[u % 2]; bA = bacc[u % 2]
                SL = sl[u % 2]; bSL = bsl[u % 2]; GK = gk[u % 2]; bGK = bgk[u % 2]
                kb.dma("sp", X[:], src[ti * 128:(ti + 1) * 128, :], reads=[self.db("XA", ti)], writes=[bX])
                kb.dma("sp", SL[:], S["SLOT"][ti], reads=[self.db("SLOT", ti)], writes=[bSL])
                kb.dma("sp", GK[:], S["GK"][ti], reads=[self.db("GK", ti)], writes=[bGK])
                for k in range(4):
                    Yk = yb[(u % 2) * 4 + k]; bYk = byb[(u % 2) * 4 + k]
                    kb.idma(Yk[:], None, S["YG"], SL[:, k:k + 1], 32 * cap - 1, reads=[bSL], writes=[bYk])
                    if k == 0:
                        kb.op("dve", lambda e, Yk=Yk, A=A, GK=GK: e.tensor_scalar(out=A[:], in0=Yk[:], scalar1=GK[:, 0:1], scalar2=None,
                                                                                 op0=ALU.mult), reads=[bYk, bGK], writes=[bA])
                    else:
                        kb.op("dve", lambda e, Yk=Yk, A=A, GK=GK, k=k: e.scalar_tensor_tensor(
                            out=A[:], in0=Yk[:], scalar=GK[:, k:k + 1], in1=A[:], op0=ALU.mult, op1=ALU.add),
                            reads=[bYk, bGK, bA], writes=[bA])
                kb.op("pool", lambda e, A=A: e.tensor_tensor(out=A[:], in0=A[:], in1=gate2[:], op=ALU.mult), reads=[bA, bg2], writes=[bA])
                kb.op("pool", lambda e, A=A, X=X: e.tensor_tensor(out=X[:], in0=A[:], in1=X[:], op=ALU.add), reads=[bA, bX], writes=[bX])
                kb.dma("sp", dst[ti * 128:(ti + 1) * 128, :], X[:], reads=[bX], writes=[self.db(dst_name, ti)])
                u += 1
        kb.pop()

    def phase3(self):
        kb, I, S, nseq = self.kb, self.I, self.S, self.nseq
        kb.push()
        bw = Buf()
        w_qkv = kb.sb("w_qkv", [128, 8, 1280], BF16)
        w_out = kb.sb("w_out", [128, 8, D], BF16)
        self.load_w_bf16(w_qkv, I["swa_w_qkv"], 8, bw)
        self.load_w_bf16(w_out, I["swa_w_out"], 8, bw)
        bqkv = kb.sb("bqkv", [128, 1280], F32)
        bout = kb.sb("bout", [128, D], F32)
        gq = kb.sb("gq", [128, 64], F32)
        gk = kb.sb("gk", [128, 64], F32)
        sk = kb.sb("sk", [128, 16], F32)
        self.bcast_load(bqkv[:], I["swa_b_qkv"], bw)
        self.bcast_load(bout[:], I["swa_b_out"], bw)
        self.bcast_load(gq[:], I["swa_q_head_g"], bw)
        self.bcast_load(gk[:], I["swa_k_head_g"], bw)
        self.bcast_load(sk[:], I["swa_sinks"], bw)
        kb.op("act", lambda e: e.activation(out=sk[:], in_=sk[:], func=AF.Exp), reads=[bw], writes=[bw])
        mask2 = kb.sb("mask2", [128, 4, 2, 128], BF16)
        for hh in range(4):
            kb.op("pool", lambda e, hh=hh: e.tensor_copy(out=mask2[:, hh, 0, :], in_=self.mask_gt[:]), reads=[self.b_const], writes=[bw])
            kb.op("pool", lambda e, hh=hh: e.tensor_copy(out=mask2[:, hh, 1, :], in_=self.mask_le[:]), reads=[self.b_const], writes=[bw])
        r = self.alloc_router(1)

        mods = {n: kb.sb(n, [128, D], F32) for n in ("gmod1", "shift1", "gate1", "gmod2", "shift2")}
        bmod = Buf()
        x_t = [kb.sb("x_t%d" % i, [128, D], F32) for i in range(2)]; bx = [Buf(), Buf()]
        tmp = kb.sb("tmp", [128, D], F32); btmp = Buf()
        h_bf = kb.sb("h_bf", [128, D], BF16); bh = Buf()
        hT = kb.sb("hT", [128, 8, 128], BF16); bhT = Buf()
        ss = kb.sb("ss", [128, 4], F32); bss = Buf()
        qkv = kb.sb("qkv", [128, 20, 64], F32); bqkvs = Buf()
        sq = kb.sb("sq", [128, 18, 64], F32); bsq = Buf()
        r18 = kb.sb("r18", [128, 18], F32); br18 = Buf()
        qn = kb.sb("qn", [128, 18, 64], F32); bqn = Buf()
        rt = [kb.sb("rt%d" % i, [128, 18, 32], F32) for i in range(4)]; brt = Buf()
        q_bf = kb.sb("q_bf", [128, 16, 64], BF16); bqb = Buf()
        kdup = kb.sb("kdup", [128, 2, 2, 64], BF16); bkd = Buf()
        qT = kb.sb("qT", [128, 8, 128], BF16); bqT = Buf()
        kT = [kb.sb("kT%d" % i, [128, 2, 128], BF16) for i in range(2)]; bkT = [Buf(), Buf()]
        Va = [kb.sb("Va%d" % i, [128, 2, 65], BF16) for i in range(2)]; bVa = [Buf(), Buf()]
        bVones = Buf()
        for i in range(2):
            kb.op("pool", lambda e, i=i: e.memset(Va[i][:, :, 64:65], 1.0), writes=[bVones])
        PT = [kb.sb("PT%d" % i, [128, 4, 2, 128], BF16) for i in range(2)]; bPT = [Buf(), Buf()]
        den = kb.sb("den", [128, 16], F32); bden = Buf()
        attn = kb.sb("attn", [128, 16, 64], BF16); battn = Buf()
        attT = kb.sb("attT", [128, 8, 128], BF16); battT = Buf()
        x1 = kb.sb("x1", [128, D], F32); bx1 = Buf()

        tp = [kb.ps("tp%d" % i, [128, 8, 128], BF16) for i in range(2)]; btp = [Buf(), Buf()]
        mm = [kb.ps("mm%d" % i, [128, 512], F32) for i in range(2)]; bmm = [Buf(), Buf()]
        s2 = kb.ps("s2", [128, 2, 512], F32); bs2 = [Buf(), Buf()]
        oo = [kb.ps("oo%d" % i, [128, 512], F32) for i in range(2)]; boo = [Buf(), Buf()]
        xi = 0
        gu_ = 0
        for s in range(nseq):
            cos, sin, brope = self.rope_tables(s, 32, "k_invf32", "c%d" % s)
            for n_, part in (("gmod1", 1), ("shift1", 0), ("gate1", 2), ("gmod2", 4), ("shift2", 3)):
                kb.dma("sp", mods[n_][:], S["MOD"][s, 1, part], reads=[self.db("MOD", (s, 1, part))], writes=[bmod])
            for t in range(self.ntl):
                ti = s * NT + t
                cur, prv = t % 2, (t + 1) % 2
                X = x_t[xi % 2]; bX = bx[xi % 2]; xi += 1
                kb.dma("sp", X[:], S["XB"][ti * 128:(ti + 1) * 128, :], reads=[self.db("XB", ti)], writes=[bX])
                self.norm_mod_T(X, bX, mods["gmod1"], mods["shift1"], bmod, tmp, btmp, h_bf, bh, tp[0], btp[0], hT, bhT, ss, bss)
                qkvf = qkv[:].rearrange("p h d -> p (h d)")
                for gi, (c0, c1) in enumerate(((0, 512), (512, 1024), (1024, 1280))):
                    p_ = mm[gi % 2]; bp_ = bmm[gi % 2]
                    for k in range(8):
                        kb.op("pe", lambda e, k=k, c0=c0, c1=c1, p_=p_: e.matmul(p_[:, 0:c1 - c0], lhsT=hT[:, k, :], rhs=w_qkv[:, k, c0:c1],
                                                                              start=(k == 0), stop=(k == 7)), reads=[bhT, bw], writes=[bp_])
                    kb.op("dve", lambda e, c0=c0, c1=c1, p_=p_: e.tensor_tensor(out=qkvf[:, c0:c1], in0=p_[:, 0:c1 - c0], in1=bqkv[:, c0:c1],
                                                                              op=ALU.add), reads=[bp_, bw], writes=[bqkvs])
                kb.op("pool", lambda e: e.tensor_tensor(out=sq[:], in0=qkv[:, 0:18, :], in1=qkv[:, 0:18, :], op=ALU.mult), reads=[bqkvs], writes=[bsq])
                kb.op("dve", lambda e: e.tensor_reduce(out=r18[:], in_=sq[:], axis=AX.X, op=ALU.add), reads=[bsq], writes=[br18])
                kb.op("dve", lambda e: e.tensor_scalar(out=r18[:], in0=r18[:], scalar1=1.0 / 64, scalar2=EPS, op0=ALU.mult, op1=ALU.add),
                      reads=[br18], writes=[br18])
                kb.op("pool", lambda e: e.tensor_tensor(out=r18[:, 0:16], in0=r18[:, 0:16], in1=self.neghalf[:, 0:16], op=ALU.pow),
                      reads=[br18, self.b_const], writes=[br18])
                kb.op("pool", lambda e: e.tensor_tensor(out=r18[:, 16:18], in0=r18[:, 16:18], in1=self.neghalf[:, 0:2], op=ALU.pow),
                      reads=[br18, self.b_const], writes=[br18])
                kb.op("dve", lambda e: e.tensor_tensor(out=qn[:], in0=qkv[:, 0:18, :], in1=bc_last(r18[:, :], 64), op=ALU.mult),
                      reads=[bqkvs, br18], writes=[bqn])
                kb.op("pool", lambda e: e.tensor_tensor(out=qn[:, 0:16, :], in0=qn[:, 0:16, :], in1=bc_mid(gq[:, :], 16), op=ALU.mult),
                      reads=[bqn, bw], writes=[bqn])
                kb.op("pool", lambda e: e.tensor_tensor(out=qn[:, 16:18, :], in0=qn[:, 16:18, :], in1=bc_mid(gk[:, :], 2), op=ALU.mult),
                      reads=[bqn, bw], writes=[bqn])
                cb = bc_mid(cos[:, t, :], 18)
                sb_ = bc_mid(sin[:, t, :], 18)
                x1_ = qn[:, :, 0:32]; x2_ = qn[:, :, 32:64]
                kb.op("dve", lambda e: e.tensor_tensor(out=rt[0][:], in0=x1_, in1=cb, op=ALU.mult), reads=[bqn, brope], writes=[brt])
                kb.op("dve", lambda e: e.tensor_tensor(out=rt[1][:], in0=x2_, in1=sb_, op=ALU.mult), reads=[bqn, brope], writes=[brt])
                kb.op("pool", lambda e: e.tensor_tensor(out=rt[2][:], in0=x2_, in1=cb, op=ALU.mult), reads=[bqn, brope], writes=[brt])
                kb.op("pool", lambda e: e.tensor_tensor(out=rt[3][:], in0=x1_, in1=sb_, op=ALU.mult), reads=[bqn, brope], writes=[brt])
                kb.op("dve", lambda e: e.tensor_tensor(out=q_bf[:, :, 0:32], in0=rt[0][:, 0:16, :], in1=rt[1][:, 0:16, :], op=ALU.subtract),
                      reads=[brt], writes=[bqb])
                kb.op("pool", lambda e: e.tensor_tensor(out=q_bf[:, :, 32:64], in0=rt[2][:, 0:16, :], in1=rt[3][:, 0:16, :], op=ALU.add),
                      reads=[brt], writes=[bqb])
                for dup in range(2):
                    kb.op("dve", lambda e, dup=dup: e.tensor_tensor(out=kdup[:, :, dup, 0:32], in0=rt[0][:, 16:18, :], in1=rt[1][:, 16:18, :],
                                                                   op=ALU.subtract), reads=[brt], writes=[bkd])
                    kb.op("pool", lambda e, dup=dup: e.tensor_tensor(out=kdup[:, :, dup, 32:64], in0=rt[2][:, 16:18, :], in1=rt[3][:, 16:18, :],
                                                                    op=ALU.add), reads=[brt], writes=[bkd])
                kb.op("act", lambda e, cur=cur: e.copy(out=Va[cur][:, :, 0:64], in_=qkv[:, 18:20, :]), reads=[bqkvs, bVones], writes=[bVa[cur]])
                for i in range(8):
                    kb.op("pe", lambda e, i=i: e.transpose(out=tp[1][:, i, :], in_=q_bf[:, 2 * i:2 * i + 2, :].rearrange("p h d -> p (h d)"),
                                                           identity=self.ident_b[:]), reads=[bqb, self.b_const], writes=[btp[1]])
                kb.op("act", lambda e: e.copy(out=qT[:], in_=tp[1][:]), reads=[btp[1]], writes=[bqT])
                for g in range(2):
                    kb.op("pe", lambda e, g=g: e.transpose(out=tp[0][:, g, :], in_=kdup[:, g, :, :].rearrange("p a d -> p (a d)"),
                                                           identity=self.ident_b[:]), reads=[bkd, self.b_const], writes=[btp[0]])
                kb.op("dve", lambda e, cur=cur: e.tensor_copy(out=kT[cur][:], in_=tp[0][:, 0:2, :]), reads=[btp[0]], writes=[bkT[cur]])
                for gq4 in range(4):
                    sbank = s2
                    bsb = bs2
                    P = PT[gu_ % 2]; bP = bPT[gu_ % 2]
                    ob = oo[gu_ % 2]; bob = boo[gu_ % 2]
                    gu_ += 1
                    for hh in range(4):
                        hq = gq4 * 4 + hh
                        i, o = hq // 2, (hq % 2) * 64
                        g = hq // 8
                        for w_, kt in ((0, prv), (1, cur)):
                            if t == 0 and w_ == 0:
                                continue
                            col = ((hh % 2) * 2 + hh // 2) * 256 + w_ * 128
                            kb.op("pe", lambda e, i=i, o=o, g=g, kt=kt, col=col, sbank=sbank: e.matmul(
                                sbank[:, col // 512, col % 512:col % 512 + 128], lhsT=kT[kt][o:o + 64, g, :], rhs=qT[o:o + 64, i, :],
                                start=True, stop=True), reads=[bkT[kt], bqT], writes=[bsb[col // 512]])
                    for bk in range(2):
                        if t == 0:
                            for hh2 in range(2):
                                kb.op("act", lambda e, bk=bk, hh2=hh2, P=P, sbank=sbank: e.activation(
                                    out=P[:, bk * 2 + hh2, 1, :], in_=sbank[:, bk, hh2 * 256 + 128:hh2 * 256 + 256], func=AF.Exp, scale=0.125),
                                    reads=[bsb[bk]], writes=[bP])
                        else:
                            kb.op("act", lambda e, bk=bk, P=P, sbank=sbank: e.activation(
                                out=P[:, bk * 2:bk * 2 + 2, :, :].rearrange("p a b c -> p (a b c)"), in_=sbank[:, bk, :], func=AF.Exp, scale=0.125),
                                reads=[bsb[bk]], writes=[bP])
                    if t == 0:
                        kb.op("dve", lambda e, P=P: e.tensor_tensor(out=P[:, :, 1, :], in0=P[:, :, 1, :], in1=mask2[:, :, 1, :], op=ALU.mult),
                              reads=[bP, bw], writes=[bP])
                    else:
                        kb.op("dve", lambda e, P=P: e.tensor_tensor(out=P[:].rearrange("p a b c -> p (a b c)"), in0=P[:].rearrange("p a b c -> p (a b c)"),
                                                                   in1=mask2[:].rearrange("p a b c -> p (a b c)"), op=ALU.mult),
                              reads=[bP, bw], writes=[bP])
                    for hh in range(4):
                        hq = gq4 * 4 + hh
                        g = hq // 8
                        sl = (hh % 2) * 2 + hh // 2
                        if t > 0:
                            kb.op("pe", lambda e, hh=hh, g=g, P=P, ob=ob, prv=prv, sl=sl: e.matmul(ob[:, hh * 65:hh * 65 + 65], lhsT=P[:, sl, 0, :],
                                                                                          rhs=Va[prv][:, g, :], start=True, stop=False),
                                  reads=[bP, bVa[prv]], writes=[bob])
                        kb.op("pe", lambda e, hh=hh, g=g, P=P, ob=ob, cur=cur, sl=sl: e.matmul(ob[:, hh * 65:hh * 65 + 65], lhsT=P[:, sl, 1, :],
                                                                                      rhs=Va[cur][:, g, :], start=(t == 0), stop=True),
                              reads=[bP, bVa[cur]], writes=[bob])
                    ov = ob[:, 0:260].rearrange("p (h d) -> p h d", d=65)
                    dsl = den[:, gq4 * 4:(gq4 + 1) * 4]
                    kb.op("dve", lambda e, ov=ov, dsl=dsl, gq4=gq4: e.tensor_tensor(out=dsl, in0=ov[:, :, 64], in1=sk[:, gq4 * 4:(gq4 + 1) * 4], op=ALU.add),
                          reads=[bob, bw], writes=[bden])
                    kb.op("dve", lambda e, dsl=dsl: e.reciprocal(out=dsl, in_=dsl), reads=[bden], writes=[bden])
                    kb.op("dve", lambda e, ov=ov, dsl=dsl, gq4=gq4: e.tensor_tensor(out=attn[:, gq4 * 4:(gq4 + 1) * 4, :], in0=ov[:, :, 0:64],
                                                                                 in1=bc_last(dsl, 64), op=ALU.mult), reads=[bob, bden], writes=[battn])
                af = attn[:].rearrange("p h d -> p (h d)")
                for k in range(8):
                    kb.op("pe", lambda e, k=k: e.transpose(out=tp[0][:, k, :], in_=af[:, k * 128:(k + 1) * 128], identity=self.ident_b[:]),
                          reads=[battn, self.b_const], writes=[btp[0]])
                kb.op("act", lambda e: e.copy(out=attT[:], in_=tp[0][:]), reads=[btp[0]], writes=[battT])
                for half in range(2):
                    hs = slice(half * 512, (half + 1) * 512)
                    for k in range(8):
                        kb.op("pe", lambda e, k=k, half=half, hs=hs: e.matmul(mm[half][:], lhsT=attT[:, k, :], rhs=w_out[:, k, hs],
                                                                            start=(k == 0), stop=(k == 7)), reads=[battT, bw], writes=[bmm[half]])
                    kb.op("dve", lambda e, half=half, hs=hs: e.tensor_tensor(out=tmp[:, hs], in0=mm[half][:], in1=bout[:, hs], op=ALU.add),
                          reads=[bmm[half], bw], writes=[btmp])
                    kb.op("pool", lambda e, hs=hs: e.tensor_tensor(out=tmp[:, hs], in0=tmp[:, hs], in1=mods["gate1"][:, hs], op=ALU.mult),
                          reads=[btmp, bmod], writes=[btmp])
                    kb.op("pool", lambda e, hs=hs: e.tensor_tensor(out=x1[:, hs], in0=tmp[:, hs], in1=X[:, hs], op=ALU.add),
                          reads=[btmp, bX], writes=[bx1])
                kb.dma("sp", S["XA"][ti * 128:(ti + 1) * 128, :], x1[:], reads=[bx1], writes=[self.db("XA", ti)])
                self.norm2_router(r, x1, bx1, mods["gmod2"], mods["shift2"], bmod, tmp, btmp, tp, btp, mm[0], bmm[0], ti)
        kb.pop()

    def build(self):
        self.setup_consts()
        ph = self.phases
        if "p0" in ph:
            self.phase0()
        if "p1a" in ph:
            self.phase1a()
        if "p1b" in ph:
            self.phase1b()
        if "p2a" in ph:
            self.phase2e(0)
            self.phase2c(0, self.S["XA"], self.S["XB"], "XB", True)
        if "p3" in ph:
            self.phase3()
        if "p2b" in ph:
            self.phase2e(1)
            self.phase2c(1, self.S["XA"], self.out, "OUT", False)
        self.kb.finish()
        return self.nc


def module_consts():
    idx = np.arange(128, dtype=np.float64)
    lg = np.log1p(-np.exp2(-5.0 - np.arange(8, dtype=np.float64)))
    k = {}
    k["k_invf16"] = (10000.0 ** (-np.arange(16, dtype=np.float32) / 16)).astype(np.float32)
    k["k_invf32"] = (10000.0 ** (-np.arange(32, dtype=np.float32) / 32)).astype(np.float32)
    diff = idx[None, :] - idx[:, None]
    dec = np.where(diff[:, None, :] >= 0, np.exp(lg[None, :, None] * np.maximum(diff[:, None, :], 0.0)), 0.0)
    k["k_decayT"] = np.ascontiguousarray(dec.reshape(128, 4, 2, 128).transpose(0, 2, 1, 3)).reshape(128, 8 * 128).astype(np.float32)
    k["k_qdec"] = np.exp(lg[None, :] * (idx + 1.0)[:, None]).astype(np.float32)
    k["k_kdec"] = np.exp(lg[None, :] * (127.0 - idx)[:, None]).astype(np.float32)
    cd = np.zeros((128, 4), np.float64)
    for i in range(4):
        cd[0:64, i] = np.exp(lg[2 * i] * 128)
        cd[64:128, i] = np.exp(lg[2 * i + 1] * 128)
    k["k_cdec"] = cd.astype(np.float32)
    return k


def make_in_maps(inputs, nseq, n_cores):
    f = lambda a: np.ascontiguousarray(np.asarray(a))
    shared = {}
    for name in ("ada_w", "ada_b", "norm1_g", "norm2_g", "router_w", "router_b", "exp_w_gu", "exp_w_down", "exp_b_down"):
        shared[name] = f(inputs[name])
    for name in ("hyb_w_in", "mla_cq_norm_g", "mla_ckv_norm_g", "mla_w_uq", "mla_w_ukv", "mla_q_head_g", "mla_k_head_g",
                 "hyb_w_out", "swa_w_qkv", "swa_b_qkv", "swa_q_head_g", "swa_k_head_g", "swa_sinks", "swa_w_out", "swa_b_out"):
        shared[name] = f(np.asarray(inputs[name])[0])
    shared["ret_norm_g"] = f(np.asarray(inputs["ret_norm_g"])[0].reshape(512))
    bgu = np.asarray(inputs["exp_b_gu"])
    shared["exp_b_gu_pj"] = f(bgu.reshape(2, 32, 16, 128).transpose(0, 1, 3, 2))
    shared.update(module_consts())
    x = np.asarray(inputs["x"]); c = np.asarray(inputs["c"]); pos = np.asarray(inputs["positions"])
    maps = []
    for i in range(n_cores):
        b0 = i * nseq
        m = dict(shared)
        m["x"] = f(x[b0:b0 + nseq].reshape(nseq * SEQ, D))
        m["c_pk"] = f(c[b0:b0 + nseq].reshape(nseq, 8, 128).transpose(0, 2, 1))
        m["pos_pt"] = f(pos[b0:b0 + nseq].reshape(nseq, NT, 128).transpose(0, 2, 1).astype(np.int32))
        maps.append(m)
    return maps


_PROG = {}


def kernel(**inputs):
    nseq = 32 // N_CORES
    if "nc" not in _PROG:
        _PROG["nc"] = Prog(nseq).build()
    maps = make_in_maps(inputs, nseq, N_CORES)
    res = run_bass_kernel_spmd(_PROG["nc"], maps, core_ids=list(range(N_CORES)))
    out = np.concatenate([np.asarray(r["out"]).reshape(nseq, SEQ, D) for r in res.results], axis=0)
    return out.astype(np.float32)
```

```python
import contextlib
import os
import math
import numpy as np
import concourse.bass as bass
import concourse.mybir as mybir
from concourse.bass_utils import run_bass_kernel_spmd

F32 = mybir.dt.float32
BF16 = mybir.dt.bfloat16
I32 = mybir.dt.int32
AF = mybir.ActivationFunctionType
ALU = mybir.AluOpType
AX = mybir.AxisListType

SAME_ENGINE_SYNC = os.environ.get('KSES', '1') == '1'
DMA_RING = 6
N_CORES = 8
_STOP = float(os.environ.get('KSTOP', '99'))
SEQ = 2048
D = 1024
NT = SEQ // 128
EPS = 1e-6
PI = math.pi


class Buf:
    __slots__ = ("w", "rs")

    def __init__(self):
        self.w = None
        self.rs = {}


class KB:
    ENGS = ("pe", "act", "dve", "pool", "sp")

    def __init__(self, nc):
        self.nc = nc
        self.stacks = [contextlib.ExitStack()]
        self.eng = dict(pe=nc.tensor, act=nc.scalar, dve=nc.vector, pool=nc.gpsimd, sp=nc.sync)
        self.cnt = {e: 0 for e in self.ENGS}
        self.seen = {e: {} for e in self.ENGS}
        self.sems = {}
        for e in self.ENGS:
            self.sems[e] = self.stacks[0].enter_context(nc.semaphore("s_" + e))
        self.dma_n = {}
        for e in ("sp", "pool", "act"):
            self.dma_n[e] = 0
            for j in range(DMA_RING):
                self.sems[("d", e, j)] = self.stacks[0].enter_context(nc.semaphore("d_%s_%d" % (e, j)))
        self.uid = 0

    def push(self):
        self.phase_id = getattr(self, "phase_id", 0) + 1
        self.stacks.append(contextlib.ExitStack())

    def pop(self):
        self.barrier()
        self.stacks.pop().close()

    def sb(self, name, shape, dt):
        self.uid += 1
        return self.stacks[-1].enter_context(self.nc.sbuf_tensor("%s_%d" % (name, self.uid), list(shape), dt))

    def ps(self, name, shape, dt):
        self.uid += 1
        return self.stacks[-1].enter_context(self.nc.psum_tensor("%s_%d" % (name, self.uid), list(shape), dt))

    def _deps(self, e, reads, writes):
        toks = {}
        for b in reads:
            if b.w is not None and toks.get(b.w[0], 0) < b.w[1]:
                toks[b.w[0]] = b.w[1]
        for b in writes:
            if b.w is not None and toks.get(b.w[0], 0) < b.w[1]:
                toks[b.w[0]] = b.w[1]
            for k, v in b.rs.items():
                if toks.get(k, 0) < v:
                    toks[k] = v
        waits = []
        seen = self.seen[e]
        for k, v in toks.items():
            if k == e and (e == "pe" or not SAME_ENGINE_SYNC):
                continue
            if seen.get(k, 0) < v:
                seen[k] = v
                waits.append((k, v))
        return waits

    def _mark(self, tok, reads, writes):
        k, v = tok
        for b in reads:
            if b.rs.get(k, 0) < v:
                b.rs[k] = v
        for b in writes:
            b.w = tok
            b.rs = {}

    def _emit(self, e, waits, fn, key, inc):
        engine = self.eng[e]
        for k, v in waits:
            engine.wait_ge(self.sems[k], v)
        if fn is not None:
            fn(engine).then_inc(self.sems[key], inc)

    def op(self, e, fn, reads=(), writes=()):
        waits = self._deps(e, reads, writes)
        self.cnt[e] += 1
        tok = (e, self.cnt[e])
        self._emit(e, waits, fn, e, 1)
        self._mark(tok, reads, writes)
        self._handoff()
        return tok

    def dma(self, e, out, in_, reads=(), writes=(), **kw):
        waits = self._deps(e, reads, writes)
        n = self.dma_n[e]
        self.dma_n[e] += 1
        key = ("d", e, n % DMA_RING)
        val = 16 * (n // DMA_RING + 1)
        if n >= DMA_RING and self.seen[e].get(key, 0) < val - 16:
            self.seen[e][key] = val - 16
            waits.append((key, val - 16))
        tok = (key, val)
        self._emit(e, waits, (lambda eng: eng.dma_start(out=out, in_=in_, **kw)), key, 16)
        self._mark(tok, reads, writes)
        self._handoff()
        return tok

    def idma(self, out, out_idx, in_, in_idx, bound, reads=(), writes=()):
        e = "pool"
        waits = self._deps(e, reads, writes)
        n = self.dma_n[e]
        self.dma_n[e] += 1
        key = ("d", e, n % DMA_RING)
        val = 16 * (n // DMA_RING + 1)
        if n >= DMA_RING and self.seen[e].get(key, 0) < val - 16:
            self.seen[e][key] = val - 16
            waits.append((key, val - 16))
        if not hasattr(self, "_bregs"):
            self._bregs = {}
        if bound not in self._bregs:
            self._bregs[bound] = self.nc.gpsimd.to_reg(bound)
        bound = self._bregs[bound]
        oo_ = bass.IndirectOffsetOnAxis(ap=out_idx, axis=0) if out_idx is not None else None
        io_ = bass.IndirectOffsetOnAxis(ap=in_idx, axis=0) if in_idx is not None else None
        self._emit(e, waits, (lambda eng: eng.indirect_dma_start(out=out, out_offset=oo_, in_=in_, in_offset=io_,
                                                                 bounds_check=bound, oob_is_err=False)), key, 16)
        self._mark((key, val), reads, writes)
        self._handoff()

    def _handoff(self):
        st = getattr(self, "_il", None)
        if st is None:
            return
        me = getattr(st["tls"], "idx", None)
        if me is None:
            return
        cv = st["cv"]
        with cv:
            if st["alive"][1 - me]:
                st["turn"] = 1 - me
                cv.notify_all()
                while st["turn"] != me:
                    cv.wait()

    def interleave(self, fa, fb):
        import threading
        if fa is None or fb is None:
            (fa or fb)()
            return
        st = dict(cv=threading.Condition(), turn=0, alive=[True, True], tls=threading.local(), err=[])
        self._il = st

        def runner(i, f):
            cv = st["cv"]
            with cv:
                while st["turn"] != i:
                    cv.wait()
            st["tls"].idx = i
            try:
                f()
            except BaseException as ex:
                st["err"].append(ex)
            finally:
                with cv:
                    st["alive"][i] = False
                    st["turn"] = 1 - i
                    cv.notify_all()

        ths = [threading.Thread(target=runner, args=(i, f)) for i, f in enumerate((fa, fb))]
        for th in ths:
            th.start()
        for th in ths:
            th.join()
        self._il = None
        if st["err"]:
            raise st["err"][0]

    def pipeline(self, stage_a, stage_b, n):
        stage_a(0)
        for t in range(n):
            self.interleave((lambda t=t: stage_a(t + 1)) if t + 1 < n else None, lambda t=t: stage_b(t))

    def all_tokens(self):
        toks = [(e, self.cnt[e]) for e in self.ENGS if self.cnt[e] > 0]
        for e in ("sp", "pool", "act"):
            n = self.dma_n[e]
            for j in range(DMA_RING):
                c = (n - j + DMA_RING - 1) // DMA_RING if n > j else 0
                if c > 0:
                    toks.append((("d", e, j), 16 * c))
        return toks

    def barrier(self):
        toks = self.all_tokens()
        for e in self.ENGS:
            waits = []
            for k, v in toks:
                if k == e:
                    continue
                if self.seen[e].get(k, 0) < v:
                    self.seen[e][k] = v
                    waits.append((k, v))
            self._emit(e, waits, None, None, 0)

    def finish(self):
        self.barrier()
        while self.stacks:
            self.stacks.pop().close()


def bc_mid(ap2, n):
    return ap2.unsqueeze(1).broadcast_to([ap2.shape[0], n, ap2.shape[1]])


def bc_last(ap2, n):
    return ap2.unsqueeze(2).broadcast_to([ap2.shape[0], ap2.shape[1], n])


class Prog:
    def __init__(self, nseq, debug=False, phases=("p0", "p1a", "p1b", "p2a", "p3", "p2b"), ntl=NT, cap_tiles=None):
        self.nseq = nseq
        self.ntl = ntl
        if cap_tiles is None:
            mean = nseq * ntl * 128 * 4 // 32
            cap_tiles = max(4, 4 * ((2 * mean + 511) // 512))
        self.cap = cap_tiles * 128
        self.debug = debug
        self.phases = phases
        nc = self.nc = bass.Bass("TRN2", target_bir_lowering=False)
        self.kb = KB(nc)
        ntok = nseq * SEQ
        self.ntok = ntok

        def inp(name, shape, dt=F32):
            return nc.dram_tensor(name, list(shape), dt, kind="ExternalInput").ap()

        def scr(name, shape, dt=F32):
            kind = "ExternalOutput" if debug else "Internal"
            return nc.dram_tensor(name, list(shape), dt, kind=kind).ap()

        I = self.I = {}
        I["x"] = inp("x", [ntok, D])
        I["c_pk"] = inp("c_pk", [nseq, 128, 8])
        I["pos_pt"] = inp("pos_pt", [nseq, 128, NT], I32)
        I["ada_w"] = inp("ada_w", [2, D, 6 * D])
        I["ada_b"] = inp("ada_b", [2, 6 * D])
        I["norm1_g"] = inp("norm1_g", [2, D])
        I["norm2_g"] = inp("norm2_g", [2, D])
        I["hyb_w_in"] = inp("hyb_w_in", [D, 2720])
        I["mla_cq_norm_g"] = inp("mla_cq_norm_g", [384])
        I["mla_ckv_norm_g"] = inp("mla_ckv_norm_g", [256])
        I["mla_w_uq"] = inp("mla_w_uq", [384, 768])
        I["mla_w_ukv"] = inp("mla_w_ukv", [256, 1024])
        I["mla_q_head_g"] = inp("mla_q_head_g", [96])
        I["mla_k_head_g"] = inp("mla_k_head_g", [96])
        I["ret_norm_g"] = inp("ret_norm_g", [512])
        I["hyb_w_out"] = inp("hyb_w_out", [D, D])
        I["swa_w_qkv"] = inp("swa_w_qkv", [D, 1280])
        I["swa_b_qkv"] = inp("swa_b_qkv", [1280])
        I["swa_q_head_g"] = inp("swa_q_head_g", [64])
        I["swa_k_head_g"] = inp("swa_k_head_g", [64])
        I["swa_sinks"] = inp("swa_sinks", [16])
        I["swa_w_out"] = inp("swa_w_out", [D, D])
        I["swa_b_out"] = inp("swa_b_out", [D])
        I["router_w"] = inp("router_w", [2, D, 32])
        I["router_b"] = inp("router_b", [2, 32])
        I["exp_w_gu"] = inp("exp_w_gu", [2, 32, D, 2048])
        I["exp_b_gu_pj"] = inp("exp_b_gu_pj", [2, 32, 128, 16])
        I["exp_w_down"] = inp("exp_w_down", [2, 32, D, D])
        I["exp_b_down"] = inp("exp_b_down", [2, 32, D])
        I["k_invf16"] = inp("k_invf16", [16])
        I["k_invf32"] = inp("k_invf32", [32])
        I["k_decayT"] = inp("k_decayT", [128, 8 * 128])
        I["k_qdec"] = inp("k_qdec", [128, 8])
        I["k_kdec"] = inp("k_kdec", [128, 8])
        I["k_cdec"] = inp("k_cdec", [128, 4])

        S = self.S = {}
        S["MOD"] = scr("MOD", [nseq, 2, 6, 128, D])
        S["ATT"] = scr("ATT", [nseq * NT, 128, 512], BF16)
        S["XA"] = scr("XA", [ntok, D])
        S["XB"] = scr("XB", [ntok, D])
        S["H2T"] = scr("H2T", [nseq * NT, 128, 8, 128], BF16)
        S["GS"] = scr("GS", [nseq * NT, 128, 32])
        S["XG"] = scr("XG", [32 * self.cap, D], BF16)
        S["YG"] = scr("YG", [32 * self.cap, D])
        S["SLOT"] = scr("SLOT", [nseq * NT, 128, 4], I32)
        S["GK"] = scr("GK", [nseq * NT, 128, 4])
        self.out = nc.dram_tensor("out", [ntok, D], F32, kind="ExternalOutput").ap()
        self.dbufs = {}

    def db(self, name, idx):
        k = (name, idx)
        if k not in self.dbufs:
            self.dbufs[k] = Buf()
        return self.dbufs[k]

    def setup_consts(self):
        kb = self.kb
        self.ident_b = kb.sb("ident_b", [128, 128], BF16)
        self.ident_f = kb.sb("ident_f", [128, 128], F32)
        self.b_const = Buf()
        bc = self.b_const
        for idt in (self.ident_b, self.ident_f):
            kb.op("pool", lambda e, idt=idt: e.memset(idt[:], 1.0), writes=[bc])
            kb.op("pool", lambda e, idt=idt: e.affine_select(out=idt[:], in_=idt[:], pattern=[[-1, 128]],
                                                             compare_op=ALU.is_equal, fill=0.0, base=0,
                                                             channel_multiplier=1), reads=[bc], writes=[bc])
        self.mask_le = kb.sb("mask_le", [128, 128], BF16)
        self.mask_gt = kb.sb("mask_gt", [128, 128], BF16)
        kb.op("pool", lambda e: e.memset(self.mask_le[:], 1.0), writes=[bc])
        kb.op("pool", lambda e: e.affine_select(out=self.mask_le[:], in_=self.mask_le[:], pattern=[[1, 128]],
                                                compare_op=ALU.is_ge, fill=0.0, base=0, channel_multiplier=-1),
              reads=[bc], writes=[bc])
        kb.op("pool", lambda e: e.memset(self.mask_gt[:], 1.0), writes=[bc])
        kb.op("pool", lambda e: e.affine_select(out=self.mask_gt[:], in_=self.mask_gt[:], pattern=[[-1, 128]],
                                                compare_op=ALU.is_gt, fill=0.0, base=0, channel_multiplier=1),
              reads=[bc], writes=[bc])
        self.U_b = kb.sb("U_b", [128, 128], BF16)
        self.ones_b = kb.sb("ones_b", [128, 128], BF16)
        kb.op("pool", lambda e: e.memset(self.ones_b[:], 1.0), writes=[bc])
        kb.op("pool", lambda e: e.memset(self.U_b[:], 1.0), writes=[bc])
        kb.op("pool", lambda e: e.affine_select(out=self.U_b[:], in_=self.U_b[:], pattern=[[1, 128]],
                                                compare_op=ALU.is_ge, fill=0.0, base=-1, channel_multiplier=-1),
              reads=[bc], writes=[bc])
        iot_i = kb.sb("iot_i", [128, 32], I32)
        self.iotaE = kb.sb("iotaE", [128, 32], F32)
        kb.op("pool", lambda e: e.iota(out=iot_i[:], pattern=[[1, 32]], base=0, channel_multiplier=0), writes=[bc])
        kb.op("dve", lambda e: e.tensor_copy(out=self.iotaE[:], in_=iot_i[:]), reads=[bc], writes=[bc])
        kb.op("dve", lambda e: e.tensor_scalar(out=self.iotaE[:], in0=self.iotaE[:], scalar1=float(self.cap), scalar2=None,
                                               op0=ALU.mult), reads=[bc], writes=[bc])
        self.neghalf = kb.sb("neghalf", [128, 16], F32)
        kb.op("pool", lambda e: e.memset(self.neghalf[:], -0.5), writes=[bc])

    def rstd_of(self, ss, n, width, bss, tag):
        kb = self.kb
        kb.op("dve", lambda e: e.tensor_scalar(out=ss, in0=ss, scalar1=1.0 / n, scalar2=EPS,
                                               op0=ALU.mult, op1=ALU.add), reads=[bss], writes=[bss])
        kb.op("pool", lambda e: e.tensor_tensor(out=ss, in0=ss, in1=self.neghalf[:, 0:width], op=ALU.pow),
              reads=[bss, self.b_const], writes=[bss])

    def rope_tables(self, s, half, invf_name, tag):
        kb, I = self.kb, self.I
        key = (kb.phase_id, half)
        if not hasattr(self, "_rope"):
            self._rope = {}
        if key not in self._rope:
            self._rope[key] = dict(
                b=Buf(),
                pos_i=kb.sb("pos_i", [128, NT], I32), pos_f=kb.sb("pos_f", [128, NT], F32), invf=kb.sb("invf", [128, half], F32),
                ang=kb.sb("ang", [128, NT, half], F32), kq=kb.sb("kq", [128, NT, half], F32), ki=kb.sb("ki", [128, NT, half], I32),
                ys=kb.sb("ys", [128, NT, half], F32), mm=kb.sb("mmk", [128, NT, half], F32),
                cos=kb.sb("cos", [128, NT, half], F32), sin=kb.sb("sin", [128, NT, half], F32))
        R_ = self._rope[key]
        b = R_["b"]
        pos_i, pos_f, invf, ang, kq, ki, ys, mm, cos, sin = (R_[n] for n in ("pos_i", "pos_f", "invf", "ang", "kq", "ki", "ys", "mm", "cos", "sin"))
        kb.dma("sp", pos_i[:], I["pos_pt"][s], writes=[b])
        kb.dma("sp", invf[:], I[invf_name].partition_broadcast(128), writes=[b])
        kb.op("dve", lambda e: e.tensor_copy(out=pos_f[:], in_=pos_i[:]), reads=[b], writes=[b])
        kb.op("dve", lambda e: e.tensor_tensor(out=ang[:], in0=bc_last(pos_f[:, :], half), in1=bc_mid(invf[:, :], NT),
                                               op=ALU.mult), reads=[b], writes=[b])
        kb.op("dve", lambda e: e.tensor_scalar(out=kq[:], in0=ang[:], scalar1=1.0 / (2 * PI), scalar2=None,
                                               op0=ALU.mult), reads=[b], writes=[b])
        kb.op("dve", lambda e: e.tensor_copy(out=ki[:], in_=kq[:]), reads=[b], writes=[b])
        kb.op("dve", lambda e: e.tensor_copy(out=kq[:], in_=ki[:]), reads=[b], writes=[b])
        kb.op("dve", lambda e: e.scalar_tensor_tensor(out=ang[:], in0=kq[:], scalar=-2 * PI, in1=ang[:],
                                                      op0=ALU.mult, op1=ALU.add), reads=[b], writes=[b])
        lim = 3.1415925
        for shift, dst in ((0.0, sin), (PI / 2, cos)):
            kb.op("dve", lambda e, shift=shift: e.tensor_scalar(out=ys[:], in0=ang[:], scalar1=shift, scalar2=None,
                                                                op0=ALU.add), reads=[b], writes=[b])
            kb.op("dve", lambda e: e.tensor_scalar(out=mm[:], in0=ys[:], scalar1=PI, scalar2=-2 * PI,
                                                   op0=ALU.is_gt, op1=ALU.mult), reads=[b], writes=[b])
            kb.op("dve", lambda e: e.tensor_tensor(out=ys[:], in0=ys[:], in1=mm[:], op=ALU.add), reads=[b], writes=[b])
            kb.op("dve", lambda e: e.tensor_scalar(out=ys[:], in0=ys[:], scalar1=lim, scalar2=-lim,
                                                   op0=ALU.min, op1=ALU.max), reads=[b], writes=[b])
            kb.op("act", lambda e, dst=dst: e.activation(out=dst[:], in_=ys[:], func=AF.Sin), reads=[b], writes=[b])
        return cos, sin, b

    def load_w_bf16(self, dst, src, kchunks, bw):
        v = src.rearrange("(k p) n -> p k n", p=128)
        for k in range(kchunks):
            self.kb.dma("pool", dst[:, k, :], v[:, k, :], writes=[bw])

    def bcast_load(self, dst, src1d, b):
        self.kb.dma("sp", dst, src1d.partition_broadcast(128), writes=[b])

    def norm_mod_T(self, x_t, bx, gmod, shift, bmod, tmp, btmp, h_bf, bh, tp, btp, hT, bhT, ss, bss):
        kb = self.kb
        kb.op("dve", lambda e: e.scalar_tensor_tensor(out=tmp[:], in0=x_t[:], scalar=1.0, in1=x_t[:], op0=ALU.mult, op1=ALU.mult, accum_out=ss[:, 0:1]),
              reads=[bx], writes=[btmp, bss])
        self.rstd_of(ss[:, 0:1], D, 1, bss, "")
        kb.op("dve", lambda e: e.scalar_tensor_tensor(out=tmp[:], in0=x_t[:], scalar=ss[:, 0:1], in1=gmod[:],
                                                      op0=ALU.mult, op1=ALU.mult), reads=[bx, bss, bmod], writes=[btmp])
        kb.op("pool", lambda e: e.tensor_tensor(out=h_bf[:], in0=tmp[:], in1=shift[:], op=ALU.add),
              reads=[btmp, bmod], writes=[bh])
        for k in range(8):
            kb.op("pe", lambda e, k=k: e.transpose(out=tp[:, k, :], in_=h_bf[:, k * 128:(k + 1) * 128],
                                                   identity=self.ident_b[:]), reads=[bh, self.b_const], writes=[btp])
        kb.op("act", lambda e: e.copy(out=hT[:], in_=tp[:]), reads=[btp], writes=[bhT])

    def phase0(self):
        kb, I, S, nseq = self.kb, self.I, self.S, self.nseq
        kb.push()
        cin = kb.sb("cin", [128, nseq, 8], F32)
        cact = kb.sb("cact", [128, nseq, 8], F32)
        crep = kb.sb("crep", [128, nseq, 8, 128], F32)
        bcr = Buf()
        for s in range(nseq):
            kb.dma("sp", cin[:, s, :], I["c_pk"][s], writes=[bcr])
        kb.op("act", lambda e: e.activation(out=cact[:], in_=cin[:], func=AF.Silu), reads=[bcr], writes=[bcr])
        for s in range(nseq):
            kb.op("dve", lambda e, s=s: e.tensor_copy(out=crep[:, s, :, :], in_=bc_last(cact[:, s, :], 128)),
                  reads=[bcr], writes=[bcr])
        adab = kb.sb("adab", [128, 6 * D], F32)
        ng = [kb.sb("ng1", [128, D], F32), kb.sb("ng2", [128, D], F32)]
        bab = Buf()
        wch = [kb.sb("wch%d" % i, [128, 8, 512], F32) for i in range(2)]
        bwch = [Buf(), Buf()]
        pm = [kb.ps("p0pm%d" % i, [128, 512], F32) for i in range(2)]
        bpm = [Buf(), Buf()]
        modt = [kb.sb("modt%d" % i, [128, 512], F32) for i in range(3)]
        bmodt = [Buf() for _ in range(3)]
        n = 0
        u = 0
        for l in range(2):
            for q in range(6):
                kb.dma("sp", adab[:, q * D:(q + 1) * D], I["ada_b"][l, q * D:(q + 1) * D].partition_broadcast(128),
                       writes=[bab])
            self.bcast_load(ng[0][:], I["norm1_g"][l], bab)
            self.bcast_load(ng[1][:], I["norm2_g"][l], bab)
            for j in range(12):
                wv = I["ada_w"][l][:, j * 512:(j + 1) * 512].rearrange("(k p) n -> p k n", p=128)
                w_ = wch[n % 2]
                kb.dma("sp", w_[:], wv, writes=[bwch[n % 2]])
                for s in range(nseq):
                    p_ = pm[u % 2]
                    for k in range(8):
                        kb.op("pe", lambda e, k=k, s=s, p_=p_, w_=w_: e.matmul(p_[:], lhsT=crep[:, s, k, :], rhs=w_[:, k, :],
                                                                             start=(k == 0), stop=(k == 7)),
                              reads=[bcr, bwch[n % 2]], writes=[bpm[u % 2]])
                    m_ = modt[u % 3]
                    bm_ = bmodt[u % 3]
                    kb.op("dve", lambda e, p_=p_, m_=m_, j=j: e.tensor_tensor(out=m_[:], in0=p_[:],
                                                                           in1=adab[:, j * 512:(j + 1) * 512], op=ALU.add),
                          reads=[bpm[u % 2], bab], writes=[bm_])
                    part, half = j // 2, j % 2
                    if part in (1, 4):
                        g_ = ng[0] if part == 1 else ng[1]
                        kb.op("dve", lambda e, m_=m_, g_=g_, half=half: e.scalar_tensor_tensor(
                            out=m_[:], in0=m_[:], scalar=1.0, in1=g_[:, half * 512:(half + 1) * 512],
                            op0=ALU.add, op1=ALU.mult), reads=[bm_, bab], writes=[bm_])
                    kb.dma("sp", S["MOD"][s, l, part, :, half * 512:(half + 1) * 512], m_[:],
                           reads=[bm_], writes=[self.db("MOD", (s, l, part))])
                    u += 1
                n += 1
        kb.pop()

    def phase1a(self):
        kb, I, S, nseq = self.kb, self.I, self.S, self.nseq
        kb.push()
        self.zero_xg()
        bw = Buf()
        w_in = kb.sb("w_in_a", [128, 8, 672], BF16)
        w_uq = kb.sb("w_uq", [128, 3, 768], BF16)
        w_ukv = kb.sb("w_ukv", [128, 2, 1024], BF16)
        self.load_w_bf16(w_in, I["hyb_w_in"][:, 0:672], 8, bw)
        self.load_w_bf16(w_uq, I["mla_w_uq"], 3, bw)
        self.load_w_bf16(w_ukv, I["mla_w_ukv"], 2, bw)
        gcq = kb.sb("gcq", [128, 384], F32)
        gckv = kb.sb("gckv", [128, 256], F32)
        gq = kb.sb("gq", [128, 96], F32)
        gk = kb.sb("gk", [128, 96], F32)
        self.bcast_load(gcq[:], I["mla_cq_norm_g"], bw)
        self.bcast_load(gckv[:], I["mla_ckv_norm_g"], bw)
        self.bcast_load(gq[:], I["mla_q_head_g"], bw)
        self.bcast_load(gk[:], I["mla_k_head_g"], bw)

        kT = kb.sb("kT", [96, 8, SEQ], BF16)
        V = kb.sb("Vc", [128, NT, 8, 65], BF16)
        bkT = [Buf() for _ in range(NT)]
        bV = [Buf() for _ in range(NT)]
        bVones = Buf()
        kb.op("pool", lambda e: e.memset(V[:, :, :, 64:65], 1.0), writes=[bVones])

        gmod = kb.sb("gmod1", [128, D], F32)
        shift = kb.sb("shift1", [128, D], F32)
        bmod = Buf()
        x_t = [kb.sb("x_t%d" % i, [128, D], F32) for i in range(2)]
        bx = [Buf(), Buf()]
        tmp = kb.sb("tmp", [128, D], F32); btmp = Buf()
        h_bf = kb.sb("h_bf", [128, D], BF16); bh = Buf()
        hT = kb.sb("hT", [128, 8, 128], BF16); bhT = Buf()
        ss = kb.sb("ss", [128, 4], F32); bss = Buf()
        proj = kb.sb("proj", [128, 672], F32); bproj = Buf()
        sq = kb.sb("sq", [128, 8, 96], F32); bsq = Buf()
        cqn = kb.sb("cqn", [128, 384], BF16); bcqn = Buf()
        cqT = kb.sb("cqT", [128, 3, 128], BF16); bcqT = Buf()
        ckvn = kb.sb("ckvn", [128, 256], BF16); bckvn = Buf()
        ckvT = kb.sb("ckvT", [128, 2, 128], BF16); bckvT = Buf()
        q_sb = kb.sb("q_sb", [128, 8, 96], F32); bq = Buf()
        qn = kb.sb("qn", [128, 8, 96], F32); bqn = Buf()
        rq8 = kb.sb("rq8", [128, 16], F32); brq8 = Buf()
        R = kb.sb("Rr", [128, 8, 96], F32); bR = Buf()
        q_full = kb.sb("q_full", [128, 8, 96], BF16); bqf = Buf()
        qTs = [kb.sb("qT%d" % i, [96, 8, 128], BF16) for i in range(2)]; bqTs = [Buf(), Buf()]
        kv_sb = kb.sb("kv_sb", [128, 8, 128], F32); bkv = Buf()
        k_full = kb.sb("k_full", [128, 8, 96], BF16); bkf = Buf()
        kr = kb.sb("kr", [128, 32], F32); bkr = Buf()
        kr2 = kb.sb("kr2", [128, 32], F32)
        rt = [kb.sb("rt%d" % i, [128, 8, 16], F32) for i in range(4)]; brt = Buf()
        PT = [kb.sb("PT%d" % i, [128, 4, 128], BF16) for i in range(3)]; bPT = [Buf() for _ in range(3)]
        attn = kb.sb("attn", [128, 8, 64], BF16); battn = Buf()
        rden = kb.sb("rden", [128, 8], F32); brden = Buf()

        tp = [kb.ps("tp%d" % i, [128, 8, 128], BF16) for i in range(2)]; btp = [Buf(), Buf()]
        mm = [kb.ps("mm%d" % i, [128, 512], F32) for i in range(2)]; bmm = [Buf(), Buf()]
        s2 = kb.ps("s2", [128, 2, 512], F32); bs2 = [Buf(), Buf()]
        oo = [kb.ps("oo%d" % i, [128, 512], F32) for i in range(2)]; boo = [Buf(), Buf()]
        scale = 96.0 ** -0.5
        uc = {"n": 0}
        for s in range(nseq):
            cos, sin, brope = self.rope_tables(s, 16, "k_invf16", "a%d" % s)
            kb.dma("sp", gmod[:], S["MOD"][s, 0, 1], reads=[self.db("MOD", (s, 0, 1))], writes=[bmod])
            kb.dma("sp", shift[:], S["MOD"][s, 0, 0], reads=[self.db("MOD", (s, 0, 0))], writes=[bmod])
            def stage_a(t, s=s, cos=cos, sin=sin, brope=brope):
                X = x_t[t % 2]; bX = bx[t % 2]
                qT = qTs[t % 2]; bqT = bqTs[t % 2]
                kb.dma("sp", X[:], I["x"][(s * NT + t) * 128:(s * NT + t + 1) * 128, :], writes=[bX])
                self.norm_mod_T(X, bX, gmod, shift, bmod, tmp, btmp, h_bf, bh, tp[0], btp[0], hT, bhT, ss, bss)
                for gi, (c0, c1) in enumerate(((0, 512), (512, 672))):
                    for k in range(8):
                        kb.op("pe", lambda e, k=k, gi=gi, c0=c0, c1=c1: e.matmul(mm[gi][:, 0:c1 - c0], lhsT=hT[:, k, :],
                                                                              rhs=w_in[:, k, c0:c1], start=(k == 0), stop=(k == 7)),
                              reads=[bhT, bw], writes=[bmm[gi]])
                    kb.op("act", lambda e, gi=gi, c0=c0, c1=c1: e.copy(out=proj[:, c0:c1], in_=mm[gi][:, 0:c1 - c0]),
                          reads=[bmm[gi]], writes=[bproj])
                for ci, (c0, w) in enumerate(((0, 384), (384, 256), (640, 32))):
                    kb.op("dve", lambda e, c0=c0, w=w, ci=ci: e.scalar_tensor_tensor(out=tmp[:, 0:w], in0=proj[:, c0:c0 + w], scalar=1.0, in1=proj[:, c0:c0 + w], op0=ALU.mult, op1=ALU.mult, accum_out=ss[:, 1 + ci:2 + ci]), reads=[bproj], writes=[btmp, bss])
                kb.op("dve", lambda e: e.tensor_scalar(out=ss[:, 1:2], in0=ss[:, 1:2], scalar1=1.0 / 384, scalar2=EPS,
                                                       op0=ALU.mult, op1=ALU.add), reads=[bss], writes=[bss])
                kb.op("dve", lambda e: e.tensor_scalar(out=ss[:, 2:3], in0=ss[:, 2:3], scalar1=1.0 / 256, scalar2=EPS,
                                                       op0=ALU.mult, op1=ALU.add), reads=[bss], writes=[bss])
                kb.op("dve", lambda e: e.tensor_scalar(out=ss[:, 3:4], in0=ss[:, 3:4], scalar1=1.0 / 32, scalar2=EPS,
                                                       op0=ALU.mult, op1=ALU.add), reads=[bss], writes=[bss])
                kb.op("pool", lambda e: e.tensor_tensor(out=ss[:, 1:4], in0=ss[:, 1:4], in1=self.neghalf[:, 0:3], op=ALU.pow),
                      reads=[bss, self.b_const], writes=[bss])
                kb.op("dve", lambda e: e.scalar_tensor_tensor(out=cqn[:], in0=proj[:, 0:384], scalar=ss[:, 1:2], in1=gcq[:],
                                                              op0=ALU.mult, op1=ALU.mult), reads=[bproj, bss, bw], writes=[bcqn])
                kb.op("dve", lambda e: e.scalar_tensor_tensor(out=ckvn[:], in0=proj[:, 384:640], scalar=ss[:, 2:3], in1=gckv[:],
                                                              op0=ALU.mult, op1=ALU.mult), reads=[bproj, bss, bw], writes=[bckvn])
                kb.op("dve", lambda e: e.scalar_tensor_tensor(out=kr[:], in0=proj[:, 640:672], scalar=ss[:, 3:4], in1=gk[:, 64:96],
                                                              op0=ALU.mult, op1=ALU.mult), reads=[bproj, bss, bw], writes=[bkr])
                for k in range(3):
                    kb.op("pe", lambda e, k=k: e.transpose(out=tp[1][:, k, :], in_=cqn[:, k * 128:(k + 1) * 128],
                                                           identity=self.ident_b[:]), reads=[bcqn, self.b_const], writes=[btp[1]])
                for k in range(2):
                    kb.op("pe", lambda e, k=k: e.transpose(out=tp[1][:, 3 + k, :], in_=ckvn[:, k * 128:(k + 1) * 128],
                                                           identity=self.ident_b[:]), reads=[bckvn, self.b_const], writes=[btp[1]])
                kb.op("act", lambda e: e.copy(out=cqT[:], in_=tp[1][:, 0:3, :]), reads=[btp[1]], writes=[bcqT])
                kb.op("act", lambda e: e.copy(out=ckvT[:], in_=tp[1][:, 3:5, :]), reads=[btp[1]], writes=[bckvT])
                for gi, (c0, c1) in enumerate(((0, 512), (512, 768))):
                    for k in range(3):
                        kb.op("pe", lambda e, k=k, gi=gi, c0=c0, c1=c1: e.matmul(mm[gi][:, 0:c1 - c0], lhsT=cqT[:, k, :],
                                                                              rhs=w_uq[:, k, c0:c1], start=(k == 0), stop=(k == 2)),
                              reads=[bcqT, bw], writes=[bmm[gi]])
                    kb.op("act", lambda e, gi=gi, c0=c0, c1=c1: e.copy(
                        out=q_sb[:].rearrange("p h d -> p (h d)")[:, c0:c1], in_=mm[gi][:, 0:c1 - c0]),
                        reads=[bmm[gi]], writes=[bq])
                kb.op("pool", lambda e: e.tensor_tensor(out=sq[:], in0=q_sb[:], in1=q_sb[:], op=ALU.mult), reads=[bq], writes=[bsq])
                kb.op("dve", lambda e: e.tensor_reduce(out=rq8[:, 0:8], in_=sq[:, :, 0:64], axis=AX.X, op=ALU.add),
                      reads=[bsq], writes=[brq8])
                kb.op("dve", lambda e: e.tensor_reduce(out=rq8[:, 8:16], in_=sq[:, :, 64:96], axis=AX.X, op=ALU.add),
                      reads=[bsq], writes=[brq8])
                kb.op("dve", lambda e: e.tensor_scalar(out=rq8[:, 0:8], in0=rq8[:, 0:8], scalar1=1.0 / 64, scalar2=EPS,
                                                       op0=ALU.mult, op1=ALU.add), reads=[brq8], writes=[brq8])
                kb.op("dve", lambda e: e.tensor_scalar(out=rq8[:, 8:16], in0=rq8[:, 8:16], scalar1=1.0 / 32, scalar2=EPS,
                                                       op0=ALU.mult, op1=ALU.add), reads=[brq8], writes=[brq8])
                kb.op("pool", lambda e: e.tensor_tensor(out=rq8[:], in0=rq8[:], in1=self.neghalf[:, 0:16], op=ALU.pow),
                      reads=[brq8, self.b_const], writes=[brq8])
                kb.op("dve", lambda e: e.tensor_tensor(out=qn[:, :, 0:64], in0=q_sb[:, :, 0:64], in1=bc_last(rq8[:, 0:8], 64),
                                                       op=ALU.mult), reads=[bq, brq8], writes=[bqn])
                kb.op("dve", lambda e: e.tensor_tensor(out=qn[:, :, 64:96], in0=q_sb[:, :, 64:96], in1=bc_last(rq8[:, 8:16], 32),
                                                       op=ALU.mult), reads=[bq, brq8], writes=[bqn])
                kb.op("pool", lambda e: e.tensor_tensor(out=qn[:], in0=qn[:], in1=bc_mid(gq[:, :], 8), op=ALU.mult),
                      reads=[bqn, bw], writes=[bqn])
                kb.op("act", lambda e: e.copy(out=q_full[:, :, 0:64], in_=qn[:, :, 0:64]), reads=[bqn], writes=[bqf])
                cb = bc_mid(cos[:, t, :], 8)
                sb_ = bc_mid(sin[:, t, :], 8)
                x1 = qn[:, :, 64:80]
                x2 = qn[:, :, 80:96]
                kb.op("dve", lambda e: e.tensor_tensor(out=rt[0][:], in0=x1, in1=cb, op=ALU.mult), reads=[bqn, brope], writes=[brt])
                kb.op("dve", lambda e: e.tensor_tensor(out=rt[1][:], in0=x2, in1=sb_, op=ALU.mult), reads=[bqn, brope], writes=[brt])
                kb.op("pool", lambda e: e.tensor_tensor(out=rt[2][:], in0=x2, in1=cb, op=ALU.mult), reads=[bqn, brope], writes=[brt])
                kb.op("pool", lambda e: e.tensor_tensor(out=rt[3][:], in0=x1, in1=sb_, op=ALU.mult), reads=[bqn, brope], writes=[brt])
                kb.op("dve", lambda e: e.tensor_tensor(out=q_full[:, :, 64:80], in0=rt[0][:], in1=rt[1][:], op=ALU.subtract),
                      reads=[brt], writes=[bqf])
                kb.op("dve", lambda e: e.tensor_tensor(out=q_full[:, :, 80:96], in0=rt[2][:], in1=rt[3][:], op=ALU.add),
                      reads=[brt], writes=[bqf])
                for h in range(8):
                    kb.op("pe", lambda e, h=h: e.transpose(out=tp[0][0:96, h, :], in_=q_full[:, h, :], identity=self.ident_b[:]),
                          reads=[bqf, self.b_const], writes=[btp[0]])
                kb.op("act", lambda e: e.copy(out=qT[:], in_=tp[0][0:96, :, :]), reads=[btp[0]], writes=[bqT])
                for gi in range(2):
                    for k in range(2):
                        kb.op("pe", lambda e, k=k, gi=gi: e.matmul(mm[gi][:], lhsT=ckvT[:, k, :], rhs=w_ukv[:, k, gi * 512:(gi + 1) * 512],
                                                                 start=(k == 0), stop=(k == 1)), reads=[bckvT, bw], writes=[bmm[gi]])
                    kb.op("act", lambda e, gi=gi: e.copy(out=kv_sb[:].rearrange("p h d -> p (h d)")[:, gi * 512:(gi + 1) * 512],
                                                        in_=mm[gi][:]), reads=[bmm[gi]], writes=[bkv])
                kb.op("pool", lambda e, t=t: e.tensor_copy(out=V[:, t, :, 0:64], in_=kv_sb[:, :, 64:128]),
                      reads=[bkv, bVones], writes=[bV[t]])
                kb.op("pool", lambda e: e.tensor_tensor(out=sq[:, :, 0:64], in0=kv_sb[:, :, 0:64], in1=kv_sb[:, :, 0:64], op=ALU.mult),
                      reads=[bkv], writes=[bsq])
                kb.op("dve", lambda e: e.tensor_reduce(out=rq8[:, 0:8], in_=sq[:, :, 0:64], axis=AX.X, op=ALU.add),
                      reads=[bsq], writes=[brq8])
                kb.op("dve", lambda e: e.tensor_scalar(out=rq8[:, 0:8], in0=rq8[:, 0:8], scalar1=1.0 / 64, scalar2=EPS,
                                                       op0=ALU.mult, op1=ALU.add), reads=[brq8], writes=[brq8])
                kb.op("pool", lambda e: e.tensor_tensor(out=rq8[:, 0:8], in0=rq8[:, 0:8], in1=self.neghalf[:, 0:8], op=ALU.pow),
                      reads=[brq8, self.b_const], writes=[brq8])
                kb.op("dve", lambda e: e.tensor_tensor(out=sq[:, :, 0:64], in0=kv_sb[:, :, 0:64], in1=bc_last(rq8[:, 0:8], 64),
                                                       op=ALU.mult), reads=[bkv, brq8], writes=[bsq])
                kb.op("pool", lambda e: e.tensor_tensor(out=k_full[:, :, 0:64], in0=sq[:, :, 0:64], in1=bc_mid(gk[:, 0:64], 8),
                                                        op=ALU.mult), reads=[bsq, bw], writes=[bkf])
                c1_ = cos[:, t, :]
                s1_ = sin[:, t, :]
                kb.op("dve", lambda e: e.tensor_tensor(out=rt[0][:, 0, :], in0=kr[:, 0:16], in1=c1_, op=ALU.mult), reads=[bkr, brope], writes=[brt])
                kb.op("dve", lambda e: e.tensor_tensor(out=rt[1][:, 0, :], in0=kr[:, 16:32], in1=s1_, op=ALU.mult), reads=[bkr, brope], writes=[brt])
                kb.op("dve", lambda e: e.tensor_tensor(out=rt[2][:, 0, :], in0=kr[:, 16:32], in1=c1_, op=ALU.mult), reads=[bkr, brope], writes=[brt])
                kb.op("dve", lambda e: e.tensor_tensor(out=rt[3][:, 0, :], in0=kr[:, 0:16], in1=s1_, op=ALU.mult), reads=[bkr, brope], writes=[brt])
                kb.op("dve", lambda e: e.tensor_tensor(out=kr2[:, 0:16], in0=rt[0][:, 0, :], in1=rt[1][:, 0, :], op=ALU.subtract),
                      reads=[brt], writes=[bkr])
                kb.op("dve", lambda e: e.tensor_tensor(out=kr2[:, 16:32], in0=rt[2][:, 0, :], in1=rt[3][:, 0, :], op=ALU.add),
                      reads=[brt], writes=[bkr])
                kb.op("dve", lambda e: e.tensor_copy(out=k_full[:, :, 64:96], in_=bc_mid(kr2[:, :], 8)), reads=[bkr], writes=[bkf])
                for h in range(8):
                    kb.op("pe", lambda e, h=h: e.transpose(out=tp[1][0:96, h, :], in_=k_full[:, h, :], identity=self.ident_b[:]),
                          reads=[bkf, self.b_const], writes=[btp[1]])
                kb.op("act", lambda e, t=t: e.copy(out=kT[:, :, t * 128:(t + 1) * 128], in_=tp[1][0:96, :, :]),
                      reads=[btp[1]], writes=[bkT[t]])
            def stage_b(t, s=s):
                qT = qTs[t % 2]; bqT = bqTs[t % 2]
                units = []
                for h in range(8):
                    for a in range(0, t + 1, 4):
                        units.append((h, a, min(a + 4, t + 1)))

                def emit_S(ui, u):
                    h, a, b = u
                    bank = ui % 2
                    for kt in range(a, b):
                        kb.op("pe", lambda e, kt=kt, h=h, a=a, bank=bank: e.matmul(
                            s2[:, bank, (kt - a) * 128:(kt - a + 1) * 128], lhsT=kT[:, h, kt * 128:(kt + 1) * 128],
                            rhs=qT[:, h, :], start=True, stop=True), reads=[bkT[kt], bqT], writes=[bs2[bank]])

                base = uc["n"]
                emit_S(base, units[0])
                for i, u in enumerate(units):
                    ui = base + i
                    h, a, b = u
                    if i + 1 < len(units):
                        emit_S(ui + 1, units[i + 1])
                    bank = ui % 2
                    P = PT[ui % 3]; bP = bPT[ui % 3]
                    n = (b - a) * 128
                    kb.op("act", lambda e, P=P, bank=bank, n=n: e.activation(
                        out=P[:].rearrange("p a b -> p (a b)")[:, 0:n], in_=s2[:, bank, 0:n], func=AF.Exp, scale=scale),
                        reads=[bs2[bank]], writes=[bP])
                    if b == t + 1:
                        kb.op("dve", lambda e, P=P, j=t - a: e.tensor_tensor(out=P[:, j, :], in0=P[:, j, :], in1=self.mask_le[:],
                                                                           op=ALU.mult), reads=[bP, self.b_const], writes=[bP])
                    ob = oo[h // 4]
                    for kt in range(a, b):
                        kb.op("pe", lambda e, kt=kt, h=h, a=a, P=P, ob=ob: e.matmul(
                            ob[:, (h % 4) * 65:(h % 4) * 65 + 65], lhsT=P[:, kt - a, :], rhs=V[:, kt, h, :],
                            start=(kt == 0), stop=(kt == t)), reads=[bP, bV[kt], bVones], writes=[boo[h // 4]])
                uc["n"] += len(units)
                for hb in range(2):
                    ov = oo[hb][:, 0:260].rearrange("p (h d) -> p h d", d=65)
                    kb.op("dve", lambda e, hb=hb, ov=ov: e.reciprocal(out=rden[:, hb * 4:(hb + 1) * 4], in_=ov[:, :, 64]),
                          reads=[boo[hb]], writes=[brden])
                    kb.op("dve", lambda e, hb=hb, ov=ov: e.tensor_tensor(out=attn[:, hb * 4:(hb + 1) * 4, :], in0=ov[:, :, 0:64],
                                                                        in1=bc_last(rden[:, hb * 4:(hb + 1) * 4], 64), op=ALU.mult),
                          reads=[boo[hb], brden], writes=[battn])
                kb.dma("sp", S["ATT"][s * NT + t], attn[:].rearrange("p h d -> p (h d)"), reads=[battn],
                       writes=[self.db("ATT", s * NT + t)])
            kb.pipeline(stage_a, stage_b, self.ntl)
        kb.pop()

    def alloc_router(self, l):
        kb, I = self.kb, self.I
        r = {}
        r["bw"] = Buf()
        r["rw"] = kb.sb("rw", [128, 8, 32], F32)
        kb.dma("sp", r["rw"][:], I["router_w"][l].rearrange("(k p) n -> p k n", p=128), writes=[r["bw"]])
        r["rb"] = kb.sb("rb", [128, 32], F32)
        self.bcast_load(r["rb"][:], I["router_b"][l], r["bw"])
        r["rwh"] = kb.sb("rwh", [128, 8, 32], BF16)
        r["rwl"] = kb.sb("rwl", [128, 8, 32], BF16)
        kb.op("dve", lambda e: e.tensor_copy(out=r["rwh"][:], in_=r["rw"][:]), reads=[r["bw"]], writes=[r["bw"]])
        kb.op("dve", lambda e: e.tensor_tensor(out=r["rwl"][:], in0=r["rw"][:], in1=r["rwh"][:], op=ALU.subtract),
              reads=[r["bw"]], writes=[r["bw"]])
        r["h2f"] = kb.sb("h2f", [128, D], F32); r["bh2f"] = Buf()
        r["h2hi"] = kb.sb("h2hi", [128, D], BF16); r["bh2hi"] = Buf()
        r["h2lo"] = kb.sb("h2lo", [128, D], BF16); r["bh2lo"] = Buf()
        r["h2Tl"] = kb.sb("h2Tl", [128, 8, 128], BF16); r["bh2Tl"] = Buf()
        r["h2Tb"] = kb.sb("h2Tb", [128, 8, 128], BF16); r["bh2Tb"] = Buf()
        r["lg"] = kb.sb("lg", [128, 32], F32); r["blg"] = Buf()
        r["m8"] = kb.sb("m8", [128, 8], F32)
        r["msk"] = kb.sb("msk", [128, 32], F32)
        r["ex"] = kb.sb("ex", [128, 32], F32)
        r["den"] = kb.sb("den", [128, 2], F32)
        r["G"] = kb.sb("Gt", [128, 32], F32); r["bG"] = Buf()
        r["ss"] = kb.sb("ss2", [128, 1], F32); r["bss"] = Buf()
        r["cnt"] = kb.sb("cnt_b", [128, 32], F32); r["bcnt"] = Buf()
        kb.op("pool", lambda e: e.memset(r["cnt"][:], 0.0), writes=[r["bcnt"]])
        r["Mb"] = kb.sb("Mb", [128, 32], BF16)
        for n_ in ("posf", "valid", "slotm", "oh", "junk", "Gv"):
            r[n_] = kb.sb(n_, [128, 32], F32)
        r["slotf"] = kb.sb("slotf", [128, 4], F32)
        r["sloti"] = kb.sb("sloti", [128, 4], I32); r["bsloti"] = Buf()
        r["gk"] = kb.sb("gk", [128, 4], F32); r["bgk"] = Buf()
        r["brt"] = Buf()
        return r

    def norm2_router(self, r, x1, bx1, gmod2, shift2, bmod, tmp, btmp, tp, btp, mmp, bmmp, tile_idx):
        kb, S = self.kb, self.S
        ss, bss = r["ss"], r["bss"]
        kb.op("dve", lambda e: e.scalar_tensor_tensor(out=tmp[:], in0=x1[:], scalar=1.0, in1=x1[:], op0=ALU.mult, op1=ALU.mult, accum_out=ss[:, 0:1]),
              reads=[bx1], writes=[btmp, bss])
        self.rstd_of(ss[:, 0:1], D, 1, bss, "")
        kb.op("dve", lambda e: e.scalar_tensor_tensor(out=tmp[:], in0=x1[:], scalar=ss[:, 0:1], in1=gmod2[:],
                                                      op0=ALU.mult, op1=ALU.mult), reads=[bx1, bss, bmod], writes=[btmp])
        kb.op("pool", lambda e: e.tensor_tensor(out=r["h2f"][:], in0=tmp[:], in1=shift2[:], op=ALU.add),
              reads=[btmp, bmod], writes=[r["bh2f"]])
        kb.op("act", lambda e: e.copy(out=r["h2hi"][:], in_=r["h2f"][:]), reads=[r["bh2f"]], writes=[r["bh2hi"]])
        kb.op("dve", lambda e: e.tensor_tensor(out=r["h2lo"][:], in0=r["h2f"][:], in1=r["h2hi"][:], op=ALU.subtract),
              reads=[r["bh2f"], r["bh2hi"]], writes=[r["bh2lo"]])
        for k in range(8):
            kb.op("pe", lambda e, k=k: e.transpose(out=tp[0][:, k, :], in_=r["h2hi"][:, k * 128:(k + 1) * 128],
                                                   identity=self.ident_b[:]), reads=[r["bh2hi"], self.b_const], writes=[btp[0]])
        for k in range(8):
            kb.op("pe", lambda e, k=k: e.transpose(out=tp[1][:, k, :], in_=r["h2lo"][:, k * 128:(k + 1) * 128],
                                                   identity=self.ident_b[:]), reads=[r["bh2lo"], self.b_const], writes=[btp[1]])
        kb.op("act", lambda e: e.copy(out=r["h2Tb"][:], in_=tp[0][:]), reads=[btp[0]], writes=[r["bh2Tb"]])
        kb.op("dve", lambda e: e.tensor_copy(out=r["h2Tl"][:], in_=tp[1][:]), reads=[btp[1]], writes=[r["bh2Tl"]])
        if self.debug:
            kb.dma("sp", S["H2T"][tile_idx], r["h2Tb"][:], reads=[r["bh2Tb"]], writes=[self.db("H2T", tile_idx)])
        if _STOP <= 6:
            return
        passes = [("h2Tb", "rwh"), ("h2Tl", "rwh"), ("h2Tb", "rwl")]
        for pi, (a_, w_) in enumerate(passes):
            for k in range(8):
                kb.op("pe", lambda e, k=k, a_=a_, w_=w_, pi=pi: e.matmul(mmp[:, 0:32], lhsT=r[a_][:, k, :], rhs=r[w_][:, k, :],
                                                                       start=(pi == 0 and k == 0), stop=(pi == 2 and k == 7)),
                      reads=[r["bh2Tb"], r["bh2Tl"], r["bw"]], writes=[bmmp])
        lg, m8, msk, ex, den, G = r["lg"], r["m8"], r["msk"], r["ex"], r["den"], r["G"]
        bl = r["blg"]
        kb.op("dve", lambda e: e.tensor_tensor(out=lg[:], in0=mmp[:, 0:32], in1=r["rb"][:], op=ALU.add),
              reads=[bmmp, r["bw"]], writes=[bl])
        if _STOP <= 7:
            return
        kb.op("dve", lambda e: e.max(out=m8[:], in_=lg[:]), reads=[bl], writes=[bl])
        kb.op("dve", lambda e: e.tensor_scalar(out=msk[:], in0=lg[:], scalar1=m8[:, 3:4], scalar2=None, op0=ALU.is_ge),
              reads=[bl], writes=[bl])
        kb.op("dve", lambda e: e.tensor_scalar(out=den[:, 1:2], in0=m8[:, 0:1], scalar1=-1.0, scalar2=None, op0=ALU.mult),
              reads=[bl], writes=[bl])
        kb.op("act", lambda e: e.activation(out=ex[:], in_=lg[:], func=AF.Exp, bias=den[:, 1:2], scale=1.0),
              reads=[bl], writes=[bl])
        kb.op("dve", lambda e: e.scalar_tensor_tensor(out=ex[:], in0=ex[:], scalar=1.0, in1=msk[:], op0=ALU.mult, op1=ALU.mult, accum_out=den[:, 0:1]), reads=[bl], writes=[bl])
        kb.op("dve", lambda e: e.reciprocal(out=den[:, 0:1], in_=den[:, 0:1]), reads=[bl], writes=[bl])
        kb.op("dve", lambda e: e.tensor_scalar(out=G[:], in0=ex[:], scalar1=den[:, 0:1], scalar2=None, op0=ALU.mult),
              reads=[bl], writes=[r["bG"]])
        if self.debug:
            kb.dma("sp", S["GS"][tile_idx], G[:], reads=[r["bG"]], writes=[self.db("GS", tile_idx)])
        cap = self.cap
        brt = r["brt"]
        kb.op("dve", lambda e: e.tensor_copy(out=r["Mb"][:], in_=msk[:]), reads=[bl], writes=[brt])
        kb.op("pe", lambda e: e.matmul(mmp[:, 32:64], lhsT=self.U_b[:], rhs=r["Mb"][:], start=True, stop=True),
              reads=[brt, self.b_const], writes=[bmmp])
        kb.op("pe", lambda e: e.matmul(mmp[:, 64:96], lhsT=self.ones_b[:], rhs=r["Mb"][:], start=True, stop=True),
              reads=[brt, self.b_const], writes=[bmmp])
        kb.op("dve", lambda e: e.tensor_tensor(out=r["posf"][:], in0=mmp[:, 32:64], in1=r["cnt"][:], op=ALU.add),
              reads=[bmmp, r["bcnt"]], writes=[brt])
        kb.op("dve", lambda e: e.tensor_tensor(out=r["cnt"][:], in0=mmp[:, 64:96], in1=r["cnt"][:], op=ALU.add),
              reads=[bmmp, r["bcnt"]], writes=[r["bcnt"]])
        kb.op("dve", lambda e: e.tensor_scalar(out=r["valid"][:], in0=r["posf"][:], scalar1=float(cap), scalar2=None, op0=ALU.is_lt),
              reads=[brt], writes=[brt])
        kb.op("dve", lambda e: e.tensor_tensor(out=r["slotm"][:], in0=r["posf"][:], in1=self.iotaE[:], op=ALU.add),
              reads=[brt, self.b_const], writes=[brt])
        kb.op("dve", lambda e: e.tensor_scalar(out=r["junk"][:], in0=r["valid"][:], scalar1=-1.0e6, scalar2=1.0e6,
                                               op0=ALU.mult, op1=ALU.add), reads=[brt], writes=[brt])
        kb.op("dve", lambda e: e.tensor_tensor(out=r["slotm"][:], in0=r["slotm"][:], in1=r["junk"][:], op=ALU.add),
              reads=[brt], writes=[brt])
        kb.op("dve", lambda e: e.tensor_tensor(out=r["Gv"][:], in0=G[:], in1=r["valid"][:], op=ALU.mult),
              reads=[brt, r["bG"]], writes=[brt])
        for k in range(4):
            kb.op("dve", lambda e, k=k: e.tensor_scalar(out=r["oh"][:], in0=lg[:], scalar1=m8[:, k:k + 1], scalar2=None, op0=ALU.is_equal),
                  reads=[bl, brt], writes=[brt])
            kb.op("dve", lambda e, k=k: e.scalar_tensor_tensor(out=r["junk"][:], in0=r["oh"][:], scalar=1.0, in1=r["slotm"][:],
                                                              op0=ALU.mult, op1=ALU.mult, accum_out=r["slotf"][:, k:k + 1]),
                  reads=[brt], writes=[brt])
            kb.op("dve", lambda e, k=k: e.scalar_tensor_tensor(out=r["junk"][:], in0=r["oh"][:], scalar=1.0, in1=r["Gv"][:],
                                                              op0=ALU.mult, op1=ALU.mult, accum_out=r["gk"][:, k:k + 1]),
                  reads=[brt, r["bgk"]], writes=[brt, r["bgk"]])
        kb.op("dve", lambda e: e.tensor_copy(out=r["sloti"][:], in_=r["slotf"][:]), reads=[brt, r["bsloti"]], writes=[r["bsloti"]])
        kb.dma("sp", S["SLOT"][tile_idx], r["sloti"][:], reads=[r["bsloti"]], writes=[self.db("SLOT", tile_idx)])
        kb.dma("sp", S["GK"][tile_idx], r["gk"][:], reads=[r["bgk"]], writes=[self.db("GK", tile_idx)])
        for k in range(4):
            kb.idma(S["XG"], r["sloti"][:, k:k + 1], r["h2hi"][:], None, 32 * cap - 1, reads=[r["bsloti"], r["bh2hi"]])

    def phase1b(self):
        kb, I, S, nseq = self.kb, self.I, self.S, self.nseq
        kb.push()
        bw = Buf()
        w_in = kb.sb("w_in_b", [128, 8, 2048], BF16)
        w_out = kb.sb("w_out", [128, 8, D], BF16)
        self.load_w_bf16(w_in, I["hyb_w_in"][:, 672:2720], 8, bw)
        self.load_w_bf16(w_out, I["hyb_w_out"], 8, bw)
        retg = kb.sb("retg", [128, 512], F32)
        self.bcast_load(retg[:], I["ret_norm_g"], bw)
        decT = kb.sb("decT", [128, 8 * 128], F32)
        qdec = kb.sb("qdec", [128, 8], F32)
        kdec = kb.sb("kdec", [128, 8], F32)
        cdec = kb.sb("cdec", [128, 4], F32)
        kb.dma("sp", decT[:], I["k_decayT"], writes=[bw])
        kb.dma("sp", qdec[:], I["k_qdec"], writes=[bw])
        kb.dma("sp", kdec[:], I["k_kdec"], writes=[bw])
        kb.dma("sp", cdec[:], I["k_cdec"], writes=[bw])
        r = self.alloc_router(0)

        mods = {n: kb.sb(n, [128, D], F32) for n in ("gmod1", "shift1", "gate1", "gmod2", "shift2")}
        bmod = Buf()
        x_t = [kb.sb("x_t%d" % i, [128, D], F32) for i in range(2)]; bx = [Buf(), Buf()]
        tmp = kb.sb("tmp", [128, D], F32); btmp = Buf()
        h_bf = kb.sb("h_bf", [128, D], BF16); bh = Buf()
        hT = kb.sb("hT", [128, 8, 128], BF16); bhT = Buf()
        ss = kb.sb("ss", [128, 4], F32); bss = Buf()
        raw = [kb.sb("raw%d" % i, [128, 8, 64], F32) for i in range(2)]; braw = [Buf(), Buf()]
        rr = [kb.sb("rr%d" % i, [128, 8, 64], F32) for i in range(2)]; brr = [Buf(), Buf()]
        rt = [kb.sb("rt%d" % i, [128, 8, 32], F32) for i in range(4)]; brt = Buf()
        rq_bf = kb.sb("rq_bf", [128, 8, 64], BF16); brqb = Buf()
        rqd_bf = kb.sb("rqd_bf", [128, 8, 64], BF16); brqd = Buf()
        rk_bf = kb.sb("rk_bf", [128, 8, 64], BF16); brkb = Buf()
        rkd_bf_2 = [kb.sb("rkd_bf%d" % i, [128, 8, 64], BF16) for i in range(2)]; brkd_2 = [Buf(), Buf()]
        v_bf_2 = [kb.sb("v_bf%d" % i, [128, 8, 64], BF16) for i in range(2)]; bv_2 = [Buf(), Buf()]
        sg_2 = [kb.sb("sg%d" % i, [128, 512], F32) for i in range(2)]; bsg_2 = [Buf(), Buf()]
        rqT_2 = [kb.sb("rqT%d" % i, [128, 8, 128], BF16) for i in range(2)]; brqT_2 = [Buf(), Buf()]
        rkT_2 = [kb.sb("rkT%d" % i, [128, 4, 128], BF16) for i in range(2)]; brkT_2 = [Buf(), Buf()]
        Sd = kb.sb("Sd", [128, 8, 128], BF16); bSd = Buf()
        st_f = kb.sb("st_f", [128, 4, 128], F32); bstf = Buf()
        st_b = kb.sb("st_b", [128, 4, 128], BF16); bstb = Buf()
        kb.op("pool", lambda e: e.memset(st_f[:], 0.0), writes=[bstf])
        o_sb = kb.sb("o_sb", [128, 8, 64], F32); bo = Buf()
        oc = kb.sb("oc", [128, 8, 64], F32); boc = Buf()
        st8 = kb.sb("st8", [128, 16], F32); bst8 = Buf()
        mixcat_2 = [kb.sb("mixcat%d" % i, [128, D], BF16) for i in range(2)]; bmixa_2 = [Buf(), Buf()]; bmixy_2 = [Buf(), Buf()]
        tmpB = kb.sb("tmpB", [128, D], F32); btmpB = Buf()
        mixT = kb.sb("mixT", [128, 8, 128], BF16); bmixT = Buf()
        x1 = kb.sb("x1", [128, D], F32); bx1 = Buf()

        tp = [kb.ps("tp%d" % i, [128, 8, 128], BF16) for i in range(2)]; btp = [Buf(), Buf()]
        mm = [kb.ps("mm%d" % i, [128, 512], F32) for i in range(2)]; bmm = [Buf(), Buf()]
        s2 = kb.ps("s2", [128, 2, 512], F32); bs2 = [Buf(), Buf()]
        oo = [kb.ps("oo%d" % i, [128, 512], F32) for i in range(2)]; boo = [Buf(), Buf()]
        tpB = [s2[:, i, :].bitcast(BF16).rearrange("p (k m) -> p k m", m=128) for i in range(2)]
        for s in range(nseq):
            cos, sin, brope = self.rope_tables(s, 32, "k_invf32", "b%d" % s)
            for n_, part in (("gmod1", 1), ("shift1", 0), ("gate1", 2), ("gmod2", 4), ("shift2", 3)):
                kb.dma("sp", mods[n_][:], S["MOD"][s, 0, part], reads=[self.db("MOD", (s, 0, part))], writes=[bmod])
            def stage_a(t, s=s, cos=cos, sin=sin, brope=brope):
                P_ = t % 2
                X = x_t[P_]; bX = bx[P_]
                v_bf = v_bf_2[P_]; bv = bv_2[P_]; sg = sg_2[P_]; bsg = bsg_2[P_]; rqT = rqT_2[P_]; brqT = brqT_2[P_]
                rkT = rkT_2[P_]; brkT = brkT_2[P_]; rkd_bf = rkd_bf_2[P_]; brkd = brkd_2[P_]
                mixcat = mixcat_2[P_]; bmixa = bmixa_2[P_]; bmixy = bmixy_2[P_]
                ti = s * NT + t
                kb.dma("sp", X[:], I["x"][ti * 128:(ti + 1) * 128, :], writes=[bX])
                kb.dma("sp", mixcat[:, 0:512], S["ATT"][ti], reads=[self.db("ATT", ti)], writes=[bmixa])
                self.norm_mod_T(X, bX, mods["gmod1"], mods["shift1"], bmod, tmp, btmp, h_bf, bh, tp[0], btp[0], hT, bhT, ss, bss)
                cb = bc_mid(cos[:, t, :], 8)
                sb_ = bc_mid(sin[:, t, :], 8)
                for gi in range(4):
                    p_ = mm[gi % 2]; bp_ = bmm[gi % 2]
                    for k in range(8):
                        kb.op("pe", lambda e, k=k, gi=gi, p_=p_: e.matmul(p_[:], lhsT=hT[:, k, :], rhs=w_in[:, k, gi * 512:(gi + 1) * 512],
                                                                       start=(k == 0), stop=(k == 7)), reads=[bhT, bw], writes=[bp_])
                    if gi < 2:
                        rw_ = raw[gi]; brw_ = braw[gi]; ro = rr[gi]; bro = brr[gi]
                        kb.op("act", lambda e, p_=p_, rw_=rw_: e.copy(out=rw_[:].rearrange("p h d -> p (h d)"), in_=p_[:]),
                              reads=[bp_], writes=[brw_])
                        x1_ = rw_[:, :, 0:32]; x2_ = rw_[:, :, 32:64]
                        kb.op("dve", lambda e, x1_=x1_: e.tensor_tensor(out=rt[0][:], in0=x1_, in1=cb, op=ALU.mult), reads=[brw_, brope], writes=[brt])
                        kb.op("dve", lambda e, x2_=x2_: e.tensor_tensor(out=rt[1][:], in0=x2_, in1=sb_, op=ALU.mult), reads=[brw_, brope], writes=[brt])
                        kb.op("pool", lambda e, x2_=x2_: e.tensor_tensor(out=rt[2][:], in0=x2_, in1=cb, op=ALU.mult), reads=[brw_, brope], writes=[brt])
                        kb.op("pool", lambda e, x1_=x1_: e.tensor_tensor(out=rt[3][:], in0=x1_, in1=sb_, op=ALU.mult), reads=[brw_, brope], writes=[brt])
                        kb.op("dve", lambda e, ro=ro: e.tensor_tensor(out=ro[:, :, 0:32], in0=rt[0][:], in1=rt[1][:], op=ALU.subtract),
                              reads=[brt], writes=[bro])
                        kb.op("pool", lambda e, ro=ro: e.tensor_tensor(out=ro[:, :, 32:64], in0=rt[2][:], in1=rt[3][:], op=ALU.add),
                              reads=[brt], writes=[bro])
                        if gi == 0:
                            kb.op("act", lambda e, ro=ro: e.copy(out=rq_bf[:], in_=ro[:]), reads=[bro], writes=[brqb])
                            kb.op("pool", lambda e, ro=ro: e.tensor_tensor(out=rqd_bf[:], in0=ro[:], in1=bc_last(qdec[:, :], 64), op=ALU.mult),
                                  reads=[bro, bw], writes=[brqd])
                        else:
                            kb.op("act", lambda e, ro=ro: e.mul(out=rk_bf[:], in_=ro[:], mul=0.125), reads=[bro], writes=[brkb])
                            kb.op("dve", lambda e, ro=ro: e.scalar_tensor_tensor(out=rkd_bf[:], in0=ro[:], scalar=0.125,
                                                                                in1=bc_last(kdec[:, :], 64), op0=ALU.mult, op1=ALU.mult),
                                  reads=[bro, bw], writes=[brkd])
                    elif gi == 2:
                        kb.op("act", lambda e, p_=p_: e.copy(out=v_bf[:].rearrange("p h d -> p (h d)"), in_=p_[:]), reads=[bp_], writes=[bv])
                    else:
                        kb.op("act", lambda e, p_=p_: e.activation(out=sg[:], in_=p_[:], func=AF.Silu), reads=[bp_], writes=[bsg])
                for i in range(4):
                    kb.op("pe", lambda e, i=i: e.transpose(out=tp[1][:, i, :], in_=rq_bf[:, 2 * i:2 * i + 2, :].rearrange("p h d -> p (h d)"),
                                                           identity=self.ident_b[:]), reads=[brqb, self.b_const], writes=[btp[1]])
                for i in range(4):
                    kb.op("pe", lambda e, i=i: e.transpose(out=tp[1][:, 4 + i, :], in_=rqd_bf[:, 2 * i:2 * i + 2, :].rearrange("p h d -> p (h d)"),
                                                           identity=self.ident_b[:]), reads=[brqd, self.b_const], writes=[btp[1]])
                for i in range(4):
                    kb.op("pe", lambda e, i=i: e.transpose(out=tp[0][:, i, :], in_=rk_bf[:, 2 * i:2 * i + 2, :].rearrange("p h d -> p (h d)"),
                                                           identity=self.ident_b[:]), reads=[brkb, self.b_const], writes=[btp[0]])
                kb.op("act", lambda e: e.copy(out=rqT[:], in_=tp[1][:]), reads=[btp[1]], writes=[brqT])
                kb.op("dve", lambda e: e.tensor_copy(out=rkT[:], in_=tp[0][:, 0:4, :]), reads=[btp[0]], writes=[brkT])
            def stage_b(t, s=s):
                P_ = t % 2
                X = x_t[P_]; bX = bx[P_]
                v_bf = v_bf_2[P_]; bv = bv_2[P_]; sg = sg_2[P_]; bsg = bsg_2[P_]; rqT = rqT_2[P_]; brqT = brqT_2[P_]
                rkT = rkT_2[P_]; brkT = brkT_2[P_]; rkd_bf = rkd_bf_2[P_]; brkd = brkd_2[P_]
                mixcat = mixcat_2[P_]; bmixa = bmixa_2[P_]; bmixy = bmixy_2[P_]
                ti = s * NT + t
                tmp = tmpB; btmp = btmpB
                for h in range(8):
                    i, o = h // 2, (h % 2) * 64
                    kb.op("pe", lambda e, h=h, i=i, o=o: e.matmul(s2[:, h % 2, i * 128:(i + 1) * 128],
                                                                lhsT=rkT[o:o + 64, i, :], rhs=rqT[o:o + 64, i, :], start=True, stop=True),
                          reads=[brkT, brqT], writes=[bs2[h % 2]])
                for hb in range(2):
                    kb.op("dve", lambda e, hb=hb: e.tensor_tensor(out=Sd[:, hb * 4:(hb + 1) * 4, :].rearrange("p h q -> p (h q)"),
                                                                 in0=s2[:, hb, :], in1=decT[:, hb * 512:(hb + 1) * 512], op=ALU.mult),
                          reads=[bs2[hb], bw], writes=[bSd])
                if _STOP <= 1.5:
                    return
                for i in range(4):
                    if t > 0:
                        kb.op("pe", lambda e, i=i: e.matmul(oo[0][:, i * 128:(i + 1) * 128], lhsT=rqT[:, 4 + i, :],
                                                            rhs=st_b[:, i, :], start=True, stop=False, skip_group_check=True),
                              reads=[brqT, bstb], writes=[boo[0]])
                    for par in range(2):
                        h = 2 * i + par
                        kb.op("pe", lambda e, h=h, i=i, par=par: e.matmul(oo[0][:, h * 64:(h + 1) * 64], lhsT=Sd[:, par * 4 + i, :],
                                                                        rhs=v_bf[:, h, :], start=(t == 0), stop=(t == 0 or par == 1),
                                                                        skip_group_check=(t > 0)),
                              reads=[bSd, bv], writes=[boo[0]])
                if _STOP <= 2:
                    return
                for i in range(4):
                    kb.op("pe", lambda e, i=i: e.matmul(oo[1][:, i * 128:(i + 1) * 128],
                                                        lhsT=rkd_bf[:, 2 * i:2 * i + 2, :].rearrange("p h d -> p (h d)"),
                                                        rhs=v_bf[:, 2 * i:2 * i + 2, :].rearrange("p h d -> p (h d)"), start=True, stop=True),
                          reads=[brkd, bv], writes=[boo[1]])
                kvv = oo[1][:].rearrange("p (i c) -> p i c", c=128)
                for half in range(2):
                    po = half * 64
                    if t == 0:
                        kb.op("dve", lambda e, po=po: e.tensor_copy(out=st_f[po:po + 64, :, po:po + 64], in_=kvv[po:po + 64, :, po:po + 64]),
                              reads=[boo[1]], writes=[bstf])
                    else:
                        for i in range(4):
                            kb.op("dve", lambda e, po=po, i=i: e.scalar_tensor_tensor(
                                out=st_f[po:po + 64, i, po:po + 64], in0=st_f[po:po + 64, i, po:po + 64], scalar=cdec[po:po + 64, i:i + 1],
                                in1=kvv[po:po + 64, i, po:po + 64], op0=ALU.mult, op1=ALU.add), reads=[boo[1], bstf, bw], writes=[bstf])
                kb.op("act", lambda e: e.copy(out=st_b[:], in_=st_f[:]), reads=[bstf], writes=[bstb])
                if _STOP <= 3:
                    return
                kb.op("act", lambda e: e.copy(out=o_sb[:].rearrange("p h d -> p (h d)"), in_=oo[0][:]), reads=[boo[0]], writes=[bo])
                kb.op("dve", lambda e: e.tensor_reduce(out=st8[:, 0:8], in_=o_sb[:], axis=AX.X, op=ALU.add), reads=[bo], writes=[bst8])
                kb.op("dve", lambda e: e.tensor_scalar(out=st8[:, 0:8], in0=st8[:, 0:8], scalar1=-1.0 / 64, scalar2=None, op0=ALU.mult),
                      reads=[bst8], writes=[bst8])
                kb.op("pool", lambda e: e.tensor_tensor(out=oc[:], in0=o_sb[:], in1=bc_last(st8[:, 0:8], 64), op=ALU.add),
                      reads=[bo, bst8], writes=[boc])
                kb.op("pool", lambda e: e.tensor_tensor(out=o_sb[:], in0=oc[:], in1=oc[:], op=ALU.mult), reads=[boc], writes=[bo])
                kb.op("dve", lambda e: e.tensor_reduce(out=st8[:, 8:16], in_=o_sb[:], axis=AX.X, op=ALU.add), reads=[bo], writes=[bst8])
                kb.op("dve", lambda e: e.tensor_scalar(out=st8[:, 8:16], in0=st8[:, 8:16], scalar1=1.0 / 64, scalar2=EPS,
                                                       op0=ALU.mult, op1=ALU.add), reads=[bst8], writes=[bst8])
                kb.op("pool", lambda e: e.tensor_tensor(out=st8[:, 8:16], in0=st8[:, 8:16], in1=self.neghalf[:, 0:8], op=ALU.pow),
                      reads=[bst8, self.b_const], writes=[bst8])
                kb.op("dve", lambda e: e.tensor_tensor(out=oc[:], in0=oc[:], in1=bc_last(st8[:, 8:16], 64), op=ALU.mult),
                      reads=[boc, bst8], writes=[boc])
                kb.op("pool", lambda e: e.tensor_tensor(out=oc[:].rearrange("p h d -> p (h d)"), in0=oc[:].rearrange("p h d -> p (h d)"),
                                                        in1=retg[:], op=ALU.mult), reads=[boc, bw], writes=[boc])
                kb.op("dve", lambda e: e.tensor_tensor(out=mixcat[:, 512:1024], in0=oc[:].rearrange("p h d -> p (h d)"), in1=sg[:],
                                                       op=ALU.mult), reads=[boc, bsg], writes=[bmixy])
                if _STOP <= 4:
                    return
                for k in range(8):
                    kb.op("pe", lambda e, k=k: e.transpose(out=tpB[0][:, k, :], in_=mixcat[:, k * 128:(k + 1) * 128], identity=self.ident_b[:]),
                          reads=[bmixa, bmixy, self.b_const], writes=[bs2[0]])
                kb.op("act", lambda e: e.copy(out=mixT[:], in_=tpB[0]), reads=[bs2[0]], writes=[bmixT])
                for half in range(2):
                    for k in range(8):
                        kb.op("pe", lambda e, k=k, half=half: e.matmul(oo[half][:], lhsT=mixT[:, k, :], rhs=w_out[:, k, half * 512:(half + 1) * 512],
                                                                     start=(k == 0), stop=(k == 7)), reads=[bmixT, bw], writes=[boo[half]])
                    hs = slice(half * 512, (half + 1) * 512)
                    kb.op("dve", lambda e, half=half, hs=hs: e.tensor_tensor(out=tmp[:, hs], in0=oo[half][:], in1=mods["gate1"][:, hs], op=ALU.mult),
                          reads=[boo[half], bmod], writes=[btmp])
                    kb.op("pool", lambda e, hs=hs: e.tensor_tensor(out=x1[:, hs], in0=tmp[:, hs], in1=X[:, hs], op=ALU.add),
                          reads=[btmp, bX], writes=[bx1])
                kb.dma("sp", S["XA"][ti * 128:(ti + 1) * 128, :], x1[:], reads=[bx1], writes=[self.db("XA", ti)])
                if _STOP <= 5:
                    return
                self.norm2_router(r, x1, bx1, mods["gmod2"], mods["shift2"], bmod, tmp, btmp, tpB, bs2, oo[0], boo[0], ti)
            kb.pipeline(stage_a, stage_b, self.ntl)
        kb.pop()

    def zero_xg(self):
        kb, S = self.kb, self.S
        z = kb.sb("zeros", [128, 4096], BF16); bz = Buf()
        kb.op("pool", lambda e: e.memset(z[:], 0.0), writes=[bz])
        nrows = 32 * self.cap
        for r0 in range(0, nrows, 512):
            kb.dma("sp", S["XG"][r0:r0 + 512, :].rearrange("(p a) d -> p (a d)", p=128), z[:], reads=[bz])

    def phase2e(self, l):
        kb, I, S = self.kb, self.I, self.S
        kb.push()
        cap = self.cap
        nblk = cap // 512
        wgu = [kb.sb("wgu%d" % i, [128, 8, 2048], BF16) for i in range(2)]
        wdn = [kb.sb("wdn%d" % i, [128, 8, D], BF16) for i in range(2)]
        bgu = [kb.sb("bgu%d" % i, [128, 16], F32) for i in range(2)]
        bdn = [kb.sb("bdn%d" % i, [1, D], BF16) for i in range(2)]
        bwt = [Buf(), Buf()]
        ones1 = kb.sb("ones1", [1, 128], BF16); bones = Buf()
        kb.op("pool", lambda e: e.memset(ones1[:], 1.0), writes=[bones])
        xg = [kb.sb("xg%d" % i, [128, D], BF16) for i in range(12)]; bxg = [Buf() for _ in range(12)]
        xgT = [kb.sb("xgT%d" % i, [128, 8, 512], BF16) for i in range(2)]; bxgT = [Buf(), Buf()]
        glu = [kb.sb("glu%d" % i, [128, 512], F32) for i in range(2)]; bglu = [Buf(), Buf()]
        sig = [kb.sb("sig%d" % i, [128, 512], F32) for i in range(2)]; bsig = [Buf(), Buf()]
        lin = [kb.sb("lin%d" % i, [128, 512], F32) for i in range(2)]; blin = [Buf(), Buf()]
        actT = [kb.sb("actT%d" % i, [128, 8, 512], BF16) for i in range(2)]; bact = [Buf(), Buf()]
        yg = [kb.sb("yg%d" % i, [128, D], F32) for i in range(3)]; byg = [Buf() for _ in range(3)]
        tp = [kb.ps("tp%d" % i, [128, 8, 128], BF16) for i in range(2)]; btp = [Buf(), Buf()]
        pA = [kb.ps("pA%d" % i, [128, 512], F32) for i in range(2)]; bpA = [Buf(), Buf()]
        pB = [kb.ps("pB%d" % i, [128, 512], F32) for i in range(2)]; bpB = [Buf(), Buf()]
        pC = [kb.ps("pC%d" % i, [128, 512], F32) for i in range(2)]; bpC = [Buf(), Buf()]

        def load_expert(e, slot):
            self.load_w_bf16(wgu[slot], I["exp_w_gu"][l, e], 8, bwt[slot])
            self.load_w_bf16(wdn[slot], I["exp_w_down"][l, e], 8, bwt[slot])
            kb.dma("sp", bgu[slot][:], I["exp_b_gu_pj"][l, e], writes=[bwt[slot]])
            kb.op("dve", lambda en, slot=slot: en.tensor_scalar(out=bgu[slot][:, 8:16], in0=bgu[slot][:, 8:16], scalar1=1.0, scalar2=None,
                                                               op0=ALU.add), reads=[bwt[slot]], writes=[bwt[slot]])
            kb.dma("pool", bdn[slot][:], I["exp_b_down"][l, e:e + 1, :], writes=[bwt[slot]])

        load_expert(0, 0)
        blocks = [(ex, blk) for ex in range(32) for blk in range(nblk)]
        state = dict(xu=0, tu=0)

        def emit_loads(bi):
            ex, blk = blocks[bi]
            r0 = ex * cap + blk * 512
            tiles = []
            for st in range(4):
                i = state["xu"] % len(xg); state["xu"] += 1
                kb.dma("sp", xg[i][:], S["XG"][r0 + st * 128:r0 + (st + 1) * 128, :], writes=[bxg[i]])
                tiles.append(i)
            return tiles

        def emit_transposes(bi, tiles):
            XT = xgT[bi % 2]; bXT = bxgT[bi % 2]
            for st, i in enumerate(tiles):
                T = tp[state["tu"] % 2]; bT = btp[state["tu"] % 2]; state["tu"] += 1
                for k in range(8):
                    kb.op("pe", lambda e, k=k, i=i, T=T: e.transpose(out=T[:, k, :], in_=xg[i][:, k * 128:(k + 1) * 128],
                                                                   identity=self.ident_b[:]), reads=[bxg[i], self.b_const], writes=[bT])
                kb.op("act", lambda e, T=T, XT=XT, st=st: e.copy(out=XT[:, :, st * 128:(st + 1) * 128], in_=T[:]),
                      reads=[bT], writes=[bXT])

        pu = cu = yu = 0
        tl0 = emit_loads(0)
        tl1 = emit_loads(1) if len(blocks) > 1 else None
        emit_transposes(0, tl0)
        for bi, (ex, blk) in enumerate(blocks):
            slot = ex % 2
            if blk == 0 and ex + 1 < 32:
                load_expert(ex + 1, (ex + 1) % 2)
            XT = xgT[bi % 2]; bXT = bxgT[bi % 2]
            A = actT[bi % 2]; bA = bact[bi % 2]
            r0 = ex * cap + blk * 512
            for j in range(8):
                pa = pA[pu % 2]; bpa = bpA[pu % 2]; pb = pB[pu % 2]; bpb = bpB[pu % 2]
                gl = glu[pu % 2]; bgl = bglu[pu % 2]; sg_ = sig[pu % 2]; bsg_ = bsig[pu % 2]; ln = lin[pu % 2]; bln = blin[pu % 2]
                pu += 1
                for k in range(8):
                    kb.op("pe", lambda e, k=k, j=j, pa=pa, slot=slot, XT=XT: e.matmul(
                        pa[:], lhsT=wgu[slot][:, k, j * 128:(j + 1) * 128], rhs=XT[:, k, :],
                        start=(k == 0), stop=(k == 7)), reads=[bwt[slot], bXT], writes=[bpa])
                for k in range(8):
                    kb.op("pe", lambda e, k=k, j=j, pb=pb, slot=slot, XT=XT: e.matmul(
                        pb[:], lhsT=wgu[slot][:, k, 1024 + j * 128:1024 + (j + 1) * 128], rhs=XT[:, k, :],
                        start=(k == 0), stop=(k == 7)), reads=[bwt[slot], bXT], writes=[bpb])
                kb.op("dve", lambda e, pa=pa, gl=gl, j=j, slot=slot: e.tensor_scalar(
                    out=gl[:], in0=pa[:], scalar1=bgu[slot][:, j:j + 1], scalar2=7.0, op0=ALU.add, op1=ALU.min),
                    reads=[bpa, bwt[slot]], writes=[bgl])
                kb.op("act", lambda e, gl=gl, sg_=sg_: e.activation(out=sg_[:], in_=gl[:], func=AF.Sigmoid, scale=1.702),
                      reads=[bgl], writes=[bsg_])
                kb.op("dve", lambda e, pb=pb, ln=ln, j=j, slot=slot: e.tensor_scalar(
                    out=ln[:], in0=pb[:], scalar1=bgu[slot][:, 8 + j:9 + j], scalar2=8.0, op0=ALU.add, op1=ALU.min),
                    reads=[bpb, bwt[slot]], writes=[bln])
                kb.op("dve", lambda e, gl=gl, sg_=sg_: e.tensor_tensor(out=gl[:], in0=gl[:], in1=sg_[:], op=ALU.mult),
                      reads=[bgl, bsg_], writes=[bgl])
                kb.op("dve", lambda e, gl=gl, ln=ln, A=A, j=j: e.scalar_tensor_tensor(out=A[:, j, :], in0=ln[:], scalar=-6.0, in1=gl[:],
                                                                                 op0=ALU.max, op1=ALU.mult),
                      reads=[bgl, bln], writes=[bA])
            if bi + 1 < len(blocks):
                emit_transposes(bi + 1, tl1)
                tl0, tl1 = tl1, (emit_loads(bi + 2) if bi + 2 < len(blocks) else None)
            for st in range(4):
                Y = yg[yu % 3]; bY = byg[yu % 3]; yu += 1
                for half in range(2):
                    pc = pC[cu % 2]; bpc = bpC[cu % 2]; cu += 1
                    for k in range(8):
                        kb.op("pe", lambda e, k=k, st=st, half=half, pc=pc, A=A, slot=slot: e.matmul(
                            pc[:], lhsT=A[:, k, st * 128:(st + 1) * 128], rhs=wdn[slot][:, k, half * 512:(half + 1) * 512],
                            start=(k == 0), stop=False), reads=[bA, bwt[slot]], writes=[bpc])
                    kb.op("pe", lambda e, half=half, pc=pc, slot=slot: e.matmul(
                        pc[:], lhsT=ones1[:, :], rhs=bdn[slot][:, half * 512:(half + 1) * 512], start=False, stop=True),
                        reads=[bones, bwt[slot]], writes=[bpc])
                    if half == 0:
                        kb.op("act", lambda e, pc=pc, Y=Y: e.copy(out=Y[:, 0:512], in_=pc[:]), reads=[bpc], writes=[bY])
                    else:
                        kb.op("dve", lambda e, pc=pc, Y=Y: e.tensor_copy(out=Y[:, 512:1024], in_=pc[:]), reads=[bpc], writes=[bY])
                kb.dma("pool", S["YG"][r0 + st * 128:r0 + (st + 1) * 128, :], Y[:], reads=[bY])
        kb.pop()

    def phase2c(self, l, src, dst, dst_name, zero_after):
        kb, I, S, nseq = self.kb, self.I, self.S, self.nseq
        kb.push()
        cap = self.cap
        gate2 = kb.sb("gate2", [128, D], F32); bg2 = Buf()
        xin = [kb.sb("xin%d" % i, [128, D], F32) for i in range(2)]; bxin = [Buf(), Buf()]
        acc = [kb.sb("acc%d" % i, [128, D], F32) for i in range(2)]; bacc = [Buf(), Buf()]
        yb = [kb.sb("yb%d" % i, [128, D], F32) for i in range(8)]; byb = [Buf() for _ in range(8)]
        sl = [kb.sb("sl%d" % i, [128, 4], I32) for i in range(2)]; bsl = [Buf(), Buf()]
        gk = [kb.sb("gkc%d" % i, [128, 4], F32) for i in range(2)]; bgk = [Buf(), Buf()]
        for i in range(8):
            kb.op("pool", lambda e, i=i: e.memset(yb[i][:], 0.0), writes=[byb[i]])
        if zero_after:
            self.zero_xg()
        u = 0
        for s in range(nseq):
            kb.dma("sp", gate2[:], S["MOD"][s, l, 5], reads=[self.db("MOD", (s, l, 5))], writes=[bg2])
            for t in range(self.ntl):
                ti = s * NT + t
                X = xin[u % 2]; bX = bxin[u % 2]; A = acc[u % 2]; bA = bacc[u % 2]
                SL = sl[u % 2]; bSL = bsl[u % 2]; GK = gk[u % 2]; bGK = bgk[u % 2]
                kb.dma("sp", X[:], src[ti * 128:(ti + 1) * 128, :], reads=[self.db("XA", ti)], writes=[bX])
                kb.dma("sp", SL[:], S["SLOT"][ti], reads=[self.db("SLOT", ti)], writes=[bSL])
                kb.dma("sp", GK[:], S["GK"][ti], reads=[self.db("GK", ti)], writes=[bGK])
                for k in range(4):
                    Yk = yb[(u % 2) * 4 + k]; bYk = byb[(u % 2) * 4 + k]
                    kb.idma(Yk[:], None, S["YG"], SL[:, k:k + 1], 32 * cap - 1, reads=[bSL], writes=[bYk])
                    if k == 0:
                        kb.op("dve", lambda e, Yk=Yk, A=A, GK=GK: e.tensor_scalar(out=A[:], in0=Yk[:], scalar1=GK[:, 0:1], scalar2=None,
                                                                                 op0=ALU.mult), reads=[bYk, bGK], writes=[bA])
                    else:
                        kb.op("dve", lambda e, Yk=Yk, A=A, GK=GK, k=k: e.scalar_tensor_tensor(
                            out=A[:], in0=Yk[:], scalar=GK[:, k:k + 1], in1=A[:], op0=ALU.mult, op1=ALU.add),
                            reads=[bYk, bGK, bA], writes=[bA])
                kb.op("pool", lambda e, A=A: e.tensor_tensor(out=A[:], in0=A[:], in1=gate2[:], op=ALU.mult), reads=[bA, bg2], writes=[bA])
                kb.op("pool", lambda e, A=A, X=X: e.tensor_tensor(out=X[:], in0=A[:], in1=X[:], op=ALU.add), reads=[bA, bX], writes=[bX])
                kb.dma("sp", dst[ti * 128:(ti + 1) * 128, :], X[:], reads=[bX], writes=[self.db(dst_name, ti)])
                u += 1
        kb.pop()

    def phase3(self):
        kb, I, S, nseq = self.kb, self.I, self.S, self.nseq
        kb.push()
        bw = Buf()
        w_qkv = kb.sb("w_qkv", [128, 8, 1280], BF16)
        w_out = kb.sb("w_out", [128, 8, D], BF16)
        self.load_w_bf16(w_qkv, I["swa_w_qkv"], 8, bw)
        self.load_w_bf16(w_out, I["swa_w_out"], 8, bw)
        bqkv = kb.sb("bqkv", [128, 1280], F32)
        bout = kb.sb("bout", [128, D], F32)
        gq = kb.sb("gq", [128, 64], F32)
        gk = kb.sb("gk", [128, 64], F32)
        sk = kb.sb("sk", [128, 16], F32)
        self.bcast_load(bqkv[:], I["swa_b_qkv"], bw)
        self.bcast_load(bout[:], I["swa_b_out"], bw)
        self.bcast_load(gq[:], I["swa_q_head_g"], bw)
        self.bcast_load(gk[:], I["swa_k_head_g"], bw)
        self.bcast_load(sk[:], I["swa_sinks"], bw)
        kb.op("act", lambda e: e.activation(out=sk[:], in_=sk[:], func=AF.Exp), reads=[bw], writes=[bw])
        mask2 = kb.sb("mask2", [128, 4, 2, 128], BF16)
        for hh in range(4):
            kb.op("pool", lambda e, hh=hh: e.tensor_copy(out=mask2[:, hh, 0, :], in_=self.mask_gt[:]), reads=[self.b_const], writes=[bw])
            kb.op("pool", lambda e, hh=hh: e.tensor_copy(out=mask2[:, hh, 1, :], in_=self.mask_le[:]), reads=[self.b_const], writes=[bw])
        r = self.alloc_router(1)

        mods = {n: kb.sb(n, [128, D], F32) for n in ("gmod1", "shift1", "gate1", "gmod2", "shift2")}
        bmod = Buf()
        x_t = [kb.sb("x_t%d" % i, [128, D], F32) for i in range(2)]; bx = [Buf(), Buf()]
        tmp = kb.sb("tmp", [128, D], F32); btmp = Buf()
        h_bf = kb.sb("h_bf", [128, D], BF16); bh = Buf()
        hT = kb.sb("hT", [128, 8, 128], BF16); bhT = Buf()
        ss = kb.sb("ss", [128, 4], F32); bss = Buf()
        qkv = kb.sb("qkv", [128, 20, 64], F32); bqkvs = Buf()
        sq = kb.sb("sq", [128, 18, 64], F32); bsq = Buf()
        r18 = kb.sb("r18", [128, 18], F32); br18 = Buf()
        qn = kb.sb("qn", [128, 18, 64], F32); bqn = Buf()
        rt = [kb.sb("rt%d" % i, [128, 18, 32], F32) for i in range(4)]; brt = Buf()
        q_bf = kb.sb("q_bf", [128, 16, 64], BF16); bqb = Buf()
        kdup = kb.sb("kdup", [128, 2, 2, 64], BF16); bkd = Buf()
        qT_2 = [kb.sb("qT%d" % i, [128, 8, 128], BF16) for i in range(2)]; bqT_2 = [Buf(), Buf()]
        tmpB = kb.sb("tmpB", [128, D], F32); btmpB = Buf()
        kT = [kb.sb("kT%d" % i, [128, 2, 128], BF16) for i in range(3)]; bkT = [Buf() for _ in range(3)]
        Va = [kb.sb("Va%d" % i, [128, 2, 65], BF16) for i in range(3)]; bVa = [Buf() for _ in range(3)]
        bVones = Buf()
        for i in range(3):
            kb.op("pool", lambda e, i=i: e.memset(Va[i][:, :, 64:65], 1.0), writes=[bVones])
        PT = [kb.sb("PT%d" % i, [128, 4, 2, 128], BF16) for i in range(2)]; bPT = [Buf(), Buf()]
        den = kb.sb("den", [128, 16], F32); bden = Buf()
        attn = kb.sb("attn", [128, 16, 64], BF16); battn = Buf()
        attT = kb.sb("attT", [128, 8, 128], BF16); battT = Buf()
        x1 = kb.sb("x1", [128, D], F32); bx1 = Buf()

        tp = [kb.ps("tp%d" % i, [128, 8, 128], BF16) for i in range(2)]; btp = [Buf(), Buf()]
        mm = [kb.ps("mm%d" % i, [128, 512], F32) for i in range(2)]; bmm = [Buf(), Buf()]
        s2 = kb.ps("s2", [128, 2, 512], F32); bs2 = [Buf(), Buf()]
        oo = [kb.ps("oo%d" % i, [128, 512], F32) for i in range(2)]; boo = [Buf(), Buf()]
        tpB = [s2[:, i, :].bitcast(BF16).rearrange("p (k m) -> p k m", m=128) for i in range(2)]
        gc = {"n": 0}
        for s in range(nseq):
            cos, sin, brope = self.rope_tables(s, 32, "k_invf32", "c%d" % s)
            for n_, part in (("gmod1", 1), ("shift1", 0), ("gate1", 2), ("gmod2", 4), ("shift2", 3)):
                kb.dma("sp", mods[n_][:], S["MOD"][s, 1, part], reads=[self.db("MOD", (s, 1, part))], writes=[bmod])
            def stage_a(t, s=s, cos=cos, sin=sin, brope=brope):
                ti = s * NT + t
                cur, prv = t % 3, (t - 1) % 3
                X = x_t[t % 2]; bX = bx[t % 2]
                qT = qT_2[t % 2]; bqT = bqT_2[t % 2]
                kb.dma("sp", X[:], S["XB"][ti * 128:(ti + 1) * 128, :], reads=[self.db("XB", ti)], writes=[bX])
                self.norm_mod_T(X, bX, mods["gmod1"], mods["shift1"], bmod, tmp, btmp, h_bf, bh, tp[0], btp[0], hT, bhT, ss, bss)
                qkvf = qkv[:].rearrange("p h d -> p (h d)")
                for gi, (c0, c1) in enumerate(((0, 512), (512, 1024), (1024, 1280))):
                    p_ = mm[gi % 2]; bp_ = bmm[gi % 2]
                    for k in range(8):
                        kb.op("pe", lambda e, k=k, c0=c0, c1=c1, p_=p_: e.matmul(p_[:, 0:c1 - c0], lhsT=hT[:, k, :], rhs=w_qkv[:, k, c0:c1],
                                                                              start=(k == 0), stop=(k == 7)), reads=[bhT, bw], writes=[bp_])
                    kb.op("dve", lambda e, c0=c0, c1=c1, p_=p_: e.tensor_tensor(out=qkvf[:, c0:c1], in0=p_[:, 0:c1 - c0], in1=bqkv[:, c0:c1],
                                                                              op=ALU.add), reads=[bp_, bw], writes=[bqkvs])
                kb.op("pool", lambda e: e.tensor_tensor(out=sq[:], in0=qkv[:, 0:18, :], in1=qkv[:, 0:18, :], op=ALU.mult), reads=[bqkvs], writes=[bsq])
                kb.op("dve", lambda e: e.tensor_reduce(out=r18[:], in_=sq[:], axis=AX.X, op=ALU.add), reads=[bsq], writes=[br18])
                kb.op("dve", lambda e: e.tensor_scalar(out=r18[:], in0=r18[:], scalar1=1.0 / 64, scalar2=EPS, op0=ALU.mult, op1=ALU.add),
                      reads=[br18], writes=[br18])
                kb.op("pool", lambda e: e.tensor_tensor(out=r18[:, 0:16], in0=r18[:, 0:16], in1=self.neghalf[:, 0:16], op=ALU.pow),
                      reads=[br18, self.b_const], writes=[br18])
                kb.op("pool", lambda e: e.tensor_tensor(out=r18[:, 16:18], in0=r18[:, 16:18], in1=self.neghalf[:, 0:2], op=ALU.pow),
                      reads=[br18, self.b_const], writes=[br18])
                kb.op("dve", lambda e: e.tensor_tensor(out=qn[:], in0=qkv[:, 0:18, :], in1=bc_last(r18[:, :], 64), op=ALU.mult),
                      reads=[bqkvs, br18], writes=[bqn])
                kb.op("pool", lambda e: e.tensor_tensor(out=qn[:, 0:16, :], in0=qn[:, 0:16, :], in1=bc_mid(gq[:, :], 16), op=ALU.mult),
                      reads=[bqn, bw], writes=[bqn])
                kb.op("pool", lambda e: e.tensor_tensor(out=qn[:, 16:18, :], in0=qn[:, 16:18, :], in1=bc_mid(gk[:, :], 2), op=ALU.mult),
                      reads=[bqn, bw], writes=[bqn])
                cb = bc_mid(cos[:, t, :], 18)
                sb_ = bc_mid(sin[:, t, :], 18)
                x1_ = qn[:, :, 0:32]; x2_ = qn[:, :, 32:64]
                kb.op("dve", lambda e: e.tensor_tensor(out=rt[0][:], in0=x1_, in1=cb, op=ALU.mult), reads=[bqn, brope], writes=[brt])
                kb.op("dve", lambda e: e.tensor_tensor(out=rt[1][:], in0=x2_, in1=sb_, op=ALU.mult), reads=[bqn, brope], writes=[brt])
                kb.op("pool", lambda e: e.tensor_tensor(out=rt[2][:], in0=x2_, in1=cb, op=ALU.mult), reads=[bqn, brope], writes=[brt])
                kb.op("pool", lambda e: e.tensor_tensor(out=rt[3][:], in0=x1_, in1=sb_, op=ALU.mult), reads=[bqn, brope], writes=[brt])
                kb.op("dve", lambda e: e.tensor_tensor(out=q_bf[:, :, 0:32], in0=rt[0][:, 0:16, :], in1=rt[1][:, 0:16, :], op=ALU.subtract),
                      reads=[brt], writes=[bqb])
                kb.op("pool", lambda e: e.tensor_tensor(out=q_bf[:, :, 32:64], in0=rt[2][:, 0:16, :], in1=rt[3][:, 0:16, :], op=ALU.add),
                      reads=[brt], writes=[bqb])
                for dup in range(2):
                    kb.op("dve", lambda e, dup=dup: e.tensor_tensor(out=kdup[:, :, dup, 0:32], in0=rt[0][:, 16:18, :], in1=rt[1][:, 16:18, :],
                                                                   op=ALU.subtract), reads=[brt], writes=[bkd])
                    kb.op("pool", lambda e, dup=dup: e.tensor_tensor(out=kdup[:, :, dup, 32:64], in0=rt[2][:, 16:18, :], in1=rt[3][:, 16:18, :],
                                                                    op=ALU.add), reads=[brt], writes=[bkd])
                kb.op("act", lambda e, cur=cur: e.copy(out=Va[cur][:, :, 0:64], in_=qkv[:, 18:20, :]), reads=[bqkvs, bVones], writes=[bVa[cur]])
                for i in range(8):
                    kb.op("pe", lambda e, i=i: e.transpose(out=tp[1][:, i, :], in_=q_bf[:, 2 * i:2 * i + 2, :].rearrange("p h d -> p (h d)"),
                                                           identity=self.ident_b[:]), reads=[bqb, self.b_const], writes=[btp[1]])
                kb.op("act", lambda e: e.copy(out=qT[:], in_=tp[1][:]), reads=[btp[1]], writes=[bqT])
                for g in range(2):
                    kb.op("pe", lambda e, g=g: e.transpose(out=tp[0][:, g, :], in_=kdup[:, g, :, :].rearrange("p a d -> p (a d)"),
                                                           identity=self.ident_b[:]), reads=[bkd, self.b_const], writes=[btp[0]])
                kb.op("dve", lambda e, cur=cur: e.tensor_copy(out=kT[cur][:], in_=tp[0][:, 0:2, :]), reads=[btp[0]], writes=[bkT[cur]])
            def stage_b(t, s=s):
                ti = s * NT + t
                cur, prv = t % 3, (t - 1) % 3
                X = x_t[t % 2]; bX = bx[t % 2]
                qT = qT_2[t % 2]; bqT = bqT_2[t % 2]
                tmp = tmpB; btmp = btmpB
                for gq4 in range(4):
                    sbank = s2
                    bsb = bs2
                    P = PT[gc["n"] % 2]; bP = bPT[gc["n"] % 2]
                    ob = oo[gc["n"] % 2]; bob = boo[gc["n"] % 2]
                    gc["n"] += 1
                    for hh in range(4):
                        hq = gq4 * 4 + hh
                        i, o = hq // 2, (hq % 2) * 64
                        g = hq // 8
                        for w_, kt in ((0, prv), (1, cur)):
                            if t == 0 and w_ == 0:
                                continue
                            col = ((hh % 2) * 2 + hh // 2) * 256 + w_ * 128
                            kb.op("pe", lambda e, i=i, o=o, g=g, kt=kt, col=col, sbank=sbank: e.matmul(
                                sbank[:, col // 512, col % 512:col % 512 + 128], lhsT=kT[kt][o:o + 64, g, :], rhs=qT[o:o + 64, i, :],
                                start=True, stop=True), reads=[bkT[kt], bqT], writes=[bsb[col // 512]])
                    for bk in range(2):
                        if t == 0:
                            for hh2 in range(2):
                                kb.op("act", lambda e, bk=bk, hh2=hh2, P=P, sbank=sbank: e.activation(
                                    out=P[:, bk * 2 + hh2, 1, :], in_=sbank[:, bk, hh2 * 256 + 128:hh2 * 256 + 256], func=AF.Exp, scale=0.125),
                                    reads=[bsb[bk]], writes=[bP])
                        else:
                            kb.op("act", lambda e, bk=bk, P=P, sbank=sbank: e.activation(
                                out=P[:, bk * 2:bk * 2 + 2, :, :].rearrange("p a b c -> p (a b c)"), in_=sbank[:, bk, :], func=AF.Exp, scale=0.125),
                                reads=[bsb[bk]], writes=[bP])
                    if t == 0:
                        kb.op("dve", lambda e, P=P: e.tensor_tensor(out=P[:, :, 1, :], in0=P[:, :, 1, :], in1=mask2[:, :, 1, :], op=ALU.mult),
                              reads=[bP, bw], writes=[bP])
                    else:
                        kb.op("dve", lambda e, P=P: e.tensor_tensor(out=P[:].rearrange("p a b c -> p (a b c)"), in0=P[:].rearrange("p a b c -> p (a b c)"),
                                                                   in1=mask2[:].rearrange("p a b c -> p (a b c)"), op=ALU.mult),
                              reads=[bP, bw], writes=[bP])
                    for hh in range(4):
                        hq = gq4 * 4 + hh
                        g = hq // 8
                        sl = (hh % 2) * 2 + hh // 2
                        if t > 0:
                            kb.op("pe", lambda e, hh=hh, g=g, P=P, ob=ob, prv=prv, sl=sl: e.matmul(ob[:, hh * 65:hh * 65 + 65], lhsT=P[:, sl, 0, :],
                                                                                          rhs=Va[prv][:, g, :], start=True, stop=False),
                                  reads=[bP, bVa[prv]], writes=[bob])
                        kb.op("pe", lambda e, hh=hh, g=g, P=P, ob=ob, cur=cur, sl=sl: e.matmul(ob[:, hh * 65:hh * 65 + 65], lhsT=P[:, sl, 1, :],
                                                                                      rhs=Va[cur][:, g, :], start=(t == 0), stop=True),
                              reads=[bP, bVa[cur]], writes=[bob])
                    ov = ob[:, 0:260].rearrange("p (h d) -> p h d", d=65)
                    dsl = den[:, gq4 * 4:(gq4 + 1) * 4]
                    kb.op("dve", lambda e, ov=ov, dsl=dsl, gq4=gq4: e.tensor_tensor(out=dsl, in0=ov[:, :, 64], in1=sk[:, gq4 * 4:(gq4 + 1) * 4], op=ALU.add),
                          reads=[bob, bw], writes=[bden])
                    kb.op("dve", lambda e, dsl=dsl: e.reciprocal(out=dsl, in_=dsl), reads=[bden], writes=[bden])
                    kb.op("dve", lambda e, ov=ov, dsl=dsl, gq4=gq4: e.tensor_tensor(out=attn[:, gq4 * 4:(gq4 + 1) * 4, :], in0=ov[:, :, 0:64],
                                                                                 in1=bc_last(dsl, 64), op=ALU.mult), reads=[bob, bden], writes=[battn])
                af = attn[:].rearrange("p h d -> p (h d)")
                for k in range(8):
                    kb.op("pe", lambda e, k=k: e.transpose(out=tpB[0][:, k, :], in_=af[:, k * 128:(k + 1) * 128], identity=self.ident_b[:]),
                          reads=[battn, self.b_const], writes=[bs2[0]])
                kb.op("act", lambda e: e.copy(out=attT[:], in_=tpB[0]), reads=[bs2[0]], writes=[battT])
                for half in range(2):
                    hs = slice(half * 512, (half + 1) * 512)
                    for k in range(8):
                        kb.op("pe", lambda e, k=k, half=half, hs=hs: e.matmul(oo[half][:], lhsT=attT[:, k, :], rhs=w_out[:, k, hs],
                                                                            start=(k == 0), stop=(k == 7)), reads=[battT, bw], writes=[boo[half]])
                    kb.op("dve", lambda e, half=half, hs=hs: e.tensor_tensor(out=tmp[:, hs], in0=oo[half][:], in1=bout[:, hs], op=ALU.add),
                          reads=[boo[half], bw], writes=[btmp])
                    kb.op("pool", lambda e, hs=hs: e.tensor_tensor(out=tmp[:, hs], in0=tmp[:, hs], in1=mods["gate1"][:, hs], op=ALU.mult),
                          reads=[btmp, bmod], writes=[btmp])
                    kb.op("pool", lambda e, hs=hs: e.tensor_tensor(out=x1[:, hs], in0=tmp[:, hs], in1=X[:, hs], op=ALU.add),
                          reads=[btmp, bX], writes=[bx1])
                kb.dma("sp", S["XA"][ti * 128:(ti + 1) * 128, :], x1[:], reads=[bx1], writes=[self.db("XA", ti)])
                self.norm2_router(r, x1, bx1, mods["gmod2"], mods["shift2"], bmod, tmp, btmp, tpB, bs2, oo[0], boo[0], ti)
            kb.pipeline(stage_a, stage_b, self.ntl)
        kb.pop()

    def build(self):
        self.setup_consts()
        ph = self.phases
        if "p0" in ph:
            self.phase0()
        if "p1a" in ph:
            self.phase1a()
        if "p1b" in ph:
            self.phase1b()
        if "p2a" in ph:
            self.phase2e(0)
            self.phase2c(0, self.S["XA"], self.S["XB"], "XB", True)
        if "p3" in ph:
            self.phase3()
        if "p2b" in ph:
            self.phase2e(1)
            self.phase2c(1, self.S["XA"], self.out, "OUT", False)
        self.kb.finish()
        return self.nc


def module_consts():
    idx = np.arange(128, dtype=np.float64)
    lg = np.log1p(-np.exp2(-5.0 - np.arange(8, dtype=np.float64)))
    k = {}
    k["k_invf16"] = (10000.0 ** (-np.arange(16, dtype=np.float32) / 16)).astype(np.float32)
    k["k_invf32"] = (10000.0 ** (-np.arange(32, dtype=np.float32) / 32)).astype(np.float32)
    diff = idx[None, :] - idx[:, None]
    dec = np.where(diff[:, None, :] >= 0, np.exp(lg[None, :, None] * np.maximum(diff[:, None, :], 0.0)), 0.0)
    k["k_decayT"] = np.ascontiguousarray(dec.reshape(128, 4, 2, 128).transpose(0, 2, 1, 3)).reshape(128, 8 * 128).astype(np.float32)
    k["k_qdec"] = np.exp(lg[None, :] * (idx + 1.0)[:, None]).astype(np.float32)
    k["k_kdec"] = np.exp(lg[None, :] * (127.0 - idx)[:, None]).astype(np.float32)
    cd = np.zeros((128, 4), np.float64)
    for i in range(4):
        cd[0:64, i] = np.exp(lg[2 * i] * 128)
        cd[64:128, i] = np.exp(lg[2 * i + 1] * 128)
    k["k_cdec"] = cd.astype(np.float32)
    return k


def make_in_maps(inputs, nseq, n_cores):
    f = lambda a: np.ascontiguousarray(np.asarray(a))
    shared = {}
    for name in ("ada_w", "ada_b", "norm1_g", "norm2_g", "router_w", "router_b", "exp_w_gu", "exp_w_down", "exp_b_down"):
        shared[name] = f(inputs[name])
    for name in ("hyb_w_in", "mla_cq_norm_g", "mla_ckv_norm_g", "mla_w_uq", "mla_w_ukv", "mla_q_head_g", "mla_k_head_g",
                 "hyb_w_out", "swa_w_qkv", "swa_b_qkv", "swa_q_head_g", "swa_k_head_g", "swa_sinks", "swa_w_out", "swa_b_out"):
        shared[name] = f(np.asarray(inputs[name])[0])
    shared["ret_norm_g"] = f(np.asarray(inputs["ret_norm_g"])[0].reshape(512))
    bgu = np.asarray(inputs["exp_b_gu"])
    shared["exp_b_gu_pj"] = f(bgu.reshape(2, 32, 16, 128).transpose(0, 1, 3, 2))
    shared.update(module_consts())
    x = np.asarray(inputs["x"]); c = np.asarray(inputs["c"]); pos = np.asarray(inputs["positions"])
    maps = []
    for i in range(n_cores):
        b0 = i * nseq
        m = dict(shared)
        m["x"] = f(x[b0:b0 + nseq].reshape(nseq * SEQ, D))
        m["c_pk"] = f(c[b0:b0 + nseq].reshape(nseq, 8, 128).transpose(0, 2, 1))
        m["pos_pt"] = f(pos[b0:b0 + nseq].reshape(nseq, NT, 128).transpose(0, 2, 1).astype(np.int32))
        maps.append(m)
    return maps


_PROG = {}


def kernel(**inputs):
    nseq = 32 // N_CORES
    if "nc" not in _PROG:
        _PROG["nc"] = Prog(nseq).build()
    maps = make_in_maps(inputs, nseq, N_CORES)
    res = run_bass_kernel_spmd(_PROG["nc"], maps, core_ids=list(range(N_CORES)))
    out = np.concatenate([np.asarray(r["out"]).reshape(nseq, SEQ, D) for r in res.results], axis=0)
    return out.astype(np.float32)
```

```python
import contextlib
import os
import math
import numpy as np
import concourse.bass as bass
import concourse.mybir as mybir
from concourse.bass_utils import run_bass_kernel_spmd

F32 = mybir.dt.float32
BF16 = mybir.dt.bfloat16
I32 = mybir.dt.int32
AF = mybir.ActivationFunctionType
ALU = mybir.AluOpType
AX = mybir.AxisListType

SAME_ENGINE_SYNC = os.environ.get('KSES', '1') == '1'
DMA_RING = 6
N_CORES = 8
_STOP = float(os.environ.get('KSTOP', '99'))
SEQ = 2048
D = 1024
NT = SEQ // 128
EPS = 1e-6
PI = math.pi


class Buf:
    __slots__ = ("w", "rs")

    def __init__(self):
        self.w = None
        self.rs = {}


class KB:
    ENGS = ("pe", "act", "dve", "pool", "sp")

    def __init__(self, nc):
        self.nc = nc
        self.stacks = [contextlib.ExitStack()]
        self.eng = dict(pe=nc.tensor, act=nc.scalar, dve=nc.vector, pool=nc.gpsimd, sp=nc.sync)
        self.cnt = {e: 0 for e in self.ENGS}
        self.seen = {e: {} for e in self.ENGS}
        self.sems = {}
        for e in self.ENGS:
            self.sems[e] = self.stacks[0].enter_context(nc.semaphore("s_" + e))
        self.dma_n = {}
        for e in ("sp", "pool", "act"):
            self.dma_n[e] = 0
            for j in range(DMA_RING):
                self.sems[("d", e, j)] = self.stacks[0].enter_context(nc.semaphore("d_%s_%d" % (e, j)))
        self.uid = 0

    def push(self):
        self.phase_id = getattr(self, "phase_id", 0) + 1
        self.stacks.append(contextlib.ExitStack())

    def pop(self):
        self.barrier()
        self.stacks.pop().close()

    def sb(self, name, shape, dt):
        self.uid += 1
        return self.stacks[-1].enter_context(self.nc.sbuf_tensor("%s_%d" % (name, self.uid), list(shape), dt))

    def ps(self, name, shape, dt):
        self.uid += 1
        return self.stacks[-1].enter_context(self.nc.psum_tensor("%s_%d" % (name, self.uid), list(shape), dt))

    def _deps(self, e, reads, writes):
        toks = {}
        for b in reads:
            if b.w is not None and toks.get(b.w[0], 0) < b.w[1]:
                toks[b.w[0]] = b.w[1]
        for b in writes:
            if b.w is not None and toks.get(b.w[0], 0) < b.w[1]:
                toks[b.w[0]] = b.w[1]
            for k, v in b.rs.items():
                if toks.get(k, 0) < v:
                    toks[k] = v
        waits = []
        seen = self.seen[e]
        for k, v in toks.items():
            if k == e and (e == "pe" or not SAME_ENGINE_SYNC):
                continue
            if seen.get(k, 0) < v:
                seen[k] = v
                waits.append((k, v))
        return waits

    def _mark(self, tok, reads, writes):
        k, v = tok
        for b in reads:
            if b.rs.get(k, 0) < v:
                b.rs[k] = v
        for b in writes:
            b.w = tok
            b.rs = {}

    def _emit(self, e, waits, fn, key, inc):
        engine = self.eng[e]
        for k, v in waits:
            engine.wait_ge(self.sems[k], v)
        if fn is not None:
            fn(engine).then_inc(self.sems[key], inc)

    def op(self, e, fn, reads=(), writes=()):
        waits = self._deps(e, reads, writes)
        self.cnt[e] += 1
        tok = (e, self.cnt[e])
        self._emit(e, waits, fn, e, 1)
        self._mark(tok, reads, writes)
        self._handoff()
        return tok

    def dma(self, e, out, in_, reads=(), writes=(), **kw):
        waits = self._deps(e, reads, writes)
        n = self.dma_n[e]
        self.dma_n[e] += 1
        key = ("d", e, n % DMA_RING)
        val = 16 * (n // DMA_RING + 1)
        if n >= DMA_RING and self.seen[e].get(key, 0) < val - 16:
            self.seen[e][key] = val - 16
            waits.append((key, val - 16))
        tok = (key, val)
        self._emit(e, waits, (lambda eng: eng.dma_start(out=out, in_=in_, **kw)), key, 16)
        self._mark(tok, reads, writes)
        self._handoff()
        return tok

    def idma(self, out, out_idx, in_, in_idx, bound, reads=(), writes=()):
        e = "pool"
        waits = self._deps(e, reads, writes)
        n = self.dma_n[e]
        self.dma_n[e] += 1
        key = ("d", e, n % DMA_RING)
        val = 16 * (n // DMA_RING + 1)
        if n >= DMA_RING and self.seen[e].get(key, 0) < val - 16:
            self.seen[e][key] = val - 16
            waits.append((key, val - 16))
        if not hasattr(self, "_bregs"):
            self._bregs = {}
        if bound not in self._bregs:
            self._bregs[bound] = self.nc.gpsimd.to_reg(bound)
        bound = self._bregs[bound]
        oo_ = bass.IndirectOffsetOnAxis(ap=out_idx, axis=0) if out_idx is not None else None
        io_ = bass.IndirectOffsetOnAxis(ap=in_idx, axis=0) if in_idx is not None else None
        self._emit(e, waits, (lambda eng: eng.indirect_dma_start(out=out, out_offset=oo_, in_=in_, in_offset=io_,
                                                                 bounds_check=bound, oob_is_err=False)), key, 16)
        self._mark((key, val), reads, writes)
        self._handoff()

    def _handoff(self):
        st = getattr(self, "_il", None)
        if st is None:
            return
        me = getattr(st["tls"], "idx", None)
        if me is None:
            return
        cv = st["cv"]
        with cv:
            if st["alive"][1 - me]:
                st["turn"] = 1 - me
                cv.notify_all()
                while st["turn"] != me:
                    cv.wait()

    def interleave(self, fa, fb):
        import threading
        if fa is None or fb is None:
            (fa or fb)()
            return
        st = dict(cv=threading.Condition(), turn=0, alive=[True, True], tls=threading.local(), err=[])
        self._il = st

        def runner(i, f):
            cv = st["cv"]
            with cv:
                while st["turn"] != i:
                    cv.wait()
            st["tls"].idx = i
            try:
                f()
            except BaseException as ex:
                st["err"].append(ex)
            finally:
                with cv:
                    st["alive"][i] = False
                    st["turn"] = 1 - i
                    cv.notify_all()

        ths = [threading.Thread(target=runner, args=(i, f)) for i, f in enumerate((fa, fb))]
        for th in ths:
            th.start()
        for th in ths:
            th.join()
        self._il = None
        if st["err"]:
            raise st["err"][0]

    def pipeline(self, stage_a, stage_b, n):
        stage_a(0)
        for t in range(n):
            self.interleave((lambda t=t: stage_a(t + 1)) if t + 1 < n else None, lambda t=t: stage_b(t))

    def all_tokens(self):
        toks = [(e, self.cnt[e]) for e in self.ENGS if self.cnt[e] > 0]
        for e in ("sp", "pool", "act"):
            n = self.dma_n[e]
            for j in range(DMA_RING):
                c = (n - j + DMA_RING - 1) // DMA_RING if n > j else 0
                if c > 0:
                    toks.append((("d", e, j), 16 * c))
        return toks

    def barrier(self):
        toks = self.all_tokens()
        for e in self.ENGS:
            waits = []
            for k, v in toks:
                if k == e:
                    continue
                if self.seen[e].get(k, 0) < v:
                    self.seen[e][k] = v
                    waits.append((k, v))
            self._emit(e, waits, None, None, 0)

    def finish(self):
        self.barrier()
        while self.stacks:
            self.stacks.pop().close()


def bc_mid(ap2, n):
    return ap2.unsqueeze(1).broadcast_to([ap2.shape[0], n, ap2.shape[1]])


def bc_last(ap2, n):
    return ap2.unsqueeze(2).broadcast_to([ap2.shape[0], ap2.shape[1], n])


class Prog:
    def __init__(self, nseq, debug=False, phases=("p0", "p1a", "p1b", "p2a", "p3", "p2b"), ntl=NT, cap_tiles=None):
        self.nseq = nseq
        self.ntl = ntl
        if cap_tiles is None:
            mean = nseq * ntl * 128 * 4 // 32
            cap_tiles = max(4, 4 * ((2 * mean + 511) // 512))
        self.cap = cap_tiles * 128
        self.debug = debug
        self.phases = phases
        nc = self.nc = bass.Bass("TRN2", target_bir_lowering=False)
        self.kb = KB(nc)
        ntok = nseq * SEQ
        self.ntok = ntok

        def inp(name, shape, dt=F32):
            return nc.dram_tensor(name, list(shape), dt, kind="ExternalInput").ap()

        def scr(name, shape, dt=F32):
            kind = "ExternalOutput" if debug else "Internal"
            return nc.dram_tensor(name, list(shape), dt, kind=kind).ap()

        I = self.I = {}
        I["x"] = inp("x", [ntok, D])
        I["c_pk"] = inp("c_pk", [nseq, 128, 8])
        I["pos_pt"] = inp("pos_pt", [nseq, 128, NT], I32)
        I["ada_w"] = inp("ada_w", [2, D, 6 * D])
        I["ada_b"] = inp("ada_b", [2, 6 * D])
        I["norm1_g"] = inp("norm1_g", [2, D])
        I["norm2_g"] = inp("norm2_g", [2, D])
        I["hyb_w_in"] = inp("hyb_w_in", [D, 2720])
        I["mla_cq_norm_g"] = inp("mla_cq_norm_g", [384])
        I["mla_ckv_norm_g"] = inp("mla_ckv_norm_g", [256])
        I["mla_w_uq"] = inp("mla_w_uq", [384, 768])
        I["mla_w_ukv"] = inp("mla_w_ukv", [256, 1024])
        I["mla_q_head_g"] = inp("mla_q_head_g", [96])
        I["mla_k_head_g"] = inp("mla_k_head_g", [96])
        I["ret_norm_g"] = inp("ret_norm_g", [512])
        I["hyb_w_out"] = inp("hyb_w_out", [D, D])
        I["swa_w_qkv"] = inp("swa_w_qkv", [D, 1280])
        I["swa_b_qkv"] = inp("swa_b_qkv", [1280])
        I["swa_q_head_g"] = inp("swa_q_head_g", [64])
        I["swa_k_head_g"] = inp("swa_k_head_g", [64])
        I["swa_sinks"] = inp("swa_sinks", [16])
        I["swa_w_out"] = inp("swa_w_out", [D, D])
        I["swa_b_out"] = inp("swa_b_out", [D])
        I["router_w"] = inp("router_w", [2, D, 32])
        I["router_b"] = inp("router_b", [2, 32])
        I["exp_w_gu"] = inp("exp_w_gu", [2, 32, D, 2048])
        I["exp_b_gu_pj"] = inp("exp_b_gu_pj", [2, 32, 128, 16])
        I["exp_w_down"] = inp("exp_w_down", [2, 32, D, D])
        I["exp_b_down"] = inp("exp_b_down", [2, 32, D])
        I["k_invf16"] = inp("k_invf16", [16])
        I["k_invf32"] = inp("k_invf32", [32])
        I["k_decayT"] = inp("k_decayT", [128, 8 * 128])
        I["k_qdec"] = inp("k_qdec", [128, 8])
        I["k_kdec"] = inp("k_kdec", [128, 8])
        I["k_cdec"] = inp("k_cdec", [128, 4])

        S = self.S = {}
        S["MOD"] = scr("MOD", [nseq, 2, 6, 128, D])
        S["ATT"] = scr("ATT", [nseq * NT, 128, 512], BF16)
        S["XA"] = scr("XA", [ntok, D])
        S["XB"] = scr("XB", [ntok, D])
        S["H2T"] = scr("H2T", [nseq * NT, 128, 8, 128], BF16)
        S["GS"] = scr("GS", [nseq * NT, 128, 32])
        S["XG"] = scr("XG", [32 * self.cap, D], BF16)
        S["YG"] = scr("YG", [32 * self.cap, D])
        S["SLOT"] = scr("SLOT", [nseq * NT, 128, 4], I32)
        S["GK"] = scr("GK", [nseq * NT, 128, 4])
        self.out = nc.dram_tensor("out", [ntok, D], F32, kind="ExternalOutput").ap()
        self.dbufs = {}

    def db(self, name, idx):
        k = (name, idx)
        if k not in self.dbufs:
            self.dbufs[k] = Buf()
        return self.dbufs[k]

    def setup_consts(self):
        kb = self.kb
        self.ident_b = kb.sb("ident_b", [128, 128], BF16)
        self.ident_f = kb.sb("ident_f", [128, 128], F32)
        self.b_const = Buf()
        bc = self.b_const
        for idt in (self.ident_b, self.ident_f):
            kb.op("pool", lambda e, idt=idt: e.memset(idt[:], 1.0), writes=[bc])
            kb.op("pool", lambda e, idt=idt: e.affine_select(out=idt[:], in_=idt[:], pattern=[[-1, 128]],
                                                             compare_op=ALU.is_equal, fill=0.0, base=0,
                                                             channel_multiplier=1), reads=[bc], writes=[bc])
        self.mask_le = kb.sb("mask_le", [128, 128], BF16)
        self.mask_gt = kb.sb("mask_gt", [128, 128], BF16)
        kb.op("pool", lambda e: e.memset(self.mask_le[:], 1.0), writes=[bc])
        kb.op("pool", lambda e: e.affine_select(out=self.mask_le[:], in_=self.mask_le[:], pattern=[[1, 128]],
                                                compare_op=ALU.is_ge, fill=0.0, base=0, channel_multiplier=-1),
              reads=[bc], writes=[bc])
        kb.op("pool", lambda e: e.memset(self.mask_gt[:], 1.0), writes=[bc])
        kb.op("pool", lambda e: e.affine_select(out=self.mask_gt[:], in_=self.mask_gt[:], pattern=[[-1, 128]],
                                                compare_op=ALU.is_gt, fill=0.0, base=0, channel_multiplier=1),
              reads=[bc], writes=[bc])
        self.U_b = kb.sb("U_b", [128, 128], BF16)
        self.ones_b = kb.sb("ones_b", [128, 128], BF16)
        kb.op("pool", lambda e: e.memset(self.ones_b[:], 1.0), writes=[bc])
        kb.op("pool", lambda e: e.memset(self.U_b[:], 1.0), writes=[bc])
        kb.op("pool", lambda e: e.affine_select(out=self.U_b[:], in_=self.U_b[:], pattern=[[1, 128]],
                                                compare_op=ALU.is_ge, fill=0.0, base=-1, channel_multiplier=-1),
              reads=[bc], writes=[bc])
        iot_i = kb.sb("iot_i", [128, 32], I32)
        self.iotaE = kb.sb("iotaE", [128, 32], F32)
        kb.op("pool", lambda e: e.iota(out=iot_i[:], pattern=[[1, 32]], base=0, channel_multiplier=0), writes=[bc])
        kb.op("dve", lambda e: e.tensor_copy(out=self.iotaE[:], in_=iot_i[:]), reads=[bc], writes=[bc])
        kb.op("dve", lambda e: e.tensor_scalar(out=self.iotaE[:], in0=self.iotaE[:], scalar1=float(self.cap), scalar2=None,
                                               op0=ALU.mult), reads=[bc], writes=[bc])
        self.neghalf = kb.sb("neghalf", [128, 16], F32)
        kb.op("pool", lambda e: e.memset(self.neghalf[:], -0.5), writes=[bc])

    def rstd_of(self, ss, n, width, bss, tag):
        kb = self.kb
        kb.op("dve", lambda e: e.tensor_scalar(out=ss, in0=ss, scalar1=1.0 / n, scalar2=EPS,
                                               op0=ALU.mult, op1=ALU.add), reads=[bss], writes=[bss])
        kb.op("pool", lambda e: e.tensor_tensor(out=ss, in0=ss, in1=self.neghalf[:, 0:width], op=ALU.pow),
              reads=[bss, self.b_const], writes=[bss])

    def rope_tables(self, s, half, invf_name, tag):
        kb, I = self.kb, self.I
        key = (kb.phase_id, half)
        if not hasattr(self, "_rope"):
            self._rope = {}
        if key not in self._rope:
            self._rope[key] = dict(
                b=Buf(),
                pos_i=kb.sb("pos_i", [128, NT], I32), pos_f=kb.sb("pos_f", [128, NT], F32), invf=kb.sb("invf", [128, half], F32),
                ang=kb.sb("ang", [128, NT, half], F32), kq=kb.sb("kq", [128, NT, half], F32), ki=kb.sb("ki", [128, NT, half], I32),
                ys=kb.sb("ys", [128, NT, half], F32), mm=kb.sb("mmk", [128, NT, half], F32),
                cos=kb.sb("cos", [128, NT, half], F32), sin=kb.sb("sin", [128, NT, half], F32))
        R_ = self._rope[key]
        b = R_["b"]
        pos_i, pos_f, invf, ang, kq, ki, ys, mm, cos, sin = (R_[n] for n in ("pos_i", "pos_f", "invf", "ang", "kq", "ki", "ys", "mm", "cos", "sin"))
        kb.dma("sp", pos_i[:], I["pos_pt"][s], writes=[b])
        kb.dma("sp", invf[:], I[invf_name].partition_broadcast(128), writes=[b])
        kb.op("dve", lambda e: e.tensor_copy(out=pos_f[:], in_=pos_i[:]), reads=[b], writes=[b])
        kb.op("dve", lambda e: e.tensor_tensor(out=ang[:], in0=bc_last(pos_f[:, :], half), in1=bc_mid(invf[:, :], NT),
                                               op=ALU.mult), reads=[b], writes=[b])
        kb.op("dve", lambda e: e.tensor_scalar(out=kq[:], in0=ang[:], scalar1=1.0 / (2 * PI), scalar2=None,
                                               op0=ALU.mult), reads=[b], writes=[b])
        kb.op("dve", lambda e: e.tensor_copy(out=ki[:], in_=kq[:]), reads=[b], writes=[b])
        kb.op("dve", lambda e: e.tensor_copy(out=kq[:], in_=ki[:]), reads=[b], writes=[b])
        kb.op("dve", lambda e: e.scalar_tensor_tensor(out=ang[:], in0=kq[:], scalar=-2 * PI, in1=ang[:],
                                                      op0=ALU.mult, op1=ALU.add), reads=[b], writes=[b])
        lim = 3.1415925
        for shift, dst in ((0.0, sin), (PI / 2, cos)):
            kb.op("dve", lambda e, shift=shift: e.tensor_scalar(out=ys[:], in0=ang[:], scalar1=shift, scalar2=None,
                                                                op0=ALU.add), reads=[b], writes=[b])
            kb.op("dve", lambda e: e.tensor_scalar(out=mm[:], in0=ys[:], scalar1=PI, scalar2=-2 * PI,
                                                   op0=ALU.is_gt, op1=ALU.mult), reads=[b], writes=[b])
            kb.op("dve", lambda e: e.tensor_tensor(out=ys[:], in0=ys[:], in1=mm[:], op=ALU.add), reads=[b], writes=[b])
            kb.op("dve", lambda e: e.tensor_scalar(out=ys[:], in0=ys[:], scalar1=lim, scalar2=-lim,
                                                   op0=ALU.min, op1=ALU.max), reads=[b], writes=[b])
            kb.op("act", lambda e, dst=dst: e.activation(out=dst[:], in_=ys[:], func=AF.Sin), reads=[b], writes=[b])
        return cos, sin, b

    def load_w_bf16(self, dst, src, kchunks, bw):
        v = src.rearrange("(k p) n -> p k n", p=128)
        for k in range(kchunks):
            self.kb.dma("pool", dst[:, k, :], v[:, k, :], writes=[bw])

    def bcast_load(self, dst, src1d, b):
        self.kb.dma("sp", dst, src1d.partition_broadcast(128), writes=[b])

    def norm_mod_T(self, x_t, bx, gmod, shift, bmod, tmp, btmp, h_bf, bh, tp, btp, hT, bhT, ss, bss):
        kb = self.kb
        kb.op("dve", lambda e: e.scalar_tensor_tensor(out=tmp[:], in0=x_t[:], scalar=1.0, in1=x_t[:], op0=ALU.mult, op1=ALU.mult, accum_out=ss[:, 0:1]),
              reads=[bx], writes=[btmp, bss])
        self.rstd_of(ss[:, 0:1], D, 1, bss, "")
        kb.op("dve", lambda e: e.scalar_tensor_tensor(out=tmp[:], in0=x_t[:], scalar=ss[:, 0:1], in1=gmod[:],
                                                      op0=ALU.mult, op1=ALU.mult), reads=[bx, bss, bmod], writes=[btmp])
        kb.op("dve", lambda e: e.tensor_tensor(out=h_bf[:], in0=tmp[:], in1=shift[:], op=ALU.add),
              reads=[btmp, bmod], writes=[bh])
        for k in range(8):
            kb.op("pe", lambda e, k=k: e.transpose(out=tp[:, k, :], in_=h_bf[:, k * 128:(k + 1) * 128],
                                                   identity=self.ident_b[:]), reads=[bh, self.b_const], writes=[btp])
        kb.op("act", lambda e: e.copy(out=hT[:], in_=tp[:]), reads=[btp], writes=[bhT])

    def phase0(self):
        kb, I, S, nseq = self.kb, self.I, self.S, self.nseq
        kb.push()
        cin = kb.sb("cin", [128, nseq, 8], F32)
        cact = kb.sb("cact", [128, nseq, 8], F32)
        crep = kb.sb("crep", [128, nseq, 8, 128], F32)
        bcr = Buf()
        for s in range(nseq):
            kb.dma("sp", cin[:, s, :], I["c_pk"][s], writes=[bcr])
        kb.op("act", lambda e: e.activation(out=cact[:], in_=cin[:], func=AF.Silu), reads=[bcr], writes=[bcr])
        for s in range(nseq):
            kb.op("dve", lambda e, s=s: e.tensor_copy(out=crep[:, s, :, :], in_=bc_last(cact[:, s, :], 128)),
                  reads=[bcr], writes=[bcr])
        adab = kb.sb("adab", [128, 6 * D], F32)
        ng = [kb.sb("ng1", [128, D], F32), kb.sb("ng2", [128, D], F32)]
        bab = Buf()
        wch = [kb.sb("wch%d" % i, [128, 8, 512], F32) for i in range(2)]
        bwch = [Buf(), Buf()]
        pm = [kb.ps("p0pm%d" % i, [128, 512], F32) for i in range(2)]
        bpm = [Buf(), Buf()]
        modt = [kb.sb("modt%d" % i, [128, 512], F32) for i in range(3)]
        bmodt = [Buf() for _ in range(3)]
        n = 0
        u = 0
        for l in range(2):
            for q in range(6):
                kb.dma("sp", adab[:, q * D:(q + 1) * D], I["ada_b"][l, q * D:(q + 1) * D].partition_broadcast(128),
                       writes=[bab])
            self.bcast_load(ng[0][:], I["norm1_g"][l], bab)
            self.bcast_load(ng[1][:], I["norm2_g"][l], bab)
            for j in range(12):
                wv = I["ada_w"][l][:, j * 512:(j + 1) * 512].rearrange("(k p) n -> p k n", p=128)
                w_ = wch[n % 2]
                kb.dma("sp", w_[:], wv, writes=[bwch[n % 2]])
                for s in range(nseq):
                    p_ = pm[u % 2]
                    for k in range(8):
                        kb.op("pe", lambda e, k=k, s=s, p_=p_, w_=w_: e.matmul(p_[:], lhsT=crep[:, s, k, :], rhs=w_[:, k, :],
                                                                             start=(k == 0), stop=(k == 7)),
                              reads=[bcr, bwch[n % 2]], writes=[bpm[u % 2]])
                    m_ = modt[u % 3]
                    bm_ = bmodt[u % 3]
                    kb.op("dve", lambda e, p_=p_, m_=m_, j=j: e.tensor_tensor(out=m_[:], in0=p_[:],
                                                                           in1=adab[:, j * 512:(j + 1) * 512], op=ALU.add),
                          reads=[bpm[u % 2], bab], writes=[bm_])
                    part, half = j // 2, j % 2
                    if part in (1, 4):
                        g_ = ng[0] if part == 1 else ng[1]
                        kb.op("dve", lambda e, m_=m_, g_=g_, half=half: e.scalar_tensor_tensor(
                            out=m_[:], in0=m_[:], scalar=1.0, in1=g_[:, half * 512:(half + 1) * 512],
                            op0=ALU.add, op1=ALU.mult), reads=[bm_, bab], writes=[bm_])
                    kb.dma("sp", S["MOD"][s, l, part, :, half * 512:(half + 1) * 512], m_[:],
                           reads=[bm_], writes=[self.db("MOD", (s, l, part))])
                    u += 1
                n += 1
        kb.pop()

    def phase1a(self):
        kb, I, S, nseq = self.kb, self.I, self.S, self.nseq
        kb.push()
        self.zero_xg()
        bw = Buf()
        w_in = kb.sb("w_in_a", [128, 8, 672], BF16)
        w_uq = kb.sb("w_uq", [128, 3, 768], BF16)
        w_ukv = kb.sb("w_ukv", [128, 2, 1024], BF16)
        self.load_w_bf16(w_in, I["hyb_w_in"][:, 0:672], 8, bw)
        self.load_w_bf16(w_uq, I["mla_w_uq"], 3, bw)
        self.load_w_bf16(w_ukv, I["mla_w_ukv"], 2, bw)
        gcq = kb.sb("gcq", [128, 384], F32)
        gckv = kb.sb("gckv", [128, 256], F32)
        gq = kb.sb("gq", [128, 96], F32)
        gk = kb.sb("gk", [128, 96], F32)
        self.bcast_load(gcq[:], I["mla_cq_norm_g"], bw)
        self.bcast_load(gckv[:], I["mla_ckv_norm_g"], bw)
        self.bcast_load(gq[:], I["mla_q_head_g"], bw)
        self.bcast_load(gk[:], I["mla_k_head_g"], bw)

        kT = kb.sb("kT", [96, 8, SEQ], BF16)
        V = kb.sb("Vc", [128, NT, 8, 65], BF16)
        bkT = [Buf() for _ in range(NT)]
        bV = [Buf() for _ in range(NT)]
        bVones = Buf()
        kb.op("pool", lambda e: e.memset(V[:, :, :, 64:65], 1.0), writes=[bVones])

        gmod = kb.sb("gmod1", [128, D], F32)
        shift = kb.sb("shift1", [128, D], F32)
        bmod = Buf()
        x_t = [kb.sb("x_t%d" % i, [128, D], F32) for i in range(2)]
        bx = [Buf(), Buf()]
        tmp = kb.sb("tmp", [128, D], F32); btmp = Buf()
        h_bf = kb.sb("h_bf", [128, D], BF16); bh = Buf()
        hT = kb.sb("hT", [128, 8, 128], BF16); bhT = Buf()
        ss = kb.sb("ss", [128, 4], F32); bss = Buf()
        proj = kb.sb("proj", [128, 672], F32); bproj = Buf()
        sq = kb.sb("sq", [128, 8, 96], F32); bsq = Buf()
        cqn = kb.sb("cqn", [128, 384], BF16); bcqn = Buf()
        cqT = kb.sb("cqT", [128, 3, 128], BF16); bcqT = Buf()
        ckvn = kb.sb("ckvn", [128, 256], BF16); bckvn = Buf()
        ckvT = kb.sb("ckvT", [128, 2, 128], BF16); bckvT = Buf()
        q_sb = kb.sb("q_sb", [128, 8, 96], F32); bq = Buf()
        qn = kb.sb("qn", [128, 8, 96], F32); bqn = Buf()
        rq8 = kb.sb("rq8", [128, 16], F32); brq8 = Buf()
        R = kb.sb("Rr", [128, 8, 96], F32); bR = Buf()
        q_full = kb.sb("q_full", [128, 8, 96], BF16); bqf = Buf()
        qTs = [kb.sb("qT%d" % i, [96, 8, 128], BF16) for i in range(2)]; bqTs = [Buf(), Buf()]
        kv_sb = kb.sb("kv_sb", [128, 8, 128], F32); bkv = Buf()
        k_full = kb.sb("k_full", [128, 8, 96], BF16); bkf = Buf()
        kr = kb.sb("kr", [128, 32], F32); bkr = Buf()
        kr2 = kb.sb("kr2", [128, 32], F32)
        rt = [kb.sb("rt%d" % i, [128, 8, 16], F32) for i in range(4)]; brt = Buf()
        PT = [kb.sb("PT%d" % i, [128, 4, 128], BF16) for i in range(3)]; bPT = [Buf() for _ in range(3)]
        attn = kb.sb("attn", [128, 8, 64], BF16); battn = Buf()
        rden = kb.sb("rden", [128, 8], F32); brden = Buf()

        tp = [kb.ps("tp%d" % i, [128, 8, 128], BF16) for i in range(2)]; btp = [Buf(), Buf()]
        mm = [kb.ps("mm%d" % i, [128, 512], F32) for i in range(2)]; bmm = [Buf(), Buf()]
        s2 = kb.ps("s2", [128, 2, 512], F32); bs2 = [Buf(), Buf()]
        oo = [kb.ps("oo%d" % i, [128, 512], F32) for i in range(2)]; boo = [Buf(), Buf()]
        scale = 96.0 ** -0.5
        uc = {"n": 0}
        for s in range(nseq):
            cos, sin, brope = self.rope_tables(s, 16, "k_invf16", "a%d" % s)
            kb.dma("sp", gmod[:], S["MOD"][s, 0, 1], reads=[self.db("MOD", (s, 0, 1))], writes=[bmod])
            kb.dma("sp", shift[:], S["MOD"][s, 0, 0], reads=[self.db("MOD", (s, 0, 0))], writes=[bmod])
            def stage_a(t, s=s, cos=cos, sin=sin, brope=brope):
                X = x_t[t % 2]; bX = bx[t % 2]
                qT = qTs[t % 2]; bqT = bqTs[t % 2]
                kb.dma("sp", X[:], I["x"][(s * NT + t) * 128:(s * NT + t + 1) * 128, :], writes=[bX])
                self.norm_mod_T(X, bX, gmod, shift, bmod, tmp, btmp, h_bf, bh, tp[0], btp[0], hT, bhT, ss, bss)
                for gi, (c0, c1) in enumerate(((0, 512), (512, 672))):
                    for k in range(8):
                        kb.op("pe", lambda e, k=k, gi=gi, c0=c0, c1=c1: e.matmul(mm[gi][:, 0:c1 - c0], lhsT=hT[:, k, :],
                                                                              rhs=w_in[:, k, c0:c1], start=(k == 0), stop=(k == 7)),
                              reads=[bhT, bw], writes=[bmm[gi]])
                    kb.op("act", lambda e, gi=gi, c0=c0, c1=c1: e.copy(out=proj[:, c0:c1], in_=mm[gi][:, 0:c1 - c0]),
                          reads=[bmm[gi]], writes=[bproj])
                for ci, (c0, w) in enumerate(((0, 384), (384, 256), (640, 32))):
                    kb.op("dve", lambda e, c0=c0, w=w, ci=ci: e.scalar_tensor_tensor(out=tmp[:, 0:w], in0=proj[:, c0:c0 + w], scalar=1.0, in1=proj[:, c0:c0 + w], op0=ALU.mult, op1=ALU.mult, accum_out=ss[:, 1 + ci:2 + ci]), reads=[bproj], writes=[btmp, bss])
                kb.op("dve", lambda e: e.tensor_scalar(out=ss[:, 1:2], in0=ss[:, 1:2], scalar1=1.0 / 384, scalar2=EPS,
                                                       op0=ALU.mult, op1=ALU.add), reads=[bss], writes=[bss])
                kb.op("dve", lambda e: e.tensor_scalar(out=ss[:, 2:3], in0=ss[:, 2:3], scalar1=1.0 / 256, scalar2=EPS,
                                                       op0=ALU.mult, op1=ALU.add), reads=[bss], writes=[bss])
                kb.op("dve", lambda e: e.tensor_scalar(out=ss[:, 3:4], in0=ss[:, 3:4], scalar1=1.0 / 32, scalar2=EPS,
                                                       op0=ALU.mult, op1=ALU.add), reads=[bss], writes=[bss])
                kb.op("pool", lambda e: e.tensor_tensor(out=ss[:, 1:4], in0=ss[:, 1:4], in1=self.neghalf[:, 0:3], op=ALU.pow),
                      reads=[bss, self.b_const], writes=[bss])
                kb.op("dve", lambda e: e.scalar_tensor_tensor(out=cqn[:], in0=proj[:, 0:384], scalar=ss[:, 1:2], in1=gcq[:],
                                                              op0=ALU.mult, op1=ALU.mult), reads=[bproj, bss, bw], writes=[bcqn])
                kb.op("dve", lambda e: e.scalar_tensor_tensor(out=ckvn[:], in0=proj[:, 384:640], scalar=ss[:, 2:3], in1=gckv[:],
                                                              op0=ALU.mult, op1=ALU.mult), reads=[bproj, bss, bw], writes=[bckvn])
                kb.op("dve", lambda e: e.scalar_tensor_tensor(out=kr[:], in0=proj[:, 640:672], scalar=ss[:, 3:4], in1=gk[:, 64:96],
                                                              op0=ALU.mult, op1=ALU.mult), reads=[bproj, bss, bw], writes=[bkr])
                for k in range(3):
                    kb.op("pe", lambda e, k=k: e.transpose(out=tp[1][:, k, :], in_=cqn[:, k * 128:(k + 1) * 128],
                                                           identity=self.ident_b[:]), reads=[bcqn, self.b_const], writes=[btp[1]])
                for k in range(2):
                    kb.op("pe", lambda e, k=k: e.transpose(out=tp[1][:, 3 + k, :], in_=ckvn[:, k * 128:(k + 1) * 128],
                                                           identity=self.ident_b[:]), reads=[bckvn, self.b_const], writes=[btp[1]])
                kb.op("act", lambda e: e.copy(out=cqT[:], in_=tp[1][:, 0:3, :]), reads=[btp[1]], writes=[bcqT])
                kb.op("act", lambda e: e.copy(out=ckvT[:], in_=tp[1][:, 3:5, :]), reads=[btp[1]], writes=[bckvT])
                for gi, (c0, c1) in enumerate(((0, 512), (512, 768))):
                    for k in range(3):
                        kb.op("pe", lambda e, k=k, gi=gi, c0=c0, c1=c1: e.matmul(mm[gi][:, 0:c1 - c0], lhsT=cqT[:, k, :],
                                                                              rhs=w_uq[:, k, c0:c1], start=(k == 0), stop=(k == 2)),
                              reads=[bcqT, bw], writes=[bmm[gi]])
                    kb.op("act", lambda e, gi=gi, c0=c0, c1=c1: e.copy(
                        out=q_sb[:].rearrange("p h d -> p (h d)")[:, c0:c1], in_=mm[gi][:, 0:c1 - c0]),
                        reads=[bmm[gi]], writes=[bq])
                kb.op("dve", lambda e: e.tensor_tensor(out=sq[:], in0=q_sb[:], in1=q_sb[:], op=ALU.mult), reads=[bq], writes=[bsq])
                kb.op("dve", lambda e: e.tensor_reduce(out=rq8[:, 0:8], in_=sq[:, :, 0:64], axis=AX.X, op=ALU.add),
                      reads=[bsq], writes=[brq8])
                kb.op("dve", lambda e: e.tensor_reduce(out=rq8[:, 8:16], in_=sq[:, :, 64:96], axis=AX.X, op=ALU.add),
                      reads=[bsq], writes=[brq8])
                kb.op("dve", lambda e: e.tensor_scalar(out=rq8[:, 0:8], in0=rq8[:, 0:8], scalar1=1.0 / 64, scalar2=EPS,
                                                       op0=ALU.mult, op1=ALU.add), reads=[brq8], writes=[brq8])
                kb.op("dve", lambda e: e.tensor_scalar(out=rq8[:, 8:16], in0=rq8[:, 8:16], scalar1=1.0 / 32, scalar2=EPS,
                                                       op0=ALU.mult, op1=ALU.add), reads=[brq8], writes=[brq8])
                kb.op("pool", lambda e: e.tensor_tensor(out=rq8[:], in0=rq8[:], in1=self.neghalf[:, 0:16], op=ALU.pow),
                      reads=[brq8, self.b_const], writes=[brq8])
                kb.op("dve", lambda e: e.tensor_tensor(out=qn[:, :, 0:64], in0=q_sb[:, :, 0:64], in1=bc_last(rq8[:, 0:8], 64),
                                                       op=ALU.mult), reads=[bq, brq8], writes=[bqn])
                kb.op("dve", lambda e: e.tensor_tensor(out=qn[:, :, 64:96], in0=q_sb[:, :, 64:96], in1=bc_last(rq8[:, 8:16], 32),
                                                       op=ALU.mult), reads=[bq, brq8], writes=[bqn])
                kb.op("dve", lambda e: e.tensor_tensor(out=qn[:], in0=qn[:], in1=bc_mid(gq[:, :], 8), op=ALU.mult),
                      reads=[bqn, bw], writes=[bqn])
                kb.op("act", lambda e: e.copy(out=q_full[:, :, 0:64], in_=qn[:, :, 0:64]), reads=[bqn], writes=[bqf])
                cb = bc_mid(cos[:, t, :], 8)
                sb_ = bc_mid(sin[:, t, :], 8)
                x1 = qn[:, :, 64:80]
                x2 = qn[:, :, 80:96]
                kb.op("dve", lambda e: e.tensor_tensor(out=rt[0][:], in0=x1, in1=cb, op=ALU.mult), reads=[bqn, brope], writes=[brt])
                kb.op("dve", lambda e: e.tensor_tensor(out=rt[1][:], in0=x2, in1=sb_, op=ALU.mult), reads=[bqn, brope], writes=[brt])
                kb.op("dve", lambda e: e.tensor_tensor(out=rt[2][:], in0=x2, in1=cb, op=ALU.mult), reads=[bqn, brope], writes=[brt])
                kb.op("dve", lambda e: e.tensor_tensor(out=rt[3][:], in0=x1, in1=sb_, op=ALU.mult), reads=[bqn, brope], writes=[brt])
                kb.op("dve", lambda e: e.tensor_tensor(out=q_full[:, :, 64:80], in0=rt[0][:], in1=rt[1][:], op=ALU.subtract),
                      reads=[brt], writes=[bqf])
                kb.op("dve", lambda e: e.tensor_tensor(out=q_full[:, :, 80:96], in0=rt[2][:], in1=rt[3][:], op=ALU.add),
                      reads=[brt], writes=[bqf])
                for h in range(8):
                    kb.op("pe", lambda e, h=h: e.transpose(out=tp[0][0:96, h, :], in_=q_full[:, h, :], identity=self.ident_b[:]),
                          reads=[bqf, self.b_const], writes=[btp[0]])
                kb.op("act", lambda e: e.copy(out=qT[:], in_=tp[0][0:96, :, :]), reads=[btp[0]], writes=[bqT])
                for gi in range(2):
                    for k in range(2):
                        kb.op("pe", lambda e, k=k, gi=gi: e.matmul(mm[gi][:], lhsT=ckvT[:, k, :], rhs=w_ukv[:, k, gi * 512:(gi + 1) * 512],
                                                                 start=(k == 0), stop=(k == 1)), reads=[bckvT, bw], writes=[bmm[gi]])
                    kb.op("act", lambda e, gi=gi: e.copy(out=kv_sb[:].rearrange("p h d -> p (h d)")[:, gi * 512:(gi + 1) * 512],
                                                        in_=mm[gi][:]), reads=[bmm[gi]], writes=[bkv])
                kb.op("dve", lambda e, t=t: e.tensor_copy(out=V[:, t, :, 0:64], in_=kv_sb[:, :, 64:128]),
                      reads=[bkv, bVones], writes=[bV[t]])
                kb.op("dve", lambda e: e.tensor_tensor(out=sq[:, :, 0:64], in0=kv_sb[:, :, 0:64], in1=kv_sb[:, :, 0:64], op=ALU.mult),
                      reads=[bkv], writes=[bsq])
                kb.op("dve", lambda e: e.tensor_reduce(out=rq8[:, 0:8], in_=sq[:, :, 0:64], axis=AX.X, op=ALU.add),
                      reads=[bsq], writes=[brq8])
                kb.op("dve", lambda e: e.tensor_scalar(out=rq8[:, 0:8], in0=rq8[:, 0:8], scalar1=1.0 / 64, scalar2=EPS,
                                                       op0=ALU.mult, op1=ALU.add), reads=[brq8], writes=[brq8])
                kb.op("pool", lambda e: e.tensor_tensor(out=rq8[:, 0:8], in0=rq8[:, 0:8], in1=self.neghalf[:, 0:8], op=ALU.pow),
                      reads=[brq8, self.b_const], writes=[brq8])
                kb.op("dve", lambda e: e.tensor_tensor(out=sq[:, :, 0:64], in0=kv_sb[:, :, 0:64], in1=bc_last(rq8[:, 0:8], 64),
                                                       op=ALU.mult), reads=[bkv, brq8], writes=[bsq])
                kb.op("dve", lambda e: e.tensor_tensor(out=k_full[:, :, 0:64], in0=sq[:, :, 0:64], in1=bc_mid(gk[:, 0:64], 8),
                                                        op=ALU.mult), reads=[bsq, bw], writes=[bkf])
                c1_ = cos[:, t, :]
                s1_ = sin[:, t, :]
                kb.op("dve", lambda e: e.tensor_tensor(out=rt[0][:, 0, :], in0=kr[:, 0:16], in1=c1_, op=ALU.mult), reads=[bkr, brope], writes=[brt])
                kb.op("dve", lambda e: e.tensor_tensor(out=rt[1][:, 0, :], in0=kr[:, 16:32], in1=s1_, op=ALU.mult), reads=[bkr, brope], writes=[brt])
                kb.op("dve", lambda e: e.tensor_tensor(out=rt[2][:, 0, :], in0=kr[:, 16:32], in1=c1_, op=ALU.mult), reads=[bkr, brope], writes=[brt])
                kb.op("dve", lambda e: e.tensor_tensor(out=rt[3][:, 0, :], in0=kr[:, 0:16], in1=s1_, op=ALU.mult), reads=[bkr, brope], writes=[brt])
                kb.op("dve", lambda e: e.tensor_tensor(out=kr2[:, 0:16], in0=rt[0][:, 0, :], in1=rt[1][:, 0, :], op=ALU.subtract),
                      reads=[brt], writes=[bkr])
                kb.op("dve", lambda e: e.tensor_tensor(out=kr2[:, 16:32], in0=rt[2][:, 0, :], in1=rt[3][:, 0, :], op=ALU.add),
                      reads=[brt], writes=[bkr])
                kb.op("dve", lambda e: e.tensor_copy(out=k_full[:, :, 64:96], in_=bc_mid(kr2[:, :], 8)), reads=[bkr], writes=[bkf])
                for h in range(8):
                    kb.op("pe", lambda e, h=h: e.transpose(out=tp[1][0:96, h, :], in_=k_full[:, h, :], identity=self.ident_b[:]),
                          reads=[bkf, self.b_const], writes=[btp[1]])
                kb.op("act", lambda e, t=t: e.copy(out=kT[:, :, t * 128:(t + 1) * 128], in_=tp[1][0:96, :, :]),
                      reads=[btp[1]], writes=[bkT[t]])
            def stage_b(t, s=s):
                qT = qTs[t % 2]; bqT = bqTs[t % 2]
                units = []
                for h in range(8):
                    for a in range(0, t + 1, 4):
                        units.append((h, a, min(a + 4, t + 1)))

                def emit_S(ui, u):
                    h, a, b = u
                    bank = ui % 2
                    for kt in range(a, b):
                        kb.op("pe", lambda e, kt=kt, h=h, a=a, bank=bank: e.matmul(
                            s2[:, bank, (kt - a) * 128:(kt - a + 1) * 128], lhsT=kT[:, h, kt * 128:(kt + 1) * 128],
                            rhs=qT[:, h, :], start=True, stop=True), reads=[bkT[kt], bqT], writes=[bs2[bank]])

                base = uc["n"]
                emit_S(base, units[0])
                for i, u in enumerate(units):
                    ui = base + i
                    h, a, b = u
                    if i + 1 < len(units):
                        emit_S(ui + 1, units[i + 1])
                    bank = ui % 2
                    P = PT[ui % 3]; bP = bPT[ui % 3]
                    n = (b - a) * 128
                    kb.op("act", lambda e, P=P, bank=bank, n=n: e.activation(
                        out=P[:].rearrange("p a b -> p (a b)")[:, 0:n], in_=s2[:, bank, 0:n], func=AF.Exp, scale=scale),
                        reads=[bs2[bank]], writes=[bP])
                    if b == t + 1:
                        kb.op("dve", lambda e, P=P, j=t - a: e.tensor_tensor(out=P[:, j, :], in0=P[:, j, :], in1=self.mask_le[:],
                                                                           op=ALU.mult), reads=[bP, self.b_const], writes=[bP])
                    ob = oo[h // 4]
                    for kt in range(a, b):
                        kb.op("pe", lambda e, kt=kt, h=h, a=a, P=P, ob=ob: e.matmul(
                            ob[:, (h % 4) * 65:(h % 4) * 65 + 65], lhsT=P[:, kt - a, :], rhs=V[:, kt, h, :],
                            start=(kt == 0), stop=(kt == t)), reads=[bP, bV[kt], bVones], writes=[boo[h // 4]])
                uc["n"] += len(units)
                for hb in range(2):
                    ov = oo[hb][:, 0:260].rearrange("p (h d) -> p h d", d=65)
                    kb.op("dve", lambda e, hb=hb, ov=ov: e.reciprocal(out=rden[:, hb * 4:(hb + 1) * 4], in_=ov[:, :, 64]),
                          reads=[boo[hb]], writes=[brden])
                    kb.op("dve", lambda e, hb=hb, ov=ov: e.tensor_tensor(out=attn[:, hb * 4:(hb + 1) * 4, :], in0=ov[:, :, 0:64],
                                                                        in1=bc_last(rden[:, hb * 4:(hb + 1) * 4], 64), op=ALU.mult),
                          reads=[boo[hb], brden], writes=[battn])
                kb.dma("sp", S["ATT"][s * NT + t], attn[:].rearrange("p h d -> p (h d)"), reads=[battn],
                       writes=[self.db("ATT", s * NT + t)])
            kb.pipeline(stage_a, stage_b, self.ntl)
        kb.pop()

    def alloc_router(self, l):
        kb, I = self.kb, self.I
        r = {}
        r["bw"] = Buf()
        r["rw"] = kb.sb("rw", [128, 8, 32], F32)
        kb.dma("sp", r["rw"][:], I["router_w"][l].rearrange("(k p) n -> p k n", p=128), writes=[r["bw"]])
        r["rb"] = kb.sb("rb", [128, 32], F32)
        self.bcast_load(r["rb"][:], I["router_b"][l], r["bw"])
        r["rwh"] = kb.sb("rwh", [128, 8, 32], BF16)
        r["rwl"] = kb.sb("rwl", [128, 8, 32], BF16)
        kb.op("dve", lambda e: e.tensor_copy(out=r["rwh"][:], in_=r["rw"][:]), reads=[r["bw"]], writes=[r["bw"]])
        kb.op("dve", lambda e: e.tensor_tensor(out=r["rwl"][:], in0=r["rw"][:], in1=r["rwh"][:], op=ALU.subtract),
              reads=[r["bw"]], writes=[r["bw"]])
        r["h2f"] = kb.sb("h2f", [128, D], F32); r["bh2f"] = Buf()
        r["h2hi"] = kb.sb("h2hi", [128, D], BF16); r["bh2hi"] = Buf()
        r["h2lo"] = kb.sb("h2lo", [128, D], BF16); r["bh2lo"] = Buf()
        r["h2Tl"] = kb.sb("h2Tl", [128, 8, 128], BF16); r["bh2Tl"] = Buf()
        r["h2Tb"] = kb.sb("h2Tb", [128, 8, 128], BF16); r["bh2Tb"] = Buf()
        r["lg"] = kb.sb("lg", [128, 32], F32); r["blg"] = Buf()
        r["m8"] = kb.sb("m8", [128, 8], F32)
        r["msk"] = kb.sb("msk", [128, 32], F32)
        r["ex"] = kb.sb("ex", [128, 32], F32)
        r["den"] = kb.sb("den", [128, 2], F32)
        r["G"] = kb.sb("Gt", [128, 32], F32); r["bG"] = Buf()
        r["ss"] = kb.sb("ss2", [128, 1], F32); r["bss"] = Buf()
        r["cnt"] = kb.sb("cnt_b", [128, 32], F32); r["bcnt"] = Buf()
        kb.op("pool", lambda e: e.memset(r["cnt"][:], 0.0), writes=[r["bcnt"]])
        r["Mb"] = kb.sb("Mb", [128, 32], BF16)
        for n_ in ("posf", "valid", "slotm", "oh", "junk", "Gv"):
            r[n_] = kb.sb(n_, [128, 32], F32)
        r["slotf"] = kb.sb("slotf", [128, 4], F32)
        r["sloti"] = kb.sb("sloti", [128, 4], I32); r["bsloti"] = Buf()
        r["gk"] = kb.sb("gk", [128, 4], F32); r["bgk"] = Buf()
        r["brt"] = Buf()
        return r

    def norm2_router(self, r, x1, bx1, gmod2, shift2, bmod, tmp, btmp, tp, btp, mmp, bmmp, tile_idx):
        kb, S = self.kb, self.S
        ss, bss = r["ss"], r["bss"]
        kb.op("dve", lambda e: e.scalar_tensor_tensor(out=tmp[:], in0=x1[:], scalar=1.0, in1=x1[:], op0=ALU.mult, op1=ALU.mult, accum_out=ss[:, 0:1]),
              reads=[bx1], writes=[btmp, bss])
        self.rstd_of(ss[:, 0:1], D, 1, bss, "")
        kb.op("dve", lambda e: e.scalar_tensor_tensor(out=tmp[:], in0=x1[:], scalar=ss[:, 0:1], in1=gmod2[:],
                                                      op0=ALU.mult, op1=ALU.mult), reads=[bx1, bss, bmod], writes=[btmp])
        kb.op("dve", lambda e: e.tensor_tensor(out=r["h2f"][:], in0=tmp[:], in1=shift2[:], op=ALU.add),
              reads=[btmp, bmod], writes=[r["bh2f"]])
        kb.op("act", lambda e: e.copy(out=r["h2hi"][:], in_=r["h2f"][:]), reads=[r["bh2f"]], writes=[r["bh2hi"]])
        kb.op("dve", lambda e: e.tensor_tensor(out=r["h2lo"][:], in0=r["h2f"][:], in1=r["h2hi"][:], op=ALU.subtract),
              reads=[r["bh2f"], r["bh2hi"]], writes=[r["bh2lo"]])
        for k in range(8):
            kb.op("pe", lambda e, k=k: e.transpose(out=tp[0][:, k, :], in_=r["h2hi"][:, k * 128:(k + 1) * 128],
                                                   identity=self.ident_b[:]), reads=[r["bh2hi"], self.b_const], writes=[btp[0]])
        for k in range(8):
            kb.op("pe", lambda e, k=k: e.transpose(out=tp[1][:, k, :], in_=r["h2lo"][:, k * 128:(k + 1) * 128],
                                                   identity=self.ident_b[:]), reads=[r["bh2lo"], self.b_const], writes=[btp[1]])
        kb.op("act", lambda e: e.copy(out=r["h2Tb"][:], in_=tp[0][:]), reads=[btp[0]], writes=[r["bh2Tb"]])
        kb.op("dve", lambda e: e.tensor_copy(out=r["h2Tl"][:], in_=tp[1][:]), reads=[btp[1]], writes=[r["bh2Tl"]])
        if self.debug:
            kb.dma("sp", S["H2T"][tile_idx], r["h2Tb"][:], reads=[r["bh2Tb"]], writes=[self.db("H2T", tile_idx)])
        if _STOP <= 6:
            return
        passes = [("h2Tb", "rwh"), ("h2Tl", "rwh"), ("h2Tb", "rwl")]
        for pi, (a_, w_) in enumerate(passes):
            for k in range(8):
                kb.op("pe", lambda e, k=k, a_=a_, w_=w_, pi=pi: e.matmul(mmp[:, 0:32], lhsT=r[a_][:, k, :], rhs=r[w_][:, k, :],
                                                                       start=(pi == 0 and k == 0), stop=(pi == 2 and k == 7)),
                      reads=[r["bh2Tb"], r["bh2Tl"], r["bw"]], writes=[bmmp])
        lg, m8, msk, ex, den, G = r["lg"], r["m8"], r["msk"], r["ex"], r["den"], r["G"]
        bl = r["blg"]
        kb.op("dve", lambda e: e.tensor_tensor(out=lg[:], in0=mmp[:, 0:32], in1=r["rb"][:], op=ALU.add),
              reads=[bmmp, r["bw"]], writes=[bl])
        if _STOP <= 7:
            return
        kb.op("dve", lambda e: e.max(out=m8[:], in_=lg[:]), reads=[bl], writes=[bl])
        kb.op("dve", lambda e: e.tensor_scalar(out=msk[:], in0=lg[:], scalar1=m8[:, 3:4], scalar2=None, op0=ALU.is_ge),
              reads=[bl], writes=[bl])
        kb.op("dve", lambda e: e.tensor_scalar(out=den[:, 1:2], in0=m8[:, 0:1], scalar1=-1.0, scalar2=None, op0=ALU.mult),
              reads=[bl], writes=[bl])
        kb.op("act", lambda e: e.activation(out=ex[:], in_=lg[:], func=AF.Exp, bias=den[:, 1:2], scale=1.0),
              reads=[bl], writes=[bl])
        kb.op("dve", lambda e: e.scalar_tensor_tensor(out=ex[:], in0=ex[:], scalar=1.0, in1=msk[:], op0=ALU.mult, op1=ALU.mult, accum_out=den[:, 0:1]), reads=[bl], writes=[bl])
        kb.op("dve", lambda e: e.reciprocal(out=den[:, 0:1], in_=den[:, 0:1]), reads=[bl], writes=[bl])
        kb.op("dve", lambda e: e.tensor_scalar(out=G[:], in0=ex[:], scalar1=den[:, 0:1], scalar2=None, op0=ALU.mult),
              reads=[bl], writes=[r["bG"]])
        if self.debug:
            kb.dma("sp", S["GS"][tile_idx], G[:], reads=[r["bG"]], writes=[self.db("GS", tile_idx)])
        cap = self.cap
        brt = r["brt"]
        kb.op("dve", lambda e: e.tensor_copy(out=r["Mb"][:], in_=msk[:]), reads=[bl], writes=[brt])
        kb.op("pe", lambda e: e.matmul(mmp[:, 32:64], lhsT=self.U_b[:], rhs=r["Mb"][:], start=True, stop=True),
              reads=[brt, self.b_const], writes=[bmmp])
        kb.op("pe", lambda e: e.matmul(mmp[:, 64:96], lhsT=self.ones_b[:], rhs=r["Mb"][:], start=True, stop=True),
              reads=[brt, self.b_const], writes=[bmmp])
        kb.op("dve", lambda e: e.tensor_tensor(out=r["posf"][:], in0=mmp[:, 32:64], in1=r["cnt"][:], op=ALU.add),
              reads=[bmmp, r["bcnt"]], writes=[brt])
        kb.op("dve", lambda e: e.tensor_tensor(out=r["cnt"][:], in0=mmp[:, 64:96], in1=r["cnt"][:], op=ALU.add),
              reads=[bmmp, r["bcnt"]], writes=[r["bcnt"]])
        kb.op("dve", lambda e: e.tensor_scalar(out=r["valid"][:], in0=r["posf"][:], scalar1=float(cap), scalar2=None, op0=ALU.is_lt),
              reads=[brt], writes=[brt])
        kb.op("dve", lambda e: e.tensor_tensor(out=r["slotm"][:], in0=r["posf"][:], in1=self.iotaE[:], op=ALU.add),
              reads=[brt, self.b_const], writes=[brt])
        kb.op("dve", lambda e: e.tensor_scalar(out=r["junk"][:], in0=r["valid"][:], scalar1=-1.0e6, scalar2=1.0e6,
                                               op0=ALU.mult, op1=ALU.add), reads=[brt], writes=[brt])
        kb.op("dve", lambda e: e.tensor_tensor(out=r["slotm"][:], in0=r["slotm"][:], in1=r["junk"][:], op=ALU.add),
              reads=[brt], writes=[brt])
        kb.op("dve", lambda e: e.tensor_tensor(out=r["Gv"][:], in0=G[:], in1=r["valid"][:], op=ALU.mult),
              reads=[brt, r["bG"]], writes=[brt])
        for k in range(4):
            kb.op("dve", lambda e, k=k: e.tensor_scalar(out=r["oh"][:], in0=lg[:], scalar1=m8[:, k:k + 1], scalar2=None, op0=ALU.is_equal),
                  reads=[bl, brt], writes=[brt])
            kb.op("dve", lambda e, k=k: e.scalar_tensor_tensor(out=r["junk"][:], in0=r["oh"][:], scalar=1.0, in1=r["slotm"][:],
                                                              op0=ALU.mult, op1=ALU.mult, accum_out=r["slotf"][:, k:k + 1]),
                  reads=[brt], writes=[brt])
            kb.op("dve", lambda e, k=k: e.scalar_tensor_tensor(out=r["junk"][:], in0=r["oh"][:], scalar=1.0, in1=r["Gv"][:],
                                                              op0=ALU.mult, op1=ALU.mult, accum_out=r["gk"][:, k:k + 1]),
                  reads=[brt, r["bgk"]], writes=[brt, r["bgk"]])
        kb.op("dve", lambda e: e.tensor_copy(out=r["sloti"][:], in_=r["slotf"][:]), reads=[brt, r["bsloti"]], writes=[r["bsloti"]])
        kb.dma("sp", S["SLOT"][tile_idx], r["sloti"][:], reads=[r["bsloti"]], writes=[self.db("SLOT", tile_idx)])
        kb.dma("sp", S["GK"][tile_idx], r["gk"][:], reads=[r["bgk"]], writes=[self.db("GK", tile_idx)])
        for k in range(4):
            kb.idma(S["XG"], r["sloti"][:, k:k + 1], r["h2hi"][:], None, 32 * cap - 1, reads=[r["bsloti"], r["bh2hi"]])

    def phase1b(self):
        kb, I, S, nseq = self.kb, self.I, self.S, self.nseq
        kb.push()
        bw = Buf()
        w_in = kb.sb("w_in_b", [128, 8, 2048], BF16)
        w_out = kb.sb("w_out", [128, 8, D], BF16)
        self.load_w_bf16(w_in, I["hyb_w_in"][:, 672:2720], 8, bw)
        self.load_w_bf16(w_out, I["hyb_w_out"], 8, bw)
        retg = kb.sb("retg", [128, 512], F32)
        self.bcast_load(retg[:], I["ret_norm_g"], bw)
        decT = kb.sb("decT", [128, 8 * 128], F32)
        qdec = kb.sb("qdec", [128, 8], F32)
        kdec = kb.sb("kdec", [128, 8], F32)
        cdec = kb.sb("cdec", [128, 4], F32)
        kb.dma("sp", decT[:], I["k_decayT"], writes=[bw])
        kb.dma("sp", qdec[:], I["k_qdec"], writes=[bw])
        kb.dma("sp", kdec[:], I["k_kdec"], writes=[bw])
        kb.dma("sp", cdec[:], I["k_cdec"], writes=[bw])
        r = self.alloc_router(0)

        mods = {n: kb.sb(n, [128, D], F32) for n in ("gmod1", "shift1", "gate1", "gmod2", "shift2")}
        bmod = Buf()
        x_t = [kb.sb("x_t%d" % i, [128, D], F32) for i in range(2)]; bx = [Buf(), Buf()]
        tmp = kb.sb("tmp", [128, D], F32); btmp = Buf()
        h_bf = kb.sb("h_bf", [128, D], BF16); bh = Buf()
        hT = kb.sb("hT", [128, 8, 128], BF16); bhT = Buf()
        ss = kb.sb("ss", [128, 4], F32); bss = Buf()
        raw = [kb.sb("raw%d" % i, [128, 8, 64], F32) for i in range(2)]; braw = [Buf(), Buf()]
        rr = [kb.sb("rr%d" % i, [128, 8, 64], F32) for i in range(2)]; brr = [Buf(), Buf()]
        rt = [kb.sb("rt%d" % i, [128, 8, 32], F32) for i in range(4)]; brt = Buf()
        rq_bf = kb.sb("rq_bf", [128, 8, 64], BF16); brqb = Buf()
        rqd_bf = kb.sb("rqd_bf", [128, 8, 64], BF16); brqd = Buf()
        rk_bf = kb.sb("rk_bf", [128, 8, 64], BF16); brkb = Buf()
        rkd_bf_2 = [kb.sb("rkd_bf%d" % i, [128, 8, 64], BF16) for i in range(2)]; brkd_2 = [Buf(), Buf()]
        v_bf_2 = [kb.sb("v_bf%d" % i, [128, 8, 64], BF16) for i in range(2)]; bv_2 = [Buf(), Buf()]
        sg_2 = [kb.sb("sg%d" % i, [128, 512], F32) for i in range(2)]; bsg_2 = [Buf(), Buf()]
        rqT_2 = [kb.sb("rqT%d" % i, [128, 8, 128], BF16) for i in range(2)]; brqT_2 = [Buf(), Buf()]
        rkT_2 = [kb.sb("rkT%d" % i, [128, 4, 128], BF16) for i in range(2)]; brkT_2 = [Buf(), Buf()]
        Sd = kb.sb("Sd", [128, 8, 128], BF16); bSd = Buf()
        st_f = kb.sb("st_f", [128, 4, 128], F32); bstf = Buf()
        st_b = kb.sb("st_b", [128, 4, 128], BF16); bstb = Buf()
        kb.op("pool", lambda e: e.memset(st_f[:], 0.0), writes=[bstf])
        o_sb = kb.sb("o_sb", [128, 8, 64], F32); bo = Buf()
        oc = kb.sb("oc", [128, 8, 64], F32); boc = Buf()
        st8 = kb.sb("st8", [128, 16], F32); bst8 = Buf()
        mixcat_2 = [kb.sb("mixcat%d" % i, [128, D], BF16) for i in range(2)]; bmixa_2 = [Buf(), Buf()]; bmixy_2 = [Buf(), Buf()]
        tmpB = kb.sb("tmpB", [128, D], F32); btmpB = Buf()
        mixT = kb.sb("mixT", [128, 8, 128], BF16); bmixT = Buf()
        x1 = kb.sb("x1", [128, D], F32); bx1 = Buf()

        tp = [kb.ps("tp%d" % i, [128, 8, 128], BF16) for i in range(2)]; btp = [Buf(), Buf()]
        mm = [kb.ps("mm%d" % i, [128, 512], F32) for i in range(2)]; bmm = [Buf(), Buf()]
        s2 = kb.ps("s2", [128, 2, 512], F32); bs2 = [Buf(), Buf()]
        oo = [kb.ps("oo%d" % i, [128, 512], F32) for i in range(2)]; boo = [Buf(), Buf()]
        tpB = [s2[:, i, :].bitcast(BF16).rearrange("p (k m) -> p k m", m=128) for i in range(2)]
        for s in range(nseq):
            cos, sin, brope = self.rope_tables(s, 32, "k_invf32", "b%d" % s)
            for n_, part in (("gmod1", 1), ("shift1", 0), ("gate1", 2), ("gmod2", 4), ("shift2", 3)):
                kb.dma("sp", mods[n_][:], S["MOD"][s, 0, part], reads=[self.db("MOD", (s, 0, part))], writes=[bmod])
            def stage_a(t, s=s, cos=cos, sin=sin, brope=brope):
                P_ = t % 2
                X = x_t[P_]; bX = bx[P_]
                v_bf = v_bf_2[P_]; bv = bv_2[P_]; sg = sg_2[P_]; bsg = bsg_2[P_]; rqT = rqT_2[P_]; brqT = brqT_2[P_]
                rkT = rkT_2[P_]; brkT = brkT_2[P_]; rkd_bf = rkd_bf_2[P_]; brkd = brkd_2[P_]
                mixcat = mixcat_2[P_]; bmixa = bmixa_2[P_]; bmixy = bmixy_2[P_]
                ti = s * NT + t
                kb.dma("sp", X[:], I["x"][ti * 128:(ti + 1) * 128, :], writes=[bX])
                kb.dma("sp", mixcat[:, 0:512], S["ATT"][ti], reads=[self.db("ATT", ti)], writes=[bmixa])
                self.norm_mod_T(X, bX, mods["gmod1"], mods["shift1"], bmod, tmp, btmp, h_bf, bh, tp[0], btp[0], hT, bhT, ss, bss)
                cb = bc_mid(cos[:, t, :], 8)
                sb_ = bc_mid(sin[:, t, :], 8)
                for gi in range(4):
                    p_ = mm[gi % 2]; bp_ = bmm[gi % 2]
                    for k in range(8):
                        kb.op("pe", lambda e, k=k, gi=gi, p_=p_: e.matmul(p_[:], lhsT=hT[:, k, :], rhs=w_in[:, k, gi * 512:(gi + 1) * 512],
                                                                       start=(k == 0), stop=(k == 7)), reads=[bhT, bw], writes=[bp_])
                    if gi < 2:
                        rw_ = raw[gi]; brw_ = braw[gi]; ro = rr[gi]; bro = brr[gi]
                        kb.op("act", lambda e, p_=p_, rw_=rw_: e.copy(out=rw_[:].rearrange("p h d -> p (h d)"), in_=p_[:]),
                              reads=[bp_], writes=[brw_])
                        x1_ = rw_[:, :, 0:32]; x2_ = rw_[:, :, 32:64]
                        kb.op("dve", lambda e, x1_=x1_: e.tensor_tensor(out=rt[0][:], in0=x1_, in1=cb, op=ALU.mult), reads=[brw_, brope], writes=[brt])
                        kb.op("dve", lambda e, x2_=x2_: e.tensor_tensor(out=rt[1][:], in0=x2_, in1=sb_, op=ALU.mult), reads=[brw_, brope], writes=[brt])
                        kb.op("dve", lambda e, x2_=x2_: e.tensor_tensor(out=rt[2][:], in0=x2_, in1=cb, op=ALU.mult), reads=[brw_, brope], writes=[brt])
                        kb.op("dve", lambda e, x1_=x1_: e.tensor_tensor(out=rt[3][:], in0=x1_, in1=sb_, op=ALU.mult), reads=[brw_, brope], writes=[brt])
                        kb.op("dve", lambda e, ro=ro: e.tensor_tensor(out=ro[:, :, 0:32], in0=rt[0][:], in1=rt[1][:], op=ALU.subtract),
                              reads=[brt], writes=[bro])
                        kb.op("dve", lambda e, ro=ro: e.tensor_tensor(out=ro[:, :, 32:64], in0=rt[2][:], in1=rt[3][:], op=ALU.add),
                              reads=[brt], writes=[bro])
                        if gi == 0:
                            kb.op("act", lambda e, ro=ro: e.copy(out=rq_bf[:], in_=ro[:]), reads=[bro], writes=[brqb])
                            kb.op("dve", lambda e, ro=ro: e.tensor_tensor(out=rqd_bf[:], in0=ro[:], in1=bc_last(qdec[:, :], 64), op=ALU.mult),
                                  reads=[bro, bw], writes=[brqd])
                        else:
                            kb.op("act", lambda e, ro=ro: e.mul(out=rk_bf[:], in_=ro[:], mul=0.125), reads=[bro], writes=[brkb])
                            kb.op("dve", lambda e, ro=ro: e.scalar_tensor_tensor(out=rkd_bf[:], in0=ro[:], scalar=0.125,
                                                                                in1=bc_last(kdec[:, :], 64), op0=ALU.mult, op1=ALU.mult),
                                  reads=[bro, bw], writes=[brkd])
                    elif gi == 2:
                        kb.op("act", lambda e, p_=p_: e.copy(out=v_bf[:].rearrange("p h d -> p (h d)"), in_=p_[:]), reads=[bp_], writes=[bv])
                    else:
                        kb.op("act", lambda e, p_=p_: e.activation(out=sg[:], in_=p_[:], func=AF.Silu), reads=[bp_], writes=[bsg])
                for i in range(4):
                    kb.op("pe", lambda e, i=i: e.transpose(out=tp[1][:, i, :], in_=rq_bf[:, 2 * i:2 * i + 2, :].rearrange("p h d -> p (h d)"),
                                                           identity=self.ident_b[:]), reads=[brqb, self.b_const], writes=[btp[1]])
                for i in range(4):
                    kb.op("pe", lambda e, i=i: e.transpose(out=tp[1][:, 4 + i, :], in_=rqd_bf[:, 2 * i:2 * i + 2, :].rearrange("p h d -> p (h d)"),
                                                           identity=self.ident_b[:]), reads=[brqd, self.b_const], writes=[btp[1]])
                for i in range(4):
                    kb.op("pe", lambda e, i=i: e.transpose(out=tp[0][:, i, :], in_=rk_bf[:, 2 * i:2 * i + 2, :].rearrange("p h d -> p (h d)"),
                                                           identity=self.ident_b[:]), reads=[brkb, self.b_const], writes=[btp[0]])
                kb.op("act", lambda e: e.copy(out=rqT[:], in_=tp[1][:]), reads=[btp[1]], writes=[brqT])
                kb.op("dve", lambda e: e.tensor_copy(out=rkT[:], in_=tp[0][:, 0:4, :]), reads=[btp[0]], writes=[brkT])
            def stage_b(t, s=s):
                P_ = t % 2
                X = x_t[P_]; bX = bx[P_]
                v_bf = v_bf_2[P_]; bv = bv_2[P_]; sg = sg_2[P_]; bsg = bsg_2[P_]; rqT = rqT_2[P_]; brqT = brqT_2[P_]
                rkT = rkT_2[P_]; brkT = brkT_2[P_]; rkd_bf = rkd_bf_2[P_]; brkd = brkd_2[P_]
                mixcat = mixcat_2[P_]; bmixa = bmixa_2[P_]; bmixy = bmixy_2[P_]
                ti = s * NT + t
                tmp = tmpB; btmp = btmpB
                for h in range(8):
                    i, o = h // 2, (h % 2) * 64
                    kb.op("pe", lambda e, h=h, i=i, o=o: e.matmul(s2[:, h % 2, i * 128:(i + 1) * 128],
                                                                lhsT=rkT[o:o + 64, i, :], rhs=rqT[o:o + 64, i, :], start=True, stop=True),
                          reads=[brkT, brqT], writes=[bs2[h % 2]])
                for hb in range(2):
                    kb.op("dve", lambda e, hb=hb: e.tensor_tensor(out=Sd[:, hb * 4:(hb + 1) * 4, :].rearrange("p h q -> p (h q)"),
                                                                 in0=s2[:, hb, :], in1=decT[:, hb * 512:(hb + 1) * 512], op=ALU.mult),
                          reads=[bs2[hb], bw], writes=[bSd])
                if _STOP <= 1.5:
                    return
                for i in range(4):
                    if t > 0:
                        kb.op("pe", lambda e, i=i: e.matmul(oo[0][:, i * 128:(i + 1) * 128], lhsT=rqT[:, 4 + i, :],
                                                            rhs=st_b[:, i, :], start=True, stop=False, skip_group_check=True),
                              reads=[brqT, bstb], writes=[boo[0]])
                    for par in range(2):
                        h = 2 * i + par
                        kb.op("pe", lambda e, h=h, i=i, par=par: e.matmul(oo[0][:, h * 64:(h + 1) * 64], lhsT=Sd[:, par * 4 + i, :],
                                                                        rhs=v_bf[:, h, :], start=(t == 0), stop=(t == 0 or par == 1),
                                                                        skip_group_check=(t > 0)),
                              reads=[bSd, bv], writes=[boo[0]])
                if _STOP <= 2:
                    return
                for i in range(4):
                    kb.op("pe", lambda e, i=i: e.matmul(oo[1][:, i * 128:(i + 1) * 128],
                                                        lhsT=rkd_bf[:, 2 * i:2 * i + 2, :].rearrange("p h d -> p (h d)"),
                                                        rhs=v_bf[:, 2 * i:2 * i + 2, :].rearrange("p h d -> p (h d)"), start=True, stop=True),
                          reads=[brkd, bv], writes=[boo[1]])
                kvv = oo[1][:].rearrange("p (i c) -> p i c", c=128)
                for half in range(2):
                    po = half * 64
                    if t == 0:
                        kb.op("dve", lambda e, po=po: e.tensor_copy(out=st_f[po:po + 64, :, po:po + 64], in_=kvv[po:po + 64, :, po:po + 64]),
                              reads=[boo[1]], writes=[bstf])
                    else:
                        for i in range(4):
                            kb.op("dve", lambda e, po=po, i=i: e.scalar_tensor_tensor(
                                out=st_f[po:po + 64, i, po:po + 64], in0=st_f[po:po + 64, i, po:po + 64], scalar=cdec[po:po + 64, i:i + 1],
                                in1=kvv[po:po + 64, i, po:po + 64], op0=ALU.mult, op1=ALU.add), reads=[boo[1], bstf, bw], writes=[bstf])
                kb.op("act", lambda e: e.copy(out=st_b[:], in_=st_f[:]), reads=[bstf], writes=[bstb])
                if _STOP <= 3:
                    return
                kb.op("act", lambda e: e.copy(out=o_sb[:].rearrange("p h d -> p (h d)"), in_=oo[0][:]), reads=[boo[0]], writes=[bo])
                kb.op("dve", lambda e: e.tensor_reduce(out=st8[:, 0:8], in_=o_sb[:], axis=AX.X, op=ALU.add), reads=[bo], writes=[bst8])
                kb.op("dve", lambda e: e.tensor_scalar(out=st8[:, 0:8], in0=st8[:, 0:8], scalar1=-1.0 / 64, scalar2=None, op0=ALU.mult),
                      reads=[bst8], writes=[bst8])
                kb.op("dve", lambda e: e.tensor_tensor(out=oc[:], in0=o_sb[:], in1=bc_last(st8[:, 0:8], 64), op=ALU.add),
                      reads=[bo, bst8], writes=[boc])
                kb.op("dve", lambda e: e.tensor_tensor(out=o_sb[:], in0=oc[:], in1=oc[:], op=ALU.mult), reads=[boc], writes=[bo])
                kb.op("dve", lambda e: e.tensor_reduce(out=st8[:, 8:16], in_=o_sb[:], axis=AX.X, op=ALU.add), reads=[bo], writes=[bst8])
                kb.op("dve", lambda e: e.tensor_scalar(out=st8[:, 8:16], in0=st8[:, 8:16], scalar1=1.0 / 64, scalar2=EPS,
                                                       op0=ALU.mult, op1=ALU.add), reads=[bst8], writes=[bst8])
                kb.op("pool", lambda e: e.tensor_tensor(out=st8[:, 8:16], in0=st8[:, 8:16], in1=self.neghalf[:, 0:8], op=ALU.pow),
                      reads=[bst8, self.b_const], writes=[bst8])
                kb.op("dve", lambda e: e.tensor_tensor(out=oc[:], in0=oc[:], in1=bc_last(st8[:, 8:16], 64), op=ALU.mult),
                      reads=[boc, bst8], writes=[boc])
                kb.op("dve", lambda e: e.tensor_tensor(out=oc[:].rearrange("p h d -> p (h d)"), in0=oc[:].rearrange("p h d -> p (h d)"),
                                                        in1=retg[:], op=ALU.mult), reads=[boc, bw], writes=[boc])
                kb.op("dve", lambda e: e.tensor_tensor(out=mixcat[:, 512:1024], in0=oc[:].rearrange("p h d -> p (h d)"), in1=sg[:],
                                                       op=ALU.mult), reads=[boc, bsg], writes=[bmixy])
                if _STOP <= 4:
                    return
                for k in range(8):
                    kb.op("pe", lambda e, k=k: e.transpose(out=tpB[0][:, k, :], in_=mixcat[:, k * 128:(k + 1) * 128], identity=self.ident_b[:]),
                          reads=[bmixa, bmixy, self.b_const], writes=[bs2[0]])
                kb.op("act", lambda e: e.copy(out=mixT[:], in_=tpB[0]), reads=[bs2[0]], writes=[bmixT])
                for half in range(2):
                    for k in range(8):
                        kb.op("pe", lambda e, k=k, half=half: e.matmul(oo[half][:], lhsT=mixT[:, k, :], rhs=w_out[:, k, half * 512:(half + 1) * 512],
                                                                     start=(k == 0), stop=(k == 7)), reads=[bmixT, bw], writes=[boo[half]])
                    hs = slice(half * 512, (half + 1) * 512)
                    kb.op("dve", lambda e, half=half, hs=hs: e.tensor_tensor(out=tmp[:, hs], in0=oo[half][:], in1=mods["gate1"][:, hs], op=ALU.mult),
                          reads=[boo[half], bmod], writes=[btmp])
                    kb.op("dve", lambda e, hs=hs: e.tensor_tensor(out=x1[:, hs], in0=tmp[:, hs], in1=X[:, hs], op=ALU.add),
                          reads=[btmp, bX], writes=[bx1])
                kb.dma("sp", S["XA"][ti * 128:(ti + 1) * 128, :], x1[:], reads=[bx1], writes=[self.db("XA", ti)])
                if _STOP <= 5:
                    return
                self.norm2_router(r, x1, bx1, mods["gmod2"], mods["shift2"], bmod, tmp, btmp, tpB, bs2, oo[0], boo[0], ti)
            kb.pipeline(stage_a, stage_b, self.ntl)
        kb.pop()

    def zero_xg(self):
        kb, S = self.kb, self.S
        z = kb.sb("zeros", [128, 4096], BF16); bz = Buf()
        kb.op("pool", lambda e: e.memset(z[:], 0.0), writes=[bz])
        nrows = 32 * self.cap
        for r0 in range(0, nrows, 512):
            kb.dma("sp", S["XG"][r0:r0 + 512, :].rearrange("(p a) d -> p (a d)", p=128), z[:], reads=[bz])

    def phase2e(self, l):
        kb, I, S = self.kb, self.I, self.S
        kb.push()
        cap = self.cap
        nblk = cap // 512
        wgu = [kb.sb("wgu%d" % i, [128, 8, 2048], BF16) for i in range(2)]
        wdn = [kb.sb("wdn%d" % i, [128, 8, D], BF16) for i in range(2)]
        bgu = [kb.sb("bgu%d" % i, [128, 16], F32) for i in range(2)]
        bdn = [kb.sb("bdn%d" % i, [1, D], BF16) for i in range(2)]
        bwt = [Buf(), Buf()]
        ones1 = kb.sb("ones1", [1, 128], BF16); bones = Buf()
        kb.op("pool", lambda e: e.memset(ones1[:], 1.0), writes=[bones])
        xg = [kb.sb("xg%d" % i, [128, D], BF16) for i in range(12)]; bxg = [Buf() for _ in range(12)]
        xgT = [kb.sb("xgT%d" % i, [128, 8, 512], BF16) for i in range(2)]; bxgT = [Buf(), Buf()]
        glu = [kb.sb("glu%d" % i, [128, 512], F32) for i in range(2)]; bglu = [Buf(), Buf()]
        sig = [kb.sb("sig%d" % i, [128, 512], F32) for i in range(2)]; bsig = [Buf(), Buf()]
        lin = [kb.sb("lin%d" % i, [128, 512], F32) for i in range(2)]; blin = [Buf(), Buf()]
        actT = [kb.sb("actT%d" % i, [128, 8, 512], BF16) for i in range(2)]; bact = [Buf(), Buf()]
        yg = [kb.sb("yg%d" % i, [128, D], F32) for i in range(3)]; byg = [Buf() for _ in range(3)]
        tp = [kb.ps("tp%d" % i, [128, 8, 128], BF16) for i in range(2)]; btp = [Buf(), Buf()]
        pA = [kb.ps("pA%d" % i, [128, 512], F32) for i in range(2)]; bpA = [Buf(), Buf()]
        pB = [kb.ps("pB%d" % i, [128, 512], F32) for i in range(2)]; bpB = [Buf(), Buf()]
        pC = [kb.ps("pC%d" % i, [128, 512], F32) for i in range(2)]; bpC = [Buf(), Buf()]

        def load_expert(e, slot):
            self.load_w_bf16(wgu[slot], I["exp_w_gu"][l, e], 8, bwt[slot])
            self.load_w_bf16(wdn[slot], I["exp_w_down"][l, e], 8, bwt[slot])
            kb.dma("sp", bgu[slot][:], I["exp_b_gu_pj"][l, e], writes=[bwt[slot]])
            kb.op("dve", lambda en, slot=slot: en.tensor_scalar(out=bgu[slot][:, 8:16], in0=bgu[slot][:, 8:16], scalar1=1.0, scalar2=None,
                                                               op0=ALU.add), reads=[bwt[slot]], writes=[bwt[slot]])
            kb.dma("pool", bdn[slot][:], I["exp_b_down"][l, e:e + 1, :], writes=[bwt[slot]])

        load_expert(0, 0)
        blocks = [(ex, blk) for ex in range(32) for blk in range(nblk)]
        state = dict(xu=0, tu=0)

        def emit_loads(bi):
            ex, blk = blocks[bi]
            r0 = ex * cap + blk * 512
            tiles = []
            for st in range(4):
                i = state["xu"] % len(xg); state["xu"] += 1
                kb.dma("sp", xg[i][:], S["XG"][r0 + st * 128:r0 + (st + 1) * 128, :], writes=[bxg[i]])
                tiles.append(i)
            return tiles

        def emit_transposes(bi, tiles):
            XT = xgT[bi % 2]; bXT = bxgT[bi % 2]
            for st, i in enumerate(tiles):
                T = tp[state["tu"] % 2]; bT = btp[state["tu"] % 2]; state["tu"] += 1
                for k in range(8):
                    kb.op("pe", lambda e, k=k, i=i, T=T: e.transpose(out=T[:, k, :], in_=xg[i][:, k * 128:(k + 1) * 128],
                                                                   identity=self.ident_b[:]), reads=[bxg[i], self.b_const], writes=[bT])
                kb.op("act", lambda e, T=T, XT=XT, st=st: e.copy(out=XT[:, :, st * 128:(st + 1) * 128], in_=T[:]),
                      reads=[bT], writes=[bXT])

        pu = cu = yu = 0
        tl0 = emit_loads(0)
        tl1 = emit_loads(1) if len(blocks) > 1 else None
        emit_transposes(0, tl0)
        for bi, (ex, blk) in enumerate(blocks):
            slot = ex % 2
            if blk == 0 and ex + 1 < 32:
                load_expert(ex + 1, (ex + 1) % 2)
            XT = xgT[bi % 2]; bXT = bxgT[bi % 2]
            A = actT[bi % 2]; bA = bact[bi % 2]
            r0 = ex * cap + blk * 512
            for j in range(8):
                pa = pA[pu % 2]; bpa = bpA[pu % 2]; pb = pB[pu % 2]; bpb = bpB[pu % 2]
                gl = glu[pu % 2]; bgl = bglu[pu % 2]; sg_ = sig[pu % 2]; bsg_ = bsig[pu % 2]; ln = lin[pu % 2]; bln = blin[pu % 2]
                pu += 1
                for k in range(8):
                    kb.op("pe", lambda e, k=k, j=j, pa=pa, slot=slot, XT=XT: e.matmul(
                        pa[:], lhsT=wgu[slot][:, k, j * 128:(j + 1) * 128], rhs=XT[:, k, :],
                        start=(k == 0), stop=(k == 7)), reads=[bwt[slot], bXT], writes=[bpa])
                for k in range(8):
                    kb.op("pe", lambda e, k=k, j=j, pb=pb, slot=slot, XT=XT: e.matmul(
                        pb[:], lhsT=wgu[slot][:, k, 1024 + j * 128:1024 + (j + 1) * 128], rhs=XT[:, k, :],
                        start=(k == 0), stop=(k == 7)), reads=[bwt[slot], bXT], writes=[bpb])
                kb.op("dve", lambda e, pa=pa, gl=gl, j=j, slot=slot: e.tensor_scalar(
                    out=gl[:], in0=pa[:], scalar1=bgu[slot][:, j:j + 1], scalar2=7.0, op0=ALU.add, op1=ALU.min),
                    reads=[bpa, bwt[slot]], writes=[bgl])
                kb.op("act", lambda e, gl=gl, sg_=sg_: e.activation(out=sg_[:], in_=gl[:], func=AF.Sigmoid, scale=1.702),
                      reads=[bgl], writes=[bsg_])
                kb.op("dve", lambda e, pb=pb, ln=ln, j=j, slot=slot: e.tensor_scalar(
                    out=ln[:], in0=pb[:], scalar1=bgu[slot][:, 8 + j:9 + j], scalar2=8.0, op0=ALU.add, op1=ALU.min),
                    reads=[bpb, bwt[slot]], writes=[bln])
                kb.op("dve", lambda e, gl=gl, sg_=sg_: e.tensor_tensor(out=gl[:], in0=gl[:], in1=sg_[:], op=ALU.mult),
                      reads=[bgl, bsg_], writes=[bgl])
                kb.op("dve", lambda e, gl=gl, ln=ln, A=A, j=j: e.scalar_tensor_tensor(out=A[:, j, :], in0=ln[:], scalar=-6.0, in1=gl[:],
                                                                                 op0=ALU.max, op1=ALU.mult),
                      reads=[bgl, bln], writes=[bA])
            if bi + 1 < len(blocks):
                emit_transposes(bi + 1, tl1)
                tl0, tl1 = tl1, (emit_loads(bi + 2) if bi + 2 < len(blocks) else None)
            for st in range(4):
                Y = yg[yu % 3]; bY = byg[yu % 3]; yu += 1
                for half in range(2):
                    pc = pC[cu % 2]; bpc = bpC[cu % 2]; cu += 1
                    for k in range(8):
                        kb.op("pe", lambda e, k=k, st=st, half=half, pc=pc, A=A, slot=slot: e.matmul(
                            pc[:], lhsT=A[:, k, st * 128:(st + 1) * 128], rhs=wdn[slot][:, k, half * 512:(half + 1) * 512],
                            start=(k == 0), stop=False), reads=[bA, bwt[slot]], writes=[bpc])
                    kb.op("pe", lambda e, half=half, pc=pc, slot=slot: e.matmul(
                        pc[:], lhsT=ones1[:, :], rhs=bdn[slot][:, half * 512:(half + 1) * 512], start=False, stop=True),
                        reads=[bones, bwt[slot]], writes=[bpc])
                    if half == 0:
                        kb.op("act", lambda e, pc=pc, Y=Y: e.copy(out=Y[:, 0:512], in_=pc[:]), reads=[bpc], writes=[bY])
                    else:
                        kb.op("dve", lambda e, pc=pc, Y=Y: e.tensor_copy(out=Y[:, 512:1024], in_=pc[:]), reads=[bpc], writes=[bY])
                kb.dma("pool", S["YG"][r0 + st * 128:r0 + (st + 1) * 128, :], Y[:], reads=[bY])
        kb.pop()

    def phase2c(self, l, src, dst, dst_name, zero_after):
        kb, I, S, nseq = self.kb, self.I, self.S, self.nseq
        kb.push()
        cap = self.cap
        gate2 = kb.sb("gate2", [128, D], F32); bg2 = Buf()
        xin = [kb.sb("xin%d" % i, [128, D], F32) for i in range(2)]; bxin = [Buf(), Buf()]
        acc = [kb.sb("acc%d" % i, [128, D], F32) for i in range(2)]; bacc = [Buf(), Buf()]
        yb = [kb.sb("yb%d" % i, [128, D], F32) for i in range(8)]; byb = [Buf() for _ in range(8)]
        sl = [kb.sb("sl%d" % i, [128, 4], I32) for i in range(2)]; bsl = [Buf(), Buf()]
        gk = [kb.sb("gkc%d" % i, [128, 4], F32) for i in range(2)]; bgk = [Buf(), Buf()]
        for i in range(8):
            kb.op("pool", lambda e, i=i: e.memset(yb[i][:], 0.0), writes=[byb[i]])
        if zero_after:
            self.zero_xg()
        u = 0
        for s in range(nseq):
            kb.dma("sp", gate2[:], S["MOD"][s, l, 5], reads=[self.db("MOD", (s, l, 5))], writes=[bg2])
            for t in range(self.ntl):
                ti = s * NT + t
                X = xin[u % 2]; bX = bxin[u % 2]; A = acc[u % 2]; bA = bacc[u % 2]
                SL = sl[u % 2]; bSL = bsl[u % 2]; GK = gk[u % 2]; bGK = bgk[u % 2]
                kb.dma("sp", X[:], src[ti * 128:(ti + 1) * 128, :], reads=[self.db("XA", ti)], writes=[bX])
                kb.dma("sp", SL[:], S["SLOT"][ti], reads=[self.db("SLOT", ti)], writes=[bSL])
                kb.dma("sp", GK[:], S["GK"][ti], reads=[self.db("GK", ti)], writes=[bGK])
                for k in range(4):
                    Yk = yb[(u % 2) * 4 + k]; bYk = byb[(u % 2) * 4 + k]
                    kb.idma(Yk[:], None, S["YG"], SL[:, k:k + 1], 32 * cap - 1, reads=[bSL], writes=[bYk])
                    if k == 0:
                        kb.op("dve", lambda e, Yk=Yk, A=A, GK=GK: e.tensor_scalar(out=A[:], in0=Yk[:], scalar1=GK[:, 0:1], scalar2=None,
                                                                                 op0=ALU.mult), reads=[bYk, bGK], writes=[bA])
                    else:
                        kb.op("dve", lambda e, Yk=Yk, A=A, GK=GK, k=k: e.scalar_tensor_tensor(
                            out=A[:], in0=Yk[:], scalar=GK[:, k:k + 1], in1=A[:], op0=ALU.mult, op1=ALU.add),
                            reads=[bYk, bGK, bA], writes=[bA])
                kb.op("dve", lambda e, A=A: e.tensor_tensor(out=A[:], in0=A[:], in1=gate2[:], op=ALU.mult), reads=[bA, bg2], writes=[bA])
                kb.op("dve", lambda e, A=A, X=X: e.tensor_tensor(out=X[:], in0=A[:], in1=X[:], op=ALU.add), reads=[bA, bX], writes=[bX])
                kb.dma("sp", dst[ti * 128:(ti + 1) * 128, :], X[:], reads=[bX], writes=[self.db(dst_name, ti)])
                u += 1
        kb.pop()

    def phase3(self):
        kb, I, S, nseq = self.kb, self.I, self.S, self.nseq
        kb.push()
        bw = Buf()
        w_qkv = kb.sb("w_qkv", [128, 8, 1280], BF16)
        w_out = kb.sb("w_out", [128, 8, D], BF16)
        self.load_w_bf16(w_qkv, I["swa_w_qkv"], 8, bw)
        self.load_w_bf16(w_out, I["swa_w_out"], 8, bw)
        bqkv = kb.sb("bqkv", [128, 1280], F32)
        bout = kb.sb("bout", [128, D], F32)
        gq = kb.sb("gq", [128, 64], F32)
        gk = kb.sb("gk", [128, 64], F32)
        sk = kb.sb("sk", [128, 16], F32)
        self.bcast_load(bqkv[:], I["swa_b_qkv"], bw)
        self.bcast_load(bout[:], I["swa_b_out"], bw)
        self.bcast_load(gq[:], I["swa_q_head_g"], bw)
        self.bcast_load(gk[:], I["swa_k_head_g"], bw)
        self.bcast_load(sk[:], I["swa_sinks"], bw)
        kb.op("act", lambda e: e.activation(out=sk[:], in_=sk[:], func=AF.Exp), reads=[bw], writes=[bw])
        mask2 = kb.sb("mask2", [128, 4, 2, 128], BF16)
        for hh in range(4):
            kb.op("dve", lambda e, hh=hh: e.tensor_copy(out=mask2[:, hh, 0, :], in_=self.mask_gt[:]), reads=[self.b_const], writes=[bw])
            kb.op("dve", lambda e, hh=hh: e.tensor_copy(out=mask2[:, hh, 1, :], in_=self.mask_le[:]), reads=[self.b_const], writes=[bw])
        r = self.alloc_router(1)

        mods = {n: kb.sb(n, [128, D], F32) for n in ("gmod1", "shift1", "gate1", "gmod2", "shift2")}
        bmod = Buf()
        x_t = [kb.sb("x_t%d" % i, [128, D], F32) for i in range(2)]; bx = [Buf(), Buf()]
        tmp = kb.sb("tmp", [128, D], F32); btmp = Buf()
        h_bf = kb.sb("h_bf", [128, D], BF16); bh = Buf()
        hT = kb.sb("hT", [128, 8, 128], BF16); bhT = Buf()
        ss = kb.sb("ss", [128, 4], F32); bss = Buf()
        qkv = kb.sb("qkv", [128, 20, 64], F32); bqkvs = Buf()
        sq = kb.sb("sq", [128, 18, 64], F32); bsq = Buf()
        r18 = kb.sb("r18", [128, 18], F32); br18 = Buf()
        qn = kb.sb("qn", [128, 18, 64], F32); bqn = Buf()
        rt = [kb.sb("rt%d" % i, [128, 18, 32], F32) for i in range(4)]; brt = Buf()
        q_bf = kb.sb("q_bf", [128, 16, 64], BF16); bqb = Buf()
        kdup = kb.sb("kdup", [128, 2, 2, 64], BF16); bkd = Buf()
        qT_2 = [kb.sb("qT%d" % i, [128, 8, 128], BF16) for i in range(2)]; bqT_2 = [Buf(), Buf()]
        tmpB = kb.sb("tmpB", [128, D], F32); btmpB = Buf()
        kT = [kb.sb("kT%d" % i, [128, 2, 128], BF16) for i in range(3)]; bkT = [Buf() for _ in range(3)]
        Va = [kb.sb("Va%d" % i, [128, 2, 65], BF16) for i in range(3)]; bVa = [Buf() for _ in range(3)]
        bVones = Buf()
        for i in range(3):
            kb.op("pool", lambda e, i=i: e.memset(Va[i][:, :, 64:65], 1.0), writes=[bVones])
        PT = [kb.sb("PT%d" % i, [128, 4, 2, 128], BF16) for i in range(2)]; bPT = [Buf(), Buf()]
        den = kb.sb("den", [128, 16], F32); bden = Buf()
        attn = kb.sb("attn", [128, 16, 64], BF16); battn = Buf()
        attT = kb.sb("attT", [128, 8, 128], BF16); battT = Buf()
        x1 = kb.sb("x1", [128, D], F32); bx1 = Buf()

        tp = [kb.ps("tp%d" % i, [128, 8, 128], BF16) for i in range(2)]; btp = [Buf(), Buf()]
        mm = [kb.ps("mm%d" % i, [128, 512], F32) for i in range(2)]; bmm = [Buf(), Buf()]
        s2 = kb.ps("s2", [128, 2, 512], F32); bs2 = [Buf(), Buf()]
        oo = [kb.ps("oo%d" % i, [128, 512], F32) for i in range(2)]; boo = [Buf(), Buf()]
        tpB = [s2[:, i, :].bitcast(BF16).rearrange("p (k m) -> p k m", m=128) for i in range(2)]
        gc = {"n": 0}
        for s in range(nseq):
            cos, sin, brope = self.rope_tables(s, 32, "k_invf32", "c%d" % s)
            for n_, part in (("gmod1", 1), ("shift1", 0), ("gate1", 2), ("gmod2", 4), ("shift2", 3)):
                kb.dma("sp", mods[n_][:], S["MOD"][s, 1, part], reads=[self.db("MOD", (s, 1, part))], writes=[bmod])
            def stage_a(t, s=s, cos=cos, sin=sin, brope=brope):
                ti = s * NT + t
                cur, prv = t % 3, (t - 1) % 3
                X = x_t[t % 2]; bX = bx[t % 2]
                qT = qT_2[t % 2]; bqT = bqT_2[t % 2]
                kb.dma("sp", X[:], S["XB"][ti * 128:(ti + 1) * 128, :], reads=[self.db("XB", ti)], writes=[bX])
                self.norm_mod_T(X, bX, mods["gmod1"], mods["shift1"], bmod, tmp, btmp, h_bf, bh, tp[0], btp[0], hT, bhT, ss, bss)
                qkvf = qkv[:].rearrange("p h d -> p (h d)")
                for gi, (c0, c1) in enumerate(((0, 512), (512, 1024), (1024, 1280))):
                    p_ = mm[gi % 2]; bp_ = bmm[gi % 2]
                    for k in range(8):
                        kb.op("pe", lambda e, k=k, c0=c0, c1=c1, p_=p_: e.matmul(p_[:, 0:c1 - c0], lhsT=hT[:, k, :], rhs=w_qkv[:, k, c0:c1],
                                                                              start=(k == 0), stop=(k == 7)), reads=[bhT, bw], writes=[bp_])
                    kb.op("dve", lambda e, c0=c0, c1=c1, p_=p_: e.tensor_tensor(out=qkvf[:, c0:c1], in0=p_[:, 0:c1 - c0], in1=bqkv[:, c0:c1],
                                                                              op=ALU.add), reads=[bp_, bw], writes=[bqkvs])
                kb.op("dve", lambda e: e.tensor_tensor(out=sq[:], in0=qkv[:, 0:18, :], in1=qkv[:, 0:18, :], op=ALU.mult), reads=[bqkvs], writes=[bsq])
                kb.op("dve", lambda e: e.tensor_reduce(out=r18[:], in_=sq[:], axis=AX.X, op=ALU.add), reads=[bsq], writes=[br18])
                kb.op("dve", lambda e: e.tensor_scalar(out=r18[:], in0=r18[:], scalar1=1.0 / 64, scalar2=EPS, op0=ALU.mult, op1=ALU.add),
                      reads=[br18], writes=[br18])
                kb.op("pool", lambda e: e.tensor_tensor(out=r18[:, 0:16], in0=r18[:, 0:16], in1=self.neghalf[:, 0:16], op=ALU.pow),
                      reads=[br18, self.b_const], writes=[br18])
                kb.op("pool", lambda e: e.tensor_tensor(out=r18[:, 16:18], in0=r18[:, 16:18], in1=self.neghalf[:, 0:2], op=ALU.pow),
                      reads=[br18, self.b_const], writes=[br18])
                kb.op("dve", lambda e: e.tensor_tensor(out=qn[:], in0=qkv[:, 0:18, :], in1=bc_last(r18[:, :], 64), op=ALU.mult),
                      reads=[bqkvs, br18], writes=[bqn])
                kb.op("dve", lambda e: e.tensor_tensor(out=qn[:, 0:16, :], in0=qn[:, 0:16, :], in1=bc_mid(gq[:, :], 16), op=ALU.mult),
                      reads=[bqn, bw], writes=[bqn])
                kb.op("dve", lambda e: e.tensor_tensor(out=qn[:, 16:18, :], in0=qn[:, 16:18, :], in1=bc_mid(gk[:, :], 2), op=ALU.mult),
                      reads=[bqn, bw], writes=[bqn])
                cb = bc_mid(cos[:, t, :], 18)
                sb_ = bc_mid(sin[:, t, :], 18)
                x1_ = qn[:, :, 0:32]; x2_ = qn[:, :, 32:64]
                kb.op("dve", lambda e: e.tensor_tensor(out=rt[0][:], in0=x1_, in1=cb, op=ALU.mult), reads=[bqn, brope], writes=[brt])
                kb.op("dve", lambda e: e.tensor_tensor(out=rt[1][:], in0=x2_, in1=sb_, op=ALU.mult), reads=[bqn, brope], writes=[brt])
                kb.op("dve", lambda e: e.tensor_tensor(out=rt[2][:], in0=x2_, in1=cb, op=ALU.mult), reads=[bqn, brope], writes=[brt])
                kb.op("dve", lambda e: e.tensor_tensor(out=rt[3][:], in0=x1_, in1=sb_, op=ALU.mult), reads=[bqn, brope], writes=[brt])
                kb.op("dve", lambda e: e.tensor_tensor(out=q_bf[:, :, 0:32], in0=rt[0][:, 0:16, :], in1=rt[1][:, 0:16, :], op=ALU.subtract),
                      reads=[brt], writes=[bqb])
                kb.op("dve", lambda e: e.tensor_tensor(out=q_bf[:, :, 32:64], in0=rt[2][:, 0:16, :], in1=rt[3][:, 0:16, :], op=ALU.add),
                      reads=[brt], writes=[bqb])
                for dup in range(2):
                    kb.op("dve", lambda e, dup=dup: e.tensor_tensor(out=kdup[:, :, dup, 0:32], in0=rt[0][:, 16:18, :], in1=rt[1][:, 16:18, :],
                                                                   op=ALU.subtract), reads=[brt], writes=[bkd])
                    kb.op("dve", lambda e, dup=dup: e.tensor_tensor(out=kdup[:, :, dup, 32:64], in0=rt[2][:, 16:18, :], in1=rt[3][:, 16:18, :],
                                                                    op=ALU.add), reads=[brt], writes=[bkd])
                kb.op("act", lambda e, cur=cur: e.copy(out=Va[cur][:, :, 0:64], in_=qkv[:, 18:20, :]), reads=[bqkvs, bVones], writes=[bVa[cur]])
                for i in range(8):
                    kb.op("pe", lambda e, i=i: e.transpose(out=tp[1][:, i, :], in_=q_bf[:, 2 * i:2 * i + 2, :].rearrange("p h d -> p (h d)"),
                                                           identity=self.ident_b[:]), reads=[bqb, self.b_const], writes=[btp[1]])
                kb.op("act", lambda e: e.copy(out=qT[:], in_=tp[1][:]), reads=[btp[1]], writes=[bqT])
                for g in range(2):
                    kb.op("pe", lambda e, g=g: e.transpose(out=tp[0][:, g, :], in_=kdup[:, g, :, :].rearrange("p a d -> p (a d)"),
                                                           identity=self.ident_b[:]), reads=[bkd, self.b_const], writes=[btp[0]])
                kb.op("dve", lambda e, cur=cur: e.tensor_copy(out=kT[cur][:], in_=tp[0][:, 0:2, :]), reads=[btp[0]], writes=[bkT[cur]])
            def stage_b(t, s=s):
                ti = s * NT + t
                cur, prv = t % 3, (t - 1) % 3
                X = x_t[t % 2]; bX = bx[t % 2]
                qT = qT_2[t % 2]; bqT = bqT_2[t % 2]
                tmp = tmpB; btmp = btmpB
                for gq4 in range(4):
                    sbank = s2
                    bsb = bs2
                    P = PT[gc["n"] % 2]; bP = bPT[gc["n"] % 2]
                    ob = oo[gc["n"] % 2]; bob = boo[gc["n"] % 2]
                    gc["n"] += 1
                    for hh in range(4):
                        hq = gq4 * 4 + hh
                        i, o = hq // 2, (hq % 2) * 64
                        g = hq // 8
                        for w_, kt in ((0, prv), (1, cur)):
                            if t == 0 and w_ == 0:
                                continue
                            col = ((hh % 2) * 2 + hh // 2) * 256 + w_ * 128
                            kb.op("pe", lambda e, i=i, o=o, g=g, kt=kt, col=col, sbank=sbank: e.matmul(
                                sbank[:, col // 512, col % 512:col % 512 + 128], lhsT=kT[kt][o:o + 64, g, :], rhs=qT[o:o + 64, i, :],
                                start=True, stop=True), reads=[bkT[kt], bqT], writes=[bsb[col // 512]])
                    for bk in range(2):
                        if t == 0:
                            for hh2 in range(2):
                                kb.op("act", lambda e, bk=bk, hh2=hh2, P=P, sbank=sbank: e.activation(
                                    out=P[:, bk * 2 + hh2, 1, :], in_=sbank[:, bk, hh2 * 256 + 128:hh2 * 256 + 256], func=AF.Exp, scale=0.125),
                                    reads=[bsb[bk]], writes=[bP])
                        else:
                            kb.op("act", lambda e, bk=bk, P=P, sbank=sbank: e.activation(
                                out=P[:, bk * 2:bk * 2 + 2, :, :].rearrange("p a b c -> p (a b c)"), in_=sbank[:, bk, :], func=AF.Exp, scale=0.125),
                                reads=[bsb[bk]], writes=[bP])
                    if t == 0:
                        kb.op("dve", lambda e, P=P: e.tensor_tensor(out=P[:, :, 1, :], in0=P[:, :, 1, :], in1=mask2[:, :, 1, :], op=ALU.mult),
                              reads=[bP, bw], writes=[bP])
                    else:
                        kb.op("dve", lambda e, P=P: e.tensor_tensor(out=P[:].rearrange("p a b c -> p (a b c)"), in0=P[:].rearrange("p a b c -> p (a b c)"),
                                                                   in1=mask2[:].rearrange("p a b c -> p (a b c)"), op=ALU.mult),
                              reads=[bP, bw], writes=[bP])
                    for hh in range(4):
                        hq = gq4 * 4 + hh
                        g = hq // 8
                        sl = (hh % 2) * 2 + hh // 2
                        if t > 0:
                            kb.op("pe", lambda e, hh=hh, g=g, P=P, ob=ob, prv=prv, sl=sl: e.matmul(ob[:, hh * 65:hh * 65 + 65], lhsT=P[:, sl, 0, :],
                                                                                          rhs=Va[prv][:, g, :], start=True, stop=False),
                                  reads=[bP, bVa[prv]], writes=[bob])
                        kb.op("pe", lambda e, hh=hh, g=g, P=P, ob=ob, cur=cur, sl=sl: e.matmul(ob[:, hh * 65:hh * 65 + 65], lhsT=P[:, sl, 1, :],
                                                                                      rhs=Va[cur][:, g, :], start=(t == 0), stop=True),
                              reads=[bP, bVa[cur]], writes=[bob])
                    ov = ob[:, 0:260].rearrange("p (h d) -> p h d", d=65)
                    dsl = den[:, gq4 * 4:(gq4 + 1) * 4]
                    kb.op("dve", lambda e, ov=ov, dsl=dsl, gq4=gq4: e.tensor_tensor(out=dsl, in0=ov[:, :, 64], in1=sk[:, gq4 * 4:(gq4 + 1) * 4], op=ALU.add),
                          reads=[bob, bw], writes=[bden])
                    kb.op("dve", lambda e, dsl=dsl: e.reciprocal(out=dsl, in_=dsl), reads=[bden], writes=[bden])
                    kb.op("dve", lambda e, ov=ov, dsl=dsl, gq4=gq4: e.tensor_tensor(out=attn[:, gq4 * 4:(gq4 + 1) * 4, :], in0=ov[:, :, 0:64],
                                                                                 in1=bc_last(dsl, 64), op=ALU.mult), reads=[bob, bden], writes=[battn])
                af = attn[:].rearrange("p h d -> p (h d)")
                for k in range(8):
                    kb.op("pe", lambda e, k=k: e.transpose(out=tpB[0][:, k, :], in_=af[:, k * 128:(k + 1) * 128], identity=self.ident_b[:]),
                          reads=[battn, self.b_const], writes=[bs2[0]])
                kb.op("act", lambda e: e.copy(out=attT[:], in_=tpB[0]), reads=[bs2[0]], writes=[battT])
                for half in range(2):
                    hs = slice(half * 512, (half + 1) * 512)
                    for k in range(8):
                        kb.op("pe", lambda e, k=k, half=half, hs=hs: e.matmul(oo[half][:], lhsT=attT[:, k, :], rhs=w_out[:, k, hs],
                                                                            start=(k == 0), stop=(k == 7)), reads=[battT, bw], writes=[boo[half]])
                    kb.op("dve", lambda e, half=half, hs=hs: e.tensor_tensor(out=tmp[:, hs], in0=oo[half][:], in1=bout[:, hs], op=ALU.add),
                          reads=[boo[half], bw], writes=[btmp])
                    kb.op("dve", lambda e, hs=hs: e.tensor_tensor(out=tmp[:, hs], in0=tmp[:, hs], in1=mods["gate1"][:, hs], op=ALU.mult),
                          reads=[btmp, bmod], writes=[btmp])
                    kb.op("dve", lambda e, hs=hs: e.tensor_tensor(out=x1[:, hs], in0=tmp[:, hs], in1=X[:, hs], op=ALU.add),
                          reads=[btmp, bX], writes=[bx1])
                kb.dma("sp", S["XA"][ti * 128:(ti + 1) * 128, :], x1[:], reads=[bx1], writes=[self.db("XA", ti)])
                self.norm2_router(r, x1, bx1, mods["gmod2"], mods["shift2"], bmod, tmp, btmp, tpB, bs2, oo[0], boo[0], ti)
            kb.pipeline(stage_a, stage_b, self.ntl)
        kb.pop()

    def build(self):
        self.setup_consts()
        ph = self.phases
        if "p0" in ph:
            self.phase0()
        if "p1a" in ph:
            self.phase1a()
        if "p1b" in ph:
            self.phase1b()
        if "p2a" in ph:
            self.phase2e(0)
            self.phase2c(0, self.S["XA"], self.S["XB"], "XB", True)
        if "p3" in ph:
            self.phase3()
        if "p2b" in ph:
            self.phase2e(1)
            self.phase2c(1, self.S["XA"], self.out, "OUT", False)
        self.kb.finish()
        return self.nc


def module_consts():
    idx = np.arange(128, dtype=np.float64)
    lg = np.log1p(-np.exp2(-5.0 - np.arange(8, dtype=np.float64)))
    k = {}
    k["k_invf16"] = (10000.0 ** (-np.arange(16, dtype=np.float32) / 16)).astype(np.float32)
    k["k_invf32"] = (10000.0 ** (-np.arange(32, dtype=np.float32) / 32)).astype(np.float32)
    diff = idx[None, :] - idx[:, None]
    dec = np.where(diff[:, None, :] >= 0, np.exp(lg[None, :, None] * np.maximum(diff[:, None, :], 0.0)), 0.0)
    k["k_decayT"] = np.ascontiguousarray(dec.reshape(128, 4, 2, 128).transpose(0, 2, 1, 3)).reshape(128, 8 * 128).astype(np.float32)
    k["k_qdec"] = np.exp(lg[None, :] * (idx + 1.0)[:, None]).astype(np.float32)
    k["k_kdec"] = np.exp(lg[None, :] * (127.0 - idx)[:, None]).astype(np.float32)
    cd = np.zeros((128, 4), np.float64)
    for i in range(4):
        cd[0:64, i] = np.exp(lg[2 * i] * 128)
        cd[64:128, i] = np.exp(lg[2 * i + 1] * 128)
    k["k_cdec"] = cd.astype(np.float32)
    return k


def make_in_maps(inputs, nseq, n_cores):
    f = lambda a: np.ascontiguousarray(np.asarray(a))
    shared = {}
    for name in ("ada_w", "ada_b", "norm1_g", "norm2_g", "router_w", "router_b", "exp_w_gu", "exp_w_down", "exp_b_down"):
        shared[name] = f(inputs[name])
    for name in ("hyb_w_in", "mla_cq_norm_g", "mla_ckv_norm_g", "mla_w_uq", "mla_w_ukv", "mla_q_head_g", "mla_k_head_g",
                 "hyb_w_out", "swa_w_qkv", "swa_b_qkv", "swa_q_head_g", "swa_k_head_g", "swa_sinks", "swa_w_out", "swa_b_out"):
        shared[name] = f(np.asarray(inputs[name])[0])
    shared["ret_norm_g"] = f(np.asarray(inputs["ret_norm_g"])[0].reshape(512))
    bgu = np.asarray(inputs["exp_b_gu"])
    shared["exp_b_gu_pj"] = f(bgu.reshape(2, 32, 16, 128).transpose(0, 1, 3, 2))
    shared.update(module_consts())
    x = np.asarray(inputs["x"]); c = np.asarray(inputs["c"]); pos = np.asarray(inputs["positions"])
    maps = []
    for i in range(n_cores):
        b0 = i * nseq
        m = dict(shared)
        m["x"] = f(x[b0:b0 + nseq].reshape(nseq * SEQ, D))
        m["c_pk"] = f(c[b0:b0 + nseq].reshape(nseq, 8, 128).transpose(0, 2, 1))
        m["pos_pt"] = f(pos[b0:b0 + nseq].reshape(nseq, NT, 128).transpose(0, 2, 1).astype(np.int32))
        maps.append(m)
    return maps


_PROG = {}


def kernel(**inputs):
    nseq = 32 // N_CORES
    if "nc" not in _PROG:
        _PROG["nc"] = Prog(nseq).build()
    maps = make_in_maps(inputs, nseq, N_CORES)
    res = run_bass_kernel_spmd(_PROG["nc"], maps, core_ids=list(range(N_CORES)))
    out = np.concatenate([np.asarray(r["out"]).reshape(nseq, SEQ, D) for r in res.results], axis=0)
    return out.astype(np.float32)
```

```python
import contextlib
import os
import math
import numpy as np
import concourse.bass as bass
import concourse.mybir as mybir
from concourse.bass_utils import run_bass_kernel_spmd

F32 = mybir.dt.float32
BF16 = mybir.dt.bfloat16
I32 = mybir.dt.int32
AF = mybir.ActivationFunctionType
ALU = mybir.AluOpType
AX = mybir.AxisListType

SAME_ENGINE_SYNC = os.environ.get('KSES', '1') == '1'
DMA_RING = 12
N_CORES = 8
_STOP = float(os.environ.get('KSTOP', '99'))
SEQ = 2048
D = 1024
NT = SEQ // 128
EPS = 1e-6
PI = math.pi


class Buf:
    __slots__ = ("w", "rs")

    def __init__(self):
        self.w = None
        self.rs = {}


class KB:
    ENGS = ("pe", "act", "dve", "pool", "sp")

    def __init__(self, nc):
        self.nc = nc
        self.stacks = [contextlib.ExitStack()]
        self.eng = dict(pe=nc.tensor, act=nc.scalar, dve=nc.vector, pool=nc.gpsimd, sp=nc.sync)
        self.cnt = {e: 0 for e in self.ENGS}
        self.seen = {e: {} for e in self.ENGS}
        self.sems = {}
        for e in self.ENGS:
            self.sems[e] = self.stacks[0].enter_context(nc.semaphore("s_" + e))
        self.dma_n = {}
        for e in ("sp", "pool", "act"):
            self.dma_n[e] = 0
            for j in range(DMA_RING):
                self.sems[("d", e, j)] = self.stacks[0].enter_context(nc.semaphore("d_%s_%d" % (e, j)))
        self.uid = 0

    def push(self):
        self.phase_id = getattr(self, "phase_id", 0) + 1
        self.stacks.append(contextlib.ExitStack())

    def pop(self):
        self.barrier()
        self.stacks.pop().close()

    def sb(self, name, shape, dt):
        self.uid += 1
        return self.stacks[-1].enter_context(self.nc.sbuf_tensor("%s_%d" % (name, self.uid), list(shape), dt))

    def ps(self, name, shape, dt):
        self.uid += 1
        return self.stacks[-1].enter_context(self.nc.psum_tensor("%s_%d" % (name, self.uid), list(shape), dt))

    def _deps(self, e, reads, writes):
        toks = {}
        for b in reads:
            if b.w is not None and toks.get(b.w[0], 0) < b.w[1]:
                toks[b.w[0]] = b.w[1]
        for b in writes:
            if b.w is not None and toks.get(b.w[0], 0) < b.w[1]:
                toks[b.w[0]] = b.w[1]
            for k, v in b.rs.items():
                if toks.get(k, 0) < v:
                    toks[k] = v
        waits = []
        seen = self.seen[e]
        for k, v in toks.items():
            if k == e and (e == "pe" or not SAME_ENGINE_SYNC):
                continue
            if seen.get(k, 0) < v:
                seen[k] = v
                waits.append((k, v))
        return waits

    def _mark(self, tok, reads, writes):
        k, v = tok
        for b in reads:
            if b.rs.get(k, 0) < v:
                b.rs[k] = v
        for b in writes:
            b.w = tok
            b.rs = {}

    def _emit(self, e, waits, fn, key, inc):
        engine = self.eng[e]
        for k, v in waits:
            engine.wait_ge(self.sems[k], v)
        if fn is not None:
            fn(engine).then_inc(self.sems[key], inc)

    def op(self, e, fn, reads=(), writes=()):
        waits = self._deps(e, reads, writes)
        self.cnt[e] += 1
        tok = (e, self.cnt[e])
        self._emit(e, waits, fn, e, 1)
        self._mark(tok, reads, writes)
        self._handoff()
        return tok

    def dma(self, e, out, in_, reads=(), writes=(), **kw):
        waits = self._deps(e, reads, writes)
        n = self.dma_n[e]
        self.dma_n[e] += 1
        key = ("d", e, n % DMA_RING)
        val = 16 * (n // DMA_RING + 1)
        if n >= DMA_RING and self.seen[e].get(key, 0) < val - 16:
            self.seen[e][key] = val - 16
            waits.append((key, val - 16))
        tok = (key, val)
        self._emit(e, waits, (lambda eng: eng.dma_start(out=out, in_=in_, **kw)), key, 16)
        self._mark(tok, reads, writes)
        self._handoff()
        return tok

    def idma(self, out, out_idx, in_, in_idx, bound, reads=(), writes=()):
        e = "pool"
        waits = self._deps(e, reads, writes)
        n = self.dma_n[e]
        self.dma_n[e] += 1
        key = ("d", e, n % DMA_RING)
        val = 16 * (n // DMA_RING + 1)
        if n >= DMA_RING and self.seen[e].get(key, 0) < val - 16:
            self.seen[e][key] = val - 16
            waits.append((key, val - 16))
        if not hasattr(self, "_bregs"):
            self._bregs = {}
        if bound not in self._bregs:
            self._bregs[bound] = self.nc.gpsimd.to_reg(bound)
        bound = self._bregs[bound]
        oo_ = bass.IndirectOffsetOnAxis(ap=out_idx, axis=0) if out_idx is not None else None
        io_ = bass.IndirectOffsetOnAxis(ap=in_idx, axis=0) if in_idx is not None else None
        self._emit(e, waits, (lambda eng: eng.indirect_dma_start(out=out, out_offset=oo_, in_=in_, in_offset=io_,
                                                                 bounds_check=bound, oob_is_err=False)), key, 16)
        self._mark((key, val), reads, writes)
        self._handoff()

    def _handoff(self):
        st = getattr(self, "_il", None)
        if st is None:
            return
        me = getattr(st["tls"], "idx", None)
        if me is None:
            return
        cv = st["cv"]
        with cv:
            if st["alive"][1 - me]:
                st["turn"] = 1 - me
                cv.notify_all()
                while st["turn"] != me:
                    cv.wait()

    def interleave(self, fa, fb):
        import threading
        if fa is None or fb is None:
            (fa or fb)()
            return
        st = dict(cv=threading.Condition(), turn=0, alive=[True, True], tls=threading.local(), err=[])
        self._il = st

        def runner(i, f):
            cv = st["cv"]
            with cv:
                while st["turn"] != i:
                    cv.wait()
            st["tls"].idx = i
            try:
                f()
            except BaseException as ex:
                st["err"].append(ex)
            finally:
                with cv:
                    st["alive"][i] = False
                    st["turn"] = 1 - i
                    cv.notify_all()

        ths = [threading.Thread(target=runner, args=(i, f)) for i, f in enumerate((fa, fb))]
        for th in ths:
            th.start()
        for th in ths:
            th.join()
        self._il = None
        if st["err"]:
            raise st["err"][0]

    def pipeline(self, stage_a, stage_b, n):
        stage_a(0)
        for t in range(n):
            self.interleave((lambda t=t: stage_a(t + 1)) if t + 1 < n else None, lambda t=t: stage_b(t))

    def all_tokens(self):
        toks = [(e, self.cnt[e]) for e in self.ENGS if self.cnt[e] > 0]
        for e in ("sp", "pool", "act"):
            n = self.dma_n[e]
            for j in range(DMA_RING):
                c = (n - j + DMA_RING - 1) // DMA_RING if n > j else 0
                if c > 0:
                    toks.append((("d", e, j), 16 * c))
        return toks

    def barrier(self):
        toks = self.all_tokens()
        for e in self.ENGS:
            waits = []
            for k, v in toks:
                if k == e:
                    continue
                if self.seen[e].get(k, 0) < v:
                    self.seen[e][k] = v
                    waits.append((k, v))
            self._emit(e, waits, None, None, 0)

    def finish(self):
        self.barrier()
        while self.stacks:
            self.stacks.pop().close()


def bc_mid(ap2, n):
    return ap2.unsqueeze(1).broadcast_to([ap2.shape[0], n, ap2.shape[1]])


def bc_last(ap2, n):
    return ap2.unsqueeze(2).broadcast_to([ap2.shape[0], ap2.shape[1], n])


class Prog:
    def __init__(self, nseq, debug=False, phases=("p0", "p1a", "p1b", "p2a", "p3", "p2b"), ntl=NT, cap_tiles=None):
        self.nseq = nseq
        self.ntl = ntl
        if cap_tiles is None:
            mean = nseq * ntl * 128 * 4 // 32
            cap_tiles = max(4, 4 * ((2 * mean + 511) // 512))
        self.cap = cap_tiles * 128
        self.debug = debug
        self.phases = phases
        nc = self.nc = bass.Bass("TRN2", target_bir_lowering=False)
        self.kb = KB(nc)
        ntok = nseq * SEQ
        self.ntok = ntok

        def inp(name, shape, dt=F32):
            return nc.dram_tensor(name, list(shape), dt, kind="ExternalInput").ap()

        def scr(name, shape, dt=F32):
            kind = "ExternalOutput" if debug else "Internal"
            return nc.dram_tensor(name, list(shape), dt, kind=kind).ap()

        I = self.I = {}
        I["x"] = inp("x", [ntok, D])
        I["c_pk"] = inp("c_pk", [nseq, 128, 8])
        I["pos_pt"] = inp("pos_pt", [nseq, 128, NT], I32)
        I["ada_w"] = inp("ada_w", [2, D, 6 * D])
        I["ada_b"] = inp("ada_b", [2, 6 * D])
        I["norm1_g"] = inp("norm1_g", [2, D])
        I["norm2_g"] = inp("norm2_g", [2, D])
        I["hyb_w_in"] = inp("hyb_w_in", [D, 2720])
        I["mla_cq_norm_g"] = inp("mla_cq_norm_g", [384])
        I["mla_ckv_norm_g"] = inp("mla_ckv_norm_g", [256])
        I["mla_w_uq"] = inp("mla_w_uq", [384, 768])
        I["mla_w_ukv"] = inp("mla_w_ukv", [256, 1024])
        I["mla_q_head_g"] = inp("mla_q_head_g", [96])
        I["mla_k_head_g"] = inp("mla_k_head_g", [96])
        I["ret_norm_g"] = inp("ret_norm_g", [512])
        I["hyb_w_out"] = inp("hyb_w_out", [D, D])
        I["swa_w_qkv"] = inp("swa_w_qkv", [D, 1280])
        I["swa_b_qkv"] = inp("swa_b_qkv", [1280])
        I["swa_q_head_g"] = inp("swa_q_head_g", [64])
        I["swa_k_head_g"] = inp("swa_k_head_g", [64])
        I["swa_sinks"] = inp("swa_sinks", [16])
        I["swa_w_out"] = inp("swa_w_out", [D, D])
        I["swa_b_out"] = inp("swa_b_out", [D])
        I["router_w"] = inp("router_w", [2, D, 32])
        I["router_b"] = inp("router_b", [2, 32])
        I["exp_w_gu"] = inp("exp_w_gu", [2, 32, D, 2048])
        I["exp_b_gu_pj"] = inp("exp_b_gu_pj", [2, 32, 128, 16])
        I["exp_w_down"] = inp("exp_w_down", [2, 32, D, D])
        I["exp_b_down"] = inp("exp_b_down", [2, 32, D])
        I["k_invf16"] = inp("k_invf16", [16])
        I["k_invf32"] = inp("k_invf32", [32])
        I["k_decayT"] = inp("k_decayT", [128, 8 * 128])
        I["k_qdec"] = inp("k_qdec", [128, 8])
        I["k_kdec"] = inp("k_kdec", [128, 8])
        I["k_cdec"] = inp("k_cdec", [128, 4])

        S = self.S = {}
        S["MOD"] = scr("MOD", [nseq, 2, 6, D])
        S["ATT"] = scr("ATT", [nseq * NT, 128, 512], BF16)
        S["XA"] = scr("XA", [ntok, D])
        S["XB"] = scr("XB", [ntok, D])
        S["H2T"] = scr("H2T", [nseq * NT, 128, 8, 128], BF16)
        S["GS"] = scr("GS", [nseq * NT, 128, 32])
        S["XG"] = scr("XG", [32 * self.cap, D], BF16)
        S["YG"] = scr("YG", [32 * self.cap, D])
        S["SLOT"] = scr("SLOT", [nseq * NT, 128, 4], I32)
        S["GK"] = scr("GK", [nseq * NT, 128, 4])
        self.out = nc.dram_tensor("out", [ntok, D], F32, kind="ExternalOutput").ap()
        self.dbufs = {}

    def db(self, name, idx):
        k = (name, idx)
        if k not in self.dbufs:
            self.dbufs[k] = Buf()
        return self.dbufs[k]

    def setup_consts(self):
        kb = self.kb
        self.ident_b = kb.sb("ident_b", [128, 128], BF16)
        self.ident_f = kb.sb("ident_f", [128, 128], F32)
        self.b_const = Buf()
        bc = self.b_const
        for idt in (self.ident_b, self.ident_f):
            kb.op("pool", lambda e, idt=idt: e.memset(idt[:], 1.0), writes=[bc])
            kb.op("pool", lambda e, idt=idt: e.affine_select(out=idt[:], in_=idt[:], pattern=[[-1, 128]],
                                                             compare_op=ALU.is_equal, fill=0.0, base=0,
                                                             channel_multiplier=1), reads=[bc], writes=[bc])
        self.mask_le = kb.sb("mask_le", [128, 128], BF16)
        self.mask_gt = kb.sb("mask_gt", [128, 128], BF16)
        kb.op("pool", lambda e: e.memset(self.mask_le[:], 1.0), writes=[bc])
        kb.op("pool", lambda e: e.affine_select(out=self.mask_le[:], in_=self.mask_le[:], pattern=[[1, 128]],
                                                compare_op=ALU.is_ge, fill=0.0, base=0, channel_multiplier=-1),
              reads=[bc], writes=[bc])
        kb.op("pool", lambda e: e.memset(self.mask_gt[:], 1.0), writes=[bc])
        kb.op("pool", lambda e: e.affine_select(out=self.mask_gt[:], in_=self.mask_gt[:], pattern=[[-1, 128]],
                                                compare_op=ALU.is_gt, fill=0.0, base=0, channel_multiplier=1),
              reads=[bc], writes=[bc])
        self.U_b = kb.sb("U_b", [128, 128], BF16)
        self.ones_b = kb.sb("ones_b", [128, 128], BF16)
        kb.op("pool", lambda e: e.memset(self.ones_b[:], 1.0), writes=[bc])
        kb.op("pool", lambda e: e.memset(self.U_b[:], 1.0), writes=[bc])
        kb.op("pool", lambda e: e.affine_select(out=self.U_b[:], in_=self.U_b[:], pattern=[[1, 128]],
                                                compare_op=ALU.is_ge, fill=0.0, base=-1, channel_multiplier=-1),
              reads=[bc], writes=[bc])
        iot_i = kb.sb("iot_i", [128, 32], I32)
        self.iotaE = kb.sb("iotaE", [128, 32], F32)
        kb.op("pool", lambda e: e.iota(out=iot_i[:], pattern=[[1, 32]], base=0, channel_multiplier=0), writes=[bc])
        kb.op("dve", lambda e: e.tensor_copy(out=self.iotaE[:], in_=iot_i[:]), reads=[bc], writes=[bc])
        kb.op("dve", lambda e: e.tensor_scalar(out=self.iotaE[:], in0=self.iotaE[:], scalar1=float(self.cap), scalar2=None,
                                               op0=ALU.mult), reads=[bc], writes=[bc])
        self.neghalf = kb.sb("neghalf", [128, 16], F32)
        kb.op("pool", lambda e: e.memset(self.neghalf[:], -0.5), writes=[bc])

    def rstd_of(self, ss, n, width, bss, tag):
        kb = self.kb
        kb.op("dve", lambda e: e.tensor_scalar(out=ss, in0=ss, scalar1=1.0 / n, scalar2=EPS,
                                               op0=ALU.mult, op1=ALU.add), reads=[bss], writes=[bss])
        kb.op("pool", lambda e: e.tensor_tensor(out=ss, in0=ss, in1=self.neghalf[:, 0:width], op=ALU.pow),
              reads=[bss, self.b_const], writes=[bss])

    def rope_tables(self, s, half, invf_name, tag):
        kb, I = self.kb, self.I
        key = (kb.phase_id, half)
        if not hasattr(self, "_rope"):
            self._rope = {}
        if key not in self._rope:
            self._rope[key] = dict(
                b=Buf(),
                pos_i=kb.sb("pos_i", [128, NT], I32), pos_f=kb.sb("pos_f", [128, NT], F32), invf=kb.sb("invf", [128, half], F32),
                ang=kb.sb("ang", [128, NT, half], F32), kq=kb.sb("kq", [128, NT, half], F32), ki=kb.sb("ki", [128, NT, half], I32),
                ys=kb.sb("ys", [128, NT, half], F32), mm=kb.sb("mmk", [128, NT, half], F32),
                cos=kb.sb("cos", [128, NT, half], F32), sin=kb.sb("sin", [128, NT, half], F32))
        R_ = self._rope[key]
        b = R_["b"]
        pos_i, pos_f, invf, ang, kq, ki, ys, mm, cos, sin = (R_[n] for n in ("pos_i", "pos_f", "invf", "ang", "kq", "ki", "ys", "mm", "cos", "sin"))
        kb.dma("sp", pos_i[:], I["pos_pt"][s], writes=[b])
        kb.dma("sp", invf[:], I[invf_name].partition_broadcast(128), writes=[b])
        kb.op("dve", lambda e: e.tensor_copy(out=pos_f[:], in_=pos_i[:]), reads=[b], writes=[b])
        kb.op("dve", lambda e: e.tensor_tensor(out=ang[:], in0=bc_last(pos_f[:, :], half), in1=bc_mid(invf[:, :], NT),
                                               op=ALU.mult), reads=[b], writes=[b])
        kb.op("dve", lambda e: e.tensor_scalar(out=kq[:], in0=ang[:], scalar1=1.0 / (2 * PI), scalar2=None,
                                               op0=ALU.mult), reads=[b], writes=[b])
        kb.op("dve", lambda e: e.tensor_copy(out=ki[:], in_=kq[:]), reads=[b], writes=[b])
        kb.op("dve", lambda e: e.tensor_copy(out=kq[:], in_=ki[:]), reads=[b], writes=[b])
        kb.op("dve", lambda e: e.scalar_tensor_tensor(out=ang[:], in0=kq[:], scalar=-2 * PI, in1=ang[:],
                                                      op0=ALU.mult, op1=ALU.add), reads=[b], writes=[b])
        lim = 3.1415925
        for shift, dst in ((0.0, sin), (PI / 2, cos)):
            kb.op("dve", lambda e, shift=shift: e.tensor_scalar(out=ys[:], in0=ang[:], scalar1=shift, scalar2=None,
                                                                op0=ALU.add), reads=[b], writes=[b])
            kb.op("dve", lambda e: e.tensor_scalar(out=mm[:], in0=ys[:], scalar1=PI, scalar2=-2 * PI,
                                                   op0=ALU.is_gt, op1=ALU.mult), reads=[b], writes=[b])
            kb.op("dve", lambda e: e.tensor_tensor(out=ys[:], in0=ys[:], in1=mm[:], op=ALU.add), reads=[b], writes=[b])
            kb.op("dve", lambda e: e.tensor_scalar(out=ys[:], in0=ys[:], scalar1=lim, scalar2=-lim,
                                                   op0=ALU.min, op1=ALU.max), reads=[b], writes=[b])
            kb.op("act", lambda e, dst=dst: e.activation(out=dst[:], in_=ys[:], func=AF.Sin), reads=[b], writes=[b])
        return cos, sin, b

    def load_w_bf16(self, dst, src, kchunks, bw):
        v = src.rearrange("(k p) n -> p k n", p=128)
        for k in range(kchunks):
            self.kb.dma("pool", dst[:, k, :], v[:, k, :], writes=[bw])

    def bcast_load(self, dst, src1d, b):
        self.kb.dma("sp", dst, src1d.partition_broadcast(128), writes=[b])

    def norm_mod_T(self, x_t, bx, gmod, shift, bmod, tmp, btmp, h_bf, bh, tp, btp, hT, bhT, ss, bss):
        kb = self.kb
        kb.op("dve", lambda e: e.scalar_tensor_tensor(out=tmp[:], in0=x_t[:], scalar=1.0, in1=x_t[:], op0=ALU.mult, op1=ALU.mult, accum_out=ss[:, 0:1]),
              reads=[bx], writes=[btmp, bss])
        self.rstd_of(ss[:, 0:1], D, 1, bss, "")
        kb.op("dve", lambda e: e.scalar_tensor_tensor(out=tmp[:], in0=x_t[:], scalar=ss[:, 0:1], in1=gmod[:],
                                                      op0=ALU.mult, op1=ALU.mult), reads=[bx, bss, bmod], writes=[btmp])
        kb.op("dve", lambda e: e.tensor_tensor(out=h_bf[:], in0=tmp[:], in1=shift[:], op=ALU.add),
              reads=[btmp, bmod], writes=[bh])
        for k in range(8):
            kb.op("pe", lambda e, k=k: e.transpose(out=tp[:, k, :], in_=h_bf[:, k * 128:(k + 1) * 128],
                                                   identity=self.ident_b[:]), reads=[bh, self.b_const], writes=[btp])
        kb.op("act", lambda e: e.copy(out=hT[:], in_=tp[:]), reads=[btp], writes=[bhT])

    def phase0(self):
        kb, I, S, nseq = self.kb, self.I, self.S, self.nseq
        kb.push()
        cin = kb.sb("cin", [128, nseq, 8], F32)
        cact = kb.sb("cact", [128, 8, nseq], F32)
        bcr = Buf()
        for s in range(nseq):
            kb.dma("sp", cin[:, s, :], I["c_pk"][s], writes=[bcr])
        kb.op("act", lambda e: e.activation(out=cact[:].rearrange("p k s -> p s k"), in_=cin[:], func=AF.Silu), reads=[bcr], writes=[bcr])
        adab = kb.sb("adab", [nseq, 6 * D], F32)
        ng = [kb.sb("ng1", [nseq, D], F32), kb.sb("ng2", [nseq, D], F32)]
        bab = Buf()
        wch = [kb.sb("wch%d" % i, [128, 8, 512], F32) for i in range(3)]
        bwch = [Buf() for _ in range(3)]
        pm = [kb.ps("p0pm%d" % i, [128, 512], F32) for i in range(2)]
        bpm = [Buf(), Buf()]
        modt = [kb.sb("modt%d" % i, [nseq, 512], F32) for i in range(3)]
        bmodt = [Buf() for _ in range(3)]
        n = 0
        for l in range(2):
            kb.dma("sp", adab[:], I["ada_b"][l].partition_broadcast(nseq), writes=[bab])
            kb.dma("sp", ng[0][:], I["norm1_g"][l].partition_broadcast(nseq), writes=[bab])
            kb.dma("sp", ng[1][:], I["norm2_g"][l].partition_broadcast(nseq), writes=[bab])
            for j in range(12):
                wv = I["ada_w"][l][:, j * 512:(j + 1) * 512].rearrange("(k p) n -> p k n", p=128)
                w_ = wch[n % 3]
                kb.dma("sp", w_[:], wv, writes=[bwch[n % 3]])
                p_ = pm[n % 2]
                for k in range(8):
                    kb.op("pe", lambda e, k=k, p_=p_, w_=w_: e.matmul(p_[0:nseq, :], lhsT=cact[:, k, :], rhs=w_[:, k, :],
                                                                    start=(k == 0), stop=(k == 7)),
                          reads=[bcr, bwch[n % 3]], writes=[bpm[n % 2]])
                m_ = modt[n % 3]
                bm_ = bmodt[n % 3]
                kb.op("dve", lambda e, p_=p_, m_=m_, j=j: e.tensor_tensor(out=m_[:], in0=p_[0:nseq, :],
                                                                       in1=adab[:, j * 512:(j + 1) * 512], op=ALU.add),
                      reads=[bpm[n % 2], bab], writes=[bm_])
                part, half = j // 2, j % 2
                if part in (1, 4):
                    g_ = ng[0] if part == 1 else ng[1]
                    kb.op("dve", lambda e, m_=m_, g_=g_, half=half: e.scalar_tensor_tensor(
                        out=m_[:], in0=m_[:], scalar=1.0, in1=g_[:, half * 512:(half + 1) * 512],
                        op0=ALU.add, op1=ALU.mult), reads=[bm_, bab], writes=[bm_])
                for s in range(nseq):
                    kb.dma("sp", S["MOD"][s, l, part, half * 512:(half + 1) * 512], m_[s:s + 1, :],
                           reads=[bm_], writes=[self.db("MOD", (s, l, part))])
                n += 1
        kb.pop()

    def phase1a(self):
        kb, I, S, nseq = self.kb, self.I, self.S, self.nseq
        kb.push()
        self.zero_xg()
        bw = Buf()
        w_in = kb.sb("w_in_a", [128, 8, 672], BF16)
        w_uq = kb.sb("w_uq", [128, 3, 768], BF16)
        w_ukv = kb.sb("w_ukv", [128, 2, 1024], BF16)
        self.load_w_bf16(w_in, I["hyb_w_in"][:, 0:672], 8, bw)
        self.load_w_bf16(w_uq, I["mla_w_uq"], 3, bw)
        self.load_w_bf16(w_ukv, I["mla_w_ukv"], 2, bw)
        gcq = kb.sb("gcq", [128, 384], F32)
        gckv = kb.sb("gckv", [128, 256], F32)
        gq = kb.sb("gq", [128, 96], F32)
        gk = kb.sb("gk", [128, 96], F32)
        self.bcast_load(gcq[:], I["mla_cq_norm_g"], bw)
        self.bcast_load(gckv[:], I["mla_ckv_norm_g"], bw)
        self.bcast_load(gq[:], I["mla_q_head_g"], bw)
        self.bcast_load(gk[:], I["mla_k_head_g"], bw)

        kT = kb.sb("kT", [96, 8, SEQ], BF16)
        V = kb.sb("Vc", [128, NT, 8, 65], BF16)
        bkT = [Buf() for _ in range(NT)]
        bV = [Buf() for _ in range(NT)]
        bVones = Buf()
        kb.op("pool", lambda e: e.memset(V[:, :, :, 64:65], 1.0), writes=[bVones])

        gmod = kb.sb("gmod1", [128, D], F32)
        shift = kb.sb("shift1", [128, D], F32)
        bmod = Buf()
        x_t = [kb.sb("x_t%d" % i, [128, D], F32) for i in range(2)]
        bx = [Buf(), Buf()]
        tmp = kb.sb("tmp", [128, D], F32); btmp = Buf()
        h_bf = kb.sb("h_bf", [128, D], BF16); bh = Buf()
        hT = kb.sb("hT", [128, 8, 128], BF16); bhT = Buf()
        ss = kb.sb("ss", [128, 4], F32); bss = Buf()
        proj = kb.sb("proj", [128, 672], F32); bproj = Buf()
        sq = kb.sb("sq", [128, 8, 96], F32); bsq = Buf()
        cqn = kb.sb("cqn", [128, 384], BF16); bcqn = Buf()
        cqT = kb.sb("cqT", [128, 3, 128], BF16); bcqT = Buf()
        ckvn = kb.sb("ckvn", [128, 256], BF16); bckvn = Buf()
        ckvT = kb.sb("ckvT", [128, 2, 128], BF16); bckvT = Buf()
        q_sb = kb.sb("q_sb", [128, 8, 96], F32); bq = Buf()
        qn = kb.sb("qn", [128, 8, 96], F32); bqn = Buf()
        rq8 = kb.sb("rq8", [128, 16], F32); brq8 = Buf()
        R = kb.sb("Rr", [128, 8, 96], F32); bR = Buf()
        q_full = kb.sb("q_full", [128, 8, 96], BF16); bqf = Buf()
        qTs = [kb.sb("qT%d" % i, [96, 8, 128], BF16) for i in range(2)]; bqTs = [Buf(), Buf()]
        kv_sb = kb.sb("kv_sb", [128, 8, 128], F32); bkv = Buf()
        k_full = kb.sb("k_full", [128, 8, 96], BF16); bkf = Buf()
        kr = kb.sb("kr", [128, 32], F32); bkr = Buf()
        kr2 = kb.sb("kr2", [128, 32], F32)
        rt = [kb.sb("rt%d" % i, [128, 8, 16], F32) for i in range(4)]; brt = Buf()
        PT = [kb.sb("PT%d" % i, [128, 4, 128], BF16) for i in range(3)]; bPT = [Buf() for _ in range(3)]
        attn = kb.sb("attn", [128, 8, 64], BF16); battn = Buf()
        rden = kb.sb("rden", [128, 8], F32); brden = Buf()

        tp = [kb.ps("tp%d" % i, [128, 8, 128], BF16) for i in range(2)]; btp = [Buf(), Buf()]
        mm = [kb.ps("mm%d" % i, [128, 512], F32) for i in range(2)]; bmm = [Buf(), Buf()]
        s2 = kb.ps("s2", [128, 2, 512], F32); bs2 = [Buf(), Buf()]
        oo = [kb.ps("oo%d" % i, [128, 512], F32) for i in range(2)]; boo = [Buf(), Buf()]
        scale = 96.0 ** -0.5
        uc = {"n": 0}
        for s in range(nseq):
            cos, sin, brope = self.rope_tables(s, 16, "k_invf16", "a%d" % s)
            kb.dma("sp", gmod[:], S["MOD"][s, 0, 1].partition_broadcast(128), reads=[self.db("MOD", (s, 0, 1))], writes=[bmod])
            kb.dma("sp", shift[:], S["MOD"][s, 0, 0].partition_broadcast(128), reads=[self.db("MOD", (s, 0, 0))], writes=[bmod])
            def stage_a(t, s=s, cos=cos, sin=sin, brope=brope):
                X = x_t[t % 2]; bX = bx[t % 2]
                qT = qTs[t % 2]; bqT = bqTs[t % 2]
                kb.dma("sp", X[:], I["x"][(s * NT + t) * 128:(s * NT + t + 1) * 128, :], writes=[bX])
                self.norm_mod_T(X, bX, gmod, shift, bmod, tmp, btmp, h_bf, bh, tp[0], btp[0], hT, bhT, ss, bss)
                for gi, (c0, c1) in enumerate(((0, 512), (512, 672))):
                    for k in range(8):
                        kb.op("pe", lambda e, k=k, gi=gi, c0=c0, c1=c1: e.matmul(mm[gi][:, 0:c1 - c0], lhsT=hT[:, k, :],
                                                                              rhs=w_in[:, k, c0:c1], start=(k == 0), stop=(k == 7)),
                              reads=[bhT, bw], writes=[bmm[gi]])
                    kb.op("act", lambda e, gi=gi, c0=c0, c1=c1: e.copy(out=proj[:, c0:c1], in_=mm[gi][:, 0:c1 - c0]),
                          reads=[bmm[gi]], writes=[bproj])
                for ci, (c0, w) in enumerate(((0, 384), (384, 256), (640, 32))):
                    kb.op("dve", lambda e, c0=c0, w=w, ci=ci: e.scalar_tensor_tensor(out=tmp[:, 0:w], in0=proj[:, c0:c0 + w], scalar=1.0, in1=proj[:, c0:c0 + w], op0=ALU.mult, op1=ALU.mult, accum_out=ss[:, 1 + ci:2 + ci]), reads=[bproj], writes=[btmp, bss])
                kb.op("dve", lambda e: e.tensor_scalar(out=ss[:, 1:2], in0=ss[:, 1:2], scalar1=1.0 / 384, scalar2=EPS,
                                                       op0=ALU.mult, op1=ALU.add), reads=[bss], writes=[bss])
                kb.op("dve", lambda e: e.tensor_scalar(out=ss[:, 2:3], in0=ss[:, 2:3], scalar1=1.0 / 256, scalar2=EPS,
                                                       op0=ALU.mult, op1=ALU.add), reads=[bss], writes=[bss])
                kb.op("dve", lambda e: e.tensor_scalar(out=ss[:, 3:4], in0=ss[:, 3:4], scalar1=1.0 / 32, scalar2=EPS,
                                                       op0=ALU.mult, op1=ALU.add), reads=[bss], writes=[bss])
                kb.op("pool", lambda e: e.tensor_tensor(out=ss[:, 1:4], in0=ss[:, 1:4], in1=self.neghalf[:, 0:3], op=ALU.pow),
                      reads=[bss, self.b_const], writes=[bss])
                kb.op("dve", lambda e: e.scalar_tensor_tensor(out=cqn[:], in0=proj[:, 0:384], scalar=ss[:, 1:2], in1=gcq[:],
                                                              op0=ALU.mult, op1=ALU.mult), reads=[bproj, bss, bw], writes=[bcqn])
                kb.op("dve", lambda e: e.scalar_tensor_tensor(out=ckvn[:], in0=proj[:, 384:640], scalar=ss[:, 2:3], in1=gckv[:],
                                                              op0=ALU.mult, op1=ALU.mult), reads=[bproj, bss, bw], writes=[bckvn])
                kb.op("dve", lambda e: e.scalar_tensor_tensor(out=kr[:], in0=proj[:, 640:672], scalar=ss[:, 3:4], in1=gk[:, 64:96],
                                                              op0=ALU.mult, op1=ALU.mult), reads=[bproj, bss, bw], writes=[bkr])
                for k in range(3):
                    kb.op("pe", lambda e, k=k: e.transpose(out=tp[1][:, k, :], in_=cqn[:, k * 128:(k + 1) * 128],
                                                           identity=self.ident_b[:]), reads=[bcqn, self.b_const], writes=[btp[1]])
                for k in range(2):
                    kb.op("pe", lambda e, k=k: e.transpose(out=tp[1][:, 3 + k, :], in_=ckvn[:, k * 128:(k + 1) * 128],
                                                           identity=self.ident_b[:]), reads=[bckvn, self.b_const], writes=[btp[1]])
                kb.op("act", lambda e: e.copy(out=cqT[:], in_=tp[1][:, 0:3, :]), reads=[btp[1]], writes=[bcqT])
                kb.op("act", lambda e: e.copy(out=ckvT[:], in_=tp[1][:, 3:5, :]), reads=[btp[1]], writes=[bckvT])
                for gi, (c0, c1) in enumerate(((0, 512), (512, 768))):
                    for k in range(3):
                        kb.op("pe", lambda e, k=k, gi=gi, c0=c0, c1=c1: e.matmul(mm[gi][:, 0:c1 - c0], lhsT=cqT[:, k, :],
                                                                              rhs=w_uq[:, k, c0:c1], start=(k == 0), stop=(k == 2)),
                              reads=[bcqT, bw], writes=[bmm[gi]])
                    kb.op("act", lambda e, gi=gi, c0=c0, c1=c1: e.copy(
                        out=q_sb[:].rearrange("p h d -> p (h d)")[:, c0:c1], in_=mm[gi][:, 0:c1 - c0]),
                        reads=[bmm[gi]], writes=[bq])
                kb.op("dve", lambda e: e.tensor_tensor(out=sq[:], in0=q_sb[:], in1=q_sb[:], op=ALU.mult), reads=[bq], writes=[bsq])
                kb.op("dve", lambda e: e.tensor_reduce(out=rq8[:, 0:8], in_=sq[:, :, 0:64], axis=AX.X, op=ALU.add),
                      reads=[bsq], writes=[brq8])
                kb.op("dve", lambda e: e.tensor_reduce(out=rq8[:, 8:16], in_=sq[:, :, 64:96], axis=AX.X, op=ALU.add),
                      reads=[bsq], writes=[brq8])
                kb.op("dve", lambda e: e.tensor_scalar(out=rq8[:, 0:8], in0=rq8[:, 0:8], scalar1=1.0 / 64, scalar2=EPS,
                                                       op0=ALU.mult, op1=ALU.add), reads=[brq8], writes=[brq8])
                kb.op("dve", lambda e: e.tensor_scalar(out=rq8[:, 8:16], in0=rq8[:, 8:16], scalar1=1.0 / 32, scalar2=EPS,
                                                       op0=ALU.mult, op1=ALU.add), reads=[brq8], writes=[brq8])
                kb.op("pool", lambda e: e.tensor_tensor(out=rq8[:], in0=rq8[:], in1=self.neghalf[:, 0:16], op=ALU.pow),
                      reads=[brq8, self.b_const], writes=[brq8])
                kb.op("dve", lambda e: e.tensor_tensor(out=qn[:, :, 0:64], in0=q_sb[:, :, 0:64], in1=bc_last(rq8[:, 0:8], 64),
                                                       op=ALU.mult), reads=[bq, brq8], writes=[bqn])
                kb.op("dve", lambda e: e.tensor_tensor(out=qn[:, :, 64:96], in0=q_sb[:, :, 64:96], in1=bc_last(rq8[:, 8:16], 32),
                                                       op=ALU.mult), reads=[bq, brq8], writes=[bqn])
                kb.op("dve", lambda e: e.tensor_tensor(out=qn[:], in0=qn[:], in1=bc_mid(gq[:, :], 8), op=ALU.mult),
                      reads=[bqn, bw], writes=[bqn])
                kb.op("act", lambda e: e.copy(out=q_full[:, :, 0:64], in_=qn[:, :, 0:64]), reads=[bqn], writes=[bqf])
                cb = bc_mid(cos[:, t, :], 8)
                sb_ = bc_mid(sin[:, t, :], 8)
                x1 = qn[:, :, 64:80]
                x2 = qn[:, :, 80:96]
                kb.op("dve", lambda e: e.tensor_tensor(out=rt[0][:], in0=x1, in1=cb, op=ALU.mult), reads=[bqn, brope], writes=[brt])
                kb.op("dve", lambda e: e.tensor_tensor(out=rt[1][:], in0=x2, in1=sb_, op=ALU.mult), reads=[bqn, brope], writes=[brt])
                kb.op("dve", lambda e: e.tensor_tensor(out=rt[2][:], in0=x2, in1=cb, op=ALU.mult), reads=[bqn, brope], writes=[brt])
                kb.op("dve", lambda e: e.tensor_tensor(out=rt[3][:], in0=x1, in1=sb_, op=ALU.mult), reads=[bqn, brope], writes=[brt])
                kb.op("dve", lambda e: e.tensor_tensor(out=q_full[:, :, 64:80], in0=rt[0][:], in1=rt[1][:], op=ALU.subtract),
                      reads=[brt], writes=[bqf])
                kb.op("dve", lambda e: e.tensor_tensor(out=q_full[:, :, 80:96], in0=rt[2][:], in1=rt[3][:], op=ALU.add),
                      reads=[brt], writes=[bqf])
                for h in range(8):
                    kb.op("pe", lambda e, h=h: e.transpose(out=tp[0][0:96, h, :], in_=q_full[:, h, :], identity=self.ident_b[:]),
                          reads=[bqf, self.b_const], writes=[btp[0]])
                kb.op("act", lambda e: e.copy(out=qT[:], in_=tp[0][0:96, :, :]), reads=[btp[0]], writes=[bqT])
                for gi in range(2):
                    for k in range(2):
                        kb.op("pe", lambda e, k=k, gi=gi: e.matmul(mm[gi][:], lhsT=ckvT[:, k, :], rhs=w_ukv[:, k, gi * 512:(gi + 1) * 512],
                                                                 start=(k == 0), stop=(k == 1)), reads=[bckvT, bw], writes=[bmm[gi]])
                    kb.op("act", lambda e, gi=gi: e.copy(out=kv_sb[:].rearrange("p h d -> p (h d)")[:, gi * 512:(gi + 1) * 512],
                                                        in_=mm[gi][:]), reads=[bmm[gi]], writes=[bkv])
                kb.op("dve", lambda e, t=t: e.tensor_copy(out=V[:, t, :, 0:64], in_=kv_sb[:, :, 64:128]),
                      reads=[bkv, bVones], writes=[bV[t]])
                kb.op("dve", lambda e: e.tensor_tensor(out=sq[:, :, 0:64], in0=kv_sb[:, :, 0:64], in1=kv_sb[:, :, 0:64], op=ALU.mult),
                      reads=[bkv], writes=[bsq])
                kb.op("dve", lambda e: e.tensor_reduce(out=rq8[:, 0:8], in_=sq[:, :, 0:64], axis=AX.X, op=ALU.add),
                      reads=[bsq], writes=[brq8])
                kb.op("dve", lambda e: e.tensor_scalar(out=rq8[:, 0:8], in0=rq8[:, 0:8], scalar1=1.0 / 64, scalar2=EPS,
                                                       op0=ALU.mult, op1=ALU.add), reads=[brq8], writes=[brq8])
                kb.op("pool", lambda e: e.tensor_tensor(out=rq8[:, 0:8], in0=rq8[:, 0:8], in1=self.neghalf[:, 0:8], op=ALU.pow),
                      reads=[brq8, self.b_const], writes=[brq8])
                kb.op("dve", lambda e: e.tensor_tensor(out=sq[:, :, 0:64], in0=kv_sb[:, :, 0:64], in1=bc_last(rq8[:, 0:8], 64),
                                                       op=ALU.mult), reads=[bkv, brq8], writes=[bsq])
                kb.op("dve", lambda e: e.tensor_tensor(out=k_full[:, :, 0:64], in0=sq[:, :, 0:64], in1=bc_mid(gk[:, 0:64], 8),
                                                        op=ALU.mult), reads=[bsq, bw], writes=[bkf])
                c1_ = cos[:, t, :]
                s1_ = sin[:, t, :]
                kb.op("dve", lambda e: e.tensor_tensor(out=rt[0][:, 0, :], in0=kr[:, 0:16], in1=c1_, op=ALU.mult), reads=[bkr, brope], writes=[brt])
                kb.op("dve", lambda e: e.tensor_tensor(out=rt[1][:, 0, :], in0=kr[:, 16:32], in1=s1_, op=ALU.mult), reads=[bkr, brope], writes=[brt])
                kb.op("dve", lambda e: e.tensor_tensor(out=rt[2][:, 0, :], in0=kr[:, 16:32], in1=c1_, op=ALU.mult), reads=[bkr, brope], writes=[brt])
                kb.op("dve", lambda e: e.tensor_tensor(out=rt[3][:, 0, :], in0=kr[:, 0:16], in1=s1_, op=ALU.mult), reads=[bkr, brope], writes=[brt])
                kb.op("dve", lambda e: e.tensor_tensor(out=kr2[:, 0:16], in0=rt[0][:, 0, :], in1=rt[1][:, 0, :], op=ALU.subtract),
                      reads=[brt], writes=[bkr])
                kb.op("dve", lambda e: e.tensor_tensor(out=kr2[:, 16:32], in0=rt[2][:, 0, :], in1=rt[3][:, 0, :], op=ALU.add),
                      reads=[brt], writes=[bkr])
                kb.op("dve", lambda e: e.tensor_copy(out=k_full[:, :, 64:96], in_=bc_mid(kr2[:, :], 8)), reads=[bkr], writes=[bkf])
                for h in range(8):
                    kb.op("pe", lambda e, h=h: e.transpose(out=tp[1][0:96, h, :], in_=k_full[:, h, :], identity=self.ident_b[:]),
                          reads=[bkf, self.b_const], writes=[btp[1]])
                kb.op("act", lambda e, t=t: e.copy(out=kT[:, :, t * 128:(t + 1) * 128], in_=tp[1][0:96, :, :]),
                      reads=[btp[1]], writes=[bkT[t]])
            def stage_b(t, s=s):
                qT = qTs[t % 2]; bqT = bqTs[t % 2]
                units = []
                for h in range(8):
                    for a in range(0, t + 1, 4):
                        units.append((h, a, min(a + 4, t + 1)))

                def emit_S(ui, u):
                    h, a, b = u
                    bank = ui % 2
                    for kt in range(a, b):
                        kb.op("pe", lambda e, kt=kt, h=h, a=a, bank=bank: e.matmul(
                            s2[:, bank, (kt - a) * 128:(kt - a + 1) * 128], lhsT=kT[:, h, kt * 128:(kt + 1) * 128],
                            rhs=qT[:, h, :], start=True, stop=True), reads=[bkT[kt], bqT], writes=[bs2[bank]])

                base = uc["n"]
                emit_S(base, units[0])
                for i, u in enumerate(units):
                    ui = base + i
                    h, a, b = u
                    if i + 1 < len(units):
                        emit_S(ui + 1, units[i + 1])
                    bank = ui % 2
                    P = PT[ui % 3]; bP = bPT[ui % 3]
                    n = (b - a) * 128
                    kb.op("act", lambda e, P=P, bank=bank, n=n: e.activation(
                        out=P[:].rearrange("p a b -> p (a b)")[:, 0:n], in_=s2[:, bank, 0:n], func=AF.Exp, scale=scale),
                        reads=[bs2[bank]], writes=[bP])
                    if b == t + 1:
                        kb.op("dve", lambda e, P=P, j=t - a: e.tensor_tensor(out=P[:, j, :], in0=P[:, j, :], in1=self.mask_le[:],
                                                                           op=ALU.mult), reads=[bP, self.b_const], writes=[bP])
                    ob = oo[h // 4]
                    for kt in range(a, b):
                        kb.op("pe", lambda e, kt=kt, h=h, a=a, P=P, ob=ob: e.matmul(
                            ob[:, (h % 4) * 65:(h % 4) * 65 + 65], lhsT=P[:, kt - a, :], rhs=V[:, kt, h, :],
                            start=(kt == 0), stop=(kt == t)), reads=[bP, bV[kt], bVones], writes=[boo[h // 4]])
                uc["n"] += len(units)
                for hb in range(2):
                    ov = oo[hb][:, 0:260].rearrange("p (h d) -> p h d", d=65)
                    kb.op("dve", lambda e, hb=hb, ov=ov: e.reciprocal(out=rden[:, hb * 4:(hb + 1) * 4], in_=ov[:, :, 64]),
                          reads=[boo[hb]], writes=[brden])
                    kb.op("dve", lambda e, hb=hb, ov=ov: e.tensor_tensor(out=attn[:, hb * 4:(hb + 1) * 4, :], in0=ov[:, :, 0:64],
                                                                        in1=bc_last(rden[:, hb * 4:(hb + 1) * 4], 64), op=ALU.mult),
                          reads=[boo[hb], brden], writes=[battn])
                kb.dma("sp", S["ATT"][s * NT + t], attn[:].rearrange("p h d -> p (h d)"), reads=[battn],
                       writes=[self.db("ATT", s * NT + t)])
            kb.pipeline(stage_a, stage_b, self.ntl)
        kb.pop()

    def alloc_router(self, l):
        kb, I = self.kb, self.I
        r = {}
        r["bw"] = Buf()
        r["rw"] = kb.sb("rw", [128, 8, 32], F32)
        kb.dma("sp", r["rw"][:], I["router_w"][l].rearrange("(k p) n -> p k n", p=128), writes=[r["bw"]])
        r["rb"] = kb.sb("rb", [128, 32], F32)
        self.bcast_load(r["rb"][:], I["router_b"][l], r["bw"])
        r["rwh"] = kb.sb("rwh", [128, 8, 32], BF16)
        r["rwl"] = kb.sb("rwl", [128, 8, 32], BF16)
        kb.op("dve", lambda e: e.tensor_copy(out=r["rwh"][:], in_=r["rw"][:]), reads=[r["bw"]], writes=[r["bw"]])
        kb.op("dve", lambda e: e.tensor_tensor(out=r["rwl"][:], in0=r["rw"][:], in1=r["rwh"][:], op=ALU.subtract),
              reads=[r["bw"]], writes=[r["bw"]])
        r["h2f"] = kb.sb("h2f", [128, D], F32); r["bh2f"] = Buf()
        r["h2hi"] = kb.sb("h2hi", [128, D], BF16); r["bh2hi"] = Buf()
        r["h2lo"] = kb.sb("h2lo", [128, D], BF16); r["bh2lo"] = Buf()
        r["h2Tl"] = kb.sb("h2Tl", [128, 8, 128], BF16); r["bh2Tl"] = Buf()
        r["h2Tb"] = kb.sb("h2Tb", [128, 8, 128], BF16); r["bh2Tb"] = Buf()
        r["lg"] = kb.sb("lg", [128, 32], F32); r["blg"] = Buf()
        r["m8"] = kb.sb("m8", [128, 8], F32)
        r["msk"] = kb.sb("msk", [128, 32], F32)
        r["ex"] = kb.sb("ex", [128, 32], F32)
        r["den"] = kb.sb("den", [128, 2], F32)
        r["G"] = kb.sb("Gt", [128, 32], F32); r["bG"] = Buf()
        r["ss"] = kb.sb("ss2", [128, 1], F32); r["bss"] = Buf()
        r["cnt"] = kb.sb("cnt_b", [128, 32], F32); r["bcnt"] = Buf()
        kb.op("pool", lambda e: e.memset(r["cnt"][:], 0.0), writes=[r["bcnt"]])
        r["Mb"] = kb.sb("Mb", [128, 32], BF16)
        for n_ in ("posf", "valid", "slotm", "oh", "junk", "Gv"):
            r[n_] = kb.sb(n_, [128, 32], F32)
        r["slotf"] = kb.sb("slotf", [128, 4], F32)
        r["sloti"] = kb.sb("sloti", [128, 4], I32); r["bsloti"] = Buf()
        r["gk"] = kb.sb("gk", [128, 4], F32); r["bgk"] = Buf()
        r["brt"] = Buf()
        return r

    def norm2_router(self, r, x1, bx1, gmod2, shift2, bmod, tmp, btmp, tp, btp, mmp, bmmp, tile_idx):
        kb, S = self.kb, self.S
        ss, bss = r["ss"], r["bss"]
        kb.op("dve", lambda e: e.scalar_tensor_tensor(out=tmp[:], in0=x1[:], scalar=1.0, in1=x1[:], op0=ALU.mult, op1=ALU.mult, accum_out=ss[:, 0:1]),
              reads=[bx1], writes=[btmp, bss])
        self.rstd_of(ss[:, 0:1], D, 1, bss, "")
        kb.op("dve", lambda e: e.scalar_tensor_tensor(out=tmp[:], in0=x1[:], scalar=ss[:, 0:1], in1=gmod2[:],
                                                      op0=ALU.mult, op1=ALU.mult), reads=[bx1, bss, bmod], writes=[btmp])
        kb.op("dve", lambda e: e.tensor_tensor(out=r["h2f"][:], in0=tmp[:], in1=shift2[:], op=ALU.add),
              reads=[btmp, bmod], writes=[r["bh2f"]])
        kb.op("act", lambda e: e.copy(out=r["h2hi"][:], in_=r["h2f"][:]), reads=[r["bh2f"]], writes=[r["bh2hi"]])
        kb.op("dve", lambda e: e.tensor_tensor(out=r["h2lo"][:], in0=r["h2f"][:], in1=r["h2hi"][:], op=ALU.subtract),
              reads=[r["bh2f"], r["bh2hi"]], writes=[r["bh2lo"]])
        for k in range(8):
            kb.op("pe", lambda e, k=k: e.transpose(out=tp[0][:, k, :], in_=r["h2hi"][:, k * 128:(k + 1) * 128],
                                                   identity=self.ident_b[:]), reads=[r["bh2hi"], self.b_const], writes=[btp[0]])
        for k in range(8):
            kb.op("pe", lambda e, k=k: e.transpose(out=tp[1][:, k, :], in_=r["h2lo"][:, k * 128:(k + 1) * 128],
                                                   identity=self.ident_b[:]), reads=[r["bh2lo"], self.b_const], writes=[btp[1]])
        kb.op("act", lambda e: e.copy(out=r["h2Tb"][:], in_=tp[0][:]), reads=[btp[0]], writes=[r["bh2Tb"]])
        kb.op("dve", lambda e: e.tensor_copy(out=r["h2Tl"][:], in_=tp[1][:]), reads=[btp[1]], writes=[r["bh2Tl"]])
        if self.debug:
            kb.dma("sp", S["H2T"][tile_idx], r["h2Tb"][:], reads=[r["bh2Tb"]], writes=[self.db("H2T", tile_idx)])
        if _STOP <= 6:
            return
        passes = [("h2Tb", "rwh"), ("h2Tl", "rwh"), ("h2Tb", "rwl")]
        for pi, (a_, w_) in enumerate(passes):
            for k in range(8):
                kb.op("pe", lambda e, k=k, a_=a_, w_=w_, pi=pi: e.matmul(mmp[:, 0:32], lhsT=r[a_][:, k, :], rhs=r[w_][:, k, :],
                                                                       start=(pi == 0 and k == 0), stop=(pi == 2 and k == 7)),
                      reads=[r["bh2Tb"], r["bh2Tl"], r["bw"]], writes=[bmmp])
        lg, m8, msk, ex, den, G = r["lg"], r["m8"], r["msk"], r["ex"], r["den"], r["G"]
        bl = r["blg"]
        kb.op("dve", lambda e: e.tensor_tensor(out=lg[:], in0=mmp[:, 0:32], in1=r["rb"][:], op=ALU.add),
              reads=[bmmp, r["bw"]], writes=[bl])
        if _STOP <= 7:
            return
        kb.op("dve", lambda e: e.max(out=m8[:], in_=lg[:]), reads=[bl], writes=[bl])
        kb.op("dve", lambda e: e.tensor_scalar(out=msk[:], in0=lg[:], scalar1=m8[:, 3:4], scalar2=None, op0=ALU.is_ge),
              reads=[bl], writes=[bl])
        kb.op("dve", lambda e: e.tensor_scalar(out=den[:, 1:2], in0=m8[:, 0:1], scalar1=-1.0, scalar2=None, op0=ALU.mult),
              reads=[bl], writes=[bl])
        kb.op("act", lambda e: e.activation(out=ex[:], in_=lg[:], func=AF.Exp, bias=den[:, 1:2], scale=1.0),
              reads=[bl], writes=[bl])
        kb.op("dve", lambda e: e.scalar_tensor_tensor(out=ex[:], in0=ex[:], scalar=1.0, in1=msk[:], op0=ALU.mult, op1=ALU.mult, accum_out=den[:, 0:1]), reads=[bl], writes=[bl])
        kb.op("dve", lambda e: e.reciprocal(out=den[:, 0:1], in_=den[:, 0:1]), reads=[bl], writes=[bl])
        kb.op("dve", lambda e: e.tensor_scalar(out=G[:], in0=ex[:], scalar1=den[:, 0:1], scalar2=None, op0=ALU.mult),
              reads=[bl], writes=[r["bG"]])
        if self.debug:
            kb.dma("sp", S["GS"][tile_idx], G[:], reads=[r["bG"]], writes=[self.db("GS", tile_idx)])
        cap = self.cap
        brt = r["brt"]
        kb.op("dve", lambda e: e.tensor_copy(out=r["Mb"][:], in_=msk[:]), reads=[bl], writes=[brt])
        kb.op("pe", lambda e: e.matmul(mmp[:, 32:64], lhsT=self.U_b[:], rhs=r["Mb"][:], start=True, stop=True),
              reads=[brt, self.b_const], writes=[bmmp])
        kb.op("pe", lambda e: e.matmul(mmp[:, 64:96], lhsT=self.ones_b[:], rhs=r["Mb"][:], start=True, stop=True),
              reads=[brt, self.b_const], writes=[bmmp])
        kb.op("dve", lambda e: e.tensor_tensor(out=r["posf"][:], in0=mmp[:, 32:64], in1=r["cnt"][:], op=ALU.add),
              reads=[bmmp, r["bcnt"]], writes=[brt])
        kb.op("dve", lambda e: e.tensor_tensor(out=r["cnt"][:], in0=mmp[:, 64:96], in1=r["cnt"][:], op=ALU.add),
              reads=[bmmp, r["bcnt"]], writes=[r["bcnt"]])
        kb.op("dve", lambda e: e.tensor_scalar(out=r["valid"][:], in0=r["posf"][:], scalar1=float(cap), scalar2=None, op0=ALU.is_lt),
              reads=[brt], writes=[brt])
        kb.op("dve", lambda e: e.tensor_tensor(out=r["slotm"][:], in0=r["posf"][:], in1=self.iotaE[:], op=ALU.add),
              reads=[brt, self.b_const], writes=[brt])
        kb.op("dve", lambda e: e.tensor_scalar(out=r["junk"][:], in0=r["valid"][:], scalar1=-1.0e6, scalar2=1.0e6,
                                               op0=ALU.mult, op1=ALU.add), reads=[brt], writes=[brt])
        kb.op("dve", lambda e: e.tensor_tensor(out=r["slotm"][:], in0=r["slotm"][:], in1=r["junk"][:], op=ALU.add),
              reads=[brt], writes=[brt])
        kb.op("dve", lambda e: e.tensor_tensor(out=r["Gv"][:], in0=G[:], in1=r["valid"][:], op=ALU.mult),
              reads=[brt, r["bG"]], writes=[brt])
        for k in range(4):
            kb.op("dve", lambda e, k=k: e.tensor_scalar(out=r["oh"][:], in0=lg[:], scalar1=m8[:, k:k + 1], scalar2=None, op0=ALU.is_equal),
                  reads=[bl, brt], writes=[brt])
            kb.op("dve", lambda e, k=k: e.scalar_tensor_tensor(out=r["junk"][:], in0=r["oh"][:], scalar=1.0, in1=r["slotm"][:],
                                                              op0=ALU.mult, op1=ALU.mult, accum_out=r["slotf"][:, k:k + 1]),
                  reads=[brt], writes=[brt])
            kb.op("dve", lambda e, k=k: e.scalar_tensor_tensor(out=r["junk"][:], in0=r["oh"][:], scalar=1.0, in1=r["Gv"][:],
                                                              op0=ALU.mult, op1=ALU.mult, accum_out=r["gk"][:, k:k + 1]),
                  reads=[brt, r["bgk"]], writes=[brt, r["bgk"]])
        kb.op("dve", lambda e: e.tensor_copy(out=r["sloti"][:], in_=r["slotf"][:]), reads=[brt, r["bsloti"]], writes=[r["bsloti"]])
        kb.dma("sp", S["SLOT"][tile_idx], r["sloti"][:], reads=[r["bsloti"]], writes=[self.db("SLOT", tile_idx)])
        kb.dma("sp", S["GK"][tile_idx], r["gk"][:], reads=[r["bgk"]], writes=[self.db("GK", tile_idx)])
        for k in range(4):
            kb.idma(S["XG"], r["sloti"][:, k:k + 1], r["h2hi"][:], None, 32 * cap - 1, reads=[r["bsloti"], r["bh2hi"]])

    def phase1b(self):
        kb, I, S, nseq = self.kb, self.I, self.S, self.nseq
        kb.push()
        bw = Buf()
        w_in = kb.sb("w_in_b", [128, 8, 2048], BF16)
        w_out = kb.sb("w_out", [128, 8, D], BF16)
        self.load_w_bf16(w_in, I["hyb_w_in"][:, 672:2720], 8, bw)
        self.load_w_bf16(w_out, I["hyb_w_out"], 8, bw)
        retg = kb.sb("retg", [128, 512], F32)
        self.bcast_load(retg[:], I["ret_norm_g"], bw)
        decT = kb.sb("decT", [128, 8 * 128], F32)
        qdec = kb.sb("qdec", [128, 8], F32)
        kdec = kb.sb("kdec", [128, 8], F32)
        cdec = kb.sb("cdec", [128, 4], F32)
        kb.dma("sp", decT[:], I["k_decayT"], writes=[bw])
        kb.dma("sp", qdec[:], I["k_qdec"], writes=[bw])
        kb.dma("sp", kdec[:], I["k_kdec"], writes=[bw])
        kb.dma("sp", cdec[:], I["k_cdec"], writes=[bw])
        r = self.alloc_router(0)

        mods = {n: kb.sb(n, [128, D], F32) for n in ("gmod1", "shift1", "gate1", "gmod2", "shift2")}
        bmod = Buf()
        x_t = [kb.sb("x_t%d" % i, [128, D], F32) for i in range(2)]; bx = [Buf(), Buf()]
        tmp = kb.sb("tmp", [128, D], F32); btmp = Buf()
        h_bf = kb.sb("h_bf", [128, D], BF16); bh = Buf()
        hT = kb.sb("hT", [128, 8, 128], BF16); bhT = Buf()
        ss = kb.sb("ss", [128, 4], F32); bss = Buf()
        raw = [kb.sb("raw%d" % i, [128, 8, 64], F32) for i in range(2)]; braw = [Buf(), Buf()]
        rr = [kb.sb("rr%d" % i, [128, 8, 64], F32) for i in range(2)]; brr = [Buf(), Buf()]
        rt = [kb.sb("rt%d" % i, [128, 8, 32], F32) for i in range(4)]; brt = Buf()
        rq_bf = kb.sb("rq_bf", [128, 8, 64], BF16); brqb = Buf()
        rqd_bf = kb.sb("rqd_bf", [128, 8, 64], BF16); brqd = Buf()
        rk_bf = kb.sb("rk_bf", [128, 8, 64], BF16); brkb = Buf()
        rkd_bf_2 = [kb.sb("rkd_bf%d" % i, [128, 8, 64], BF16) for i in range(2)]; brkd_2 = [Buf(), Buf()]
        v_bf_2 = [kb.sb("v_bf%d" % i, [128, 8, 64], BF16) for i in range(2)]; bv_2 = [Buf(), Buf()]
        sg_2 = [kb.sb("sg%d" % i, [128, 512], F32) for i in range(2)]; bsg_2 = [Buf(), Buf()]
        rqT_2 = [kb.sb("rqT%d" % i, [128, 8, 128], BF16) for i in range(2)]; brqT_2 = [Buf(), Buf()]
        rkT_2 = [kb.sb("rkT%d" % i, [128, 4, 128], BF16) for i in range(2)]; brkT_2 = [Buf(), Buf()]
        Sd = kb.sb("Sd", [128, 8, 128], BF16); bSd = Buf()
        st_f = kb.sb("st_f", [128, 4, 128], F32); bstf = Buf()
        st_b = kb.sb("st_b", [128, 4, 128], BF16); bstb = Buf()
        kb.op("pool", lambda e: e.memset(st_f[:], 0.0), writes=[bstf])
        o_sb = kb.sb("o_sb", [128, 8, 64], F32); bo = Buf()
        oc = kb.sb("oc", [128, 8, 64], F32); boc = Buf()
        st8 = kb.sb("st8", [128, 16], F32); bst8 = Buf()
        mixcat_2 = [kb.sb("mixcat%d" % i, [128, D], BF16) for i in range(2)]; bmixa_2 = [Buf(), Buf()]; bmixy_2 = [Buf(), Buf()]
        tmpB = kb.sb("tmpB", [128, D], F32); btmpB = Buf()
        mixT = kb.sb("mixT", [128, 8, 128], BF16); bmixT = Buf()
        x1 = kb.sb("x1", [128, D], F32); bx1 = Buf()

        tp = [kb.ps("tp%d" % i, [128, 8, 128], BF16) for i in range(2)]; btp = [Buf(), Buf()]
        mm = [kb.ps("mm%d" % i, [128, 512], F32) for i in range(2)]; bmm = [Buf(), Buf()]
        s2 = kb.ps("s2", [128, 2, 512], F32); bs2 = [Buf(), Buf()]
        oo = [kb.ps("oo%d" % i, [128, 512], F32) for i in range(2)]; boo = [Buf(), Buf()]
        tpB = [s2[:, i, :].bitcast(BF16).rearrange("p (k m) -> p k m", m=128) for i in range(2)]
        for s in range(nseq):
            cos, sin, brope = self.rope_tables(s, 32, "k_invf32", "b%d" % s)
            for n_, part in (("gmod1", 1), ("shift1", 0), ("gate1", 2), ("gmod2", 4), ("shift2", 3)):
                kb.dma("sp", mods[n_][:], S["MOD"][s, 0, part].partition_broadcast(128), reads=[self.db("MOD", (s, 0, part))], writes=[bmod])
            def stage_a(t, s=s, cos=cos, sin=sin, brope=brope):
                P_ = t % 2
                X = x_t[P_]; bX = bx[P_]
                v_bf = v_bf_2[P_]; bv = bv_2[P_]; sg = sg_2[P_]; bsg = bsg_2[P_]; rqT = rqT_2[P_]; brqT = brqT_2[P_]
                rkT = rkT_2[P_]; brkT = brkT_2[P_]; rkd_bf = rkd_bf_2[P_]; brkd = brkd_2[P_]
                mixcat = mixcat_2[P_]; bmixa = bmixa_2[P_]; bmixy = bmixy_2[P_]
                ti = s * NT + t
                kb.dma("sp", X[:], I["x"][ti * 128:(ti + 1) * 128, :], writes=[bX])
                kb.dma("sp", mixcat[:, 0:512], S["ATT"][ti], reads=[self.db("ATT", ti)], writes=[bmixa])
                self.norm_mod_T(X, bX, mods["gmod1"], mods["shift1"], bmod, tmp, btmp, h_bf, bh, tp[0], btp[0], hT, bhT, ss, bss)
                cb = bc_mid(cos[:, t, :], 8)
                sb_ = bc_mid(sin[:, t, :], 8)
                for gi in range(4):
                    p_ = mm[gi % 2]; bp_ = bmm[gi % 2]
                    for k in range(8):
                        kb.op("pe", lambda e, k=k, gi=gi, p_=p_: e.matmul(p_[:], lhsT=hT[:, k, :], rhs=w_in[:, k, gi * 512:(gi + 1) * 512],
                                                                       start=(k == 0), stop=(k == 7)), reads=[bhT, bw], writes=[bp_])
                    if gi < 2:
                        rw_ = raw[gi]; brw_ = braw[gi]; ro = rr[gi]; bro = brr[gi]
                        kb.op("act", lambda e, p_=p_, rw_=rw_: e.copy(out=rw_[:].rearrange("p h d -> p (h d)"), in_=p_[:]),
                              reads=[bp_], writes=[brw_])
                        x1_ = rw_[:, :, 0:32]; x2_ = rw_[:, :, 32:64]
                        kb.op("dve", lambda e, x1_=x1_: e.tensor_tensor(out=rt[0][:], in0=x1_, in1=cb, op=ALU.mult), reads=[brw_, brope], writes=[brt])
                        kb.op("dve", lambda e, x2_=x2_: e.tensor_tensor(out=rt[1][:], in0=x2_, in1=sb_, op=ALU.mult), reads=[brw_, brope], writes=[brt])
                        kb.op("dve", lambda e, x2_=x2_: e.tensor_tensor(out=rt[2][:], in0=x2_, in1=cb, op=ALU.mult), reads=[brw_, brope], writes=[brt])
                        kb.op("dve", lambda e, x1_=x1_: e.tensor_tensor(out=rt[3][:], in0=x1_, in1=sb_, op=ALU.mult), reads=[brw_, brope], writes=[brt])
                        kb.op("dve", lambda e, ro=ro: e.tensor_tensor(out=ro[:, :, 0:32], in0=rt[0][:], in1=rt[1][:], op=ALU.subtract),
                              reads=[brt], writes=[bro])
                        kb.op("dve", lambda e, ro=ro: e.tensor_tensor(out=ro[:, :, 32:64], in0=rt[2][:], in1=rt[3][:], op=ALU.add),
                              reads=[brt], writes=[bro])
                        if gi == 0:
                            kb.op("act", lambda e, ro=ro: e.copy(out=rq_bf[:], in_=ro[:]), reads=[bro], writes=[brqb])
                            kb.op("dve", lambda e, ro=ro: e.tensor_tensor(out=rqd_bf[:], in0=ro[:], in1=bc_last(qdec[:, :], 64), op=ALU.mult),
                                  reads=[bro, bw], writes=[brqd])
                        else:
                            kb.op("act", lambda e, ro=ro: e.mul(out=rk_bf[:], in_=ro[:], mul=0.125), reads=[bro], writes=[brkb])
                            kb.op("dve", lambda e, ro=ro: e.scalar_tensor_tensor(out=rkd_bf[:], in0=ro[:], scalar=0.125,
                                                                                in1=bc_last(kdec[:, :], 64), op0=ALU.mult, op1=ALU.mult),
                                  reads=[bro, bw], writes=[brkd])
                    elif gi == 2:
                        kb.op("act", lambda e, p_=p_: e.copy(out=v_bf[:].rearrange("p h d -> p (h d)"), in_=p_[:]), reads=[bp_], writes=[bv])
                    else:
                        kb.op("act", lambda e, p_=p_: e.activation(out=sg[:], in_=p_[:], func=AF.Silu), reads=[bp_], writes=[bsg])
                for i in range(4):
                    kb.op("pe", lambda e, i=i: e.transpose(out=tp[1][:, i, :], in_=rq_bf[:, 2 * i:2 * i + 2, :].rearrange("p h d -> p (h d)"),
                                                           identity=self.ident_b[:]), reads=[brqb, self.b_const], writes=[btp[1]])
                for i in range(4):
                    kb.op("pe", lambda e, i=i: e.transpose(out=tp[1][:, 4 + i, :], in_=rqd_bf[:, 2 * i:2 * i + 2, :].rearrange("p h d -> p (h d)"),
                                                           identity=self.ident_b[:]), reads=[brqd, self.b_const], writes=[btp[1]])
                for i in range(4):
                    kb.op("pe", lambda e, i=i: e.transpose(out=tp[0][:, i, :], in_=rk_bf[:, 2 * i:2 * i + 2, :].rearrange("p h d -> p (h d)"),
                                                           identity=self.ident_b[:]), reads=[brkb, self.b_const], writes=[btp[0]])
                kb.op("act", lambda e: e.copy(out=rqT[:], in_=tp[1][:]), reads=[btp[1]], writes=[brqT])
                kb.op("dve", lambda e: e.tensor_copy(out=rkT[:], in_=tp[0][:, 0:4, :]), reads=[btp[0]], writes=[brkT])
            def stage_b(t, s=s):
                P_ = t % 2
                X = x_t[P_]; bX = bx[P_]
                v_bf = v_bf_2[P_]; bv = bv_2[P_]; sg = sg_2[P_]; bsg = bsg_2[P_]; rqT = rqT_2[P_]; brqT = brqT_2[P_]
                rkT = rkT_2[P_]; brkT = brkT_2[P_]; rkd_bf = rkd_bf_2[P_]; brkd = brkd_2[P_]
                mixcat = mixcat_2[P_]; bmixa = bmixa_2[P_]; bmixy = bmixy_2[P_]
                ti = s * NT + t
                tmp = tmpB; btmp = btmpB
                for h in range(8):
                    i, o = h // 2, (h % 2) * 64
                    kb.op("pe", lambda e, h=h, i=i, o=o: e.matmul(s2[:, h % 2, i * 128:(i + 1) * 128],
                                                                lhsT=rkT[o:o + 64, i, :], rhs=rqT[o:o + 64, i, :], start=True, stop=True),
                          reads=[brkT, brqT], writes=[bs2[h % 2]])
                for hb in range(2):
                    kb.op("dve", lambda e, hb=hb: e.tensor_tensor(out=Sd[:, hb * 4:(hb + 1) * 4, :].rearrange("p h q -> p (h q)"),
                                                                 in0=s2[:, hb, :], in1=decT[:, hb * 512:(hb + 1) * 512], op=ALU.mult),
                          reads=[bs2[hb], bw], writes=[bSd])
                if _STOP <= 1.5:
                    return
                for i in range(4):
                    if t > 0:
                        kb.op("pe", lambda e, i=i: e.matmul(oo[0][:, i * 128:(i + 1) * 128], lhsT=rqT[:, 4 + i, :],
                                                            rhs=st_b[:, i, :], start=True, stop=False, skip_group_check=True),
                              reads=[brqT, bstb], writes=[boo[0]])
                    for par in range(2):
                        h = 2 * i + par
                        kb.op("pe", lambda e, h=h, i=i, par=par: e.matmul(oo[0][:, h * 64:(h + 1) * 64], lhsT=Sd[:, par * 4 + i, :],
                                                                        rhs=v_bf[:, h, :], start=(t == 0), stop=(t == 0 or par == 1),
                                                                        skip_group_check=(t > 0)),
                              reads=[bSd, bv], writes=[boo[0]])
                if _STOP <= 2:
                    return
                for i in range(4):
                    kb.op("pe", lambda e, i=i: e.matmul(oo[1][:, i * 128:(i + 1) * 128],
                                                        lhsT=rkd_bf[:, 2 * i:2 * i + 2, :].rearrange("p h d -> p (h d)"),
                                                        rhs=v_bf[:, 2 * i:2 * i + 2, :].rearrange("p h d -> p (h d)"), start=True, stop=True),
                          reads=[brkd, bv], writes=[boo[1]])
                kvv = oo[1][:].rearrange("p (i c) -> p i c", c=128)
                for half in range(2):
                    po = half * 64
                    if t == 0:
                        kb.op("dve", lambda e, po=po: e.tensor_copy(out=st_f[po:po + 64, :, po:po + 64], in_=kvv[po:po + 64, :, po:po + 64]),
                              reads=[boo[1]], writes=[bstf])
                    else:
                        for i in range(4):
                            kb.op("dve", lambda e, po=po, i=i: e.scalar_tensor_tensor(
                                out=st_f[po:po + 64, i, po:po + 64], in0=st_f[po:po + 64, i, po:po + 64], scalar=cdec[po:po + 64, i:i + 1],
                                in1=kvv[po:po + 64, i, po:po + 64], op0=ALU.mult, op1=ALU.add), reads=[boo[1], bstf, bw], writes=[bstf])
                kb.op("act", lambda e: e.copy(out=st_b[:], in_=st_f[:]), reads=[bstf], writes=[bstb])
                if _STOP <= 3:
                    return
                kb.op("act", lambda e: e.copy(out=o_sb[:].rearrange("p h d -> p (h d)"), in_=oo[0][:]), reads=[boo[0]], writes=[bo])
                kb.op("dve", lambda e: e.tensor_reduce(out=st8[:, 0:8], in_=o_sb[:], axis=AX.X, op=ALU.add), reads=[bo], writes=[bst8])
                kb.op("dve", lambda e: e.tensor_scalar(out=st8[:, 0:8], in0=st8[:, 0:8], scalar1=-1.0 / 64, scalar2=None, op0=ALU.mult),
                      reads=[bst8], writes=[bst8])
                kb.op("dve", lambda e: e.tensor_tensor(out=oc[:], in0=o_sb[:], in1=bc_last(st8[:, 0:8], 64), op=ALU.add),
                      reads=[bo, bst8], writes=[boc])
                kb.op("dve", lambda e: e.tensor_tensor(out=o_sb[:], in0=oc[:], in1=oc[:], op=ALU.mult), reads=[boc], writes=[bo])
                kb.op("dve", lambda e: e.tensor_reduce(out=st8[:, 8:16], in_=o_sb[:], axis=AX.X, op=ALU.add), reads=[bo], writes=[bst8])
                kb.op("dve", lambda e: e.tensor_scalar(out=st8[:, 8:16], in0=st8[:, 8:16], scalar1=1.0 / 64, scalar2=EPS,
                                                       op0=ALU.mult, op1=ALU.add), reads=[bst8], writes=[bst8])
                kb.op("pool", lambda e: e.tensor_tensor(out=st8[:, 8:16], in0=st8[:, 8:16], in1=self.neghalf[:, 0:8], op=ALU.pow),
                      reads=[bst8, self.b_const], writes=[bst8])
                kb.op("dve", lambda e: e.tensor_tensor(out=oc[:], in0=oc[:], in1=bc_last(st8[:, 8:16], 64), op=ALU.mult),
                      reads=[boc, bst8], writes=[boc])
                kb.op("dve", lambda e: e.tensor_tensor(out=oc[:].rearrange("p h d -> p (h d)"), in0=oc[:].rearrange("p h d -> p (h d)"),
                                                        in1=retg[:], op=ALU.mult), reads=[boc, bw], writes=[boc])
                kb.op("dve", lambda e: e.tensor_tensor(out=mixcat[:, 512:1024], in0=oc[:].rearrange("p h d -> p (h d)"), in1=sg[:],
                                                       op=ALU.mult), reads=[boc, bsg], writes=[bmixy])
                if _STOP <= 4:
                    return
                for k in range(8):
                    kb.op("pe", lambda e, k=k: e.transpose(out=tpB[0][:, k, :], in_=mixcat[:, k * 128:(k + 1) * 128], identity=self.ident_b[:]),
                          reads=[bmixa, bmixy, self.b_const], writes=[bs2[0]])
                kb.op("act", lambda e: e.copy(out=mixT[:], in_=tpB[0]), reads=[bs2[0]], writes=[bmixT])
                for half in range(2):
                    for k in range(8):
                        kb.op("pe", lambda e, k=k, half=half: e.matmul(oo[half][:], lhsT=mixT[:, k, :], rhs=w_out[:, k, half * 512:(half + 1) * 512],
                                                                     start=(k == 0), stop=(k == 7)), reads=[bmixT, bw], writes=[boo[half]])
                    hs = slice(half * 512, (half + 1) * 512)
                    kb.op("dve", lambda e, half=half, hs=hs: e.tensor_tensor(out=tmp[:, hs], in0=oo[half][:], in1=mods["gate1"][:, hs], op=ALU.mult),
                          reads=[boo[half], bmod], writes=[btmp])
                    kb.op("dve", lambda e, hs=hs: e.tensor_tensor(out=x1[:, hs], in0=tmp[:, hs], in1=X[:, hs], op=ALU.add),
                          reads=[btmp, bX], writes=[bx1])
                kb.dma("sp", S["XA"][ti * 128:(ti + 1) * 128, :], x1[:], reads=[bx1], writes=[self.db("XA", ti)])
                if _STOP <= 5:
                    return
                self.norm2_router(r, x1, bx1, mods["gmod2"], mods["shift2"], bmod, tmp, btmp, tpB, bs2, oo[0], boo[0], ti)
            kb.pipeline(stage_a, stage_b, self.ntl)
        kb.pop()

    def zero_xg(self):
        kb, S = self.kb, self.S
        z = kb.sb("zeros", [128, 4096], BF16); bz = Buf()
        kb.op("pool", lambda e: e.memset(z[:], 0.0), writes=[bz])
        nrows = 32 * self.cap
        for r0 in range(0, nrows, 512):
            kb.dma("sp", S["XG"][r0:r0 + 512, :].rearrange("(p a) d -> p (a d)", p=128), z[:], reads=[bz])

    def phase2e(self, l):
        kb, I, S = self.kb, self.I, self.S
        kb.push()
        cap = self.cap
        nblk = cap // 512
        wgu = [kb.sb("wgu%d" % i, [128, 8, 2048], BF16) for i in range(2)]
        wdn = [kb.sb("wdn%d" % i, [128, 8, D], BF16) for i in range(2)]
        bgu = [kb.sb("bgu%d" % i, [128, 16], F32) for i in range(2)]
        bdn = [kb.sb("bdn%d" % i, [1, D], BF16) for i in range(2)]
        bwt = [Buf(), Buf()]
        ones1 = kb.sb("ones1", [1, 128], BF16); bones = Buf()
        kb.op("pool", lambda e: e.memset(ones1[:], 1.0), writes=[bones])
        xg = [kb.sb("xg%d" % i, [128, D], BF16) for i in range(12)]; bxg = [Buf() for _ in range(12)]
        xgT = [kb.sb("xgT%d" % i, [128, 8, 512], BF16) for i in range(2)]; bxgT = [Buf(), Buf()]
        glu = [kb.sb("glu%d" % i, [128, 512], F32) for i in range(2)]; bglu = [Buf(), Buf()]
        sig = [kb.sb("sig%d" % i, [128, 512], F32) for i in range(2)]; bsig = [Buf(), Buf()]
        lin = [kb.sb("lin%d" % i, [128, 512], F32) for i in range(2)]; blin = [Buf(), Buf()]
        actT = [kb.sb("actT%d" % i, [128, 8, 512], BF16) for i in range(2)]; bact = [Buf(), Buf()]
        yg = [kb.sb("yg%d" % i, [128, D], F32) for i in range(3)]; byg = [Buf() for _ in range(3)]
        tp = [kb.ps("tp%d" % i, [128, 8, 128], BF16) for i in range(2)]; btp = [Buf(), Buf()]
        pA = [kb.ps("pA%d" % i, [128, 512], F32) for i in range(2)]; bpA = [Buf(), Buf()]
        pB = [kb.ps("pB%d" % i, [128, 512], F32) for i in range(2)]; bpB = [Buf(), Buf()]
        pC = [kb.ps("pC%d" % i, [128, 512], F32) for i in range(2)]; bpC = [Buf(), Buf()]

        def load_expert(e, slot):
            self.load_w_bf16(wgu[slot], I["exp_w_gu"][l, e], 8, bwt[slot])
            self.load_w_bf16(wdn[slot], I["exp_w_down"][l, e], 8, bwt[slot])
            kb.dma("sp", bgu[slot][:], I["exp_b_gu_pj"][l, e], writes=[bwt[slot]])
            kb.op("dve", lambda en, slot=slot: en.tensor_scalar(out=bgu[slot][:, 8:16], in0=bgu[slot][:, 8:16], scalar1=1.0, scalar2=None,
                                                               op0=ALU.add), reads=[bwt[slot]], writes=[bwt[slot]])
            kb.dma("pool", bdn[slot][:], I["exp_b_down"][l, e:e + 1, :], writes=[bwt[slot]])

        load_expert(0, 0)
        blocks = [(ex, blk) for ex in range(32) for blk in range(nblk)]
        state = dict(xu=0, tu=0)

        def emit_loads(bi):
            ex, blk = blocks[bi]
            r0 = ex * cap + blk * 512
            tiles = []
            for st in range(4):
                i = state["xu"] % len(xg); state["xu"] += 1
                kb.dma("sp", xg[i][:], S["XG"][r0 + st * 128:r0 + (st + 1) * 128, :], writes=[bxg[i]])
                tiles.append(i)
            return tiles

        def emit_transposes(bi, tiles):
            XT = xgT[bi % 2]; bXT = bxgT[bi % 2]
            for st, i in enumerate(tiles):
                T = tp[state["tu"] % 2]; bT = btp[state["tu"] % 2]; state["tu"] += 1
                for k in range(8):
                    kb.op("pe", lambda e, k=k, i=i, T=T: e.transpose(out=T[:, k, :], in_=xg[i][:, k * 128:(k + 1) * 128],
                                                                   identity=self.ident_b[:]), reads=[bxg[i], self.b_const], writes=[bT])
                kb.op("act", lambda e, T=T, XT=XT, st=st: e.copy(out=XT[:, :, st * 128:(st + 1) * 128], in_=T[:]),
                      reads=[bT], writes=[bXT])

        pu = cu = yu = 0
        tl0 = emit_loads(0)
        tl1 = emit_loads(1) if len(blocks) > 1 else None
        emit_transposes(0, tl0)
        for bi, (ex, blk) in enumerate(blocks):
            slot = ex % 2
            if blk == 0 and ex + 1 < 32:
                load_expert(ex + 1, (ex + 1) % 2)
            XT = xgT[bi % 2]; bXT = bxgT[bi % 2]
            A = actT[bi % 2]; bA = bact[bi % 2]
            r0 = ex * cap + blk * 512
            for j in range(8):
                pa = pA[pu % 2]; bpa = bpA[pu % 2]; pb = pB[pu % 2]; bpb = bpB[pu % 2]
                gl = glu[pu % 2]; bgl = bglu[pu % 2]; sg_ = sig[pu % 2]; bsg_ = bsig[pu % 2]; ln = lin[pu % 2]; bln = blin[pu % 2]
                pu += 1
                for k in range(8):
                    kb.op("pe", lambda e, k=k, j=j, pa=pa, slot=slot, XT=XT: e.matmul(
                        pa[:], lhsT=wgu[slot][:, k, j * 128:(j + 1) * 128], rhs=XT[:, k, :],
                        start=(k == 0), stop=(k == 7)), reads=[bwt[slot], bXT], writes=[bpa])
                for k in range(8):
                    kb.op("pe", lambda e, k=k, j=j, pb=pb, slot=slot, XT=XT: e.matmul(
                        pb[:], lhsT=wgu[slot][:, k, 1024 + j * 128:1024 + (j + 1) * 128], rhs=XT[:, k, :],
                        start=(k == 0), stop=(k == 7)), reads=[bwt[slot], bXT], writes=[bpb])
                kb.op("dve", lambda e, pa=pa, gl=gl, j=j, slot=slot: e.tensor_scalar(
                    out=gl[:], in0=pa[:], scalar1=bgu[slot][:, j:j + 1], scalar2=7.0, op0=ALU.add, op1=ALU.min),
                    reads=[bpa, bwt[slot]], writes=[bgl])
                kb.op("act", lambda e, gl=gl, sg_=sg_: e.activation(out=sg_[:], in_=gl[:], func=AF.Sigmoid, scale=1.702),
                      reads=[bgl], writes=[bsg_])
                kb.op("dve", lambda e, pb=pb, ln=ln, j=j, slot=slot: e.tensor_scalar(
                    out=ln[:], in0=pb[:], scalar1=bgu[slot][:, 8 + j:9 + j], scalar2=8.0, op0=ALU.add, op1=ALU.min),
                    reads=[bpb, bwt[slot]], writes=[bln])
                kb.op("dve", lambda e, gl=gl, sg_=sg_: e.tensor_tensor(out=gl[:], in0=gl[:], in1=sg_[:], op=ALU.mult),
                      reads=[bgl, bsg_], writes=[bgl])
                kb.op("dve", lambda e, gl=gl, ln=ln, A=A, j=j: e.scalar_tensor_tensor(out=A[:, j, :], in0=ln[:], scalar=-6.0, in1=gl[:],
                                                                                 op0=ALU.max, op1=ALU.mult),
                      reads=[bgl, bln], writes=[bA])
            if bi + 1 < len(blocks):
                emit_transposes(bi + 1, tl1)
                tl0, tl1 = tl1, (emit_loads(bi + 2) if bi + 2 < len(blocks) else None)
            for st in range(4):
                Y = yg[yu % 3]; bY = byg[yu % 3]; yu += 1
                for half in range(2):
                    pc = pC[cu % 2]; bpc = bpC[cu % 2]; cu += 1
                    for k in range(8):
                        kb.op("pe", lambda e, k=k, st=st, half=half, pc=pc, A=A, slot=slot: e.matmul(
                            pc[:], lhsT=A[:, k, st * 128:(st + 1) * 128], rhs=wdn[slot][:, k, half * 512:(half + 1) * 512],
                            start=(k == 0), stop=False), reads=[bA, bwt[slot]], writes=[bpc])
                    kb.op("pe", lambda e, half=half, pc=pc, slot=slot: e.matmul(
                        pc[:], lhsT=ones1[:, :], rhs=bdn[slot][:, half * 512:(half + 1) * 512], start=False, stop=True),
                        reads=[bones, bwt[slot]], writes=[bpc])
                    if half == 0:
                        kb.op("act", lambda e, pc=pc, Y=Y: e.copy(out=Y[:, 0:512], in_=pc[:]), reads=[bpc], writes=[bY])
                    else:
                        kb.op("dve", lambda e, pc=pc, Y=Y: e.tensor_copy(out=Y[:, 512:1024], in_=pc[:]), reads=[bpc], writes=[bY])
                kb.dma("pool", S["YG"][r0 + st * 128:r0 + (st + 1) * 128, :], Y[:], reads=[bY])
        kb.pop()

    def phase2c(self, l, src, dst, dst_name, zero_after):
        kb, I, S, nseq = self.kb, self.I, self.S, self.nseq
        kb.push()
        cap = self.cap
        gate2 = kb.sb("gate2", [128, D], F32); bg2 = Buf()
        xin = [kb.sb("xin%d" % i, [128, D], F32) for i in range(2)]; bxin = [Buf(), Buf()]
        acc = [kb.sb("acc%d" % i, [128, D], F32) for i in range(2)]; bacc = [Buf(), Buf()]
        yb = [kb.sb("yb%d" % i, [128, D], F32) for i in range(8)]; byb = [Buf() for _ in range(8)]
        sl = [kb.sb("sl%d" % i, [128, 4], I32) for i in range(2)]; bsl = [Buf(), Buf()]
        gk = [kb.sb("gkc%d" % i, [128, 4], F32) for i in range(2)]; bgk = [Buf(), Buf()]
        for i in range(8):
            kb.op("pool", lambda e, i=i: e.memset(yb[i][:], 0.0), writes=[byb[i]])
        if zero_after:
            self.zero_xg()
        u = 0
        for s in range(nseq):
            kb.dma("sp", gate2[:], S["MOD"][s, l, 5].partition_broadcast(128), reads=[self.db("MOD", (s, l, 5))], writes=[bg2])
            for t in range(self.ntl):
                ti = s * NT + t
                X = xin[u % 2]; bX = bxin[u % 2]; A = acc[u % 2]; bA = bacc[u % 2]
                SL = sl[u % 2]; bSL = bsl[u % 2]; GK = gk[u % 2]; bGK = bgk[u % 2]
                kb.dma("sp", X[:], src[ti * 128:(ti + 1) * 128, :], reads=[self.db("XA", ti)], writes=[bX])
                kb.dma("sp", SL[:], S["SLOT"][ti], reads=[self.db("SLOT", ti)], writes=[bSL])
                kb.dma("sp", GK[:], S["GK"][ti], reads=[self.db("GK", ti)], writes=[bGK])
                for k in range(4):
                    Yk = yb[(u % 2) * 4 + k]; bYk = byb[(u % 2) * 4 + k]
                    kb.idma(Yk[:], None, S["YG"], SL[:, k:k + 1], 32 * cap - 1, reads=[bSL], writes=[bYk])
                    if k == 0:
                        kb.op("dve", lambda e, Yk=Yk, A=A, GK=GK: e.tensor_scalar(out=A[:], in0=Yk[:], scalar1=GK[:, 0:1], scalar2=None,
                                                                                 op0=ALU.mult), reads=[bYk, bGK], writes=[bA])
                    else:
                        kb.op("dve", lambda e, Yk=Yk, A=A, GK=GK, k=k: e.scalar_tensor_tensor(
                            out=A[:], in0=Yk[:], scalar=GK[:, k:k + 1], in1=A[:], op0=ALU.mult, op1=ALU.add),
                            reads=[bYk, bGK, bA], writes=[bA])
                kb.op("dve", lambda e, A=A: e.tensor_tensor(out=A[:], in0=A[:], in1=gate2[:], op=ALU.mult), reads=[bA, bg2], writes=[bA])
                kb.op("dve", lambda e, A=A, X=X: e.tensor_tensor(out=X[:], in0=A[:], in1=X[:], op=ALU.add), reads=[bA, bX], writes=[bX])
                kb.dma("sp", dst[ti * 128:(ti + 1) * 128, :], X[:], reads=[bX], writes=[self.db(dst_name, ti)])
                u += 1
        kb.pop()

    def phase3(self):
        kb, I, S, nseq = self.kb, self.I, self.S, self.nseq
        kb.push()
        bw = Buf()
        w_qkv = kb.sb("w_qkv", [128, 8, 1280], BF16)
        w_out = kb.sb("w_out", [128, 8, D], BF16)
        self.load_w_bf16(w_qkv, I["swa_w_qkv"], 8, bw)
        self.load_w_bf16(w_out, I["swa_w_out"], 8, bw)
        bqkv = kb.sb("bqkv", [128, 1280], F32)
        bout = kb.sb("bout", [128, D], F32)
        gq = kb.sb("gq", [128, 64], F32)
        gk = kb.sb("gk", [128, 64], F32)
        sk = kb.sb("sk", [128, 16], F32)
        self.bcast_load(bqkv[:], I["swa_b_qkv"], bw)
        self.bcast_load(bout[:], I["swa_b_out"], bw)
        self.bcast_load(gq[:], I["swa_q_head_g"], bw)
        self.bcast_load(gk[:], I["swa_k_head_g"], bw)
        self.bcast_load(sk[:], I["swa_sinks"], bw)
        kb.op("act", lambda e: e.activation(out=sk[:], in_=sk[:], func=AF.Exp), reads=[bw], writes=[bw])
        mask2 = kb.sb("mask2", [128, 4, 2, 128], BF16)
        for hh in range(4):
            kb.op("dve", lambda e, hh=hh: e.tensor_copy(out=mask2[:, hh, 0, :], in_=self.mask_gt[:]), reads=[self.b_const], writes=[bw])
            kb.op("dve", lambda e, hh=hh: e.tensor_copy(out=mask2[:, hh, 1, :], in_=self.mask_le[:]), reads=[self.b_const], writes=[bw])
        r = self.alloc_router(1)

        mods = {n: kb.sb(n, [128, D], F32) for n in ("gmod1", "shift1", "gate1", "gmod2", "shift2")}
        bmod = Buf()
        x_t = [kb.sb("x_t%d" % i, [128, D], F32) for i in range(2)]; bx = [Buf(), Buf()]
        tmp = kb.sb("tmp", [128, D], F32); btmp = Buf()
        h_bf = kb.sb("h_bf", [128, D], BF16); bh = Buf()
        hT = kb.sb("hT", [128, 8, 128], BF16); bhT = Buf()
        ss = kb.sb("ss", [128, 4], F32); bss = Buf()
        qkv = kb.sb("qkv", [128, 20, 64], F32); bqkvs = Buf()
        sq = kb.sb("sq", [128, 18, 64], F32); bsq = Buf()
        r18 = kb.sb("r18", [128, 18], F32); br18 = Buf()
        qn = kb.sb("qn", [128, 18, 64], F32); bqn = Buf()
        rt = [kb.sb("rt%d" % i, [128, 18, 32], F32) for i in range(4)]; brt = Buf()
        q_bf = kb.sb("q_bf", [128, 16, 64], BF16); bqb = Buf()
        kdup = kb.sb("kdup", [128, 2, 2, 64], BF16); bkd = Buf()
        qT_2 = [kb.sb("qT%d" % i, [128, 8, 128], BF16) for i in range(2)]; bqT_2 = [Buf(), Buf()]
        tmpB = kb.sb("tmpB", [128, D], F32); btmpB = Buf()
        kT = [kb.sb("kT%d" % i, [128, 2, 128], BF16) for i in range(3)]; bkT = [Buf() for _ in range(3)]
        Va = [kb.sb("Va%d" % i, [128, 2, 65], BF16) for i in range(3)]; bVa = [Buf() for _ in range(3)]
        bVones = Buf()
        for i in range(3):
            kb.op("pool", lambda e, i=i: e.memset(Va[i][:, :, 64:65], 1.0), writes=[bVones])
        PT = [kb.sb("PT%d" % i, [128, 4, 2, 128], BF16) for i in range(2)]; bPT = [Buf(), Buf()]
        den = kb.sb("den", [128, 16], F32); bden = Buf()
        attn = kb.sb("attn", [128, 16, 64], BF16); battn = Buf()
        attT = kb.sb("attT", [128, 8, 128], BF16); battT = Buf()
        x1 = kb.sb("x1", [128, D], F32); bx1 = Buf()

        tp = [kb.ps("tp%d" % i, [128, 8, 128], BF16) for i in range(2)]; btp = [Buf(), Buf()]
        mm = [kb.ps("mm%d" % i, [128, 512], F32) for i in range(2)]; bmm = [Buf(), Buf()]
        s2 = kb.ps("s2", [128, 2, 512], F32); bs2 = [Buf(), Buf()]
        oo = [kb.ps("oo%d" % i, [128, 512], F32) for i in range(2)]; boo = [Buf(), Buf()]
        tpB = [s2[:, i, :].bitcast(BF16).rearrange("p (k m) -> p k m", m=128) for i in range(2)]
        gc = {"n": 0}
        for s in range(nseq):
            cos, sin, brope = self.rope_tables(s, 32, "k_invf32", "c%d" % s)
            for n_, part in (("gmod1", 1), ("shift1", 0), ("gate1", 2), ("gmod2", 4), ("shift2", 3)):
                kb.dma("sp", mods[n_][:], S["MOD"][s, 1, part].partition_broadcast(128), reads=[self.db("MOD", (s, 1, part))], writes=[bmod])
            def stage_a(t, s=s, cos=cos, sin=sin, brope=brope):
                ti = s * NT + t
                cur, prv = t % 3, (t - 1) % 3
                X = x_t[t % 2]; bX = bx[t % 2]
                qT = qT_2[t % 2]; bqT = bqT_2[t % 2]
                kb.dma("sp", X[:], S["XB"][ti * 128:(ti + 1) * 128, :], reads=[self.db("XB", ti)], writes=[bX])
                self.norm_mod_T(X, bX, mods["gmod1"], mods["shift1"], bmod, tmp, btmp, h_bf, bh, tp[0], btp[0], hT, bhT, ss, bss)
                qkvf = qkv[:].rearrange("p h d -> p (h d)")
                for gi, (c0, c1) in enumerate(((0, 512), (512, 1024), (1024, 1280))):
                    p_ = mm[gi % 2]; bp_ = bmm[gi % 2]
                    for k in range(8):
                        kb.op("pe", lambda e, k=k, c0=c0, c1=c1, p_=p_: e.matmul(p_[:, 0:c1 - c0], lhsT=hT[:, k, :], rhs=w_qkv[:, k, c0:c1],
                                                                              start=(k == 0), stop=(k == 7)), reads=[bhT, bw], writes=[bp_])
                    kb.op("dve", lambda e, c0=c0, c1=c1, p_=p_: e.tensor_tensor(out=qkvf[:, c0:c1], in0=p_[:, 0:c1 - c0], in1=bqkv[:, c0:c1],
                                                                              op=ALU.add), reads=[bp_, bw], writes=[bqkvs])
                kb.op("dve", lambda e: e.tensor_tensor(out=sq[:], in0=qkv[:, 0:18, :], in1=qkv[:, 0:18, :], op=ALU.mult), reads=[bqkvs], writes=[bsq])
                kb.op("dve", lambda e: e.tensor_reduce(out=r18[:], in_=sq[:], axis=AX.X, op=ALU.add), reads=[bsq], writes=[br18])
                kb.op("dve", lambda e: e.tensor_scalar(out=r18[:], in0=r18[:], scalar1=1.0 / 64, scalar2=EPS, op0=ALU.mult, op1=ALU.add),
                      reads=[br18], writes=[br18])
                kb.op("pool", lambda e: e.tensor_tensor(out=r18[:, 0:16], in0=r18[:, 0:16], in1=self.neghalf[:, 0:16], op=ALU.pow),
                      reads=[br18, self.b_const], writes=[br18])
                kb.op("pool", lambda e: e.tensor_tensor(out=r18[:, 16:18], in0=r18[:, 16:18], in1=self.neghalf[:, 0:2], op=ALU.pow),
                      reads=[br18, self.b_const], writes=[br18])
                kb.op("dve", lambda e: e.tensor_tensor(out=qn[:], in0=qkv[:, 0:18, :], in1=bc_last(r18[:, :], 64), op=ALU.mult),
                      reads=[bqkvs, br18], writes=[bqn])
                kb.op("dve", lambda e: e.tensor_tensor(out=qn[:, 0:16, :], in0=qn[:, 0:16, :], in1=bc_mid(gq[:, :], 16), op=ALU.mult),
                      reads=[bqn, bw], writes=[bqn])
                kb.op("dve", lambda e: e.tensor_tensor(out=qn[:, 16:18, :], in0=qn[:, 16:18, :], in1=bc_mid(gk[:, :], 2), op=ALU.mult),
                      reads=[bqn, bw], writes=[bqn])
                cb = bc_mid(cos[:, t, :], 18)
                sb_ = bc_mid(sin[:, t, :], 18)
                x1_ = qn[:, :, 0:32]; x2_ = qn[:, :, 32:64]
                kb.op("dve", lambda e: e.tensor_tensor(out=rt[0][:], in0=x1_, in1=cb, op=ALU.mult), reads=[bqn, brope], writes=[brt])
                kb.op("dve", lambda e: e.tensor_tensor(out=rt[1][:], in0=x2_, in1=sb_, op=ALU.mult), reads=[bqn, brope], writes=[brt])
                kb.op("dve", lambda e: e.tensor_tensor(out=rt[2][:], in0=x2_, in1=cb, op=ALU.mult), reads=[bqn, brope], writes=[brt])
                kb.op("dve", lambda e: e.tensor_tensor(out=rt[3][:], in0=x1_, in1=sb_, op=ALU.mult), reads=[bqn, brope], writes=[brt])
                kb.op("dve", lambda e: e.tensor_tensor(out=q_bf[:, :, 0:32], in0=rt[0][:, 0:16, :], in1=rt[1][:, 0:16, :], op=ALU.subtract),
                      reads=[brt], writes=[bqb])
                kb.op("dve", lambda e: e.tensor_tensor(out=q_bf[:, :, 32:64], in0=rt[2][:, 0:16, :], in1=rt[3][:, 0:16, :], op=ALU.add),
                      reads=[brt], writes=[bqb])
                for dup in range(2):
                    kb.op("dve", lambda e, dup=dup: e.tensor_tensor(out=kdup[:, :, dup, 0:32], in0=rt[0][:, 16:18, :], in1=rt[1][:, 16:18, :],
                                                                   op=ALU.subtract), reads=[brt], writes=[bkd])
                    kb.op("dve", lambda e, dup=dup: e.tensor_tensor(out=kdup[:, :, dup, 32:64], in0=rt[2][:, 16:18, :], in1=rt[3][:, 16:18, :],
                                                                    op=ALU.add), reads=[brt], writes=[bkd])
                kb.op("act", lambda e, cur=cur: e.copy(out=Va[cur][:, :, 0:64], in_=qkv[:, 18:20, :]), reads=[bqkvs, bVones], writes=[bVa[cur]])
                for i in range(8):
                    kb.op("pe", lambda e, i=i: e.transpose(out=tp[1][:, i, :], in_=q_bf[:, 2 * i:2 * i + 2, :].rearrange("p h d -> p (h d)"),
                                                           identity=self.ident_b[:]), reads=[bqb, self.b_const], writes=[btp[1]])
                kb.op("act", lambda e: e.copy(out=qT[:], in_=tp[1][:]), reads=[btp[1]], writes=[bqT])
                for g in range(2):
                    kb.op("pe", lambda e, g=g: e.transpose(out=tp[0][:, g, :], in_=kdup[:, g, :, :].rearrange("p a d -> p (a d)"),
                                                           identity=self.ident_b[:]), reads=[bkd, self.b_const], writes=[btp[0]])
                kb.op("dve", lambda e, cur=cur: e.tensor_copy(out=kT[cur][:], in_=tp[0][:, 0:2, :]), reads=[btp[0]], writes=[bkT[cur]])
            def stage_b(t, s=s):
                ti = s * NT + t
                cur, prv = t % 3, (t - 1) % 3
                X = x_t[t % 2]; bX = bx[t % 2]
                qT = qT_2[t % 2]; bqT = bqT_2[t % 2]
                tmp = tmpB; btmp = btmpB
                for gq4 in range(4):
                    sbank = s2
                    bsb = bs2
                    P = PT[gc["n"] % 2]; bP = bPT[gc["n"] % 2]
                    ob = oo[gc["n"] % 2]; bob = boo[gc["n"] % 2]
                    gc["n"] += 1
                    for hh in range(4):
                        hq = gq4 * 4 + hh
                        i, o = hq // 2, (hq % 2) * 64
                        g = hq // 8
                        for w_, kt in ((0, prv), (1, cur)):
                            if t == 0 and w_ == 0:
                                continue
                            col = ((hh % 2) * 2 + hh // 2) * 256 + w_ * 128
                            kb.op("pe", lambda e, i=i, o=o, g=g, kt=kt, col=col, sbank=sbank: e.matmul(
                                sbank[:, col // 512, col % 512:col % 512 + 128], lhsT=kT[kt][o:o + 64, g, :], rhs=qT[o:o + 64, i, :],
                                start=True, stop=True), reads=[bkT[kt], bqT], writes=[bsb[col // 512]])
                    for bk in range(2):
                        if t == 0:
                            for hh2 in range(2):
                                kb.op("act", lambda e, bk=bk, hh2=hh2, P=P, sbank=sbank: e.activation(
                                    out=P[:, bk * 2 + hh2, 1, :], in_=sbank[:, bk, hh2 * 256 + 128:hh2 * 256 + 256], func=AF.Exp, scale=0.125),
                                    reads=[bsb[bk]], writes=[bP])
                        else:
                            kb.op("act", lambda e, bk=bk, P=P, sbank=sbank: e.activation(
                                out=P[:, bk * 2:bk * 2 + 2, :, :].rearrange("p a b c -> p (a b c)"), in_=sbank[:, bk, :], func=AF.Exp, scale=0.125),
                                reads=[bsb[bk]], writes=[bP])
                    if t == 0:
                        kb.op("dve", lambda e, P=P: e.tensor_tensor(out=P[:, :, 1, :], in0=P[:, :, 1, :], in1=mask2[:, :, 1, :], op=ALU.mult),
                              reads=[bP, bw], writes=[bP])
                    else:
                        kb.op("dve", lambda e, P=P: e.tensor_tensor(out=P[:].rearrange("p a b c -> p (a b c)"), in0=P[:].rearrange("p a b c -> p (a b c)"),
                                                                   in1=mask2[:].rearrange("p a b c -> p (a b c)"), op=ALU.mult),
                              reads=[bP, bw], writes=[bP])
                    for hh in range(4):
                        hq = gq4 * 4 + hh
                        g = hq // 8
                        sl = (hh % 2) * 2 + hh // 2
                        if t > 0:
                            kb.op("pe", lambda e, hh=hh, g=g, P=P, ob=ob, prv=prv, sl=sl: e.matmul(ob[:, hh * 65:hh * 65 + 65], lhsT=P[:, sl, 0, :],
                                                                                          rhs=Va[prv][:, g, :], start=True, stop=False),
                                  reads=[bP, bVa[prv]], writes=[bob])
                        kb.op("pe", lambda e, hh=hh, g=g, P=P, ob=ob, cur=cur, sl=sl: e.matmul(ob[:, hh * 65:hh * 65 + 65], lhsT=P[:, sl, 1, :],
                                                                                      rhs=Va[cur][:, g, :], start=(t == 0), stop=True),
                              reads=[bP, bVa[cur]], writes=[bob])
                    ov = ob[:, 0:260].rearrange("p (h d) -> p h d", d=65)
                    dsl = den[:, gq4 * 4:(gq4 + 1) * 4]
                    kb.op("dve", lambda e, ov=ov, dsl=dsl, gq4=gq4: e.tensor_tensor(out=dsl, in0=ov[:, :, 64], in1=sk[:, gq4 * 4:(gq4 + 1) * 4], op=ALU.add),
                          reads=[bob, bw], writes=[bden])
                    kb.op("dve", lambda e, dsl=dsl: e.reciprocal(out=dsl, in_=dsl), reads=[bden], writes=[bden])
                    kb.op("dve", lambda e, ov=ov, dsl=dsl, gq4=gq4: e.tensor_tensor(out=attn[:, gq4 * 4:(gq4 + 1) * 4, :], in0=ov[:, :, 0:64],
                                                                                 in1=bc_last(dsl, 64), op=ALU.mult), reads=[bob, bden], writes=[battn])
                af = attn[:].rearrange("p h d -> p (h d)")
                for k in range(8):
                    kb.op("pe", lambda e, k=k: e.transpose(out=tpB[0][:, k, :], in_=af[:, k * 128:(k + 1) * 128], identity=self.ident_b[:]),
                          reads=[battn, self.b_const], writes=[bs2[0]])
                kb.op("act", lambda e: e.copy(out=attT[:], in_=tpB[0]), reads=[bs2[0]], writes=[battT])
                for half in range(2):
                    hs = slice(half * 512, (half + 1) * 512)
                    for k in range(8):
                        kb.op("pe", lambda e, k=k, half=half, hs=hs: e.matmul(oo[half][:], lhsT=attT[:, k, :], rhs=w_out[:, k, hs],
                                                                            start=(k == 0), stop=(k == 7)), reads=[battT, bw], writes=[boo[half]])
                    kb.op("dve", lambda e, half=half, hs=hs: e.tensor_tensor(out=tmp[:, hs], in0=oo[half][:], in1=bout[:, hs], op=ALU.add),
                          reads=[boo[half], bw], writes=[btmp])
                    kb.op("dve", lambda e, hs=hs: e.tensor_tensor(out=tmp[:, hs], in0=tmp[:, hs], in1=mods["gate1"][:, hs], op=ALU.mult),
                          reads=[btmp, bmod], writes=[btmp])
                    kb.op("dve", lambda e, hs=hs: e.tensor_tensor(out=x1[:, hs], in0=tmp[:, hs], in1=X[:, hs], op=ALU.add),
                          reads=[btmp, bX], writes=[bx1])
                kb.dma("sp", S["XA"][ti * 128:(ti + 1) * 128, :], x1[:], reads=[bx1], writes=[self.db("XA", ti)])
                self.norm2_router(r, x1, bx1, mods["gmod2"], mods["shift2"], bmod, tmp, btmp, tpB, bs2, oo[0], boo[0], ti)
            kb.pipeline(stage_a, stage_b, self.ntl)
        kb.pop()

    def build(self):
        self.setup_consts()
        ph = self.phases
        if "p0" in ph:
            self.phase0()
        if "p1a" in ph:
            self.phase1a()
        if "p1b" in ph:
            self.phase1b()
        if "p2a" in ph:
            self.phase2e(0)
            self.phase2c(0, self.S["XA"], self.S["XB"], "XB", False)
        if "p3" in ph:
            self.phase3()
        if "p2b" in ph:
            self.phase2e(1)
            self.phase2c(1, self.S["XA"], self.out, "OUT", False)
        self.kb.finish()
        return self.nc


def module_consts():
    idx = np.arange(128, dtype=np.float64)
    lg = np.log1p(-np.exp2(-5.0 - np.arange(8, dtype=np.float64)))
    k = {}
    k["k_invf16"] = (10000.0 ** (-np.arange(16, dtype=np.float32) / 16)).astype(np.float32)
    k["k_invf32"] = (10000.0 ** (-np.arange(32, dtype=np.float32) / 32)).astype(np.float32)
    diff = idx[None, :] - idx[:, None]
    dec = np.where(diff[:, None, :] >= 0, np.exp(lg[None, :, None] * np.maximum(diff[:, None, :], 0.0)), 0.0)
    k["k_decayT"] = np.ascontiguousarray(dec.reshape(128, 4, 2, 128).transpose(0, 2, 1, 3)).reshape(128, 8 * 128).astype(np.float32)
    k["k_qdec"] = np.exp(lg[None, :] * (idx + 1.0)[:, None]).astype(np.float32)
    k["k_kdec"] = np.exp(lg[None, :] * (127.0 - idx)[:, None]).astype(np.float32)
    cd = np.zeros((128, 4), np.float64)
    for i in range(4):
        cd[0:64, i] = np.exp(lg[2 * i] * 128)
        cd[64:128, i] = np.exp(lg[2 * i + 1] * 128)
    k["k_cdec"] = cd.astype(np.float32)
    return k


def make_in_maps(inputs, nseq, n_cores):
    f = lambda a: np.ascontiguousarray(np.asarray(a))
    shared = {}
    for name in ("ada_w", "ada_b", "norm1_g", "norm2_g", "router_w", "router_b", "exp_w_gu", "exp_w_down", "exp_b_down"):
        shared[name] = f(inputs[name])
    for name in ("hyb_w_in", "mla_cq_norm_g", "mla_ckv_norm_g", "mla_w_uq", "mla_w_ukv", "mla_q_head_g", "mla_k_head_g",
                 "hyb_w_out", "swa_w_qkv", "swa_b_qkv", "swa_q_head_g", "swa_k_head_g", "swa_sinks", "swa_w_out", "swa_b_out"):
        shared[name] = f(np.asarray(inputs[name])[0])
    shared["ret_norm_g"] = f(np.asarray(inputs["ret_norm_g"])[0].reshape(512))
    bgu = np.asarray(inputs["exp_b_gu"])
    shared["exp_b_gu_pj"] = f(bgu.reshape(2, 32, 16, 128).transpose(0, 1, 3, 2))
    shared.update(module_consts())
    x = np.asarray(inputs["x"]); c = np.asarray(inputs["c"]); pos = np.asarray(inputs["positions"])
    maps = []
    for i in range(n_cores):
        b0 = i * nseq
        m = dict(shared)
        m["x"] = f(x[b0:b0 + nseq].reshape(nseq * SEQ, D))
        m["c_pk"] = f(c[b0:b0 + nseq].reshape(nseq, 8, 128).transpose(0, 2, 1))
        m["pos_pt"] = f(pos[b0:b0 + nseq].reshape(nseq, NT, 128).transpose(0, 2, 1).astype(np.int32))
        maps.append(m)
    return maps


_PROG = {}


def kernel(**inputs):
    nseq = 32 // N_CORES
    if "nc" not in _PROG:
        _PROG["nc"] = Prog(nseq).build()
    maps = make_in_maps(inputs, nseq, N_CORES)
    res = run_bass_kernel_spmd(_PROG["nc"], maps, core_ids=list(range(N_CORES)))
    out = np.concatenate([np.asarray(r["out"]).reshape(nseq, SEQ, D) for r in res.results], axis=0)
    return out.astype(np.float32)
```

```python
import contextlib
import os
import math
import numpy as np
import concourse.bass as bass
import concourse.mybir as mybir
from concourse.bass_utils import run_bass_kernel_spmd

F32 = mybir.dt.float32
BF16 = mybir.dt.bfloat16
I32 = mybir.dt.int32
AF = mybir.ActivationFunctionType
ALU = mybir.AluOpType
AX = mybir.AxisListType

SAME_ENGINE_SYNC = os.environ.get('KSES', '1') == '1'
DMA_RING = 12
N_CORES = 8
_STOP = float(os.environ.get('KSTOP', '99'))
SEQ = 2048
D = 1024
NT = SEQ // 128
EPS = 1e-6
PI = math.pi


class Buf:
    __slots__ = ("w", "rs")

    def __init__(self):
        self.w = None
        self.rs = {}


class KB:
    ENGS = ("pe", "act", "dve", "pool", "sp")

    def __init__(self, nc):
        self.nc = nc
        self.stacks = [contextlib.ExitStack()]
        self.eng = dict(pe=nc.tensor, act=nc.scalar, dve=nc.vector, pool=nc.gpsimd, sp=nc.sync)
        self.cnt = {e: 0 for e in self.ENGS}
        self.seen = {e: {} for e in self.ENGS}
        self.sems = {}
        for e in self.ENGS:
            self.sems[e] = self.stacks[0].enter_context(nc.semaphore("s_" + e))
        self.dma_n = {}
        for e in ("sp", "pool", "act"):
            self.dma_n[e] = 0
            for j in range(DMA_RING):
                self.sems[("d", e, j)] = self.stacks[0].enter_context(nc.semaphore("d_%s_%d" % (e, j)))
        self.uid = 0

    def push(self):
        self.phase_id = getattr(self, "phase_id", 0) + 1
        self.stacks.append(contextlib.ExitStack())

    def pop(self):
        self.barrier()
        self.stacks.pop().close()

    def sb(self, name, shape, dt):
        self.uid += 1
        return self.stacks[-1].enter_context(self.nc.sbuf_tensor("%s_%d" % (name, self.uid), list(shape), dt))

    def ps(self, name, shape, dt):
        self.uid += 1
        return self.stacks[-1].enter_context(self.nc.psum_tensor("%s_%d" % (name, self.uid), list(shape), dt))

    def _deps(self, e, reads, writes):
        toks = {}
        for b in reads:
            if b.w is not None and toks.get(b.w[0], 0) < b.w[1]:
                toks[b.w[0]] = b.w[1]
        for b in writes:
            if b.w is not None and toks.get(b.w[0], 0) < b.w[1]:
                toks[b.w[0]] = b.w[1]
            for k, v in b.rs.items():
                if toks.get(k, 0) < v:
                    toks[k] = v
        waits = []
        seen = self.seen[e]
        for k, v in toks.items():
            if k == e and (e == "pe" or not SAME_ENGINE_SYNC):
                continue
            if seen.get(k, 0) < v:
                seen[k] = v
                waits.append((k, v))
        return waits

    def _mark(self, tok, reads, writes):
        k, v = tok
        for b in reads:
            if b.rs.get(k, 0) < v:
                b.rs[k] = v
        for b in writes:
            b.w = tok
            b.rs = {}

    def _emit(self, e, waits, fn, key, inc):
        engine = self.eng[e]
        for k, v in waits:
            engine.wait_ge(self.sems[k], v)
        if fn is not None:
            fn(engine).then_inc(self.sems[key], inc)

    def op(self, e, fn, reads=(), writes=()):
        waits = self._deps(e, reads, writes)
        self.cnt[e] += 1
        tok = (e, self.cnt[e])
        self._emit(e, waits, fn, e, 1)
        self._mark(tok, reads, writes)
        self._handoff()
        return tok

    def dma(self, e, out, in_, reads=(), writes=(), **kw):
        waits = self._deps(e, reads, writes)
        n = self.dma_n[e]
        self.dma_n[e] += 1
        key = ("d", e, n % DMA_RING)
        val = 16 * (n // DMA_RING + 1)
        if n >= DMA_RING and self.seen[e].get(key, 0) < val - 16:
            self.seen[e][key] = val - 16
            waits.append((key, val - 16))
        tok = (key, val)
        self._emit(e, waits, (lambda eng: eng.dma_start(out=out, in_=in_, **kw)), key, 16)
        self._mark(tok, reads, writes)
        self._handoff()
        return tok

    def idma(self, out, out_idx, in_, in_idx, bound, reads=(), writes=()):
        e = "pool"
        waits = self._deps(e, reads, writes)
        n = self.dma_n[e]
        self.dma_n[e] += 1
        key = ("d", e, n % DMA_RING)
        val = 16 * (n // DMA_RING + 1)
        if n >= DMA_RING and self.seen[e].get(key, 0) < val - 16:
            self.seen[e][key] = val - 16
            waits.append((key, val - 16))
        if not hasattr(self, "_bregs"):
            self._bregs = {}
        if bound not in self._bregs:
            self._bregs[bound] = self.nc.gpsimd.to_reg(bound)
        bound = self._bregs[bound]
        oo_ = bass.IndirectOffsetOnAxis(ap=out_idx, axis=0) if out_idx is not None else None
        io_ = bass.IndirectOffsetOnAxis(ap=in_idx, axis=0) if in_idx is not None else None
        self._emit(e, waits, (lambda eng: eng.indirect_dma_start(out=out, out_offset=oo_, in_=in_, in_offset=io_,
                                                                 bounds_check=bound, oob_is_err=False)), key, 16)
        self._mark((key, val), reads, writes)
        self._handoff()

    def _handoff(self):
        st = getattr(self, "_il", None)
        if st is None:
            return
        me = getattr(st["tls"], "idx", None)
        if me is None:
            return
        cv = st["cv"]
        with cv:
            if st["alive"][1 - me]:
                st["turn"] = 1 - me
                cv.notify_all()
                while st["turn"] != me:
                    cv.wait()

    def interleave(self, fa, fb):
        import threading
        if fa is None or fb is None:
            (fa or fb)()
            return
        st = dict(cv=threading.Condition(), turn=0, alive=[True, True], tls=threading.local(), err=[])
        self._il = st

        def runner(i, f):
            cv = st["cv"]
            with cv:
                while st["turn"] != i:
                    cv.wait()
            st["tls"].idx = i
            try:
                f()
            except BaseException as ex:
                st["err"].append(ex)
            finally:
                with cv:
                    st["alive"][i] = False
                    st["turn"] = 1 - i
                    cv.notify_all()

        ths = [threading.Thread(target=runner, args=(i, f)) for i, f in enumerate((fa, fb))]
        for th in ths:
            th.start()
        for th in ths:
            th.join()
        self._il = None
        if st["err"]:
            raise st["err"][0]

    def pipeline(self, stage_a, stage_b, n):
        stage_a(0)
        for t in range(n):
            self.interleave((lambda t=t: stage_a(t + 1)) if t + 1 < n else None, lambda t=t: stage_b(t))

    def all_tokens(self):
        toks = [(e, self.cnt[e]) for e in self.ENGS if self.cnt[e] > 0]
        for e in ("sp", "pool", "act"):
            n = self.dma_n[e]
            for j in range(DMA_RING):
                c = (n - j + DMA_RING - 1) // DMA_RING if n > j else 0
                if c > 0:
                    toks.append((("d", e, j), 16 * c))
        return toks

    def barrier(self):
        toks = self.all_tokens()
        for e in self.ENGS:
            waits = []
            for k, v in toks:
                if k == e:
                    continue
                if self.seen[e].get(k, 0) < v:
                    self.seen[e][k] = v
                    waits.append((k, v))
            self._emit(e, waits, None, None, 0)

    def finish(self):
        self.barrier()
        while self.stacks:
            self.stacks.pop().close()


def bc_mid(ap2, n):
    return ap2.unsqueeze(1).broadcast_to([ap2.shape[0], n, ap2.shape[1]])


def bc_last(ap2, n):
    return ap2.unsqueeze(2).broadcast_to([ap2.shape[0], ap2.shape[1], n])


class Prog:
    def __init__(self, nseq, debug=False, phases=("p0", "p1a", "p1b", "p2a", "p3", "p2b"), ntl=NT, cap_tiles=None):
        self.nseq = nseq
        self.ntl = ntl
        if cap_tiles is None:
            mean = nseq * ntl * 128 * 4 // 32
            cap_tiles = max(4, 4 * ((2 * mean + 511) // 512))
        self.cap = cap_tiles * 128
        self.debug = debug
        self.phases = phases
        nc = self.nc = bass.Bass("TRN2", target_bir_lowering=False)
        self.kb = KB(nc)
        ntok = nseq * SEQ
        self.ntok = ntok

        def inp(name, shape, dt=F32):
            return nc.dram_tensor(name, list(shape), dt, kind="ExternalInput").ap()

        def scr(name, shape, dt=F32):
            kind = "ExternalOutput" if (debug is True or (debug and name in debug)) else "Internal"
            return nc.dram_tensor(name, list(shape), dt, kind=kind).ap()

        I = self.I = {}
        I["x"] = inp("x", [ntok, D])
        I["c_pk"] = inp("c_pk", [nseq, 128, 8])
        I["pos_pt"] = inp("pos_pt", [nseq, 128, NT], I32)
        I["ada_w"] = inp("ada_w", [2, D, 6 * D])
        I["ada_b"] = inp("ada_b", [2, 6 * D])
        I["norm1_g"] = inp("norm1_g", [2, D])
        I["norm2_g"] = inp("norm2_g", [2, D])
        I["hyb_w_in"] = inp("hyb_w_in", [D, 2720])
        I["mla_cq_norm_g"] = inp("mla_cq_norm_g", [384])
        I["mla_ckv_norm_g"] = inp("mla_ckv_norm_g", [256])
        I["mla_w_uq"] = inp("mla_w_uq", [384, 768])
        I["mla_w_ukv"] = inp("mla_w_ukv", [256, 1024])
        I["mla_q_head_g"] = inp("mla_q_head_g", [96])
        I["mla_k_head_g"] = inp("mla_k_head_g", [96])
        I["ret_norm_g"] = inp("ret_norm_g", [512])
        I["hyb_w_out"] = inp("hyb_w_out", [D, D])
        I["swa_w_qkv"] = inp("swa_w_qkv", [D, 1280])
        I["swa_b_qkv"] = inp("swa_b_qkv", [1280])
        I["swa_q_head_g"] = inp("swa_q_head_g", [64])
        I["swa_k_head_g"] = inp("swa_k_head_g", [64])
        I["swa_sinks"] = inp("swa_sinks", [16])
        I["swa_w_out"] = inp("swa_w_out", [D, D])
        I["swa_b_out"] = inp("swa_b_out", [D])
        I["router_w"] = inp("router_w", [2, D, 32])
        I["router_b"] = inp("router_b", [2, 32])
        I["exp_w_gu"] = inp("exp_w_gu", [2, 32, D, 2048])
        I["exp_b_gu_pj"] = inp("exp_b_gu_pj", [2, 32, 128, 16])
        I["exp_w_down"] = inp("exp_w_down", [2, 32, D, D])
        I["exp_b_down"] = inp("exp_b_down", [2, 32, D])
        I["k_invf16"] = inp("k_invf16", [16])
        I["k_invf32"] = inp("k_invf32", [32])
        I["k_decayT"] = inp("k_decayT", [128, 8 * 128])
        I["k_qdec"] = inp("k_qdec", [128, 8])
        I["k_kdec"] = inp("k_kdec", [128, 8])
        I["k_cdec"] = inp("k_cdec", [128, 4])

        S = self.S = {}
        S["MOD"] = scr("MOD", [nseq, 2, 6, D])
        S["ATT"] = scr("ATT", [nseq * NT, 128, 512], BF16)
        S["XA"] = scr("XA", [ntok, D])
        S["XB"] = scr("XB", [ntok, D])
        S["H2T"] = scr("H2T", [nseq * NT, 128, 8, 128], BF16)
        S["GS"] = scr("GS", [nseq * NT, 128, 32])
        S["XG"] = scr("XG", [32 * self.cap, D], BF16)
        S["YG"] = scr("YG", [32 * self.cap, D])
        S["SLOT"] = scr("SLOT", [nseq * NT, 128, 4], I32)
        S["GK"] = scr("GK", [nseq * NT, 128, 4])
        self.out = nc.dram_tensor("out", [ntok, D], F32, kind="ExternalOutput").ap()
        self.dbufs = {}

    def db(self, name, idx):
        k = (name, idx)
        if k not in self.dbufs:
            self.dbufs[k] = Buf()
        return self.dbufs[k]

    def setup_consts(self):
        kb = self.kb
        self.ident_b = kb.sb("ident_b", [128, 128], BF16)
        self.ident_f = kb.sb("ident_f", [128, 128], F32)
        self.b_const = Buf()
        bc = self.b_const
        for idt in (self.ident_b, self.ident_f):
            kb.op("pool", lambda e, idt=idt: e.memset(idt[:], 1.0), writes=[bc])
            kb.op("pool", lambda e, idt=idt: e.affine_select(out=idt[:], in_=idt[:], pattern=[[-1, 128]],
                                                             compare_op=ALU.is_equal, fill=0.0, base=0,
                                                             channel_multiplier=1), reads=[bc], writes=[bc])
        self.mask_le = kb.sb("mask_le", [128, 128], BF16)
        self.mask_gt = kb.sb("mask_gt", [128, 128], BF16)
        kb.op("pool", lambda e: e.memset(self.mask_le[:], 1.0), writes=[bc])
        kb.op("pool", lambda e: e.affine_select(out=self.mask_le[:], in_=self.mask_le[:], pattern=[[1, 128]],
                                                compare_op=ALU.is_ge, fill=0.0, base=0, channel_multiplier=-1),
              reads=[bc], writes=[bc])
        kb.op("pool", lambda e: e.memset(self.mask_gt[:], 1.0), writes=[bc])
        kb.op("pool", lambda e: e.affine_select(out=self.mask_gt[:], in_=self.mask_gt[:], pattern=[[-1, 128]],
                                                compare_op=ALU.is_gt, fill=0.0, base=0, channel_multiplier=1),
              reads=[bc], writes=[bc])
        self.U_b = kb.sb("U_b", [128, 128], BF16)
        self.ones_b = kb.sb("ones_b", [128, 128], BF16)
        kb.op("pool", lambda e: e.memset(self.ones_b[:], 1.0), writes=[bc])
        kb.op("pool", lambda e: e.memset(self.U_b[:], 1.0), writes=[bc])
        kb.op("pool", lambda e: e.affine_select(out=self.U_b[:], in_=self.U_b[:], pattern=[[1, 128]],
                                                compare_op=ALU.is_ge, fill=0.0, base=-1, channel_multiplier=-1),
              reads=[bc], writes=[bc])
        iot_i = kb.sb("iot_i", [128, 32], I32)
        self.iotaE = kb.sb("iotaE", [128, 32], F32)
        kb.op("pool", lambda e: e.iota(out=iot_i[:], pattern=[[1, 32]], base=0, channel_multiplier=0), writes=[bc])
        kb.op("dve", lambda e: e.tensor_copy(out=self.iotaE[:], in_=iot_i[:]), reads=[bc], writes=[bc])
        kb.op("dve", lambda e: e.tensor_scalar(out=self.iotaE[:], in0=self.iotaE[:], scalar1=float(self.cap), scalar2=None,
                                               op0=ALU.mult), reads=[bc], writes=[bc])
        self.neghalf = kb.sb("neghalf", [128, 16], F32)
        kb.op("pool", lambda e: e.memset(self.neghalf[:], -0.5), writes=[bc])

    def rstd_of(self, ss, n, width, bss, tag):
        kb = self.kb
        kb.op("dve", lambda e: e.tensor_scalar(out=ss, in0=ss, scalar1=1.0 / n, scalar2=EPS,
                                               op0=ALU.mult, op1=ALU.add), reads=[bss], writes=[bss])
        kb.op("pool", lambda e: e.tensor_tensor(out=ss, in0=ss, in1=self.neghalf[:, 0:width], op=ALU.pow),
              reads=[bss, self.b_const], writes=[bss])

    def rope_tables(self, s, half, invf_name, tag):
        kb, I = self.kb, self.I
        key = (kb.phase_id, half)
        if not hasattr(self, "_rope"):
            self._rope = {}
        if key not in self._rope:
            self._rope[key] = dict(
                b=Buf(),
                pos_i=kb.sb("pos_i", [128, NT], I32), pos_f=kb.sb("pos_f", [128, NT], F32), invf=kb.sb("invf", [128, half], F32),
                ang=kb.sb("ang", [128, NT, half], F32), kq=kb.sb("kq", [128, NT, half], F32), ki=kb.sb("ki", [128, NT, half], I32),
                ys=kb.sb("ys", [128, NT, half], F32), mm=kb.sb("mmk", [128, NT, half], F32),
                cos=kb.sb("cos", [128, NT, half], F32), sin=kb.sb("sin", [128, NT, half], F32))
        R_ = self._rope[key]
        b = R_["b"]
        pos_i, pos_f, invf, ang, kq, ki, ys, mm, cos, sin = (R_[n] for n in ("pos_i", "pos_f", "invf", "ang", "kq", "ki", "ys", "mm", "cos", "sin"))
        kb.dma("sp", pos_i[:], I["pos_pt"][s], writes=[b])
        kb.dma("sp", invf[:], I[invf_name].partition_broadcast(128), writes=[b])
        kb.op("dve", lambda e: e.tensor_copy(out=pos_f[:], in_=pos_i[:]), reads=[b], writes=[b])
        kb.op("dve", lambda e: e.tensor_tensor(out=ang[:], in0=bc_last(pos_f[:, :], half), in1=bc_mid(invf[:, :], NT),
                                               op=ALU.mult), reads=[b], writes=[b])
        kb.op("dve", lambda e: e.tensor_scalar(out=kq[:], in0=ang[:], scalar1=1.0 / (2 * PI), scalar2=None,
                                               op0=ALU.mult), reads=[b], writes=[b])
        kb.op("dve", lambda e: e.tensor_copy(out=ki[:], in_=kq[:]), reads=[b], writes=[b])
        kb.op("dve", lambda e: e.tensor_copy(out=kq[:], in_=ki[:]), reads=[b], writes=[b])
        kb.op("dve", lambda e: e.scalar_tensor_tensor(out=ang[:], in0=kq[:], scalar=-2 * PI, in1=ang[:],
                                                      op0=ALU.mult, op1=ALU.add), reads=[b], writes=[b])
        lim = 3.1415925
        for shift, dst in ((0.0, sin), (PI / 2, cos)):
            kb.op("dve", lambda e, shift=shift: e.tensor_scalar(out=ys[:], in0=ang[:], scalar1=shift, scalar2=None,
                                                                op0=ALU.add), reads=[b], writes=[b])
            kb.op("dve", lambda e: e.tensor_scalar(out=mm[:], in0=ys[:], scalar1=PI, scalar2=-2 * PI,
                                                   op0=ALU.is_gt, op1=ALU.mult), reads=[b], writes=[b])
            kb.op("dve", lambda e: e.tensor_tensor(out=ys[:], in0=ys[:], in1=mm[:], op=ALU.add), reads=[b], writes=[b])
            kb.op("dve", lambda e: e.tensor_scalar(out=ys[:], in0=ys[:], scalar1=lim, scalar2=-lim,
                                                   op0=ALU.min, op1=ALU.max), reads=[b], writes=[b])
            kb.op("act", lambda e, dst=dst: e.activation(out=dst[:], in_=ys[:], func=AF.Sin), reads=[b], writes=[b])
        return cos, sin, b

    def load_w_bf16(self, dst, src, kchunks, bw):
        v = src.rearrange("(k p) n -> p k n", p=128)
        for k in range(kchunks):
            self.kb.dma("pool", dst[:, k, :], v[:, k, :], writes=[bw])

    def bcast_load(self, dst, src1d, b):
        self.kb.dma("sp", dst, src1d.partition_broadcast(128), writes=[b])

    def norm_mod_T(self, x_t, bx, gmod, shift, bmod, tmp, btmp, h_bf, bh, tp, btp, hT, bhT, ss, bss):
        kb = self.kb
        kb.op("dve", lambda e: e.scalar_tensor_tensor(out=tmp[:], in0=x_t[:], scalar=1.0, in1=x_t[:], op0=ALU.mult, op1=ALU.mult, accum_out=ss[:, 0:1]),
              reads=[bx], writes=[btmp, bss])
        self.rstd_of(ss[:, 0:1], D, 1, bss, "")
        kb.op("dve", lambda e: e.scalar_tensor_tensor(out=tmp[:], in0=x_t[:], scalar=ss[:, 0:1], in1=gmod[:],
                                                      op0=ALU.mult, op1=ALU.mult), reads=[bx, bss, bmod], writes=[btmp])
        kb.op("dve", lambda e: e.tensor_tensor(out=h_bf[:], in0=tmp[:], in1=shift[:], op=ALU.add),
              reads=[btmp, bmod], writes=[bh])
        for k in range(8):
            kb.op("pe", lambda e, k=k: e.transpose(out=tp[:, k, :], in_=h_bf[:, k * 128:(k + 1) * 128],
                                                   identity=self.ident_b[:]), reads=[bh, self.b_const], writes=[btp])
        kb.op("act", lambda e: e.copy(out=hT[:], in_=tp[:]), reads=[btp], writes=[bhT])

    def phase0(self):
        kb, I, S, nseq = self.kb, self.I, self.S, self.nseq
        kb.push()
        cin = kb.sb("cin", [128, nseq, 8], F32)
        cact = kb.sb("cact", [128, 8, nseq], F32)
        bcr = Buf()
        for s in range(nseq):
            kb.dma("sp", cin[:, s, :], I["c_pk"][s], writes=[bcr])
        kb.op("act", lambda e: e.activation(out=cact[:].rearrange("p k s -> p s k"), in_=cin[:], func=AF.Silu), reads=[bcr], writes=[bcr])
        adab = kb.sb("adab", [nseq, 6 * D], F32)
        ng = [kb.sb("ng1", [nseq, D], F32), kb.sb("ng2", [nseq, D], F32)]
        bab = Buf()
        wch = [kb.sb("wch%d" % i, [128, 8, 512], F32) for i in range(3)]
        bwch = [Buf() for _ in range(3)]
        pm = [kb.ps("p0pm%d" % i, [128, 512], F32) for i in range(2)]
        bpm = [Buf(), Buf()]
        modt = [kb.sb("modt%d" % i, [nseq, 512], F32) for i in range(3)]
        bmodt = [Buf() for _ in range(3)]
        n = 0
        for l in range(2):
            kb.dma("sp", adab[:], I["ada_b"][l].partition_broadcast(nseq), writes=[bab])
            kb.dma("sp", ng[0][:], I["norm1_g"][l].partition_broadcast(nseq), writes=[bab])
            kb.dma("sp", ng[1][:], I["norm2_g"][l].partition_broadcast(nseq), writes=[bab])
            for j in range(12):
                wv = I["ada_w"][l][:, j * 512:(j + 1) * 512].rearrange("(k p) n -> p k n", p=128)
                w_ = wch[n % 3]
                kb.dma("sp", w_[:], wv, writes=[bwch[n % 3]])
                p_ = pm[n % 2]
                for k in range(8):
                    kb.op("pe", lambda e, k=k, p_=p_, w_=w_: e.matmul(p_[0:nseq, :], lhsT=cact[:, k, :], rhs=w_[:, k, :],
                                                                    start=(k == 0), stop=(k == 7)),
                          reads=[bcr, bwch[n % 3]], writes=[bpm[n % 2]])
                m_ = modt[n % 3]
                bm_ = bmodt[n % 3]
                kb.op("dve", lambda e, p_=p_, m_=m_, j=j: e.tensor_tensor(out=m_[:], in0=p_[0:nseq, :],
                                                                       in1=adab[:, j * 512:(j + 1) * 512], op=ALU.add),
                      reads=[bpm[n % 2], bab], writes=[bm_])
                part, half = j // 2, j % 2
                if part in (1, 4):
                    g_ = ng[0] if part == 1 else ng[1]
                    kb.op("dve", lambda e, m_=m_, g_=g_, half=half: e.scalar_tensor_tensor(
                        out=m_[:], in0=m_[:], scalar=1.0, in1=g_[:, half * 512:(half + 1) * 512],
                        op0=ALU.add, op1=ALU.mult), reads=[bm_, bab], writes=[bm_])
                for s in range(nseq):
                    kb.dma("sp", S["MOD"][s, l, part, half * 512:(half + 1) * 512], m_[s:s + 1, :],
                           reads=[bm_], writes=[self.db("MOD", (s, l, part))])
                n += 1
        kb.pop()

    def phase1a(self):
        kb, I, S, nseq = self.kb, self.I, self.S, self.nseq
        kb.push()
        self.zero_xg()
        bw = Buf()
        w_in = kb.sb("w_in_a", [128, 8, 672], BF16)
        w_uq = kb.sb("w_uq", [128, 3, 768], BF16)
        w_ukv = kb.sb("w_ukv", [128, 2, 1024], BF16)
        self.load_w_bf16(w_in, I["hyb_w_in"][:, 0:672], 8, bw)
        self.load_w_bf16(w_uq, I["mla_w_uq"], 3, bw)
        self.load_w_bf16(w_ukv, I["mla_w_ukv"], 2, bw)
        gcq = kb.sb("gcq", [128, 384], F32)
        gckv = kb.sb("gckv", [128, 256], F32)
        gq = kb.sb("gq", [128, 96], F32)
        gk = kb.sb("gk", [128, 96], F32)
        self.bcast_load(gcq[:], I["mla_cq_norm_g"], bw)
        self.bcast_load(gckv[:], I["mla_ckv_norm_g"], bw)
        self.bcast_load(gq[:], I["mla_q_head_g"], bw)
        self.bcast_load(gk[:], I["mla_k_head_g"], bw)

        kT = kb.sb("kT", [96, 8, SEQ], BF16)
        V = kb.sb("Vc", [128, NT, 8, 65], BF16)
        bkT = [Buf() for _ in range(NT)]
        bV = [Buf() for _ in range(NT)]
        bVones = Buf()
        kb.op("pool", lambda e: e.memset(V[:, :, :, 64:65], 1.0), writes=[bVones])

        gmod = kb.sb("gmod1", [128, D], F32)
        shift = kb.sb("shift1", [128, D], F32)
        bmod = Buf()
        x_t = [kb.sb("x_t%d" % i, [128, D], F32) for i in range(2)]
        bx = [Buf(), Buf()]
        tmp = kb.sb("tmp", [128, D], F32); btmp = Buf()
        h_bf = kb.sb("h_bf", [128, D], BF16); bh = Buf()
        hT = kb.sb("hT", [128, 8, 128], BF16); bhT = Buf()
        ss = kb.sb("ss", [128, 4], F32); bss = Buf()
        proj = kb.sb("proj", [128, 672], F32); bproj = Buf()
        sq = kb.sb("sq", [128, 8, 96], F32); bsq = Buf()
        cqn = kb.sb("cqn", [128, 384], BF16); bcqn = Buf()
        cqT = kb.sb("cqT", [128, 3, 128], BF16); bcqT = Buf()
        ckvn = kb.sb("ckvn", [128, 256], BF16); bckvn = Buf()
        ckvT = kb.sb("ckvT", [128, 2, 128], BF16); bckvT = Buf()
        q_sb = kb.sb("q_sb", [128, 8, 96], F32); bq = Buf()
        qn = kb.sb("qn", [128, 8, 96], F32); bqn = Buf()
        rq8 = kb.sb("rq8", [128, 16], F32); brq8 = Buf()
        R = kb.sb("Rr", [128, 8, 96], F32); bR = Buf()
        q_full = kb.sb("q_full", [128, 8, 96], BF16); bqf = Buf()
        qTs = [kb.sb("qT%d" % i, [96, 8, 128], BF16) for i in range(2)]; bqTs = [Buf(), Buf()]
        kv_sb = kb.sb("kv_sb", [128, 8, 128], F32); bkv = Buf()
        k_full = kb.sb("k_full", [128, 8, 96], BF16); bkf = Buf()
        kr = kb.sb("kr", [128, 32], F32); bkr = Buf()
        kr2 = kb.sb("kr2", [128, 32], F32)
        rt = [kb.sb("rt%d" % i, [128, 8, 16], F32) for i in range(4)]; brt = Buf()
        PT = [kb.sb("PT%d" % i, [128, 4, 128], BF16) for i in range(3)]; bPT = [Buf() for _ in range(3)]
        attn = kb.sb("attn", [128, 8, 64], BF16); battn = Buf()
        rden = kb.sb("rden", [128, 8], F32); brden = Buf()

        tp = [kb.ps("tp%d" % i, [128, 8, 128], BF16) for i in range(2)]; btp = [Buf(), Buf()]
        mm = [kb.ps("mm%d" % i, [128, 512], F32) for i in range(2)]; bmm = [Buf(), Buf()]
        s2 = kb.ps("s2", [128, 2, 512], F32); bs2 = [Buf(), Buf()]
        oo = [kb.ps("oo%d" % i, [128, 512], F32) for i in range(2)]; boo = [Buf(), Buf()]
        scale = 96.0 ** -0.5
        uc = {"n": 0}
        for s in range(nseq):
            cos, sin, brope = self.rope_tables(s, 16, "k_invf16", "a%d" % s)
            kb.dma("sp", gmod[:], S["MOD"][s, 0, 1].partition_broadcast(128), reads=[self.db("MOD", (s, 0, 1))], writes=[bmod])
            kb.dma("sp", shift[:], S["MOD"][s, 0, 0].partition_broadcast(128), reads=[self.db("MOD", (s, 0, 0))], writes=[bmod])
            def stage_a(t, s=s, cos=cos, sin=sin, brope=brope):
                X = x_t[t % 2]; bX = bx[t % 2]
                qT = qTs[t % 2]; bqT = bqTs[t % 2]
                kb.dma("sp", X[:], I["x"][(s * NT + t) * 128:(s * NT + t + 1) * 128, :], writes=[bX])
                self.norm_mod_T(X, bX, gmod, shift, bmod, tmp, btmp, h_bf, bh, tp[0], btp[0], hT, bhT, ss, bss)
                for gi, (c0, c1) in enumerate(((0, 512), (512, 672))):
                    for k in range(8):
                        kb.op("pe", lambda e, k=k, gi=gi, c0=c0, c1=c1: e.matmul(mm[gi][:, 0:c1 - c0], lhsT=hT[:, k, :],
                                                                              rhs=w_in[:, k, c0:c1], start=(k == 0), stop=(k == 7)),
                              reads=[bhT, bw], writes=[bmm[gi]])
                    kb.op("act", lambda e, gi=gi, c0=c0, c1=c1: e.copy(out=proj[:, c0:c1], in_=mm[gi][:, 0:c1 - c0]),
                          reads=[bmm[gi]], writes=[bproj])
                for ci, (c0, w) in enumerate(((0, 384), (384, 256), (640, 32))):
                    kb.op("dve", lambda e, c0=c0, w=w, ci=ci: e.scalar_tensor_tensor(out=tmp[:, 0:w], in0=proj[:, c0:c0 + w], scalar=1.0, in1=proj[:, c0:c0 + w], op0=ALU.mult, op1=ALU.mult, accum_out=ss[:, 1 + ci:2 + ci]), reads=[bproj], writes=[btmp, bss])
                kb.op("dve", lambda e: e.tensor_scalar(out=ss[:, 1:2], in0=ss[:, 1:2], scalar1=1.0 / 384, scalar2=EPS,
                                                       op0=ALU.mult, op1=ALU.add), reads=[bss], writes=[bss])
                kb.op("dve", lambda e: e.tensor_scalar(out=ss[:, 2:3], in0=ss[:, 2:3], scalar1=1.0 / 256, scalar2=EPS,
                                                       op0=ALU.mult, op1=ALU.add), reads=[bss], writes=[bss])
                kb.op("dve", lambda e: e.tensor_scalar(out=ss[:, 3:4], in0=ss[:, 3:4], scalar1=1.0 / 32, scalar2=EPS,
                                                       op0=ALU.mult, op1=ALU.add), reads=[bss], writes=[bss])
                kb.op("pool", lambda e: e.tensor_tensor(out=ss[:, 1:4], in0=ss[:, 1:4], in1=self.neghalf[:, 0:3], op=ALU.pow),
                      reads=[bss, self.b_const], writes=[bss])
                kb.op("dve", lambda e: e.scalar_tensor_tensor(out=cqn[:], in0=proj[:, 0:384], scalar=ss[:, 1:2], in1=gcq[:],
                                                              op0=ALU.mult, op1=ALU.mult), reads=[bproj, bss, bw], writes=[bcqn])
                kb.op("dve", lambda e: e.scalar_tensor_tensor(out=ckvn[:], in0=proj[:, 384:640], scalar=ss[:, 2:3], in1=gckv[:],
                                                              op0=ALU.mult, op1=ALU.mult), reads=[bproj, bss, bw], writes=[bckvn])
                kb.op("dve", lambda e: e.scalar_tensor_tensor(out=kr[:], in0=proj[:, 640:672], scalar=ss[:, 3:4], in1=gk[:, 64:96],
                                                              op0=ALU.mult, op1=ALU.mult), reads=[bproj, bss, bw], writes=[bkr])
                for k in range(3):
                    kb.op("pe", lambda e, k=k: e.transpose(out=tp[1][:, k, :], in_=cqn[:, k * 128:(k + 1) * 128],
                                                           identity=self.ident_b[:]), reads=[bcqn, self.b_const], writes=[btp[1]])
                for k in range(2):
                    kb.op("pe", lambda e, k=k: e.transpose(out=tp[1][:, 3 + k, :], in_=ckvn[:, k * 128:(k + 1) * 128],
                                                           identity=self.ident_b[:]), reads=[bckvn, self.b_const], writes=[btp[1]])
                kb.op("act", lambda e: e.copy(out=cqT[:], in_=tp[1][:, 0:3, :]), reads=[btp[1]], writes=[bcqT])
                kb.op("act", lambda e: e.copy(out=ckvT[:], in_=tp[1][:, 3:5, :]), reads=[btp[1]], writes=[bckvT])
                for gi, (c0, c1) in enumerate(((0, 512), (512, 768))):
                    for k in range(3):
                        kb.op("pe", lambda e, k=k, gi=gi, c0=c0, c1=c1: e.matmul(mm[gi][:, 0:c1 - c0], lhsT=cqT[:, k, :],
                                                                              rhs=w_uq[:, k, c0:c1], start=(k == 0), stop=(k == 2)),
                              reads=[bcqT, bw], writes=[bmm[gi]])
                    kb.op("act", lambda e, gi=gi, c0=c0, c1=c1: e.copy(
                        out=q_sb[:].rearrange("p h d -> p (h d)")[:, c0:c1], in_=mm[gi][:, 0:c1 - c0]),
                        reads=[bmm[gi]], writes=[bq])
                kb.op("dve", lambda e: e.tensor_tensor(out=sq[:], in0=q_sb[:], in1=q_sb[:], op=ALU.mult), reads=[bq], writes=[bsq])
                kb.op("dve", lambda e: e.tensor_reduce(out=rq8[:, 0:8], in_=sq[:, :, 0:64], axis=AX.X, op=ALU.add),
                      reads=[bsq], writes=[brq8])
                kb.op("dve", lambda e: e.tensor_reduce(out=rq8[:, 8:16], in_=sq[:, :, 64:96], axis=AX.X, op=ALU.add),
                      reads=[bsq], writes=[brq8])
                kb.op("dve", lambda e: e.tensor_scalar(out=rq8[:, 0:8], in0=rq8[:, 0:8], scalar1=1.0 / 64, scalar2=EPS,
                                                       op0=ALU.mult, op1=ALU.add), reads=[brq8], writes=[brq8])
                kb.op("dve", lambda e: e.tensor_scalar(out=rq8[:, 8:16], in0=rq8[:, 8:16], scalar1=1.0 / 32, scalar2=EPS,
                                                       op0=ALU.mult, op1=ALU.add), reads=[brq8], writes=[brq8])
                kb.op("pool", lambda e: e.tensor_tensor(out=rq8[:], in0=rq8[:], in1=self.neghalf[:, 0:16], op=ALU.pow),
                      reads=[brq8, self.b_const], writes=[brq8])
                kb.op("dve", lambda e: e.tensor_tensor(out=qn[:, :, 0:64], in0=q_sb[:, :, 0:64], in1=bc_last(rq8[:, 0:8], 64),
                                                       op=ALU.mult), reads=[bq, brq8], writes=[bqn])
                kb.op("dve", lambda e: e.tensor_tensor(out=qn[:, :, 64:96], in0=q_sb[:, :, 64:96], in1=bc_last(rq8[:, 8:16], 32),
                                                       op=ALU.mult), reads=[bq, brq8], writes=[bqn])
                kb.op("dve", lambda e: e.tensor_tensor(out=qn[:], in0=qn[:], in1=bc_mid(gq[:, :], 8), op=ALU.mult),
                      reads=[bqn, bw], writes=[bqn])
                kb.op("act", lambda e: e.copy(out=q_full[:, :, 0:64], in_=qn[:, :, 0:64]), reads=[bqn], writes=[bqf])
                cb = bc_mid(cos[:, t, :], 8)
                sb_ = bc_mid(sin[:, t, :], 8)
                x1 = qn[:, :, 64:80]
                x2 = qn[:, :, 80:96]
                kb.op("dve", lambda e: e.tensor_tensor(out=rt[0][:], in0=x1, in1=cb, op=ALU.mult), reads=[bqn, brope], writes=[brt])
                kb.op("dve", lambda e: e.tensor_tensor(out=rt[1][:], in0=x2, in1=sb_, op=ALU.mult), reads=[bqn, brope], writes=[brt])
                kb.op("dve", lambda e: e.tensor_tensor(out=rt[2][:], in0=x2, in1=cb, op=ALU.mult), reads=[bqn, brope], writes=[brt])
                kb.op("dve", lambda e: e.tensor_tensor(out=rt[3][:], in0=x1, in1=sb_, op=ALU.mult), reads=[bqn, brope], writes=[brt])
                kb.op("dve", lambda e: e.tensor_tensor(out=q_full[:, :, 64:80], in0=rt[0][:], in1=rt[1][:], op=ALU.subtract),
                      reads=[brt], writes=[bqf])
                kb.op("dve", lambda e: e.tensor_tensor(out=q_full[:, :, 80:96], in0=rt[2][:], in1=rt[3][:], op=ALU.add),
                      reads=[brt], writes=[bqf])
                for h in range(8):
                    kb.op("pe", lambda e, h=h: e.transpose(out=tp[0][0:96, h, :], in_=q_full[:, h, :], identity=self.ident_b[:]),
                          reads=[bqf, self.b_const], writes=[btp[0]])
                kb.op("act", lambda e: e.copy(out=qT[:], in_=tp[0][0:96, :, :]), reads=[btp[0]], writes=[bqT])
                for gi in range(2):
                    for k in range(2):
                        kb.op("pe", lambda e, k=k, gi=gi: e.matmul(mm[gi][:], lhsT=ckvT[:, k, :], rhs=w_ukv[:, k, gi * 512:(gi + 1) * 512],
                                                                 start=(k == 0), stop=(k == 1)), reads=[bckvT, bw], writes=[bmm[gi]])
                    kb.op("act", lambda e, gi=gi: e.copy(out=kv_sb[:].rearrange("p h d -> p (h d)")[:, gi * 512:(gi + 1) * 512],
                                                        in_=mm[gi][:]), reads=[bmm[gi]], writes=[bkv])
                kb.op("dve", lambda e, t=t: e.tensor_copy(out=V[:, t, :, 0:64], in_=kv_sb[:, :, 64:128]),
                      reads=[bkv, bVones], writes=[bV[t]])
                kb.op("dve", lambda e: e.tensor_tensor(out=sq[:, :, 0:64], in0=kv_sb[:, :, 0:64], in1=kv_sb[:, :, 0:64], op=ALU.mult),
                      reads=[bkv], writes=[bsq])
                kb.op("dve", lambda e: e.tensor_reduce(out=rq8[:, 0:8], in_=sq[:, :, 0:64], axis=AX.X, op=ALU.add),
                      reads=[bsq], writes=[brq8])
                kb.op("dve", lambda e: e.tensor_scalar(out=rq8[:, 0:8], in0=rq8[:, 0:8], scalar1=1.0 / 64, scalar2=EPS,
                                                       op0=ALU.mult, op1=ALU.add), reads=[brq8], writes=[brq8])
                kb.op("pool", lambda e: e.tensor_tensor(out=rq8[:, 0:8], in0=rq8[:, 0:8], in1=self.neghalf[:, 0:8], op=ALU.pow),
                      reads=[brq8, self.b_const], writes=[brq8])
                kb.op("dve", lambda e: e.tensor_tensor(out=sq[:, :, 0:64], in0=kv_sb[:, :, 0:64], in1=bc_last(rq8[:, 0:8], 64),
                                                       op=ALU.mult), reads=[bkv, brq8], writes=[bsq])
                kb.op("dve", lambda e: e.tensor_tensor(out=k_full[:, :, 0:64], in0=sq[:, :, 0:64], in1=bc_mid(gk[:, 0:64], 8),
                                                        op=ALU.mult), reads=[bsq, bw], writes=[bkf])
                c1_ = cos[:, t, :]
                s1_ = sin[:, t, :]
                kb.op("dve", lambda e: e.tensor_tensor(out=rt[0][:, 0, :], in0=kr[:, 0:16], in1=c1_, op=ALU.mult), reads=[bkr, brope], writes=[brt])
                kb.op("dve", lambda e: e.tensor_tensor(out=rt[1][:, 0, :], in0=kr[:, 16:32], in1=s1_, op=ALU.mult), reads=[bkr, brope], writes=[brt])
                kb.op("dve", lambda e: e.tensor_tensor(out=rt[2][:, 0, :], in0=kr[:, 16:32], in1=c1_, op=ALU.mult), reads=[bkr, brope], writes=[brt])
                kb.op("dve", lambda e: e.tensor_tensor(out=rt[3][:, 0, :], in0=kr[:, 0:16], in1=s1_, op=ALU.mult), reads=[bkr, brope], writes=[brt])
                kb.op("dve", lambda e: e.tensor_tensor(out=kr2[:, 0:16], in0=rt[0][:, 0, :], in1=rt[1][:, 0, :], op=ALU.subtract),
                      reads=[brt], writes=[bkr])
                kb.op("dve", lambda e: e.tensor_tensor(out=kr2[:, 16:32], in0=rt[2][:, 0, :], in1=rt[3][:, 0, :], op=ALU.add),
                      reads=[brt], writes=[bkr])
                kb.op("dve", lambda e: e.tensor_copy(out=k_full[:, :, 64:96], in_=bc_mid(kr2[:, :], 8)), reads=[bkr], writes=[bkf])
                for h in range(8):
                    kb.op("pe", lambda e, h=h: e.transpose(out=tp[1][0:96, h, :], in_=k_full[:, h, :], identity=self.ident_b[:]),
                          reads=[bkf, self.b_const], writes=[btp[1]])
                kb.op("act", lambda e, t=t: e.copy(out=kT[:, :, t * 128:(t + 1) * 128], in_=tp[1][0:96, :, :]),
                      reads=[btp[1]], writes=[bkT[t]])
            def stage_b(t, s=s):
                qT = qTs[t % 2]; bqT = bqTs[t % 2]
                units = []
                for h in range(8):
                    for a in range(0, t + 1, 4):
                        units.append((h, a, min(a + 4, t + 1)))

                def emit_S(ui, u):
                    h, a, b = u
                    bank = ui % 2
                    for kt in range(a, b):
                        kb.op("pe", lambda e, kt=kt, h=h, a=a, bank=bank: e.matmul(
                            s2[:, bank, (kt - a) * 128:(kt - a + 1) * 128], lhsT=kT[:, h, kt * 128:(kt + 1) * 128],
                            rhs=qT[:, h, :], start=True, stop=True), reads=[bkT[kt], bqT], writes=[bs2[bank]])

                base = uc["n"]
                emit_S(base, units[0])
                for i, u in enumerate(units):
                    ui = base + i
                    h, a, b = u
                    if i + 1 < len(units):
                        emit_S(ui + 1, units[i + 1])
                    bank = ui % 2
                    P = PT[ui % 3]; bP = bPT[ui % 3]
                    n = (b - a) * 128
                    kb.op("act", lambda e, P=P, bank=bank, n=n: e.activation(
                        out=P[:].rearrange("p a b -> p (a b)")[:, 0:n], in_=s2[:, bank, 0:n], func=AF.Exp, scale=scale),
                        reads=[bs2[bank]], writes=[bP])
                    if b == t + 1:
                        kb.op("dve", lambda e, P=P, j=t - a: e.tensor_tensor(out=P[:, j, :], in0=P[:, j, :], in1=self.mask_le[:],
                                                                           op=ALU.mult), reads=[bP, self.b_const], writes=[bP])
                    ob = oo[h // 4]
                    for kt in range(a, b):
                        kb.op("pe", lambda e, kt=kt, h=h, a=a, P=P, ob=ob: e.matmul(
                            ob[:, (h % 4) * 65:(h % 4) * 65 + 65], lhsT=P[:, kt - a, :], rhs=V[:, kt, h, :],
                            start=(kt == 0), stop=(kt == t)), reads=[bP, bV[kt], bVones], writes=[boo[h // 4]])
                uc["n"] += len(units)
                for hb in range(2):
                    ov = oo[hb][:, 0:260].rearrange("p (h d) -> p h d", d=65)
                    kb.op("dve", lambda e, hb=hb, ov=ov: e.reciprocal(out=rden[:, hb * 4:(hb + 1) * 4], in_=ov[:, :, 64]),
                          reads=[boo[hb]], writes=[brden])
                    kb.op("dve", lambda e, hb=hb, ov=ov: e.tensor_tensor(out=attn[:, hb * 4:(hb + 1) * 4, :], in0=ov[:, :, 0:64],
                                                                        in1=bc_last(rden[:, hb * 4:(hb + 1) * 4], 64), op=ALU.mult),
                          reads=[boo[hb], brden], writes=[battn])
                kb.dma("sp", S["ATT"][s * NT + t], attn[:].rearrange("p h d -> p (h d)"), reads=[battn],
                       writes=[self.db("ATT", s * NT + t)])
            kb.pipeline(stage_a, stage_b, self.ntl)
        kb.pop()

    def alloc_router(self, l):
        kb, I = self.kb, self.I
        r = {}
        r["bw"] = Buf()
        r["rw"] = kb.sb("rw", [128, 8, 32], F32)
        kb.dma("sp", r["rw"][:], I["router_w"][l].rearrange("(k p) n -> p k n", p=128), writes=[r["bw"]])
        r["rb"] = kb.sb("rb", [128, 32], F32)
        self.bcast_load(r["rb"][:], I["router_b"][l], r["bw"])
        r["rwh"] = kb.sb("rwh", [128, 8, 32], BF16)
        r["rwl"] = kb.sb("rwl", [128, 8, 32], BF16)
        kb.op("dve", lambda e: e.tensor_copy(out=r["rwh"][:], in_=r["rw"][:]), reads=[r["bw"]], writes=[r["bw"]])
        kb.op("dve", lambda e: e.tensor_tensor(out=r["rwl"][:], in0=r["rw"][:], in1=r["rwh"][:], op=ALU.subtract),
              reads=[r["bw"]], writes=[r["bw"]])
        r["h2f"] = kb.sb("h2f", [128, D], F32); r["bh2f"] = Buf()
        r["h2hi"] = kb.sb("h2hi", [128, D], BF16); r["bh2hi"] = Buf()
        r["h2lo"] = kb.sb("h2lo", [128, D], BF16); r["bh2lo"] = Buf()
        r["h2Tl"] = kb.sb("h2Tl", [128, 8, 128], BF16); r["bh2Tl"] = Buf()
        r["h2Tb"] = kb.sb("h2Tb", [128, 8, 128], BF16); r["bh2Tb"] = Buf()
        r["lg"] = kb.sb("lg", [128, 32], F32); r["blg"] = Buf()
        r["m8"] = kb.sb("m8", [128, 8], F32)
        r["msk"] = kb.sb("msk", [128, 32], F32)
        r["ex"] = kb.sb("ex", [128, 32], F32)
        r["den"] = kb.sb("den", [128, 2], F32)
        r["G"] = kb.sb("Gt", [128, 32], F32); r["bG"] = Buf()
        r["ss"] = kb.sb("ss2", [128, 1], F32); r["bss"] = Buf()
        r["cnt"] = kb.sb("cnt_b", [128, 32], F32); r["bcnt"] = Buf()
        kb.op("pool", lambda e: e.memset(r["cnt"][:], 0.0), writes=[r["bcnt"]])
        r["Mb"] = kb.sb("Mb", [128, 32], BF16)
        for n_ in ("posf", "valid", "slotm", "oh", "junk", "Gv"):
            r[n_] = kb.sb(n_, [128, 32], F32)
        r["slotf"] = kb.sb("slotf", [128, 4], F32)
        r["sloti"] = kb.sb("sloti", [128, 4], I32); r["bsloti"] = Buf()
        r["gk"] = kb.sb("gk", [128, 4], F32); r["bgk"] = Buf()
        r["brt"] = Buf()
        return r

    def norm2_router(self, r, x1, bx1, gmod2, shift2, bmod, tmp, btmp, tp, btp, mmp, bmmp, tile_idx):
        kb, S = self.kb, self.S
        ss, bss = r["ss"], r["bss"]
        kb.op("dve", lambda e: e.scalar_tensor_tensor(out=tmp[:], in0=x1[:], scalar=1.0, in1=x1[:], op0=ALU.mult, op1=ALU.mult, accum_out=ss[:, 0:1]),
              reads=[bx1], writes=[btmp, bss])
        self.rstd_of(ss[:, 0:1], D, 1, bss, "")
        kb.op("dve", lambda e: e.scalar_tensor_tensor(out=tmp[:], in0=x1[:], scalar=ss[:, 0:1], in1=gmod2[:],
                                                      op0=ALU.mult, op1=ALU.mult), reads=[bx1, bss, bmod], writes=[btmp])
        kb.op("dve", lambda e: e.tensor_tensor(out=r["h2f"][:], in0=tmp[:], in1=shift2[:], op=ALU.add),
              reads=[btmp, bmod], writes=[r["bh2f"]])
        kb.op("act", lambda e: e.copy(out=r["h2hi"][:], in_=r["h2f"][:]), reads=[r["bh2f"]], writes=[r["bh2hi"]])
        kb.op("dve", lambda e: e.tensor_tensor(out=r["h2lo"][:], in0=r["h2f"][:], in1=r["h2hi"][:], op=ALU.subtract),
              reads=[r["bh2f"], r["bh2hi"]], writes=[r["bh2lo"]])
        for k in range(8):
            kb.op("pe", lambda e, k=k: e.transpose(out=tp[0][:, k, :], in_=r["h2hi"][:, k * 128:(k + 1) * 128],
                                                   identity=self.ident_b[:]), reads=[r["bh2hi"], self.b_const], writes=[btp[0]])
        for k in range(8):
            kb.op("pe", lambda e, k=k: e.transpose(out=tp[1][:, k, :], in_=r["h2lo"][:, k * 128:(k + 1) * 128],
                                                   identity=self.ident_b[:]), reads=[r["bh2lo"], self.b_const], writes=[btp[1]])
        kb.op("act", lambda e: e.copy(out=r["h2Tb"][:], in_=tp[0][:]), reads=[btp[0]], writes=[r["bh2Tb"]])
        kb.op("dve", lambda e: e.tensor_copy(out=r["h2Tl"][:], in_=tp[1][:]), reads=[btp[1]], writes=[r["bh2Tl"]])
        if self.debug:
            kb.dma("sp", S["H2T"][tile_idx], r["h2Tb"][:], reads=[r["bh2Tb"]], writes=[self.db("H2T", tile_idx)])
        if _STOP <= 6:
            return
        passes = [("h2Tb", "rwh"), ("h2Tl", "rwh"), ("h2Tb", "rwl")]
        for pi, (a_, w_) in enumerate(passes):
            for k in range(8):
                kb.op("pe", lambda e, k=k, a_=a_, w_=w_, pi=pi: e.matmul(mmp[:, 0:32], lhsT=r[a_][:, k, :], rhs=r[w_][:, k, :],
                                                                       start=(pi == 0 and k == 0), stop=(pi == 2 and k == 7)),
                      reads=[r["bh2Tb"], r["bh2Tl"], r["bw"]], writes=[bmmp])
        lg, m8, msk, ex, den, G = r["lg"], r["m8"], r["msk"], r["ex"], r["den"], r["G"]
        bl = r["blg"]
        kb.op("dve", lambda e: e.tensor_tensor(out=lg[:], in0=mmp[:, 0:32], in1=r["rb"][:], op=ALU.add),
              reads=[bmmp, r["bw"]], writes=[bl])
        if _STOP <= 7:
            return
        kb.op("dve", lambda e: e.max(out=m8[:], in_=lg[:]), reads=[bl], writes=[bl])
        kb.op("dve", lambda e: e.tensor_scalar(out=msk[:], in0=lg[:], scalar1=m8[:, 3:4], scalar2=None, op0=ALU.is_ge),
              reads=[bl], writes=[bl])
        kb.op("dve", lambda e: e.tensor_scalar(out=den[:, 1:2], in0=m8[:, 0:1], scalar1=-1.0, scalar2=None, op0=ALU.mult),
              reads=[bl], writes=[bl])
        kb.op("act", lambda e: e.activation(out=ex[:], in_=lg[:], func=AF.Exp, bias=den[:, 1:2], scale=1.0),
              reads=[bl], writes=[bl])
        kb.op("dve", lambda e: e.scalar_tensor_tensor(out=ex[:], in0=ex[:], scalar=1.0, in1=msk[:], op0=ALU.mult, op1=ALU.mult, accum_out=den[:, 0:1]), reads=[bl], writes=[bl])
        kb.op("dve", lambda e: e.reciprocal(out=den[:, 0:1], in_=den[:, 0:1]), reads=[bl], writes=[bl])
        kb.op("dve", lambda e: e.tensor_scalar(out=G[:], in0=ex[:], scalar1=den[:, 0:1], scalar2=None, op0=ALU.mult),
              reads=[bl], writes=[r["bG"]])
        if self.debug:
            kb.dma("sp", S["GS"][tile_idx], G[:], reads=[r["bG"]], writes=[self.db("GS", tile_idx)])
        cap = self.cap
        brt = r["brt"]
        kb.op("dve", lambda e: e.tensor_copy(out=r["Mb"][:], in_=msk[:]), reads=[bl], writes=[brt])
        kb.op("pe", lambda e: e.matmul(mmp[:, 32:64], lhsT=self.U_b[:], rhs=r["Mb"][:], start=True, stop=True),
              reads=[brt, self.b_const], writes=[bmmp])
        kb.op("pe", lambda e: e.matmul(mmp[:, 64:96], lhsT=self.ones_b[:], rhs=r["Mb"][:], start=True, stop=True),
              reads=[brt, self.b_const], writes=[bmmp])
        kb.op("dve", lambda e: e.tensor_tensor(out=r["posf"][:], in0=mmp[:, 32:64], in1=r["cnt"][:], op=ALU.add),
              reads=[bmmp, r["bcnt"]], writes=[brt])
        kb.op("dve", lambda e: e.tensor_tensor(out=r["cnt"][:], in0=mmp[:, 64:96], in1=r["cnt"][:], op=ALU.add),
              reads=[bmmp, r["bcnt"]], writes=[r["bcnt"]])
        kb.op("dve", lambda e: e.tensor_scalar(out=r["valid"][:], in0=r["posf"][:], scalar1=float(cap), scalar2=None, op0=ALU.is_lt),
              reads=[brt], writes=[brt])
        kb.op("dve", lambda e: e.tensor_tensor(out=r["slotm"][:], in0=r["posf"][:], in1=self.iotaE[:], op=ALU.add),
              reads=[brt, self.b_const], writes=[brt])
        kb.op("dve", lambda e: e.tensor_scalar(out=r["junk"][:], in0=r["valid"][:], scalar1=-1.0e6, scalar2=1.0e6,
                                               op0=ALU.mult, op1=ALU.add), reads=[brt], writes=[brt])
        kb.op("dve", lambda e: e.tensor_tensor(out=r["slotm"][:], in0=r["slotm"][:], in1=r["junk"][:], op=ALU.add),
              reads=[brt], writes=[brt])
        kb.op("dve", lambda e: e.tensor_tensor(out=r["Gv"][:], in0=G[:], in1=r["valid"][:], op=ALU.mult),
              reads=[brt, r["bG"]], writes=[brt])
        for k in range(4):
            kb.op("dve", lambda e, k=k: e.tensor_scalar(out=r["oh"][:], in0=lg[:], scalar1=m8[:, k:k + 1], scalar2=None, op0=ALU.is_equal),
                  reads=[bl, brt], writes=[brt])
            kb.op("dve", lambda e, k=k: e.scalar_tensor_tensor(out=r["junk"][:], in0=r["oh"][:], scalar=1.0, in1=r["slotm"][:],
                                                              op0=ALU.mult, op1=ALU.mult, accum_out=r["slotf"][:, k:k + 1]),
                  reads=[brt], writes=[brt])
            kb.op("dve", lambda e, k=k: e.scalar_tensor_tensor(out=r["junk"][:], in0=r["oh"][:], scalar=1.0, in1=r["Gv"][:],
                                                              op0=ALU.mult, op1=ALU.mult, accum_out=r["gk"][:, k:k + 1]),
                  reads=[brt, r["bgk"]], writes=[brt, r["bgk"]])
        kb.op("dve", lambda e: e.tensor_copy(out=r["sloti"][:], in_=r["slotf"][:]), reads=[brt, r["bsloti"]], writes=[r["bsloti"]])
        kb.dma("sp", S["SLOT"][tile_idx], r["sloti"][:], reads=[r["bsloti"]], writes=[self.db("SLOT", tile_idx)])
        kb.dma("sp", S["GK"][tile_idx], r["gk"][:], reads=[r["bgk"]], writes=[self.db("GK", tile_idx)])
        for k in range(4):
            kb.idma(S["XG"], r["sloti"][:, k:k + 1], r["h2hi"][:], None, 32 * cap - 1, reads=[r["bsloti"], r["bh2hi"]])

    def phase1b(self):
        kb, I, S, nseq = self.kb, self.I, self.S, self.nseq
        kb.push()
        bw = Buf()
        w_in = kb.sb("w_in_b", [128, 8, 2048], BF16)
        w_out = kb.sb("w_out", [128, 8, D], BF16)
        self.load_w_bf16(w_in, I["hyb_w_in"][:, 672:2720], 8, bw)
        self.load_w_bf16(w_out, I["hyb_w_out"], 8, bw)
        retg = kb.sb("retg", [128, 512], F32)
        self.bcast_load(retg[:], I["ret_norm_g"], bw)
        decT = kb.sb("decT", [128, 8 * 128], F32)
        qdec = kb.sb("qdec", [128, 8], F32)
        kdec = kb.sb("kdec", [128, 8], F32)
        cdec = kb.sb("cdec", [128, 4], F32)
        kb.dma("sp", decT[:], I["k_decayT"], writes=[bw])
        kb.dma("sp", qdec[:], I["k_qdec"], writes=[bw])
        kb.dma("sp", kdec[:], I["k_kdec"], writes=[bw])
        kb.dma("sp", cdec[:], I["k_cdec"], writes=[bw])
        r = self.alloc_router(0)

        mods = {n: kb.sb(n, [128, D], F32) for n in ("gmod1", "shift1", "gate1", "gmod2", "shift2")}
        bmod = Buf()
        x_t = [kb.sb("x_t%d" % i, [128, D], F32) for i in range(2)]; bx = [Buf(), Buf()]
        tmp = kb.sb("tmp", [128, D], F32); btmp = Buf()
        h_bf = kb.sb("h_bf", [128, D], BF16); bh = Buf()
        hT = kb.sb("hT", [128, 8, 128], BF16); bhT = Buf()
        ss = kb.sb("ss", [128, 4], F32); bss = Buf()
        raw = [kb.sb("raw%d" % i, [128, 8, 64], F32) for i in range(2)]; braw = [Buf(), Buf()]
        rr = [kb.sb("rr%d" % i, [128, 8, 64], F32) for i in range(2)]; brr = [Buf(), Buf()]
        rt = [kb.sb("rt%d" % i, [128, 8, 32], F32) for i in range(4)]; brt = Buf()
        rq_bf = kb.sb("rq_bf", [128, 8, 64], BF16); brqb = Buf()
        rqd_bf = kb.sb("rqd_bf", [128, 8, 64], BF16); brqd = Buf()
        rk_bf = kb.sb("rk_bf", [128, 8, 64], BF16); brkb = Buf()
        rkd_bf_2 = [kb.sb("rkd_bf%d" % i, [128, 8, 64], BF16) for i in range(2)]; brkd_2 = [Buf(), Buf()]
        v_bf_2 = [kb.sb("v_bf%d" % i, [128, 8, 64], BF16) for i in range(2)]; bv_2 = [Buf(), Buf()]
        sg_2 = [kb.sb("sg%d" % i, [128, 512], F32) for i in range(2)]; bsg_2 = [Buf(), Buf()]
        rqT_2 = [kb.sb("rqT%d" % i, [128, 8, 128], BF16) for i in range(2)]; brqT_2 = [Buf(), Buf()]
        rkT_2 = [kb.sb("rkT%d" % i, [128, 4, 128], BF16) for i in range(2)]; brkT_2 = [Buf(), Buf()]
        Sd = kb.sb("Sd", [128, 8, 128], BF16); bSd = Buf()
        st_f = kb.sb("st_f", [128, 4, 128], F32); bstf = Buf()
        st_b = kb.sb("st_b", [128, 4, 128], BF16); bstb = Buf()
        kb.op("pool", lambda e: e.memset(st_f[:], 0.0), writes=[bstf])
        o_sb = kb.sb("o_sb", [128, 8, 64], F32); bo = Buf()
        oc = kb.sb("oc", [128, 8, 64], F32); boc = Buf()
        st8 = kb.sb("st8", [128, 16], F32); bst8 = Buf()
        mixcat_2 = [kb.sb("mixcat%d" % i, [128, D], BF16) for i in range(2)]; bmixa_2 = [Buf(), Buf()]; bmixy_2 = [Buf(), Buf()]
        tmpB = kb.sb("tmpB", [128, D], F32); btmpB = Buf()
        mixT = kb.sb("mixT", [128, 8, 128], BF16); bmixT = Buf()
        x1 = kb.sb("x1", [128, D], F32); bx1 = Buf()

        tp = [kb.ps("tp%d" % i, [128, 8, 128], BF16) for i in range(2)]; btp = [Buf(), Buf()]
        mm = [kb.ps("mm%d" % i, [128, 512], F32) for i in range(2)]; bmm = [Buf(), Buf()]
        s2 = kb.ps("s2", [128, 2, 512], F32); bs2 = [Buf(), Buf()]
        oo = [kb.ps("oo%d" % i, [128, 512], F32) for i in range(2)]; boo = [Buf(), Buf()]
        tpB = [s2[:, i, :].bitcast(BF16).rearrange("p (k m) -> p k m", m=128) for i in range(2)]
        for s in range(nseq):
            cos, sin, brope = self.rope_tables(s, 32, "k_invf32", "b%d" % s)
            for n_, part in (("gmod1", 1), ("shift1", 0), ("gate1", 2), ("gmod2", 4), ("shift2", 3)):
                kb.dma("sp", mods[n_][:], S["MOD"][s, 0, part].partition_broadcast(128), reads=[self.db("MOD", (s, 0, part))], writes=[bmod])
            def stage_a(t, s=s, cos=cos, sin=sin, brope=brope):
                P_ = t % 2
                X = x_t[P_]; bX = bx[P_]
                v_bf = v_bf_2[P_]; bv = bv_2[P_]; sg = sg_2[P_]; bsg = bsg_2[P_]; rqT = rqT_2[P_]; brqT = brqT_2[P_]
                rkT = rkT_2[P_]; brkT = brkT_2[P_]; rkd_bf = rkd_bf_2[P_]; brkd = brkd_2[P_]
                mixcat = mixcat_2[P_]; bmixa = bmixa_2[P_]; bmixy = bmixy_2[P_]
                ti = s * NT + t
                kb.dma("sp", X[:], I["x"][ti * 128:(ti + 1) * 128, :], writes=[bX])
                kb.dma("sp", mixcat[:, 0:512], S["ATT"][ti], reads=[self.db("ATT", ti)], writes=[bmixa])
                self.norm_mod_T(X, bX, mods["gmod1"], mods["shift1"], bmod, tmp, btmp, h_bf, bh, tp[0], btp[0], hT, bhT, ss, bss)
                cb = bc_mid(cos[:, t, :], 8)
                sb_ = bc_mid(sin[:, t, :], 8)
                for gi in range(4):
                    p_ = mm[gi % 2]; bp_ = bmm[gi % 2]
                    for k in range(8):
                        kb.op("pe", lambda e, k=k, gi=gi, p_=p_: e.matmul(p_[:], lhsT=hT[:, k, :], rhs=w_in[:, k, gi * 512:(gi + 1) * 512],
                                                                       start=(k == 0), stop=(k == 7)), reads=[bhT, bw], writes=[bp_])
                    if gi < 2:
                        rw_ = raw[gi]; brw_ = braw[gi]; ro = rr[gi]; bro = brr[gi]
                        kb.op("act", lambda e, p_=p_, rw_=rw_: e.copy(out=rw_[:].rearrange("p h d -> p (h d)"), in_=p_[:]),
                              reads=[bp_], writes=[brw_])
                        x1_ = rw_[:, :, 0:32]; x2_ = rw_[:, :, 32:64]
                        kb.op("dve", lambda e, x1_=x1_: e.tensor_tensor(out=rt[0][:], in0=x1_, in1=cb, op=ALU.mult), reads=[brw_, brope], writes=[brt])
                        kb.op("dve", lambda e, x2_=x2_: e.tensor_tensor(out=rt[1][:], in0=x2_, in1=sb_, op=ALU.mult), reads=[brw_, brope], writes=[brt])
                        kb.op("dve", lambda e, x2_=x2_: e.tensor_tensor(out=rt[2][:], in0=x2_, in1=cb, op=ALU.mult), reads=[brw_, brope], writes=[brt])
                        kb.op("dve", lambda e, x1_=x1_: e.tensor_tensor(out=rt[3][:], in0=x1_, in1=sb_, op=ALU.mult), reads=[brw_, brope], writes=[brt])
                        kb.op("dve", lambda e, ro=ro: e.tensor_tensor(out=ro[:, :, 0:32], in0=rt[0][:], in1=rt[1][:], op=ALU.subtract),
                              reads=[brt], writes=[bro])
                        kb.op("dve", lambda e, ro=ro: e.tensor_tensor(out=ro[:, :, 32:64], in0=rt[2][:], in1=rt[3][:], op=ALU.add),
                              reads=[brt], writes=[bro])
                        if gi == 0:
                            kb.op("act", lambda e, ro=ro: e.copy(out=rq_bf[:], in_=ro[:]), reads=[bro], writes=[brqb])
                            kb.op("dve", lambda e, ro=ro: e.tensor_tensor(out=rqd_bf[:], in0=ro[:], in1=bc_last(qdec[:, :], 64), op=ALU.mult),
                                  reads=[bro, bw], writes=[brqd])
                        else:
                            kb.op("act", lambda e, ro=ro: e.mul(out=rk_bf[:], in_=ro[:], mul=0.125), reads=[bro], writes=[brkb])
                            kb.op("dve", lambda e, ro=ro: e.scalar_tensor_tensor(out=rkd_bf[:], in0=ro[:], scalar=0.125,
                                                                                in1=bc_last(kdec[:, :], 64), op0=ALU.mult, op1=ALU.mult),
                                  reads=[bro, bw], writes=[brkd])
                    elif gi == 2:
                        kb.op("act", lambda e, p_=p_: e.copy(out=v_bf[:].rearrange("p h d -> p (h d)"), in_=p_[:]), reads=[bp_], writes=[bv])
                    else:
                        kb.op("act", lambda e, p_=p_: e.activation(out=sg[:], in_=p_[:], func=AF.Silu), reads=[bp_], writes=[bsg])
                for i in range(4):
                    kb.op("pe", lambda e, i=i: e.transpose(out=tp[1][:, i, :], in_=rq_bf[:, 2 * i:2 * i + 2, :].rearrange("p h d -> p (h d)"),
                                                           identity=self.ident_b[:]), reads=[brqb, self.b_const], writes=[btp[1]])
                for i in range(4):
                    kb.op("pe", lambda e, i=i: e.transpose(out=tp[1][:, 4 + i, :], in_=rqd_bf[:, 2 * i:2 * i + 2, :].rearrange("p h d -> p (h d)"),
                                                           identity=self.ident_b[:]), reads=[brqd, self.b_const], writes=[btp[1]])
                for i in range(4):
                    kb.op("pe", lambda e, i=i: e.transpose(out=tp[0][:, i, :], in_=rk_bf[:, 2 * i:2 * i + 2, :].rearrange("p h d -> p (h d)"),
                                                           identity=self.ident_b[:]), reads=[brkb, self.b_const], writes=[btp[0]])
                kb.op("act", lambda e: e.copy(out=rqT[:], in_=tp[1][:]), reads=[btp[1]], writes=[brqT])
                kb.op("dve", lambda e: e.tensor_copy(out=rkT[:], in_=tp[0][:, 0:4, :]), reads=[btp[0]], writes=[brkT])
            def stage_b(t, s=s):
                P_ = t % 2
                X = x_t[P_]; bX = bx[P_]
                v_bf = v_bf_2[P_]; bv = bv_2[P_]; sg = sg_2[P_]; bsg = bsg_2[P_]; rqT = rqT_2[P_]; brqT = brqT_2[P_]
                rkT = rkT_2[P_]; brkT = brkT_2[P_]; rkd_bf = rkd_bf_2[P_]; brkd = brkd_2[P_]
                mixcat = mixcat_2[P_]; bmixa = bmixa_2[P_]; bmixy = bmixy_2[P_]
                ti = s * NT + t
                tmp = tmpB; btmp = btmpB
                for h in range(8):
                    i, o = h // 2, (h % 2) * 64
                    kb.op("pe", lambda e, h=h, i=i, o=o: e.matmul(s2[:, h % 2, i * 128:(i + 1) * 128],
                                                                lhsT=rkT[o:o + 64, i, :], rhs=rqT[o:o + 64, i, :], start=True, stop=True),
                          reads=[brkT, brqT], writes=[bs2[h % 2]])
                for hb in range(2):
                    kb.op("dve", lambda e, hb=hb: e.tensor_tensor(out=Sd[:, hb * 4:(hb + 1) * 4, :].rearrange("p h q -> p (h q)"),
                                                                 in0=s2[:, hb, :], in1=decT[:, hb * 512:(hb + 1) * 512], op=ALU.mult),
                          reads=[bs2[hb], bw], writes=[bSd])
                if _STOP <= 1.5:
                    return
                for i in range(4):
                    if t > 0:
                        kb.op("pe", lambda e, i=i: e.matmul(oo[0][:, i * 128:(i + 1) * 128], lhsT=rqT[:, 4 + i, :],
                                                            rhs=st_b[:, i, :], start=True, stop=False, skip_group_check=True),
                              reads=[brqT, bstb], writes=[boo[0]])
                    for par in range(2):
                        h = 2 * i + par
                        kb.op("pe", lambda e, h=h, i=i, par=par: e.matmul(oo[0][:, h * 64:(h + 1) * 64], lhsT=Sd[:, par * 4 + i, :],
                                                                        rhs=v_bf[:, h, :], start=(t == 0), stop=(t == 0 or par == 1),
                                                                        skip_group_check=(t > 0)),
                              reads=[bSd, bv], writes=[boo[0]])
                if _STOP <= 2:
                    return
                for i in range(4):
                    kb.op("pe", lambda e, i=i: e.matmul(oo[1][:, i * 128:(i + 1) * 128],
                                                        lhsT=rkd_bf[:, 2 * i:2 * i + 2, :].rearrange("p h d -> p (h d)"),
                                                        rhs=v_bf[:, 2 * i:2 * i + 2, :].rearrange("p h d -> p (h d)"), start=True, stop=True),
                          reads=[brkd, bv], writes=[boo[1]])
                kvv = oo[1][:].rearrange("p (i c) -> p i c", c=128)
                for half in range(2):
                    po = half * 64
                    if t == 0:
                        kb.op("dve", lambda e, po=po: e.tensor_copy(out=st_f[po:po + 64, :, po:po + 64], in_=kvv[po:po + 64, :, po:po + 64]),
                              reads=[boo[1]], writes=[bstf])
                    else:
                        for i in range(4):
                            kb.op("dve", lambda e, po=po, i=i: e.scalar_tensor_tensor(
                                out=st_f[po:po + 64, i, po:po + 64], in0=st_f[po:po + 64, i, po:po + 64], scalar=cdec[po:po + 64, i:i + 1],
                                in1=kvv[po:po + 64, i, po:po + 64], op0=ALU.mult, op1=ALU.add), reads=[boo[1], bstf, bw], writes=[bstf])
                kb.op("act", lambda e: e.copy(out=st_b[:], in_=st_f[:]), reads=[bstf], writes=[bstb])
                if _STOP <= 3:
                    return
                kb.op("act", lambda e: e.copy(out=o_sb[:].rearrange("p h d -> p (h d)"), in_=oo[0][:]), reads=[boo[0]], writes=[bo])
                kb.op("dve", lambda e: e.tensor_reduce(out=st8[:, 0:8], in_=o_sb[:], axis=AX.X, op=ALU.add), reads=[bo], writes=[bst8])
                kb.op("dve", lambda e: e.tensor_scalar(out=st8[:, 0:8], in0=st8[:, 0:8], scalar1=-1.0 / 64, scalar2=None, op0=ALU.mult),
                      reads=[bst8], writes=[bst8])
                kb.op("dve", lambda e: e.tensor_tensor(out=oc[:], in0=o_sb[:], in1=bc_last(st8[:, 0:8], 64), op=ALU.add),
                      reads=[bo, bst8], writes=[boc])
                kb.op("dve", lambda e: e.tensor_tensor(out=o_sb[:], in0=oc[:], in1=oc[:], op=ALU.mult), reads=[boc], writes=[bo])
                kb.op("dve", lambda e: e.tensor_reduce(out=st8[:, 8:16], in_=o_sb[:], axis=AX.X, op=ALU.add), reads=[bo], writes=[bst8])
                kb.op("dve", lambda e: e.tensor_scalar(out=st8[:, 8:16], in0=st8[:, 8:16], scalar1=1.0 / 64, scalar2=EPS,
                                                       op0=ALU.mult, op1=ALU.add), reads=[bst8], writes=[bst8])
                kb.op("pool", lambda e: e.tensor_tensor(out=st8[:, 8:16], in0=st8[:, 8:16], in1=self.neghalf[:, 0:8], op=ALU.pow),
                      reads=[bst8, self.b_const], writes=[bst8])
                kb.op("dve", lambda e: e.tensor_tensor(out=oc[:], in0=oc[:], in1=bc_last(st8[:, 8:16], 64), op=ALU.mult),
                      reads=[boc, bst8], writes=[boc])
                kb.op("dve", lambda e: e.tensor_tensor(out=oc[:].rearrange("p h d -> p (h d)"), in0=oc[:].rearrange("p h d -> p (h d)"),
                                                        in1=retg[:], op=ALU.mult), reads=[boc, bw], writes=[boc])
                kb.op("dve", lambda e: e.tensor_tensor(out=mixcat[:, 512:1024], in0=oc[:].rearrange("p h d -> p (h d)"), in1=sg[:],
                                                       op=ALU.mult), reads=[boc, bsg], writes=[bmixy])
                if _STOP <= 4:
                    return
                for k in range(8):
                    kb.op("pe", lambda e, k=k: e.transpose(out=tpB[0][:, k, :], in_=mixcat[:, k * 128:(k + 1) * 128], identity=self.ident_b[:]),
                          reads=[bmixa, bmixy, self.b_const], writes=[bs2[0]])
                kb.op("act", lambda e: e.copy(out=mixT[:], in_=tpB[0]), reads=[bs2[0]], writes=[bmixT])
                for half in range(2):
                    for k in range(8):
                        kb.op("pe", lambda e, k=k, half=half: e.matmul(oo[half][:], lhsT=mixT[:, k, :], rhs=w_out[:, k, half * 512:(half + 1) * 512],
                                                                     start=(k == 0), stop=(k == 7)), reads=[bmixT, bw], writes=[boo[half]])
                    hs = slice(half * 512, (half + 1) * 512)
                    kb.op("dve", lambda e, half=half, hs=hs: e.tensor_tensor(out=tmp[:, hs], in0=oo[half][:], in1=mods["gate1"][:, hs], op=ALU.mult),
                          reads=[boo[half], bmod], writes=[btmp])
                    kb.op("dve", lambda e, hs=hs: e.tensor_tensor(out=x1[:, hs], in0=tmp[:, hs], in1=X[:, hs], op=ALU.add),
                          reads=[btmp, bX], writes=[bx1])
                kb.dma("sp", S["XA"][ti * 128:(ti + 1) * 128, :], x1[:], reads=[bx1], writes=[self.db("XA", ti)])
                if _STOP <= 5:
                    return
                self.norm2_router(r, x1, bx1, mods["gmod2"], mods["shift2"], bmod, tmp, btmp, tpB, bs2, oo[0], boo[0], ti)
            kb.pipeline(stage_a, stage_b, self.ntl)
        kb.pop()

    def zero_xg(self):
        kb, S = self.kb, self.S
        z = kb.sb("zeros", [128, 4096], BF16); bz = Buf()
        kb.op("pool", lambda e: e.memset(z[:], 0.0), writes=[bz])
        nrows = 32 * self.cap
        for r0 in range(0, nrows, 512):
            kb.dma("sp", S["XG"][r0:r0 + 512, :].rearrange("(p a) d -> p (a d)", p=128), z[:], reads=[bz])

    def phase2e(self, l):
        kb, I, S = self.kb, self.I, self.S
        kb.push()
        cap = self.cap
        nblk = cap // 512
        wgu = [kb.sb("wgu%d" % i, [128, 8, 2048], BF16) for i in range(2)]
        wdn = [kb.sb("wdn%d" % i, [128, 8, D], BF16) for i in range(2)]
        bgu = [kb.sb("bgu%d" % i, [128, 16], F32) for i in range(2)]
        bdn = [kb.sb("bdn%d" % i, [1, D], BF16) for i in range(2)]
        bwt = [Buf(), Buf()]
        ones1 = kb.sb("ones1", [1, 128], BF16); bones = Buf()
        kb.op("pool", lambda e: e.memset(ones1[:], 1.0), writes=[bones])
        xg = [kb.sb("xg%d" % i, [128, D], BF16) for i in range(12)]; bxg = [Buf() for _ in range(12)]
        xgT = [kb.sb("xgT%d" % i, [128, 8, 512], BF16) for i in range(2)]; bxgT = [Buf(), Buf()]
        glu = [kb.sb("glu%d" % i, [128, 512], F32) for i in range(2)]; bglu = [Buf(), Buf()]
        sig = [kb.sb("sig%d" % i, [128, 512], F32) for i in range(2)]; bsig = [Buf(), Buf()]
        lin = [kb.sb("lin%d" % i, [128, 512], F32) for i in range(2)]; blin = [Buf(), Buf()]
        actT = [kb.sb("actT%d" % i, [128, 8, 512], BF16) for i in range(2)]; bact = [Buf(), Buf()]
        yg = [kb.sb("yg%d" % i, [128, D], F32) for i in range(3)]; byg = [Buf() for _ in range(3)]
        tp = [kb.ps("tp%d" % i, [128, 8, 128], BF16) for i in range(2)]; btp = [Buf(), Buf()]
        pA = [kb.ps("pA%d" % i, [128, 512], F32) for i in range(2)]; bpA = [Buf(), Buf()]
        pB = [kb.ps("pB%d" % i, [128, 512], F32) for i in range(2)]; bpB = [Buf(), Buf()]
        pC = [kb.ps("pC%d" % i, [128, 512], F32) for i in range(2)]; bpC = [Buf(), Buf()]

        def load_expert(e, slot):
            self.load_w_bf16(wgu[slot], I["exp_w_gu"][l, e], 8, bwt[slot])
            self.load_w_bf16(wdn[slot], I["exp_w_down"][l, e], 8, bwt[slot])
            kb.dma("sp", bgu[slot][:], I["exp_b_gu_pj"][l, e], writes=[bwt[slot]])
            kb.op("dve", lambda en, slot=slot: en.tensor_scalar(out=bgu[slot][:, 8:16], in0=bgu[slot][:, 8:16], scalar1=1.0, scalar2=None,
                                                               op0=ALU.add), reads=[bwt[slot]], writes=[bwt[slot]])
            kb.dma("pool", bdn[slot][:], I["exp_b_down"][l, e:e + 1, :], writes=[bwt[slot]])

        load_expert(0, 0)
        blocks = [(ex, blk) for ex in range(32) for blk in range(nblk)]
        state = dict(xu=0, tu=0)

        def emit_loads(bi):
            ex, blk = blocks[bi]
            r0 = ex * cap + blk * 512
            tiles = []
            for st in range(4):
                i = state["xu"] % len(xg); state["xu"] += 1
                kb.dma("sp", xg[i][:], S["XG"][r0 + st * 128:r0 + (st + 1) * 128, :], writes=[bxg[i]])
                tiles.append(i)
            return tiles

        def emit_transposes(bi, tiles):
            XT = xgT[bi % 2]; bXT = bxgT[bi % 2]
            for st, i in enumerate(tiles):
                T = tp[state["tu"] % 2]; bT = btp[state["tu"] % 2]; state["tu"] += 1
                for k in range(8):
                    kb.op("pe", lambda e, k=k, i=i, T=T: e.transpose(out=T[:, k, :], in_=xg[i][:, k * 128:(k + 1) * 128],
                                                                   identity=self.ident_b[:]), reads=[bxg[i], self.b_const], writes=[bT])
                kb.op("act", lambda e, T=T, XT=XT, st=st: e.copy(out=XT[:, :, st * 128:(st + 1) * 128], in_=T[:]),
                      reads=[bT], writes=[bXT])

        pu = cu = yu = 0
        tl0 = emit_loads(0)
        tl1 = emit_loads(1) if len(blocks) > 1 else None
        emit_transposes(0, tl0)
        for bi, (ex, blk) in enumerate(blocks):
            slot = ex % 2
            if blk == 0 and ex + 1 < 32:
                load_expert(ex + 1, (ex + 1) % 2)
            XT = xgT[bi % 2]; bXT = bxgT[bi % 2]
            A = actT[bi % 2]; bA = bact[bi % 2]
            r0 = ex * cap + blk * 512
            for j in range(8):
                pa = pA[pu % 2]; bpa = bpA[pu % 2]; pb = pB[pu % 2]; bpb = bpB[pu % 2]
                gl = glu[pu % 2]; bgl = bglu[pu % 2]; sg_ = sig[pu % 2]; bsg_ = bsig[pu % 2]; ln = lin[pu % 2]; bln = blin[pu % 2]
                pu += 1
                for k in range(8):
                    kb.op("pe", lambda e, k=k, j=j, pa=pa, slot=slot, XT=XT: e.matmul(
                        pa[:], lhsT=wgu[slot][:, k, j * 128:(j + 1) * 128], rhs=XT[:, k, :],
                        start=(k == 0), stop=(k == 7)), reads=[bwt[slot], bXT], writes=[bpa])
                for k in range(8):
                    kb.op("pe", lambda e, k=k, j=j, pb=pb, slot=slot, XT=XT: e.matmul(
                        pb[:], lhsT=wgu[slot][:, k, 1024 + j * 128:1024 + (j + 1) * 128], rhs=XT[:, k, :],
                        start=(k == 0), stop=(k == 7)), reads=[bwt[slot], bXT], writes=[bpb])
                kb.op("dve", lambda e, pa=pa, gl=gl, j=j, slot=slot: e.tensor_scalar(
                    out=gl[:], in0=pa[:], scalar1=bgu[slot][:, j:j + 1], scalar2=7.0, op0=ALU.add, op1=ALU.min),
                    reads=[bpa, bwt[slot]], writes=[bgl])
                kb.op("act", lambda e, gl=gl, sg_=sg_: e.activation(out=sg_[:], in_=gl[:], func=AF.Sigmoid, scale=1.702),
                      reads=[bgl], writes=[bsg_])
                kb.op("dve", lambda e, pb=pb, ln=ln, j=j, slot=slot: e.tensor_scalar(
                    out=ln[:], in0=pb[:], scalar1=bgu[slot][:, 8 + j:9 + j], scalar2=8.0, op0=ALU.add, op1=ALU.min),
                    reads=[bpb, bwt[slot]], writes=[bln])
                kb.op("dve", lambda e, gl=gl, sg_=sg_: e.tensor_tensor(out=gl[:], in0=gl[:], in1=sg_[:], op=ALU.mult),
                      reads=[bgl, bsg_], writes=[bgl])
                kb.op("dve", lambda e, gl=gl, ln=ln, A=A, j=j: e.scalar_tensor_tensor(out=A[:, j, :], in0=ln[:], scalar=-6.0, in1=gl[:],
                                                                                 op0=ALU.max, op1=ALU.mult),
                      reads=[bgl, bln], writes=[bA])
            if bi + 1 < len(blocks):
                emit_transposes(bi + 1, tl1)
                tl0, tl1 = tl1, (emit_loads(bi + 2) if bi + 2 < len(blocks) else None)
            for st in range(4):
                Y = yg[yu % 3]; bY = byg[yu % 3]; yu += 1
                for half in range(2):
                    pc = pC[cu % 2]; bpc = bpC[cu % 2]; cu += 1
                    for k in range(8):
                        kb.op("pe", lambda e, k=k, st=st, half=half, pc=pc, A=A, slot=slot: e.matmul(
                            pc[:], lhsT=A[:, k, st * 128:(st + 1) * 128], rhs=wdn[slot][:, k, half * 512:(half + 1) * 512],
                            start=(k == 0), stop=False), reads=[bA, bwt[slot]], writes=[bpc])
                    kb.op("pe", lambda e, half=half, pc=pc, slot=slot: e.matmul(
                        pc[:], lhsT=ones1[:, :], rhs=bdn[slot][:, half * 512:(half + 1) * 512], start=False, stop=True),
                        reads=[bones, bwt[slot]], writes=[bpc])
                    if half == 0:
                        kb.op("act", lambda e, pc=pc, Y=Y: e.copy(out=Y[:, 0:512], in_=pc[:]), reads=[bpc], writes=[bY])
                    else:
                        kb.op("dve", lambda e, pc=pc, Y=Y: e.tensor_copy(out=Y[:, 512:1024], in_=pc[:]), reads=[bpc], writes=[bY])
                kb.dma("pool", S["YG"][r0 + st * 128:r0 + (st + 1) * 128, :], Y[:], reads=[bY])
        kb.pop()

    def phase2c(self, l, src, dst, dst_name, zero_after):
        kb, I, S, nseq = self.kb, self.I, self.S, self.nseq
        kb.push()
        cap = self.cap
        gate2 = kb.sb("gate2", [128, D], F32); bg2 = Buf()
        NB = 4
        xin = [kb.sb("xin%d" % i, [128, D], F32) for i in range(NB)]; bxin = [Buf() for _ in range(NB)]
        acc = [kb.sb("acc%d" % i, [128, D], F32) for i in range(NB)]; bacc = [Buf() for _ in range(NB)]
        yb = [kb.sb("yb%d" % i, [128, D], F32) for i in range(4 * NB)]; byb = [Buf() for _ in range(4 * NB)]
        sl = [kb.sb("sl%d" % i, [128, 4], I32) for i in range(NB)]; bsl = [Buf() for _ in range(NB)]
        gk = [kb.sb("gkc%d" % i, [128, 4], F32) for i in range(NB)]; bgk = [Buf() for _ in range(NB)]
        for i in range(4 * NB):
            kb.op("pool", lambda e, i=i: e.memset(yb[i][:], 0.0), writes=[byb[i]])
        if zero_after:
            self.zero_xg()
        u = 0
        for s in range(nseq):
            kb.dma("sp", gate2[:], S["MOD"][s, l, 5].partition_broadcast(128), reads=[self.db("MOD", (s, l, 5))], writes=[bg2])
            for t in range(self.ntl):
                ti = s * NT + t
                X = xin[u % NB]; bX = bxin[u % NB]; A = acc[u % NB]; bA = bacc[u % NB]
                SL = sl[u % NB]; bSL = bsl[u % NB]; GK = gk[u % NB]; bGK = bgk[u % NB]
                kb.dma("sp", X[:], src[ti * 128:(ti + 1) * 128, :], reads=[self.db("XA", ti)], writes=[bX])
                kb.dma("sp", SL[:], S["SLOT"][ti], reads=[self.db("SLOT", ti)], writes=[bSL])
                kb.dma("sp", GK[:], S["GK"][ti], reads=[self.db("GK", ti)], writes=[bGK])
                for k in range(4):
                    Yk = yb[(u % NB) * 4 + k]; bYk = byb[(u % NB) * 4 + k]
                    kb.idma(Yk[:], None, S["YG"], SL[:, k:k + 1], 32 * cap - 1, reads=[bSL], writes=[bYk])
                    if k == 0:
                        kb.op("dve", lambda e, Yk=Yk, A=A, GK=GK: e.tensor_scalar(out=A[:], in0=Yk[:], scalar1=GK[:, 0:1], scalar2=None,
                                                                                 op0=ALU.mult), reads=[bYk, bGK], writes=[bA])
                    else:
                        kb.op("dve", lambda e, Yk=Yk, A=A, GK=GK, k=k: e.scalar_tensor_tensor(
                            out=A[:], in0=Yk[:], scalar=GK[:, k:k + 1], in1=A[:], op0=ALU.mult, op1=ALU.add),
                            reads=[bYk, bGK, bA], writes=[bA])
                kb.op("dve", lambda e, A=A: e.tensor_tensor(out=A[:], in0=A[:], in1=gate2[:], op=ALU.mult), reads=[bA, bg2], writes=[bA])
                kb.op("dve", lambda e, A=A, X=X: e.tensor_tensor(out=X[:], in0=A[:], in1=X[:], op=ALU.add), reads=[bA, bX], writes=[bX])
                kb.dma("sp", dst[ti * 128:(ti + 1) * 128, :], X[:], reads=[bX], writes=[self.db(dst_name, ti)])
                u += 1
        kb.pop()

    def phase3(self):
        kb, I, S, nseq = self.kb, self.I, self.S, self.nseq
        kb.push()
        bw = Buf()
        w_qkv = kb.sb("w_qkv", [128, 8, 1280], BF16)
        w_out = kb.sb("w_out", [128, 8, D], BF16)
        self.load_w_bf16(w_qkv, I["swa_w_qkv"], 8, bw)
        self.load_w_bf16(w_out, I["swa_w_out"], 8, bw)
        bqkv = kb.sb("bqkv", [128, 1280], F32)
        bout = kb.sb("bout", [128, D], F32)
        gq = kb.sb("gq", [128, 64], F32)
        gk = kb.sb("gk", [128, 64], F32)
        sk = kb.sb("sk", [128, 16], F32)
        self.bcast_load(bqkv[:], I["swa_b_qkv"], bw)
        self.bcast_load(bout[:], I["swa_b_out"], bw)
        self.bcast_load(gq[:], I["swa_q_head_g"], bw)
        self.bcast_load(gk[:], I["swa_k_head_g"], bw)
        self.bcast_load(sk[:], I["swa_sinks"], bw)
        kb.op("act", lambda e: e.activation(out=sk[:], in_=sk[:], func=AF.Exp), reads=[bw], writes=[bw])
        mask2 = kb.sb("mask2", [128, 4, 2, 128], BF16)
        for hh in range(4):
            kb.op("dve", lambda e, hh=hh: e.tensor_copy(out=mask2[:, hh, 0, :], in_=self.mask_gt[:]), reads=[self.b_const], writes=[bw])
            kb.op("dve", lambda e, hh=hh: e.tensor_copy(out=mask2[:, hh, 1, :], in_=self.mask_le[:]), reads=[self.b_const], writes=[bw])
        r = self.alloc_router(1)

        mods = {n: kb.sb(n, [128, D], F32) for n in ("gmod1", "shift1", "gate1", "gmod2", "shift2")}
        bmod = Buf()
        x_t = [kb.sb("x_t%d" % i, [128, D], F32) for i in range(2)]; bx = [Buf(), Buf()]
        tmp = kb.sb("tmp", [128, D], F32); btmp = Buf()
        h_bf = kb.sb("h_bf", [128, D], BF16); bh = Buf()
        hT = kb.sb("hT", [128, 8, 128], BF16); bhT = Buf()
        ss = kb.sb("ss", [128, 4], F32); bss = Buf()
        qkv = kb.sb("qkv", [128, 20, 64], F32); bqkvs = Buf()
        sq = kb.sb("sq", [128, 18, 64], F32); bsq = Buf()
        r18 = kb.sb("r18", [128, 18], F32); br18 = Buf()
        qn = kb.sb("qn", [128, 18, 64], F32); bqn = Buf()
        rt = [kb.sb("rt%d" % i, [128, 18, 32], F32) for i in range(4)]; brt = Buf()
        q_bf = kb.sb("q_bf", [128, 16, 64], BF16); bqb = Buf()
        kdup = kb.sb("kdup", [128, 2, 2, 64], BF16); bkd = Buf()
        qT_2 = [kb.sb("qT%d" % i, [128, 8, 128], BF16) for i in range(2)]; bqT_2 = [Buf(), Buf()]
        tmpB = kb.sb("tmpB", [128, D], F32); btmpB = Buf()
        kT = [kb.sb("kT%d" % i, [128, 2, 128], BF16) for i in range(3)]; bkT = [Buf() for _ in range(3)]
        Va = [kb.sb("Va%d" % i, [128, 2, 65], BF16) for i in range(3)]; bVa = [Buf() for _ in range(3)]
        bVones = Buf()
        for i in range(3):
            kb.op("pool", lambda e, i=i: e.memset(Va[i][:, :, 64:65], 1.0), writes=[bVones])
        PT = [kb.sb("PT%d" % i, [128, 4, 2, 128], BF16) for i in range(2)]; bPT = [Buf(), Buf()]
        den = kb.sb("den", [128, 16], F32); bden = Buf()
        attn = kb.sb("attn", [128, 16, 64], BF16); battn = Buf()
        attT = kb.sb("attT", [128, 8, 128], BF16); battT = Buf()
        x1 = kb.sb("x1", [128, D], F32); bx1 = Buf()

        tp = [kb.ps("tp%d" % i, [128, 8, 128], BF16) for i in range(2)]; btp = [Buf(), Buf()]
        mm = [kb.ps("mm%d" % i, [128, 512], F32) for i in range(2)]; bmm = [Buf(), Buf()]
        s2 = kb.ps("s2", [128, 2, 512], F32); bs2 = [Buf(), Buf()]
        oo = [kb.ps("oo%d" % i, [128, 512], F32) for i in range(2)]; boo = [Buf(), Buf()]
        tpB = [s2[:, i, :].bitcast(BF16).rearrange("p (k m) -> p k m", m=128) for i in range(2)]
        gc = {"n": 0}
        for s in range(nseq):
            cos, sin, brope = self.rope_tables(s, 32, "k_invf32", "c%d" % s)
            for n_, part in (("gmod1", 1), ("shift1", 0), ("gate1", 2), ("gmod2", 4), ("shift2", 3)):
                kb.dma("sp", mods[n_][:], S["MOD"][s, 1, part].partition_broadcast(128), reads=[self.db("MOD", (s, 1, part))], writes=[bmod])
            def stage_a(t, s=s, cos=cos, sin=sin, brope=brope):
                ti = s * NT + t
                cur, prv = t % 3, (t - 1) % 3
                X = x_t[t % 2]; bX = bx[t % 2]
                qT = qT_2[t % 2]; bqT = bqT_2[t % 2]
                kb.dma("sp", X[:], S["XB"][ti * 128:(ti + 1) * 128, :], reads=[self.db("XB", ti)], writes=[bX])
                self.norm_mod_T(X, bX, mods["gmod1"], mods["shift1"], bmod, tmp, btmp, h_bf, bh, tp[0], btp[0], hT, bhT, ss, bss)
                qkvf = qkv[:].rearrange("p h d -> p (h d)")
                for gi, (c0, c1) in enumerate(((0, 512), (512, 1024), (1024, 1280))):
                    p_ = mm[gi % 2]; bp_ = bmm[gi % 2]
                    for k in range(8):
                        kb.op("pe", lambda e, k=k, c0=c0, c1=c1, p_=p_: e.matmul(p_[:, 0:c1 - c0], lhsT=hT[:, k, :], rhs=w_qkv[:, k, c0:c1],
                                                                              start=(k == 0), stop=(k == 7)), reads=[bhT, bw], writes=[bp_])
                    kb.op("dve", lambda e, c0=c0, c1=c1, p_=p_: e.tensor_tensor(out=qkvf[:, c0:c1], in0=p_[:, 0:c1 - c0], in1=bqkv[:, c0:c1],
                                                                              op=ALU.add), reads=[bp_, bw], writes=[bqkvs])
                kb.op("dve", lambda e: e.tensor_tensor(out=sq[:], in0=qkv[:, 0:18, :], in1=qkv[:, 0:18, :], op=ALU.mult), reads=[bqkvs], writes=[bsq])
                kb.op("dve", lambda e: e.tensor_reduce(out=r18[:], in_=sq[:], axis=AX.X, op=ALU.add), reads=[bsq], writes=[br18])
                kb.op("dve", lambda e: e.tensor_scalar(out=r18[:], in0=r18[:], scalar1=1.0 / 64, scalar2=EPS, op0=ALU.mult, op1=ALU.add),
                      reads=[br18], writes=[br18])
                kb.op("pool", lambda e: e.tensor_tensor(out=r18[:, 0:16], in0=r18[:, 0:16], in1=self.neghalf[:, 0:16], op=ALU.pow),
                      reads=[br18, self.b_const], writes=[br18])
                kb.op("pool", lambda e: e.tensor_tensor(out=r18[:, 16:18], in0=r18[:, 16:18], in1=self.neghalf[:, 0:2], op=ALU.pow),
                      reads=[br18, self.b_const], writes=[br18])
                kb.op("dve", lambda e: e.tensor_tensor(out=qn[:], in0=qkv[:, 0:18, :], in1=bc_last(r18[:, :], 64), op=ALU.mult),
                      reads=[bqkvs, br18], writes=[bqn])
                kb.op("dve", lambda e: e.tensor_tensor(out=qn[:, 0:16, :], in0=qn[:, 0:16, :], in1=bc_mid(gq[:, :], 16), op=ALU.mult),
                      reads=[bqn, bw], writes=[bqn])
                kb.op("dve", lambda e: e.tensor_tensor(out=qn[:, 16:18, :], in0=qn[:, 16:18, :], in1=bc_mid(gk[:, :], 2), op=ALU.mult),
                      reads=[bqn, bw], writes=[bqn])
                cb = bc_mid(cos[:, t, :], 18)
                sb_ = bc_mid(sin[:, t, :], 18)
                x1_ = qn[:, :, 0:32]; x2_ = qn[:, :, 32:64]
                kb.op("dve", lambda e: e.tensor_tensor(out=rt[0][:], in0=x1_, in1=cb, op=ALU.mult), reads=[bqn, brope], writes=[brt])
                kb.op("dve", lambda e: e.tensor_tensor(out=rt[1][:], in0=x2_, in1=sb_, op=ALU.mult), reads=[bqn, brope], writes=[brt])
                kb.op("dve", lambda e: e.tensor_tensor(out=rt[2][:], in0=x2_, in1=cb, op=ALU.mult), reads=[bqn, brope], writes=[brt])
                kb.op("dve", lambda e: e.tensor_tensor(out=rt[3][:], in0=x1_, in1=sb_, op=ALU.mult), reads=[bqn, brope], writes=[brt])
                kb.op("dve", lambda e: e.tensor_tensor(out=q_bf[:, :, 0:32], in0=rt[0][:, 0:16, :], in1=rt[1][:, 0:16, :], op=ALU.subtract),
                      reads=[brt], writes=[bqb])
                kb.op("dve", lambda e: e.tensor_tensor(out=q_bf[:, :, 32:64], in0=rt[2][:, 0:16, :], in1=rt[3][:, 0:16, :], op=ALU.add),
                      reads=[brt], writes=[bqb])
                for dup in range(2):
                    kb.op("dve", lambda e, dup=dup: e.tensor_tensor(out=kdup[:, :, dup, 0:32], in0=rt[0][:, 16:18, :], in1=rt[1][:, 16:18, :],
                                                                   op=ALU.subtract), reads=[brt], writes=[bkd])
                    kb.op("dve", lambda e, dup=dup: e.tensor_tensor(out=kdup[:, :, dup, 32:64], in0=rt[2][:, 16:18, :], in1=rt[3][:, 16:18, :],
                                                                    op=ALU.add), reads=[brt], writes=[bkd])
                kb.op("act", lambda e, cur=cur: e.copy(out=Va[cur][:, :, 0:64], in_=qkv[:, 18:20, :]), reads=[bqkvs, bVones], writes=[bVa[cur]])
                for i in range(8):
                    kb.op("pe", lambda e, i=i: e.transpose(out=tp[1][:, i, :], in_=q_bf[:, 2 * i:2 * i + 2, :].rearrange("p h d -> p (h d)"),
                                                           identity=self.ident_b[:]), reads=[bqb, self.b_const], writes=[btp[1]])
                kb.op("act", lambda e: e.copy(out=qT[:], in_=tp[1][:]), reads=[btp[1]], writes=[bqT])
                for g in range(2):
                    kb.op("pe", lambda e, g=g: e.transpose(out=tp[0][:, g, :], in_=kdup[:, g, :, :].rearrange("p a d -> p (a d)"),
                                                           identity=self.ident_b[:]), reads=[bkd, self.b_const], writes=[btp[0]])
                kb.op("dve", lambda e, cur=cur: e.tensor_copy(out=kT[cur][:], in_=tp[0][:, 0:2, :]), reads=[btp[0]], writes=[bkT[cur]])
            def stage_b(t, s=s):
                ti = s * NT + t
                cur, prv = t % 3, (t - 1) % 3
                X = x_t[t % 2]; bX = bx[t % 2]
                qT = qT_2[t % 2]; bqT = bqT_2[t % 2]
                tmp = tmpB; btmp = btmpB
                for gq4 in range(4):
                    sbank = s2
                    bsb = bs2
                    P = PT[gc["n"] % 2]; bP = bPT[gc["n"] % 2]
                    ob = oo[gc["n"] % 2]; bob = boo[gc["n"] % 2]
                    gc["n"] += 1
                    for hh in range(4):
                        hq = gq4 * 4 + hh
                        i, o = hq // 2, (hq % 2) * 64
                        g = hq // 8
                        for w_, kt in ((0, prv), (1, cur)):
                            if t == 0 and w_ == 0:
                                continue
                            col = ((hh % 2) * 2 + hh // 2) * 256 + w_ * 128
                            kb.op("pe", lambda e, i=i, o=o, g=g, kt=kt, col=col, sbank=sbank: e.matmul(
                                sbank[:, col // 512, col % 512:col % 512 + 128], lhsT=kT[kt][o:o + 64, g, :], rhs=qT[o:o + 64, i, :],
                                start=True, stop=True), reads=[bkT[kt], bqT], writes=[bsb[col // 512]])
                    for bk in range(2):
                        if t == 0:
                            for hh2 in range(2):
                                kb.op("act", lambda e, bk=bk, hh2=hh2, P=P, sbank=sbank: e.activation(
                                    out=P[:, bk * 2 + hh2, 1, :], in_=sbank[:, bk, hh2 * 256 + 128:hh2 * 256 + 256], func=AF.Exp, scale=0.125),
                                    reads=[bsb[bk]], writes=[bP])
                        else:
                            kb.op("act", lambda e, bk=bk, P=P, sbank=sbank: e.activation(
                                out=P[:, bk * 2:bk * 2 + 2, :, :].rearrange("p a b c -> p (a b c)"), in_=sbank[:, bk, :], func=AF.Exp, scale=0.125),
                                reads=[bsb[bk]], writes=[bP])
                    if t == 0:
                        kb.op("dve", lambda e, P=P: e.tensor_tensor(out=P[:, :, 1, :], in0=P[:, :, 1, :], in1=mask2[:, :, 1, :], op=ALU.mult),
                              reads=[bP, bw], writes=[bP])
                    else:
                        kb.op("dve", lambda e, P=P: e.tensor_tensor(out=P[:].rearrange("p a b c -> p (a b c)"), in0=P[:].rearrange("p a b c -> p (a b c)"),
                                                                   in1=mask2[:].rearrange("p a b c -> p (a b c)"), op=ALU.mult),
                              reads=[bP, bw], writes=[bP])
                    for hh in range(4):
                        hq = gq4 * 4 + hh
                        g = hq // 8
                        sl = (hh % 2) * 2 + hh // 2
                        if t > 0:
                            kb.op("pe", lambda e, hh=hh, g=g, P=P, ob=ob, prv=prv, sl=sl: e.matmul(ob[:, hh * 65:hh * 65 + 65], lhsT=P[:, sl, 0, :],
                                                                                          rhs=Va[prv][:, g, :], start=True, stop=False),
                                  reads=[bP, bVa[prv]], writes=[bob])
                        kb.op("pe", lambda e, hh=hh, g=g, P=P, ob=ob, cur=cur, sl=sl: e.matmul(ob[:, hh * 65:hh * 65 + 65], lhsT=P[:, sl, 1, :],
                                                                                      rhs=Va[cur][:, g, :], start=(t == 0), stop=True),
                              reads=[bP, bVa[cur]], writes=[bob])
                    ov = ob[:, 0:260].rearrange("p (h d) -> p h d", d=65)
                    dsl = den[:, gq4 * 4:(gq4 + 1) * 4]
                    kb.op("dve", lambda e, ov=ov, dsl=dsl, gq4=gq4: e.tensor_tensor(out=dsl, in0=ov[:, :, 64], in1=sk[:, gq4 * 4:(gq4 + 1) * 4], op=ALU.add),
                          reads=[bob, bw], writes=[bden])
                    kb.op("dve", lambda e, dsl=dsl: e.reciprocal(out=dsl, in_=dsl), reads=[bden], writes=[bden])
                    kb.op("dve", lambda e, ov=ov, dsl=dsl, gq4=gq4: e.tensor_tensor(out=attn[:, gq4 * 4:(gq4 + 1) * 4, :], in0=ov[:, :, 0:64],
                                                                                 in1=bc_last(dsl, 64), op=ALU.mult), reads=[bob, bden], writes=[battn])
                af = attn[:].rearrange("p h d -> p (h d)")
                for k in range(8):
                    kb.op("pe", lambda e, k=k: e.transpose(out=tpB[0][:, k, :], in_=af[:, k * 128:(k + 1) * 128], identity=self.ident_b[:]),
                          reads=[battn, self.b_const], writes=[bs2[0]])
                kb.op("act", lambda e: e.copy(out=attT[:], in_=tpB[0]), reads=[bs2[0]], writes=[battT])
                for half in range(2):
                    hs = slice(half * 512, (half + 1) * 512)
                    for k in range(8):
                        kb.op("pe", lambda e, k=k, half=half, hs=hs: e.matmul(oo[half][:], lhsT=attT[:, k, :], rhs=w_out[:, k, hs],
                                                                            start=(k == 0), stop=(k == 7)), reads=[battT, bw], writes=[boo[half]])
                    kb.op("dve", lambda e, half=half, hs=hs: e.tensor_tensor(out=tmp[:, hs], in0=oo[half][:], in1=bout[:, hs], op=ALU.add),
                          reads=[boo[half], bw], writes=[btmp])
                    kb.op("dve", lambda e, hs=hs: e.tensor_tensor(out=tmp[:, hs], in0=tmp[:, hs], in1=mods["gate1"][:, hs], op=ALU.mult),
                          reads=[btmp, bmod], writes=[btmp])
                    kb.op("dve", lambda e, hs=hs: e.tensor_tensor(out=x1[:, hs], in0=tmp[:, hs], in1=X[:, hs], op=ALU.add),
                          reads=[btmp, bX], writes=[bx1])
                kb.dma("sp", S["XA"][ti * 128:(ti + 1) * 128, :], x1[:], reads=[bx1], writes=[self.db("XA", ti)])
                self.norm2_router(r, x1, bx1, mods["gmod2"], mods["shift2"], bmod, tmp, btmp, tpB, bs2, oo[0], boo[0], ti)
            kb.pipeline(stage_a, stage_b, self.ntl)
        kb.pop()

    def build(self):
        self.setup_consts()
        ph = self.phases
        if "p0" in ph:
            self.phase0()
        if "p1a" in ph:
            self.phase1a()
        if "p1b" in ph:
            self.phase1b()
        if "p2a" in ph:
            self.phase2e(0)
            self.phase2c(0, self.S["XA"], self.S["XB"], "XB", False)
        if "p3" in ph:
            self.phase3()
        if "p2b" in ph:
            self.phase2e(1)
            self.phase2c(1, self.S["XA"], self.out, "OUT", False)
        self.kb.finish()
        return self.nc


def module_consts():
    idx = np.arange(128, dtype=np.float64)
    lg = np.log1p(-np.exp2(-5.0 - np.arange(8, dtype=np.float64)))
    k = {}
    k["k_invf16"] = (10000.0 ** (-np.arange(16, dtype=np.float32) / 16)).astype(np.float32)
    k["k_invf32"] = (10000.0 ** (-np.arange(32, dtype=np.float32) / 32)).astype(np.float32)
    diff = idx[None, :] - idx[:, None]
    dec = np.where(diff[:, None, :] >= 0, np.exp(lg[None, :, None] * np.maximum(diff[:, None, :], 0.0)), 0.0)
    k["k_decayT"] = np.ascontiguousarray(dec.reshape(128, 4, 2, 128).transpose(0, 2, 1, 3)).reshape(128, 8 * 128).astype(np.float32)
    k["k_qdec"] = np.exp(lg[None, :] * (idx + 1.0)[:, None]).astype(np.float32)
    k["k_kdec"] = np.exp(lg[None, :] * (127.0 - idx)[:, None]).astype(np.float32)
    cd = np.zeros((128, 4), np.float64)
    for i in range(4):
        cd[0:64, i] = np.exp(lg[2 * i] * 128)
        cd[64:128, i] = np.exp(lg[2 * i + 1] * 128)
    k["k_cdec"] = cd.astype(np.float32)
    return k


def make_in_maps(inputs, nseq, n_cores):
    f = lambda a: np.ascontiguousarray(np.asarray(a))
    shared = {}
    for name in ("ada_w", "ada_b", "norm1_g", "norm2_g", "router_w", "router_b", "exp_w_gu", "exp_w_down", "exp_b_down"):
        shared[name] = f(inputs[name])
    for name in ("hyb_w_in", "mla_cq_norm_g", "mla_ckv_norm_g", "mla_w_uq", "mla_w_ukv", "mla_q_head_g", "mla_k_head_g",
                 "hyb_w_out", "swa_w_qkv", "swa_b_qkv", "swa_q_head_g", "swa_k_head_g", "swa_sinks", "swa_w_out", "swa_b_out"):
        shared[name] = f(np.asarray(inputs[name])[0])
    shared["ret_norm_g"] = f(np.asarray(inputs["ret_norm_g"])[0].reshape(512))
    bgu = np.asarray(inputs["exp_b_gu"])
    shared["exp_b_gu_pj"] = f(bgu.reshape(2, 32, 16, 128).transpose(0, 1, 3, 2))
    shared.update(module_consts())
    x = np.asarray(inputs["x"]); c = np.asarray(inputs["c"]); pos = np.asarray(inputs["positions"])
    maps = []
    for i in range(n_cores):
        b0 = i * nseq
        m = dict(shared)
        m["x"] = f(x[b0:b0 + nseq].reshape(nseq * SEQ, D))
        m["c_pk"] = f(c[b0:b0 + nseq].reshape(nseq, 8, 128).transpose(0, 2, 1))
        m["pos_pt"] = f(pos[b0:b0 + nseq].reshape(nseq, NT, 128).transpose(0, 2, 1).astype(np.int32))
        maps.append(m)
    return maps


_PROG = {}


def kernel(**inputs):
    nseq = 32 // N_CORES
    if "nc" not in _PROG:
        _PROG["nc"] = Prog(nseq).build()
    maps = make_in_maps(inputs, nseq, N_CORES)
    res = run_bass_kernel_spmd(_PROG["nc"], maps, core_ids=list(range(N_CORES)))
    out = np.concatenate([np.asarray(r["out"]).reshape(nseq, SEQ, D) for r in res.results], axis=0)
    return out.astype(np.float32)
```

```python
import contextlib
import os
import math
import numpy as np
import concourse.bass as bass
import concourse.mybir as mybir
from concourse.bass_utils import run_bass_kernel_spmd

F32 = mybir.dt.float32
BF16 = mybir.dt.bfloat16
I32 = mybir.dt.int32
AF = mybir.ActivationFunctionType
ALU = mybir.AluOpType
AX = mybir.AxisListType

SAME_ENGINE_SYNC = os.environ.get('KSES', '1') == '1'
DMA_RING = 12
N_CORES = 8
_STOP = float(os.environ.get('KSTOP', '99'))
SEQ = 2048
D = 1024
NT = SEQ // 128
EPS = 1e-6
PI = math.pi


class Buf:
    __slots__ = ("w", "rs")

    def __init__(self):
        self.w = None
        self.rs = {}


class KB:
    ENGS = ("pe", "act", "dve", "pool", "sp")

    def __init__(self, nc):
        self.nc = nc
        self.stacks = [contextlib.ExitStack()]
        self.eng = dict(pe=nc.tensor, act=nc.scalar, dve=nc.vector, pool=nc.gpsimd, sp=nc.sync)
        self.cnt = {e: 0 for e in self.ENGS}
        self.seen = {e: {} for e in self.ENGS}
        self.sems = {}
        for e in self.ENGS:
            self.sems[e] = self.stacks[0].enter_context(nc.semaphore("s_" + e))
        self.dma_n = {}
        for e in ("sp", "pool", "act"):
            self.dma_n[e] = 0
            for j in range(DMA_RING):
                self.sems[("d", e, j)] = self.stacks[0].enter_context(nc.semaphore("d_%s_%d" % (e, j)))
        self.uid = 0

    def push(self):
        self.phase_id = getattr(self, "phase_id", 0) + 1
        self.stacks.append(contextlib.ExitStack())

    def pop(self):
        self.barrier()
        self.stacks.pop().close()

    def sb(self, name, shape, dt):
        self.uid += 1
        return self.stacks[-1].enter_context(self.nc.sbuf_tensor("%s_%d" % (name, self.uid), list(shape), dt))

    def ps(self, name, shape, dt):
        self.uid += 1
        return self.stacks[-1].enter_context(self.nc.psum_tensor("%s_%d" % (name, self.uid), list(shape), dt))

    def _deps(self, e, reads, writes):
        toks = {}
        for b in reads:
            if b.w is not None and toks.get(b.w[0], 0) < b.w[1]:
                toks[b.w[0]] = b.w[1]
        for b in writes:
            if b.w is not None and toks.get(b.w[0], 0) < b.w[1]:
                toks[b.w[0]] = b.w[1]
            for k, v in b.rs.items():
                if toks.get(k, 0) < v:
                    toks[k] = v
        waits = []
        seen = self.seen[e]
        for k, v in toks.items():
            if k == e and (e == "pe" or not SAME_ENGINE_SYNC):
                continue
            if seen.get(k, 0) < v:
                seen[k] = v
                waits.append((k, v))
        return waits

    def _mark(self, tok, reads, writes):
        k, v = tok
        for b in reads:
            if b.rs.get(k, 0) < v:
                b.rs[k] = v
        for b in writes:
            b.w = tok
            b.rs = {}

    def _emit(self, e, waits, fn, key, inc):
        engine = self.eng[e]
        for k, v in waits:
            engine.wait_ge(self.sems[k], v)
        if fn is not None:
            fn(engine).then_inc(self.sems[key], inc)

    def op(self, e, fn, reads=(), writes=()):
        waits = self._deps(e, reads, writes)
        self.cnt[e] += 1
        tok = (e, self.cnt[e])
        self._emit(e, waits, fn, e, 1)
        self._mark(tok, reads, writes)
        self._handoff()
        return tok

    def dma(self, e, out, in_, reads=(), writes=(), **kw):
        waits = self._deps(e, reads, writes)
        n = self.dma_n[e]
        self.dma_n[e] += 1
        key = ("d", e, n % DMA_RING)
        val = 16 * (n // DMA_RING + 1)
        if n >= DMA_RING and self.seen[e].get(key, 0) < val - 16:
            self.seen[e][key] = val - 16
            waits.append((key, val - 16))
        tok = (key, val)
        self._emit(e, waits, (lambda eng: eng.dma_start(out=out, in_=in_, **kw)), key, 16)
        self._mark(tok, reads, writes)
        self._handoff()
        return tok

    def idma(self, out, out_idx, in_, in_idx, bound, reads=(), writes=()):
        e = "pool"
        waits = self._deps(e, reads, writes)
        n = self.dma_n[e]
        self.dma_n[e] += 1
        key = ("d", e, n % DMA_RING)
        val = 16 * (n // DMA_RING + 1)
        if n >= DMA_RING and self.seen[e].get(key, 0) < val - 16:
            self.seen[e][key] = val - 16
            waits.append((key, val - 16))
        if not hasattr(self, "_bregs"):
            self._bregs = {}
        if bound not in self._bregs:
            self._bregs[bound] = self.nc.gpsimd.to_reg(bound)
        bound = self._bregs[bound]
        oo_ = bass.IndirectOffsetOnAxis(ap=out_idx, axis=0) if out_idx is not None else None
        io_ = bass.IndirectOffsetOnAxis(ap=in_idx, axis=0) if in_idx is not None else None
        self._emit(e, waits, (lambda eng: eng.indirect_dma_start(out=out, out_offset=oo_, in_=in_, in_offset=io_,
                                                                 bounds_check=bound, oob_is_err=False)), key, 16)
        self._mark((key, val), reads, writes)
        self._handoff()

    def _handoff(self):
        st = getattr(self, "_il", None)
        if st is None:
            return
        me = getattr(st["tls"], "idx", None)
        if me is None:
            return
        cv = st["cv"]
        with cv:
            if st["alive"][1 - me]:
                st["turn"] = 1 - me
                cv.notify_all()
                while st["turn"] != me:
                    cv.wait()

    def interleave(self, fa, fb):
        import threading
        if fa is None or fb is None:
            (fa or fb)()
            return
        st = dict(cv=threading.Condition(), turn=0, alive=[True, True], tls=threading.local(), err=[])
        self._il = st

        def runner(i, f):
            cv = st["cv"]
            with cv:
                while st["turn"] != i:
                    cv.wait()
            st["tls"].idx = i
            try:
                f()
            except BaseException as ex:
                st["err"].append(ex)
            finally:
                with cv:
                    st["alive"][i] = False
                    st["turn"] = 1 - i
                    cv.notify_all()

        ths = [threading.Thread(target=runner, args=(i, f)) for i, f in enumerate((fa, fb))]
        for th in ths:
            th.start()
        for th in ths:
            th.join()
        self._il = None
        if st["err"]:
            raise st["err"][0]

    def pipeline(self, stage_a, stage_b, n):
        stage_a(0)
        for t in range(n):
            self.interleave((lambda t=t: stage_a(t + 1)) if t + 1 < n else None, lambda t=t: stage_b(t))

    def all_tokens(self):
        toks = [(e, self.cnt[e]) for e in self.ENGS if self.cnt[e] > 0]
        for e in ("sp", "pool", "act"):
            n = self.dma_n[e]
            for j in range(DMA_RING):
                c = (n - j + DMA_RING - 1) // DMA_RING if n > j else 0
                if c > 0:
                    toks.append((("d", e, j), 16 * c))
        return toks

    def barrier(self):
        toks = self.all_tokens()
        for e in self.ENGS:
            waits = []
            for k, v in toks:
                if k == e:
                    continue
                if self.seen[e].get(k, 0) < v:
                    self.seen[e][k] = v
                    waits.append((k, v))
            self._emit(e, waits, None, None, 0)

    def finish(self):
        self.barrier()
        while self.stacks:
            self.stacks.pop().close()


def bc_mid(ap2, n):
    return ap2.unsqueeze(1).broadcast_to([ap2.shape[0], n, ap2.shape[1]])


def bc_last(ap2, n):
    return ap2.unsqueeze(2).broadcast_to([ap2.shape[0], ap2.shape[1], n])


class Prog:
    def __init__(self, nseq, debug=False, phases=("p0", "p1a", "p1b", "p2a", "p3", "p2b"), ntl=NT, cap_tiles=None):
        self.nseq = nseq
        self.ntl = ntl
        if cap_tiles is None:
            mean = nseq * ntl * 128 * 4 // 32
            cap_tiles = max(4, 4 * ((2 * mean + 511) // 512))
        self.cap = cap_tiles * 128
        self.debug = debug
        self.phases = phases
        nc = self.nc = bass.Bass("TRN2", target_bir_lowering=False)
        self.kb = KB(nc)
        ntok = nseq * SEQ
        self.ntok = ntok

        def inp(name, shape, dt=F32):
            return nc.dram_tensor(name, list(shape), dt, kind="ExternalInput").ap()

        def scr(name, shape, dt=F32):
            kind = "ExternalOutput" if (debug is True or (debug and name in debug)) else "Internal"
            return nc.dram_tensor(name, list(shape), dt, kind=kind).ap()

        I = self.I = {}
        I["x"] = inp("x", [ntok, D])
        I["c_pk"] = inp("c_pk", [nseq, 128, 8])
        I["pos_pt"] = inp("pos_pt", [nseq, 128, NT], I32)
        I["ada_w"] = inp("ada_w", [2, D, 6 * D])
        I["ada_b"] = inp("ada_b", [2, 6 * D])
        I["norm1_g"] = inp("norm1_g", [2, D])
        I["norm2_g"] = inp("norm2_g", [2, D])
        I["hyb_w_in"] = inp("hyb_w_in", [D, 2720])
        I["mla_cq_norm_g"] = inp("mla_cq_norm_g", [384])
        I["mla_ckv_norm_g"] = inp("mla_ckv_norm_g", [256])
        I["mla_w_uq"] = inp("mla_w_uq", [384, 768])
        I["mla_w_ukv"] = inp("mla_w_ukv", [256, 1024])
        I["mla_q_head_g"] = inp("mla_q_head_g", [96])
        I["mla_k_head_g"] = inp("mla_k_head_g", [96])
        I["ret_norm_g"] = inp("ret_norm_g", [512])
        I["hyb_w_out"] = inp("hyb_w_out", [D, D])
        I["swa_w_qkv"] = inp("swa_w_qkv", [D, 1280])
        I["swa_b_qkv"] = inp("swa_b_qkv", [1280])
        I["swa_q_head_g"] = inp("swa_q_head_g", [64])
        I["swa_k_head_g"] = inp("swa_k_head_g", [64])
        I["swa_sinks"] = inp("swa_sinks", [16])
        I["swa_w_out"] = inp("swa_w_out", [D, D])
        I["swa_b_out"] = inp("swa_b_out", [D])
        I["router_w"] = inp("router_w", [2, D, 32])
        I["router_b"] = inp("router_b", [2, 32])
        I["exp_w_gu"] = inp("exp_w_gu", [2, 32, D, 2048])
        I["exp_b_gu_pj"] = inp("exp_b_gu_pj", [2, 32, 128, 16])
        I["exp_w_down"] = inp("exp_w_down", [2, 32, D, D])
        I["exp_b_down"] = inp("exp_b_down", [2, 32, D])
        I["k_invf16"] = inp("k_invf16", [16])
        I["k_invf32"] = inp("k_invf32", [32])
        I["k_decayT"] = inp("k_decayT", [128, 8 * 128])
        I["k_qdec"] = inp("k_qdec", [128, 8])
        I["k_kdec"] = inp("k_kdec", [128, 8])
        I["k_cdec"] = inp("k_cdec", [128, 4])

        S = self.S = {}
        S["MOD"] = scr("MOD", [nseq, 2, 6, D])
        S["ATT"] = scr("ATT", [nseq * NT, 128, 512], BF16)
        S["XA"] = scr("XA", [ntok, D])
        S["XB"] = scr("XB", [ntok, D])
        S["H2T"] = scr("H2T", [nseq * NT, 128, 8, 128], BF16)
        S["GS"] = scr("GS", [nseq * NT, 128, 32])
        S["XG"] = scr("XG", [32 * self.cap, D], BF16)
        S["YG"] = scr("YG", [32 * self.cap, D])
        S["SLOT"] = scr("SLOT", [nseq * NT, 128, 4], I32)
        S["GK"] = scr("GK", [nseq * NT, 128, 4])
        self.out = nc.dram_tensor("out", [ntok, D], F32, kind="ExternalOutput").ap()
        self.dbufs = {}

    def db(self, name, idx):
        k = (name, idx)
        if k not in self.dbufs:
            self.dbufs[k] = Buf()
        return self.dbufs[k]

    def setup_consts(self):
        kb = self.kb
        self.ident_b = kb.sb("ident_b", [128, 128], BF16)
        self.ident_f = kb.sb("ident_f", [128, 128], F32)
        self.b_const = Buf()
        bc = self.b_const
        for idt in (self.ident_b, self.ident_f):
            kb.op("pool", lambda e, idt=idt: e.memset(idt[:], 1.0), writes=[bc])
            kb.op("pool", lambda e, idt=idt: e.affine_select(out=idt[:], in_=idt[:], pattern=[[-1, 128]],
                                                             compare_op=ALU.is_equal, fill=0.0, base=0,
                                                             channel_multiplier=1), reads=[bc], writes=[bc])
        self.mask_le = kb.sb("mask_le", [128, 128], BF16)
        self.mask_gt = kb.sb("mask_gt", [128, 128], BF16)
        kb.op("pool", lambda e: e.memset(self.mask_le[:], 1.0), writes=[bc])
        kb.op("pool", lambda e: e.affine_select(out=self.mask_le[:], in_=self.mask_le[:], pattern=[[1, 128]],
                                                compare_op=ALU.is_ge, fill=0.0, base=0, channel_multiplier=-1),
              reads=[bc], writes=[bc])
        kb.op("pool", lambda e: e.memset(self.mask_gt[:], 1.0), writes=[bc])
        kb.op("pool", lambda e: e.affine_select(out=self.mask_gt[:], in_=self.mask_gt[:], pattern=[[-1, 128]],
                                                compare_op=ALU.is_gt, fill=0.0, base=0, channel_multiplier=1),
              reads=[bc], writes=[bc])
        self.U_b = kb.sb("U_b", [128, 128], BF16)
        self.ones_b = kb.sb("ones_b", [128, 128], BF16)
        kb.op("pool", lambda e: e.memset(self.ones_b[:], 1.0), writes=[bc])
        kb.op("pool", lambda e: e.memset(self.U_b[:], 1.0), writes=[bc])
        kb.op("pool", lambda e: e.affine_select(out=self.U_b[:], in_=self.U_b[:], pattern=[[1, 128]],
                                                compare_op=ALU.is_ge, fill=0.0, base=-1, channel_multiplier=-1),
              reads=[bc], writes=[bc])
        iot_i = kb.sb("iot_i", [128, 32], I32)
        self.iotaE = kb.sb("iotaE", [128, 32], F32)
        kb.op("pool", lambda e: e.iota(out=iot_i[:], pattern=[[1, 32]], base=0, channel_multiplier=0), writes=[bc])
        kb.op("dve", lambda e: e.tensor_copy(out=self.iotaE[:], in_=iot_i[:]), reads=[bc], writes=[bc])
        kb.op("dve", lambda e: e.tensor_scalar(out=self.iotaE[:], in0=self.iotaE[:], scalar1=float(self.cap), scalar2=None,
                                               op0=ALU.mult), reads=[bc], writes=[bc])
        self.neghalf = kb.sb("neghalf", [128, 16], F32)
        kb.op("pool", lambda e: e.memset(self.neghalf[:], -0.5), writes=[bc])

    def rstd_of(self, ss, n, width, bss, tag):
        kb = self.kb
        kb.op("dve", lambda e: e.tensor_scalar(out=ss, in0=ss, scalar1=1.0 / n, scalar2=EPS,
                                               op0=ALU.mult, op1=ALU.add), reads=[bss], writes=[bss])
        kb.op("pool", lambda e: e.tensor_tensor(out=ss, in0=ss, in1=self.neghalf[:, 0:width], op=ALU.pow),
              reads=[bss, self.b_const], writes=[bss])

    def rope_tables(self, s, half, invf_name, tag):
        kb, I = self.kb, self.I
        key = (kb.phase_id, half)
        if not hasattr(self, "_rope"):
            self._rope = {}
        if key not in self._rope:
            self._rope[key] = dict(
                b=Buf(),
                pos_i=kb.sb("pos_i", [128, NT], I32), pos_f=kb.sb("pos_f", [128, NT], F32), invf=kb.sb("invf", [128, half], F32),
                ang=kb.sb("ang", [128, NT, half], F32), kq=kb.sb("kq", [128, NT, half], F32), ki=kb.sb("ki", [128, NT, half], I32),
                ys=kb.sb("ys", [128, NT, half], F32), mm=kb.sb("mmk", [128, NT, half], F32),
                cos=kb.sb("cos", [128, NT, half], F32), sin=kb.sb("sin", [128, NT, half], F32))
        R_ = self._rope[key]
        b = R_["b"]
        pos_i, pos_f, invf, ang, kq, ki, ys, mm, cos, sin = (R_[n] for n in ("pos_i", "pos_f", "invf", "ang", "kq", "ki", "ys", "mm", "cos", "sin"))
        kb.dma("sp", pos_i[:], I["pos_pt"][s], writes=[b])
        kb.dma("sp", invf[:], I[invf_name].partition_broadcast(128), writes=[b])
        kb.op("dve", lambda e: e.tensor_copy(out=pos_f[:], in_=pos_i[:]), reads=[b], writes=[b])
        kb.op("dve", lambda e: e.tensor_tensor(out=ang[:], in0=bc_last(pos_f[:, :], half), in1=bc_mid(invf[:, :], NT),
                                               op=ALU.mult), reads=[b], writes=[b])
        kb.op("dve", lambda e: e.tensor_scalar(out=kq[:], in0=ang[:], scalar1=1.0 / (2 * PI), scalar2=None,
                                               op0=ALU.mult), reads=[b], writes=[b])
        kb.op("dve", lambda e: e.tensor_copy(out=ki[:], in_=kq[:]), reads=[b], writes=[b])
        kb.op("dve", lambda e: e.tensor_copy(out=kq[:], in_=ki[:]), reads=[b], writes=[b])
        kb.op("dve", lambda e: e.scalar_tensor_tensor(out=ang[:], in0=kq[:], scalar=-2 * PI, in1=ang[:],
                                                      op0=ALU.mult, op1=ALU.add), reads=[b], writes=[b])
        lim = 3.1415925
        for shift, dst in ((0.0, sin), (PI / 2, cos)):
            kb.op("dve", lambda e, shift=shift: e.tensor_scalar(out=ys[:], in0=ang[:], scalar1=shift, scalar2=None,
                                                                op0=ALU.add), reads=[b], writes=[b])
            kb.op("dve", lambda e: e.tensor_scalar(out=mm[:], in0=ys[:], scalar1=PI, scalar2=-2 * PI,
                                                   op0=ALU.is_gt, op1=ALU.mult), reads=[b], writes=[b])
            kb.op("dve", lambda e: e.tensor_tensor(out=ys[:], in0=ys[:], in1=mm[:], op=ALU.add), reads=[b], writes=[b])
            kb.op("dve", lambda e: e.tensor_scalar(out=ys[:], in0=ys[:], scalar1=lim, scalar2=-lim,
                                                   op0=ALU.min, op1=ALU.max), reads=[b], writes=[b])
            kb.op("act", lambda e, dst=dst: e.activation(out=dst[:], in_=ys[:], func=AF.Sin), reads=[b], writes=[b])
        return cos, sin, b

    def load_w_bf16(self, dst, src, kchunks, bw):
        v = src.rearrange("(k p) n -> p k n", p=128)
        for k in range(kchunks):
            self.kb.dma("pool", dst[:, k, :], v[:, k, :], writes=[bw])

    def bcast_load(self, dst, src1d, b):
        self.kb.dma("sp", dst, src1d.partition_broadcast(128), writes=[b])

    def norm_mod_T(self, x_t, bx, gmod, shift, bmod, tmp, btmp, h_bf, bh, tp, btp, hT, bhT, ss, bss):
        kb = self.kb
        kb.op("dve", lambda e: e.scalar_tensor_tensor(out=tmp[:], in0=x_t[:], scalar=1.0, in1=x_t[:], op0=ALU.mult, op1=ALU.mult, accum_out=ss[:, 0:1]),
              reads=[bx], writes=[btmp, bss])
        self.rstd_of(ss[:, 0:1], D, 1, bss, "")
        kb.op("dve", lambda e: e.scalar_tensor_tensor(out=tmp[:], in0=x_t[:], scalar=ss[:, 0:1], in1=gmod[:],
                                                      op0=ALU.mult, op1=ALU.mult), reads=[bx, bss, bmod], writes=[btmp])
        kb.op("dve", lambda e: e.tensor_tensor(out=h_bf[:], in0=tmp[:], in1=shift[:], op=ALU.add),
              reads=[btmp, bmod], writes=[bh])
        for k in range(8):
            kb.op("pe", lambda e, k=k: e.transpose(out=tp[:, k, :], in_=h_bf[:, k * 128:(k + 1) * 128],
                                                   identity=self.ident_b[:]), reads=[bh, self.b_const], writes=[btp])
        kb.op("act", lambda e: e.copy(out=hT[:], in_=tp[:]), reads=[btp], writes=[bhT])

    def phase0(self):
        kb, I, S, nseq = self.kb, self.I, self.S, self.nseq
        kb.push()
        cin = kb.sb("cin", [128, nseq, 8], F32)
        cact = kb.sb("cact", [128, 8, nseq], F32)
        bcr = Buf()
        for s in range(nseq):
            kb.dma("sp", cin[:, s, :], I["c_pk"][s], writes=[bcr])
        kb.op("act", lambda e: e.activation(out=cact[:].rearrange("p k s -> p s k"), in_=cin[:], func=AF.Silu), reads=[bcr], writes=[bcr])
        adab = kb.sb("adab", [nseq, 6 * D], F32)
        ng = [kb.sb("ng1", [nseq, D], F32), kb.sb("ng2", [nseq, D], F32)]
        bab = Buf()
        wch = [kb.sb("wch%d" % i, [128, 8, 512], F32) for i in range(3)]
        bwch = [Buf() for _ in range(3)]
        pm = [kb.ps("p0pm%d" % i, [128, 512], F32) for i in range(2)]
        bpm = [Buf(), Buf()]
        modt = [kb.sb("modt%d" % i, [nseq, 512], F32) for i in range(3)]
        bmodt = [Buf() for _ in range(3)]
        n = 0
        for l in range(2):
            kb.dma("sp", adab[:], I["ada_b"][l].partition_broadcast(nseq), writes=[bab])
            kb.dma("sp", ng[0][:], I["norm1_g"][l].partition_broadcast(nseq), writes=[bab])
            kb.dma("sp", ng[1][:], I["norm2_g"][l].partition_broadcast(nseq), writes=[bab])
            for j in range(12):
                wv = I["ada_w"][l][:, j * 512:(j + 1) * 512].rearrange("(k p) n -> p k n", p=128)
                w_ = wch[n % 3]
                kb.dma("sp", w_[:], wv, writes=[bwch[n % 3]])
                p_ = pm[n % 2]
                for k in range(8):
                    kb.op("pe", lambda e, k=k, p_=p_, w_=w_: e.matmul(p_[0:nseq, :], lhsT=cact[:, k, :], rhs=w_[:, k, :],
                                                                    start=(k == 0), stop=(k == 7)),
                          reads=[bcr, bwch[n % 3]], writes=[bpm[n % 2]])
                m_ = modt[n % 3]
                bm_ = bmodt[n % 3]
                kb.op("dve", lambda e, p_=p_, m_=m_, j=j: e.tensor_tensor(out=m_[:], in0=p_[0:nseq, :],
                                                                       in1=adab[:, j * 512:(j + 1) * 512], op=ALU.add),
                      reads=[bpm[n % 2], bab], writes=[bm_])
                part, half = j // 2, j % 2
                if part in (1, 4):
                    g_ = ng[0] if part == 1 else ng[1]
                    kb.op("dve", lambda e, m_=m_, g_=g_, half=half: e.scalar_tensor_tensor(
                        out=m_[:], in0=m_[:], scalar=1.0, in1=g_[:, half * 512:(half + 1) * 512],
                        op0=ALU.add, op1=ALU.mult), reads=[bm_, bab], writes=[bm_])
                for s in range(nseq):
                    kb.dma("sp", S["MOD"][s, l, part, half * 512:(half + 1) * 512], m_[s:s + 1, :],
                           reads=[bm_], writes=[self.db("MOD", (s, l, part))])
                n += 1
        kb.pop()

    def phase1a(self):
        kb, I, S, nseq = self.kb, self.I, self.S, self.nseq
        kb.push()
        bw = Buf()
        w_in = kb.sb("w_in_a", [128, 8, 672], BF16)
        w_uq = kb.sb("w_uq", [128, 3, 768], BF16)
        w_ukv = kb.sb("w_ukv", [128, 2, 1024], BF16)
        self.load_w_bf16(w_in, I["hyb_w_in"][:, 0:672], 8, bw)
        self.load_w_bf16(w_uq, I["mla_w_uq"], 3, bw)
        self.load_w_bf16(w_ukv, I["mla_w_ukv"], 2, bw)
        gcq = kb.sb("gcq", [128, 384], F32)
        gckv = kb.sb("gckv", [128, 256], F32)
        gq = kb.sb("gq", [128, 96], F32)
        gk = kb.sb("gk", [128, 96], F32)
        self.bcast_load(gcq[:], I["mla_cq_norm_g"], bw)
        self.bcast_load(gckv[:], I["mla_ckv_norm_g"], bw)
        self.bcast_load(gq[:], I["mla_q_head_g"], bw)
        self.bcast_load(gk[:], I["mla_k_head_g"], bw)
        self.zero_xg("pool")

        kT = kb.sb("kT", [96, 8, SEQ], BF16)
        V = kb.sb("Vc", [128, NT, 8, 65], BF16)
        bkT = [Buf() for _ in range(NT)]
        bV = [Buf() for _ in range(NT)]
        bVones = Buf()
        kb.op("pool", lambda e: e.memset(V[:, :, :, 64:65], 1.0), writes=[bVones])

        gmod = kb.sb("gmod1", [128, D], F32)
        shift = kb.sb("shift1", [128, D], F32)
        bmod = Buf()
        x_t = [kb.sb("x_t%d" % i, [128, D], F32) for i in range(2)]
        bx = [Buf(), Buf()]
        tmp = kb.sb("tmp", [128, D], F32); btmp = Buf()
        h_bf = kb.sb("h_bf", [128, D], BF16); bh = Buf()
        hT = kb.sb("hT", [128, 8, 128], BF16); bhT = Buf()
        ss = kb.sb("ss", [128, 4], F32); bss = Buf()
        proj = kb.sb("proj", [128, 672], F32); bproj = Buf()
        sq = kb.sb("sq", [128, 8, 96], F32); bsq = Buf()
        cqn = kb.sb("cqn", [128, 384], BF16); bcqn = Buf()
        cqT = kb.sb("cqT", [128, 3, 128], BF16); bcqT = Buf()
        ckvn = kb.sb("ckvn", [128, 256], BF16); bckvn = Buf()
        ckvT = kb.sb("ckvT", [128, 2, 128], BF16); bckvT = Buf()
        q_sb = kb.sb("q_sb", [128, 8, 96], F32); bq = Buf()
        qn = kb.sb("qn", [128, 8, 96], F32); bqn = Buf()
        rq8 = kb.sb("rq8", [128, 16], F32); brq8 = Buf()
        R = kb.sb("Rr", [128, 8, 96], F32); bR = Buf()
        q_full = kb.sb("q_full", [128, 8, 96], BF16); bqf = Buf()
        qTs = [kb.sb("qT%d" % i, [96, 8, 128], BF16) for i in range(2)]; bqTs = [Buf(), Buf()]
        kv_sb = kb.sb("kv_sb", [128, 8, 128], F32); bkv = Buf()
        k_full = kb.sb("k_full", [128, 8, 96], BF16); bkf = Buf()
        kr = kb.sb("kr", [128, 32], F32); bkr = Buf()
        kr2 = kb.sb("kr2", [128, 32], F32)
        rt = [kb.sb("rt%d" % i, [128, 8, 16], F32) for i in range(4)]; brt = Buf()
        PT = [kb.sb("PT%d" % i, [128, 4, 128], BF16) for i in range(3)]; bPT = [Buf() for _ in range(3)]
        attn = kb.sb("attn", [128, 8, 64], BF16); battn = Buf()
        rden = kb.sb("rden", [128, 8], F32); brden = Buf()

        tp = [kb.ps("tp%d" % i, [128, 8, 128], BF16) for i in range(2)]; btp = [Buf(), Buf()]
        mm = [kb.ps("mm%d" % i, [128, 512], F32) for i in range(2)]; bmm = [Buf(), Buf()]
        s2 = kb.ps("s2", [128, 2, 512], F32); bs2 = [Buf(), Buf()]
        oo = [kb.ps("oo%d" % i, [128, 512], F32) for i in range(2)]; boo = [Buf(), Buf()]
        scale = 96.0 ** -0.5
        uc = {"n": 0}
        for s in range(nseq):
            cos, sin, brope = self.rope_tables(s, 16, "k_invf16", "a%d" % s)
            kb.dma("sp", gmod[:], S["MOD"][s, 0, 1].partition_broadcast(128), reads=[self.db("MOD", (s, 0, 1))], writes=[bmod])
            kb.dma("sp", shift[:], S["MOD"][s, 0, 0].partition_broadcast(128), reads=[self.db("MOD", (s, 0, 0))], writes=[bmod])
            def stage_a(t, s=s, cos=cos, sin=sin, brope=brope):
                X = x_t[t % 2]; bX = bx[t % 2]
                qT = qTs[t % 2]; bqT = bqTs[t % 2]
                kb.dma("sp", X[:], I["x"][(s * NT + t) * 128:(s * NT + t + 1) * 128, :], writes=[bX])
                self.norm_mod_T(X, bX, gmod, shift, bmod, tmp, btmp, h_bf, bh, tp[0], btp[0], hT, bhT, ss, bss)
                for gi, (c0, c1) in enumerate(((0, 512), (512, 672))):
                    for k in range(8):
                        kb.op("pe", lambda e, k=k, gi=gi, c0=c0, c1=c1: e.matmul(mm[gi][:, 0:c1 - c0], lhsT=hT[:, k, :],
                                                                              rhs=w_in[:, k, c0:c1], start=(k == 0), stop=(k == 7)),
                              reads=[bhT, bw], writes=[bmm[gi]])
                    kb.op("act", lambda e, gi=gi, c0=c0, c1=c1: e.copy(out=proj[:, c0:c1], in_=mm[gi][:, 0:c1 - c0]),
                          reads=[bmm[gi]], writes=[bproj])
                for ci, (c0, w) in enumerate(((0, 384), (384, 256), (640, 32))):
                    kb.op("dve", lambda e, c0=c0, w=w, ci=ci: e.scalar_tensor_tensor(out=tmp[:, 0:w], in0=proj[:, c0:c0 + w], scalar=1.0, in1=proj[:, c0:c0 + w], op0=ALU.mult, op1=ALU.mult, accum_out=ss[:, 1 + ci:2 + ci]), reads=[bproj], writes=[btmp, bss])
                kb.op("dve", lambda e: e.tensor_scalar(out=ss[:, 1:2], in0=ss[:, 1:2], scalar1=1.0 / 384, scalar2=EPS,
                                                       op0=ALU.mult, op1=ALU.add), reads=[bss], writes=[bss])
                kb.op("dve", lambda e: e.tensor_scalar(out=ss[:, 2:3], in0=ss[:, 2:3], scalar1=1.0 / 256, scalar2=EPS,
                                                       op0=ALU.mult, op1=ALU.add), reads=[bss], writes=[bss])
                kb.op("dve", lambda e: e.tensor_scalar(out=ss[:, 3:4], in0=ss[:, 3:4], scalar1=1.0 / 32, scalar2=EPS,
                                                       op0=ALU.mult, op1=ALU.add), reads=[bss], writes=[bss])
                kb.op("pool", lambda e: e.tensor_tensor(out=ss[:, 1:4], in0=ss[:, 1:4], in1=self.neghalf[:, 0:3], op=ALU.pow),
                      reads=[bss, self.b_const], writes=[bss])
                kb.op("dve", lambda e: e.scalar_tensor_tensor(out=cqn[:], in0=proj[:, 0:384], scalar=ss[:, 1:2], in1=gcq[:],
                                                              op0=ALU.mult, op1=ALU.mult), reads=[bproj, bss, bw], writes=[bcqn])
                kb.op("dve", lambda e: e.scalar_tensor_tensor(out=ckvn[:], in0=proj[:, 384:640], scalar=ss[:, 2:3], in1=gckv[:],
                                                              op0=ALU.mult, op1=ALU.mult), reads=[bproj, bss, bw], writes=[bckvn])
                kb.op("dve", lambda e: e.scalar_tensor_tensor(out=kr[:], in0=proj[:, 640:672], scalar=ss[:, 3:4], in1=gk[:, 64:96],
                                                              op0=ALU.mult, op1=ALU.mult), reads=[bproj, bss, bw], writes=[bkr])
                for k in range(3):
                    kb.op("pe", lambda e, k=k: e.transpose(out=tp[1][:, k, :], in_=cqn[:, k * 128:(k + 1) * 128],
                                                           identity=self.ident_b[:]), reads=[bcqn, self.b_const], writes=[btp[1]])
                for k in range(2):
                    kb.op("pe", lambda e, k=k: e.transpose(out=tp[1][:, 3 + k, :], in_=ckvn[:, k * 128:(k + 1) * 128],
                                                           identity=self.ident_b[:]), reads=[bckvn, self.b_const], writes=[btp[1]])
                kb.op("act", lambda e: e.copy(out=cqT[:], in_=tp[1][:, 0:3, :]), reads=[btp[1]], writes=[bcqT])
                kb.op("act", lambda e: e.copy(out=ckvT[:], in_=tp[1][:, 3:5, :]), reads=[btp[1]], writes=[bckvT])
                for gi, (c0, c1) in enumerate(((0, 512), (512, 768))):
                    for k in range(3):
                        kb.op("pe", lambda e, k=k, gi=gi, c0=c0, c1=c1: e.matmul(mm[gi][:, 0:c1 - c0], lhsT=cqT[:, k, :],
                                                                              rhs=w_uq[:, k, c0:c1], start=(k == 0), stop=(k == 2)),
                              reads=[bcqT, bw], writes=[bmm[gi]])
                    kb.op("act", lambda e, gi=gi, c0=c0, c1=c1: e.copy(
                        out=q_sb[:].rearrange("p h d -> p (h d)")[:, c0:c1], in_=mm[gi][:, 0:c1 - c0]),
                        reads=[bmm[gi]], writes=[bq])
                kb.op("dve", lambda e: e.tensor_tensor(out=sq[:], in0=q_sb[:], in1=q_sb[:], op=ALU.mult), reads=[bq], writes=[bsq])
                kb.op("dve", lambda e: e.tensor_reduce(out=rq8[:, 0:8], in_=sq[:, :, 0:64], axis=AX.X, op=ALU.add),
                      reads=[bsq], writes=[brq8])
                kb.op("dve", lambda e: e.tensor_reduce(out=rq8[:, 8:16], in_=sq[:, :, 64:96], axis=AX.X, op=ALU.add),
                      reads=[bsq], writes=[brq8])
                kb.op("dve", lambda e: e.tensor_scalar(out=rq8[:, 0:8], in0=rq8[:, 0:8], scalar1=1.0 / 64, scalar2=EPS,
                                                       op0=ALU.mult, op1=ALU.add), reads=[brq8], writes=[brq8])
                kb.op("dve", lambda e: e.tensor_scalar(out=rq8[:, 8:16], in0=rq8[:, 8:16], scalar1=1.0 / 32, scalar2=EPS,
                                                       op0=ALU.mult, op1=ALU.add), reads=[brq8], writes=[brq8])
                kb.op("pool", lambda e: e.tensor_tensor(out=rq8[:], in0=rq8[:], in1=self.neghalf[:, 0:16], op=ALU.pow),
                      reads=[brq8, self.b_const], writes=[brq8])
                kb.op("dve", lambda e: e.tensor_tensor(out=qn[:, :, 0:64], in0=q_sb[:, :, 0:64], in1=bc_last(rq8[:, 0:8], 64),
                                                       op=ALU.mult), reads=[bq, brq8], writes=[bqn])
                kb.op("dve", lambda e: e.tensor_tensor(out=qn[:, :, 64:96], in0=q_sb[:, :, 64:96], in1=bc_last(rq8[:, 8:16], 32),
                                                       op=ALU.mult), reads=[bq, brq8], writes=[bqn])
                kb.op("dve", lambda e: e.tensor_tensor(out=qn[:], in0=qn[:], in1=bc_mid(gq[:, :], 8), op=ALU.mult),
                      reads=[bqn, bw], writes=[bqn])
                kb.op("act", lambda e: e.copy(out=q_full[:, :, 0:64], in_=qn[:, :, 0:64]), reads=[bqn], writes=[bqf])
                cb = bc_mid(cos[:, t, :], 8)
                sb_ = bc_mid(sin[:, t, :], 8)
                x1 = qn[:, :, 64:80]
                x2 = qn[:, :, 80:96]
                kb.op("dve", lambda e: e.tensor_tensor(out=rt[0][:], in0=x1, in1=cb, op=ALU.mult), reads=[bqn, brope], writes=[brt])
                kb.op("dve", lambda e: e.tensor_tensor(out=rt[1][:], in0=x2, in1=sb_, op=ALU.mult), reads=[bqn, brope], writes=[brt])
                kb.op("dve", lambda e: e.tensor_tensor(out=rt[2][:], in0=x2, in1=cb, op=ALU.mult), reads=[bqn, brope], writes=[brt])
                kb.op("dve", lambda e: e.tensor_tensor(out=rt[3][:], in0=x1, in1=sb_, op=ALU.mult), reads=[bqn, brope], writes=[brt])
                kb.op("dve", lambda e: e.tensor_tensor(out=q_full[:, :, 64:80], in0=rt[0][:], in1=rt[1][:], op=ALU.subtract),
                      reads=[brt], writes=[bqf])
                kb.op("dve", lambda e: e.tensor_tensor(out=q_full[:, :, 80:96], in0=rt[2][:], in1=rt[3][:], op=ALU.add),
                      reads=[brt], writes=[bqf])
                for h in range(8):
                    kb.op("pe", lambda e, h=h: e.transpose(out=tp[0][0:96, h, :], in_=q_full[:, h, :], identity=self.ident_b[:]),
                          reads=[bqf, self.b_const], writes=[btp[0]])
                kb.op("act", lambda e: e.copy(out=qT[:], in_=tp[0][0:96, :, :]), reads=[btp[0]], writes=[bqT])
                for gi in range(2):
                    for k in range(2):
                        kb.op("pe", lambda e, k=k, gi=gi: e.matmul(mm[gi][:], lhsT=ckvT[:, k, :], rhs=w_ukv[:, k, gi * 512:(gi + 1) * 512],
                                                                 start=(k == 0), stop=(k == 1)), reads=[bckvT, bw], writes=[bmm[gi]])
                    kb.op("act", lambda e, gi=gi: e.copy(out=kv_sb[:].rearrange("p h d -> p (h d)")[:, gi * 512:(gi + 1) * 512],
                                                        in_=mm[gi][:]), reads=[bmm[gi]], writes=[bkv])
                kb.op("dve", lambda e, t=t: e.tensor_copy(out=V[:, t, :, 0:64], in_=kv_sb[:, :, 64:128]),
                      reads=[bkv, bVones], writes=[bV[t]])
                kb.op("dve", lambda e: e.tensor_tensor(out=sq[:, :, 0:64], in0=kv_sb[:, :, 0:64], in1=kv_sb[:, :, 0:64], op=ALU.mult),
                      reads=[bkv], writes=[bsq])
                kb.op("dve", lambda e: e.tensor_reduce(out=rq8[:, 0:8], in_=sq[:, :, 0:64], axis=AX.X, op=ALU.add),
                      reads=[bsq], writes=[brq8])
                kb.op("dve", lambda e: e.tensor_scalar(out=rq8[:, 0:8], in0=rq8[:, 0:8], scalar1=1.0 / 64, scalar2=EPS,
                                                       op0=ALU.mult, op1=ALU.add), reads=[brq8], writes=[brq8])
                kb.op("pool", lambda e: e.tensor_tensor(out=rq8[:, 0:8], in0=rq8[:, 0:8], in1=self.neghalf[:, 0:8], op=ALU.pow),
                      reads=[brq8, self.b_const], writes=[brq8])
                kb.op("dve", lambda e: e.tensor_tensor(out=sq[:, :, 0:64], in0=kv_sb[:, :, 0:64], in1=bc_last(rq8[:, 0:8], 64),
                                                       op=ALU.mult), reads=[bkv, brq8], writes=[bsq])
                kb.op("dve", lambda e: e.tensor_tensor(out=k_full[:, :, 0:64], in0=sq[:, :, 0:64], in1=bc_mid(gk[:, 0:64], 8),
                                                        op=ALU.mult), reads=[bsq, bw], writes=[bkf])
                c1_ = cos[:, t, :]
                s1_ = sin[:, t, :]
                kb.op("dve", lambda e: e.tensor_tensor(out=rt[0][:, 0, :], in0=kr[:, 0:16], in1=c1_, op=ALU.mult), reads=[bkr, brope], writes=[brt])
                kb.op("dve", lambda e: e.tensor_tensor(out=rt[1][:, 0, :], in0=kr[:, 16:32], in1=s1_, op=ALU.mult), reads=[bkr, brope], writes=[brt])
                kb.op("dve", lambda e: e.tensor_tensor(out=rt[2][:, 0, :], in0=kr[:, 16:32], in1=c1_, op=ALU.mult), reads=[bkr, brope], writes=[brt])
                kb.op("dve", lambda e: e.tensor_tensor(out=rt[3][:, 0, :], in0=kr[:, 0:16], in1=s1_, op=ALU.mult), reads=[bkr, brope], writes=[brt])
                kb.op("dve", lambda e: e.tensor_tensor(out=kr2[:, 0:16], in0=rt[0][:, 0, :], in1=rt[1][:, 0, :], op=ALU.subtract),
                      reads=[brt], writes=[bkr])
                kb.op("dve", lambda e: e.tensor_tensor(out=kr2[:, 16:32], in0=rt[2][:, 0, :], in1=rt[3][:, 0, :], op=ALU.add),
                      reads=[brt], writes=[bkr])
                kb.op("dve", lambda e: e.tensor_copy(out=k_full[:, :, 64:96], in_=bc_mid(kr2[:, :], 8)), reads=[bkr], writes=[bkf])
                for h in range(8):
                    kb.op("pe", lambda e, h=h: e.transpose(out=tp[1][0:96, h, :], in_=k_full[:, h, :], identity=self.ident_b[:]),
                          reads=[bkf, self.b_const], writes=[btp[1]])
                kb.op("act", lambda e, t=t: e.copy(out=kT[:, :, t * 128:(t + 1) * 128], in_=tp[1][0:96, :, :]),
                      reads=[btp[1]], writes=[bkT[t]])
            def stage_b(t, s=s):
                qT = qTs[t % 2]; bqT = bqTs[t % 2]
                units = []
                for h in range(8):
                    for a in range(0, t + 1, 4):
                        units.append((h, a, min(a + 4, t + 1)))

                def emit_S(ui, u):
                    h, a, b = u
                    bank = ui % 2
                    for kt in range(a, b):
                        kb.op("pe", lambda e, kt=kt, h=h, a=a, bank=bank: e.matmul(
                            s2[:, bank, (kt - a) * 128:(kt - a + 1) * 128], lhsT=kT[:, h, kt * 128:(kt + 1) * 128],
                            rhs=qT[:, h, :], start=True, stop=True), reads=[bkT[kt], bqT], writes=[bs2[bank]])

                base = uc["n"]
                emit_S(base, units[0])
                for i, u in enumerate(units):
                    ui = base + i
                    h, a, b = u
                    if i + 1 < len(units):
                        emit_S(ui + 1, units[i + 1])
                    bank = ui % 2
                    P = PT[ui % 3]; bP = bPT[ui % 3]
                    n = (b - a) * 128
                    kb.op("act", lambda e, P=P, bank=bank, n=n: e.activation(
                        out=P[:].rearrange("p a b -> p (a b)")[:, 0:n], in_=s2[:, bank, 0:n], func=AF.Exp, scale=scale),
                        reads=[bs2[bank]], writes=[bP])
                    if b == t + 1:
                        kb.op("dve", lambda e, P=P, j=t - a: e.tensor_tensor(out=P[:, j, :], in0=P[:, j, :], in1=self.mask_le[:],
                                                                           op=ALU.mult), reads=[bP, self.b_const], writes=[bP])
                    ob = oo[h // 4]
                    for kt in range(a, b):
                        kb.op("pe", lambda e, kt=kt, h=h, a=a, P=P, ob=ob: e.matmul(
                            ob[:, (h % 4) * 65:(h % 4) * 65 + 65], lhsT=P[:, kt - a, :], rhs=V[:, kt, h, :],
                            start=(kt == 0), stop=(kt == t)), reads=[bP, bV[kt], bVones], writes=[boo[h // 4]])
                uc["n"] += len(units)
                for hb in range(2):
                    ov = oo[hb][:, 0:260].rearrange("p (h d) -> p h d", d=65)
                    kb.op("dve", lambda e, hb=hb, ov=ov: e.reciprocal(out=rden[:, hb * 4:(hb + 1) * 4], in_=ov[:, :, 64]),
                          reads=[boo[hb]], writes=[brden])
                    kb.op("dve", lambda e, hb=hb, ov=ov: e.tensor_tensor(out=attn[:, hb * 4:(hb + 1) * 4, :], in0=ov[:, :, 0:64],
                                                                        in1=bc_last(rden[:, hb * 4:(hb + 1) * 4], 64), op=ALU.mult),
                          reads=[boo[hb], brden], writes=[battn])
                kb.dma("sp", S["ATT"][s * NT + t], attn[:].rearrange("p h d -> p (h d)"), reads=[battn],
                       writes=[self.db("ATT", s * NT + t)])
            kb.pipeline(stage_a, stage_b, self.ntl)
        kb.pop()

    def alloc_router(self, l):
        kb, I = self.kb, self.I
        r = {}
        r["bw"] = Buf()
        r["rw"] = kb.sb("rw", [128, 8, 32], F32)
        kb.dma("sp", r["rw"][:], I["router_w"][l].rearrange("(k p) n -> p k n", p=128), writes=[r["bw"]])
        r["rb"] = kb.sb("rb", [128, 32], F32)
        self.bcast_load(r["rb"][:], I["router_b"][l], r["bw"])
        r["rwh"] = kb.sb("rwh", [128, 8, 32], BF16)
        r["rwl"] = kb.sb("rwl", [128, 8, 32], BF16)
        kb.op("dve", lambda e: e.tensor_copy(out=r["rwh"][:], in_=r["rw"][:]), reads=[r["bw"]], writes=[r["bw"]])
        kb.op("dve", lambda e: e.tensor_tensor(out=r["rwl"][:], in0=r["rw"][:], in1=r["rwh"][:], op=ALU.subtract),
              reads=[r["bw"]], writes=[r["bw"]])
        r["h2f"] = kb.sb("h2f", [128, D], F32); r["bh2f"] = Buf()
        r["h2hi"] = kb.sb("h2hi", [128, D], BF16); r["bh2hi"] = Buf()
        r["h2lo"] = kb.sb("h2lo", [128, D], BF16); r["bh2lo"] = Buf()
        r["h2Tl"] = kb.sb("h2Tl", [128, 8, 128], BF16); r["bh2Tl"] = Buf()
        r["h2Tb"] = kb.sb("h2Tb", [128, 8, 128], BF16); r["bh2Tb"] = Buf()
        r["lg"] = kb.sb("lg", [128, 32], F32); r["blg"] = Buf()
        r["m8"] = kb.sb("m8", [128, 8], F32)
        r["msk"] = kb.sb("msk", [128, 32], F32)
        r["ex"] = kb.sb("ex", [128, 32], F32)
        r["den"] = kb.sb("den", [128, 2], F32)
        r["G"] = kb.sb("Gt", [128, 32], F32); r["bG"] = Buf()
        r["ss"] = kb.sb("ss2", [128, 1], F32); r["bss"] = Buf()
        r["cnt"] = kb.sb("cnt_b", [128, 32], F32); r["bcnt"] = Buf()
        kb.op("pool", lambda e: e.memset(r["cnt"][:], 0.0), writes=[r["bcnt"]])
        r["Mb"] = kb.sb("Mb", [128, 32], BF16)
        for n_ in ("posf", "valid", "slotm", "oh", "junk", "Gv"):
            r[n_] = kb.sb(n_, [128, 32], F32)
        r["slotf"] = kb.sb("slotf", [128, 4], F32)
        r["sloti"] = kb.sb("sloti", [128, 4], I32); r["bsloti"] = Buf()
        r["gk"] = kb.sb("gk", [128, 4], F32); r["bgk"] = Buf()
        r["brt"] = Buf()
        return r

    def norm2_router(self, r, x1, bx1, gmod2, shift2, bmod, tmp, btmp, tp, btp, mmp, bmmp, tile_idx):
        kb, S = self.kb, self.S
        ss, bss = r["ss"], r["bss"]
        kb.op("dve", lambda e: e.scalar_tensor_tensor(out=tmp[:], in0=x1[:], scalar=1.0, in1=x1[:], op0=ALU.mult, op1=ALU.mult, accum_out=ss[:, 0:1]),
              reads=[bx1], writes=[btmp, bss])
        self.rstd_of(ss[:, 0:1], D, 1, bss, "")
        kb.op("dve", lambda e: e.scalar_tensor_tensor(out=tmp[:], in0=x1[:], scalar=ss[:, 0:1], in1=gmod2[:],
                                                      op0=ALU.mult, op1=ALU.mult), reads=[bx1, bss, bmod], writes=[btmp])
        kb.op("dve", lambda e: e.tensor_tensor(out=r["h2f"][:], in0=tmp[:], in1=shift2[:], op=ALU.add),
              reads=[btmp, bmod], writes=[r["bh2f"]])
        kb.op("act", lambda e: e.copy(out=r["h2hi"][:], in_=r["h2f"][:]), reads=[r["bh2f"]], writes=[r["bh2hi"]])
        kb.op("dve", lambda e: e.tensor_tensor(out=r["h2lo"][:], in0=r["h2f"][:], in1=r["h2hi"][:], op=ALU.subtract),
              reads=[r["bh2f"], r["bh2hi"]], writes=[r["bh2lo"]])
        for k in range(8):
            kb.op("pe", lambda e, k=k: e.transpose(out=tp[0][:, k, :], in_=r["h2hi"][:, k * 128:(k + 1) * 128],
                                                   identity=self.ident_b[:]), reads=[r["bh2hi"], self.b_const], writes=[btp[0]])
        for k in range(8):
            kb.op("pe", lambda e, k=k: e.transpose(out=tp[1][:, k, :], in_=r["h2lo"][:, k * 128:(k + 1) * 128],
                                                   identity=self.ident_b[:]), reads=[r["bh2lo"], self.b_const], writes=[btp[1]])
        kb.op("act", lambda e: e.copy(out=r["h2Tb"][:], in_=tp[0][:]), reads=[btp[0]], writes=[r["bh2Tb"]])
        kb.op("dve", lambda e: e.tensor_copy(out=r["h2Tl"][:], in_=tp[1][:]), reads=[btp[1]], writes=[r["bh2Tl"]])
        if self.debug:
            kb.dma("sp", S["H2T"][tile_idx], r["h2Tb"][:], reads=[r["bh2Tb"]], writes=[self.db("H2T", tile_idx)])
        if _STOP <= 6:
            return
        passes = [("h2Tb", "rwh"), ("h2Tl", "rwh"), ("h2Tb", "rwl")]
        for pi, (a_, w_) in enumerate(passes):
            for k in range(8):
                kb.op("pe", lambda e, k=k, a_=a_, w_=w_, pi=pi: e.matmul(mmp[:, 0:32], lhsT=r[a_][:, k, :], rhs=r[w_][:, k, :],
                                                                       start=(pi == 0 and k == 0), stop=(pi == 2 and k == 7)),
                      reads=[r["bh2Tb"], r["bh2Tl"], r["bw"]], writes=[bmmp])
        lg, m8, msk, ex, den, G = r["lg"], r["m8"], r["msk"], r["ex"], r["den"], r["G"]
        bl = r["blg"]
        kb.op("dve", lambda e: e.tensor_tensor(out=lg[:], in0=mmp[:, 0:32], in1=r["rb"][:], op=ALU.add),
              reads=[bmmp, r["bw"]], writes=[bl])
        if _STOP <= 7:
            return
        kb.op("dve", lambda e: e.max(out=m8[:], in_=lg[:]), reads=[bl], writes=[bl])
        kb.op("dve", lambda e: e.tensor_scalar(out=msk[:], in0=lg[:], scalar1=m8[:, 3:4], scalar2=None, op0=ALU.is_ge),
              reads=[bl], writes=[bl])
        kb.op("dve", lambda e: e.tensor_scalar(out=den[:, 1:2], in0=m8[:, 0:1], scalar1=-1.0, scalar2=None, op0=ALU.mult),
              reads=[bl], writes=[bl])
        kb.op("act", lambda e: e.activation(out=ex[:], in_=lg[:], func=AF.Exp, bias=den[:, 1:2], scale=1.0),
              reads=[bl], writes=[bl])
        kb.op("dve", lambda e: e.scalar_tensor_tensor(out=ex[:], in0=ex[:], scalar=1.0, in1=msk[:], op0=ALU.mult, op1=ALU.mult, accum_out=den[:, 0:1]), reads=[bl], writes=[bl])
        kb.op("dve", lambda e: e.reciprocal(out=den[:, 0:1], in_=den[:, 0:1]), reads=[bl], writes=[bl])
        kb.op("dve", lambda e: e.tensor_scalar(out=G[:], in0=ex[:], scalar1=den[:, 0:1], scalar2=None, op0=ALU.mult),
              reads=[bl], writes=[r["bG"]])
        if self.debug:
            kb.dma("sp", S["GS"][tile_idx], G[:], reads=[r["bG"]], writes=[self.db("GS", tile_idx)])
        cap = self.cap
        brt = r["brt"]
        kb.op("dve", lambda e: e.tensor_copy(out=r["Mb"][:], in_=msk[:]), reads=[bl], writes=[brt])
        kb.op("pe", lambda e: e.matmul(mmp[:, 32:64], lhsT=self.U_b[:], rhs=r["Mb"][:], start=True, stop=True),
              reads=[brt, self.b_const], writes=[bmmp])
        kb.op("pe", lambda e: e.matmul(mmp[:, 64:96], lhsT=self.ones_b[:], rhs=r["Mb"][:], start=True, stop=True),
              reads=[brt, self.b_const], writes=[bmmp])
        kb.op("dve", lambda e: e.tensor_tensor(out=r["posf"][:], in0=mmp[:, 32:64], in1=r["cnt"][:], op=ALU.add),
              reads=[bmmp, r["bcnt"]], writes=[brt])
        kb.op("dve", lambda e: e.tensor_tensor(out=r["cnt"][:], in0=mmp[:, 64:96], in1=r["cnt"][:], op=ALU.add),
              reads=[bmmp, r["bcnt"]], writes=[r["bcnt"]])
        kb.op("dve", lambda e: e.tensor_scalar(out=r["valid"][:], in0=r["posf"][:], scalar1=float(cap), scalar2=None, op0=ALU.is_lt),
              reads=[brt], writes=[brt])
        kb.op("dve", lambda e: e.tensor_tensor(out=r["slotm"][:], in0=r["posf"][:], in1=self.iotaE[:], op=ALU.add),
              reads=[brt, self.b_const], writes=[brt])
        kb.op("dve", lambda e: e.tensor_scalar(out=r["junk"][:], in0=r["valid"][:], scalar1=-1.0e6, scalar2=1.0e6,
                                               op0=ALU.mult, op1=ALU.add), reads=[brt], writes=[brt])
        kb.op("dve", lambda e: e.tensor_tensor(out=r["slotm"][:], in0=r["slotm"][:], in1=r["junk"][:], op=ALU.add),
              reads=[brt], writes=[brt])
        kb.op("dve", lambda e: e.tensor_tensor(out=r["Gv"][:], in0=G[:], in1=r["valid"][:], op=ALU.mult),
              reads=[brt, r["bG"]], writes=[brt])
        for k in range(4):
            kb.op("dve", lambda e, k=k: e.tensor_scalar(out=r["oh"][:], in0=lg[:], scalar1=m8[:, k:k + 1], scalar2=None, op0=ALU.is_equal),
                  reads=[bl, brt], writes=[brt])
            kb.op("dve", lambda e, k=k: e.scalar_tensor_tensor(out=r["junk"][:], in0=r["oh"][:], scalar=1.0, in1=r["slotm"][:],
                                                              op0=ALU.mult, op1=ALU.mult, accum_out=r["slotf"][:, k:k + 1]),
                  reads=[brt], writes=[brt])
            kb.op("dve", lambda e, k=k: e.scalar_tensor_tensor(out=r["junk"][:], in0=r["oh"][:], scalar=1.0, in1=r["Gv"][:],
                                                              op0=ALU.mult, op1=ALU.mult, accum_out=r["gk"][:, k:k + 1]),
                  reads=[brt, r["bgk"]], writes=[brt, r["bgk"]])
        kb.op("dve", lambda e: e.tensor_copy(out=r["sloti"][:], in_=r["slotf"][:]), reads=[brt, r["bsloti"]], writes=[r["bsloti"]])
        kb.dma("sp", S["SLOT"][tile_idx], r["sloti"][:], reads=[r["bsloti"]], writes=[self.db("SLOT", tile_idx)])
        kb.dma("sp", S["GK"][tile_idx], r["gk"][:], reads=[r["bgk"]], writes=[self.db("GK", tile_idx)])
        for k in range(4):
            kb.idma(S["XG"], r["sloti"][:, k:k + 1], r["h2hi"][:], None, 32 * cap - 1, reads=[r["bsloti"], r["bh2hi"]])

    def phase1b(self):
        kb, I, S, nseq = self.kb, self.I, self.S, self.nseq
        kb.push()
        bw = Buf()
        w_in = kb.sb("w_in_b", [128, 8, 2048], BF16)
        w_out = kb.sb("w_out", [128, 8, D], BF16)
        self.load_w_bf16(w_in, I["hyb_w_in"][:, 672:2720], 8, bw)
        self.load_w_bf16(w_out, I["hyb_w_out"], 8, bw)
        retg = kb.sb("retg", [128, 512], F32)
        self.bcast_load(retg[:], I["ret_norm_g"], bw)
        decT = kb.sb("decT", [128, 8 * 128], F32)
        qdec = kb.sb("qdec", [128, 8], F32)
        kdec = kb.sb("kdec", [128, 8], F32)
        cdec = kb.sb("cdec", [128, 4], F32)
        kb.dma("sp", decT[:], I["k_decayT"], writes=[bw])
        kb.dma("sp", qdec[:], I["k_qdec"], writes=[bw])
        kb.dma("sp", kdec[:], I["k_kdec"], writes=[bw])
        kb.dma("sp", cdec[:], I["k_cdec"], writes=[bw])
        r = self.alloc_router(0)

        mods = {n: kb.sb(n, [128, D], F32) for n in ("gmod1", "shift1", "gate1", "gmod2", "shift2")}
        bmod = Buf()
        x_t = [kb.sb("x_t%d" % i, [128, D], F32) for i in range(2)]; bx = [Buf(), Buf()]
        tmp = kb.sb("tmp", [128, D], F32); btmp = Buf()
        h_bf = kb.sb("h_bf", [128, D], BF16); bh = Buf()
        hT = kb.sb("hT", [128, 8, 128], BF16); bhT = Buf()
        ss = kb.sb("ss", [128, 4], F32); bss = Buf()
        raw = [kb.sb("raw%d" % i, [128, 8, 64], F32) for i in range(2)]; braw = [Buf(), Buf()]
        rr = [kb.sb("rr%d" % i, [128, 8, 64], F32) for i in range(2)]; brr = [Buf(), Buf()]
        rt = [kb.sb("rt%d" % i, [128, 8, 32], F32) for i in range(4)]; brt = Buf()
        rq_bf = kb.sb("rq_bf", [128, 8, 64], BF16); brqb = Buf()
        rqd_bf = kb.sb("rqd_bf", [128, 8, 64], BF16); brqd = Buf()
        rk_bf = kb.sb("rk_bf", [128, 8, 64], BF16); brkb = Buf()
        rkd_bf_2 = [kb.sb("rkd_bf%d" % i, [128, 8, 64], BF16) for i in range(2)]; brkd_2 = [Buf(), Buf()]
        v_bf_2 = [kb.sb("v_bf%d" % i, [128, 8, 64], BF16) for i in range(2)]; bv_2 = [Buf(), Buf()]
        sg_2 = [kb.sb("sg%d" % i, [128, 512], F32) for i in range(2)]; bsg_2 = [Buf(), Buf()]
        rqT_2 = [kb.sb("rqT%d" % i, [128, 8, 128], BF16) for i in range(2)]; brqT_2 = [Buf(), Buf()]
        rkT_2 = [kb.sb("rkT%d" % i, [128, 4, 128], BF16) for i in range(2)]; brkT_2 = [Buf(), Buf()]
        Sd = kb.sb("Sd", [128, 8, 128], BF16); bSd = Buf()
        st_f = kb.sb("st_f", [128, 4, 128], F32); bstf = Buf()
        st_b = kb.sb("st_b", [128, 4, 128], BF16); bstb = Buf()
        kb.op("pool", lambda e: e.memset(st_f[:], 0.0), writes=[bstf])
        o_sb = kb.sb("o_sb", [128, 8, 64], F32); bo = Buf()
        oc = kb.sb("oc", [128, 8, 64], F32); boc = Buf()
        st8 = kb.sb("st8", [128, 16], F32); bst8 = Buf()
        mixcat_2 = [kb.sb("mixcat%d" % i, [128, D], BF16) for i in range(2)]; bmixa_2 = [Buf(), Buf()]; bmixy_2 = [Buf(), Buf()]
        tmpB = kb.sb("tmpB", [128, D], F32); btmpB = Buf()
        mixT = kb.sb("mixT", [128, 8, 128], BF16); bmixT = Buf()
        x1 = kb.sb("x1", [128, D], F32); bx1 = Buf()

        tp = [kb.ps("tp%d" % i, [128, 8, 128], BF16) for i in range(2)]; btp = [Buf(), Buf()]
        mm = [kb.ps("mm%d" % i, [128, 512], F32) for i in range(2)]; bmm = [Buf(), Buf()]
        s2 = kb.ps("s2", [128, 2, 512], F32); bs2 = [Buf(), Buf()]
        oo = [kb.ps("oo%d" % i, [128, 512], F32) for i in range(2)]; boo = [Buf(), Buf()]
        tpB = [s2[:, i, :].bitcast(BF16).rearrange("p (k m) -> p k m", m=128) for i in range(2)]
        for s in range(nseq):
            cos, sin, brope = self.rope_tables(s, 32, "k_invf32", "b%d" % s)
            for n_, part in (("gmod1", 1), ("shift1", 0), ("gate1", 2), ("gmod2", 4), ("shift2", 3)):
                kb.dma("sp", mods[n_][:], S["MOD"][s, 0, part].partition_broadcast(128), reads=[self.db("MOD", (s, 0, part))], writes=[bmod])
            def stage_a(t, s=s, cos=cos, sin=sin, brope=brope):
                P_ = t % 2
                X = x_t[P_]; bX = bx[P_]
                v_bf = v_bf_2[P_]; bv = bv_2[P_]; sg = sg_2[P_]; bsg = bsg_2[P_]; rqT = rqT_2[P_]; brqT = brqT_2[P_]
                rkT = rkT_2[P_]; brkT = brkT_2[P_]; rkd_bf = rkd_bf_2[P_]; brkd = brkd_2[P_]
                mixcat = mixcat_2[P_]; bmixa = bmixa_2[P_]; bmixy = bmixy_2[P_]
                ti = s * NT + t
                kb.dma("sp", X[:], I["x"][ti * 128:(ti + 1) * 128, :], writes=[bX])
                kb.dma("sp", mixcat[:, 0:512], S["ATT"][ti], reads=[self.db("ATT", ti)], writes=[bmixa])
                self.norm_mod_T(X, bX, mods["gmod1"], mods["shift1"], bmod, tmp, btmp, h_bf, bh, tp[0], btp[0], hT, bhT, ss, bss)
                cb = bc_mid(cos[:, t, :], 8)
                sb_ = bc_mid(sin[:, t, :], 8)
                for gi in range(4):
                    p_ = mm[gi % 2]; bp_ = bmm[gi % 2]
                    for k in range(8):
                        kb.op("pe", lambda e, k=k, gi=gi, p_=p_: e.matmul(p_[:], lhsT=hT[:, k, :], rhs=w_in[:, k, gi * 512:(gi + 1) * 512],
                                                                       start=(k == 0), stop=(k == 7)), reads=[bhT, bw], writes=[bp_])
                    if gi < 2:
                        rw_ = raw[gi]; brw_ = braw[gi]; ro = rr[gi]; bro = brr[gi]
                        kb.op("act", lambda e, p_=p_, rw_=rw_: e.copy(out=rw_[:].rearrange("p h d -> p (h d)"), in_=p_[:]),
                              reads=[bp_], writes=[brw_])
                        x1_ = rw_[:, :, 0:32]; x2_ = rw_[:, :, 32:64]
                        kb.op("dve", lambda e, x1_=x1_: e.tensor_tensor(out=rt[0][:], in0=x1_, in1=cb, op=ALU.mult), reads=[brw_, brope], writes=[brt])
                        kb.op("dve", lambda e, x2_=x2_: e.tensor_tensor(out=rt[1][:], in0=x2_, in1=sb_, op=ALU.mult), reads=[brw_, brope], writes=[brt])
                        kb.op("dve", lambda e, x2_=x2_: e.tensor_tensor(out=rt[2][:], in0=x2_, in1=cb, op=ALU.mult), reads=[brw_, brope], writes=[brt])
                        kb.op("dve", lambda e, x1_=x1_: e.tensor_tensor(out=rt[3][:], in0=x1_, in1=sb_, op=ALU.mult), reads=[brw_, brope], writes=[brt])
                        kb.op("dve", lambda e, ro=ro: e.tensor_tensor(out=ro[:, :, 0:32], in0=rt[0][:], in1=rt[1][:], op=ALU.subtract),
                              reads=[brt], writes=[bro])
                        kb.op("dve", lambda e, ro=ro: e.tensor_tensor(out=ro[:, :, 32:64], in0=rt[2][:], in1=rt[3][:], op=ALU.add),
                              reads=[brt], writes=[bro])
                        if gi == 0:
                            kb.op("act", lambda e, ro=ro: e.copy(out=rq_bf[:], in_=ro[:]), reads=[bro], writes=[brqb])
                            kb.op("dve", lambda e, ro=ro: e.tensor_tensor(out=rqd_bf[:], in0=ro[:], in1=bc_last(qdec[:, :], 64), op=ALU.mult),
                                  reads=[bro, bw], writes=[brqd])
                        else:
                            kb.op("act", lambda e, ro=ro: e.mul(out=rk_bf[:], in_=ro[:], mul=0.125), reads=[bro], writes=[brkb])
                            kb.op("dve", lambda e, ro=ro: e.scalar_tensor_tensor(out=rkd_bf[:], in0=ro[:], scalar=0.125,
                                                                                in1=bc_last(kdec[:, :], 64), op0=ALU.mult, op1=ALU.mult),
                                  reads=[bro, bw], writes=[brkd])
                    elif gi == 2:
                        kb.op("act", lambda e, p_=p_: e.copy(out=v_bf[:].rearrange("p h d -> p (h d)"), in_=p_[:]), reads=[bp_], writes=[bv])
                    else:
                        kb.op("act", lambda e, p_=p_: e.activation(out=sg[:], in_=p_[:], func=AF.Silu), reads=[bp_], writes=[bsg])
                for i in range(4):
                    kb.op("pe", lambda e, i=i: e.transpose(out=tp[1][:, i, :], in_=rq_bf[:, 2 * i:2 * i + 2, :].rearrange("p h d -> p (h d)"),
                                                           identity=self.ident_b[:]), reads=[brqb, self.b_const], writes=[btp[1]])
                for i in range(4):
                    kb.op("pe", lambda e, i=i: e.transpose(out=tp[1][:, 4 + i, :], in_=rqd_bf[:, 2 * i:2 * i + 2, :].rearrange("p h d -> p (h d)"),
                                                           identity=self.ident_b[:]), reads=[brqd, self.b_const], writes=[btp[1]])
                for i in range(4):
                    kb.op("pe", lambda e, i=i: e.transpose(out=tp[0][:, i, :], in_=rk_bf[:, 2 * i:2 * i + 2, :].rearrange("p h d -> p (h d)"),
                                                           identity=self.ident_b[:]), reads=[brkb, self.b_const], writes=[btp[0]])
                kb.op("act", lambda e: e.copy(out=rqT[:], in_=tp[1][:]), reads=[btp[1]], writes=[brqT])
                kb.op("dve", lambda e: e.tensor_copy(out=rkT[:], in_=tp[0][:, 0:4, :]), reads=[btp[0]], writes=[brkT])
            def stage_b(t, s=s):
                P_ = t % 2
                X = x_t[P_]; bX = bx[P_]
                v_bf = v_bf_2[P_]; bv = bv_2[P_]; sg = sg_2[P_]; bsg = bsg_2[P_]; rqT = rqT_2[P_]; brqT = brqT_2[P_]
                rkT = rkT_2[P_]; brkT = brkT_2[P_]; rkd_bf = rkd_bf_2[P_]; brkd = brkd_2[P_]
                mixcat = mixcat_2[P_]; bmixa = bmixa_2[P_]; bmixy = bmixy_2[P_]
                ti = s * NT + t
                tmp = tmpB; btmp = btmpB
                for h in range(8):
                    i, o = h // 2, (h % 2) * 64
                    kb.op("pe", lambda e, h=h, i=i, o=o: e.matmul(s2[:, h % 2, i * 128:(i + 1) * 128],
                                                                lhsT=rkT[o:o + 64, i, :], rhs=rqT[o:o + 64, i, :], start=True, stop=True),
                          reads=[brkT, brqT], writes=[bs2[h % 2]])
                for hb in range(2):
                    kb.op("dve", lambda e, hb=hb: e.tensor_tensor(out=Sd[:, hb * 4:(hb + 1) * 4, :].rearrange("p h q -> p (h q)"),
                                                                 in0=s2[:, hb, :], in1=decT[:, hb * 512:(hb + 1) * 512], op=ALU.mult),
                          reads=[bs2[hb], bw], writes=[bSd])
                if _STOP <= 1.5:
                    return
                for i in range(4):
                    if t > 0:
                        kb.op("pe", lambda e, i=i: e.matmul(oo[0][:, i * 128:(i + 1) * 128], lhsT=rqT[:, 4 + i, :],
                                                            rhs=st_b[:, i, :], start=True, stop=False, skip_group_check=True),
                              reads=[brqT, bstb], writes=[boo[0]])
                    for par in range(2):
                        h = 2 * i + par
                        kb.op("pe", lambda e, h=h, i=i, par=par: e.matmul(oo[0][:, h * 64:(h + 1) * 64], lhsT=Sd[:, par * 4 + i, :],
                                                                        rhs=v_bf[:, h, :], start=(t == 0), stop=(t == 0 or par == 1),
                                                                        skip_group_check=(t > 0)),
                              reads=[bSd, bv], writes=[boo[0]])
                if _STOP <= 2:
                    return
                for i in range(4):
                    kb.op("pe", lambda e, i=i: e.matmul(oo[1][:, i * 128:(i + 1) * 128],
                                                        lhsT=rkd_bf[:, 2 * i:2 * i + 2, :].rearrange("p h d -> p (h d)"),
                                                        rhs=v_bf[:, 2 * i:2 * i + 2, :].rearrange("p h d -> p (h d)"), start=True, stop=True),
                          reads=[brkd, bv], writes=[boo[1]])
                kvv = oo[1][:].rearrange("p (i c) -> p i c", c=128)
                for half in range(2):
                    po = half * 64
                    if t == 0:
                        kb.op("dve", lambda e, po=po: e.tensor_copy(out=st_f[po:po + 64, :, po:po + 64], in_=kvv[po:po + 64, :, po:po + 64]),
                              reads=[boo[1]], writes=[bstf])
                    else:
                        for i in range(4):
                            kb.op("dve", lambda e, po=po, i=i: e.scalar_tensor_tensor(
                                out=st_f[po:po + 64, i, po:po + 64], in0=st_f[po:po + 64, i, po:po + 64], scalar=cdec[po:po + 64, i:i + 1],
                                in1=kvv[po:po + 64, i, po:po + 64], op0=ALU.mult, op1=ALU.add), reads=[boo[1], bstf, bw], writes=[bstf])
                kb.op("act", lambda e: e.copy(out=st_b[:], in_=st_f[:]), reads=[bstf], writes=[bstb])
                if _STOP <= 3:
                    return
                kb.op("act", lambda e: e.copy(out=o_sb[:].rearrange("p h d -> p (h d)"), in_=oo[0][:]), reads=[boo[0]], writes=[bo])
                kb.op("dve", lambda e: e.tensor_reduce(out=st8[:, 0:8], in_=o_sb[:], axis=AX.X, op=ALU.add), reads=[bo], writes=[bst8])
                kb.op("dve", lambda e: e.tensor_scalar(out=st8[:, 0:8], in0=st8[:, 0:8], scalar1=-1.0 / 64, scalar2=None, op0=ALU.mult),
                      reads=[bst8], writes=[bst8])
                kb.op("dve", lambda e: e.tensor_tensor(out=oc[:], in0=o_sb[:], in1=bc_last(st8[:, 0:8], 64), op=ALU.add),
                      reads=[bo, bst8], writes=[boc])
                kb.op("dve", lambda e: e.tensor_tensor(out=o_sb[:], in0=oc[:], in1=oc[:], op=ALU.mult), reads=[boc], writes=[bo])
                kb.op("dve", lambda e: e.tensor_reduce(out=st8[:, 8:16], in_=o_sb[:], axis=AX.X, op=ALU.add), reads=[bo], writes=[bst8])
                kb.op("dve", lambda e: e.tensor_scalar(out=st8[:, 8:16], in0=st8[:, 8:16], scalar1=1.0 / 64, scalar2=EPS,
                                                       op0=ALU.mult, op1=ALU.add), reads=[bst8], writes=[bst8])
                kb.op("pool", lambda e: e.tensor_tensor(out=st8[:, 8:16], in0=st8[:, 8:16], in1=self.neghalf[:, 0:8], op=ALU.pow),
                      reads=[bst8, self.b_const], writes=[bst8])
                kb.op("dve", lambda e: e.tensor_tensor(out=oc[:], in0=oc[:], in1=bc_last(st8[:, 8:16], 64), op=ALU.mult),
                      reads=[boc, bst8], writes=[boc])
                kb.op("dve", lambda e: e.tensor_tensor(out=oc[:].rearrange("p h d -> p (h d)"), in0=oc[:].rearrange("p h d -> p (h d)"),
                                                        in1=retg[:], op=ALU.mult), reads=[boc, bw], writes=[boc])
                kb.op("dve", lambda e: e.tensor_tensor(out=mixcat[:, 512:1024], in0=oc[:].rearrange("p h d -> p (h d)"), in1=sg[:],
                                                       op=ALU.mult), reads=[boc, bsg], writes=[bmixy])
                if _STOP <= 4:
                    return
                for k in range(8):
                    kb.op("pe", lambda e, k=k: e.transpose(out=tpB[0][:, k, :], in_=mixcat[:, k * 128:(k + 1) * 128], identity=self.ident_b[:]),
                          reads=[bmixa, bmixy, self.b_const], writes=[bs2[0]])
                kb.op("act", lambda e: e.copy(out=mixT[:], in_=tpB[0]), reads=[bs2[0]], writes=[bmixT])
                for half in range(2):
                    for k in range(8):
                        kb.op("pe", lambda e, k=k, half=half: e.matmul(oo[half][:], lhsT=mixT[:, k, :], rhs=w_out[:, k, half * 512:(half + 1) * 512],
                                                                     start=(k == 0), stop=(k == 7)), reads=[bmixT, bw], writes=[boo[half]])
                    hs = slice(half * 512, (half + 1) * 512)
                    kb.op("dve", lambda e, half=half, hs=hs: e.tensor_tensor(out=tmp[:, hs], in0=oo[half][:], in1=mods["gate1"][:, hs], op=ALU.mult),
                          reads=[boo[half], bmod], writes=[btmp])
                    kb.op("dve", lambda e, hs=hs: e.tensor_tensor(out=x1[:, hs], in0=tmp[:, hs], in1=X[:, hs], op=ALU.add),
                          reads=[btmp, bX], writes=[bx1])
                kb.dma("sp", S["XA"][ti * 128:(ti + 1) * 128, :], x1[:], reads=[bx1], writes=[self.db("XA", ti)])
                if _STOP <= 5:
                    return
                self.norm2_router(r, x1, bx1, mods["gmod2"], mods["shift2"], bmod, tmp, btmp, tpB, bs2, oo[0], boo[0], ti)
            kb.pipeline(stage_a, stage_b, self.ntl)
        kb.pop()

    def zero_xg(self, q="sp"):
        kb, S = self.kb, self.S
        z = kb.sb("zeros", [128, 4096], BF16); bz = Buf()
        kb.op("pool", lambda e: e.memset(z[:], 0.0), writes=[bz])
        nrows = 32 * self.cap
        for r0 in range(0, nrows, 512):
            kb.dma(q, S["XG"][r0:r0 + 512, :].rearrange("(p a) d -> p (a d)", p=128), z[:], reads=[bz])

    def phase2e(self, l):
        kb, I, S = self.kb, self.I, self.S
        kb.push()
        cap = self.cap
        nblk = cap // 512
        wgu = [kb.sb("wgu%d" % i, [128, 8, 2048], BF16) for i in range(2)]
        wdn = [kb.sb("wdn%d" % i, [128, 8, D], BF16) for i in range(2)]
        bgu = [kb.sb("bgu%d" % i, [128, 16], F32) for i in range(2)]
        bdn = [kb.sb("bdn%d" % i, [1, D], BF16) for i in range(2)]
        bwt = [Buf(), Buf()]
        ones1 = kb.sb("ones1", [1, 128], BF16); bones = Buf()
        kb.op("pool", lambda e: e.memset(ones1[:], 1.0), writes=[bones])
        xg = [kb.sb("xg%d" % i, [128, D], BF16) for i in range(12)]; bxg = [Buf() for _ in range(12)]
        xgT = [kb.sb("xgT%d" % i, [128, 8, 512], BF16) for i in range(2)]; bxgT = [Buf(), Buf()]
        glu = [kb.sb("glu%d" % i, [128, 512], F32) for i in range(2)]; bglu = [Buf(), Buf()]
        sig = [kb.sb("sig%d" % i, [128, 512], F32) for i in range(2)]; bsig = [Buf(), Buf()]
        lin = [kb.sb("lin%d" % i, [128, 512], F32) for i in range(2)]; blin = [Buf(), Buf()]
        actT = [kb.sb("actT%d" % i, [128, 8, 512], BF16) for i in range(2)]; bact = [Buf(), Buf()]
        yg = [kb.sb("yg%d" % i, [128, D], F32) for i in range(3)]; byg = [Buf() for _ in range(3)]
        tp = [kb.ps("tp%d" % i, [128, 8, 128], BF16) for i in range(2)]; btp = [Buf(), Buf()]
        pA = [kb.ps("pA%d" % i, [128, 512], F32) for i in range(2)]; bpA = [Buf(), Buf()]
        pB = [kb.ps("pB%d" % i, [128, 512], F32) for i in range(2)]; bpB = [Buf(), Buf()]
        pC = [kb.ps("pC%d" % i, [128, 512], F32) for i in range(2)]; bpC = [Buf(), Buf()]

        def load_expert(e, slot):
            self.load_w_bf16(wgu[slot], I["exp_w_gu"][l, e], 8, bwt[slot])
            self.load_w_bf16(wdn[slot], I["exp_w_down"][l, e], 8, bwt[slot])
            kb.dma("sp", bgu[slot][:], I["exp_b_gu_pj"][l, e], writes=[bwt[slot]])
            kb.op("dve", lambda en, slot=slot: en.tensor_scalar(out=bgu[slot][:, 8:16], in0=bgu[slot][:, 8:16], scalar1=1.0, scalar2=None,
                                                               op0=ALU.add), reads=[bwt[slot]], writes=[bwt[slot]])
            kb.dma("pool", bdn[slot][:], I["exp_b_down"][l, e:e + 1, :], writes=[bwt[slot]])

        load_expert(0, 0)
        blocks = [(ex, blk) for ex in range(32) for blk in range(nblk)]
        state = dict(xu=0, tu=0)

        def emit_loads(bi):
            ex, blk = blocks[bi]
            r0 = ex * cap + blk * 512
            tiles = []
            for st in range(4):
                i = state["xu"] % len(xg); state["xu"] += 1
                kb.dma("sp", xg[i][:], S["XG"][r0 + st * 128:r0 + (st + 1) * 128, :], writes=[bxg[i]])
                tiles.append(i)
            return tiles

        def emit_transposes(bi, tiles):
            XT = xgT[bi % 2]; bXT = bxgT[bi % 2]
            for st, i in enumerate(tiles):
                T = tp[state["tu"] % 2]; bT = btp[state["tu"] % 2]; state["tu"] += 1
                for k in range(8):
                    kb.op("pe", lambda e, k=k, i=i, T=T: e.transpose(out=T[:, k, :], in_=xg[i][:, k * 128:(k + 1) * 128],
                                                                   identity=self.ident_b[:]), reads=[bxg[i], self.b_const], writes=[bT])
                kb.op("act", lambda e, T=T, XT=XT, st=st: e.copy(out=XT[:, :, st * 128:(st + 1) * 128], in_=T[:]),
                      reads=[bT], writes=[bXT])

        pu = cu = yu = 0
        tl0 = emit_loads(0)
        tl1 = emit_loads(1) if len(blocks) > 1 else None
        emit_transposes(0, tl0)
        for bi, (ex, blk) in enumerate(blocks):
            slot = ex % 2
            if blk == 0 and ex + 1 < 32:
                load_expert(ex + 1, (ex + 1) % 2)
            XT = xgT[bi % 2]; bXT = bxgT[bi % 2]
            A = actT[bi % 2]; bA = bact[bi % 2]
            r0 = ex * cap + blk * 512
            for j in range(8):
                pa = pA[pu % 2]; bpa = bpA[pu % 2]; pb = pB[pu % 2]; bpb = bpB[pu % 2]
                gl = glu[pu % 2]; bgl = bglu[pu % 2]; sg_ = sig[pu % 2]; bsg_ = bsig[pu % 2]; ln = lin[pu % 2]; bln = blin[pu % 2]
                pu += 1
                for k in range(8):
                    kb.op("pe", lambda e, k=k, j=j, pa=pa, slot=slot, XT=XT: e.matmul(
                        pa[:], lhsT=wgu[slot][:, k, j * 128:(j + 1) * 128], rhs=XT[:, k, :],
                        start=(k == 0), stop=(k == 7)), reads=[bwt[slot], bXT], writes=[bpa])
                for k in range(8):
                    kb.op("pe", lambda e, k=k, j=j, pb=pb, slot=slot, XT=XT: e.matmul(
                        pb[:], lhsT=wgu[slot][:, k, 1024 + j * 128:1024 + (j + 1) * 128], rhs=XT[:, k, :],
                        start=(k == 0), stop=(k == 7)), reads=[bwt[slot], bXT], writes=[bpb])
                kb.op("dve", lambda e, pa=pa, gl=gl, j=j, slot=slot: e.tensor_scalar(
                    out=gl[:], in0=pa[:], scalar1=bgu[slot][:, j:j + 1], scalar2=7.0, op0=ALU.add, op1=ALU.min),
                    reads=[bpa, bwt[slot]], writes=[bgl])
                kb.op("act", lambda e, gl=gl, sg_=sg_: e.activation(out=sg_[:], in_=gl[:], func=AF.Sigmoid, scale=1.702),
                      reads=[bgl], writes=[bsg_])
                kb.op("dve", lambda e, pb=pb, ln=ln, j=j, slot=slot: e.tensor_scalar(
                    out=ln[:], in0=pb[:], scalar1=bgu[slot][:, 8 + j:9 + j], scalar2=8.0, op0=ALU.add, op1=ALU.min),
                    reads=[bpb, bwt[slot]], writes=[bln])
                kb.op("dve", lambda e, gl=gl, sg_=sg_: e.tensor_tensor(out=gl[:], in0=gl[:], in1=sg_[:], op=ALU.mult),
                      reads=[bgl, bsg_], writes=[bgl])
                kb.op("dve", lambda e, gl=gl, ln=ln, A=A, j=j: e.scalar_tensor_tensor(out=A[:, j, :], in0=ln[:], scalar=-6.0, in1=gl[:],
                                                                                 op0=ALU.max, op1=ALU.mult),
                      reads=[bgl, bln], writes=[bA])
            if bi + 1 < len(blocks):
                emit_transposes(bi + 1, tl1)
                tl0, tl1 = tl1, (emit_loads(bi + 2) if bi + 2 < len(blocks) else None)
            for st in range(4):
                Y = yg[yu % 3]; bY = byg[yu % 3]; yu += 1
                for half in range(2):
                    pc = pC[cu % 2]; bpc = bpC[cu % 2]; cu += 1
                    for k in range(8):
                        kb.op("pe", lambda e, k=k, st=st, half=half, pc=pc, A=A, slot=slot: e.matmul(
                            pc[:], lhsT=A[:, k, st * 128:(st + 1) * 128], rhs=wdn[slot][:, k, half * 512:(half + 1) * 512],
                            start=(k == 0), stop=False), reads=[bA, bwt[slot]], writes=[bpc])
                    kb.op("pe", lambda e, half=half, pc=pc, slot=slot: e.matmul(
                        pc[:], lhsT=ones1[:, :], rhs=bdn[slot][:, half * 512:(half + 1) * 512], start=False, stop=True),
                        reads=[bones, bwt[slot]], writes=[bpc])
                    if half == 0:
                        kb.op("act", lambda e, pc=pc, Y=Y: e.copy(out=Y[:, 0:512], in_=pc[:]), reads=[bpc], writes=[bY])
                    else:
                        kb.op("dve", lambda e, pc=pc, Y=Y: e.tensor_copy(out=Y[:, 512:1024], in_=pc[:]), reads=[bpc], writes=[bY])
                kb.dma("pool", S["YG"][r0 + st * 128:r0 + (st + 1) * 128, :], Y[:], reads=[bY])
        kb.pop()

    def phase2c(self, l, src, dst, dst_name, zero_after):
        kb, I, S, nseq = self.kb, self.I, self.S, self.nseq
        kb.push()
        cap = self.cap
        gate2 = kb.sb("gate2", [128, D], F32); bg2 = Buf()
        NB = 4
        xin = [kb.sb("xin%d" % i, [128, D], F32) for i in range(NB)]; bxin = [Buf() for _ in range(NB)]
        acc = [kb.sb("acc%d" % i, [128, D], F32) for i in range(NB)]; bacc = [Buf() for _ in range(NB)]
        yb = [kb.sb("yb%d" % i, [128, D], F32) for i in range(4 * NB)]; byb = [Buf() for _ in range(4 * NB)]
        sl = [kb.sb("sl%d" % i, [128, 4], I32) for i in range(NB)]; bsl = [Buf() for _ in range(NB)]
        gk = [kb.sb("gkc%d" % i, [128, 4], F32) for i in range(NB)]; bgk = [Buf() for _ in range(NB)]
        for i in range(4 * NB):
            kb.op("pool", lambda e, i=i: e.memset(yb[i][:], 0.0), writes=[byb[i]])
        if zero_after:
            self.zero_xg()
        u = 0
        for s in range(nseq):
            kb.dma("sp", gate2[:], S["MOD"][s, l, 5].partition_broadcast(128), reads=[self.db("MOD", (s, l, 5))], writes=[bg2])
            for t in range(self.ntl):
                ti = s * NT + t
                X = xin[u % NB]; bX = bxin[u % NB]; A = acc[u % NB]; bA = bacc[u % NB]
                SL = sl[u % NB]; bSL = bsl[u % NB]; GK = gk[u % NB]; bGK = bgk[u % NB]
                kb.dma("sp", X[:], src[ti * 128:(ti + 1) * 128, :], reads=[self.db("XA", ti)], writes=[bX])
                kb.dma("sp", SL[:], S["SLOT"][ti], reads=[self.db("SLOT", ti)], writes=[bSL])
                kb.dma("sp", GK[:], S["GK"][ti], reads=[self.db("GK", ti)], writes=[bGK])
                for k in range(4):
                    Yk = yb[(u % NB) * 4 + k]; bYk = byb[(u % NB) * 4 + k]
                    kb.idma(Yk[:], None, S["YG"], SL[:, k:k + 1], 32 * cap - 1, reads=[bSL], writes=[bYk])
                    if k == 0:
                        kb.op("dve", lambda e, Yk=Yk, A=A, GK=GK: e.tensor_scalar(out=A[:], in0=Yk[:], scalar1=GK[:, 0:1], scalar2=None,
                                                                                 op0=ALU.mult), reads=[bYk, bGK], writes=[bA])
                    else:
                        kb.op("dve", lambda e, Yk=Yk, A=A, GK=GK, k=k: e.scalar_tensor_tensor(
                            out=A[:], in0=Yk[:], scalar=GK[:, k:k + 1], in1=A[:], op0=ALU.mult, op1=ALU.add),
                            reads=[bYk, bGK, bA], writes=[bA])
                kb.op("dve", lambda e, A=A: e.tensor_tensor(out=A[:], in0=A[:], in1=gate2[:], op=ALU.mult), reads=[bA, bg2], writes=[bA])
                kb.op("dve", lambda e, A=A, X=X: e.tensor_tensor(out=X[:], in0=A[:], in1=X[:], op=ALU.add), reads=[bA, bX], writes=[bX])
                kb.dma("sp", dst[ti * 128:(ti + 1) * 128, :], X[:], reads=[bX], writes=[self.db(dst_name, ti)])
                u += 1
        kb.pop()

    def phase3(self):
        kb, I, S, nseq = self.kb, self.I, self.S, self.nseq
        kb.push()
        bw = Buf()
        w_qkv = kb.sb("w_qkv", [128, 8, 1280], BF16)
        w_out = kb.sb("w_out", [128, 8, D], BF16)
        self.load_w_bf16(w_qkv, I["swa_w_qkv"], 8, bw)
        self.load_w_bf16(w_out, I["swa_w_out"], 8, bw)
        bqkv = kb.sb("bqkv", [128, 1280], F32)
        bout = kb.sb("bout", [128, D], F32)
        gq = kb.sb("gq", [128, 64], F32)
        gk = kb.sb("gk", [128, 64], F32)
        sk = kb.sb("sk", [128, 16], F32)
        self.bcast_load(bqkv[:], I["swa_b_qkv"], bw)
        self.bcast_load(bout[:], I["swa_b_out"], bw)
        self.bcast_load(gq[:], I["swa_q_head_g"], bw)
        self.bcast_load(gk[:], I["swa_k_head_g"], bw)
        self.bcast_load(sk[:], I["swa_sinks"], bw)
        kb.op("act", lambda e: e.activation(out=sk[:], in_=sk[:], func=AF.Exp), reads=[bw], writes=[bw])
        mask2 = kb.sb("mask2", [128, 4, 2, 128], BF16)
        for hh in range(4):
            kb.op("dve", lambda e, hh=hh: e.tensor_copy(out=mask2[:, hh, 0, :], in_=self.mask_gt[:]), reads=[self.b_const], writes=[bw])
            kb.op("dve", lambda e, hh=hh: e.tensor_copy(out=mask2[:, hh, 1, :], in_=self.mask_le[:]), reads=[self.b_const], writes=[bw])
        r = self.alloc_router(1)

        mods = {n: kb.sb(n, [128, D], F32) for n in ("gmod1", "shift1", "gate1", "gmod2", "shift2")}
        bmod = Buf()
        x_t = [kb.sb("x_t%d" % i, [128, D], F32) for i in range(2)]; bx = [Buf(), Buf()]
        tmp = kb.sb("tmp", [128, D], F32); btmp = Buf()
        h_bf = kb.sb("h_bf", [128, D], BF16); bh = Buf()
        hT = kb.sb("hT", [128, 8, 128], BF16); bhT = Buf()
        ss = kb.sb("ss", [128, 4], F32); bss = Buf()
        qkv = kb.sb("qkv", [128, 20, 64], F32); bqkvs = Buf()
        sq = kb.sb("sq", [128, 18, 64], F32); bsq = Buf()
        r18 = kb.sb("r18", [128, 18], F32); br18 = Buf()
        qn = kb.sb("qn", [128, 18, 64], F32); bqn = Buf()
        rt = [kb.sb("rt%d" % i, [128, 18, 32], F32) for i in range(4)]; brt = Buf()
        q_bf = kb.sb("q_bf", [128, 16, 64], BF16); bqb = Buf()
        kdup = kb.sb("kdup", [128, 2, 2, 64], BF16); bkd = Buf()
        qT_2 = [kb.sb("qT%d" % i, [128, 8, 128], BF16) for i in range(2)]; bqT_2 = [Buf(), Buf()]
        tmpB = kb.sb("tmpB", [128, D], F32); btmpB = Buf()
        kT = [kb.sb("kT%d" % i, [128, 2, 128], BF16) for i in range(3)]; bkT = [Buf() for _ in range(3)]
        Va = [kb.sb("Va%d" % i, [128, 2, 65], BF16) for i in range(3)]; bVa = [Buf() for _ in range(3)]
        bVones = Buf()
        for i in range(3):
            kb.op("pool", lambda e, i=i: e.memset(Va[i][:, :, 64:65], 1.0), writes=[bVones])
        PT = [kb.sb("PT%d" % i, [128, 4, 2, 128], BF16) for i in range(2)]; bPT = [Buf(), Buf()]
        den = kb.sb("den", [128, 16], F32); bden = Buf()
        attn = kb.sb("attn", [128, 16, 64], BF16); battn = Buf()
        attT = kb.sb("attT", [128, 8, 128], BF16); battT = Buf()
        x1 = kb.sb("x1", [128, D], F32); bx1 = Buf()

        tp = [kb.ps("tp%d" % i, [128, 8, 128], BF16) for i in range(2)]; btp = [Buf(), Buf()]
        mm = [kb.ps("mm%d" % i, [128, 512], F32) for i in range(2)]; bmm = [Buf(), Buf()]
        s2 = kb.ps("s2", [128, 2, 512], F32); bs2 = [Buf(), Buf()]
        oo = [kb.ps("oo%d" % i, [128, 512], F32) for i in range(2)]; boo = [Buf(), Buf()]
        tpB = [s2[:, i, :].bitcast(BF16).rearrange("p (k m) -> p k m", m=128) for i in range(2)]
        gc = {"n": 0}
        for s in range(nseq):
            cos, sin, brope = self.rope_tables(s, 32, "k_invf32", "c%d" % s)
            for n_, part in (("gmod1", 1), ("shift1", 0), ("gate1", 2), ("gmod2", 4), ("shift2", 3)):
                kb.dma("sp", mods[n_][:], S["MOD"][s, 1, part].partition_broadcast(128), reads=[self.db("MOD", (s, 1, part))], writes=[bmod])
            def stage_a(t, s=s, cos=cos, sin=sin, brope=brope):
                ti = s * NT + t
                cur, prv = t % 3, (t - 1) % 3
                X = x_t[t % 2]; bX = bx[t % 2]
                qT = qT_2[t % 2]; bqT = bqT_2[t % 2]
                kb.dma("sp", X[:], S["XB"][ti * 128:(ti + 1) * 128, :], reads=[self.db("XB", ti)], writes=[bX])
                self.norm_mod_T(X, bX, mods["gmod1"], mods["shift1"], bmod, tmp, btmp, h_bf, bh, tp[0], btp[0], hT, bhT, ss, bss)
                qkvf = qkv[:].rearrange("p h d -> p (h d)")
                for gi, (c0, c1) in enumerate(((0, 512), (512, 1024), (1024, 1280))):
                    p_ = mm[gi % 2]; bp_ = bmm[gi % 2]
                    for k in range(8):
                        kb.op("pe", lambda e, k=k, c0=c0, c1=c1, p_=p_: e.matmul(p_[:, 0:c1 - c0], lhsT=hT[:, k, :], rhs=w_qkv[:, k, c0:c1],
                                                                              start=(k == 0), stop=(k == 7)), reads=[bhT, bw], writes=[bp_])
                    kb.op("dve", lambda e, c0=c0, c1=c1, p_=p_: e.tensor_tensor(out=qkvf[:, c0:c1], in0=p_[:, 0:c1 - c0], in1=bqkv[:, c0:c1],
                                                                              op=ALU.add), reads=[bp_, bw], writes=[bqkvs])
                kb.op("dve", lambda e: e.tensor_tensor(out=sq[:], in0=qkv[:, 0:18, :], in1=qkv[:, 0:18, :], op=ALU.mult), reads=[bqkvs], writes=[bsq])
                kb.op("dve", lambda e: e.tensor_reduce(out=r18[:], in_=sq[:], axis=AX.X, op=ALU.add), reads=[bsq], writes=[br18])
                kb.op("dve", lambda e: e.tensor_scalar(out=r18[:], in0=r18[:], scalar1=1.0 / 64, scalar2=EPS, op0=ALU.mult, op1=ALU.add),
                      reads=[br18], writes=[br18])
                kb.op("pool", lambda e: e.tensor_tensor(out=r18[:, 0:16], in0=r18[:, 0:16], in1=self.neghalf[:, 0:16], op=ALU.pow),
                      reads=[br18, self.b_const], writes=[br18])
                kb.op("pool", lambda e: e.tensor_tensor(out=r18[:, 16:18], in0=r18[:, 16:18], in1=self.neghalf[:, 0:2], op=ALU.pow),
                      reads=[br18, self.b_const], writes=[br18])
                kb.op("dve", lambda e: e.tensor_tensor(out=qn[:], in0=qkv[:, 0:18, :], in1=bc_last(r18[:, :], 64), op=ALU.mult),
                      reads=[bqkvs, br18], writes=[bqn])
                kb.op("dve", lambda e: e.tensor_tensor(out=qn[:, 0:16, :], in0=qn[:, 0:16, :], in1=bc_mid(gq[:, :], 16), op=ALU.mult),
                      reads=[bqn, bw], writes=[bqn])
                kb.op("dve", lambda e: e.tensor_tensor(out=qn[:, 16:18, :], in0=qn[:, 16:18, :], in1=bc_mid(gk[:, :], 2), op=ALU.mult),
                      reads=[bqn, bw], writes=[bqn])
                cb = bc_mid(cos[:, t, :], 18)
                sb_ = bc_mid(sin[:, t, :], 18)
                x1_ = qn[:, :, 0:32]; x2_ = qn[:, :, 32:64]
                kb.op("dve", lambda e: e.tensor_tensor(out=rt[0][:], in0=x1_, in1=cb, op=ALU.mult), reads=[bqn, brope], writes=[brt])
                kb.op("dve", lambda e: e.tensor_tensor(out=rt[1][:], in0=x2_, in1=sb_, op=ALU.mult), reads=[bqn, brope], writes=[brt])
                kb.op("dve", lambda e: e.tensor_tensor(out=rt[2][:], in0=x2_, in1=cb, op=ALU.mult), reads=[bqn, brope], writes=[brt])
                kb.op("dve", lambda e: e.tensor_tensor(out=rt[3][:], in0=x1_, in1=sb_, op=ALU.mult), reads=[bqn, brope], writes=[brt])
                kb.op("dve", lambda e: e.tensor_tensor(out=q_bf[:, :, 0:32], in0=rt[0][:, 0:16, :], in1=rt[1][:, 0:16, :], op=ALU.subtract),
                      reads=[brt], writes=[bqb])
                kb.op("dve", lambda e: e.tensor_tensor(out=q_bf[:, :, 32:64], in0=rt[2][:, 0:16, :], in1=rt[3][:, 0:16, :], op=ALU.add),
                      reads=[brt], writes=[bqb])
                for dup in range(2):
                    kb.op("dve", lambda e, dup=dup: e.tensor_tensor(out=kdup[:, :, dup, 0:32], in0=rt[0][:, 16:18, :], in1=rt[1][:, 16:18, :],
                                                                   op=ALU.subtract), reads=[brt], writes=[bkd])
                    kb.op("dve", lambda e, dup=dup: e.tensor_tensor(out=kdup[:, :, dup, 32:64], in0=rt[2][:, 16:18, :], in1=rt[3][:, 16:18, :],
                                                                    op=ALU.add), reads=[brt], writes=[bkd])
                kb.op("act", lambda e, cur=cur: e.copy(out=Va[cur][:, :, 0:64], in_=qkv[:, 18:20, :]), reads=[bqkvs, bVones], writes=[bVa[cur]])
                for i in range(8):
                    kb.op("pe", lambda e, i=i: e.transpose(out=tp[1][:, i, :], in_=q_bf[:, 2 * i:2 * i + 2, :].rearrange("p h d -> p (h d)"),
                                                           identity=self.ident_b[:]), reads=[bqb, self.b_const], writes=[btp[1]])
                kb.op("act", lambda e: e.copy(out=qT[:], in_=tp[1][:]), reads=[btp[1]], writes=[bqT])
                for g in range(2):
                    kb.op("pe", lambda e, g=g: e.transpose(out=tp[0][:, g, :], in_=kdup[:, g, :, :].rearrange("p a d -> p (a d)"),
                                                           identity=self.ident_b[:]), reads=[bkd, self.b_const], writes=[btp[0]])
                kb.op("dve", lambda e, cur=cur: e.tensor_copy(out=kT[cur][:], in_=tp[0][:, 0:2, :]), reads=[btp[0]], writes=[bkT[cur]])
            def stage_b(t, s=s):
                ti = s * NT + t
                cur, prv = t % 3, (t - 1) % 3
                X = x_t[t % 2]; bX = bx[t % 2]
                qT = qT_2[t % 2]; bqT = bqT_2[t % 2]
                tmp = tmpB; btmp = btmpB
                for gq4 in range(4):
                    sbank = s2
                    bsb = bs2
                    P = PT[gc["n"] % 2]; bP = bPT[gc["n"] % 2]
                    ob = oo[gc["n"] % 2]; bob = boo[gc["n"] % 2]
                    gc["n"] += 1
                    for hh in range(4):
                        hq = gq4 * 4 + hh
                        i, o = hq // 2, (hq % 2) * 64
                        g = hq // 8
                        for w_, kt in ((0, prv), (1, cur)):
                            if t == 0 and w_ == 0:
                                continue
                            col = ((hh % 2) * 2 + hh // 2) * 256 + w_ * 128
                            kb.op("pe", lambda e, i=i, o=o, g=g, kt=kt, col=col, sbank=sbank: e.matmul(
                                sbank[:, col // 512, col % 512:col % 512 + 128], lhsT=kT[kt][o:o + 64, g, :], rhs=qT[o:o + 64, i, :],
                                start=True, stop=True), reads=[bkT[kt], bqT], writes=[bsb[col // 512]])
                    for bk in range(2):
                        if t == 0:
                            for hh2 in range(2):
                                kb.op("act", lambda e, bk=bk, hh2=hh2, P=P, sbank=sbank: e.activation(
                                    out=P[:, bk * 2 + hh2, 1, :], in_=sbank[:, bk, hh2 * 256 + 128:hh2 * 256 + 256], func=AF.Exp, scale=0.125),
                                    reads=[bsb[bk]], writes=[bP])
                        else:
                            kb.op("act", lambda e, bk=bk, P=P, sbank=sbank: e.activation(
                                out=P[:, bk * 2:bk * 2 + 2, :, :].rearrange("p a b c -> p (a b c)"), in_=sbank[:, bk, :], func=AF.Exp, scale=0.125),
                                reads=[bsb[bk]], writes=[bP])
                    if t == 0:
                        kb.op("dve", lambda e, P=P: e.tensor_tensor(out=P[:, :, 1, :], in0=P[:, :, 1, :], in1=mask2[:, :, 1, :], op=ALU.mult),
                              reads=[bP, bw], writes=[bP])
                    else:
                        kb.op("dve", lambda e, P=P: e.tensor_tensor(out=P[:].rearrange("p a b c -> p (a b c)"), in0=P[:].rearrange("p a b c -> p (a b c)"),
                                                                   in1=mask2[:].rearrange("p a b c -> p (a b c)"), op=ALU.mult),
                              reads=[bP, bw], writes=[bP])
                    for hh in range(4):
                        hq = gq4 * 4 + hh
                        g = hq // 8
                        sl = (hh % 2) * 2 + hh // 2
                        if t > 0:
                            kb.op("pe", lambda e, hh=hh, g=g, P=P, ob=ob, prv=prv, sl=sl: e.matmul(ob[:, hh * 65:hh * 65 + 65], lhsT=P[:, sl, 0, :],
                                                                                          rhs=Va[prv][:, g, :], start=True, stop=False),
                                  reads=[bP, bVa[prv]], writes=[bob])
                        kb.op("pe", lambda e, hh=hh, g=g, P=P, ob=ob, cur=cur, sl=sl: e.matmul(ob[:, hh * 65:hh * 65 + 65], lhsT=P[:, sl, 1, :],
                                                                                      rhs=Va[cur][:, g, :], start=(t == 0), stop=True),
                              reads=[bP, bVa[cur]], writes=[bob])
                    ov = ob[:, 0:260].rearrange("p (h d) -> p h d", d=65)
                    dsl = den[:, gq4 * 4:(gq4 + 1) * 4]
                    kb.op("dve", lambda e, ov=ov, dsl=dsl, gq4=gq4: e.tensor_tensor(out=dsl, in0=ov[:, :, 64], in1=sk[:, gq4 * 4:(gq4 + 1) * 4], op=ALU.add),
                          reads=[bob, bw], writes=[bden])
                    kb.op("dve", lambda e, dsl=dsl: e.reciprocal(out=dsl, in_=dsl), reads=[bden], writes=[bden])
                    kb.op("dve", lambda e, ov=ov, dsl=dsl, gq4=gq4: e.tensor_tensor(out=attn[:, gq4 * 4:(gq4 + 1) * 4, :], in0=ov[:, :, 0:64],
                                                                                 in1=bc_last(dsl, 64), op=ALU.mult), reads=[bob, bden], writes=[battn])
                af = attn[:].rearrange("p h d -> p (h d)")
                for k in range(8):
                    kb.op("pe", lambda e, k=k: e.transpose(out=tpB[0][:, k, :], in_=af[:, k * 128:(k + 1) * 128], identity=self.ident_b[:]),
                          reads=[battn, self.b_const], writes=[bs2[0]])
                kb.op("act", lambda e: e.copy(out=attT[:], in_=tpB[0]), reads=[bs2[0]], writes=[battT])
                for half in range(2):
                    hs = slice(half * 512, (half + 1) * 512)
                    for k in range(8):
                        kb.op("pe", lambda e, k=k, half=half, hs=hs: e.matmul(oo[half][:], lhsT=attT[:, k, :], rhs=w_out[:, k, hs],
                                                                            start=(k == 0), stop=(k == 7)), reads=[battT, bw], writes=[boo[half]])
                    kb.op("dve", lambda e, half=half, hs=hs: e.tensor_tensor(out=tmp[:, hs], in0=oo[half][:], in1=bout[:, hs], op=ALU.add),
                          reads=[boo[half], bw], writes=[btmp])
                    kb.op("dve", lambda e, hs=hs: e.tensor_tensor(out=tmp[:, hs], in0=tmp[:, hs], in1=mods["gate1"][:, hs], op=ALU.mult),
                          reads=[btmp, bmod], writes=[btmp])
                    kb.op("dve", lambda e, hs=hs: e.tensor_tensor(out=x1[:, hs], in0=tmp[:, hs], in1=X[:, hs], op=ALU.add),
                          reads=[btmp, bX], writes=[bx1])
                kb.dma("sp", S["XA"][ti * 128:(ti + 1) * 128, :], x1[:], reads=[bx1], writes=[self.db("XA", ti)])
                self.norm2_router(r, x1, bx1, mods["gmod2"], mods["shift2"], bmod, tmp, btmp, tpB, bs2, oo[0], boo[0], ti)
            kb.pipeline(stage_a, stage_b, self.ntl)
        kb.pop()

    def build(self):
        self.setup_consts()
        ph = self.phases
        if "p0" in ph:
            self.phase0()
        if "p1a" in ph:
            self.phase1a()
        if "p1b" in ph:
            self.phase1b()
        if "p2a" in ph:
            self.phase2e(0)
            self.phase2c(0, self.S["XA"], self.S["XB"], "XB", False)
        if "p3" in ph:
            self.phase3()
        if "p2b" in ph:
            self.phase2e(1)
            self.phase2c(1, self.S["XA"], self.out, "OUT", False)
        self.kb.finish()
        return self.nc


def module_consts():
    idx = np.arange(128, dtype=np.float64)
    lg = np.log1p(-np.exp2(-5.0 - np.arange(8, dtype=np.float64)))
    k = {}
    k["k_invf16"] = (10000.0 ** (-np.arange(16, dtype=np.float32) / 16)).astype(np.float32)
    k["k_invf32"] = (10000.0 ** (-np.arange(32, dtype=np.float32) / 32)).astype(np.float32)
    diff = idx[None, :] - idx[:, None]
    dec = np.where(diff[:, None, :] >= 0, np.exp(lg[None, :, None] * np.maximum(diff[:, None, :], 0.0)), 0.0)
    k["k_decayT"] = np.ascontiguousarray(dec.reshape(128, 4, 2, 128).transpose(0, 2, 1, 3)).reshape(128, 8 * 128).astype(np.float32)
    k["k_qdec"] = np.exp(lg[None, :] * (idx + 1.0)[:, None]).astype(np.float32)
    k["k_kdec"] = np.exp(lg[None, :] * (127.0 - idx)[:, None]).astype(np.float32)
    cd = np.zeros((128, 4), np.float64)
    for i in range(4):
        cd[0:64, i] = np.exp(lg[2 * i] * 128)
        cd[64:128, i] = np.exp(lg[2 * i + 1] * 128)
    k["k_cdec"] = cd.astype(np.float32)
    return k


def make_in_maps(inputs, nseq, n_cores):
    f = lambda a: np.ascontiguousarray(np.asarray(a))
    shared = {}
    for name in ("ada_w", "ada_b", "norm1_g", "norm2_g", "router_w", "router_b", "exp_w_gu", "exp_w_down", "exp_b_down"):
        shared[name] = f(inputs[name])
    for name in ("hyb_w_in", "mla_cq_norm_g", "mla_ckv_norm_g", "mla_w_uq", "mla_w_ukv", "mla_q_head_g", "mla_k_head_g",
                 "hyb_w_out", "swa_w_qkv", "swa_b_qkv", "swa_q_head_g", "swa_k_head_g", "swa_sinks", "swa_w_out", "swa_b_out"):
        shared[name] = f(np.asarray(inputs[name])[0])
    shared["ret_norm_g"] = f(np.asarray(inputs["ret_norm_g"])[0].reshape(512))
    bgu = np.asarray(inputs["exp_b_gu"])
    shared["exp_b_gu_pj"] = f(bgu.reshape(2, 32, 16, 128).transpose(0, 1, 3, 2))
    shared.update(module_consts())
    x = np.asarray(inputs["x"]); c = np.asarray(inputs["c"]); pos = np.asarray(inputs["positions"])
    maps = []
    for i in range(n_cores):
        b0 = i * nseq
        m = dict(shared)
        m["x"] = f(x[b0:b0 + nseq].reshape(nseq * SEQ, D))
        m["c_pk"] = f(c[b0:b0 + nseq].reshape(nseq, 8, 128).transpose(0, 2, 1))
        m["pos_pt"] = f(pos[b0:b0 + nseq].reshape(nseq, NT, 128).transpose(0, 2, 1).astype(np.int32))
        maps.append(m)
    return maps


_PROG = {}


def kernel(**inputs):
    nseq = 32 // N_CORES
    if "nc" not in _PROG:
        _PROG["nc"] = Prog(nseq).build()
    maps = make_in_maps(inputs, nseq, N_CORES)
    res = run_bass_kernel_spmd(_PROG["nc"], maps, core_ids=list(range(N_CORES)))
    out = np.concatenate([np.asarray(r["out"]).reshape(nseq, SEQ, D) for r in res.results], axis=0)
    return out.astype(np.float32)
```

```python
import contextlib
import os
import math
import numpy as np
import concourse.bass as bass
import concourse.mybir as mybir
from concourse.bass_utils import run_bass_kernel_spmd

F32 = mybir.dt.float32
BF16 = mybir.dt.bfloat16
I32 = mybir.dt.int32
AF = mybir.ActivationFunctionType
ALU = mybir.AluOpType
AX = mybir.AxisListType

SAME_ENGINE_SYNC = os.environ.get('KSES', '1') == '1'
DMA_RING = 12
N_CORES = 8
_STOP = float(os.environ.get('KSTOP', '99'))
SEQ = 2048
D = 1024
NT = SEQ // 128
EPS = 1e-6
PI = math.pi


class Buf:
    __slots__ = ("w", "rs")

    def __init__(self):
        self.w = None
        self.rs = {}


class KB:
    ENGS = ("pe", "act", "dve", "pool", "sp")

    def __init__(self, nc):
        self.nc = nc
        self.stacks = [contextlib.ExitStack()]
        self.eng = dict(pe=nc.tensor, act=nc.scalar, dve=nc.vector, pool=nc.gpsimd, sp=nc.sync)
        self.cnt = {e: 0 for e in self.ENGS}
        self.seen = {e: {} for e in self.ENGS}
        self.sems = {}
        for e in self.ENGS:
            self.sems[e] = self.stacks[0].enter_context(nc.semaphore("s_" + e))
        self.dma_n = {}
        for e in ("sp", "pool", "act"):
            self.dma_n[e] = 0
            for j in range(DMA_RING):
                self.sems[("d", e, j)] = self.stacks[0].enter_context(nc.semaphore("d_%s_%d" % (e, j)))
        self.uid = 0

    def push(self):
        self.phase_id = getattr(self, "phase_id", 0) + 1
        self.stacks.append(contextlib.ExitStack())

    def pop(self):
        self.barrier()
        self.stacks.pop().close()

    def sb(self, name, shape, dt):
        self.uid += 1
        return self.stacks[-1].enter_context(self.nc.sbuf_tensor("%s_%d" % (name, self.uid), list(shape), dt))

    def ps(self, name, shape, dt):
        self.uid += 1
        return self.stacks[-1].enter_context(self.nc.psum_tensor("%s_%d" % (name, self.uid), list(shape), dt))

    def _deps(self, e, reads, writes):
        toks = {}
        for b in reads:
            if b.w is not None and toks.get(b.w[0], 0) < b.w[1]:
                toks[b.w[0]] = b.w[1]
        for b in writes:
            if b.w is not None and toks.get(b.w[0], 0) < b.w[1]:
                toks[b.w[0]] = b.w[1]
            for k, v in b.rs.items():
                if toks.get(k, 0) < v:
                    toks[k] = v
        waits = []
        seen = self.seen[e]
        for k, v in toks.items():
            if k == e and (e == "pe" or not SAME_ENGINE_SYNC):
                continue
            if seen.get(k, 0) < v:
                seen[k] = v
                waits.append((k, v))
        return waits

    def _mark(self, tok, reads, writes):
        k, v = tok
        for b in reads:
            if b.rs.get(k, 0) < v:
                b.rs[k] = v
        for b in writes:
            b.w = tok
            b.rs = {}

    def _emit(self, e, waits, fn, key, inc):
        engine = self.eng[e]
        for k, v in waits:
            engine.wait_ge(self.sems[k], v)
        if fn is not None:
            fn(engine).then_inc(self.sems[key], inc)

    def op(self, e, fn, reads=(), writes=()):
        waits = self._deps(e, reads, writes)
        self.cnt[e] += 1
        tok = (e, self.cnt[e])
        self._emit(e, waits, fn, e, 1)
        self._mark(tok, reads, writes)
        self._handoff()
        return tok

    def dma(self, e, out, in_, reads=(), writes=(), **kw):
        waits = self._deps(e, reads, writes)
        n = self.dma_n[e]
        self.dma_n[e] += 1
        key = ("d", e, n % DMA_RING)
        val = 16 * (n // DMA_RING + 1)
        if n >= DMA_RING and self.seen[e].get(key, 0) < val - 16:
            self.seen[e][key] = val - 16
            waits.append((key, val - 16))
        tok = (key, val)
        self._emit(e, waits, (lambda eng: eng.dma_start(out=out, in_=in_, **kw)), key, 16)
        self._mark(tok, reads, writes)
        self._handoff()
        return tok

    def idma(self, out, out_idx, in_, in_idx, bound, reads=(), writes=()):
        e = "pool"
        waits = self._deps(e, reads, writes)
        n = self.dma_n[e]
        self.dma_n[e] += 1
        key = ("d", e, n % DMA_RING)
        val = 16 * (n // DMA_RING + 1)
        if n >= DMA_RING and self.seen[e].get(key, 0) < val - 16:
            self.seen[e][key] = val - 16
            waits.append((key, val - 16))
        if not hasattr(self, "_bregs"):
            self._bregs = {}
        if bound not in self._bregs:
            self._bregs[bound] = self.nc.gpsimd.to_reg(bound)
        bound = self._bregs[bound]
        oo_ = bass.IndirectOffsetOnAxis(ap=out_idx, axis=0) if out_idx is not None else None
        io_ = bass.IndirectOffsetOnAxis(ap=in_idx, axis=0) if in_idx is not None else None
        self._emit(e, waits, (lambda eng: eng.indirect_dma_start(out=out, out_offset=oo_, in_=in_, in_offset=io_,
                                                                 bounds_check=bound, oob_is_err=False)), key, 16)
        self._mark((key, val), reads, writes)
        self._handoff()

    def _handoff(self):
        st = getattr(self, "_il", None)
        if st is None:
            return
        me = getattr(st["tls"], "idx", None)
        if me is None:
            return
        cv = st["cv"]
        with cv:
            if st["alive"][1 - me]:
                st["turn"] = 1 - me
                cv.notify_all()
                while st["turn"] != me:
                    cv.wait()

    def interleave(self, fa, fb):
        import threading
        if fa is None or fb is None:
            (fa or fb)()
            return
        st = dict(cv=threading.Condition(), turn=0, alive=[True, True], tls=threading.local(), err=[])
        self._il = st

        def runner(i, f):
            cv = st["cv"]
            with cv:
                while st["turn"] != i:
                    cv.wait()
            st["tls"].idx = i
            try:
                f()
            except BaseException as ex:
                st["err"].append(ex)
            finally:
                with cv:
                    st["alive"][i] = False
                    st["turn"] = 1 - i
                    cv.notify_all()

        ths = [threading.Thread(target=runner, args=(i, f)) for i, f in enumerate((fa, fb))]
        for th in ths:
            th.start()
        for th in ths:
            th.join()
        self._il = None
        if st["err"]:
            raise st["err"][0]

    def pipeline(self, stage_a, stage_b, n):
        stage_a(0)
        for t in range(n):
            self.interleave((lambda t=t: stage_a(t + 1)) if t + 1 < n else None, lambda t=t: stage_b(t))

    def all_tokens(self):
        toks = [(e, self.cnt[e]) for e in self.ENGS if self.cnt[e] > 0]
        for e in ("sp", "pool", "act"):
            n = self.dma_n[e]
            for j in range(DMA_RING):
                c = (n - j + DMA_RING - 1) // DMA_RING if n > j else 0
                if c > 0:
                    toks.append((("d", e, j), 16 * c))
        return toks

    def barrier(self):
        toks = self.all_tokens()
        for e in self.ENGS:
            waits = []
            for k, v in toks:
                if k == e:
                    continue
                if self.seen[e].get(k, 0) < v:
                    self.seen[e][k] = v
                    waits.append((k, v))
            self._emit(e, waits, None, None, 0)

    def finish(self):
        self.barrier()
        while self.stacks:
            self.stacks.pop().close()


def bc_mid(ap2, n):
    return ap2.unsqueeze(1).broadcast_to([ap2.shape[0], n, ap2.shape[1]])


def bc_last(ap2, n):
    return ap2.unsqueeze(2).broadcast_to([ap2.shape[0], ap2.shape[1], n])


class Prog:
    def __init__(self, nseq, debug=False, phases=("p0", "p1a", "p1b", "p2a", "p3", "p2b"), ntl=NT, cap_tiles=None):
        self.nseq = nseq
        self.ntl = ntl
        if cap_tiles is None:
            mean = nseq * ntl * 128 * 4 // 32
            cap_tiles = max(4, 4 * ((2 * mean + 511) // 512))
        self.cap = cap_tiles * 128
        self.debug = debug
        self.phases = phases
        nc = self.nc = bass.Bass("TRN2", target_bir_lowering=False)
        self.kb = KB(nc)
        ntok = nseq * SEQ
        self.ntok = ntok

        def inp(name, shape, dt=F32):
            return nc.dram_tensor(name, list(shape), dt, kind="ExternalInput").ap()

        def scr(name, shape, dt=F32):
            kind = "ExternalOutput" if (debug is True or (debug and name in debug)) else "Internal"
            return nc.dram_tensor(name, list(shape), dt, kind=kind).ap()

        I = self.I = {}
        I["x"] = inp("x", [ntok, D])
        I["c_pk"] = inp("c_pk", [nseq, 128, 8])
        I["pos_pt"] = inp("pos_pt", [nseq, 128, NT], I32)
        I["ada_w"] = inp("ada_w", [2, D, 6 * D])
        I["ada_b"] = inp("ada_b", [2, 6 * D])
        I["norm1_g"] = inp("norm1_g", [2, D])
        I["norm2_g"] = inp("norm2_g", [2, D])
        I["hyb_w_in"] = inp("hyb_w_in", [D, 2720])
        I["mla_cq_norm_g"] = inp("mla_cq_norm_g", [384])
        I["mla_ckv_norm_g"] = inp("mla_ckv_norm_g", [256])
        I["mla_w_uq"] = inp("mla_w_uq", [384, 768])
        I["mla_w_ukv"] = inp("mla_w_ukv", [256, 1024])
        I["mla_q_head_g"] = inp("mla_q_head_g", [96])
        I["mla_k_head_g"] = inp("mla_k_head_g", [96])
        I["ret_norm_g"] = inp("ret_norm_g", [512])
        I["hyb_w_out"] = inp("hyb_w_out", [D, D])
        I["swa_w_qkv"] = inp("swa_w_qkv", [D, 1280])
        I["swa_b_qkv"] = inp("swa_b_qkv", [1280])
        I["swa_q_head_g"] = inp("swa_q_head_g", [64])
        I["swa_k_head_g"] = inp("swa_k_head_g", [64])
        I["swa_sinks"] = inp("swa_sinks", [16])
        I["swa_w_out"] = inp("swa_w_out", [D, D])
        I["swa_b_out"] = inp("swa_b_out", [D])
        I["router_w"] = inp("router_w", [2, D, 32])
        I["router_b"] = inp("router_b", [2, 32])
        I["exp_w_gu"] = inp("exp_w_gu", [2, 32, D, 2048])
        I["exp_b_gu_pj"] = inp("exp_b_gu_pj", [2, 32, 128, 16])
        I["exp_w_down"] = inp("exp_w_down", [2, 32, D, D])
        I["exp_b_down"] = inp("exp_b_down", [2, 32, D])
        I["k_invf16"] = inp("k_invf16", [16])
        I["k_invf32"] = inp("k_invf32", [32])
        I["k_decayT"] = inp("k_decayT", [128, 8 * 128])
        I["k_qdec"] = inp("k_qdec", [128, 8])
        I["k_kdec"] = inp("k_kdec", [128, 8])
        I["k_cdec"] = inp("k_cdec", [128, 4])

        S = self.S = {}
        S["MOD"] = scr("MOD", [nseq, 2, 6, D])
        S["ATT"] = scr("ATT", [nseq * NT, 128, 512], BF16)
        S["XA"] = scr("XA", [ntok, D])
        S["XB"] = scr("XB", [ntok, D])
        S["H2T"] = scr("H2T", [nseq * NT, 128, 8, 128], BF16)
        S["GS"] = scr("GS", [nseq * NT, 128, 32])
        S["XG"] = scr("XG", [32 * self.cap, D], BF16)
        S["YG"] = scr("YG", [32 * self.cap, D])
        S["SLOT"] = scr("SLOT", [nseq * NT, 128, 4], I32)
        S["GK"] = scr("GK", [nseq * NT, 128, 4])
        self.out = nc.dram_tensor("out", [ntok, D], F32, kind="ExternalOutput").ap()
        self.dbufs = {}

    def db(self, name, idx):
        k = (name, idx)
        if k not in self.dbufs:
            self.dbufs[k] = Buf()
        return self.dbufs[k]

    def setup_consts(self):
        kb = self.kb
        self.ident_b = kb.sb("ident_b", [128, 128], BF16)
        self.ident_f = kb.sb("ident_f", [128, 128], F32)
        self.b_const = Buf()
        bc = self.b_const
        for idt in (self.ident_b, self.ident_f):
            kb.op("pool", lambda e, idt=idt: e.memset(idt[:], 1.0), writes=[bc])
            kb.op("pool", lambda e, idt=idt: e.affine_select(out=idt[:], in_=idt[:], pattern=[[-1, 128]],
                                                             compare_op=ALU.is_equal, fill=0.0, base=0,
                                                             channel_multiplier=1), reads=[bc], writes=[bc])
        self.mask_le = kb.sb("mask_le", [128, 128], BF16)
        self.mask_gt = kb.sb("mask_gt", [128, 128], BF16)
        kb.op("pool", lambda e: e.memset(self.mask_le[:], 1.0), writes=[bc])
        kb.op("pool", lambda e: e.affine_select(out=self.mask_le[:], in_=self.mask_le[:], pattern=[[1, 128]],
                                                compare_op=ALU.is_ge, fill=0.0, base=0, channel_multiplier=-1),
              reads=[bc], writes=[bc])
        kb.op("pool", lambda e: e.memset(self.mask_gt[:], 1.0), writes=[bc])
        kb.op("pool", lambda e: e.affine_select(out=self.mask_gt[:], in_=self.mask_gt[:], pattern=[[-1, 128]],
                                                compare_op=ALU.is_gt, fill=0.0, base=0, channel_multiplier=1),
              reads=[bc], writes=[bc])
        self.U_b = kb.sb("U_b", [128, 128], BF16)
        self.ones_b = kb.sb("ones_b", [128, 128], BF16)
        kb.op("pool", lambda e: e.memset(self.ones_b[:], 1.0), writes=[bc])
        kb.op("pool", lambda e: e.memset(self.U_b[:], 1.0), writes=[bc])
        kb.op("pool", lambda e: e.affine_select(out=self.U_b[:], in_=self.U_b[:], pattern=[[1, 128]],
                                                compare_op=ALU.is_ge, fill=0.0, base=-1, channel_multiplier=-1),
              reads=[bc], writes=[bc])
        iot_i = kb.sb("iot_i", [128, 32], I32)
        self.iotaE = kb.sb("iotaE", [128, 32], F32)
        kb.op("pool", lambda e: e.iota(out=iot_i[:], pattern=[[1, 32]], base=0, channel_multiplier=0), writes=[bc])
        kb.op("dve", lambda e: e.tensor_copy(out=self.iotaE[:], in_=iot_i[:]), reads=[bc], writes=[bc])
        kb.op("dve", lambda e: e.tensor_scalar(out=self.iotaE[:], in0=self.iotaE[:], scalar1=float(self.cap), scalar2=None,
                                               op0=ALU.mult), reads=[bc], writes=[bc])
        self.neghalf = kb.sb("neghalf", [128, 16], F32)
        kb.op("pool", lambda e: e.memset(self.neghalf[:], -0.5), writes=[bc])

    def rstd_of(self, ss, n, width, bss, tag):
        kb = self.kb
        kb.op("dve", lambda e: e.tensor_scalar(out=ss, in0=ss, scalar1=1.0 / n, scalar2=EPS,
                                               op0=ALU.mult, op1=ALU.add), reads=[bss], writes=[bss])
        kb.op("pool", lambda e: e.tensor_tensor(out=ss, in0=ss, in1=self.neghalf[:, 0:width], op=ALU.pow),
              reads=[bss, self.b_const], writes=[bss])

    def rope_tables(self, s, half, invf_name, tag):
        kb, I = self.kb, self.I
        key = (kb.phase_id, half)
        if not hasattr(self, "_rope"):
            self._rope = {}
        if key not in self._rope:
            self._rope[key] = dict(
                b=Buf(),
                pos_i=kb.sb("pos_i", [128, NT], I32), pos_f=kb.sb("pos_f", [128, NT], F32), invf=kb.sb("invf", [128, half], F32),
                ang=kb.sb("ang", [128, NT, half], F32), kq=kb.sb("kq", [128, NT, half], F32), ki=kb.sb("ki", [128, NT, half], I32),
                ys=kb.sb("ys", [128, NT, half], F32), mm=kb.sb("mmk", [128, NT, half], F32),
                cos=kb.sb("cos", [128, NT, half], F32), sin=kb.sb("sin", [128, NT, half], F32))
        R_ = self._rope[key]
        b = R_["b"]
        pos_i, pos_f, invf, ang, kq, ki, ys, mm, cos, sin = (R_[n] for n in ("pos_i", "pos_f", "invf", "ang", "kq", "ki", "ys", "mm", "cos", "sin"))
        kb.dma("sp", pos_i[:], I["pos_pt"][s], writes=[b])
        kb.dma("sp", invf[:], I[invf_name].partition_broadcast(128), writes=[b])
        kb.op("dve", lambda e: e.tensor_copy(out=pos_f[:], in_=pos_i[:]), reads=[b], writes=[b])
        kb.op("dve", lambda e: e.tensor_tensor(out=ang[:], in0=bc_last(pos_f[:, :], half), in1=bc_mid(invf[:, :], NT),
                                               op=ALU.mult), reads=[b], writes=[b])
        kb.op("dve", lambda e: e.tensor_scalar(out=kq[:], in0=ang[:], scalar1=1.0 / (2 * PI), scalar2=None,
                                               op0=ALU.mult), reads=[b], writes=[b])
        kb.op("dve", lambda e: e.tensor_copy(out=ki[:], in_=kq[:]), reads=[b], writes=[b])
        kb.op("dve", lambda e: e.tensor_copy(out=kq[:], in_=ki[:]), reads=[b], writes=[b])
        kb.op("dve", lambda e: e.scalar_tensor_tensor(out=ang[:], in0=kq[:], scalar=-2 * PI, in1=ang[:],
                                                      op0=ALU.mult, op1=ALU.add), reads=[b], writes=[b])
        lim = 3.1415925
        for shift, dst in ((0.0, sin), (PI / 2, cos)):
            kb.op("dve", lambda e, shift=shift: e.tensor_scalar(out=ys[:], in0=ang[:], scalar1=shift, scalar2=None,
                                                                op0=ALU.add), reads=[b], writes=[b])
            kb.op("dve", lambda e: e.tensor_scalar(out=mm[:], in0=ys[:], scalar1=PI, scalar2=-2 * PI,
                                                   op0=ALU.is_gt, op1=ALU.mult), reads=[b], writes=[b])
            kb.op("dve", lambda e: e.tensor_tensor(out=ys[:], in0=ys[:], in1=mm[:], op=ALU.add), reads=[b], writes=[b])
            kb.op("dve", lambda e: e.tensor_scalar(out=ys[:], in0=ys[:], scalar1=lim, scalar2=-lim,
                                                   op0=ALU.min, op1=ALU.max), reads=[b], writes=[b])
            kb.op("act", lambda e, dst=dst: e.activation(out=dst[:], in_=ys[:], func=AF.Sin), reads=[b], writes=[b])
        return cos, sin, b

    def load_w_bf16(self, dst, src, kchunks, bw):
        v = src.rearrange("(k p) n -> p k n", p=128)
        for k in range(kchunks):
            self.kb.dma("pool", dst[:, k, :], v[:, k, :], writes=[bw])

    def bcast_load(self, dst, src1d, b):
        self.kb.dma("sp", dst, src1d.partition_broadcast(128), writes=[b])

    def norm_mod_T(self, x_t, bx, gmod, shift, bmod, tmp, btmp, h_bf, bh, tp, btp, hT, bhT, ss, bss):
        kb = self.kb
        kb.op("dve", lambda e: e.scalar_tensor_tensor(out=tmp[:], in0=x_t[:], scalar=1.0, in1=x_t[:], op0=ALU.mult, op1=ALU.mult, accum_out=ss[:, 0:1]),
              reads=[bx], writes=[btmp, bss])
        self.rstd_of(ss[:, 0:1], D, 1, bss, "")
        kb.op("dve", lambda e: e.scalar_tensor_tensor(out=tmp[:], in0=x_t[:], scalar=ss[:, 0:1], in1=gmod[:],
                                                      op0=ALU.mult, op1=ALU.mult), reads=[bx, bss, bmod], writes=[btmp])
        kb.op("dve", lambda e: e.tensor_tensor(out=h_bf[:], in0=tmp[:], in1=shift[:], op=ALU.add),
              reads=[btmp, bmod], writes=[bh])
        for k in range(8):
            kb.op("pe", lambda e, k=k: e.transpose(out=tp[:, k, :], in_=h_bf[:, k * 128:(k + 1) * 128],
                                                   identity=self.ident_b[:]), reads=[bh, self.b_const], writes=[btp])
        kb.op("act", lambda e: e.copy(out=hT[:], in_=tp[:]), reads=[btp], writes=[bhT])

    def phase0(self):
        kb, I, S, nseq = self.kb, self.I, self.S, self.nseq
        kb.push()
        cin = kb.sb("cin", [128, nseq, 8], F32)
        cact = kb.sb("cact", [128, 8, nseq], F32)
        bcr = Buf()
        for s in range(nseq):
            kb.dma("sp", cin[:, s, :], I["c_pk"][s], writes=[bcr])
        kb.op("act", lambda e: e.activation(out=cact[:].rearrange("p k s -> p s k"), in_=cin[:], func=AF.Silu), reads=[bcr], writes=[bcr])
        adab = kb.sb("adab", [nseq, 6 * D], F32)
        ng = [kb.sb("ng1", [nseq, D], F32), kb.sb("ng2", [nseq, D], F32)]
        bab = Buf()
        wch = [kb.sb("wch%d" % i, [128, 8, 512], F32) for i in range(3)]
        bwch = [Buf() for _ in range(3)]
        pm = [kb.ps("p0pm%d" % i, [128, 512], F32) for i in range(2)]
        bpm = [Buf(), Buf()]
        modt = [kb.sb("modt%d" % i, [nseq, 512], F32) for i in range(3)]
        bmodt = [Buf() for _ in range(3)]
        n = 0
        for l in range(2):
            kb.dma("sp", adab[:], I["ada_b"][l].partition_broadcast(nseq), writes=[bab])
            kb.dma("sp", ng[0][:], I["norm1_g"][l].partition_broadcast(nseq), writes=[bab])
            kb.dma("sp", ng[1][:], I["norm2_g"][l].partition_broadcast(nseq), writes=[bab])
            for j in range(12):
                wv = I["ada_w"][l][:, j * 512:(j + 1) * 512].rearrange("(k p) n -> p k n", p=128)
                w_ = wch[n % 3]
                kb.dma("sp", w_[:], wv, writes=[bwch[n % 3]])
                p_ = pm[n % 2]
                for k in range(8):
                    kb.op("pe", lambda e, k=k, p_=p_, w_=w_: e.matmul(p_[0:nseq, :], lhsT=cact[:, k, :], rhs=w_[:, k, :],
                                                                    start=(k == 0), stop=(k == 7)),
                          reads=[bcr, bwch[n % 3]], writes=[bpm[n % 2]])
                m_ = modt[n % 3]
                bm_ = bmodt[n % 3]
                kb.op("dve", lambda e, p_=p_, m_=m_, j=j: e.tensor_tensor(out=m_[:], in0=p_[0:nseq, :],
                                                                       in1=adab[:, j * 512:(j + 1) * 512], op=ALU.add),
                      reads=[bpm[n % 2], bab], writes=[bm_])
                part, half = j // 2, j % 2
                if part in (1, 4):
                    g_ = ng[0] if part == 1 else ng[1]
                    kb.op("dve", lambda e, m_=m_, g_=g_, half=half: e.scalar_tensor_tensor(
                        out=m_[:], in0=m_[:], scalar=1.0, in1=g_[:, half * 512:(half + 1) * 512],
                        op0=ALU.add, op1=ALU.mult), reads=[bm_, bab], writes=[bm_])
                for s in range(nseq):
                    kb.dma("sp", S["MOD"][s, l, part, half * 512:(half + 1) * 512], m_[s:s + 1, :],
                           reads=[bm_], writes=[self.db("MOD", (s, l, part))])
                n += 1
        kb.pop()

    def phase1a(self):
        kb, I, S, nseq = self.kb, self.I, self.S, self.nseq
        kb.push()
        bw = Buf()
        w_in = kb.sb("w_in_a", [128, 8, 672], BF16)
        w_uq = kb.sb("w_uq", [128, 3, 768], BF16)
        w_ukv = kb.sb("w_ukv", [128, 2, 1024], BF16)
        self.load_w_bf16(w_in, I["hyb_w_in"][:, 0:672], 8, bw)
        self.load_w_bf16(w_uq, I["mla_w_uq"], 3, bw)
        self.load_w_bf16(w_ukv, I["mla_w_ukv"], 2, bw)
        gcq = kb.sb("gcq", [128, 384], F32)
        gckv = kb.sb("gckv", [128, 256], F32)
        gq = kb.sb("gq", [128, 96], F32)
        gk = kb.sb("gk", [128, 96], F32)
        self.bcast_load(gcq[:], I["mla_cq_norm_g"], bw)
        self.bcast_load(gckv[:], I["mla_ckv_norm_g"], bw)
        self.bcast_load(gq[:], I["mla_q_head_g"], bw)
        self.bcast_load(gk[:], I["mla_k_head_g"], bw)
        self.zero_xg("pool")

        kT = kb.sb("kT", [96, 8, SEQ], BF16)
        V = kb.sb("Vc", [128, NT, 8, 65], BF16)
        bkT = [Buf() for _ in range(NT)]
        bV = [Buf() for _ in range(NT)]
        bVones = Buf()
        kb.op("pool", lambda e: e.memset(V[:, :, :, 64:65], 1.0), writes=[bVones])

        gmod = kb.sb("gmod1", [128, D], F32)
        shift = kb.sb("shift1", [128, D], F32)
        bmod = Buf()
        x_t = [kb.sb("x_t%d" % i, [128, D], F32) for i in range(2)]
        bx = [Buf(), Buf()]
        tmp = kb.sb("tmp", [128, D], F32); btmp = Buf()
        h_bf = kb.sb("h_bf", [128, D], BF16); bh = Buf()
        hT = kb.sb("hT", [128, 8, 128], BF16); bhT = Buf()
        ss = kb.sb("ss", [128, 4], F32); bss = Buf()
        proj = kb.sb("proj", [128, 672], F32); bproj = Buf()
        sq = kb.sb("sq", [128, 8, 96], F32); bsq = Buf()
        cqn = kb.sb("cqn", [128, 384], BF16); bcqn = Buf()
        cqT = kb.sb("cqT", [128, 3, 128], BF16); bcqT = Buf()
        ckvn = kb.sb("ckvn", [128, 256], BF16); bckvn = Buf()
        ckvT = kb.sb("ckvT", [128, 2, 128], BF16); bckvT = Buf()
        q_sb = kb.sb("q_sb", [128, 8, 96], F32); bq = Buf()
        qn = kb.sb("qn", [128, 8, 96], F32); bqn = Buf()
        rq8 = kb.sb("rq8", [128, 16], F32); brq8 = Buf()
        R = kb.sb("Rr", [128, 8, 96], F32); bR = Buf()
        q_full = kb.sb("q_full", [128, 8, 96], BF16); bqf = Buf()
        qTs = [kb.sb("qT%d" % i, [96, 8, 128], BF16) for i in range(2)]; bqTs = [Buf(), Buf()]
        kv_sb = kb.sb("kv_sb", [128, 8, 128], F32); bkv = Buf()
        k_full = kb.sb("k_full", [128, 8, 96], BF16); bkf = Buf()
        kr = kb.sb("kr", [128, 32], F32); bkr = Buf()
        kr2 = kb.sb("kr2", [128, 32], F32)
        rt = [kb.sb("rt%d" % i, [128, 8, 16], F32) for i in range(4)]; brt = Buf()
        PT = [kb.sb("PT%d" % i, [128, 4, 128], BF16) for i in range(3)]; bPT = [Buf() for _ in range(3)]
        attn = kb.sb("attn", [128, 8, 64], BF16); battn = Buf()
        rden = kb.sb("rden", [128, 8], F32); brden = Buf()

        tp = [kb.ps("tp%d" % i, [128, 8, 128], BF16) for i in range(2)]; btp = [Buf(), Buf()]
        mm = [kb.ps("mm%d" % i, [128, 512], F32) for i in range(2)]; bmm = [Buf(), Buf()]
        s2 = kb.ps("s2", [128, 2, 512], F32); bs2 = [Buf(), Buf()]
        oo = [kb.ps("oo%d" % i, [128, 512], F32) for i in range(2)]; boo = [Buf(), Buf()]
        scale = 96.0 ** -0.5
        uc = {"n": 0}
        for s in range(nseq):
            cos, sin, brope = self.rope_tables(s, 16, "k_invf16", "a%d" % s)
            kb.dma("sp", gmod[:], S["MOD"][s, 0, 1].partition_broadcast(128), reads=[self.db("MOD", (s, 0, 1))], writes=[bmod])
            kb.dma("sp", shift[:], S["MOD"][s, 0, 0].partition_broadcast(128), reads=[self.db("MOD", (s, 0, 0))], writes=[bmod])
            def stage_a(t, s=s, cos=cos, sin=sin, brope=brope):
                X = x_t[t % 2]; bX = bx[t % 2]
                qT = qTs[t % 2]; bqT = bqTs[t % 2]
                kb.dma("sp", X[:], I["x"][(s * NT + t) * 128:(s * NT + t + 1) * 128, :], writes=[bX])
                self.norm_mod_T(X, bX, gmod, shift, bmod, tmp, btmp, h_bf, bh, tp[0], btp[0], hT, bhT, ss, bss)
                for gi, (c0, c1) in enumerate(((0, 512), (512, 672))):
                    for k in range(8):
                        kb.op("pe", lambda e, k=k, gi=gi, c0=c0, c1=c1: e.matmul(mm[gi][:, 0:c1 - c0], lhsT=hT[:, k, :],
                                                                              rhs=w_in[:, k, c0:c1], start=(k == 0), stop=(k == 7)),
                              reads=[bhT, bw], writes=[bmm[gi]])
                    kb.op("act", lambda e, gi=gi, c0=c0, c1=c1: e.copy(out=proj[:, c0:c1], in_=mm[gi][:, 0:c1 - c0]),
                          reads=[bmm[gi]], writes=[bproj])
                for ci, (c0, w) in enumerate(((0, 384), (384, 256), (640, 32))):
                    kb.op("dve", lambda e, c0=c0, w=w, ci=ci: e.scalar_tensor_tensor(out=tmp[:, 0:w], in0=proj[:, c0:c0 + w], scalar=1.0, in1=proj[:, c0:c0 + w], op0=ALU.mult, op1=ALU.mult, accum_out=ss[:, 1 + ci:2 + ci]), reads=[bproj], writes=[btmp, bss])
                kb.op("dve", lambda e: e.tensor_scalar(out=ss[:, 1:2], in0=ss[:, 1:2], scalar1=1.0 / 384, scalar2=EPS,
                                                       op0=ALU.mult, op1=ALU.add), reads=[bss], writes=[bss])
                kb.op("dve", lambda e: e.tensor_scalar(out=ss[:, 2:3], in0=ss[:, 2:3], scalar1=1.0 / 256, scalar2=EPS,
                                                       op0=ALU.mult, op1=ALU.add), reads=[bss], writes=[bss])
                kb.op("dve", lambda e: e.tensor_scalar(out=ss[:, 3:4], in0=ss[:, 3:4], scalar1=1.0 / 32, scalar2=EPS,
                                                       op0=ALU.mult, op1=ALU.add), reads=[bss], writes=[bss])
                kb.op("pool", lambda e: e.tensor_tensor(out=ss[:, 1:4], in0=ss[:, 1:4], in1=self.neghalf[:, 0:3], op=ALU.pow),
                      reads=[bss, self.b_const], writes=[bss])
                kb.op("dve", lambda e: e.scalar_tensor_tensor(out=cqn[:], in0=proj[:, 0:384], scalar=ss[:, 1:2], in1=gcq[:],
                                                              op0=ALU.mult, op1=ALU.mult), reads=[bproj, bss, bw], writes=[bcqn])
                kb.op("dve", lambda e: e.scalar_tensor_tensor(out=ckvn[:], in0=proj[:, 384:640], scalar=ss[:, 2:3], in1=gckv[:],
                                                              op0=ALU.mult, op1=ALU.mult), reads=[bproj, bss, bw], writes=[bckvn])
                kb.op("dve", lambda e: e.scalar_tensor_tensor(out=kr[:], in0=proj[:, 640:672], scalar=ss[:, 3:4], in1=gk[:, 64:96],
                                                              op0=ALU.mult, op1=ALU.mult), reads=[bproj, bss, bw], writes=[bkr])
                for k in range(3):
                    kb.op("pe", lambda e, k=k: e.transpose(out=tp[1][:, k, :], in_=cqn[:, k * 128:(k + 1) * 128],
                                                           identity=self.ident_b[:]), reads=[bcqn, self.b_const], writes=[btp[1]])
                for k in range(2):
                    kb.op("pe", lambda e, k=k: e.transpose(out=tp[1][:, 3 + k, :], in_=ckvn[:, k * 128:(k + 1) * 128],
                                                           identity=self.ident_b[:]), reads=[bckvn, self.b_const], writes=[btp[1]])
                kb.op("act", lambda e: e.copy(out=cqT[:], in_=tp[1][:, 0:3, :]), reads=[btp[1]], writes=[bcqT])
                kb.op("act", lambda e: e.copy(out=ckvT[:], in_=tp[1][:, 3:5, :]), reads=[btp[1]], writes=[bckvT])
                for gi, (c0, c1) in enumerate(((0, 512), (512, 768))):
                    for k in range(3):
                        kb.op("pe", lambda e, k=k, gi=gi, c0=c0, c1=c1: e.matmul(mm[gi][:, 0:c1 - c0], lhsT=cqT[:, k, :],
                                                                              rhs=w_uq[:, k, c0:c1], start=(k == 0), stop=(k == 2)),
                              reads=[bcqT, bw], writes=[bmm[gi]])
                    kb.op("act", lambda e, gi=gi, c0=c0, c1=c1: e.copy(
                        out=q_sb[:].rearrange("p h d -> p (h d)")[:, c0:c1], in_=mm[gi][:, 0:c1 - c0]),
                        reads=[bmm[gi]], writes=[bq])
                kb.op("dve", lambda e: e.tensor_tensor(out=sq[:], in0=q_sb[:], in1=q_sb[:], op=ALU.mult), reads=[bq], writes=[bsq])
                kb.op("dve", lambda e: e.tensor_reduce(out=rq8[:, 0:8], in_=sq[:, :, 0:64], axis=AX.X, op=ALU.add),
                      reads=[bsq], writes=[brq8])
                kb.op("dve", lambda e: e.tensor_reduce(out=rq8[:, 8:16], in_=sq[:, :, 64:96], axis=AX.X, op=ALU.add),
                      reads=[bsq], writes=[brq8])
                kb.op("dve", lambda e: e.tensor_scalar(out=rq8[:, 0:8], in0=rq8[:, 0:8], scalar1=1.0 / 64, scalar2=EPS,
                                                       op0=ALU.mult, op1=ALU.add), reads=[brq8], writes=[brq8])
                kb.op("dve", lambda e: e.tensor_scalar(out=rq8[:, 8:16], in0=rq8[:, 8:16], scalar1=1.0 / 32, scalar2=EPS,
                                                       op0=ALU.mult, op1=ALU.add), reads=[brq8], writes=[brq8])
                kb.op("pool", lambda e: e.tensor_tensor(out=rq8[:], in0=rq8[:], in1=self.neghalf[:, 0:16], op=ALU.pow),
                      reads=[brq8, self.b_const], writes=[brq8])
                kb.op("dve", lambda e: e.tensor_tensor(out=qn[:, :, 0:64], in0=q_sb[:, :, 0:64], in1=bc_last(rq8[:, 0:8], 64),
                                                       op=ALU.mult), reads=[bq, brq8], writes=[bqn])
                kb.op("dve", lambda e: e.tensor_tensor(out=qn[:, :, 64:96], in0=q_sb[:, :, 64:96], in1=bc_last(rq8[:, 8:16], 32),
                                                       op=ALU.mult), reads=[bq, brq8], writes=[bqn])
                kb.op("dve", lambda e: e.tensor_tensor(out=qn[:], in0=qn[:], in1=bc_mid(gq[:, :], 8), op=ALU.mult),
                      reads=[bqn, bw], writes=[bqn])
                kb.op("act", lambda e: e.copy(out=q_full[:, :, 0:64], in_=qn[:, :, 0:64]), reads=[bqn], writes=[bqf])
                cb = bc_mid(cos[:, t, :], 8)
                sb_ = bc_mid(sin[:, t, :], 8)
                x1 = qn[:, :, 64:80]
                x2 = qn[:, :, 80:96]
                kb.op("dve", lambda e: e.tensor_tensor(out=rt[0][:], in0=x1, in1=cb, op=ALU.mult), reads=[bqn, brope], writes=[brt])
                kb.op("dve", lambda e: e.tensor_tensor(out=rt[1][:], in0=x2, in1=sb_, op=ALU.mult), reads=[bqn, brope], writes=[brt])
                kb.op("dve", lambda e: e.tensor_tensor(out=rt[2][:], in0=x2, in1=cb, op=ALU.mult), reads=[bqn, brope], writes=[brt])
                kb.op("dve", lambda e: e.tensor_tensor(out=rt[3][:], in0=x1, in1=sb_, op=ALU.mult), reads=[bqn, brope], writes=[brt])
                kb.op("dve", lambda e: e.tensor_tensor(out=q_full[:, :, 64:80], in0=rt[0][:], in1=rt[1][:], op=ALU.subtract),
                      reads=[brt], writes=[bqf])
                kb.op("dve", lambda e: e.tensor_tensor(out=q_full[:, :, 80:96], in0=rt[2][:], in1=rt[3][:], op=ALU.add),
                      reads=[brt], writes=[bqf])
                for h in range(8):
                    kb.op("pe", lambda e, h=h: e.transpose(out=tp[0][0:96, h, :], in_=q_full[:, h, :], identity=self.ident_b[:]),
                          reads=[bqf, self.b_const], writes=[btp[0]])
                kb.op("act", lambda e: e.copy(out=qT[:], in_=tp[0][0:96, :, :]), reads=[btp[0]], writes=[bqT])
                for gi in range(2):
                    for k in range(2):
                        kb.op("pe", lambda e, k=k, gi=gi: e.matmul(mm[gi][:], lhsT=ckvT[:, k, :], rhs=w_ukv[:, k, gi * 512:(gi + 1) * 512],
                                                                 start=(k == 0), stop=(k == 1)), reads=[bckvT, bw], writes=[bmm[gi]])
                    kb.op("act", lambda e, gi=gi: e.copy(out=kv_sb[:].rearrange("p h d -> p (h d)")[:, gi * 512:(gi + 1) * 512],
                                                        in_=mm[gi][:]), reads=[bmm[gi]], writes=[bkv])
                kb.op("dve", lambda e, t=t: e.tensor_copy(out=V[:, t, :, 0:64], in_=kv_sb[:, :, 64:128]),
                      reads=[bkv, bVones], writes=[bV[t]])
                kb.op("dve", lambda e: e.tensor_tensor(out=sq[:, :, 0:64], in0=kv_sb[:, :, 0:64], in1=kv_sb[:, :, 0:64], op=ALU.mult),
                      reads=[bkv], writes=[bsq])
                kb.op("dve", lambda e: e.tensor_reduce(out=rq8[:, 0:8], in_=sq[:, :, 0:64], axis=AX.X, op=ALU.add),
                      reads=[bsq], writes=[brq8])
                kb.op("dve", lambda e: e.tensor_scalar(out=rq8[:, 0:8], in0=rq8[:, 0:8], scalar1=1.0 / 64, scalar2=EPS,
                                                       op0=ALU.mult, op1=ALU.add), reads=[brq8], writes=[brq8])
                kb.op("pool", lambda e: e.tensor_tensor(out=rq8[:, 0:8], in0=rq8[:, 0:8], in1=self.neghalf[:, 0:8], op=ALU.pow),
                      reads=[brq8, self.b_const], writes=[brq8])
                kb.op("dve", lambda e: e.tensor_tensor(out=sq[:, :, 0:64], in0=kv_sb[:, :, 0:64], in1=bc_last(rq8[:, 0:8], 64),
                                                       op=ALU.mult), reads=[bkv, brq8], writes=[bsq])
                kb.op("dve", lambda e: e.tensor_tensor(out=k_full[:, :, 0:64], in0=sq[:, :, 0:64], in1=bc_mid(gk[:, 0:64], 8),
                                                        op=ALU.mult), reads=[bsq, bw], writes=[bkf])
                c1_ = cos[:, t, :]
                s1_ = sin[:, t, :]
                kb.op("dve", lambda e: e.tensor_tensor(out=rt[0][:, 0, :], in0=kr[:, 0:16], in1=c1_, op=ALU.mult), reads=[bkr, brope], writes=[brt])
                kb.op("dve", lambda e: e.tensor_tensor(out=rt[1][:, 0, :], in0=kr[:, 16:32], in1=s1_, op=ALU.mult), reads=[bkr, brope], writes=[brt])
                kb.op("dve", lambda e: e.tensor_tensor(out=rt[2][:, 0, :], in0=kr[:, 16:32], in1=c1_, op=ALU.mult), reads=[bkr, brope], writes=[brt])
                kb.op("dve", lambda e: e.tensor_tensor(out=rt[3][:, 0, :], in0=kr[:, 0:16], in1=s1_, op=ALU.mult), reads=[bkr, brope], writes=[brt])
                kb.op("dve", lambda e: e.tensor_tensor(out=kr2[:, 0:16], in0=rt[0][:, 0, :], in1=rt[1][:, 0, :], op=ALU.subtract),
                      reads=[brt], writes=[bkr])
                kb.op("dve", lambda e: e.tensor_tensor(out=kr2[:, 16:32], in0=rt[2][:, 0, :], in1=rt[3][:, 0, :], op=ALU.add),
                      reads=[brt], writes=[bkr])
                kb.op("dve", lambda e: e.tensor_copy(out=k_full[:, :, 64:96], in_=bc_mid(kr2[:, :], 8)), reads=[bkr], writes=[bkf])
                for h in range(8):
                    kb.op("pe", lambda e, h=h: e.transpose(out=tp[1][0:96, h, :], in_=k_full[:, h, :], identity=self.ident_b[:]),
                          reads=[bkf, self.b_const], writes=[btp[1]])
                kb.op("act", lambda e, t=t: e.copy(out=kT[:, :, t * 128:(t + 1) * 128], in_=tp[1][0:96, :, :]),
                      reads=[btp[1]], writes=[bkT[t]])
            def stage_b(t, s=s):
                qT = qTs[t % 2]; bqT = bqTs[t % 2]
                units = []
                for h in range(8):
                    for a in range(0, t + 1, 4):
                        units.append((h, a, min(a + 4, t + 1)))

                def emit_S(ui, u):
                    h, a, b = u
                    bank = ui % 2
                    for kt in range(a, b):
                        kb.op("pe", lambda e, kt=kt, h=h, a=a, bank=bank: e.matmul(
                            s2[:, bank, (kt - a) * 128:(kt - a + 1) * 128], lhsT=kT[:, h, kt * 128:(kt + 1) * 128],
                            rhs=qT[:, h, :], start=True, stop=True), reads=[bkT[kt], bqT], writes=[bs2[bank]])

                base = uc["n"]
                emit_S(base, units[0])
                for i, u in enumerate(units):
                    ui = base + i
                    h, a, b = u
                    if i + 1 < len(units):
                        emit_S(ui + 1, units[i + 1])
                    bank = ui % 2
                    P = PT[ui % 3]; bP = bPT[ui % 3]
                    n = (b - a) * 128
                    kb.op("act", lambda e, P=P, bank=bank, n=n: e.activation(
                        out=P[:].rearrange("p a b -> p (a b)")[:, 0:n], in_=s2[:, bank, 0:n], func=AF.Exp, scale=scale),
                        reads=[bs2[bank]], writes=[bP])
                    if b == t + 1:
                        kb.op("dve", lambda e, P=P, j=t - a: e.tensor_tensor(out=P[:, j, :], in0=P[:, j, :], in1=self.mask_le[:],
                                                                           op=ALU.mult), reads=[bP, self.b_const], writes=[bP])
                    ob = oo[h // 4]
                    for kt in range(a, b):
                        kb.op("pe", lambda e, kt=kt, h=h, a=a, P=P, ob=ob: e.matmul(
                            ob[:, (h % 4) * 65:(h % 4) * 65 + 65], lhsT=P[:, kt - a, :], rhs=V[:, kt, h, :],
                            start=(kt == 0), stop=(kt == t)), reads=[bP, bV[kt], bVones], writes=[boo[h // 4]])
                uc["n"] += len(units)
                for hb in range(2):
                    ov = oo[hb][:, 0:260].rearrange("p (h d) -> p h d", d=65)
                    kb.op("dve", lambda e, hb=hb, ov=ov: e.reciprocal(out=rden[:, hb * 4:(hb + 1) * 4], in_=ov[:, :, 64]),
                          reads=[boo[hb]], writes=[brden])
                    kb.op("dve", lambda e, hb=hb, ov=ov: e.tensor_tensor(out=attn[:, hb * 4:(hb + 1) * 4, :], in0=ov[:, :, 0:64],
                                                                        in1=bc_last(rden[:, hb * 4:(hb + 1) * 4], 64), op=ALU.mult),
                          reads=[boo[hb], brden], writes=[battn])
                kb.dma("sp", S["ATT"][s * NT + t], attn[:].rearrange("p h d -> p (h d)"), reads=[battn],
                       writes=[self.db("ATT", s * NT + t)])
            kb.pipeline(stage_a, stage_b, self.ntl)
        kb.pop()

    def alloc_router(self, l):
        kb, I = self.kb, self.I
        r = {}
        r["bw"] = Buf()
        r["rw"] = kb.sb("rw", [128, 8, 32], F32)
        kb.dma("sp", r["rw"][:], I["router_w"][l].rearrange("(k p) n -> p k n", p=128), writes=[r["bw"]])
        r["rb"] = kb.sb("rb", [128, 32], F32)
        self.bcast_load(r["rb"][:], I["router_b"][l], r["bw"])
        r["rwh"] = kb.sb("rwh", [128, 8, 32], BF16)
        r["rwl"] = kb.sb("rwl", [128, 8, 32], BF16)
        kb.op("dve", lambda e: e.tensor_copy(out=r["rwh"][:], in_=r["rw"][:]), reads=[r["bw"]], writes=[r["bw"]])
        kb.op("dve", lambda e: e.tensor_tensor(out=r["rwl"][:], in0=r["rw"][:], in1=r["rwh"][:], op=ALU.subtract),
              reads=[r["bw"]], writes=[r["bw"]])
        r["h2f"] = kb.sb("h2f", [128, D], F32); r["bh2f"] = Buf()
        r["h2hi"] = kb.sb("h2hi", [128, D], BF16); r["bh2hi"] = Buf()
        r["h2lo"] = kb.sb("h2lo", [128, D], BF16); r["bh2lo"] = Buf()
        r["h2Tl"] = kb.sb("h2Tl", [128, 8, 128], BF16); r["bh2Tl"] = Buf()
        r["h2Tb"] = kb.sb("h2Tb", [128, 8, 128], BF16); r["bh2Tb"] = Buf()
        r["lg"] = kb.sb("lg", [128, 32], F32); r["blg"] = Buf()
        r["m8"] = kb.sb("m8", [128, 8], F32)
        r["msk"] = kb.sb("msk", [128, 32], F32)
        r["ex"] = kb.sb("ex", [128, 32], F32)
        r["den"] = kb.sb("den", [128, 2], F32)
        r["G"] = kb.sb("Gt", [128, 32], F32); r["bG"] = Buf()
        r["ss"] = kb.sb("ss2", [128, 1], F32); r["bss"] = Buf()
        r["cnt"] = kb.sb("cnt_b", [128, 32], F32); r["bcnt"] = Buf()
        kb.op("pool", lambda e: e.memset(r["cnt"][:], 0.0), writes=[r["bcnt"]])
        r["Mb"] = kb.sb("Mb", [128, 32], BF16)
        for n_ in ("posf", "valid", "slotm", "oh", "junk", "Gv"):
            r[n_] = kb.sb(n_, [128, 32], F32)
        r["slotf"] = kb.sb("slotf", [128, 4], F32)
        r["sloti"] = kb.sb("sloti", [128, 4], I32); r["bsloti"] = Buf()
        r["gk"] = kb.sb("gk", [128, 4], F32); r["bgk"] = Buf()
        r["brt"] = Buf()
        return r

    def norm2_router(self, r, x1, bx1, gmod2, shift2, bmod, tmp, btmp, tp, btp, mmp, bmmp, tile_idx):
        kb, S = self.kb, self.S
        ss, bss = r["ss"], r["bss"]
        kb.op("dve", lambda e: e.scalar_tensor_tensor(out=tmp[:], in0=x1[:], scalar=1.0, in1=x1[:], op0=ALU.mult, op1=ALU.mult, accum_out=ss[:, 0:1]),
              reads=[bx1], writes=[btmp, bss])
        self.rstd_of(ss[:, 0:1], D, 1, bss, "")
        kb.op("dve", lambda e: e.scalar_tensor_tensor(out=tmp[:], in0=x1[:], scalar=ss[:, 0:1], in1=gmod2[:],
                                                      op0=ALU.mult, op1=ALU.mult), reads=[bx1, bss, bmod], writes=[btmp])
        kb.op("dve", lambda e: e.tensor_tensor(out=r["h2f"][:], in0=tmp[:], in1=shift2[:], op=ALU.add),
              reads=[btmp, bmod], writes=[r["bh2f"]])
        kb.op("act", lambda e: e.copy(out=r["h2hi"][:], in_=r["h2f"][:]), reads=[r["bh2f"]], writes=[r["bh2hi"]])
        kb.op("dve", lambda e: e.tensor_tensor(out=r["h2lo"][:], in0=r["h2f"][:], in1=r["h2hi"][:], op=ALU.subtract),
              reads=[r["bh2f"], r["bh2hi"]], writes=[r["bh2lo"]])
        for k in range(8):
            kb.op("pe", lambda e, k=k: e.transpose(out=tp[0][:, k, :], in_=r["h2hi"][:, k * 128:(k + 1) * 128],
                                                   identity=self.ident_b[:]), reads=[r["bh2hi"], self.b_const], writes=[btp[0]])
        for k in range(8):
            kb.op("pe", lambda e, k=k: e.transpose(out=tp[1][:, k, :], in_=r["h2lo"][:, k * 128:(k + 1) * 128],
                                                   identity=self.ident_b[:]), reads=[r["bh2lo"], self.b_const], writes=[btp[1]])
        kb.op("act", lambda e: e.copy(out=r["h2Tb"][:], in_=tp[0][:]), reads=[btp[0]], writes=[r["bh2Tb"]])
        kb.op("dve", lambda e: e.tensor_copy(out=r["h2Tl"][:], in_=tp[1][:]), reads=[btp[1]], writes=[r["bh2Tl"]])
        if self.debug:
            kb.dma("sp", S["H2T"][tile_idx], r["h2Tb"][:], reads=[r["bh2Tb"]], writes=[self.db("H2T", tile_idx)])
        if _STOP <= 6:
            return
        passes = [("h2Tb", "rwh"), ("h2Tl", "rwh"), ("h2Tb", "rwl")]
        for pi, (a_, w_) in enumerate(passes):
            for k in range(8):
                kb.op("pe", lambda e, k=k, a_=a_, w_=w_, pi=pi: e.matmul(mmp[:, 0:32], lhsT=r[a_][:, k, :], rhs=r[w_][:, k, :],
                                                                       start=(pi == 0 and k == 0), stop=(pi == 2 and k == 7)),
                      reads=[r["bh2Tb"], r["bh2Tl"], r["bw"]], writes=[bmmp])
        lg, m8, msk, ex, den, G = r["lg"], r["m8"], r["msk"], r["ex"], r["den"], r["G"]
        bl = r["blg"]
        kb.op("dve", lambda e: e.tensor_tensor(out=lg[:], in0=mmp[:, 0:32], in1=r["rb"][:], op=ALU.add),
              reads=[bmmp, r["bw"]], writes=[bl])
        if _STOP <= 7:
            return
        kb.op("dve", lambda e: e.max(out=m8[:], in_=lg[:]), reads=[bl], writes=[bl])
        kb.op("dve", lambda e: e.tensor_scalar(out=msk[:], in0=lg[:], scalar1=m8[:, 3:4], scalar2=None, op0=ALU.is_ge),
              reads=[bl], writes=[bl])
        kb.op("dve", lambda e: e.tensor_scalar(out=den[:, 1:2], in0=m8[:, 0:1], scalar1=-1.0, scalar2=None, op0=ALU.mult),
              reads=[bl], writes=[bl])
        kb.op("act", lambda e: e.activation(out=ex[:], in_=lg[:], func=AF.Exp, bias=den[:, 1:2], scale=1.0),
              reads=[bl], writes=[bl])
        kb.op("dve", lambda e: e.scalar_tensor_tensor(out=ex[:], in0=ex[:], scalar=1.0, in1=msk[:], op0=ALU.mult, op1=ALU.mult, accum_out=den[:, 0:1]), reads=[bl], writes=[bl])
        kb.op("dve", lambda e: e.reciprocal(out=den[:, 0:1], in_=den[:, 0:1]), reads=[bl], writes=[bl])
        kb.op("dve", lambda e: e.tensor_scalar(out=G[:], in0=ex[:], scalar1=den[:, 0:1], scalar2=None, op0=ALU.mult),
              reads=[bl], writes=[r["bG"]])
        if self.debug:
            kb.dma("sp", S["GS"][tile_idx], G[:], reads=[r["bG"]], writes=[self.db("GS", tile_idx)])
        cap = self.cap
        brt = r["brt"]
        kb.op("dve", lambda e: e.tensor_copy(out=r["Mb"][:], in_=msk[:]), reads=[bl], writes=[brt])
        kb.op("pe", lambda e: e.matmul(mmp[:, 32:64], lhsT=self.U_b[:], rhs=r["Mb"][:], start=True, stop=True),
              reads=[brt, self.b_const], writes=[bmmp])
        kb.op("pe", lambda e: e.matmul(mmp[:, 64:96], lhsT=self.ones_b[:], rhs=r["Mb"][:], start=True, stop=True),
              reads=[brt, self.b_const], writes=[bmmp])
        kb.op("dve", lambda e: e.tensor_tensor(out=r["posf"][:], in0=mmp[:, 32:64], in1=r["cnt"][:], op=ALU.add),
              reads=[bmmp, r["bcnt"]], writes=[brt])
        kb.op("dve", lambda e: e.tensor_tensor(out=r["cnt"][:], in0=mmp[:, 64:96], in1=r["cnt"][:], op=ALU.add),
              reads=[bmmp, r["bcnt"]], writes=[r["bcnt"]])
        kb.op("dve", lambda e: e.tensor_scalar(out=r["valid"][:], in0=r["posf"][:], scalar1=float(cap), scalar2=None, op0=ALU.is_lt),
              reads=[brt], writes=[brt])
        kb.op("dve", lambda e: e.tensor_tensor(out=r["slotm"][:], in0=r["posf"][:], in1=self.iotaE[:], op=ALU.add),
              reads=[brt, self.b_const], writes=[brt])
        kb.op("dve", lambda e: e.tensor_scalar(out=r["junk"][:], in0=r["valid"][:], scalar1=-1.0e6, scalar2=1.0e6,
                                               op0=ALU.mult, op1=ALU.add), reads=[brt], writes=[brt])
        kb.op("dve", lambda e: e.tensor_tensor(out=r["slotm"][:], in0=r["slotm"][:], in1=r["junk"][:], op=ALU.add),
              reads=[brt], writes=[brt])
        kb.op("dve", lambda e: e.tensor_tensor(out=r["Gv"][:], in0=G[:], in1=r["valid"][:], op=ALU.mult),
              reads=[brt, r["bG"]], writes=[brt])
        for k in range(4):
            kb.op("dve", lambda e, k=k: e.tensor_scalar(out=r["oh"][:], in0=lg[:], scalar1=m8[:, k:k + 1], scalar2=None, op0=ALU.is_equal),
                  reads=[bl, brt], writes=[brt])
            kb.op("dve", lambda e, k=k: e.scalar_tensor_tensor(out=r["junk"][:], in0=r["oh"][:], scalar=1.0, in1=r["slotm"][:],
                                                              op0=ALU.mult, op1=ALU.mult, accum_out=r["slotf"][:, k:k + 1]),
                  reads=[brt], writes=[brt])
            kb.op("dve", lambda e, k=k: e.scalar_tensor_tensor(out=r["junk"][:], in0=r["oh"][:], scalar=1.0, in1=r["Gv"][:],
                                                              op0=ALU.mult, op1=ALU.mult, accum_out=r["gk"][:, k:k + 1]),
                  reads=[brt, r["bgk"]], writes=[brt, r["bgk"]])
        kb.op("dve", lambda e: e.tensor_copy(out=r["sloti"][:], in_=r["slotf"][:]), reads=[brt, r["bsloti"]], writes=[r["bsloti"]])
        kb.dma("sp", S["SLOT"][tile_idx], r["sloti"][:], reads=[r["bsloti"]], writes=[self.db("SLOT", tile_idx)])
        kb.dma("sp", S["GK"][tile_idx], r["gk"][:], reads=[r["bgk"]], writes=[self.db("GK", tile_idx)])
        for k in range(4):
            kb.idma(S["XG"], r["sloti"][:, k:k + 1], r["h2hi"][:], None, 32 * cap - 1, reads=[r["bsloti"], r["bh2hi"]])

    def phase1b(self):
        kb, I, S, nseq = self.kb, self.I, self.S, self.nseq
        kb.push()
        bw = Buf()
        w_in = kb.sb("w_in_b", [128, 8, 2048], BF16)
        w_out = kb.sb("w_out", [128, 8, D], BF16)
        self.load_w_bf16(w_in, I["hyb_w_in"][:, 672:2720], 8, bw)
        self.load_w_bf16(w_out, I["hyb_w_out"], 8, bw)
        retg = kb.sb("retg", [128, 512], F32)
        self.bcast_load(retg[:], I["ret_norm_g"], bw)
        decT = kb.sb("decT", [128, 8 * 128], F32)
        qdec = kb.sb("qdec", [128, 8], F32)
        kdec = kb.sb("kdec", [128, 8], F32)
        cdec = kb.sb("cdec", [128, 4], F32)
        kb.dma("sp", decT[:], I["k_decayT"], writes=[bw])
        kb.dma("sp", qdec[:], I["k_qdec"], writes=[bw])
        kb.dma("sp", kdec[:], I["k_kdec"], writes=[bw])
        kb.dma("sp", cdec[:], I["k_cdec"], writes=[bw])
        r = self.alloc_router(0)

        mods = {n: kb.sb(n, [128, D], F32) for n in ("gmod1", "shift1", "gate1", "gmod2", "shift2")}
        bmod = Buf()
        x_t = [kb.sb("x_t%d" % i, [128, D], F32) for i in range(2)]; bx = [Buf(), Buf()]
        tmp = kb.sb("tmp", [128, D], F32); btmp = Buf()
        h_bf = kb.sb("h_bf", [128, D], BF16); bh = Buf()
        hT = kb.sb("hT", [128, 8, 128], BF16); bhT = Buf()
        ss = kb.sb("ss", [128, 4], F32); bss = Buf()
        raw = [kb.sb("raw%d" % i, [128, 8, 64], F32) for i in range(2)]; braw = [Buf(), Buf()]
        rr = [kb.sb("rr%d" % i, [128, 8, 64], F32) for i in range(2)]; brr = [Buf(), Buf()]
        rt = [kb.sb("rt%d" % i, [128, 8, 32], F32) for i in range(4)]; brt = Buf()
        rq_bf = kb.sb("rq_bf", [128, 8, 64], BF16); brqb = Buf()
        rqd_bf = kb.sb("rqd_bf", [128, 8, 64], BF16); brqd = Buf()
        rk_bf = kb.sb("rk_bf", [128, 8, 64], BF16); brkb = Buf()
        rkd_bf_2 = [kb.sb("rkd_bf%d" % i, [128, 8, 64], BF16) for i in range(2)]; brkd_2 = [Buf(), Buf()]
        v_bf_2 = [kb.sb("v_bf%d" % i, [128, 8, 64], BF16) for i in range(2)]; bv_2 = [Buf(), Buf()]
        sg_2 = [kb.sb("sg%d" % i, [128, 512], F32) for i in range(2)]; bsg_2 = [Buf(), Buf()]
        rqT_2 = [kb.sb("rqT%d" % i, [128, 8, 128], BF16) for i in range(2)]; brqT_2 = [Buf(), Buf()]
        rkT_2 = [kb.sb("rkT%d" % i, [128, 4, 128], BF16) for i in range(2)]; brkT_2 = [Buf(), Buf()]
        Sd = kb.sb("Sd", [128, 8, 128], BF16); bSd = Buf()
        st_f = kb.sb("st_f", [128, 4, 128], F32); bstf = Buf()
        st_b = kb.sb("st_b", [128, 4, 128], BF16); bstb = Buf()
        kb.op("pool", lambda e: e.memset(st_f[:], 0.0), writes=[bstf])
        o_sb = kb.sb("o_sb", [128, 8, 64], F32); bo = Buf()
        oc = kb.sb("oc", [128, 8, 64], F32); boc = Buf()
        st8 = kb.sb("st8", [128, 16], F32); bst8 = Buf()
        mixcat_2 = [kb.sb("mixcat%d" % i, [128, D], BF16) for i in range(2)]; bmixa_2 = [Buf(), Buf()]; bmixy_2 = [Buf(), Buf()]
        tmpB = kb.sb("tmpB", [128, D], F32); btmpB = Buf()
        mixT = kb.sb("mixT", [128, 8, 128], BF16); bmixT = Buf()
        x1 = kb.sb("x1", [128, D], F32); bx1 = Buf()

        tp = [kb.ps("tp%d" % i, [128, 8, 128], BF16) for i in range(2)]; btp = [Buf(), Buf()]
        mm = [kb.ps("mm%d" % i, [128, 512], F32) for i in range(2)]; bmm = [Buf(), Buf()]
        s2 = kb.ps("s2", [128, 2, 512], F32); bs2 = [Buf(), Buf()]
        oo = [kb.ps("oo%d" % i, [128, 512], F32) for i in range(2)]; boo = [Buf(), Buf()]
        tpB = [s2[:, i, :].bitcast(BF16).rearrange("p (k m) -> p k m", m=128) for i in range(2)]
        for s in range(nseq):
            cos, sin, brope = self.rope_tables(s, 32, "k_invf32", "b%d" % s)
            for n_, part in (("gmod1", 1), ("shift1", 0), ("gate1", 2), ("gmod2", 4), ("shift2", 3)):
                kb.dma("sp", mods[n_][:], S["MOD"][s, 0, part].partition_broadcast(128), reads=[self.db("MOD", (s, 0, part))], writes=[bmod])
            def stage_a(t, s=s, cos=cos, sin=sin, brope=brope):
                P_ = t % 2
                X = x_t[P_]; bX = bx[P_]
                v_bf = v_bf_2[P_]; bv = bv_2[P_]; sg = sg_2[P_]; bsg = bsg_2[P_]; rqT = rqT_2[P_]; brqT = brqT_2[P_]
                rkT = rkT_2[P_]; brkT = brkT_2[P_]; rkd_bf = rkd_bf_2[P_]; brkd = brkd_2[P_]
                mixcat = mixcat_2[P_]; bmixa = bmixa_2[P_]; bmixy = bmixy_2[P_]
                ti = s * NT + t
                kb.dma("sp", X[:], I["x"][ti * 128:(ti + 1) * 128, :], writes=[bX])
                kb.dma("sp", mixcat[:, 0:512], S["ATT"][ti], reads=[self.db("ATT", ti)], writes=[bmixa])
                self.norm_mod_T(X, bX, mods["gmod1"], mods["shift1"], bmod, tmp, btmp, h_bf, bh, tp[0], btp[0], hT, bhT, ss, bss)
                cb = bc_mid(cos[:, t, :], 8)
                sb_ = bc_mid(sin[:, t, :], 8)
                for gi in range(4):
                    p_ = mm[gi % 2]; bp_ = bmm[gi % 2]
                    for k in range(8):
                        kb.op("pe", lambda e, k=k, gi=gi, p_=p_: e.matmul(p_[:], lhsT=hT[:, k, :], rhs=w_in[:, k, gi * 512:(gi + 1) * 512],
                                                                       start=(k == 0), stop=(k == 7)), reads=[bhT, bw], writes=[bp_])
                    if gi < 2:
                        rw_ = raw[gi]; brw_ = braw[gi]; ro = rr[gi]; bro = brr[gi]
                        kb.op("act", lambda e, p_=p_, rw_=rw_: e.copy(out=rw_[:].rearrange("p h d -> p (h d)"), in_=p_[:]),
                              reads=[bp_], writes=[brw_])
                        x1_ = rw_[:, :, 0:32]; x2_ = rw_[:, :, 32:64]
                        kb.op("dve", lambda e, x1_=x1_: e.tensor_tensor(out=rt[0][:], in0=x1_, in1=cb, op=ALU.mult), reads=[brw_, brope], writes=[brt])
                        kb.op("dve", lambda e, x2_=x2_: e.tensor_tensor(out=rt[1][:], in0=x2_, in1=sb_, op=ALU.mult), reads=[brw_, brope], writes=[brt])
                        kb.op("dve", lambda e, x2_=x2_: e.tensor_tensor(out=rt[2][:], in0=x2_, in1=cb, op=ALU.mult), reads=[brw_, brope], writes=[brt])
                        kb.op("dve", lambda e, x1_=x1_: e.tensor_tensor(out=rt[3][:], in0=x1_, in1=sb_, op=ALU.mult), reads=[brw_, brope], writes=[brt])
                        kb.op("dve", lambda e, ro=ro: e.tensor_tensor(out=ro[:, :, 0:32], in0=rt[0][:], in1=rt[1][:], op=ALU.subtract),
                              reads=[brt], writes=[bro])
                        kb.op("dve", lambda e, ro=ro: e.tensor_tensor(out=ro[:, :, 32:64], in0=rt[2][:], in1=rt[3][:], op=ALU.add),
                              reads=[brt], writes=[bro])
                        if gi == 0:
                            kb.op("act", lambda e, ro=ro: e.copy(out=rq_bf[:], in_=ro[:]), reads=[bro], writes=[brqb])
                            kb.op("dve", lambda e, ro=ro: e.tensor_tensor(out=rqd_bf[:], in0=ro[:], in1=bc_last(qdec[:, :], 64), op=ALU.mult),
                                  reads=[bro, bw], writes=[brqd])
                        else:
                            kb.op("act", lambda e, ro=ro: e.mul(out=rk_bf[:], in_=ro[:], mul=0.125), reads=[bro], writes=[brkb])
                            kb.op("dve", lambda e, ro=ro: e.scalar_tensor_tensor(out=rkd_bf[:], in0=ro[:], scalar=0.125,
                                                                                in1=bc_last(kdec[:, :], 64), op0=ALU.mult, op1=ALU.mult),
                                  reads=[bro, bw], writes=[brkd])
                    elif gi == 2:
                        kb.op("act", lambda e, p_=p_: e.copy(out=v_bf[:].rearrange("p h d -> p (h d)"), in_=p_[:]), reads=[bp_], writes=[bv])
                    else:
                        kb.op("act", lambda e, p_=p_: e.activation(out=sg[:], in_=p_[:], func=AF.Silu), reads=[bp_], writes=[bsg])
                for i in range(4):
                    kb.op("pe", lambda e, i=i: e.transpose(out=tp[1][:, i, :], in_=rq_bf[:, 2 * i:2 * i + 2, :].rearrange("p h d -> p (h d)"),
                                                           identity=self.ident_b[:]), reads=[brqb, self.b_const], writes=[btp[1]])
                for i in range(4):
                    kb.op("pe", lambda e, i=i: e.transpose(out=tp[1][:, 4 + i, :], in_=rqd_bf[:, 2 * i:2 * i + 2, :].rearrange("p h d -> p (h d)"),
                                                           identity=self.ident_b[:]), reads=[brqd, self.b_const], writes=[btp[1]])
                for i in range(4):
                    kb.op("pe", lambda e, i=i: e.transpose(out=tp[0][:, i, :], in_=rk_bf[:, 2 * i:2 * i + 2, :].rearrange("p h d -> p (h d)"),
                                                           identity=self.ident_b[:]), reads=[brkb, self.b_const], writes=[btp[0]])
                kb.op("act", lambda e: e.copy(out=rqT[:], in_=tp[1][:]), reads=[btp[1]], writes=[brqT])
                kb.op("dve", lambda e: e.tensor_copy(out=rkT[:], in_=tp[0][:, 0:4, :]), reads=[btp[0]], writes=[brkT])
            def stage_b(t, s=s):
                P_ = t % 2
                X = x_t[P_]; bX = bx[P_]
                v_bf = v_bf_2[P_]; bv = bv_2[P_]; sg = sg_2[P_]; bsg = bsg_2[P_]; rqT = rqT_2[P_]; brqT = brqT_2[P_]
                rkT = rkT_2[P_]; brkT = brkT_2[P_]; rkd_bf = rkd_bf_2[P_]; brkd = brkd_2[P_]
                mixcat = mixcat_2[P_]; bmixa = bmixa_2[P_]; bmixy = bmixy_2[P_]
                ti = s * NT + t
                tmp = tmpB; btmp = btmpB
                for h in range(8):
                    i, o = h // 2, (h % 2) * 64
                    kb.op("pe", lambda e, h=h, i=i, o=o: e.matmul(s2[:, h % 2, i * 128:(i + 1) * 128],
                                                                lhsT=rkT[o:o + 64, i, :], rhs=rqT[o:o + 64, i, :], start=True, stop=True),
                          reads=[brkT, brqT], writes=[bs2[h % 2]])
                for hb in range(2):
                    kb.op("dve", lambda e, hb=hb: e.tensor_tensor(out=Sd[:, hb * 4:(hb + 1) * 4, :].rearrange("p h q -> p (h q)"),
                                                                 in0=s2[:, hb, :], in1=decT[:, hb * 512:(hb + 1) * 512], op=ALU.mult),
                          reads=[bs2[hb], bw], writes=[bSd])
                if _STOP <= 1.5:
                    return
                for i in range(4):
                    if t > 0:
                        kb.op("pe", lambda e, i=i: e.matmul(oo[0][:, i * 128:(i + 1) * 128], lhsT=rqT[:, 4 + i, :],
                                                            rhs=st_b[:, i, :], start=True, stop=False, skip_group_check=True),
                              reads=[brqT, bstb], writes=[boo[0]])
                    for par in range(2):
                        h = 2 * i + par
                        kb.op("pe", lambda e, h=h, i=i, par=par: e.matmul(oo[0][:, h * 64:(h + 1) * 64], lhsT=Sd[:, par * 4 + i, :],
                                                                        rhs=v_bf[:, h, :], start=(t == 0), stop=(t == 0 or par == 1),
                                                                        skip_group_check=(t > 0)),
                              reads=[bSd, bv], writes=[boo[0]])
                if _STOP <= 2:
                    return
                for i in range(4):
                    kb.op("pe", lambda e, i=i: e.matmul(oo[1][:, i * 128:(i + 1) * 128],
                                                        lhsT=rkd_bf[:, 2 * i:2 * i + 2, :].rearrange("p h d -> p (h d)"),
                                                        rhs=v_bf[:, 2 * i:2 * i + 2, :].rearrange("p h d -> p (h d)"), start=True, stop=True),
                          reads=[brkd, bv], writes=[boo[1]])
                kvv = oo[1][:].rearrange("p (i c) -> p i c", c=128)
                for half in range(2):
                    po = half * 64
                    if t == 0:
                        kb.op("dve", lambda e, po=po: e.tensor_copy(out=st_f[po:po + 64, :, po:po + 64], in_=kvv[po:po + 64, :, po:po + 64]),
                              reads=[boo[1]], writes=[bstf])
                    else:
                        for i in range(4):
                            kb.op("dve", lambda e, po=po, i=i: e.scalar_tensor_tensor(
                                out=st_f[po:po + 64, i, po:po + 64], in0=st_f[po:po + 64, i, po:po + 64], scalar=cdec[po:po + 64, i:i + 1],
                                in1=kvv[po:po + 64, i, po:po + 64], op0=ALU.mult, op1=ALU.add), reads=[boo[1], bstf, bw], writes=[bstf])
                kb.op("act", lambda e: e.copy(out=st_b[:], in_=st_f[:]), reads=[bstf], writes=[bstb])
                if _STOP <= 3:
                    return
                kb.op("act", lambda e: e.copy(out=o_sb[:].rearrange("p h d -> p (h d)"), in_=oo[0][:]), reads=[boo[0]], writes=[bo])
                kb.op("dve", lambda e: e.tensor_reduce(out=st8[:, 0:8], in_=o_sb[:], axis=AX.X, op=ALU.add), reads=[bo], writes=[bst8])
                kb.op("dve", lambda e: e.tensor_scalar(out=st8[:, 0:8], in0=st8[:, 0:8], scalar1=-1.0 / 64, scalar2=None, op0=ALU.mult),
                      reads=[bst8], writes=[bst8])
                kb.op("dve", lambda e: e.tensor_tensor(out=oc[:], in0=o_sb[:], in1=bc_last(st8[:, 0:8], 64), op=ALU.add),
                      reads=[bo, bst8], writes=[boc])
                kb.op("dve", lambda e: e.tensor_tensor(out=o_sb[:], in0=oc[:], in1=oc[:], op=ALU.mult), reads=[boc], writes=[bo])
                kb.op("dve", lambda e: e.tensor_reduce(out=st8[:, 8:16], in_=o_sb[:], axis=AX.X, op=ALU.add), reads=[bo], writes=[bst8])
                kb.op("dve", lambda e: e.tensor_scalar(out=st8[:, 8:16], in0=st8[:, 8:16], scalar1=1.0 / 64, scalar2=EPS,
                                                       op0=ALU.mult, op1=ALU.add), reads=[bst8], writes=[bst8])
                kb.op("pool", lambda e: e.tensor_tensor(out=st8[:, 8:16], in0=st8[:, 8:16], in1=self.neghalf[:, 0:8], op=ALU.pow),
                      reads=[bst8, self.b_const], writes=[bst8])
                kb.op("dve", lambda e: e.tensor_tensor(out=oc[:], in0=oc[:], in1=bc_last(st8[:, 8:16], 64), op=ALU.mult),
                      reads=[boc, bst8], writes=[boc])
                kb.op("dve", lambda e: e.tensor_tensor(out=oc[:].rearrange("p h d -> p (h d)"), in0=oc[:].rearrange("p h d -> p (h d)"),
                                                        in1=retg[:], op=ALU.mult), reads=[boc, bw], writes=[boc])
                kb.op("dve", lambda e: e.tensor_tensor(out=mixcat[:, 512:1024], in0=oc[:].rearrange("p h d -> p (h d)"), in1=sg[:],
                                                       op=ALU.mult), reads=[boc, bsg], writes=[bmixy])
                if _STOP <= 4:
                    return
                for k in range(8):
                    kb.op("pe", lambda e, k=k: e.transpose(out=tpB[0][:, k, :], in_=mixcat[:, k * 128:(k + 1) * 128], identity=self.ident_b[:]),
                          reads=[bmixa, bmixy, self.b_const], writes=[bs2[0]])
                kb.op("act", lambda e: e.copy(out=mixT[:], in_=tpB[0]), reads=[bs2[0]], writes=[bmixT])
                for half in range(2):
                    for k in range(8):
                        kb.op("pe", lambda e, k=k, half=half: e.matmul(oo[half][:], lhsT=mixT[:, k, :], rhs=w_out[:, k, half * 512:(half + 1) * 512],
                                                                     start=(k == 0), stop=(k == 7)), reads=[bmixT, bw], writes=[boo[half]])
                    hs = slice(half * 512, (half + 1) * 512)
                    kb.op("dve", lambda e, half=half, hs=hs: e.tensor_tensor(out=tmp[:, hs], in0=oo[half][:], in1=mods["gate1"][:, hs], op=ALU.mult),
                          reads=[boo[half], bmod], writes=[btmp])
                    kb.op("dve", lambda e, hs=hs: e.tensor_tensor(out=x1[:, hs], in0=tmp[:, hs], in1=X[:, hs], op=ALU.add),
                          reads=[btmp, bX], writes=[bx1])
                kb.dma("sp", S["XA"][ti * 128:(ti + 1) * 128, :], x1[:], reads=[bx1], writes=[self.db("XA", ti)])
                if _STOP <= 5:
                    return
                self.norm2_router(r, x1, bx1, mods["gmod2"], mods["shift2"], bmod, tmp, btmp, tpB, bs2, oo[0], boo[0], ti)
            kb.pipeline(stage_a, stage_b, self.ntl)
        kb.pop()

    def zero_xg(self, q="sp"):
        kb, S = self.kb, self.S
        z = kb.sb("zeros", [128, 4096], BF16); bz = Buf()
        kb.op("pool", lambda e: e.memset(z[:], 0.0), writes=[bz])
        nrows = 32 * self.cap
        for r0 in range(0, nrows, 512):
            kb.dma(q, S["XG"][r0:r0 + 512, :].rearrange("(p a) d -> p (a d)", p=128), z[:], reads=[bz])

    def phase2e(self, l):
        kb, I, S = self.kb, self.I, self.S
        kb.push()
        cap = self.cap
        nblk = cap // 512
        wgu = [kb.sb("wgu%d" % i, [128, 8, 2048], BF16) for i in range(2)]
        wdn = [kb.sb("wdn%d" % i, [128, 8, D], BF16) for i in range(2)]
        bgu = [kb.sb("bgu%d" % i, [128, 16], F32) for i in range(2)]
        bdn = [kb.sb("bdn%d" % i, [1, D], BF16) for i in range(2)]
        bwt = [Buf(), Buf()]
        ones1 = kb.sb("ones1", [1, 128], BF16); bones = Buf()
        kb.op("pool", lambda e: e.memset(ones1[:], 1.0), writes=[bones])
        xg = [kb.sb("xg%d" % i, [128, D], BF16) for i in range(12)]; bxg = [Buf() for _ in range(12)]
        xgT = [kb.sb("xgT%d" % i, [128, 8, 512], BF16) for i in range(2)]; bxgT = [Buf(), Buf()]
        glu = [kb.sb("glu%d" % i, [128, 512], F32) for i in range(2)]; bglu = [Buf(), Buf()]
        sig = [kb.sb("sig%d" % i, [128, 512], F32) for i in range(2)]; bsig = [Buf(), Buf()]
        lin = [kb.sb("lin%d" % i, [128, 512], F32) for i in range(2)]; blin = [Buf(), Buf()]
        actT = [kb.sb("actT%d" % i, [128, 8, 512], BF16) for i in range(2)]; bact = [Buf(), Buf()]
        yg = [kb.sb("yg%d" % i, [128, D], F32) for i in range(3)]; byg = [Buf() for _ in range(3)]
        tp = [kb.ps("tp%d" % i, [128, 8, 128], BF16) for i in range(2)]; btp = [Buf(), Buf()]
        pA = [kb.ps("pA%d" % i, [128, 512], F32) for i in range(2)]; bpA = [Buf(), Buf()]
        pB = [kb.ps("pB%d" % i, [128, 512], F32) for i in range(2)]; bpB = [Buf(), Buf()]
        pC = [kb.ps("pC%d" % i, [128, 512], F32) for i in range(2)]; bpC = [Buf(), Buf()]

        def load_expert(e, slot):
            self.load_w_bf16(wgu[slot], I["exp_w_gu"][l, e], 8, bwt[slot])
            self.load_w_bf16(wdn[slot], I["exp_w_down"][l, e], 8, bwt[slot])
            kb.dma("sp", bgu[slot][:], I["exp_b_gu_pj"][l, e], writes=[bwt[slot]])
            kb.op("dve", lambda en, slot=slot: en.tensor_scalar(out=bgu[slot][:, 8:16], in0=bgu[slot][:, 8:16], scalar1=1.0, scalar2=None,
                                                               op0=ALU.add), reads=[bwt[slot]], writes=[bwt[slot]])
            kb.dma("pool", bdn[slot][:], I["exp_b_down"][l, e:e + 1, :], writes=[bwt[slot]])

        load_expert(0, 0)
        blocks = [(ex, blk) for ex in range(32) for blk in range(nblk)]
        state = dict(xu=0, tu=0)

        def emit_loads(bi):
            ex, blk = blocks[bi]
            r0 = ex * cap + blk * 512
            tiles = []
            for st in range(4):
                i = state["xu"] % len(xg); state["xu"] += 1
                kb.dma("sp", xg[i][:], S["XG"][r0 + st * 128:r0 + (st + 1) * 128, :], writes=[bxg[i]])
                tiles.append(i)
            return tiles

        def emit_transposes(bi, tiles):
            XT = xgT[bi % 2]; bXT = bxgT[bi % 2]
            for st, i in enumerate(tiles):
                T = tp[state["tu"] % 2]; bT = btp[state["tu"] % 2]; state["tu"] += 1
                for k in range(8):
                    kb.op("pe", lambda e, k=k, i=i, T=T: e.transpose(out=T[:, k, :], in_=xg[i][:, k * 128:(k + 1) * 128],
                                                                   identity=self.ident_b[:]), reads=[bxg[i], self.b_const], writes=[bT])
                kb.op("act", lambda e, T=T, XT=XT, st=st: e.copy(out=XT[:, :, st * 128:(st + 1) * 128], in_=T[:]),
                      reads=[bT], writes=[bXT])

        pu = cu = yu = 0
        tl0 = emit_loads(0)
        tl1 = emit_loads(1) if len(blocks) > 1 else None
        emit_transposes(0, tl0)
        for bi, (ex, blk) in enumerate(blocks):
            slot = ex % 2
            if blk == 0 and ex + 1 < 32:
                load_expert(ex + 1, (ex + 1) % 2)
            XT = xgT[bi % 2]; bXT = bxgT[bi % 2]
            A = actT[bi % 2]; bA = bact[bi % 2]
            r0 = ex * cap + blk * 512
            for j in range(8):
                pa = pA[pu % 2]; bpa = bpA[pu % 2]; pb = pB[pu % 2]; bpb = bpB[pu % 2]
                gl = glu[pu % 2]; bgl = bglu[pu % 2]; sg_ = sig[pu % 2]; bsg_ = bsig[pu % 2]; ln = lin[pu % 2]; bln = blin[pu % 2]
                pu += 1
                for k in range(8):
                    kb.op("pe", lambda e, k=k, j=j, pa=pa, slot=slot, XT=XT: e.matmul(
                        pa[:], lhsT=wgu[slot][:, k, j * 128:(j + 1) * 128], rhs=XT[:, k, :],
                        start=(k == 0), stop=(k == 7)), reads=[bwt[slot], bXT], writes=[bpa])
                for k in range(8):
                    kb.op("pe", lambda e, k=k, j=j, pb=pb, slot=slot, XT=XT: e.matmul(
                        pb[:], lhsT=wgu[slot][:, k, 1024 + j * 128:1024 + (j + 1) * 128], rhs=XT[:, k, :],
                        start=(k == 0), stop=(k == 7)), reads=[bwt[slot], bXT], writes=[bpb])
                kb.op("dve", lambda e, pa=pa, gl=gl, j=j, slot=slot: e.tensor_scalar(
                    out=gl[:], in0=pa[:], scalar1=bgu[slot][:, j:j + 1], scalar2=7.0, op0=ALU.add, op1=ALU.min),
                    reads=[bpa, bwt[slot]], writes=[bgl])
                kb.op("act", lambda e, gl=gl, sg_=sg_: e.activation(out=sg_[:], in_=gl[:], func=AF.Sigmoid, scale=1.702),
                      reads=[bgl], writes=[bsg_])
                kb.op("dve", lambda e, pb=pb, ln=ln, j=j, slot=slot: e.tensor_scalar(
                    out=ln[:], in0=pb[:], scalar1=bgu[slot][:, 8 + j:9 + j], scalar2=8.0, op0=ALU.add, op1=ALU.min),
                    reads=[bpb, bwt[slot]], writes=[bln])
                kb.op("dve", lambda e, gl=gl, sg_=sg_: e.tensor_tensor(out=gl[:], in0=gl[:], in1=sg_[:], op=ALU.mult),
                      reads=[bgl, bsg_], writes=[bgl])
                kb.op("dve", lambda e, gl=gl, ln=ln, A=A, j=j: e.scalar_tensor_tensor(out=A[:, j, :], in0=ln[:], scalar=-6.0, in1=gl[:],
                                                                                 op0=ALU.max, op1=ALU.mult),
                      reads=[bgl, bln], writes=[bA])
            if bi + 1 < len(blocks):
                emit_transposes(bi + 1, tl1)
                tl0, tl1 = tl1, (emit_loads(bi + 2) if bi + 2 < len(blocks) else None)
            for st in range(4):
                Y = yg[yu % 3]; bY = byg[yu % 3]; yu += 1
                for half in range(2):
                    pc = pC[cu % 2]; bpc = bpC[cu % 2]; cu += 1
                    for k in range(8):
                        kb.op("pe", lambda e, k=k, st=st, half=half, pc=pc, A=A, slot=slot: e.matmul(
                            pc[:], lhsT=A[:, k, st * 128:(st + 1) * 128], rhs=wdn[slot][:, k, half * 512:(half + 1) * 512],
                            start=(k == 0), stop=False), reads=[bA, bwt[slot]], writes=[bpc])
                    kb.op("pe", lambda e, half=half, pc=pc, slot=slot: e.matmul(
                        pc[:], lhsT=ones1[:, :], rhs=bdn[slot][:, half * 512:(half + 1) * 512], start=False, stop=True),
                        reads=[bones, bwt[slot]], writes=[bpc])
                    if half == 0:
                        kb.op("act", lambda e, pc=pc, Y=Y: e.copy(out=Y[:, 0:512], in_=pc[:]), reads=[bpc], writes=[bY])
                    else:
                        kb.op("dve", lambda e, pc=pc, Y=Y: e.tensor_copy(out=Y[:, 512:1024], in_=pc[:]), reads=[bpc], writes=[bY])
                kb.dma("pool", S["YG"][r0 + st * 128:r0 + (st + 1) * 128, :], Y[:], reads=[bY])
        kb.pop()

    def phase2c(self, l, src, dst, dst_name, zero_after):
        kb, I, S, nseq = self.kb, self.I, self.S, self.nseq
        kb.push()
        cap = self.cap
        gate2 = kb.sb("gate2", [128, D], F32); bg2 = Buf()
        NB = 4
        xin = [kb.sb("xin%d" % i, [128, D], F32) for i in range(NB)]; bxin = [Buf() for _ in range(NB)]
        acc = [kb.sb("acc%d" % i, [128, D], F32) for i in range(NB)]; bacc = [Buf() for _ in range(NB)]
        yb = [kb.sb("yb%d" % i, [128, D], F32) for i in range(4 * NB)]; byb = [Buf() for _ in range(4 * NB)]
        sl = [kb.sb("sl%d" % i, [128, 4], I32) for i in range(NB)]; bsl = [Buf() for _ in range(NB)]
        gk = [kb.sb("gkc%d" % i, [128, 4], F32) for i in range(NB)]; bgk = [Buf() for _ in range(NB)]
        for i in range(4 * NB):
            kb.op("pool", lambda e, i=i: e.memset(yb[i][:], 0.0), writes=[byb[i]])
        if zero_after:
            self.zero_xg()
        u = 0
        for s in range(nseq):
            kb.dma("sp", gate2[:], S["MOD"][s, l, 5].partition_broadcast(128), reads=[self.db("MOD", (s, l, 5))], writes=[bg2])
            for t in range(self.ntl):
                ti = s * NT + t
                X = xin[u % NB]; bX = bxin[u % NB]; A = acc[u % NB]; bA = bacc[u % NB]
                SL = sl[u % NB]; bSL = bsl[u % NB]; GK = gk[u % NB]; bGK = bgk[u % NB]
                kb.dma("sp", X[:], src[ti * 128:(ti + 1) * 128, :], reads=[self.db("XA", ti)], writes=[bX])
                kb.dma("sp", SL[:], S["SLOT"][ti], reads=[self.db("SLOT", ti)], writes=[bSL])
                kb.dma("sp", GK[:], S["GK"][ti], reads=[self.db("GK", ti)], writes=[bGK])
                for k in range(4):
                    Yk = yb[(u % NB) * 4 + k]; bYk = byb[(u % NB) * 4 + k]
                    kb.idma(Yk[:], None, S["YG"], SL[:, k:k + 1], 32 * cap - 1, reads=[bSL], writes=[bYk])
                    if k == 0:
                        kb.op("dve", lambda e, Yk=Yk, A=A, GK=GK: e.tensor_scalar(out=A[:], in0=Yk[:], scalar1=GK[:, 0:1], scalar2=None,
                                                                                 op0=ALU.mult), reads=[bYk, bGK], writes=[bA])
                    else:
                        kb.op("dve", lambda e, Yk=Yk, A=A, GK=GK, k=k: e.scalar_tensor_tensor(
                            out=A[:], in0=Yk[:], scalar=GK[:, k:k + 1], in1=A[:], op0=ALU.mult, op1=ALU.add),
                            reads=[bYk, bGK, bA], writes=[bA])
                kb.op("dve", lambda e, A=A: e.tensor_tensor(out=A[:], in0=A[:], in1=gate2[:], op=ALU.mult), reads=[bA, bg2], writes=[bA])
                kb.op("dve", lambda e, A=A, X=X: e.tensor_tensor(out=X[:], in0=A[:], in1=X[:], op=ALU.add), reads=[bA, bX], writes=[bX])
                kb.dma("act", dst[ti * 128:(ti + 1) * 128, :], X[:], reads=[bX], writes=[self.db(dst_name, ti)])
                u += 1
        kb.pop()

    def phase3(self):
        kb, I, S, nseq = self.kb, self.I, self.S, self.nseq
        kb.push()
        bw = Buf()
        w_qkv = kb.sb("w_qkv", [128, 8, 1280], BF16)
        w_out = kb.sb("w_out", [128, 8, D], BF16)
        self.load_w_bf16(w_qkv, I["swa_w_qkv"], 8, bw)
        self.load_w_bf16(w_out, I["swa_w_out"], 8, bw)
        bqkv = kb.sb("bqkv", [128, 1280], F32)
        bout = kb.sb("bout", [128, D], F32)
        gq = kb.sb("gq", [128, 64], F32)
        gk = kb.sb("gk", [128, 64], F32)
        sk = kb.sb("sk", [128, 16], F32)
        self.bcast_load(bqkv[:], I["swa_b_qkv"], bw)
        self.bcast_load(bout[:], I["swa_b_out"], bw)
        self.bcast_load(gq[:], I["swa_q_head_g"], bw)
        self.bcast_load(gk[:], I["swa_k_head_g"], bw)
        self.bcast_load(sk[:], I["swa_sinks"], bw)
        kb.op("act", lambda e: e.activation(out=sk[:], in_=sk[:], func=AF.Exp), reads=[bw], writes=[bw])
        mask2 = kb.sb("mask2", [128, 4, 2, 128], BF16)
        for hh in range(4):
            kb.op("dve", lambda e, hh=hh: e.tensor_copy(out=mask2[:, hh, 0, :], in_=self.mask_gt[:]), reads=[self.b_const], writes=[bw])
            kb.op("dve", lambda e, hh=hh: e.tensor_copy(out=mask2[:, hh, 1, :], in_=self.mask_le[:]), reads=[self.b_const], writes=[bw])
        r = self.alloc_router(1)

        mods = {n: kb.sb(n, [128, D], F32) for n in ("gmod1", "shift1", "gate1", "gmod2", "shift2")}
        bmod = Buf()
        x_t = [kb.sb("x_t%d" % i, [128, D], F32) for i in range(2)]; bx = [Buf(), Buf()]
        tmp = kb.sb("tmp", [128, D], F32); btmp = Buf()
        h_bf = kb.sb("h_bf", [128, D], BF16); bh = Buf()
        hT = kb.sb("hT", [128, 8, 128], BF16); bhT = Buf()
        ss = kb.sb("ss", [128, 4], F32); bss = Buf()
        qkv = kb.sb("qkv", [128, 20, 64], F32); bqkvs = Buf()
        sq = kb.sb("sq", [128, 18, 64], F32); bsq = Buf()
        r18 = kb.sb("r18", [128, 18], F32); br18 = Buf()
        qn = kb.sb("qn", [128, 18, 64], F32); bqn = Buf()
        rt = [kb.sb("rt%d" % i, [128, 18, 32], F32) for i in range(4)]; brt = Buf()
        q_bf = kb.sb("q_bf", [128, 16, 64], BF16); bqb = Buf()
        kdup = kb.sb("kdup", [128, 2, 2, 64], BF16); bkd = Buf()
        qT_2 = [kb.sb("qT%d" % i, [128, 8, 128], BF16) for i in range(2)]; bqT_2 = [Buf(), Buf()]
        tmpB = kb.sb("tmpB", [128, D], F32); btmpB = Buf()
        kT = [kb.sb("kT%d" % i, [128, 2, 128], BF16) for i in range(3)]; bkT = [Buf() for _ in range(3)]
        Va = [kb.sb("Va%d" % i, [128, 2, 65], BF16) for i in range(3)]; bVa = [Buf() for _ in range(3)]
        bVones = Buf()
        for i in range(3):
            kb.op("pool", lambda e, i=i: e.memset(Va[i][:, :, 64:65], 1.0), writes=[bVones])
        PT = [kb.sb("PT%d" % i, [128, 4, 2, 128], BF16) for i in range(2)]; bPT = [Buf(), Buf()]
        den = kb.sb("den", [128, 16], F32); bden = Buf()
        attn = kb.sb("attn", [128, 16, 64], BF16); battn = Buf()
        attT = kb.sb("attT", [128, 8, 128], BF16); battT = Buf()
        x1 = kb.sb("x1", [128, D], F32); bx1 = Buf()

        tp = [kb.ps("tp%d" % i, [128, 8, 128], BF16) for i in range(2)]; btp = [Buf(), Buf()]
        mm = [kb.ps("mm%d" % i, [128, 512], F32) for i in range(2)]; bmm = [Buf(), Buf()]
        s2 = kb.ps("s2", [128, 2, 512], F32); bs2 = [Buf(), Buf()]
        oo = [kb.ps("oo%d" % i, [128, 512], F32) for i in range(2)]; boo = [Buf(), Buf()]
        tpB = [s2[:, i, :].bitcast(BF16).rearrange("p (k m) -> p k m", m=128) for i in range(2)]
        gc = {"n": 0}
        for s in range(nseq):
            cos, sin, brope = self.rope_tables(s, 32, "k_invf32", "c%d" % s)
            for n_, part in (("gmod1", 1), ("shift1", 0), ("gate1", 2), ("gmod2", 4), ("shift2", 3)):
                kb.dma("sp", mods[n_][:], S["MOD"][s, 1, part].partition_broadcast(128), reads=[self.db("MOD", (s, 1, part))], writes=[bmod])
            def stage_a(t, s=s, cos=cos, sin=sin, brope=brope):
                ti = s * NT + t
                cur, prv = t % 3, (t - 1) % 3
                X = x_t[t % 2]; bX = bx[t % 2]
                qT = qT_2[t % 2]; bqT = bqT_2[t % 2]
                kb.dma("sp", X[:], S["XB"][ti * 128:(ti + 1) * 128, :], reads=[self.db("XB", ti)], writes=[bX])
                self.norm_mod_T(X, bX, mods["gmod1"], mods["shift1"], bmod, tmp, btmp, h_bf, bh, tp[0], btp[0], hT, bhT, ss, bss)
                qkvf = qkv[:].rearrange("p h d -> p (h d)")
                for gi, (c0, c1) in enumerate(((0, 512), (512, 1024), (1024, 1280))):
                    p_ = mm[gi % 2]; bp_ = bmm[gi % 2]
                    for k in range(8):
                        kb.op("pe", lambda e, k=k, c0=c0, c1=c1, p_=p_: e.matmul(p_[:, 0:c1 - c0], lhsT=hT[:, k, :], rhs=w_qkv[:, k, c0:c1],
                                                                              start=(k == 0), stop=(k == 7)), reads=[bhT, bw], writes=[bp_])
                    kb.op("dve", lambda e, c0=c0, c1=c1, p_=p_: e.tensor_tensor(out=qkvf[:, c0:c1], in0=p_[:, 0:c1 - c0], in1=bqkv[:, c0:c1],
                                                                              op=ALU.add), reads=[bp_, bw], writes=[bqkvs])
                kb.op("dve", lambda e: e.tensor_tensor(out=sq[:], in0=qkv[:, 0:18, :], in1=qkv[:, 0:18, :], op=ALU.mult), reads=[bqkvs], writes=[bsq])
                kb.op("dve", lambda e: e.tensor_reduce(out=r18[:], in_=sq[:], axis=AX.X, op=ALU.add), reads=[bsq], writes=[br18])
                kb.op("dve", lambda e: e.tensor_scalar(out=r18[:], in0=r18[:], scalar1=1.0 / 64, scalar2=EPS, op0=ALU.mult, op1=ALU.add),
                      reads=[br18], writes=[br18])
                kb.op("pool", lambda e: e.tensor_tensor(out=r18[:, 0:16], in0=r18[:, 0:16], in1=self.neghalf[:, 0:16], op=ALU.pow),
                      reads=[br18, self.b_const], writes=[br18])
                kb.op("pool", lambda e: e.tensor_tensor(out=r18[:, 16:18], in0=r18[:, 16:18], in1=self.neghalf[:, 0:2], op=ALU.pow),
                      reads=[br18, self.b_const], writes=[br18])
                kb.op("dve", lambda e: e.tensor_tensor(out=qn[:], in0=qkv[:, 0:18, :], in1=bc_last(r18[:, :], 64), op=ALU.mult),
                      reads=[bqkvs, br18], writes=[bqn])
                kb.op("dve", lambda e: e.tensor_tensor(out=qn[:, 0:16, :], in0=qn[:, 0:16, :], in1=bc_mid(gq[:, :], 16), op=ALU.mult),
                      reads=[bqn, bw], writes=[bqn])
                kb.op("dve", lambda e: e.tensor_tensor(out=qn[:, 16:18, :], in0=qn[:, 16:18, :], in1=bc_mid(gk[:, :], 2), op=ALU.mult),
                      reads=[bqn, bw], writes=[bqn])
                cb = bc_mid(cos[:, t, :], 18)
                sb_ = bc_mid(sin[:, t, :], 18)
                x1_ = qn[:, :, 0:32]; x2_ = qn[:, :, 32:64]
                kb.op("dve", lambda e: e.tensor_tensor(out=rt[0][:], in0=x1_, in1=cb, op=ALU.mult), reads=[bqn, brope], writes=[brt])
                kb.op("dve", lambda e: e.tensor_tensor(out=rt[1][:], in0=x2_, in1=sb_, op=ALU.mult), reads=[bqn, brope], writes=[brt])
                kb.op("dve", lambda e: e.tensor_tensor(out=rt[2][:], in0=x2_, in1=cb, op=ALU.mult), reads=[bqn, brope], writes=[brt])
                kb.op("dve", lambda e: e.tensor_tensor(out=rt[3][:], in0=x1_, in1=sb_, op=ALU.mult), reads=[bqn, brope], writes=[brt])
                kb.op("dve", lambda e: e.tensor_tensor(out=q_bf[:, :, 0:32], in0=rt[0][:, 0:16, :], in1=rt[1][:, 0:16, :], op=ALU.subtract),
                      reads=[brt], writes=[bqb])
                kb.op("dve", lambda e: e.tensor_tensor(out=q_bf[:, :, 32:64], in0=rt[2][:, 0:16, :], in1=rt[3][:, 0:16, :], op=ALU.add),
                      reads=[brt], writes=[bqb])
                for dup in range(2):
                    kb.op("dve", lambda e, dup=dup: e.tensor_tensor(out=kdup[:, :, dup, 0:32], in0=rt[0][:, 16:18, :], in1=rt[1][:, 16:18, :],
                                                                   op=ALU.subtract), reads=[brt], writes=[bkd])
                    kb.op("dve", lambda e, dup=dup: e.tensor_tensor(out=kdup[:, :, dup, 32:64], in0=rt[2][:, 16:18, :], in1=rt[3][:, 16:18, :],
                                                                    op=ALU.add), reads=[brt], writes=[bkd])
                kb.op("act", lambda e, cur=cur: e.copy(out=Va[cur][:, :, 0:64], in_=qkv[:, 18:20, :]), reads=[bqkvs, bVones], writes=[bVa[cur]])
                for i in range(8):
                    kb.op("pe", lambda e, i=i: e.transpose(out=tp[1][:, i, :], in_=q_bf[:, 2 * i:2 * i + 2, :].rearrange("p h d -> p (h d)"),
                                                           identity=self.ident_b[:]), reads=[bqb, self.b_const], writes=[btp[1]])
                kb.op("act", lambda e: e.copy(out=qT[:], in_=tp[1][:]), reads=[btp[1]], writes=[bqT])
                for g in range(2):
                    kb.op("pe", lambda e, g=g: e.transpose(out=tp[0][:, g, :], in_=kdup[:, g, :, :].rearrange("p a d -> p (a d)"),
                                                           identity=self.ident_b[:]), reads=[bkd, self.b_const], writes=[btp[0]])
                kb.op("dve", lambda e, cur=cur: e.tensor_copy(out=kT[cur][:], in_=tp[0][:, 0:2, :]), reads=[btp[0]], writes=[bkT[cur]])
            def stage_b(t, s=s):
                ti = s * NT + t
                cur, prv = t % 3, (t - 1) % 3
                X = x_t[t % 2]; bX = bx[t % 2]
                qT = qT_2[t % 2]; bqT = bqT_2[t % 2]
                tmp = tmpB; btmp = btmpB
                for gq4 in range(4):
                    sbank = s2
                    bsb = bs2
                    P = PT[gc["n"] % 2]; bP = bPT[gc["n"] % 2]
                    ob = oo[gc["n"] % 2]; bob = boo[gc["n"] % 2]
                    gc["n"] += 1
                    for hh in range(4):
                        hq = gq4 * 4 + hh
                        i, o = hq // 2, (hq % 2) * 64
                        g = hq // 8
                        for w_, kt in ((0, prv), (1, cur)):
                            if t == 0 and w_ == 0:
                                continue
                            col = ((hh % 2) * 2 + hh // 2) * 256 + w_ * 128
                            kb.op("pe", lambda e, i=i, o=o, g=g, kt=kt, col=col, sbank=sbank: e.matmul(
                                sbank[:, col // 512, col % 512:col % 512 + 128], lhsT=kT[kt][o:o + 64, g, :], rhs=qT[o:o + 64, i, :],
                                start=True, stop=True), reads=[bkT[kt], bqT], writes=[bsb[col // 512]])
                    for bk in range(2):
                        if t == 0:
                            for hh2 in range(2):
                                kb.op("act", lambda e, bk=bk, hh2=hh2, P=P, sbank=sbank: e.activation(
                                    out=P[:, bk * 2 + hh2, 1, :], in_=sbank[:, bk, hh2 * 256 + 128:hh2 * 256 + 256], func=AF.Exp, scale=0.125),
                                    reads=[bsb[bk]], writes=[bP])
                        else:
                            kb.op("act", lambda e, bk=bk, P=P, sbank=sbank: e.activation(
                                out=P[:, bk * 2:bk * 2 + 2, :, :].rearrange("p a b c -> p (a b c)"), in_=sbank[:, bk, :], func=AF.Exp, scale=0.125),
                                reads=[bsb[bk]], writes=[bP])
                    if t == 0:
                        kb.op("dve", lambda e, P=P: e.tensor_tensor(out=P[:, :, 1, :], in0=P[:, :, 1, :], in1=mask2[:, :, 1, :], op=ALU.mult),
                              reads=[bP, bw], writes=[bP])
                    else:
                        kb.op("dve", lambda e, P=P: e.tensor_tensor(out=P[:].rearrange("p a b c -> p (a b c)"), in0=P[:].rearrange("p a b c -> p (a b c)"),
                                                                   in1=mask2[:].rearrange("p a b c -> p (a b c)"), op=ALU.mult),
                              reads=[bP, bw], writes=[bP])
                    for hh in range(4):
                        hq = gq4 * 4 + hh
                        g = hq // 8
                        sl = (hh % 2) * 2 + hh // 2
                        if t > 0:
                            kb.op("pe", lambda e, hh=hh, g=g, P=P, ob=ob, prv=prv, sl=sl: e.matmul(ob[:, hh * 65:hh * 65 + 65], lhsT=P[:, sl, 0, :],
                                                                                          rhs=Va[prv][:, g, :], start=True, stop=False),
                                  reads=[bP, bVa[prv]], writes=[bob])
                        kb.op("pe", lambda e, hh=hh, g=g, P=P, ob=ob, cur=cur, sl=sl: e.matmul(ob[:, hh * 65:hh * 65 + 65], lhsT=P[:, sl, 1, :],
                                                                                      rhs=Va[cur][:, g, :], start=(t == 0), stop=True),
                              reads=[bP, bVa[cur]], writes=[bob])
                    ov = ob[:, 0:260].rearrange("p (h d) -> p h d", d=65)
                    dsl = den[:, gq4 * 4:(gq4 + 1) * 4]
                    kb.op("dve", lambda e, ov=ov, dsl=dsl, gq4=gq4: e.tensor_tensor(out=dsl, in0=ov[:, :, 64], in1=sk[:, gq4 * 4:(gq4 + 1) * 4], op=ALU.add),
                          reads=[bob, bw], writes=[bden])
                    kb.op("dve", lambda e, dsl=dsl: e.reciprocal(out=dsl, in_=dsl), reads=[bden], writes=[bden])
                    kb.op("dve", lambda e, ov=ov, dsl=dsl, gq4=gq4: e.tensor_tensor(out=attn[:, gq4 * 4:(gq4 + 1) * 4, :], in0=ov[:, :, 0:64],
                                                                                 in1=bc_last(dsl, 64), op=ALU.mult), reads=[bob, bden], writes=[battn])
                af = attn[:].rearrange("p h d -> p (h d)")
                for k in range(8):
                    kb.op("pe", lambda e, k=k: e.transpose(out=tpB[0][:, k, :], in_=af[:, k * 128:(k + 1) * 128], identity=self.ident_b[:]),
                          reads=[battn, self.b_const], writes=[bs2[0]])
                kb.op("act", lambda e: e.copy(out=attT[:], in_=tpB[0]), reads=[bs2[0]], writes=[battT])
                for half in range(2):
                    hs = slice(half * 512, (half + 1) * 512)
                    for k in range(8):
                        kb.op("pe", lambda e, k=k, half=half, hs=hs: e.matmul(oo[half][:], lhsT=attT[:, k, :], rhs=w_out[:, k, hs],
                                                                            start=(k == 0), stop=(k == 7)), reads=[battT, bw], writes=[boo[half]])
                    kb.op("dve", lambda e, half=half, hs=hs: e.tensor_tensor(out=tmp[:, hs], in0=oo[half][:], in1=bout[:, hs], op=ALU.add),
                          reads=[boo[half], bw], writes=[btmp])
                    kb.op("dve", lambda e, hs=hs: e.tensor_tensor(out=tmp[:, hs], in0=tmp[:, hs], in1=mods["gate1"][:, hs], op=ALU.mult),
                          reads=[btmp, bmod], writes=[btmp])
                    kb.op("dve", lambda e, hs=hs: e.tensor_tensor(out=x1[:, hs], in0=tmp[:, hs], in1=X[:, hs], op=ALU.add),
                          reads=[btmp, bX], writes=[bx1])
                kb.dma("sp", S["XA"][ti * 128:(ti + 1) * 128, :], x1[:], reads=[bx1], writes=[self.db("XA", ti)])
                self.norm2_router(r, x1, bx1, mods["gmod2"], mods["shift2"], bmod, tmp, btmp, tpB, bs2, oo[0], boo[0], ti)
            kb.pipeline(stage_a, stage_b, self.ntl)
        kb.pop()

    def build(self):
        self.setup_consts()
        ph = self.phases
        if "p0" in ph:
            self.phase0()
        if "p1a" in ph:
            self.phase1a()
        if "p1b" in ph:
            self.phase1b()
        if "p2a" in ph:
            self.phase2e(0)
            self.phase2c(0, self.S["XA"], self.S["XB"], "XB", False)
        if "p3" in ph:
            self.phase3()
        if "p2b" in ph:
            self.phase2e(1)
            self.phase2c(1, self.S["XA"], self.out, "OUT", False)
        self.kb.finish()
        return self.nc


def module_consts():
    idx = np.arange(128, dtype=np.float64)
    lg = np.log1p(-np.exp2(-5.0 - np.arange(8, dtype=np.float64)))
    k = {}
    k["k_invf16"] = (10000.0 ** (-np.arange(16, dtype=np.float32) / 16)).astype(np.float32)
    k["k_invf32"] = (10000.0 ** (-np.arange(32, dtype=np.float32) / 32)).astype(np.float32)
    diff = idx[None, :] - idx[:, None]
    dec = np.where(diff[:, None, :] >= 0, np.exp(lg[None, :, None] * np.maximum(diff[:, None, :], 0.0)), 0.0)
    k["k_decayT"] = np.ascontiguousarray(dec.reshape(128, 4, 2, 128).transpose(0, 2, 1, 3)).reshape(128, 8 * 128).astype(np.float32)
    k["k_qdec"] = np.exp(lg[None, :] * (idx + 1.0)[:, None]).astype(np.float32)
    k["k_kdec"] = np.exp(lg[None, :] * (127.0 - idx)[:, None]).astype(np.float32)
    cd = np.zeros((128, 4), np.float64)
    for i in range(4):
        cd[0:64, i] = np.exp(lg[2 * i] * 128)
        cd[64:128, i] = np.exp(lg[2 * i + 1] * 128)
    k["k_cdec"] = cd.astype(np.float32)
    return k


def make_in_maps(inputs, nseq, n_cores):
    f = lambda a: np.ascontiguousarray(np.asarray(a))
    shared = {}
    for name in ("ada_w", "ada_b", "norm1_g", "norm2_g", "router_w", "router_b", "exp_w_gu", "exp_w_down", "exp_b_down"):
        shared[name] = f(inputs[name])
    for name in ("hyb_w_in", "mla_cq_norm_g", "mla_ckv_norm_g", "mla_w_uq", "mla_w_ukv", "mla_q_head_g", "mla_k_head_g",
                 "hyb_w_out", "swa_w_qkv", "swa_b_qkv", "swa_q_head_g", "swa_k_head_g", "swa_sinks", "swa_w_out", "swa_b_out"):
        shared[name] = f(np.asarray(inputs[name])[0])
    shared["ret_norm_g"] = f(np.asarray(inputs["ret_norm_g"])[0].reshape(512))
    bgu = np.asarray(inputs["exp_b_gu"])
    shared["exp_b_gu_pj"] = f(bgu.reshape(2, 32, 16, 128).transpose(0, 1, 3, 2))
    shared.update(module_consts())
    x = np.asarray(inputs["x"]); c = np.asarray(inputs["c"]); pos = np.asarray(inputs["positions"])
    maps = []
    for i in range(n_cores):
        b0 = i * nseq
        m = dict(shared)
        m["x"] = f(x[b0:b0 + nseq].reshape(nseq * SEQ, D))
        m["c_pk"] = f(c[b0:b0 + nseq].reshape(nseq, 8, 128).transpose(0, 2, 1))
        m["pos_pt"] = f(pos[b0:b0 + nseq].reshape(nseq, NT, 128).transpose(0, 2, 1).astype(np.int32))
        maps.append(m)
    return maps


_PROG = {}


def kernel(**inputs):
    nseq = 32 // N_CORES
    if "nc" not in _PROG:
        _PROG["nc"] = Prog(nseq).build()
    maps = make_in_maps(inputs, nseq, N_CORES)
    res = run_bass_kernel_spmd(_PROG["nc"], maps, core_ids=list(range(N_CORES)))
    out = np.concatenate([np.asarray(r["out"]).reshape(nseq, SEQ, D) for r in res.results], axis=0)
    return out.astype(np.float32)
```
